# Optimizing a Trainium2 kernel written in Bass

```python
import functools
import jax, jax.numpy as jnp
from jax import lax
import numpy as np

D_MODEL = 1024
BATCH = 32
SEQ = 2048
DEPTH = 1
DEC_BATCH = 128
DEC_SEQ = 1
PAST_LEN = 8192
PAGE_SIZE = 128

N_HEADS = 8
N_KV_HEADS = 2
HEAD_DIM = 64
ATTN_WIDTH = N_HEADS * HEAD_DIM
KV_WIDTH = N_KV_HEADS * HEAD_DIM
IDX_HEADS = 4
IDX_DIM = 64
TOPK_MAX = 256
Q_BLOCK = 128
GDN_HEADS = 4
GDN_DK = 128
GDN_DV = 128
GDN_KW = GDN_HEADS * GDN_DK
GDN_VW = GDN_HEADS * GDN_DV
CONV_W = 4
CONV_DIM = 2 * GDN_KW + GDN_VW
GDN_CHUNK = 64
MIX_WIDTH = ATTN_WIDTH + GDN_VW
MEM_LEN = 256
MEM_HEADS = 4
MEM_HEAD_DIM = 64
MEM_WIDTH = MEM_HEADS * MEM_HEAD_DIM
D_FF = -(-8 * D_MODEL // (3 * 256)) * 256
IN_SPLITS = (ATTN_WIDTH, KV_WIDTH, KV_WIDTH, IDX_HEADS * IDX_DIM, IDX_DIM, IDX_HEADS,
             GDN_KW, GDN_KW, GDN_VW, GDN_VW, GDN_HEADS, GDN_HEADS)
SPLIT_POINTS = tuple(int(s) for s in np.cumsum(IN_SPLITS)[:-1])
IN_WIDTH = int(sum(IN_SPLITS))

kernel_name = 'hybrid_dsa_gdn_decoder_step'

F32 = jnp.float32


def rmsnorm(x, g, eps=1e-6):
    xf = x.astype(F32)
    y = xf * lax.rsqrt(jnp.mean(xf * xf, axis=-1, keepdims=True) + eps)
    return (y * g.astype(F32)).astype(x.dtype)


def l2norm(x, eps=1e-6):
    xf = x.astype(F32)
    return xf * lax.rsqrt(jnp.sum(xf * xf, axis=-1, keepdims=True) + eps)


def gather_rows(rows, idx):
    return jax.vmap(lambda r, i: r[i])(rows, idx)


def causal_conv(u, buf, conv_w):
    t = u.shape[1]
    full = jnp.concatenate([buf.astype(u.dtype), u], axis=1)
    y = full[:, 0:t] * conv_w[0]
    for j in range(1, CONV_W):
        y = y + full[:, j:j + t] * conv_w[j]
    return jax.nn.silu(y), full[:, t:]


def index_select(qi, wi, kidx, qpos, topk):
    n_keys = kidx.shape[1]
    rel = jax.nn.relu(jnp.einsum('bthd,bsd->bths', qi.astype(F32), kidx.astype(F32)) * IDX_DIM ** -0.5)
    score = jnp.einsum('bth,bths->bts', wi.astype(F32), rel)
    admissible = jnp.arange(n_keys)[None, :] <= qpos[:, None]
    score = jnp.where(admissible[None], score, -jnp.inf)
    _, idx = lax.top_k(score, topk)
    valid = idx <= qpos[None, :, None]
    return idx, valid


def sparse_attend(q, k_sel, v_sel, valid):
    b, t = q.shape[:2]
    qg = q.reshape(b, t, N_KV_HEADS, N_HEADS // N_KV_HEADS, HEAD_DIM)
    logits = jnp.einsum('btgrd,btsgd->btgrs', qg.astype(F32), k_sel.astype(F32)) * HEAD_DIM ** -0.5
    logits = jnp.where(valid[:, :, None, None, :], logits, -jnp.inf)
    p = jax.nn.softmax(logits, axis=-1)
    o = jnp.einsum('btgrs,btsgd->btgrd', p, v_sel.astype(F32))
    return o.reshape(b, t, ATTN_WIDTH).astype(q.dtype)


def dsa_prompt(q, k, v, qi, wi, ki):
    b, t = q.shape[:2]
    topk = min(TOPK_MAX, t // 4)
    nblk = t // Q_BLOCK

    def blockify(a):
        return jnp.moveaxis(a.reshape((b, nblk, Q_BLOCK) + a.shape[2:]), 1, 0)

    def one_block(args):
        blk, qb, qib, wib = args
        qpos = blk * Q_BLOCK + jnp.arange(Q_BLOCK)
        idx, valid = index_select(qib, wib, ki, qpos, topk)
        return sparse_attend(qb, gather_rows(k, idx), gather_rows(v, idx), valid)

    out = lax.map(one_block, (jnp.arange(nblk), blockify(q), blockify(qi), blockify(wi)))
    return jnp.moveaxis(out, 0, 1).reshape(b, t, ATTN_WIDTH)


def dsa_sample(q, k, v, qi, wi, ki, cache_k, cache_v, cache_kidx, page_table):
    b, t = q.shape[:2]
    past = page_table.shape[1] * PAGE_SIZE
    topk = min(TOPK_MAX, (past + t) // 4)
    kidx_past = cache_kidx[page_table].reshape(b, past, IDX_DIM)
    kidx_all = jnp.concatenate([kidx_past, ki.astype(kidx_past.dtype)], axis=1)
    qpos = past + jnp.arange(t)
    idx, valid = index_select(qi, wi, kidx_all, qpos, topk)
    in_past = (idx < past)[..., None, None]
    pidx = jnp.minimum(idx, past - 1)
    phys = gather_rows(page_table, pidx // PAGE_SIZE)
    off = pidx % PAGE_SIZE
    nidx = jnp.clip(idx - past, 0, t - 1)
    k_sel = jnp.where(in_past, cache_k[phys, off], gather_rows(k, nidx).astype(cache_k.dtype))
    v_sel = jnp.where(in_past, cache_v[phys, off], gather_rows(v, nidx).astype(cache_v.dtype))
    return sparse_attend(q, k_sel, v_sel, valid)


def gated_delta_chunked(q, k, v, g, beta, s0):
    b, t, h, dk = q.shape
    c = min(GDN_CHUNK, t)
    n = -(-t // c)
    pad = n * c - t

    def prep(a):
        a = jnp.pad(a, [(0, 0), (0, pad)] + [(0, 0)] * (a.ndim - 2))
        return jnp.moveaxis(a.reshape((b, n, c) + a.shape[2:]), 3, 1)

    q, k, v, g, beta = (prep(a) for a in (q * dk ** -0.5, k, v, g, beta))
    gc = jnp.cumsum(g, axis=-1)
    tril = jnp.tril(jnp.ones((c, c), bool))
    strict = jnp.tril(jnp.ones((c, c), bool), -1)
    diff = gc[..., :, None] - gc[..., None, :]
    decay = jnp.where(tril, jnp.exp(jnp.where(tril, diff, 0.0)), 0.0)
    kb = k * beta[..., None]
    vb = v * beta[..., None]
    a_mat = jnp.where(strict, jnp.einsum('bhncd,bhnsd->bhncs', kb, k) * decay, 0.0)
    eye = jnp.eye(c, dtype=F32)
    tinv = lax.linalg.triangular_solve(eye + a_mat, jnp.broadcast_to(eye, a_mat.shape),
                                       left_side=True, lower=True, unit_diagonal=True)
    u = jnp.einsum('bhncs,bhnsd->bhncd', tinv, vb)
    w = jnp.einsum('bhncs,bhnsd->bhncd', tinv, kb * jnp.exp(gc)[..., None])
    qk = jnp.where(tril, jnp.einsum('bhncd,bhnsd->bhncs', q, k) * decay, 0.0)

    def step(s, xs):
        q_i, k_i, u_i, w_i, qk_i, gc_i = xs
        v_new = u_i - jnp.einsum('bhcd,bhde->bhce', w_i, s)
        o = (jnp.einsum('bhcd,bhde->bhce', q_i * jnp.exp(gc_i)[..., None], s)
             + jnp.einsum('bhcs,bhse->bhce', qk_i, v_new))
        g_last = gc_i[..., -1]
        s = (s * jnp.exp(g_last)[..., None, None]
             + jnp.einsum('bhcd,bhce->bhde', k_i * jnp.exp(g_last[..., None] - gc_i)[..., None], v_new))
        return s, o

    xs = tuple(jnp.moveaxis(a, 2, 0) for a in (q, k, u, w, qk, gc))
    s_fin, o = lax.scan(step, s0, xs)
    o = jnp.transpose(o, (1, 0, 3, 2, 4)).reshape(b, n * c, h, o.shape[-1])[:, :t]
    return o, s_fin


def mem_kv(mem, mem_norm_g, w_mk, w_mv, mk_norm_g):
    b, m, _ = mem.shape
    hm = rmsnorm(mem, mem_norm_g)
    mk = rmsnorm((hm @ w_mk).reshape(b, m, MEM_HEADS, MEM_HEAD_DIM), mk_norm_g)
    mv = (hm @ w_mv).reshape(b, m, MEM_HEADS, MEM_HEAD_DIM)
    return mk, mv


def mem_attend(h, mem_k, mem_v, w_mq, mq_norm_g, w_mo):
    b, t, _ = h.shape
    q = rmsnorm((h @ w_mq).reshape(b, t, MEM_HEADS, MEM_HEAD_DIM), mq_norm_g)
    logits = jnp.einsum('bthd,bmhd->bhtm', q.astype(F32), mem_k.astype(F32)) * MEM_HEAD_DIM ** -0.5
    p = jax.nn.softmax(logits, axis=-1)
    o = jnp.einsum('bhtm,bmhd->bthd', p, mem_v.astype(F32))
    return o.reshape(b, t, MEM_WIDTH).astype(h.dtype) @ w_mo


def trunk_layer(x, mem_k, mem_v, conv_buf, ssm0, dsa_fn,
                attn_norm_g, w_in, q_norm_g, k_norm_g, conv_w, a_log, dt_bias, gdn_norm_g, w_out,
                xattn_norm_g, w_mq, mq_norm_g, w_mo, ffn_norm_g, w_gate, w_up, w_down):
    b, t, _ = x.shape
    h = rmsnorm(x, attn_norm_g)
    (q, k, v, qi, ki, wi, gq, gk, gv, gz, ga, gb) = jnp.split(h @ w_in, SPLIT_POINTS, axis=-1)
    q = rmsnorm(q.reshape(b, t, N_HEADS, HEAD_DIM), q_norm_g)
    k = rmsnorm(k.reshape(b, t, N_KV_HEADS, HEAD_DIM), k_norm_g)
    v = v.reshape(b, t, N_KV_HEADS, HEAD_DIM)
    qi = qi.reshape(b, t, IDX_HEADS, IDX_DIM)
    wi = wi * IDX_HEADS ** -0.5
    o_attn = dsa_fn(q, k, v, qi, wi, ki)
    conv_out, conv_new = causal_conv(jnp.concatenate([gq, gk, gv], axis=-1), conv_buf, conv_w)
    cq, ck, cv = jnp.split(conv_out, [GDN_KW, 2 * GDN_KW], axis=-1)
    cq = l2norm(cq.reshape(b, t, GDN_HEADS, GDN_DK))
    ck = l2norm(ck.reshape(b, t, GDN_HEADS, GDN_DK))
    cv = cv.reshape(b, t, GDN_HEADS, GDN_DV).astype(F32)
    beta = jax.nn.sigmoid(gb.astype(F32))
    g = -jnp.exp(a_log.astype(F32)) * jax.nn.softplus(ga.astype(F32) + dt_bias.astype(F32))
    o_gdn, ssm_new = gated_delta_chunked(cq, ck, cv, g, beta, ssm0.astype(F32))
    o_gdn = rmsnorm(o_gdn, gdn_norm_g) * jax.nn.silu(gz.reshape(b, t, GDN_HEADS, GDN_DV).astype(F32))
    o_gdn = o_gdn.reshape(b, t, GDN_VW).astype(x.dtype)
    x = x + jnp.concatenate([o_attn, o_gdn], axis=-1) @ w_out
    x = x + mem_attend(rmsnorm(x, xattn_norm_g), mem_k, mem_v, w_mq, mq_norm_g, w_mo)
    hf = rmsnorm(x, ffn_norm_g)
    x = x + (jax.nn.silu(hf @ w_gate) * (hf @ w_up)) @ w_down
    return x, (k, v, ki, conv_new, ssm_new.astype(ssm0.dtype))


def setup_inputs(seed: int = 0) -> dict:
    key = jax.random.key(seed)
    keys = iter(jax.random.split(key, 48))

    def nrm(shape, scale=1.0):
        return scale * jax.random.normal(next(keys), shape, jnp.float32)

    def gain(n):
        return 1.0 + nrm((DEPTH, n), 0.02)

    n_pages = PAST_LEN // PAGE_SIZE
    n_pool = (DEC_BATCH * n_pages * 5) // 4
    perm = jax.random.permutation(next(keys), n_pool)
    page_table = perm[:DEC_BATCH * n_pages].reshape(DEC_BATCH, n_pages).astype(jnp.int32)
    a_log = jnp.log(jax.random.uniform(next(keys), (DEPTH, GDN_HEADS), jnp.float32, 1.0, 16.0))
    return {
        'x_prompt': nrm((BATCH, SEQ, D_MODEL)),
        'x_sample': nrm((DEC_BATCH, DEC_SEQ, D_MODEL)),
        'mem_prompt': nrm((BATCH, MEM_LEN, D_MODEL)),
        'cache_k': nrm((DEPTH, n_pool, PAGE_SIZE, N_KV_HEADS, HEAD_DIM)),
        'cache_v': nrm((DEPTH, n_pool, PAGE_SIZE, N_KV_HEADS, HEAD_DIM)),
        'cache_kidx': nrm((DEPTH, n_pool, PAGE_SIZE, IDX_DIM)),
        'page_table': page_table,
        'state_conv': nrm((DEPTH, DEC_BATCH, CONV_W - 1, CONV_DIM)),
        'state_ssm': nrm((DEPTH, DEC_BATCH, GDN_HEADS, GDN_DK, GDN_DV), 0.1),
        'cache_mem_k': nrm((DEPTH, DEC_BATCH, MEM_LEN, MEM_HEADS, MEM_HEAD_DIM)),
        'cache_mem_v': nrm((DEPTH, DEC_BATCH, MEM_LEN, MEM_HEADS, MEM_HEAD_DIM)),
        'attn_norm_g': gain(D_MODEL),
        'w_in': nrm((DEPTH, D_MODEL, IN_WIDTH), D_MODEL ** -0.5),
        'q_norm_g': gain(HEAD_DIM),
        'k_norm_g': gain(HEAD_DIM),
        'conv_w': nrm((DEPTH, CONV_W, CONV_DIM), CONV_W ** -0.5),
        'a_log': a_log,
        'dt_bias': nrm((DEPTH, GDN_HEADS), 0.1) - 4.0,
        'gdn_norm_g': gain(GDN_DV),
        'w_out': nrm((DEPTH, MIX_WIDTH, D_MODEL), MIX_WIDTH ** -0.5),
        'xattn_norm_g': gain(D_MODEL),
        'mem_norm_g': gain(D_MODEL),
        'w_mq': nrm((DEPTH, D_MODEL, MEM_WIDTH), D_MODEL ** -0.5),
        'w_mk': nrm((DEPTH, D_MODEL, MEM_WIDTH), D_MODEL ** -0.5),
        'w_mv': nrm((DEPTH, D_MODEL, MEM_WIDTH), D_MODEL ** -0.5),
        'mq_norm_g': gain(MEM_HEAD_DIM),
        'mk_norm_g': gain(MEM_HEAD_DIM),
        'w_mo': nrm((DEPTH, MEM_WIDTH, D_MODEL), MEM_WIDTH ** -0.5),
        'ffn_norm_g': gain(D_MODEL),
        'w_gate': nrm((DEPTH, D_MODEL, D_FF), D_MODEL ** -0.5),
        'w_up': nrm((DEPTH, D_MODEL, D_FF), D_MODEL ** -0.5),
        'w_down': nrm((DEPTH, D_FF, D_MODEL), D_FF ** -0.5),
    }


def reference(x_prompt, x_sample, mem_prompt, cache_k, cache_v, cache_kidx, page_table,
              state_conv, state_ssm, cache_mem_k, cache_mem_v,
              attn_norm_g, w_in, q_norm_g, k_norm_g, conv_w, a_log, dt_bias, gdn_norm_g, w_out,
              xattn_norm_g, mem_norm_g, w_mq, w_mk, w_mv, mq_norm_g, mk_norm_g, w_mo,
              ffn_norm_g, w_gate, w_up, w_down):
    layer_w = (attn_norm_g, w_in, q_norm_g, k_norm_g, conv_w, a_log, dt_bias, gdn_norm_g, w_out,
               xattn_norm_g, w_mq, mq_norm_g, w_mo, ffn_norm_g, w_gate, w_up, w_down)
    xp, xs = x_prompt, x_sample
    new_p, new_s = [], []
    for l in range(DEPTH):
        wl = [a[l] for a in layer_w]
        mk_p, mv_p = mem_kv(mem_prompt, mem_norm_g[l], w_mk[l], w_mv[l], mk_norm_g[l])
        conv0 = jnp.zeros((xp.shape[0], CONV_W - 1, CONV_DIM), xp.dtype)
        ssm0 = jnp.zeros((xp.shape[0], GDN_HEADS, GDN_DK, GDN_DV), xp.dtype)
        xp, st_p = trunk_layer(xp, mk_p, mv_p, conv0, ssm0, dsa_prompt, *wl)
        dsa_s = functools.partial(dsa_sample, cache_k=cache_k[l], cache_v=cache_v[l],
                                  cache_kidx=cache_kidx[l], page_table=page_table)
        xs, st_s = trunk_layer(xs, cache_mem_k[l], cache_mem_v[l], state_conv[l], state_ssm[l], dsa_s, *wl)
        new_p.append(st_p + (mk_p, mv_p))
        new_s.append(st_s)
    k_prompt, v_prompt, kidx_prompt, conv_prompt, ssm_prompt, memk_prompt, memv_prompt = [
        jnp.stack(z) for z in zip(*new_p)]
    k_sample, v_sample, kidx_sample, conv_sample, ssm_sample = [jnp.stack(z) for z in zip(*new_s)]
    return (xp, xs, k_prompt, v_prompt, kidx_prompt, conv_prompt, ssm_prompt, memk_prompt, memv_prompt,
            k_sample, v_sample, kidx_sample, conv_sample, ssm_sample)
```

```python
import os
import numpy as np
import concourse.bass as bass
import concourse.mybir as mybir
from concourse.bass_utils import run_bass_kernel_spmd

F32 = mybir.dt.float32
BF16 = mybir.dt.bfloat16
I32 = mybir.dt.int32
AF = mybir.ActivationFunctionType
ALU = mybir.AluOpType
AX = mybir.AxisListType

NCORES = 8
D = 1024
SEQ = 2048
NT = SEQ // 128
MEM = 256
INW = 3148
EPS = 1e-6
NDS = 32


class Tok:
    __slots__ = ("w", "r")

    def __init__(self):
        self.w = None
        self.r = {}


class Prog:
    def __init__(self, nc):
        self.nc = nc
        self.names = ["pe", "act", "dve", "pool", "sp"]
        self.sems = []
        self.esem = {}
        for k in self.names:
            self.esem[k] = len(self.sems)
            self.sems.append(nc.alloc_semaphore("es_" + k))
        self.dsem = []
        for i in range(NDS):
            self.dsem.append(len(self.sems))
            self.sems.append(nc.alloc_semaphore("ds_%d" % i))
        self.dval = [0] * NDS
        self.dnext = 0
        self.dnext_sw = 0
        self.cnt = {k: 0 for k in self.names}
        self.seen = {k: {} for k in self.names}
        self.th = {k: [] for k in self.names}

    def _deps(self, e, reads, writes, extra=()):
        d = {}

        def add(s, v):
            if d.get(s, 0) < v:
                d[s] = v

        for t in reads:
            if t.w is not None:
                add(*t.w)
        for t in writes:
            if t.w is not None:
                add(*t.w)
            for s, v in t.r.items():
                add(s, v)
        for s, v in extra:
            add(s, v)
        out = []
        for s, v in d.items():
            if e == "pe" and s == self.esem["pe"]:
                continue
            if self.seen[e].get(s, 0) >= v:
                continue
            self.seen[e][s] = v
            out.append((s, v))
        return out

    def op(self, e, fn, reads=(), writes=()):
        waits = self._deps(e, reads, writes)
        self.cnt[e] += 1
        n = self.cnt[e]
        s = self.esem[e]
        self.th[e].append((waits, fn, s, 1))
        for t in reads:
            if t.r.get(s, 0) < n:
                t.r[s] = n
        for t in writes:
            t.w = (s, n)
            t.r = {}

    def dma(self, q, out, in_, reads=(), writes=(), **kw):
        if q == "pool":
            i = NDS - 8 + self.dnext_sw
            self.dnext_sw = (self.dnext_sw + 1) % 8
        else:
            i = self.dnext
            self.dnext = (self.dnext + 1) % (NDS - 8)
        s = self.dsem[i]
        extra = [(s, self.dval[i])] if self.dval[i] else []
        waits = self._deps(q, reads, writes, extra)
        self.dval[i] += 16
        v = self.dval[i]
        if "indirect" in kw:
            ioff = kw.pop("indirect")
            self.th[q].append((waits, lambda eng: eng.indirect_dma_start(out=out, out_offset=None, in_=in_,
                                                                         in_offset=ioff), s, 16))
        else:
            self.th[q].append((waits, lambda eng: eng.dma_start(out=out, in_=in_, **kw), s, 16))
        for t in reads:
            t.r[s] = v
        for t in writes:
            t.w = (s, v)
            t.r = {}

    def barrier(self):
        tgt = []
        for i in range(NDS):
            if self.dval[i]:
                tgt.append((self.dsem[i], self.dval[i]))
        for k in self.names:
            if self.cnt[k]:
                tgt.append((self.esem[k], self.cnt[k]))
        for e in self.names:
            waits = []
            for s_, v in tgt:
                if s_ == self.esem[e] and e in ("pe", "sp"):
                    continue
                if self.seen[e].get(s_, 0) >= v:
                    continue
                self.seen[e][s_] = v
                waits.append((s_, v))
            self.th[e].append((waits, None, None, 0))

    def finish(self):
        waits = []
        for i in range(NDS):
            if self.dval[i]:
                waits.append((self.dsem[i], self.dval[i]))
        for k in self.names:
            if k != "sp" and self.cnt[k]:
                waits.append((self.esem[k], self.cnt[k]))
        self.th["sp"].append((waits, None, None, 0))

    def emit(self):
        nc = self.nc
        sems = self.sems

        def run(eng, lst):
            for waits, fn, s, inc in lst:
                for ws, wv in waits:
                    eng.wait_ge(sems[ws], wv)
                if fn is not None:
                    fn(eng).then_inc(sems[s], inc)

        with nc.Block() as block:
            @block.tensor
            def _(e):
                run(e, self.th["pe"])

            @block.scalar
            def _(e):
                run(e, self.th["act"])

            @block.vector
            def _(e):
                run(e, self.th["dve"])

            @block.gpsimd
            def _(e):
                run(e, self.th["pool"])

            @block.sync
            def _(e):
                run(e, self.th["sp"])


class K:
    pass


def build(NSEQ, NS=16, STOP=99, NPOOL=10240):
    GCUT = int(os.environ.get('GCUT', '99'))
    DCUT = int(os.environ.get('DCUT', '99'))
    PCUT = int(os.environ.get('PCUT', '99'))
    CCUT = int(os.environ.get('CCUT', '99'))
    nc = bass.Bass("TRN2", target_bir_lowering=False)
    P = Prog(nc)
    k = K()

    def din(name, shape, dt=F32):
        return nc.dram_tensor(name, list(shape), dt, kind="ExternalInput").ap()

    def dout(name, shape, dt=F32):
        return nc.dram_tensor(name, list(shape), dt, kind="ExternalOutput").ap()

    ARENA_W = 53000
    arena = nc.alloc_sbuf_tensor("arena", [128, ARENA_W], F32).ap()
    k.top = 0

    def sb(name, shape, dt=F32):
        n = 1
        for d_ in shape[1:]:
            n *= d_
        words = n if dt in (F32, I32, mybir.dt.uint32) else (n + 1) // 2
        words = (words + 7) // 8 * 8
        off = k.top
        k.top += words
        assert k.top <= ARENA_W, ("SBUF arena overflow", name, k.top)
        a = arena[:, off:off + words]
        if dt != F32:
            a = a.bitcast(dt)
        a = a[:, 0:n]
        if len(shape) > 2:
            names = " ".join("d%d" % i for i in range(len(shape) - 1))
            kw = {"d%d" % i: shape[i + 1] for i in range(len(shape) - 2)}
            a = a.rearrange("p (%s) -> p %s" % (names, names), **kw)
        if shape[0] != 128:
            a = a[0:shape[0]]
        return a

    def sbt(name, shape, dt=F32):
        return sb(name, shape, dt), Tok()

    xp = din("xp", [NSEQ * SEQ, D])
    memp = din("memp", [NSEQ * MEM, D])
    xs = din("xs", [NS, D])
    st_conv = din("st_conv", [NS * 3, 1536])
    ssm_in = din("ssm_in", [NS * 512, 128])
    eye16_d = din("eye16", [256])
    selpair_d = din("selpair", [8, NS, 128])
    selone_d = din("selone", [NS, NS, 128])
    iota64_d = din("iota64", [64])
    ptab = din("ptab", [NS, 64], I32)
    cache_kidx_d = din("cache_kidx", [NPOOL, 8192])
    cache_k_d = din("cache_k", [NPOOL * 128, 128])
    cache_v_d = din("cache_v", [NPOOL * 128, 128])
    cmk_d = din("cmk", [NS * 256, 256])
    cmv_d = din("cmv", [NS * 256, 256])
    ident_d = din("ident", [128, 128])
    trile_d = din("trile", [128, 128])
    sgt_d = din("sgt", [128, 128])
    pow2_d = din("pow2", [32])
    g_attn = din("attn_norm_g", [D])
    w_in = din("w_in", [D, INW])
    g_q = din("q_norm_g", [64])
    g_k = din("k_norm_g", [64])
    conv_w = din("conv_w", [4, 1536])
    a_log = din("a_log", [4])
    dt_bias = din("dt_bias", [4])
    g_gdn = din("gdn_norm_g", [128])
    w_out = din("w_out", [D, D])
    g_x = din("xattn_norm_g", [D])
    g_mem = din("mem_norm_g", [D])
    w_mq = din("w_mq", [D, 256])
    w_mk = din("w_mk", [D, 256])
    w_mv = din("w_mv", [D, 256])
    g_mq = din("mq_norm_g", [64])
    g_mk = din("mk_norm_g", [64])
    w_mo = din("w_mo", [256, D])
    g_f = din("ffn_norm_g", [D])
    w_gate = din("w_gate", [D, 2816])
    w_up = din("w_up", [D, 2816])
    w_down = din("w_down", [2816, D])

    y_p = dout("y_p", [NSEQ * SEQ, D])
    y_s = dout("y_s", [NS, D])
    k_p = dout("k_p", [NSEQ * SEQ, 128])
    v_p = dout("v_p", [NSEQ * SEQ, 128])
    kidx_p = dout("kidx_p", [NSEQ * SEQ, 64])
    conv_p = dout("conv_p", [NSEQ * 3, 1536])
    ssm_p = dout("ssm_p", [NSEQ * 512, 128])
    memk_p = dout("memk_p", [NSEQ * MEM, 256])
    memv_p = dout("memv_p", [NSEQ * MEM, 256])
    k_s = dout("k_s", [NS, 128])
    v_s = dout("v_s", [NS, 128])
    kidx_s = dout("kidx_s", [NS, 64])
    conv_s = dout("conv_s", [NS * 3, 1536])
    ssm_s = dout("ssm_s", [NS * 512, 128])

    identf, t_identf = sbt("identf", [128, 128])
    identb, t_identb = sbt("identb", [128, 128], BF16)
    trile, t_trile = sbt("trile", [128, 128])
    sgt, t_sgt = sbt("sgt", [128, 128])
    onesf, t_onesf = sbt("onesf", [128, 128])
    onesb, t_onesb = sbt("onesb", [128, 128], BF16)
    tribias, t_tribias = sbt("tribias", [128, 128])
    lowinc, t_lowinc = sbt("lowinc", [128, 128])
    P.dma("sp", identf, ident_d, writes=[t_identf])
    P.dma("sp", trile, trile_d, writes=[t_trile])
    P.dma("sp", sgt, sgt_d, writes=[t_sgt])
    P.op("dve", lambda e: e.tensor_copy(out=identb, in_=identf), [t_identf], [t_identb])
    P.op("pool", lambda e: e.memset(onesf, 1.0), [], [t_onesf])
    P.op("pool", lambda e: e.memset(onesb, 1.0), [], [t_onesb])
    P.op("dve", lambda e: e.tensor_tensor(out=lowinc, in0=sgt, in1=identf, op=ALU.add),
         [t_sgt, t_identf], [t_lowinc])
    P.op("dve", lambda e: e.tensor_scalar(out=tribias, in0=lowinc, scalar1=-1.0, scalar2=1e30,
                                          op0=ALU.add, op1=ALU.mult), [t_lowinc], [t_tribias])

    def col_layout(name, src, n):
        t, tok = sbt(name, [128, n])
        P.dma("sp", t, src.rearrange("(c p) -> p c", p=128), writes=[tok],
              allow_slow_non_contiguous=True)
        return t, tok

    def bcast_layout(name, src, n):
        t, tok = sbt(name, [128, n])
        P.dma("sp", t, src.partition_broadcast(128), writes=[tok])
        return t, tok

    gA, t_gA = col_layout("gA", g_attn, 8)
    gM, t_gM = col_layout("gM", g_mem, 8)
    gX, t_gX = col_layout("gX", g_x, 8)
    gF, t_gF = col_layout("gF", g_f, 8)
    gk_b, t_gk = bcast_layout("gk_b", g_k, 64)
    gmk_b, t_gmk = bcast_layout("gmk_b", g_mk, 64)
    ggdn_b, t_ggdn = bcast_layout("ggdn_b", g_gdn, 128)
    dtb_b, t_dtb = bcast_layout("dtb_b", dt_bias, 4)
    nea_b, t_nea = bcast_layout("nea_b", a_log, 4)
    pow2_b, t_pow2 = bcast_layout("pow2_b", pow2_d, 32)
    P.op("act", lambda e: e.activation(out=nea_b, in_=nea_b, func=AF.Exp), [t_nea], [t_nea])
    P.op("dve", lambda e: e.tensor_scalar(out=nea_b, in0=nea_b, scalar1=-1.0, scalar2=None,
                                          op0=ALU.mult), [t_nea], [t_nea])
    gq8, t_gq8 = sbt("gq8", [128, 1])
    gmq8, t_gmq8 = sbt("gmq8", [128, 1])
    for (dst, tdst, src) in ((gq8, t_gq8, g_q), (gmq8, t_gmq8, g_mq)):
        for hh in range(2):
            P.dma("sp", dst[64 * hh:64 * hh + 64, :], src.rearrange("(d o) -> d o", o=1),
                  writes=[tdst], allow_slow_non_contiguous=True)
        P.op("pool", lambda e, dst=dst: e.tensor_scalar(out=dst, in0=dst, scalar1=0.125, scalar2=1.0,
                                                        op0=ALU.mult, op1=ALU.mult), [tdst], [tdst])
    cw, t_cw = sbt("cw", [128, 12, 4])
    for tap in range(4):
        P.dma("sp", cw[:, :, tap], conv_w[tap].rearrange("(j p) -> p j", p=128), writes=[t_cw],
              allow_slow_non_contiguous=True)
    neghalf, t_nh = sbt("neghalf", [128, 8])
    P.op("pool", lambda e: e.memset(neghalf, -0.5), [], [t_nh])

    ps = [nc.alloc_psum_tensor("ps%d" % i, [128, 512], F32).ap() for i in range(8)]
    t_ps = [Tok() for _ in range(8)]
    psb16 = [p_.bitcast(BF16) for p_ in ps]

    stage = [None, None]
    t_stage = [Tok(), Tok()]
    k.wl = 0

    def alloc_stage(n):
        for i in range(2):
            stage[i] = sb("wstage%d" % i, [128, n], F32)

    def load_rows(dst_kc, t_dst, src_rows, n, gain_col, t_gain):
        i = k.wl % 2
        k.wl += 1
        st = stage[i][:, 0:n]
        P.dma("sp", st, src_rows, writes=[t_stage[i]])
        if gain_col is None:
            if k.wl % 2 == 0:
                P.op("act", lambda e: e.activation(out=dst_kc, in_=st, func=AF.Copy),
                     [t_stage[i]], [t_dst])
            else:
                P.op("pool", lambda e: e.tensor_copy(out=dst_kc, in_=st), [t_stage[i]], [t_dst])
        else:
            if k.wl % 2 == 0:
                P.op("act", lambda e: e.activation(out=dst_kc, in_=st, func=AF.Copy, scale=gain_col),
                     [t_stage[i], t_gain], [t_dst])
            else:
                P.op("pool", lambda e: e.tensor_scalar(out=dst_kc, in0=st, scalar1=gain_col, scalar2=1.0,
                                                       op0=ALU.mult, op1=ALU.mult),
                     [t_stage[i], t_gain], [t_dst])

    def load_weight(dst, t_dst, src, c0, c1, gain, t_gain, d0=0):
        n = c1 - c0
        for kc in range(8):
            load_rows(dst[:, kc, d0:d0 + n], t_dst, src[kc * 128:(kc + 1) * 128, c0:c1], n,
                      None if gain is None else gain[:, kc:kc + 1], t_gain)

    xt = [sb("xt%d" % i, [128, D], F32) for i in range(2)]
    t_xt = [Tok(), Tok()]
    hb = [sb("hb%d" % i, [128, D], BF16) for i in range(2)]
    t_hb = [Tok(), Tok()]
    sq_scr, t_sq = sbt("sq_scr", [128, D], BF16)
    stat = [sb("stat%d" % i, [128, 4], F32) for i in range(2)]
    t_stat = [Tok(), Tok()]
    k.nt = 0
    for i in range(2):
        P.op("pool", lambda e, i=i: e.memset(xt[i], 0.0), [], [t_xt[i]])

    def load_x(src_rows, nrows):
        i = k.nt % 2
        k.nt += 1
        P.dma("sp", xt[i][0:nrows, :], src_rows, writes=[t_xt[i]])
        return xt[i], t_xt[i], i

    def norm_T(x, tx, i, hT, t_hT, col, psb):
        s, ts = stat[i], t_stat[i]
        P.op("act", lambda e: e.activation(out=sq_scr, in_=x, func=AF.Square,
                                           accum_out=s[:, 0:1]), [tx], [t_sq, ts])
        P.op("pool", lambda e: e.tensor_scalar(out=s[:, 1:2], in0=s[:, 0:1], scalar1=1.0 / D,
                                               scalar2=EPS, op0=ALU.mult, op1=ALU.add), [ts], [ts])
        P.op("pool", lambda e: e.tensor_tensor(out=s[:, 2:3], in0=s[:, 1:2], in1=neghalf[:, 0:1],
                                               op=ALU.pow), [ts, t_nh], [ts])
        h, th = hb[i], t_hb[i]
        P.op("act", lambda e: e.activation(out=h, in_=x, func=AF.Copy, scale=s[:, 2:3]),
             [tx, ts], [th])
        pb = psb16[psb]
        for kc in range(8):
            P.op("pe", lambda e, kc=kc: e.transpose(out=pb[:, kc * 128:(kc + 1) * 128],
                                                    in_=h[:, kc * 128:(kc + 1) * 128],
                                                    identity=identb),
                 [th, t_identb], [t_ps[psb]])
        P.op("dve", lambda e: e.tensor_copy(out=hT[:, :, col:col + 128],
                                            in_=pb.rearrange("p (k t) -> p k t", k=8)),
             [t_ps[psb]], [t_hT])

    def norm_transpose(src_rows, nrows, hT, t_hT, col, psb):
        x, tx, i = load_x(src_rows, nrows)
        norm_T(x, tx, i, hT, t_hT, col, psb)
        return x, tx

    def rstd_groups(dst, t_dst, ssq, t_ssq, n, width):
        P.op("pool", lambda e: e.tensor_scalar(out=dst, in0=ssq, scalar1=1.0 / width, scalar2=EPS,
                                               op0=ALU.mult, op1=ALU.add), [t_ssq], [t_dst])
        P.op("pool", lambda e: e.tensor_tensor(out=dst, in0=dst, in1=neghalf[:, 0:n], op=ALU.pow),
             [t_dst, t_nh], [t_dst])

    def head_rms(psrc, t_psrc, nh, scr, t_scr, ss, t_ss):
        P.op("act", lambda e: e.activation(out=scr[:, 0:nh * 64], in_=psrc, func=AF.Square),
             [t_psrc], [t_scr])
        P.op("dve", lambda e: e.tensor_reduce(out=ss[:, 0:nh],
                                              in_=scr[:, 0:nh * 64].rearrange("p (h d) -> p h d", h=nh),
                                              axis=AX.X, op=ALU.add), [t_scr], [t_ss])
        rstd_groups(ss[:, nh:2 * nh], t_ss, ss[:, 0:nh], t_ss, nh, 64)

    qscr, t_qscr = sbt("qscr", [128, 512])
    qss, t_qss = sbt("qss", [128, 16])
    PERSIST_TOP = k.top

    def prompt_seq(sq):
        k.top = PERSIST_TOP
        if STOP <= 0:
            return
        row_base = sq * SEQ
        mkT2, t_mkT2 = sbt("mkT2", [128, 2, 256], BF16)
        mv_b, t_mv = sbt("mv_b", [128, 2, 256], BF16)
        o_gdnT, t_ogT = sbt("o_gdnT", [128, 4, SEQ], BF16)
        SEQ_TOP = k.top
        alloc_stage(256)
        wmkv, t_wmkv = sbt("wmkv", [128, 8, 512], BF16)
        load_weight(wmkv, t_wmkv, w_mk, 0, 256, gM, t_gM, 0)
        load_weight(wmkv, t_wmkv, w_mv, 0, 256, gM, t_gM, 256)
        hTm, t_hTm = sbt("hTm", [128, 8, 128], BF16)
        mko = [sbt("mko%d" % i, [128, 512]) for i in range(2)]
        mkb, t_mkb = sbt("mkb", [128, 256], BF16)
        for mt in range(2):
            row0 = sq * MEM + mt * 128
            norm_transpose(memp[row0:row0 + 128, :], 128, hTm, t_hTm, 0, 0)
            for kc in range(8):
                P.op("pe", lambda e, kc=kc: e.matmul(ps[1], lhsT=hTm[:, kc, :], rhs=wmkv[:, kc, :],
                                                     start=(kc == 0), stop=(kc == 7)),
                     [t_hTm, t_wmkv], [t_ps[1]])
            o, to = mko[mt]
            P.op("act", lambda e, o=o: e.activation(out=o[:, 256:512], in_=ps[1][:, 256:512], func=AF.Copy),
                 [t_ps[1]], [to])
            P.op("act", lambda e, mt=mt: e.activation(out=mv_b[:, mt, :], in_=ps[1][:, 256:512], func=AF.Copy),
                 [t_ps[1]], [t_mv])
            head_rms(ps[1][:, 0:256], t_ps[1], 4, qscr, t_qscr, qss, t_qss)
            P.op("dve", lambda e, o=o: e.tensor_tensor(
                out=o[:, 0:256].rearrange("p (h d) -> p h d", h=4),
                in0=ps[1][:, 0:256].rearrange("p (h d) -> p h d", h=4),
                in1=qss[:, 4:8].unsqueeze(2).to_broadcast([128, 4, 64]), op=ALU.mult),
                [t_ps[1], t_qss], [to])
            P.op("pool", lambda e, o=o: e.tensor_tensor(
                out=o[:, 0:256].rearrange("p (h d) -> p h d", h=4),
                in0=o[:, 0:256].rearrange("p (h d) -> p h d", h=4),
                in1=gmk_b.unsqueeze(1).to_broadcast([128, 4, 64]), op=ALU.mult),
                [to, t_gmk], [to])
            P.dma("sp", memk_p[row0:row0 + 128, :], o[:, 0:256], reads=[to])
            P.dma("sp", memv_p[row0:row0 + 128, :], o[:, 256:512], reads=[to])
            P.op("act", lambda e, o=o: e.activation(out=mkb, in_=o[:, 0:256], func=AF.Copy), [to], [t_mkb])
            pb = psb16[2]
            for a in range(2):
                P.op("pe", lambda e, a=a: e.transpose(out=pb[:, a * 128:(a + 1) * 128],
                                                      in_=mkb[:, a * 128:(a + 1) * 128], identity=identb),
                     [t_mkb, t_identb], [t_ps[2]])
            P.op("dve", lambda e, mt=mt: e.tensor_copy(
                out=mkT2[:, :, mt * 128:(mt + 1) * 128],
                in_=pb[:, 0:256].rearrange("p (a t) -> p a t", a=2)), [t_ps[2]], [t_mkT2])
        P.barrier()
        if STOP <= 1:
            return
        k.top = SEQ_TOP
        gdn_phase(sq, o_gdnT, t_ogT)
        P.barrier()
        if STOP <= 2:
            return
        k.top = SEQ_TOP
        o_atT, t_oatT = sbt("o_atT", [128, 4, SEQ], BF16)
        D_TOP = k.top
        dsa_phase(sq, o_atT, t_oatT)
        P.barrier()
        if STOP <= 3:
            return
        k.top = D_TOP
        c1_phase(sq, o_atT, t_oatT, o_gdnT, t_ogT, mkT2, t_mkT2, mv_b, t_mv)
        P.barrier()
        if STOP <= 4:
            return
        k.top = PERSIST_TOP
        c2_phase(sq)
        P.barrier()

    def gdn_phase(sq, o_gdnT, t_ogT):
        row_base = sq * SEQ
        qTg, t_qTg = sbt("qTg", [128, 4, SEQ], BF16)
        kTg, t_kTg = sbt("kTg", [128, 4, SEQ], BF16)
        k_tm, t_ktm = sbt("k_tm", [128, NT, 4, 128], BF16)
        v_tm, t_vtm = sbt("v_tm", [128, NT, 4, 128], BF16)
        sgz, t_sgz = sbt("sgz", [128, NT, 512], BF16)
        gab, t_gab = sbt("gab", [128, NT, 8])
        G_TOP = k.top
        alloc_stage(2056)
        wB, t_wB = sbt("wB", [128, 8, 2056], BF16)
        load_weight(wB, t_wB, w_in, 1092, 3148, gA, t_gA)
        hTg, t_hTg = sbt("hTg", [128, 8, 512], BF16)
        Cq, t_Cq = sbt("Cq", [128, 4, 512])
        cvT, t_cvT = sbt("cvT", [128, 4, 512], BF16)
        Uc = [sbt("Uc%d" % i, [128, 515]) for i in range(2)]
        cacc = [sbt("cacc%d" % i, [128, 512]) for i in range(2)]
        halo, t_halo = sbt("halo", [128, 12, 3])
        sqb, t_sqb = sbt("sqb", [128, 512], BF16)
        lnb, t_lnb = sbt("lnb", [128, 512])
        P.op("pool", lambda e: e.memset(halo, 0.0), [], [t_halo])
        def conv_chunk(j, grp):
            b = 3 + (j % 2)
            for kc in range(8):
                P.op("pe", lambda e, kc=kc: e.matmul(
                    ps[b], lhsT=wB[:, kc, 128 * j:128 * j + 128], rhs=hTg[:, kc, :],
                    start=(kc == 0), stop=(kc == 7)), [t_hTg, t_wB], [t_ps[b]])
            u, tu = Uc[j % 2]
            ca, tca = cacc[j % 2]
            P.op("act", lambda e: e.activation(out=u[:, 3:515], in_=ps[b], func=AF.Copy), [t_ps[b]], [tu])
            P.op("pool", lambda e: e.tensor_copy(out=u[:, 0:3], in_=halo[:, j, :]), [t_halo], [tu])
            P.op("dve", lambda e: e.tensor_scalar(out=ca, in0=u[:, 0:512], scalar1=cw[:, j, 0:1], scalar2=None,
                                                  op0=ALU.mult), [tu, t_cw], [tca])
            for tap in range(1, 4):
                P.op("dve", lambda e, tap=tap: e.scalar_tensor_tensor(
                    out=ca, in0=u[:, tap:tap + 512], scalar=cw[:, j, tap:tap + 1], in1=ca,
                    op0=ALU.mult, op1=ALU.add), [tu, t_cw, tca], [tca])
            P.op("pool", lambda e: e.tensor_copy(out=halo[:, j, :], in_=u[:, 512:515]), [tu], [t_halo])
            if j < 8:
                P.op("act", lambda e: e.activation(out=Cq[:, j % 4, :], in_=ca, func=AF.Silu), [tca], [t_Cq])
            else:
                P.op("act", lambda e: e.activation(out=cvT[:, j - 8, :], in_=ca, func=AF.Silu), [tca], [t_cvT])

        def norm_chunk(j, gc0):
            P.op("act", lambda e: e.activation(out=sqb, in_=Cq[:, j % 4, :], func=AF.Square), [t_Cq], [t_sqb])
            P.op("pe", lambda e: e.matmul(ps[5], lhsT=onesb, rhs=sqb, start=True, stop=True),
                 [t_sqb, t_onesb], [t_ps[5]])
            P.op("act", lambda e: e.activation(out=lnb, in_=ps[5], func=AF.Ln, bias=1e-6, scale=1.0),
                 [t_ps[5]], [t_lnb])
            bias = (-0.5 * float(np.log(128.0))) if j < 4 else 0.0
            P.op("act", lambda e: e.activation(out=lnb, in_=lnb, func=AF.Exp, bias=bias, scale=-0.5),
                 [t_lnb], [t_lnb])
            dstT, tdst = (qTg, t_qTg) if j < 4 else (kTg, t_kTg)
            P.op("dve", lambda e: e.tensor_tensor(out=dstT[:, j % 4, gc0:gc0 + 512], in0=Cq[:, j % 4, :], in1=lnb,
                                                  op=ALU.mult), [t_Cq, t_lnb], [tdst])

        def g_tile(grp, t4):
            ti = grp * 4 + t4
            r0 = row_base + ti * 128
            norm_transpose(xp[r0:r0 + 128, :], 128, hTg, t_hTg, t4 * 128, 0)
            for kc in range(8):
                P.op("pe", lambda e, kc=kc: e.matmul(
                    ps[1], lhsT=hTg[:, kc, t4 * 128:(t4 + 1) * 128], rhs=wB[:, kc, 1536:2048],
                    start=(kc == 0), stop=(kc == 7)), [t_hTg, t_wB], [t_ps[1]])
            for kc in range(8):
                P.op("pe", lambda e, kc=kc: e.matmul(
                    ps[2][:, 0:8], lhsT=hTg[:, kc, t4 * 128:(t4 + 1) * 128], rhs=wB[:, kc, 2048:2056],
                    start=(kc == 0), stop=(kc == 7)), [t_hTg, t_wB], [t_ps[2]])
            P.op("act", lambda e: e.activation(out=sgz[:, ti, :], in_=ps[1], func=AF.Silu), [t_ps[1]], [t_sgz])
            P.op("dve", lambda e: e.tensor_copy(out=gab[:, ti, :], in_=ps[2][:, 0:8]), [t_ps[2]], [t_gab])

        def g_transposes(grp, t4):
            ti = grp * 4 + t4
            gc0 = grp * 512
            for (srcT, tsrc, c0, dst, tdst, b) in ((kTg, t_kTg, gc0 + t4 * 128, k_tm, t_ktm, 6),
                                                   (cvT, t_cvT, t4 * 128, v_tm, t_vtm, 7)):
                pb = psb16[b]
                for h in range(4):
                    P.op("pe", lambda e, h=h, srcT=srcT, c0=c0, pb=pb: e.transpose(
                        out=pb[:, h * 128:(h + 1) * 128], in_=srcT[:, h, c0:c0 + 128], identity=identb),
                        [tsrc, t_identb], [t_ps[b]])
                P.op("act", lambda e, dst=dst, pb=pb: e.activation(
                    out=dst[:, ti, :, :], in_=pb[:, 0:512].rearrange("p (h d) -> p h d", h=4), func=AF.Copy),
                    [t_ps[b]], [tdst])

        for grp in range(4):
            for t4 in range(4):
                g_tile(grp, t4)
            for j in range(0, 4):
                conv_chunk(j, grp)
            for j in range(0, 4):
                norm_chunk(j, grp * 512)
            for j in range(4, 12):
                conv_chunk(j, grp)
            for j in range(4, 8):
                norm_chunk(j, grp * 512)
            for t4 in range(4):
                g_transposes(grp, t4)
        P.barrier()
        if STOP <= 1.5:
            return
        k.top = G_TOP
        gall, t_gall = sbt("gall", [128, NT, 4])
        ball, t_ball = sbt("ball", [128, NT, 4])
        tmpa, t_tmpa = sbt("tmpa", [128, NT, 4])
        tmpb, t_tmpb = sbt("tmpb", [128, NT, 4])
        P.op("dve", lambda e: e.tensor_tensor(out=gall, in0=gab[:, :, 0:4],
                                              in1=dtb_b.unsqueeze(1).to_broadcast([128, NT, 4]), op=ALU.add),
             [t_gab, t_dtb], [t_gall])
        P.op("dve", lambda e: e.tensor_scalar(out=tmpa, in0=gall, scalar1=-1.0, scalar2=None, op0=ALU.mult),
             [t_gall], [t_tmpa])
        P.op("dve", lambda e: e.tensor_tensor(out=tmpa, in0=tmpa, in1=gall, op=ALU.min), [t_tmpa, t_gall], [t_tmpa])
        P.op("act", lambda e: e.activation(out=tmpa, in_=tmpa, func=AF.Exp), [t_tmpa], [t_tmpa])
        P.op("act", lambda e: e.activation(out=tmpa, in_=tmpa, func=AF.Ln, bias=1.0, scale=1.0), [t_tmpa], [t_tmpa])
        P.op("dve", lambda e: e.scalar_tensor_tensor(out=tmpb, in0=gall, scalar=0.0, in1=tmpa,
                                                     op0=ALU.max, op1=ALU.add), [t_gall, t_tmpa], [t_tmpb])
        P.op("dve", lambda e: e.tensor_tensor(out=gall, in0=tmpb,
                                              in1=nea_b.unsqueeze(1).to_broadcast([128, NT, 4]), op=ALU.mult),
             [t_tmpb, t_nea], [t_gall])
        P.op("act", lambda e: e.activation(out=ball, in_=gab[:, :, 4:8], func=AF.Sigmoid), [t_gab], [t_ball])

        S, t_S = sbt("S", [128, 4, 128])
        Sb, t_Sb = sbt("Sb", [128, 4, 128], BF16)
        P.op("pool", lambda e: e.memset(S, 0.0), [], [t_S])
        P.op("pool", lambda e: e.memset(Sb, 0.0), [], [t_Sb])
        NB = 2
        bufs = []
        for i in range(NB):
            bb = {}
            for nm, dt_ in (("Gh", F32), ("E", F32), ("Du", F32), ("Dl", F32), ("L0", BF16), ("L1", BF16),
                            ("M0", BF16), ("M1", BF16), ("P0", BF16), ("P1", BF16), ("kbg", BF16),
                            ("kdec", BF16), ("vb", BF16), ("u", F32), ("wT", BF16), ("qgT", BF16),
                            ("qkT", BF16), ("vnew", BF16), ("on", F32), ("og", BF16)):
                bb[nm] = sbt("%s_%d" % (nm, i), [128, 4, 128], dt_)
            bb["sc"] = sbt("gsc_%d" % i, [128, 40])
            bufs.append(bb)
        k.pbank = 0

        def bank():
            b = k.pbank
            k.pbank = (k.pbank + 1) % 8
            return b

        def gdn_tile(ti):
            B = bufs[ti % NB]
            c0 = ti * 128
            sc, tsc = B["sc"]
            Gh, tGh = B["Gh"]
            for h in range(4):
                P.op("pool", lambda e, h=h, Gh=Gh, ti=ti: e.tensor_scalar(
                    out=Gh[:, h, :], in0=trile, scalar1=gall[:, ti, h:h + 1], scalar2=1.0,
                    op0=ALU.mult, op1=ALU.mult), [t_trile, t_gall], [tGh])
            bE, bDu, bDl, bsm = bank(), bank(), bank(), bank()
            for h in range(4):
                P.op("pe", lambda e, h=h, Gh=Gh, bE=bE: e.matmul(ps[bE][:, h * 128:(h + 1) * 128], lhsT=onesf,
                                                                 rhs=Gh[:, h, :], start=True, stop=True),
                     [tGh, t_onesf], [t_ps[bE]])
                P.op("pe", lambda e, h=h, Gh=Gh, bDu=bDu: e.matmul(ps[bDu][:, h * 128:(h + 1) * 128], lhsT=sgt,
                                                                   rhs=Gh[:, h, :], start=True, stop=True),
                     [tGh, t_sgt], [t_ps[bDu]])
                P.op("pe", lambda e, h=h, Gh=Gh, bDl=bDl: e.matmul(ps[bDl][:, h * 128:(h + 1) * 128], lhsT=Gh[:, h, :],
                                                                   rhs=sgt, start=True, stop=True),
                     [tGh, t_sgt], [t_ps[bDl]])
            if GCUT <= 1:
                return
            P.op("pe", lambda e, ti=ti, bsm=bsm: e.matmul(ps[bsm][:, 0:4], lhsT=trile, rhs=gall[:, ti, :],
                                                          start=True, stop=True), [t_trile, t_gall], [t_ps[bsm]])
            P.op("pe", lambda e, ti=ti, bsm=bsm: e.matmul(ps[bsm][:, 8:12], lhsT=onesf, rhs=gall[:, ti, :],
                                                          start=True, stop=True), [t_onesf, t_gall], [t_ps[bsm]])
            if GCUT <= 2:
                return
            E, tE = B["E"]
            Du, tDu = B["Du"]
            Dl, tDl = B["Dl"]
            fl = lambda a: a.rearrange("p h c -> p (h c)")
            P.op("act", lambda e, E=E, bE=bE: e.activation(out=fl(E), in_=ps[bE], func=AF.Exp), [t_ps[bE]], [tE])
            P.op("act", lambda e, Du=Du, bDu=bDu: e.activation(out=fl(Du), in_=ps[bDu], func=AF.Exp), [t_ps[bDu]], [tDu])
            P.op("act", lambda e, Dl=Dl, bDl=bDl: e.activation(out=fl(Dl), in_=ps[bDl], func=AF.Exp), [t_ps[bDl]], [tDl])
            P.op("pool", lambda e, Du=Du: e.tensor_tensor(out=Du, in0=Du, in1=trile.unsqueeze(1).to_broadcast([128, 4, 128]),
                                                          op=ALU.mult), [tDu, t_trile], [tDu])
            P.op("pool", lambda e, Dl=Dl: e.tensor_tensor(out=Dl, in0=Dl, in1=sgt.unsqueeze(1).to_broadcast([128, 4, 128]),
                                                          op=ALU.mult), [tDl, t_sgt], [tDl])
            if GCUT <= 3:
                return
            P.op("dve", lambda e, sc=sc, bsm=bsm: e.tensor_copy(out=sc[:, 0:4], in_=ps[bsm][:, 0:4]), [t_ps[bsm]], [tsc])
            P.op("act", lambda e, sc=sc: e.activation(out=sc[:, 4:8], in_=sc[:, 0:4], func=AF.Exp), [tsc], [tsc])
            P.op("dve", lambda e, sc=sc, bsm=bsm: e.tensor_tensor(out=sc[:, 24:28], in0=ps[bsm][:, 8:12], in1=sc[:, 0:4],
                                                                  op=ALU.subtract), [t_ps[bsm], tsc], [tsc])
            P.op("act", lambda e, sc=sc: e.activation(out=sc[:, 8:12], in_=sc[:, 24:28], func=AF.Exp), [tsc], [tsc])
            P.op("act", lambda e, sc=sc, bsm=bsm: e.activation(out=sc[:, 12:16], in_=ps[bsm][:, 8:12], func=AF.Exp),
                 [t_ps[bsm]], [tsc])
            P.op("dve", lambda e, sc=sc, ti=ti: e.tensor_tensor(out=sc[:, 16:20], in0=sc[:, 4:8], in1=ball[:, ti, :],
                                                                op=ALU.mult), [tsc, t_ball], [tsc])
            P.op("dve", lambda e, sc=sc, ti=ti: e.tensor_scalar(out=sc[:, 20:24], in0=ball[:, ti, :], scalar1=-1.0,
                                                                scalar2=None, op0=ALU.mult), [t_ball], [tsc])
            if GCUT <= 4:
                return
            kbg, tkbg = B["kbg"]
            kdec, tkdec = B["kdec"]
            vb, tvb = B["vb"]
            bc = lambda a: a.unsqueeze(2).to_broadcast([128, 4, 128])
            P.op("pool", lambda e, kbg=kbg, sc=sc, ti=ti: e.tensor_tensor(out=kbg, in0=k_tm[:, ti, :, :], in1=bc(sc[:, 16:20]),
                                                                          op=ALU.mult), [t_ktm, tsc], [tkbg])
            P.op("pool", lambda e, kdec=kdec, sc=sc, ti=ti: e.tensor_tensor(out=kdec, in0=k_tm[:, ti, :, :], in1=bc(sc[:, 8:12]),
                                                                            op=ALU.mult), [t_ktm, tsc], [tkdec])
            P.op("pool", lambda e, vb=vb, ti=ti: e.tensor_tensor(out=vb, in0=v_tm[:, ti, :, :], in1=bc(ball[:, ti, :]),
                                                                 op=ALU.mult), [t_vtm, t_ball], [tvb])
            if GCUT <= 5:
                return
            bkk = bank()
            for h in range(4):
                P.op("pe", lambda e, h=h, bkk=bkk: e.matmul(ps[bkk][:, h * 128:(h + 1) * 128], lhsT=kTg[:, h, c0:c0 + 128],
                                                            rhs=kTg[:, h, c0:c0 + 128], start=True, stop=True),
                     [t_kTg], [t_ps[bkk]])
            Lc, tLc = B["L0"]
            Ln_, tLn = B["L1"]
            Mc, tMc = B["M0"]
            Mn, tMn = B["M1"]
            Pc, tPc = B["P0"]
            Pn, tPn = B["P1"]
            for h in range(4):
                P.op("dve", lambda e, h=h, Lc=Lc, sc=sc, Dl=Dl, bkk=bkk: e.scalar_tensor_tensor(
                    out=Lc[:, h, :], in0=ps[bkk][:, h * 128:(h + 1) * 128], scalar=sc[:, 20 + h:21 + h], in1=Dl[:, h, :],
                    op0=ALU.mult, op1=ALU.mult), [t_ps[bkk], tsc, tDl], [tLc])
            if GCUT <= 6:
                return
            bM = bank()
            pbm = psb16[bM]
            for h in range(4):
                P.op("pe", lambda e, h=h, Lc=Lc, pbm=pbm: e.transpose(out=pbm[:, h * 128:(h + 1) * 128], in_=Lc[:, h, :],
                                                                      identity=identb), [tLc, t_identb], [t_ps[bM]])
            pm3 = pbm[:, 0:512].rearrange("p (h c) -> p h c", h=4)
            if GCUT == 61:
                return
            P.op("act", lambda e, Mc=Mc, pm3=pm3: e.activation(out=Mc, in_=pm3, func=AF.Copy), [t_ps[bM]], [tMc])
            if GCUT == 62:
                return
            P.op("pool", lambda e, Pc=Pc, Mc=Mc: e.tensor_tensor(out=Pc, in0=Mc,
                                                                 in1=identb.unsqueeze(1).to_broadcast([128, 4, 128]),
                                                                 op=ALU.add), [tMc, t_identb], [tPc])
            if GCUT <= 7 or GCUT in (61, 62):
                return
            for lev in range(6):
                last = (lev == 5)
                bL = bank()
                if not last:
                    bMM = bank()
                    for h in range(4):
                        P.op("pe", lambda e, h=h, Lc=Lc, Mc=Mc, bMM=bMM: e.matmul(
                            ps[bMM][:, h * 128:(h + 1) * 128], lhsT=Lc[:, h, :], rhs=Mc[:, h, :], start=True, stop=True),
                            [tLc, tMc], [t_ps[bMM]])
                for h in range(4):
                    P.op("pe", lambda e, h=h, Lc=Lc, Mc=Mc, bL=bL: e.matmul(
                        ps[bL][:, h * 128:(h + 1) * 128], lhsT=Mc[:, h, :], rhs=Lc[:, h, :], start=True, stop=True),
                        [tLc, tMc], [t_ps[bL]])
                P.op("dve", lambda e, Ln_=Ln_, bL=bL: e.tensor_copy(out=fl(Ln_), in_=ps[bL]), [t_ps[bL]], [tLn])
                if not last:
                    P.op("act", lambda e, Mn=Mn, bMM=bMM: e.activation(out=fl(Mn), in_=ps[bMM], func=AF.Copy),
                         [t_ps[bMM]], [tMn])
                bP = bank()
                for h in range(4):
                    P.op("pe", lambda e, h=h, Ln_=Ln_, Pc=Pc, bP=bP: e.matmul(
                        ps[bP][:, h * 128:(h + 1) * 128], lhsT=Ln_[:, h, :], rhs=Pc[:, h, :], start=True, stop=True),
                        [tLn, tPc], [t_ps[bP]])
                P.op("dve", lambda e, Pn=Pn, Pc=Pc, bP=bP: e.tensor_tensor(out=fl(Pn), in0=ps[bP], in1=fl(Pc), op=ALU.add),
                     [t_ps[bP], tPc], [tPn])
                Lc, tLc, Ln_, tLn = Ln_, tLn, Lc, tLc
                Mc, tMc, Mn, tMn = Mn, tMn, Mc, tMc
                Pc, tPc, Pn, tPn = Pn, tPn, Pc, tPc
            if GCUT <= 8:
                return
            bu, bw, bq = bank(), bank(), bank()
            for h in range(4):
                P.op("pe", lambda e, h=h, Pc=Pc, vb=vb, bu=bu: e.matmul(ps[bu][:, h * 128:(h + 1) * 128], lhsT=Pc[:, h, :],
                                                                        rhs=vb[:, h, :], start=True, stop=True),
                     [tPc, tvb], [t_ps[bu]])
                P.op("pe", lambda e, h=h, Pc=Pc, kbg=kbg, bw=bw: e.matmul(ps[bw][:, h * 128:(h + 1) * 128], lhsT=kbg[:, h, :],
                                                                          rhs=Pc[:, h, :], start=True, stop=True),
                     [tPc, tkbg], [t_ps[bw]])
                P.op("pe", lambda e, h=h, bq=bq: e.matmul(ps[bq][:, h * 128:(h + 1) * 128], lhsT=kTg[:, h, c0:c0 + 128],
                                                          rhs=qTg[:, h, c0:c0 + 128], start=True, stop=True),
                     [t_kTg, t_qTg], [t_ps[bq]])
            u, tu_ = B["u"]
            wT, twT = B["wT"]
            qgT, tqgT = B["qgT"]
            qkT, tqkT = B["qkT"]
            P.op("act", lambda e, u=u, bu=bu: e.activation(out=fl(u), in_=ps[bu], func=AF.Copy), [t_ps[bu]], [tu_])
            P.op("act", lambda e, wT=wT, bw=bw: e.activation(out=fl(wT), in_=ps[bw], func=AF.Copy), [t_ps[bw]], [twT])
            P.op("dve", lambda e, qkT=qkT, Du=Du, bq=bq: e.tensor_tensor(out=fl(qkT), in0=ps[bq], in1=fl(Du), op=ALU.mult),
                 [t_ps[bq], tDu], [tqkT])
            P.op("pool", lambda e, qgT=qgT, E=E: e.tensor_tensor(out=qgT, in0=qTg[:, :, c0:c0 + 128], in1=E, op=ALU.mult),
                 [t_qTg, tE], [tqgT])
            if GCUT <= 9:
                return
            bws, bo, bs = bank(), bank(), bank()
            for h in range(4):
                P.op("pe", lambda e, h=h, wT=wT, bws=bws: e.matmul(ps[bws][:, h * 128:(h + 1) * 128], lhsT=wT[:, h, :],
                                                                   rhs=Sb[:, h, :], start=True, stop=True),
                     [twT, t_Sb], [t_ps[bws]])
            vnew, tvn = B["vnew"]
            P.op("dve", lambda e, vnew=vnew, u=u, bws=bws: e.tensor_tensor(out=fl(vnew), in0=fl(u), in1=ps[bws],
                                                                           op=ALU.subtract), [tu_, t_ps[bws]], [tvn])
            for h in range(4):
                P.op("pe", lambda e, h=h, qgT=qgT, bo=bo: e.matmul(ps[bo][:, h * 128:(h + 1) * 128], lhsT=qgT[:, h, :],
                                                                   rhs=Sb[:, h, :], start=True, stop=False),
                     [tqgT, t_Sb], [t_ps[bo]])
                P.op("pe", lambda e, h=h, qkT=qkT, vnew=vnew, bo=bo: e.matmul(ps[bo][:, h * 128:(h + 1) * 128], lhsT=qkT[:, h, :],
                                                                              rhs=vnew[:, h, :], start=False, stop=True),
                     [tqkT, tvn], [t_ps[bo]])
            for h in range(4):
                P.op("pe", lambda e, h=h, kdec=kdec, vnew=vnew, bs=bs: e.matmul(ps[bs][:, h * 128:(h + 1) * 128], lhsT=kdec[:, h, :],
                                                                                rhs=vnew[:, h, :], start=True, stop=True),
                     [tkdec, tvn], [t_ps[bs]])
            for h in range(4):
                P.op("dve", lambda e, h=h, sc=sc, bs=bs: e.scalar_tensor_tensor(
                    out=S[:, h, :], in0=S[:, h, :], scalar=sc[:, 12 + h:13 + h], in1=ps[bs][:, h * 128:(h + 1) * 128],
                    op0=ALU.mult, op1=ALU.add), [t_S, tsc, t_ps[bs]], [t_S])
            P.op("act", lambda e: e.activation(out=Sb, in_=S, func=AF.Copy), [t_S], [t_Sb])
            if GCUT <= 10:
                return
            on, ton = B["on"]
            og, tog = B["og"]
            P.op("act", lambda e, on=on, bo=bo: e.activation(out=fl(on), in_=ps[bo], func=AF.Square), [t_ps[bo]], [ton])
            P.op("dve", lambda e, on=on, sc=sc: e.tensor_reduce(out=sc[:, 28:32], in_=on, axis=AX.X, op=ALU.add),
                 [ton], [tsc])
            P.op("pool", lambda e, sc=sc: e.tensor_scalar(out=sc[:, 32:36], in0=sc[:, 28:32], scalar1=1.0 / 128, scalar2=EPS,
                                                          op0=ALU.mult, op1=ALU.add), [tsc], [tsc])
            P.op("pool", lambda e, sc=sc: e.tensor_tensor(out=sc[:, 32:36], in0=sc[:, 32:36], in1=neghalf[:, 0:4], op=ALU.pow),
                 [tsc, t_nh], [tsc])
            P.op("dve", lambda e, on=on, sc=sc, bo=bo: e.tensor_tensor(
                out=on, in0=ps[bo].rearrange("p (h c) -> p h c", h=4), in1=bc(sc[:, 32:36]), op=ALU.mult),
                [t_ps[bo], tsc], [ton])
            P.op("pool", lambda e, on=on: e.tensor_tensor(out=on, in0=on, in1=ggdn_b.unsqueeze(1).to_broadcast([128, 4, 128]),
                                                          op=ALU.mult), [ton, t_ggdn], [ton])
            P.op("pool", lambda e, on=on, og=og, ti=ti: e.tensor_tensor(
                out=og, in0=on, in1=sgz[:, ti, :].rearrange("p (h c) -> p h c", h=4), op=ALU.mult),
                [ton, t_sgz], [tog])
            bt = bank()
            pbt = psb16[bt]
            for h in range(4):
                P.op("pe", lambda e, h=h, og=og, pbt=pbt: e.transpose(out=pbt[:, h * 128:(h + 1) * 128], in_=og[:, h, :],
                                                                      identity=identb), [tog, t_identb], [t_ps[bt]])
            P.op("act", lambda e, pbt=pbt: e.activation(out=o_gdnT[:, :, c0:c0 + 128],
                                                        in_=pbt[:, 0:512].rearrange("p (h c) -> p h c", h=4), func=AF.Copy),
                 [t_ps[bt]], [t_ogT])
        for ti in range(NT if STOP > 1.8 else 1):
            gdn_tile(ti)
        P.dma("sp", ssm_p[sq * 512:(sq + 1) * 512, :].rearrange("(h d) v -> d h v", h=4), S, reads=[t_S])

    NIT = 18

    def dsa_phase(sq, o_atT, t_oatT):
        row_base = sq * SEQ
        qT2, t_qT2 = sbt("qT2", [128, NT, 512], BF16)
        kT2, t_kT2 = sbt("kT2", [128, SEQ], BF16)
        v_b, t_vb = sbt("v_b", [128, NT, 128], BF16)
        qiT2, t_qiT2 = sbt("qiT2", [128, 2, SEQ], BF16)
        kiT2, t_kiT2 = sbt("kiT2", [128, SEQ], BF16)
        wi_s, t_wi = sbt("wi_s", [128, NT, 4])
        A_TOP = k.top
        alloc_stage(1536)
        wA, t_wA = sbt("wA", [128, 8, 1092], BF16)
        load_weight(wA, t_wA, w_in, 0, 1092, gA, t_gA)
        wConv, t_wConv = sbt("wConv", [128, 8, 1536], BF16)
        load_weight(wConv, t_wConv, w_in, 1092, 2628, gA, t_gA)
        hT1, t_hT1 = sbt("hT1", [128, 8, 128], BF16)
        ko = [sbt("ko%d" % i, [128, 320]) for i in range(2)]
        cvo, t_cvo = sbt("cvo", [128, 1536])
        qnb, t_qnb = sbt("qnb", [128, 512], BF16)
        kb_, t_kb = sbt("kb_", [128, 128], BF16)
        qib, t_qib = sbt("qib", [128, 256], BF16)
        kib, t_kib = sbt("kib", [128, 128], BF16)

        def proj_tile(t):
            r0 = row_base + t * 128
            c0 = t * 128
            norm_transpose(xp[r0:r0 + 128, :], 128, hT1, t_hT1, 0, 0)
            for (b, a0, a1) in ((1, 0, 512), (2, 512, 1024), (3, 1024, 1092)):
                for kc in range(8):
                    P.op("pe", lambda e, kc=kc, b=b, a0=a0, a1=a1: e.matmul(
                        ps[b][:, 0:a1 - a0], lhsT=hT1[:, kc, :], rhs=wA[:, kc, a0:a1],
                        start=(kc == 0), stop=(kc == 7)), [t_hT1, t_wA], [t_ps[b]])
            o, to = ko[t % 2]
            if PCUT <= 1:
                return
            head_rms(ps[1], t_ps[1], 8, qscr, t_qscr, qss, t_qss)
            P.op("dve", lambda e: e.tensor_tensor(
                out=qnb.rearrange("p (r g d) -> p g r d", r=4, g=2),
                in0=ps[1].rearrange("p (g r d) -> p g r d", g=2, r=4),
                in1=qss[:, 8:16].rearrange("p (g r) -> p g r", g=2).unsqueeze(3).to_broadcast([128, 2, 4, 64]),
                op=ALU.mult), [t_ps[1], t_qss], [t_qnb])
            pbq = psb16[4]
            for r in range(4):
                P.op("pe", lambda e, r=r: e.transpose(out=pbq[:, r * 128:(r + 1) * 128], in_=qnb[:, r * 128:(r + 1) * 128],
                                                      identity=identb), [t_qnb, t_identb], [t_ps[4]])
            P.op("act", lambda e: e.activation(out=qT2[:, t, :], in_=pbq[:, 0:512], func=AF.Copy),
                 [t_ps[4]], [t_qT2])
            P.op("pool", lambda e: e.tensor_scalar(out=qT2[:, t, :], in0=qT2[:, t, :], scalar1=gq8[:, 0:1], scalar2=1.0,
                                                   op0=ALU.mult, op1=ALU.mult), [t_qT2, t_gq8], [t_qT2])
            if PCUT <= 2:
                return
            head_rms(ps[2][:, 0:128], t_ps[2], 2, qscr, t_qscr, qss, t_qss)
            P.op("dve", lambda e: e.tensor_tensor(
                out=o[:, 0:128].rearrange("p (h d) -> p h d", h=2),
                in0=ps[2][:, 0:128].rearrange("p (h d) -> p h d", h=2),
                in1=qss[:, 2:4].unsqueeze(2).to_broadcast([128, 2, 64]), op=ALU.mult),
                [t_ps[2], t_qss], [to])
            P.op("pool", lambda e: e.tensor_tensor(
                out=o[:, 0:128].rearrange("p (h d) -> p h d", h=2),
                in0=o[:, 0:128].rearrange("p (h d) -> p h d", h=2),
                in1=gk_b.unsqueeze(1).to_broadcast([128, 2, 64]), op=ALU.mult), [to, t_gk], [to])
            P.op("act", lambda e: e.activation(out=kb_, in_=o[:, 0:128], func=AF.Copy), [to], [t_kb])
            pb5 = psb16[5]
            P.op("pe", lambda e: e.transpose(out=pb5[:, 0:128], in_=kb_, identity=identb), [t_kb, t_identb], [t_ps[5]])
            if PCUT <= 3:
                return
            P.op("act", lambda e: e.activation(out=o[:, 128:256], in_=ps[2][:, 128:256], func=AF.Copy), [t_ps[2]], [to])
            P.op("act", lambda e: e.activation(out=v_b[:, t, :], in_=ps[2][:, 128:256], func=AF.Copy), [t_ps[2]], [t_vb])
            P.op("act", lambda e: e.activation(out=qib, in_=ps[2][:, 256:512], func=AF.Copy, scale=0.125),
                 [t_ps[2]], [t_qib])
            for a in range(2):
                P.op("pe", lambda e, a=a: e.transpose(out=pb5[:, 128 + a * 128:256 + a * 128],
                                                      in_=qib[:, a * 128:(a + 1) * 128], identity=identb),
                     [t_qib, t_identb], [t_ps[5]])
            if PCUT <= 4:
                return
            P.op("act", lambda e: e.activation(out=o[:, 256:320], in_=ps[3][:, 0:64], func=AF.Copy), [t_ps[3]], [to])
            P.op("act", lambda e: e.activation(out=kib[:, 0:64], in_=ps[3][:, 0:64], func=AF.Copy), [t_ps[3]], [t_kib])
            P.op("act", lambda e: e.activation(out=kib[:, 64:128], in_=ps[3][:, 0:64], func=AF.Copy), [t_ps[3]], [t_kib])
            P.op("pe", lambda e: e.transpose(out=pb5[:, 384:512], in_=kib, identity=identb), [t_kib, t_identb], [t_ps[5]])
            P.op("act", lambda e: e.activation(out=wi_s[:, t, :], in_=ps[3][:, 64:68], func=AF.Copy, scale=0.5),
                 [t_ps[3]], [t_wi])
            if PCUT <= 5:
                return
            P.op("act", lambda e: e.activation(out=kT2[:, c0:c0 + 128], in_=pb5[:, 0:128], func=AF.Copy), [t_ps[5]], [t_kT2])
            if PCUT == 51:
                return
            P.op("act", lambda e: e.activation(out=qiT2[:, :, c0:c0 + 128],
                                               in_=pb5[:, 128:384].rearrange("p (a t) -> p a t", a=2), func=AF.Copy),
                 [t_ps[5]], [t_qiT2])
            if PCUT == 52:
                return
            P.op("act", lambda e: e.activation(out=kiT2[:, c0:c0 + 128], in_=pb5[:, 384:512], func=AF.Copy),
                 [t_ps[5]], [t_kiT2])
            if PCUT == 53:
                return
            P.dma("sp", k_p[r0:r0 + 128, :], o[:, 0:128], reads=[to])
            P.dma("sp", v_p[r0:r0 + 128, :], o[:, 128:256], reads=[to])
            P.dma("sp", kidx_p[r0:r0 + 128, :], o[:, 256:320], reads=[to])
            if PCUT <= 6:
                return
            if t == NT - 1:
                for b in range(3):
                    for kc in range(8):
                        P.op("pe", lambda e, kc=kc, b=b: e.matmul(
                            ps[5 + b] if b < 2 else ps[0], lhsT=hT1[:, kc, :], rhs=wConv[:, kc, b * 512:(b + 1) * 512],
                            start=(kc == 0), stop=(kc == 7)), [t_hT1, t_wConv], [t_ps[5 + b] if b < 2 else t_ps[0]])
                for b in range(3):
                    bb = 5 + b if b < 2 else 0
                    P.op("act", lambda e, b=b, bb=bb: e.activation(out=cvo[:, b * 512:(b + 1) * 512], in_=ps[bb],
                                                                   func=AF.Copy), [t_ps[bb]], [t_cvo])
                P.dma("sp", conv_p[sq * 3:(sq + 1) * 3, :], cvo[125:128, :], reads=[t_cvo])

        for t in range(NT):
            proj_tile(t)
        P.barrier()
        if DCUT <= 1:
            return
        k.top = A_TOP
        scb = [sbt("scb%d" % i, [128, SEQ]) for i in range(2)]
        rl = [sbt("rl%d" % i, [128, 512]) for i in range(2)]
        junk, t_junk = sbt("junk", [128, SEQ], BF16)
        mask, t_mask = sbt("mask", [128, SEQ], BF16)
        maskT, t_maskT = sbt("maskT", [128, NT, 128], BF16)
        PT = [sbt("PT%d" % i, [128, 4, 128], BF16) for i in range(4)]
        bis, t_bis = sbt("bis", [128, 64])
        rec, t_rec = sbt("rec", [128, 4, 128])

        def q_tile(qt):
            L = (qt + 1) * 128
            q0 = qt * 128
            S_, tS = scb[qt % 2]
            nch = (L + 511) // 512
            cnt_ = 0
            for ch in range(nch):
                k0 = ch * 512
                n = min(512, L - k0)
                for ih in range(4):
                    a, b = ih // 2, ih % 2
                    bnk = cnt_ % 2
                    r_, tr = rl[cnt_ % 2]
                    cnt_ += 1
                    P.op("pe", lambda e, a=a, b=b, bnk=bnk, k0=k0, n=n: e.matmul(
                        ps[bnk][:, 0:n], lhsT=qiT2[64 * b:64 * b + 64, a, q0:q0 + 128],
                        rhs=kiT2[64 * b:64 * b + 64, k0:k0 + n], start=True, stop=True),
                        [t_qiT2, t_kiT2], [t_ps[bnk]])
                    P.op("act", lambda e, r_=r_, bnk=bnk, n=n: e.activation(out=r_[:, 0:n], in_=ps[bnk][:, 0:n], func=AF.Relu),
                         [t_ps[bnk]], [tr])
                    if ih == 0:
                        P.op("dve", lambda e, r_=r_, k0=k0, n=n: e.tensor_scalar(
                            out=S_[:, k0:k0 + n], in0=r_[:, 0:n], scalar1=wi_s[:, qt, 0:1], scalar2=None, op0=ALU.mult),
                            [tr, t_wi], [tS])
                    else:
                        P.op("dve", lambda e, r_=r_, k0=k0, n=n, ih=ih: e.scalar_tensor_tensor(
                            out=S_[:, k0:k0 + n], in0=r_[:, 0:n], scalar=wi_s[:, qt, ih:ih + 1], in1=S_[:, k0:k0 + n],
                            op0=ALU.mult, op1=ALU.add), [tr, t_wi, tS], [tS])
            if DCUT <= 2:
                return
            dg = S_[:, q0:q0 + 128]
            P.op("dve", lambda e: e.tensor_tensor(out=dg, in0=dg, in1=lowinc, op=ALU.mult), [tS, t_lowinc], [tS])
            P.op("dve", lambda e: e.tensor_reduce(out=bis[:, 0:1], in_=S_[:, 0:L], axis=AX.X, op=ALU.max), [tS], [t_bis])
            P.op("dve", lambda e: e.tensor_reduce(out=bis[:, 1:2], in_=S_[:, 0:L], axis=AX.X, op=ALU.min), [tS], [t_bis])
            P.op("pool", lambda e: e.tensor_tensor(out=dg, in0=dg, in1=tribias, op=ALU.add), [tS, t_tribias], [tS])
            P.op("dve", lambda e: e.tensor_tensor(out=bis[:, 2:3], in0=bis[:, 0:1], in1=bis[:, 1:2], op=ALU.subtract),
                 [t_bis], [t_bis])
            P.op("dve", lambda e: e.tensor_scalar(out=bis[:, 3:4], in0=bis[:, 2:3], scalar1=0.5005, scalar2=0.0005,
                                                  op0=ALU.mult, op1=ALU.add), [t_bis], [t_bis])
            P.op("dve", lambda e: e.tensor_tensor(out=bis[:, 4:5], in0=bis[:, 0:1], in1=bis[:, 3:4], op=ALU.subtract),
                 [t_bis], [t_bis])
            P.op("dve", lambda e: e.tensor_scalar(out=bis[:, 8:9 + NIT], in0=pow2_b[:, 0:NIT + 1], scalar1=bis[:, 3:4],
                                                  scalar2=None, op0=ALU.mult), [t_bis, t_pow2], [t_bis])
            for it in range(NIT):
                P.op("dve", lambda e: e.tensor_scalar(out=junk[:, 0:L], in0=S_[:, 0:L], scalar1=bis[:, 4:5], scalar2=None,
                                                      op0=ALU.is_ge, op1=ALU.add, accum_out=bis[:, 5:6]),
                     [tS, t_bis], [t_junk, t_bis])
                P.op("dve", lambda e: e.tensor_scalar(out=bis[:, 6:7], in0=bis[:, 5:6], scalar1=255.5, scalar2=0.5,
                                                      op0=ALU.is_ge, op1=ALU.subtract), [t_bis], [t_bis])
                P.op("dve", lambda e, it=it: e.scalar_tensor_tensor(out=bis[:, 4:5], in0=bis[:, 6:7], scalar=bis[:, 8 + it:9 + it],
                                                                    in1=bis[:, 4:5], op0=ALU.mult, op1=ALU.add),
                     [t_bis], [t_bis])
            P.op("dve", lambda e: e.tensor_tensor(out=bis[:, 7:8], in0=bis[:, 4:5], in1=bis[:, 8 + NIT:9 + NIT], op=ALU.subtract),
                 [t_bis], [t_bis])
            P.op("dve", lambda e: e.tensor_scalar(out=mask[:, 0:L], in0=S_[:, 0:L], scalar1=bis[:, 7:8], scalar2=None,
                                                  op0=ALU.is_ge), [tS, t_bis], [t_mask])
            if DCUT <= 3:
                return
            for half in range((qt // 8) + 1):
                nb = min(8, qt + 1 - half * 8)
                for j in range(nb):
                    kb = half * 8 + j
                    P.op("pe", lambda e, kb=kb, j=j, half=half: e.transpose(
                        out=psb16[half][:, j * 128:(j + 1) * 128], in_=mask[:, kb * 128:(kb + 1) * 128], identity=identb),
                        [t_mask, t_identb], [t_ps[half]])
                P.op("act", lambda e, half=half, nb=nb: e.activation(
                    out=maskT[:, half * 8:half * 8 + nb, :],
                    in_=psb16[half][:, 0:nb * 128].rearrange("p (j t) -> p j t", j=nb), func=AF.Copy),
                    [t_ps[half]], [t_maskT])
            if DCUT <= 4:
                return
            cnt2 = 0
            for kb in range(qt + 1):
                for g in range(2):
                    bS = 2 + 2 * g + (kb % 2)
                    PTt, tPT = PT[cnt2 % 4]
                    cnt2 += 1
                    P.op("pe", lambda e, kb=kb, g=g, bS=bS: e.matmul(
                        ps[bS], lhsT=kT2[64 * g:64 * g + 64, kb * 128:(kb + 1) * 128],
                        rhs=qT2[64 * g:64 * g + 64, qt, :], start=True, stop=True),
                        [t_kT2, t_qT2], [t_ps[bS]])
                    P.op("act", lambda e, PTt=PTt, bS=bS: e.activation(out=PTt.rearrange("p r t -> p (r t)"), in_=ps[bS],
                                                                       func=AF.Exp), [t_ps[bS]], [tPT])
                    P.op("dve" if g == 0 else "pool", lambda e, PTt=PTt, kb=kb: e.tensor_tensor(
                        out=PTt, in0=PTt, in1=maskT[:, kb, :].unsqueeze(1).to_broadcast([128, 4, 128]), op=ALU.mult),
                        [tPT, t_maskT], [tPT])
                    P.op("pe", lambda e, PTt=PTt, kb=kb, g=g: e.matmul(
                        ps[6][64 * g:64 * g + 64, :], lhsT=v_b[:, kb, 64 * g:64 * g + 64],
                        rhs=PTt.rearrange("p r t -> p (r t)"), start=(kb == 0), stop=(kb == qt)),
                        [tPT, t_vb], [t_ps[6]])
                    P.op("pe", lambda e, PTt=PTt, kb=kb, g=g: e.matmul(
                        ps[7][64 * g:64 * g + 64, :], lhsT=onesb[:, 0:64],
                        rhs=PTt.rearrange("p r t -> p (r t)"), start=(kb == 0), stop=(kb == qt)),
                        [tPT, t_onesb], [t_ps[7]])
            P.op("dve", lambda e: e.reciprocal(out=rec.rearrange("p r t -> p (r t)"), in_=ps[7]), [t_ps[7]], [t_rec])
            P.op("dve", lambda e: e.tensor_tensor(out=o_atT[:, :, q0:q0 + 128],
                                                  in0=ps[6].rearrange("p (r t) -> p r t", r=4), in1=rec, op=ALU.mult),
                 [t_ps[6], t_rec], [t_oatT])

        for qt in range(NT):
            q_tile(qt)

    def c1_phase(sq, o_atT, t_oatT, o_gdnT, t_ogT, mkT2, t_mkT2, mv_b, t_mv):
        row_base = sq * SEQ
        alloc_stage(1024)
        Wo_a, t_Woa = sbt("Wo_a", [128, 4, 1024], BF16)
        Wo_g, t_Wog = sbt("Wo_g", [128, 4, 1024], BF16)
        Wmq, t_Wmq = sbt("Wmq", [128, 8, 256], BF16)
        Wmo, t_Wmo = sbt("Wmo", [128, 2, 1024], BF16)
        for r in range(4):
            i = k.wl % 2
            k.wl += 1
            st = stage[i][:, 0:1024]
            for g in range(2):
                P.dma("sp", st[64 * g:64 * g + 64, :], w_out[256 * g + 64 * r:256 * g + 64 * r + 64, :],
                      writes=[t_stage[i]])
            P.op("act", lambda e, st=st, r=r: e.activation(out=Wo_a[:, r, :], in_=st, func=AF.Copy),
                 [t_stage[i]], [t_Woa])
        for h in range(4):
            load_rows(Wo_g[:, h, :], t_Wog, w_out[512 + 128 * h:512 + 128 * h + 128, :], 1024, None, None)
        load_weight(Wmq, t_Wmq, w_mq, 0, 256, gX, t_gX)
        for a in range(2):
            load_rows(Wmo[:, a, :], t_Wmo, w_mo[128 * a:128 * a + 128, :], 1024, None, None)
        x1 = [sbt("x1_%d" % i, [128, D]) for i in range(2)]
        hT2, t_hT2 = sbt("hT2", [128, 8, 128], BF16)
        qmb, t_qmb = sbt("qmb", [128, 256], BF16)
        qmT2, t_qmT2 = sbt("qmT2", [128, 2, 128], BF16)
        PTm, t_PTm = sbt("PTm", [128, 2, 4, 128], BF16)
        omT2, t_omT2 = sbt("omT2", [128, 2, 128], BF16)
        recm, t_recm = sbt("recm", [128, 256])

        def c1_tile(t):
            r0 = row_base + t * 128
            c0 = t * 128
            if CCUT <= 0:
                return
            x, tx, i = load_x(xp[r0:r0 + 128, :], 128)
            xx, txx = x1[t % 2]
            for c in range(2):
                for r in range(4):
                    P.op("pe", lambda e, r=r, c=c: e.matmul(ps[1 + c], lhsT=o_atT[:, r, c0:c0 + 128],
                                                            rhs=Wo_a[:, r, c * 512:(c + 1) * 512], start=(r == 0), stop=False),
                         [t_oatT, t_Woa], [t_ps[1 + c]])
                for h in range(4):
                    P.op("pe", lambda e, h=h, c=c: e.matmul(ps[1 + c], lhsT=o_gdnT[:, h, c0:c0 + 128],
                                                            rhs=Wo_g[:, h, c * 512:(c + 1) * 512], start=False, stop=(h == 3)),
                         [t_ogT, t_Wog], [t_ps[1 + c]])
                P.op("dve", lambda e, c=c: e.tensor_tensor(out=xx[:, c * 512:(c + 1) * 512], in0=ps[1 + c],
                                                           in1=x[:, c * 512:(c + 1) * 512], op=ALU.add),
                     [t_ps[1 + c], tx], [txx])
            if CCUT <= 1:
                return
            norm_T(xx, txx, i, hT2, t_hT2, 0, 0)
            for kc in range(8):
                P.op("pe", lambda e, kc=kc: e.matmul(ps[3][:, 0:256], lhsT=hT2[:, kc, :], rhs=Wmq[:, kc, :],
                                                     start=(kc == 0), stop=(kc == 7)), [t_hT2, t_Wmq], [t_ps[3]])
            if CCUT <= 2:
                return
            head_rms(ps[3][:, 0:256], t_ps[3], 4, qscr, t_qscr, qss, t_qss)
            P.op("dve", lambda e: e.tensor_tensor(
                out=qmb.rearrange("p (h d) -> p h d", h=4), in0=ps[3][:, 0:256].rearrange("p (h d) -> p h d", h=4),
                in1=qss[:, 4:8].unsqueeze(2).to_broadcast([128, 4, 64]), op=ALU.mult), [t_ps[3], t_qss], [t_qmb])
            pb4 = psb16[4]
            for a in range(2):
                P.op("pe", lambda e, a=a: e.transpose(out=pb4[:, a * 128:(a + 1) * 128], in_=qmb[:, a * 128:(a + 1) * 128],
                                                      identity=identb), [t_qmb, t_identb], [t_ps[4]])
            P.op("act", lambda e: e.activation(out=qmT2.rearrange("p a t -> p (a t)"), in_=pb4[:, 0:256], func=AF.Copy),
                 [t_ps[4]], [t_qmT2])
            P.op("pool", lambda e: e.tensor_scalar(out=qmT2.rearrange("p a t -> p (a t)"), in0=qmT2.rearrange("p a t -> p (a t)"),
                                                   scalar1=gmq8[:, 0:1], scalar2=1.0, op0=ALU.mult, op1=ALU.mult),
                 [t_qmT2, t_gmq8], [t_qmT2])
            if CCUT <= 3:
                return
            for b in range(2):
                for mb in range(2):
                    for a in range(2):
                        j = mb * 2 + a
                        P.op("pe", lambda e, mb=mb, a=a, b=b, j=j: e.matmul(
                            ps[5 + b][:, j * 128:(j + 1) * 128], lhsT=mkT2[64 * b:64 * b + 64, a, mb * 128:(mb + 1) * 128],
                            rhs=qmT2[64 * b:64 * b + 64, a, :], start=True, stop=True), [t_mkT2, t_qmT2], [t_ps[5 + b]])
                P.op("act", lambda e, b=b: e.activation(out=PTm[:, b, :, :].rearrange("p j t -> p (j t)"),
                                                        in_=ps[5 + b], func=AF.Exp), [t_ps[5 + b]], [t_PTm])
            if CCUT <= 4:
                return
            for mh in range(4):
                a, b = mh // 2, mh % 2
                for mb in range(2):
                    P.op("pe", lambda e, mb=mb, mh=mh, a=a, b=b: e.matmul(
                        ps[7][64 * b:64 * b + 64, a * 128:(a + 1) * 128], lhsT=mv_b[:, mb, mh * 64:(mh + 1) * 64],
                        rhs=PTm[:, b, mb * 2 + a, :], start=(mb == 0), stop=(mb == 1)), [t_mv, t_PTm], [t_ps[7]])
                for mb in range(2):
                    P.op("pe", lambda e, mb=mb, mh=mh, a=a, b=b: e.matmul(
                        ps[7][64 * b:64 * b + 64, 256 + a * 128:256 + (a + 1) * 128], lhsT=onesb[:, 0:64],
                        rhs=PTm[:, b, mb * 2 + a, :], start=(mb == 0), stop=(mb == 1)), [t_onesb, t_PTm], [t_ps[7]])
            if CCUT <= 5:
                return
            P.op("dve", lambda e: e.reciprocal(out=recm, in_=ps[7][:, 256:512]), [t_ps[7]], [t_recm])
            P.op("dve", lambda e: e.tensor_tensor(out=omT2.rearrange("p a t -> p (a t)"), in0=ps[7][:, 0:256], in1=recm,
                                                  op=ALU.mult), [t_ps[7], t_recm], [t_omT2])
            for c in range(2):
                for a in range(2):
                    P.op("pe", lambda e, a=a, c=c: e.matmul(ps[1 + c], lhsT=omT2[:, a, :],
                                                            rhs=Wmo[:, a, c * 512:(c + 1) * 512], start=(a == 0), stop=(a == 1)),
                         [t_omT2, t_Wmo], [t_ps[1 + c]])
                P.op("dve", lambda e, c=c: e.tensor_tensor(out=xx[:, c * 512:(c + 1) * 512], in0=ps[1 + c],
                                                           in1=xx[:, c * 512:(c + 1) * 512], op=ALU.add),
                     [t_ps[1 + c], txx], [txx])
            P.dma("sp", y_p[r0:r0 + 128, :], xx, reads=[txx])

        for t in range(NT):
            c1_tile(t)

    t_yscr = Tok()

    def c2_phase(sq):
        row_base = sq * SEQ
        alloc_stage(2816)
        Wg, t_Wg = sbt("Wg", [128, 8, 2816], BF16)
        Wu, t_Wu = sbt("Wu", [128, 8, 2816], BF16)
        Wd, t_Wd = sbt("Wd", [128, 22, 1024], BF16)
        load_weight(Wg, t_Wg, w_gate, 0, 2816, gF, t_gF)
        load_weight(Wu, t_Wu, w_up, 0, 2816, gF, t_gF)
        for f in range(22):
            load_rows(Wd[:, f, :], t_Wd, w_down[128 * f:128 * f + 128, :], 1024, None, None)
        hT3, t_hT3 = sbt("hT3", [128, 8, 256], BF16)
        hfT, t_hfT = sbt("hfT", [128, 22, 256], BF16)
        sg = [sbt("sg%d" % i, [128, 256]) for i in range(2)]

        def c2_group(gi):
            xs_ = []
            for t2 in range(2):
                r0 = row_base + (gi * 2 + t2) * 128
                x, tx, i = load_x(y_p[r0:r0 + 128, :], 128)
                norm_T(x, tx, i, hT3, t_hT3, t2 * 128, 0)
                xs_.append((x, tx, r0))
            for f in range(22):
                b = 1 + (f % 2)
                for kc in range(8):
                    P.op("pe", lambda e, kc=kc, f=f, b=b: e.matmul(ps[b][:, 0:256], lhsT=Wg[:, kc, 128 * f:128 * f + 128],
                                                                   rhs=hT3[:, kc, :], start=(kc == 0), stop=(kc == 7)),
                         [t_Wg, t_hT3], [t_ps[b]])
                for kc in range(8):
                    P.op("pe", lambda e, kc=kc, f=f, b=b: e.matmul(ps[b][:, 256:512], lhsT=Wu[:, kc, 128 * f:128 * f + 128],
                                                                   rhs=hT3[:, kc, :], start=(kc == 0), stop=(kc == 7)),
                         [t_Wu, t_hT3], [t_ps[b]])
                s_, ts_ = sg[f % 2]
                P.op("act", lambda e, s_=s_, b=b: e.activation(out=s_, in_=ps[b][:, 0:256], func=AF.Silu), [t_ps[b]], [ts_])
                P.op("dve", lambda e, s_=s_, b=b, f=f: e.tensor_tensor(out=hfT[:, f, :], in0=s_, in1=ps[b][:, 256:512],
                                                                       op=ALU.mult), [ts_, t_ps[b]], [t_hfT])
            for t2 in range(2):
                x, tx, r0 = xs_[t2]
                for c in range(2):
                    for f in range(22):
                        P.op("pe", lambda e, f=f, c=c, t2=t2: e.matmul(ps[3 + c], lhsT=hfT[:, f, t2 * 128:(t2 + 1) * 128],
                                                                       rhs=Wd[:, f, c * 512:(c + 1) * 512],
                                                                       start=(f == 0), stop=(f == 21)),
                             [t_hfT, t_Wd], [t_ps[3 + c]])
                    P.op("dve", lambda e, c=c, x=x: e.tensor_tensor(out=x[:, c * 512:(c + 1) * 512], in0=ps[3 + c],
                                                                    in1=x[:, c * 512:(c + 1) * 512], op=ALU.add),
                         [t_ps[3 + c], tx], [tx])
                P.dma("sp", y_p[r0:r0 + 128, :], x, reads=[tx])

        for gi in range(NT // 2):
            c2_group(gi)

    def sample_group():
        k.top = PERSIST_TOP
        NSB = NS
        proj, t_proj = sbt("s_proj", [128, INW])
        x_keep, t_xk = sbt("s_xkeep", [128, D])
        sso, t_sso = sbt("s_o", [128, 320])
        ss_, t_ss = sbt("s_ss", [128, 64])
        scr, t_scr = sbt("s_scr", [128, 1536])
        S_TOP = k.top
        alloc_stage(INW)
        wAll, t_wAll = sbt("s_wAll", [128, 8, INW], BF16)
        load_weight(wAll, t_wAll, w_in, 0, INW, gA, t_gA)
        hTs, t_hTs = sbt("s_hT", [128, 8, 128], BF16)
        x, tx, xi = load_x(xs[:, :], NSB)
        P.op("pool", lambda e: e.tensor_copy(out=x_keep[0:NSB, :], in_=x[0:NSB, :]), [tx], [t_xk])
        norm_T(x, tx, xi, hTs, t_hTs, 0, 0)
        for c in range(7):
            c0 = c * 512
            n = min(512, INW - c0)
            b = 1 + (c % 2)
            for kc in range(8):
                P.op("pe", lambda e, kc=kc, b=b, c0=c0, n=n: e.matmul(
                    ps[b][0:NSB, 0:n], lhsT=hTs[:, kc, 0:NSB], rhs=wAll[:, kc, c0:c0 + n],
                    start=(kc == 0), stop=(kc == 7)), [t_hTs, t_wAll], [t_ps[b]])
            P.op("act", lambda e, b=b, c0=c0, n=n: e.activation(out=proj[0:NSB, c0:c0 + n], in_=ps[b][0:NSB, 0:n],
                                                                func=AF.Copy), [t_ps[b]], [t_proj])
        pj = proj[0:NSB]
        so = sso[0:NSB]
        P.op("dve", lambda e: e.tensor_tensor(out=scr[0:NSB, 0:128], in0=pj[:, 512:640], in1=pj[:, 512:640], op=ALU.mult),
             [t_proj], [t_scr])
        P.op("dve", lambda e: e.tensor_reduce(out=ss_[0:NSB, 0:2], in_=scr[0:NSB, 0:128].rearrange("p (h d) -> p h d", h=2),
                                              axis=AX.X, op=ALU.add), [t_scr], [t_ss])
        P.op("pool", lambda e: e.tensor_scalar(out=ss_[0:NSB, 2:4], in0=ss_[0:NSB, 0:2], scalar1=1.0 / 64, scalar2=EPS,
                                               op0=ALU.mult, op1=ALU.add), [t_ss], [t_ss])
        P.op("pool", lambda e: e.tensor_tensor(out=ss_[0:NSB, 2:4], in0=ss_[0:NSB, 2:4], in1=neghalf[0:NSB, 0:2], op=ALU.pow),
             [t_ss, t_nh], [t_ss])
        P.op("dve", lambda e: e.tensor_tensor(out=so[:, 0:128].rearrange("p (h d) -> p h d", h=2),
                                              in0=pj[:, 512:640].rearrange("p (h d) -> p h d", h=2),
                                              in1=ss_[0:NSB, 2:4].unsqueeze(2).to_broadcast([NSB, 2, 64]), op=ALU.mult),
             [t_proj, t_ss], [t_sso])
        P.op("pool", lambda e: e.tensor_tensor(out=so[:, 0:128].rearrange("p (h d) -> p h d", h=2),
                                               in0=so[:, 0:128].rearrange("p (h d) -> p h d", h=2),
                                               in1=gk_b[0:NSB].unsqueeze(1).to_broadcast([NSB, 2, 64]), op=ALU.mult),
             [t_sso, t_gk], [t_sso])
        P.op("act", lambda e: e.activation(out=so[:, 128:256], in_=pj[:, 640:768], func=AF.Copy), [t_proj], [t_sso])
        P.op("act", lambda e: e.activation(out=so[:, 256:320], in_=pj[:, 1024:1088], func=AF.Copy), [t_proj], [t_sso])
        P.dma("sp", k_s[:, :], so[:, 0:128], reads=[t_sso])
        P.dma("sp", v_s[:, :], so[:, 128:256], reads=[t_sso])
        P.dma("sp", kidx_s[:, :], so[:, 256:320], reads=[t_sso])
        conv_s3 = conv_s.rearrange("(s r) c -> s r c", r=3)
        st_conv3 = st_conv.rearrange("(s r) c -> s r c", r=3)
        P.dma("sp", conv_s3[:, 2, :], pj[:, 1092:2628], reads=[t_proj])
        P.dma("sp", conv_s3[:, 0:2, :], st_conv3[:, 1:3, :])
        P.barrier()
        k.top = S_TOP
        stc, t_stc = sbt("s_stc", [128, 3, 1536])
        cwb, t_cwb = sbt("s_cwb", [128, 4, 1536])
        P.dma("sp", stc[0:NSB], st_conv3, writes=[t_stc])
        P.dma("sp", cwb[0:NSB], conv_w.rearrange("t c -> (t c)").partition_broadcast(NSB), writes=[t_cwb])
        cc, t_cc = sbt("s_cc", [128, 1536])
        c_ = cc[0:NSB]
        sc_ = scr[0:NSB]
        P.op("dve", lambda e: e.tensor_tensor(out=c_, in0=pj[:, 1092:2628], in1=cwb[0:NSB, 3, :], op=ALU.mult),
             [t_proj, t_cwb], [t_cc])
        for j in range(3):
            P.op("dve", lambda e, j=j: e.tensor_tensor(out=sc_, in0=stc[0:NSB, j, :], in1=cwb[0:NSB, j, :], op=ALU.mult),
                 [t_stc, t_cwb], [t_scr])
            P.op("dve", lambda e: e.tensor_tensor(out=c_, in0=c_, in1=sc_, op=ALU.add), [t_cc, t_scr], [t_cc])
        P.op("act", lambda e: e.activation(out=c_, in_=c_, func=AF.Silu), [t_cc], [t_cc])
        P.op("dve", lambda e: e.tensor_tensor(out=sc_[:, 0:1024], in0=c_[:, 0:1024], in1=c_[:, 0:1024], op=ALU.mult),
             [t_cc], [t_scr])
        P.op("dve", lambda e: e.tensor_reduce(out=ss_[0:NSB, 8:16], in_=sc_[:, 0:1024].rearrange("p (h d) -> p h d", h=8),
                                              axis=AX.X, op=ALU.add), [t_scr], [t_ss])
        P.op("pool", lambda e: e.tensor_scalar(out=ss_[0:NSB, 16:24], in0=ss_[0:NSB, 8:16], scalar1=1.0, scalar2=EPS,
                                               op0=ALU.mult, op1=ALU.add), [t_ss], [t_ss])
        P.op("pool", lambda e: e.tensor_tensor(out=ss_[0:NSB, 16:24], in0=ss_[0:NSB, 16:24], in1=neghalf[0:NSB, 0:8], op=ALU.pow),
             [t_ss, t_nh], [t_ss])
        P.op("pool", lambda e: e.tensor_scalar(out=ss_[0:NSB, 16:20], in0=ss_[0:NSB, 16:20], scalar1=float(128.0 ** -0.5),
                                               scalar2=1.0, op0=ALU.mult, op1=ALU.mult), [t_ss], [t_ss])
        P.op("dve", lambda e: e.tensor_tensor(out=c_[:, 0:1024].rearrange("p (h d) -> p h d", h=8),
                                              in0=c_[:, 0:1024].rearrange("p (h d) -> p h d", h=8),
                                              in1=ss_[0:NSB, 16:24].unsqueeze(2).to_broadcast([NSB, 8, 128]), op=ALU.mult),
             [t_cc, t_ss], [t_cc])
        sv = ss_[0:NSB]
        P.op("dve", lambda e: e.tensor_tensor(out=sv[:, 24:28], in0=pj[:, 3140:3144], in1=dtb_b[0:NSB], op=ALU.add),
             [t_proj, t_dtb], [t_ss])
        P.op("dve", lambda e: e.tensor_scalar(out=sv[:, 28:32], in0=sv[:, 24:28], scalar1=-1.0, scalar2=None, op0=ALU.mult),
             [t_ss], [t_ss])
        P.op("dve", lambda e: e.tensor_tensor(out=sv[:, 28:32], in0=sv[:, 28:32], in1=sv[:, 24:28], op=ALU.min), [t_ss], [t_ss])
        P.op("act", lambda e: e.activation(out=sv[:, 28:32], in_=sv[:, 28:32], func=AF.Exp), [t_ss], [t_ss])
        P.op("act", lambda e: e.activation(out=sv[:, 28:32], in_=sv[:, 28:32], func=AF.Ln, bias=1.0, scale=1.0), [t_ss], [t_ss])
        P.op("dve", lambda e: e.scalar_tensor_tensor(out=sv[:, 32:36], in0=sv[:, 24:28], scalar=0.0, in1=sv[:, 28:32],
                                                     op0=ALU.max, op1=ALU.add), [t_ss], [t_ss])
        P.op("dve", lambda e: e.tensor_tensor(out=sv[:, 32:36], in0=sv[:, 32:36], in1=nea_b[0:NSB], op=ALU.mult),
             [t_ss, t_nea], [t_ss])
        P.op("act", lambda e: e.activation(out=sv[:, 36:40], in_=pj[:, 3144:3148], func=AF.Sigmoid), [t_proj], [t_ss])
        P.op("act", lambda e: e.activation(out=sv[:, 40:44], in_=sv[:, 32:36], func=AF.Exp), [t_ss], [t_ss])
        S0, t_S0 = sbt("s_S0", [128, NSB, 4, 128])
        for i in range(NSB):
            P.dma("sp", S0[:, i, :, :], ssm_in[i * 512:(i + 1) * 512, :].rearrange("(h d) v -> d h v", h=4), writes=[t_S0])
        kqT, t_kqT = sbt("s_kqT", [128, 8, NSB])
        for j in range(8):
            b = 1 + (j % 2)
            P.op("pe", lambda e, j=j, b=b: e.transpose(out=ps[b][:, 0:NSB], in_=c_[:, j * 128:(j + 1) * 128],
                                                       identity=identf[0:NSB, 0:NSB]), [t_cc, t_identf], [t_ps[b]])
            P.op("act", lambda e, j=j, b=b: e.activation(out=kqT[:, j, :], in_=ps[b][:, 0:NSB], func=AF.Copy),
                 [t_ps[b]], [t_kqT])
        eye_b, t_eyeb = sbt("s_eyeb", [128, NSB, NSB])
        P.dma("sp", eye_b, eye16_d.partition_broadcast(128), writes=[t_eyeb])
        kqTm, t_kqTm = sbt("s_kqTm", [128, 8, NSB, NSB])
        P.op("pool", lambda e: e.tensor_tensor(out=kqTm, in0=kqT.unsqueeze(2).to_broadcast([128, 8, NSB, NSB]),
                                               in1=eye_b.unsqueeze(1).to_broadcast([128, 8, NSB, NSB]), op=ALU.mult),
             [t_kqT, t_eyeb], [t_kqTm])
        for h in range(4):
            for i in range(NSB):
                P.op("pe", lambda e, h=h, i=i: e.matmul(ps[3][0:NSB, h * 128:(h + 1) * 128], lhsT=kqTm[:, 4 + h, i, :],
                                                        rhs=S0[:, i, h, :], start=(i == 0), stop=(i == NSB - 1)),
                     [t_kqTm, t_S0], [t_ps[3]])
        dl, t_dl = sbt("s_dl", [128, 4, 128])
        d_ = dl[0:NSB]
        bcs = lambda a: a.unsqueeze(2).to_broadcast([NSB, 4, 128])
        P.op("dve", lambda e: e.tensor_tensor(out=d_, in0=ps[3][0:NSB, :].rearrange("p (h v) -> p h v", h=4),
                                              in1=bcs(sv[:, 40:44]), op=ALU.mult), [t_ps[3], t_ss], [t_dl])
        P.op("dve", lambda e: e.tensor_tensor(out=d_, in0=c_[:, 1024:1536].rearrange("p (h v) -> p h v", h=4), in1=d_,
                                              op=ALU.subtract), [t_cc, t_dl], [t_dl])
        P.op("dve", lambda e: e.tensor_tensor(out=d_, in0=d_, in1=bcs(sv[:, 36:40]), op=ALU.mult), [t_dl, t_ss], [t_dl])
        ckm, t_ckm = sbt("s_ckm", [128, NSB, 512])
        P.op("pool", lambda e: e.tensor_tensor(out=ckm[0:NSB], in0=c_[:, 512:1024].unsqueeze(1).to_broadcast([NSB, NSB, 512]),
                                               in1=identf[0:NSB, 0:NSB].unsqueeze(2).to_broadcast([NSB, NSB, 512]), op=ALU.mult),
             [t_cc, t_identf], [t_ckm])
        adg, t_adg = sbt("s_adg", [128, NSB, 4])
        P.op("pool", lambda e: e.tensor_tensor(out=adg[0:NSB], in0=sv[:, 40:44].unsqueeze(1).to_broadcast([NSB, NSB, 4]),
                                               in1=identf[0:NSB, 0:NSB].unsqueeze(2).to_broadcast([NSB, NSB, 4]), op=ALU.mult),
             [t_ss, t_identf], [t_adg])
        P.op("pe", lambda e: e.matmul(ps[4][:, 0:NSB * 4], lhsT=onesf[0:NSB, :], rhs=adg[0:NSB].rearrange("p i h -> p (i h)"),
                                      start=True, stop=True), [t_adg, t_onesf], [t_ps[4]])
        abc, t_abc = sbt("s_abc", [128, NSB * 4])
        P.op("act", lambda e: e.activation(out=abc, in_=ps[4][:, 0:NSB * 4], func=AF.Copy), [t_ps[4]], [t_abc])
        for i in range(NSB):
            b = 5 + (i % 2)
            for h in range(4):
                P.op("pe", lambda e, h=h, i=i, b=b: e.matmul(ps[b][:, h * 128:(h + 1) * 128], lhsT=ckm[0:NSB, i, h * 128:(h + 1) * 128],
                                                             rhs=d_[:, h, :], start=True, stop=True), [t_ckm, t_dl], [t_ps[b]])
            for h in range(4):
                P.op("dve", lambda e, h=h, i=i, b=b: e.scalar_tensor_tensor(
                    out=S0[:, i, h, :], in0=S0[:, i, h, :], scalar=abc[:, i * 4 + h:i * 4 + h + 1],
                    in1=ps[b][:, h * 128:(h + 1) * 128], op0=ALU.mult, op1=ALU.add), [t_S0, t_abc, t_ps[b]], [t_S0])
            P.dma("sp", ssm_s[i * 512:(i + 1) * 512, :].rearrange("(h d) v -> d h v", h=4), S0[:, i, :, :], reads=[t_S0])
        for h in range(4):
            for i in range(NSB):
                P.op("pe", lambda e, h=h, i=i: e.matmul(ps[7][0:NSB, h * 128:(h + 1) * 128], lhsT=kqTm[:, h, i, :],
                                                        rhs=S0[:, i, h, :], start=(i == 0), stop=(i == NSB - 1)),
                     [t_kqTm, t_S0], [t_ps[7]])
        og, t_og = sbt("s_og", [128, 4, 128])
        o_ = og[0:NSB]
        P.op("act", lambda e: e.activation(out=sc_[:, 0:512], in_=ps[7][0:NSB, :], func=AF.Square), [t_ps[7]], [t_scr])
        P.op("dve", lambda e: e.tensor_reduce(out=sv[:, 44:48], in_=sc_[:, 0:512].rearrange("p (h v) -> p h v", h=4),
                                              axis=AX.X, op=ALU.add), [t_scr], [t_ss])
        P.op("pool", lambda e: e.tensor_scalar(out=sv[:, 48:52], in0=sv[:, 44:48], scalar1=1.0 / 128, scalar2=EPS,
                                               op0=ALU.mult, op1=ALU.add), [t_ss], [t_ss])
        P.op("pool", lambda e: e.tensor_tensor(out=sv[:, 48:52], in0=sv[:, 48:52], in1=neghalf[0:NSB, 0:4], op=ALU.pow),
             [t_ss, t_nh], [t_ss])
        P.op("dve", lambda e: e.tensor_tensor(out=o_, in0=ps[7][0:NSB, :].rearrange("p (h v) -> p h v", h=4),
                                              in1=bcs(sv[:, 48:52]), op=ALU.mult), [t_ps[7], t_ss], [t_og])
        P.op("pool", lambda e: e.tensor_tensor(out=o_, in0=o_, in1=ggdn_b[0:NSB].unsqueeze(1).to_broadcast([NSB, 4, 128]),
                                               op=ALU.mult), [t_og, t_ggdn], [t_og])
        P.op("act", lambda e: e.activation(out=sc_[:, 0:512], in_=pj[:, 2628:3140], func=AF.Silu), [t_proj], [t_scr])
        P.op("dve", lambda e: e.tensor_tensor(out=o_.rearrange("p h v -> p (h v)"), in0=o_.rearrange("p h v -> p (h v)"),
                                              in1=sc_[:, 0:512], op=ALU.mult), [t_og, t_scr], [t_og])
        P.op("pool", lambda e: e.tensor_copy(out=pj[:, 1092:1604], in_=o_.rearrange("p h v -> p (h v)")), [t_og], [t_proj])
        P.barrier()
        k.top = S_TOP
        if STOP == -1:
            return
        sample_dsa(proj, t_proj, sso, t_sso)
        sample_tail(proj, t_proj, x_keep, t_xk)

    def sample_dsa(proj, t_proj, sso, t_sso):
        NSB = NS
        pj = proj[0:NSB]
        U32 = mybir.dt.uint32
        selp, t_selp = sbt("s_selp", [128, 8, 128])
        selo, t_selo = sbt("s_selo", [128, NSB, 128])
        P.dma("sp", selp[0:NSB], selpair_d.rearrange("q i p -> i q p"), writes=[t_selp])
        P.dma("sp", selo[0:NSB], selone_d, writes=[t_selo])
        gq_b, t_gqb = bcast_layout("s_gq_b", g_q, 64)
        iota_b, t_iota = bcast_layout("s_iota", iota64_d, 64)
        pt_i, t_pti = sbt("s_pt_i", [128, 64], I32)
        pt_f, t_ptf = sbt("s_pt_f", [128, 64])
        P.dma("sp", pt_i[0:NSB], ptab, writes=[t_pti])
        P.op("dve", lambda e: e.tensor_copy(out=pt_f[0:NSB], in_=pt_i[0:NSB]), [t_pti], [t_ptf])
        ss2, t_ss2 = sbt("s_ss2", [128, 32])
        scr2, t_scr2 = sbt("s_scr2", [128, 512])
        qn, t_qn = sbt("s_qn", [128, 512])
        qiw, t_qiw = sbt("s_qiw", [128, 260])
        sv = ss2[0:NSB]
        P.op("dve", lambda e: e.tensor_tensor(out=scr2[0:NSB], in0=pj[:, 0:512], in1=pj[:, 0:512], op=ALU.mult), [t_proj], [t_scr2])
        P.op("dve", lambda e: e.tensor_reduce(out=sv[:, 0:8], in_=scr2[0:NSB].rearrange("p (h d) -> p h d", h=8), axis=AX.X,
                                              op=ALU.add), [t_scr2], [t_ss2])
        P.op("pool", lambda e: e.tensor_scalar(out=sv[:, 8:16], in0=sv[:, 0:8], scalar1=1.0 / 64, scalar2=EPS, op0=ALU.mult,
                                               op1=ALU.add), [t_ss2], [t_ss2])
        P.op("pool", lambda e: e.tensor_tensor(out=sv[:, 8:16], in0=sv[:, 8:16], in1=neghalf[0:NSB, 0:8], op=ALU.pow),
             [t_ss2, t_nh], [t_ss2])
        P.op("pool", lambda e: e.tensor_scalar(out=sv[:, 8:16], in0=sv[:, 8:16], scalar1=0.125, scalar2=1.0, op0=ALU.mult,
                                               op1=ALU.mult), [t_ss2], [t_ss2])
        q3 = qn[0:NSB].rearrange("p (h d) -> p h d", h=8)
        P.op("dve", lambda e: e.tensor_tensor(out=q3, in0=pj[:, 0:512].rearrange("p (h d) -> p h d", h=8),
                                              in1=sv[:, 8:16].unsqueeze(2).to_broadcast([NSB, 8, 64]), op=ALU.mult),
             [t_proj, t_ss2], [t_qn])
        P.op("pool", lambda e: e.tensor_tensor(out=q3, in0=q3, in1=gq_b[0:NSB].unsqueeze(1).to_broadcast([NSB, 8, 64]),
                                               op=ALU.mult), [t_qn, t_gqb], [t_qn])
        P.op("act", lambda e: e.activation(out=qiw[0:NSB, 0:256], in_=pj[:, 768:1024], func=AF.Copy, scale=0.125), [t_proj], [t_qiw])
        P.op("act", lambda e: e.activation(out=qiw[0:NSB, 256:260], in_=pj[:, 1088:1092], func=AF.Copy, scale=0.5), [t_proj], [t_qiw])
        scores, t_scores = sbt("s_scores", [128, 8200])
        P.op("dve", lambda e: e.tensor_tensor(out=scr2[0:NSB, 0:256].rearrange("p (h d) -> p h d", h=4),
                                              in0=qiw[0:NSB, 0:256].rearrange("p (h d) -> p h d", h=4),
                                              in1=pj[:, 1024:1088].unsqueeze(1).to_broadcast([NSB, 4, 64]), op=ALU.mult),
             [t_qiw, t_proj], [t_scr2])
        P.op("dve", lambda e: e.tensor_reduce(out=sv[:, 16:20], in_=scr2[0:NSB, 0:256].rearrange("p (h d) -> p h d", h=4),
                                              axis=AX.X, op=ALU.add), [t_scr2], [t_ss2])
        P.op("dve", lambda e: e.tensor_scalar(out=sv[:, 16:20], in0=sv[:, 16:20], scalar1=0.0, scalar2=None, op0=ALU.max),
             [t_ss2], [t_ss2])
        P.op("dve", lambda e: e.tensor_tensor(out=sv[:, 16:20], in0=sv[:, 16:20], in1=qiw[0:NSB, 256:260], op=ALU.mult),
             [t_ss2, t_qiw], [t_ss2])
        P.op("dve", lambda e: e.tensor_reduce(out=scores[0:NSB, 8192:8193], in_=sv[:, 16:20], axis=AX.X, op=ALU.add),
             [t_ss2], [t_scores])
        osT, t_osT = sbt("s_osT", [128, NSB, 8], BF16)
        K_TOP = k.top
        k.K_TOP = K_TOP
        kid = [sbt("s_kid%d" % i, [128, 8192]) for i in range(2)]
        prod, t_prod = sbt("s_prod", [128, 8192])
        ptc = [sbt("s_ptc%d" % i, [128, 1], I32) for i in range(2)]
        qrep, t_qrep = sbt("s_qrep", [128, 260])
        zz, t_zz = sbt("s_zz", [128, 128])
        sc1, t_sc1 = sbt("s_sc1", [128, 128])
        sc2 = [sbt("s_sc2_%d" % i, [128, 128]) for i in range(2)]
        kidx_pages = cache_kidx_d

        def pair(q):
            kd, tkd = kid[q % 2]
            pc, tpc = ptc[q % 2]
            P.dma("sp", pc, ptab[2 * q:2 * q + 2, :].rearrange("s (j o) -> (s j) o", o=1), writes=[tpc])
            P.dma("pool", kd, kidx_pages, reads=[tpc], writes=[tkd],
                  indirect=bass.IndirectOffsetOnAxis(ap=pc, axis=0))
            P.op("pe", lambda e: e.matmul(ps[1][:, 0:260], lhsT=selp[0:NSB, q, :], rhs=qiw[0:NSB, :], start=True, stop=True),
                 [t_selp, t_qiw], [t_ps[1]])
            P.op("act", lambda e: e.activation(out=qrep, in_=ps[1][:, 0:260], func=AF.Copy), [t_ps[1]], [t_qrep])
            so_, tso = sc2[q % 2]
            for h in range(4):
                P.op("pool", lambda e, h=h: e.tensor_tensor(
                    out=prod.rearrange("p (o d) -> p o d", d=64), in0=kd.rearrange("p (o d) -> p o d", d=64),
                    in1=qrep[:, h * 64:(h + 1) * 64].unsqueeze(1).to_broadcast([128, 128, 64]), op=ALU.mult),
                    [tkd, t_qrep], [t_prod])
                P.op("dve", lambda e: e.tensor_reduce(out=zz, in_=prod.rearrange("p (o d) -> p o d", d=64), axis=AX.X,
                                                      op=ALU.add), [t_prod], [t_zz])
                if h == 0:
                    P.op("dve", lambda e: e.tensor_scalar(out=so_, in0=zz, scalar1=0.0, scalar2=qrep[:, 256:257],
                                                          op0=ALU.max, op1=ALU.mult), [t_zz, t_qrep], [tso])
                else:
                    P.op("dve", lambda e, h=h: e.tensor_scalar(out=sc1, in0=zz, scalar1=0.0, scalar2=qrep[:, 256 + h:257 + h],
                                                               op0=ALU.max, op1=ALU.mult), [t_zz, t_qrep], [t_sc1])
                    P.op("dve", lambda e: e.tensor_tensor(out=so_, in0=so_, in1=sc1, op=ALU.add), [tso, t_sc1], [tso])
            for s2 in range(2):
                r = 2 * q + s2
                P.dma("sp", scores[r:r + 1, 0:8192].rearrange("p (j o) -> p j o", o=128), so_[64 * s2:64 * s2 + 64, :],
                      reads=[tso], writes=[t_scores])

        for q in range(NSB // 2):
            pair(q)
        P.barrier()
        k.top = K_TOP
        mx, t_mx = sbt("s_mx", [128, 256])
        ix, t_ix = sbt("s_ix", [128, 256], U32)
        TK_TOP = k.top
        W, t_W = sbt("s_W", [128, 8200])
        Wv = W[0:NSB, 0:8193]
        P.op("pool", lambda e: e.tensor_copy(out=Wv, in_=scores[0:NSB, 0:8193]), [t_scores], [t_W])
        for r in range(32):
            P.op("dve", lambda e, r=r: e.max(out=mx[0:NSB, 8 * r:8 * r + 8], in_=Wv), [t_W], [t_mx])
            P.op("dve", lambda e, r=r: e.max_index(out=ix[0:NSB, 8 * r:8 * r + 8], in_max=mx[0:NSB, 8 * r:8 * r + 8],
                                                   in_values=Wv), [t_W, t_mx], [t_ix])
            P.op("dve", lambda e, r=r: e.match_replace(out=Wv, in_to_replace=mx[0:NSB, 8 * r:8 * r + 8], in_values=Wv,
                                                       imm_value=-1e30), [t_W, t_mx], [t_W])
        P.barrier()
        k.top = TK_TOP
        ixf, t_ixf = sbt("s_ixf", [128, 256])
        pgu, t_pgu = sbt("s_pgu", [128, 256], U32)
        pgf, t_pgf = sbt("s_pgf", [128, 256])
        offf, t_offf = sbt("s_offf", [128, 256])
        eq, t_eq = sbt("s_eq", [128, 256, 64])
        phys, t_phys = sbt("s_phys", [128, 256])
        isf, t_isf = sbt("s_isf", [128, 256])
        n_ = lambda a: a[0:NSB]
        P.op("dve", lambda e: e.tensor_copy(out=n_(ixf), in_=n_(ix)), [t_ix], [t_ixf])
        P.op("dve", lambda e: e.tensor_scalar(out=n_(pgu), in0=n_(ix), scalar1=7, scalar2=None, op0=ALU.logical_shift_right),
             [t_ix], [t_pgu])
        P.op("dve", lambda e: e.tensor_copy(out=n_(pgf), in_=n_(pgu)), [t_pgu], [t_pgf])
        P.op("dve", lambda e: e.scalar_tensor_tensor(out=n_(offf), in0=n_(pgf), scalar=-128.0, in1=n_(ixf), op0=ALU.mult,
                                                     op1=ALU.add), [t_pgf, t_ixf], [t_offf])
        P.op("dve", lambda e: e.tensor_tensor(out=n_(eq), in0=n_(pgf).unsqueeze(2).to_broadcast([NSB, 256, 64]),
                                              in1=n_(iota_b).unsqueeze(1).to_broadcast([NSB, 256, 64]), op=ALU.is_equal),
             [t_pgf, t_iota], [t_eq])
        P.op("dve", lambda e: e.tensor_tensor(out=n_(eq), in0=n_(eq), in1=n_(pt_f).unsqueeze(1).to_broadcast([NSB, 256, 64]),
                                              op=ALU.mult), [t_eq, t_ptf], [t_eq])
        P.op("dve", lambda e: e.tensor_reduce(out=n_(phys), in_=n_(eq), axis=AX.X, op=ALU.add), [t_eq], [t_phys])
        P.op("dve", lambda e: e.scalar_tensor_tensor(out=n_(phys), in0=n_(phys), scalar=128.0, in1=n_(offf), op0=ALU.mult,
                                                     op1=ALU.add), [t_phys, t_offf], [t_phys])
        P.op("dve", lambda e: e.tensor_scalar(out=n_(isf), in0=n_(ixf), scalar1=8191.5, scalar2=None, op0=ALU.is_ge),
             [t_ixf], [t_isf])
        physT, t_physT = sbt("s_physT", [128, 2, NSB], I32)
        isT, t_isT = sbt("s_isT", [128, 2, NSB])
        for b in range(2):
            P.op("pe", lambda e, b=b: e.transpose(out=ps[1][:, b * NSB:(b + 1) * NSB], in_=phys[0:NSB, b * 128:(b + 1) * 128],
                                                  identity=identf[0:NSB, 0:NSB]), [t_phys, t_identf], [t_ps[1]])
            P.op("pe", lambda e, b=b: e.transpose(out=ps[2][:, b * NSB:(b + 1) * NSB], in_=isf[0:NSB, b * 128:(b + 1) * 128],
                                                  identity=identf[0:NSB, 0:NSB]), [t_isf, t_identf], [t_ps[2]])
        P.op("dve", lambda e: e.tensor_copy(out=physT.rearrange("p b i -> p (b i)"), in_=ps[1][:, 0:2 * NSB]), [t_ps[1]], [t_physT])
        P.op("act", lambda e: e.activation(out=isT.rearrange("p b i -> p (b i)"), in_=ps[2][:, 0:2 * NSB], func=AF.Copy),
             [t_ps[2]], [t_isT])
        Kg = [sbt("s_Kg%d" % i, [128, 2, 128]) for i in range(2)]
        Vg = [sbt("s_Vg%d" % i, [128, 2, 128]) for i in range(2)]
        kvrep, t_kvrep = sbt("s_kvrep", [128, 256])
        dif, t_dif = sbt("s_dif", [128, 128])
        prd, t_prd = sbt("s_prd", [128, 512])
        lg, t_lg = sbt("s_lg", [128, 2, 8])
        rcp, t_rcp = sbt("s_rcp", [128, NSB * 8])

        def att(i):
            kg, tkg = Kg[i % 2]
            vg, tvg = Vg[i % 2]
            for b in range(2):
                P.dma("pool", kg[:, b, :], cache_k_d, reads=[t_physT], writes=[tkg],
                      indirect=bass.IndirectOffsetOnAxis(ap=physT[:, b, i:i + 1], axis=0))
                P.dma("pool", vg[:, b, :], cache_v_d, reads=[t_physT], writes=[tvg],
                      indirect=bass.IndirectOffsetOnAxis(ap=physT[:, b, i:i + 1], axis=0))
            P.op("pe", lambda e: e.matmul(ps[3], lhsT=selo[0:NSB, i, :], rhs=qn[0:NSB, :], start=True, stop=True),
                 [t_selo, t_qn], [t_ps[3]])
            P.op("pe", lambda e: e.matmul(ps[4][:, 0:256], lhsT=selo[0:NSB, i, :], rhs=sso[0:NSB, 0:256], start=True, stop=True),
                 [t_selo, t_sso], [t_ps[4]])
            P.op("act", lambda e: e.activation(out=kvrep, in_=ps[4][:, 0:256], func=AF.Copy), [t_ps[4]], [t_kvrep])
            for b in range(2):
                for (t_, tt_, c0) in ((kg, tkg, 0), (vg, tvg, 128)):
                    P.op("dve", lambda e, t_=t_, c0=c0, b=b: e.tensor_tensor(out=dif, in0=kvrep[:, c0:c0 + 128], in1=t_[:, b, :],
                                                                             op=ALU.subtract), [t_kvrep, tt_], [t_dif])
                    P.op("dve", lambda e, t_=t_, b=b: e.scalar_tensor_tensor(out=t_[:, b, :], in0=dif, scalar=isT[:, b, i:i + 1],
                                                                             in1=t_[:, b, :], op0=ALU.mult, op1=ALU.add),
                         [t_dif, t_isT, tt_], [tt_])
                P.op("dve", lambda e, b=b: e.tensor_tensor(
                    out=prd.rearrange("p (g r d) -> p g r d", g=2, r=4),
                    in0=ps[3].rearrange("p (g r d) -> p g r d", g=2, r=4),
                    in1=kg[:, b, :].rearrange("p (g d) -> p g d", g=2).unsqueeze(2).to_broadcast([128, 2, 4, 64]),
                    op=ALU.mult), [t_ps[3], tkg], [t_prd])
                P.op("dve", lambda e, b=b: e.tensor_reduce(out=lg[:, b, :], in_=prd.rearrange("p (h d) -> p h d", h=8),
                                                           axis=AX.X, op=ALU.add), [t_prd], [t_lg])
            P.op("act", lambda e: e.activation(out=lg, in_=lg, func=AF.Exp), [t_lg], [t_lg])
            for g in range(2):
                for b in range(2):
                    P.op("pe", lambda e, g=g, b=b: e.matmul(ps[5][0:64, i * 8 + g * 4:i * 8 + g * 4 + 4],
                                                            lhsT=vg[:, b, g * 64:(g + 1) * 64], rhs=lg[:, b, g * 4:(g + 1) * 4],
                                                            start=(b == 0), stop=(b == 1)), [tvg, t_lg], [t_ps[5]])
                for b in range(2):
                    P.op("pe", lambda e, g=g, b=b: e.matmul(ps[6][0:64, i * 8 + g * 4:i * 8 + g * 4 + 4],
                                                            lhsT=onesf[:, 0:64], rhs=lg[:, b, g * 4:(g + 1) * 4],
                                                            start=(b == 0), stop=(b == 1)), [t_onesf, t_lg], [t_ps[6]])

        for i in range(NSB):
            att(i)
        P.op("dve", lambda e: e.reciprocal(out=rcp[0:64], in_=ps[6][0:64, 0:NSB * 8]), [t_ps[6]], [t_rcp])
        P.op("dve", lambda e: e.tensor_tensor(out=osT[0:64].rearrange("p i h -> p (i h)"), in0=ps[5][0:64, 0:NSB * 8],
                                              in1=rcp[0:64], op=ALU.mult), [t_ps[5], t_rcp], [t_osT])
        k.osT = (osT, t_osT)
        k.selo = (selo, t_selo)
        k.S2_TOP = k.top
        P.barrier()

    def sample_tail(proj, t_proj, x_keep, t_xk):
        NSB = NS
        pj = proj[0:NSB]
        osT, t_osT = k.osT
        selo, t_selo = k.selo
        k.top = k.K_TOP
        alloc_stage(1024)
        Wo_s, t_Wos = sbt("t_Wo_s", [128, 8, 1024], BF16)
        Wo_g, t_Wog = sbt("t_Wo_g", [128, 4, 1024], BF16)
        Wmq, t_Wmq = sbt("t_Wmq", [128, 8, 256], BF16)
        Wmo, t_Wmo = sbt("t_Wmo", [128, 4, 1024], BF16)
        for h in range(8):
            i = k.wl % 2
            k.wl += 1
            st = stage[i][0:64, 0:1024]
            P.dma("sp", st, w_out[64 * h:64 * h + 64, :], writes=[t_stage[i]])
            P.op("act", lambda e, st=st, h=h: e.activation(out=Wo_s[0:64, h, :], in_=st, func=AF.Copy), [t_stage[i]], [t_Wos])
        for h in range(4):
            load_rows(Wo_g[:, h, :], t_Wog, w_out[512 + 128 * h:512 + 128 * h + 128, :], 1024, None, None)
        load_weight(Wmq, t_Wmq, w_mq, 0, 256, gX, t_gX)
        for h in range(4):
            i = k.wl % 2
            k.wl += 1
            st = stage[i][0:64, 0:1024]
            P.dma("sp", st, w_mo[64 * h:64 * h + 64, :], writes=[t_stage[i]])
            P.op("act", lambda e, st=st, h=h: e.activation(out=Wmo[0:64, h, :], in_=st, func=AF.Copy), [t_stage[i]], [t_Wmo])
        gmq_b, t_gmqb = bcast_layout("t_gmq_b", g_mq, 64)
        ogT, t_ogT = sbt("t_ogT", [128, 4, NSB], BF16)
        for h in range(4):
            P.op("pe", lambda e, h=h: e.transpose(out=ps[1][:, h * NSB:(h + 1) * NSB], in_=pj[:, 1092 + h * 128:1092 + (h + 1) * 128],
                                                  identity=identf[0:NSB, 0:NSB]), [t_proj, t_identf], [t_ps[1]])
        P.op("act", lambda e: e.activation(out=ogT.rearrange("p h i -> p (h i)"), in_=ps[1][:, 0:4 * NSB], func=AF.Copy),
             [t_ps[1]], [t_ogT])
        x1, t_x1 = sbt("t_x1", [128, D])
        P.op("pool", lambda e: e.memset(x1, 0.0), [], [t_x1])
        for c in range(2):
            for h in range(8):
                P.op("pe", lambda e, h=h, c=c: e.matmul(ps[2 + c][0:NSB, :], lhsT=osT[0:64, :, h], rhs=Wo_s[0:64, h, c * 512:(c + 1) * 512],
                                                        start=(h == 0), stop=False), [t_osT, t_Wos], [t_ps[2 + c]])
            for h in range(4):
                P.op("pe", lambda e, h=h, c=c: e.matmul(ps[2 + c][0:NSB, :], lhsT=ogT[:, h, :], rhs=Wo_g[:, h, c * 512:(c + 1) * 512],
                                                        start=False, stop=(h == 3)), [t_ogT, t_Wog], [t_ps[2 + c]])
            P.op("dve", lambda e, c=c: e.tensor_tensor(out=x1[0:NSB, c * 512:(c + 1) * 512], in0=ps[2 + c][0:NSB, :],
                                                       in1=x_keep[0:NSB, c * 512:(c + 1) * 512], op=ALU.add),
                 [t_ps[2 + c], t_xk], [t_x1])
        hT2, t_hT2 = sbt("t_hT2", [128, 8, 128], BF16)
        norm_T(x1, t_x1, 0, hT2, t_hT2, 0, 0)
        for kc in range(8):
            P.op("pe", lambda e, kc=kc: e.matmul(ps[4][0:NSB, 0:256], lhsT=hT2[:, kc, 0:NSB], rhs=Wmq[:, kc, :],
                                                 start=(kc == 0), stop=(kc == 7)), [t_hT2, t_Wmq], [t_ps[4]])
        qm, t_qm = sbt("t_qm", [128, 256])
        sq2, t_sq2 = sbt("t_sq2", [128, 256])
        st2, t_st2 = sbt("t_st2", [128, 16])
        P.op("act", lambda e: e.activation(out=sq2[0:NSB], in_=ps[4][0:NSB, 0:256], func=AF.Square), [t_ps[4]], [t_sq2])
        P.op("dve", lambda e: e.tensor_reduce(out=st2[0:NSB, 0:4], in_=sq2[0:NSB].rearrange("p (h d) -> p h d", h=4), axis=AX.X,
                                              op=ALU.add), [t_sq2], [t_st2])
        P.op("pool", lambda e: e.tensor_scalar(out=st2[0:NSB, 4:8], in0=st2[0:NSB, 0:4], scalar1=1.0 / 64, scalar2=EPS,
                                               op0=ALU.mult, op1=ALU.add), [t_st2], [t_st2])
        P.op("pool", lambda e: e.tensor_tensor(out=st2[0:NSB, 4:8], in0=st2[0:NSB, 4:8], in1=neghalf[0:NSB, 0:4], op=ALU.pow),
             [t_st2, t_nh], [t_st2])
        P.op("pool", lambda e: e.tensor_scalar(out=st2[0:NSB, 4:8], in0=st2[0:NSB, 4:8], scalar1=0.125, scalar2=1.0,
                                               op0=ALU.mult, op1=ALU.mult), [t_st2], [t_st2])
        qm3 = qm[0:NSB].rearrange("p (h d) -> p h d", h=4)
        P.op("dve", lambda e: e.tensor_tensor(out=qm3, in0=ps[4][0:NSB, 0:256].rearrange("p (h d) -> p h d", h=4),
                                              in1=st2[0:NSB, 4:8].unsqueeze(2).to_broadcast([NSB, 4, 64]), op=ALU.mult),
             [t_ps[4], t_st2], [t_qm])
        P.op("pool", lambda e: e.tensor_tensor(out=qm3, in0=qm3, in1=gmq_b[0:NSB].unsqueeze(1).to_broadcast([NSB, 4, 64]),
                                               op=ALU.mult), [t_qm, t_gmqb], [t_qm])
        mkt = [sbt("t_mk%d" % i, [128, 2, 256]) for i in range(2)]
        mvt = [sbt("t_mv%d" % i, [128, 2, 256]) for i in range(2)]
        prm, t_prm = sbt("t_prm", [128, 256])
        lgm, t_lgm = sbt("t_lgm", [128, 2, 4])
        omT, t_omT = sbt("t_omT", [128, NSB, 4], BF16)
        rcm, t_rcm = sbt("t_rcm", [128, NSB * 4])

        def xatt(i):
            mk_, tmk = mkt[i % 2]
            mv_, tmv = mvt[i % 2]
            P.dma("sp", mk_, cmk_d[i * 256:(i + 1) * 256, :].rearrange("(t p) c -> p t c", p=128), writes=[tmk])
            P.dma("sp", mv_, cmv_d[i * 256:(i + 1) * 256, :].rearrange("(t p) c -> p t c", p=128), writes=[tmv])
            P.op("pe", lambda e: e.matmul(ps[5][:, 0:256], lhsT=selo[0:NSB, i, :], rhs=qm[0:NSB, :], start=True, stop=True),
                 [t_selo, t_qm], [t_ps[5]])
            for mt in range(2):
                P.op("dve", lambda e, mt=mt: e.tensor_tensor(out=prm, in0=ps[5][:, 0:256], in1=mk_[:, mt, :], op=ALU.mult),
                     [t_ps[5], tmk], [t_prm])
                P.op("dve", lambda e, mt=mt: e.tensor_reduce(out=lgm[:, mt, :], in_=prm.rearrange("p (h d) -> p h d", h=4),
                                                             axis=AX.X, op=ALU.add), [t_prm], [t_lgm])
            P.op("act", lambda e: e.activation(out=lgm, in_=lgm, func=AF.Exp), [t_lgm], [t_lgm])
            for h in range(4):
                for mt in range(2):
                    P.op("pe", lambda e, h=h, mt=mt: e.matmul(ps[6][0:64, i * 4 + h:i * 4 + h + 1], lhsT=mv_[:, mt, h * 64:(h + 1) * 64],
                                                              rhs=lgm[:, mt, h:h + 1], start=(mt == 0), stop=(mt == 1)),
                         [tmv, t_lgm], [t_ps[6]])
                for mt in range(2):
                    P.op("pe", lambda e, h=h, mt=mt: e.matmul(ps[7][0:64, i * 4 + h:i * 4 + h + 1], lhsT=onesf[:, 0:64],
                                                              rhs=lgm[:, mt, h:h + 1], start=(mt == 0), stop=(mt == 1)),
                         [t_onesf, t_lgm], [t_ps[7]])

        for i in range(NSB):
            xatt(i)
        P.op("dve", lambda e: e.reciprocal(out=rcm[0:64], in_=ps[7][0:64, 0:NSB * 4]), [t_ps[7]], [t_rcm])
        P.op("dve", lambda e: e.tensor_tensor(out=omT[0:64].rearrange("p i h -> p (i h)"), in0=ps[6][0:64, 0:NSB * 4],
                                              in1=rcm[0:64], op=ALU.mult), [t_ps[6], t_rcm], [t_omT])
        for c in range(2):
            for h in range(4):
                P.op("pe", lambda e, h=h, c=c: e.matmul(ps[2 + c][0:NSB, :], lhsT=omT[0:64, :, h], rhs=Wmo[0:64, h, c * 512:(c + 1) * 512],
                                                        start=(h == 0), stop=(h == 3)), [t_omT, t_Wmo], [t_ps[2 + c]])
            P.op("dve", lambda e, c=c: e.tensor_tensor(out=x1[0:NSB, c * 512:(c + 1) * 512], in0=ps[2 + c][0:NSB, :],
                                                       in1=x1[0:NSB, c * 512:(c + 1) * 512], op=ALU.add),
                 [t_ps[2 + c], t_x1], [t_x1])
        P.op("pool", lambda e: e.tensor_copy(out=x_keep[0:NSB, :], in_=x1[0:NSB, :]), [t_x1], [t_xk])
        hT3, t_hT3 = sbt("t_hT3", [128, 8, 128], BF16)
        norm_T(x1, t_x1, 1, hT3, t_hT3, 0, 0)
        P.barrier()
        k.top = PERSIST_TOP
        xk2, t_xk2 = sbt("t_xk2", [128, D])
        hT4, t_hT4 = sbt("t_hT4", [128, 8, NSB], BF16)
        P.op("pool", lambda e: e.tensor_copy(out=xk2[0:NSB, :], in_=x_keep[0:NSB, :]), [t_xk], [t_xk2])
        P.op("pool", lambda e: e.tensor_copy(out=hT4, in_=hT3[:, :, 0:NSB]), [t_hT3], [t_hT4])
        P.barrier()
        alloc_stage(2816)
        Wg, t_Wg = sbt("t_Wg", [128, 8, 2816], BF16)
        Wu, t_Wu = sbt("t_Wu", [128, 8, 2816], BF16)
        Wd, t_Wd = sbt("t_Wd", [128, 22, 1024], BF16)
        load_weight(Wg, t_Wg, w_gate, 0, 2816, gF, t_gF)
        load_weight(Wu, t_Wu, w_up, 0, 2816, gF, t_gF)
        for f in range(22):
            load_rows(Wd[:, f, :], t_Wd, w_down[128 * f:128 * f + 128, :], 1024, None, None)
        hf, t_hf = sbt("t_hf", [128, 2816])
        sgs, t_sgs = sbt("t_sgs", [128, 512])
        hfT, t_hfT = sbt("t_hfT", [128, 22, NSB], BF16)
        for c in range(6):
            c0 = c * 512
            n = min(512, 2816 - c0)
            for kc in range(8):
                P.op("pe", lambda e, kc=kc, c0=c0, n=n: e.matmul(ps[1][0:NSB, 0:n], lhsT=hT4[:, kc, :], rhs=Wg[:, kc, c0:c0 + n],
                                                                 start=(kc == 0), stop=(kc == 7)), [t_hT4, t_Wg], [t_ps[1]])
            for kc in range(8):
                P.op("pe", lambda e, kc=kc, c0=c0, n=n: e.matmul(ps[2][0:NSB, 0:n], lhsT=hT4[:, kc, :], rhs=Wu[:, kc, c0:c0 + n],
                                                                 start=(kc == 0), stop=(kc == 7)), [t_hT4, t_Wu], [t_ps[2]])
            P.op("act", lambda e, n=n: e.activation(out=sgs[0:NSB, 0:n], in_=ps[1][0:NSB, 0:n], func=AF.Silu), [t_ps[1]], [t_sgs])
            P.op("dve", lambda e, c0=c0, n=n: e.tensor_tensor(out=hf[0:NSB, c0:c0 + n], in0=sgs[0:NSB, 0:n], in1=ps[2][0:NSB, 0:n],
                                                              op=ALU.mult), [t_sgs, t_ps[2]], [t_hf])
        for f in range(22):
            b = 3 + (f % 2)
            P.op("pe", lambda e, f=f, b=b: e.transpose(out=ps[b][:, 0:NSB], in_=hf[0:NSB, f * 128:(f + 1) * 128],
                                                       identity=identf[0:NSB, 0:NSB]), [t_hf, t_identf], [t_ps[b]])
            P.op("act", lambda e, f=f, b=b: e.activation(out=hfT[:, f, :], in_=ps[b][:, 0:NSB], func=AF.Copy), [t_ps[b]], [t_hfT])
        for c in range(2):
            for f in range(22):
                P.op("pe", lambda e, f=f, c=c: e.matmul(ps[5 + c][0:NSB, :], lhsT=hfT[:, f, :], rhs=Wd[:, f, c * 512:(c + 1) * 512],
                                                        start=(f == 0), stop=(f == 21)), [t_hfT, t_Wd], [t_ps[5 + c]])
            P.op("dve", lambda e, c=c: e.tensor_tensor(out=xk2[0:NSB, c * 512:(c + 1) * 512], in0=ps[5 + c][0:NSB, :],
                                                       in1=xk2[0:NSB, c * 512:(c + 1) * 512], op=ALU.add),
                 [t_ps[5 + c], t_xk2], [t_xk2])
        P.dma("sp", y_s[:, :], xk2[0:NSB, :], reads=[t_xk2])

    if STOP >= 0:
        for sq in range(NSEQ):
            prompt_seq(sq)
    if STOP < 0 or STOP >= 99:
        sample_group()

    P.finish()
    P.emit()
    return nc


_CACHE = {}


def _get_nc(nseq, stop):
    key = (nseq, stop)
    if key not in _CACHE:
        _CACHE[key] = build(nseq, STOP=stop)
    return _CACHE[key]


def kernel(x_prompt, x_sample, mem_prompt, cache_k, cache_v, cache_kidx, page_table,
           state_conv, state_ssm, cache_mem_k, cache_mem_v,
           attn_norm_g, w_in, q_norm_g, k_norm_g, conv_w, a_log, dt_bias, gdn_norm_g, w_out,
           xattn_norm_g, mem_norm_g, w_mq, w_mk, w_mv, mq_norm_g, mk_norm_g, w_mo,
           ffn_norm_g, w_gate, w_up, w_down, _ncores=NCORES, _stop=99):
    B = x_prompt.shape[0]
    nseq = B // _ncores
    NS = x_sample.shape[0] // _ncores
    nc = _get_nc(nseq, _stop)
    f = lambda a: np.ascontiguousarray(np.asarray(a, dtype=np.float32))
    ii = np.arange(128)
    consts = {
        "ident": np.eye(128, dtype=np.float32),
        "trile": (ii[:, None] <= ii[None, :]).astype(np.float32),
        "sgt": (ii[:, None] > ii[None, :]).astype(np.float32),
        "pow2": (2.0 ** -np.arange(32)).astype(np.float32),
        "eye16": np.eye(16, dtype=np.float32).reshape(256),
        "selpair": np.stack([(np.arange(16)[:, None] == (2 * q_ + np.arange(128)[None, :] // 64)).astype(np.float32)
                             for q_ in range(8)]),
        "selone": np.stack([np.repeat((np.arange(16) == i_)[:, None], 128, axis=1).astype(np.float32)
                            for i_ in range(16)], axis=1),
        "iota64": np.arange(64, dtype=np.float32),
    }
    shared = {
        "attn_norm_g": f(attn_norm_g[0]), "w_in": f(w_in[0]),
        "q_norm_g": f(q_norm_g[0]), "k_norm_g": f(k_norm_g[0]),
        "conv_w": f(conv_w[0]), "a_log": f(a_log[0]), "dt_bias": f(dt_bias[0]),
        "gdn_norm_g": f(gdn_norm_g[0]), "w_out": f(w_out[0]), "xattn_norm_g": f(xattn_norm_g[0]),
        "mem_norm_g": f(mem_norm_g[0]), "w_mq": f(w_mq[0]), "w_mk": f(w_mk[0]), "w_mv": f(w_mv[0]),
        "mq_norm_g": f(mq_norm_g[0]), "mk_norm_g": f(mk_norm_g[0]), "w_mo": f(w_mo[0]),
        "ffn_norm_g": f(ffn_norm_g[0]), "w_gate": f(w_gate[0]), "w_up": f(w_up[0]), "w_down": f(w_down[0]),
    }
    npool = cache_k.shape[1]
    ck_k = f(cache_k[0]).reshape(npool * 128, 128)
    ck_v = f(cache_v[0]).reshape(npool * 128, 128)
    ck_idx = f(cache_kidx[0]).reshape(npool, 8192)
    in_maps = []
    for c in range(_ncores):
        m = {
            "xp": f(x_prompt[c * nseq:(c + 1) * nseq]).reshape(nseq * SEQ, D),
            "memp": f(mem_prompt[c * nseq:(c + 1) * nseq]).reshape(nseq * MEM, D),
            "xs": f(x_sample[c * NS:(c + 1) * NS]).reshape(NS, D),
            "st_conv": f(state_conv[0, c * NS:(c + 1) * NS]).reshape(NS * 3, 1536),
            "ssm_in": f(state_ssm[0, c * NS:(c + 1) * NS]).reshape(NS * 512, 128),
            "ptab": np.ascontiguousarray(np.asarray(page_table[c * NS:(c + 1) * NS], dtype=np.int32)),
            "cmk": f(cache_mem_k[0, c * NS:(c + 1) * NS]).reshape(NS * 256, 256),
            "cmv": f(cache_mem_v[0, c * NS:(c + 1) * NS]).reshape(NS * 256, 256),
            "cache_kidx": ck_idx, "cache_k": ck_k, "cache_v": ck_v,
        }
        m.update(consts)
        m.update(shared)
        in_maps.append(m)
    res = run_bass_kernel_spmd(nc, in_maps, core_ids=list(range(_ncores))).results
    cat = lambda name: np.concatenate([r[name] for r in res], axis=0)
    SB = x_sample.shape[0]
    outs = (
        cat("y_p").reshape(B, SEQ, D),
        cat("y_s").reshape(SB, 1, D),
        cat("k_p").reshape(1, B, SEQ, 2, 64),
        cat("v_p").reshape(1, B, SEQ, 2, 64),
        cat("kidx_p").reshape(1, B, SEQ, 64),
        cat("conv_p").reshape(1, B, 3, 1536),
        cat("ssm_p").reshape(1, B, 4, 128, 128),
        cat("memk_p").reshape(1, B, MEM, 4, 64),
        cat("memv_p").reshape(1, B, MEM, 4, 64),
        cat("k_s").reshape(1, SB, 1, 2, 64),
        cat("v_s").reshape(1, SB, 1, 2, 64),
        cat("kidx_s").reshape(1, SB, 1, 64),
        cat("conv_s").reshape(1, SB, 3, 1536),
        cat("ssm_s").reshape(1, SB, 4, 128, 128),
    )
    return outs
```

```python
import os
import numpy as np
import concourse.bass as bass
import concourse.mybir as mybir
from concourse.bass_utils import run_bass_kernel_spmd

F32 = mybir.dt.float32
BF16 = mybir.dt.bfloat16
I32 = mybir.dt.int32
AF = mybir.ActivationFunctionType
ALU = mybir.AluOpType
AX = mybir.AxisListType

NCORES = 8
D = 1024
SEQ = 2048
NT = SEQ // 128
MEM = 256
INW = 3148
EPS = 1e-6
NDS = 32


class Tok:
    __slots__ = ("w", "r")

    def __init__(self):
        self.w = None
        self.r = {}


class Prog:
    def __init__(self, nc):
        self.nc = nc
        self.names = ["pe", "act", "dve", "pool", "sp"]
        self.sems = []
        self.esem = {}
        for k in self.names:
            self.esem[k] = len(self.sems)
            self.sems.append(nc.alloc_semaphore("es_" + k))
        self.dsem = []
        for i in range(NDS):
            self.dsem.append(len(self.sems))
            self.sems.append(nc.alloc_semaphore("ds_%d" % i))
        self.dval = [0] * NDS
        self.dnext = 0
        self.dnext_sw = 0
        self.cnt = {k: 0 for k in self.names}
        self.seen = {k: {} for k in self.names}
        self.th = {k: [] for k in self.names}

    def _deps(self, e, reads, writes, extra=()):
        d = {}

        def add(s, v):
            if d.get(s, 0) < v:
                d[s] = v

        for t in reads:
            if t.w is not None:
                add(*t.w)
        for t in writes:
            if t.w is not None:
                add(*t.w)
            for s, v in t.r.items():
                add(s, v)
        for s, v in extra:
            add(s, v)
        out = []
        for s, v in d.items():
            if e == "pe" and s == self.esem["pe"]:
                continue
            if self.seen[e].get(s, 0) >= v:
                continue
            self.seen[e][s] = v
            out.append((s, v))
        return out

    def op(self, e, fn, reads=(), writes=()):
        waits = self._deps(e, reads, writes)
        self.cnt[e] += 1
        n = self.cnt[e]
        s = self.esem[e]
        self.th[e].append((waits, fn, s, 1))
        for t in reads:
            if t.r.get(s, 0) < n:
                t.r[s] = n
        for t in writes:
            t.w = (s, n)
            t.r = {}

    def dma(self, q, out, in_, reads=(), writes=(), **kw):
        if q == "pool":
            i = NDS - 8 + self.dnext_sw
            self.dnext_sw = (self.dnext_sw + 1) % 8
        else:
            i = self.dnext
            self.dnext = (self.dnext + 1) % (NDS - 8)
        s = self.dsem[i]
        extra = [(s, self.dval[i])] if self.dval[i] else []
        waits = self._deps(q, reads, writes, extra)
        self.dval[i] += 16
        v = self.dval[i]
        if "indirect" in kw:
            ioff = kw.pop("indirect")
            self.th[q].append((waits, lambda eng: eng.indirect_dma_start(out=out, out_offset=None, in_=in_,
                                                                         in_offset=ioff), s, 16))
        else:
            self.th[q].append((waits, lambda eng: eng.dma_start(out=out, in_=in_, **kw), s, 16))
        for t in reads:
            t.r[s] = v
        for t in writes:
            t.w = (s, v)
            t.r = {}

    def barrier(self):
        tgt = []
        for i in range(NDS):
            if self.dval[i]:
                tgt.append((self.dsem[i], self.dval[i]))
        for k in self.names:
            if self.cnt[k]:
                tgt.append((self.esem[k], self.cnt[k]))
        for e in self.names:
            waits = []
            for s_, v in tgt:
                if s_ == self.esem[e] and e in ("pe", "sp"):
                    continue
                if self.seen[e].get(s_, 0) >= v:
                    continue
                self.seen[e][s_] = v
                waits.append((s_, v))
            self.th[e].append((waits, None, None, 0))

    def finish(self):
        waits = []
        for i in range(NDS):
            if self.dval[i]:
                waits.append((self.dsem[i], self.dval[i]))
        for k in self.names:
            if k != "sp" and self.cnt[k]:
                waits.append((self.esem[k], self.cnt[k]))
        self.th["sp"].append((waits, None, None, 0))

    def emit(self):
        nc = self.nc
        sems = self.sems

        def run(eng, lst):
            for waits, fn, s, inc in lst:
                for ws, wv in waits:
                    eng.wait_ge(sems[ws], wv)
                if fn is not None:
                    fn(eng).then_inc(sems[s], inc)

        with nc.Block() as block:
            @block.tensor
            def _(e):
                run(e, self.th["pe"])

            @block.scalar
            def _(e):
                run(e, self.th["act"])

            @block.vector
            def _(e):
                run(e, self.th["dve"])

            @block.gpsimd
            def _(e):
                run(e, self.th["pool"])

            @block.sync
            def _(e):
                run(e, self.th["sp"])


class K:
    pass


def build(NSEQ, NS=16, STOP=99, NPOOL=10240):
    GCUT = int(os.environ.get('GCUT', '99'))
    DCUT = int(os.environ.get('DCUT', '99'))
    PCUT = int(os.environ.get('PCUT', '99'))
    CCUT = int(os.environ.get('CCUT', '99'))
    nc = bass.Bass("TRN2", target_bir_lowering=False)
    P = Prog(nc)
    k = K()

    def din(name, shape, dt=F32):
        return nc.dram_tensor(name, list(shape), dt, kind="ExternalInput").ap()

    def dout(name, shape, dt=F32):
        return nc.dram_tensor(name, list(shape), dt, kind="ExternalOutput").ap()

    ARENA_W = 53000
    arena = nc.alloc_sbuf_tensor("arena", [128, ARENA_W], F32).ap()
    k.top = 0

    def sb(name, shape, dt=F32):
        n = 1
        for d_ in shape[1:]:
            n *= d_
        words = n if dt in (F32, I32, mybir.dt.uint32) else (n + 1) // 2
        words = (words + 7) // 8 * 8
        off = k.top
        k.top += words
        assert k.top <= ARENA_W, ("SBUF arena overflow", name, k.top)
        a = arena[:, off:off + words]
        if dt != F32:
            a = a.bitcast(dt)
        a = a[:, 0:n]
        if len(shape) > 2:
            names = " ".join("d%d" % i for i in range(len(shape) - 1))
            kw = {"d%d" % i: shape[i + 1] for i in range(len(shape) - 2)}
            a = a.rearrange("p (%s) -> p %s" % (names, names), **kw)
        if shape[0] != 128:
            a = a[0:shape[0]]
        return a

    def sbt(name, shape, dt=F32):
        return sb(name, shape, dt), Tok()

    xp = din("xp", [NSEQ * SEQ, D])
    memp = din("memp", [NSEQ * MEM, D])
    xs = din("xs", [NS, D])
    st_conv = din("st_conv", [NS * 3, 1536])
    ssm_in = din("ssm_in", [NS * 512, 128])
    eye16_d = din("eye16", [256])
    selpair_d = din("selpair", [8, NS, 128])
    selone_d = din("selone", [NS, NS, 128])
    iota64_d = din("iota64", [64])
    ptab = din("ptab", [NS, 64], I32)
    cache_kidx_d = din("cache_kidx", [NPOOL, 8192])
    cache_k_d = din("cache_k", [NPOOL * 128, 128])
    cache_v_d = din("cache_v", [NPOOL * 128, 128])
    cmk_d = din("cmk", [NS * 256, 256])
    cmv_d = din("cmv", [NS * 256, 256])
    ident_d = din("ident", [128, 128])
    trile_d = din("trile", [128, 128])
    sgt_d = din("sgt", [128, 128])
    pow2_d = din("pow2", [32])
    g_attn = din("attn_norm_g", [D])
    w_in = din("w_in", [D, INW])
    g_q = din("q_norm_g", [64])
    g_k = din("k_norm_g", [64])
    conv_w = din("conv_w", [4, 1536])
    a_log = din("a_log", [4])
    dt_bias = din("dt_bias", [4])
    g_gdn = din("gdn_norm_g", [128])
    w_out = din("w_out", [D, D])
    g_x = din("xattn_norm_g", [D])
    g_mem = din("mem_norm_g", [D])
    w_mq = din("w_mq", [D, 256])
    w_mk = din("w_mk", [D, 256])
    w_mv = din("w_mv", [D, 256])
    g_mq = din("mq_norm_g", [64])
    g_mk = din("mk_norm_g", [64])
    w_mo = din("w_mo", [256, D])
    g_f = din("ffn_norm_g", [D])
    w_gate = din("w_gate", [D, 2816])
    w_up = din("w_up", [D, 2816])
    w_down = din("w_down", [2816, D])

    y_p = dout("y_p", [NSEQ * SEQ, D])
    y_s = dout("y_s", [NS, D])
    k_p = dout("k_p", [NSEQ * SEQ, 128])
    v_p = dout("v_p", [NSEQ * SEQ, 128])
    kidx_p = dout("kidx_p", [NSEQ * SEQ, 64])
    conv_p = dout("conv_p", [NSEQ * 3, 1536])
    ssm_p = dout("ssm_p", [NSEQ * 512, 128])
    memk_p = dout("memk_p", [NSEQ * MEM, 256])
    memv_p = dout("memv_p", [NSEQ * MEM, 256])
    k_s = dout("k_s", [NS, 128])
    v_s = dout("v_s", [NS, 128])
    kidx_s = dout("kidx_s", [NS, 64])
    conv_s = dout("conv_s", [NS * 3, 1536])
    ssm_s = dout("ssm_s", [NS * 512, 128])

    identf, t_identf = sbt("identf", [128, 128])
    identb, t_identb = sbt("identb", [128, 128], BF16)
    trile, t_trile = sbt("trile", [128, 128])
    sgt, t_sgt = sbt("sgt", [128, 128])
    onesf, t_onesf = sbt("onesf", [128, 128])
    onesb, t_onesb = sbt("onesb", [128, 128], BF16)
    tribias, t_tribias = sbt("tribias", [128, 128])
    lowinc, t_lowinc = sbt("lowinc", [128, 128])
    P.dma("sp", identf, ident_d, writes=[t_identf])
    P.dma("sp", trile, trile_d, writes=[t_trile])
    P.dma("sp", sgt, sgt_d, writes=[t_sgt])
    P.op("dve", lambda e: e.tensor_copy(out=identb, in_=identf), [t_identf], [t_identb])
    P.op("pool", lambda e: e.memset(onesf, 1.0), [], [t_onesf])
    P.op("pool", lambda e: e.memset(onesb, 1.0), [], [t_onesb])
    P.op("dve", lambda e: e.tensor_tensor(out=lowinc, in0=sgt, in1=identf, op=ALU.add),
         [t_sgt, t_identf], [t_lowinc])
    P.op("dve", lambda e: e.tensor_scalar(out=tribias, in0=lowinc, scalar1=-1.0, scalar2=1e30,
                                          op0=ALU.add, op1=ALU.mult), [t_lowinc], [t_tribias])

    def col_layout(name, src, n):
        t, tok = sbt(name, [128, n])
        P.dma("sp", t, src.rearrange("(c p) -> p c", p=128), writes=[tok],
              allow_slow_non_contiguous=True)
        return t, tok

    def bcast_layout(name, src, n):
        t, tok = sbt(name, [128, n])
        P.dma("sp", t, src.partition_broadcast(128), writes=[tok])
        return t, tok

    gA, t_gA = col_layout("gA", g_attn, 8)
    gM, t_gM = col_layout("gM", g_mem, 8)
    gX, t_gX = col_layout("gX", g_x, 8)
    gF, t_gF = col_layout("gF", g_f, 8)
    gk_b, t_gk = bcast_layout("gk_b", g_k, 64)
    gmk_b, t_gmk = bcast_layout("gmk_b", g_mk, 64)
    ggdn_b, t_ggdn = bcast_layout("ggdn_b", g_gdn, 128)
    dtb_b, t_dtb = bcast_layout("dtb_b", dt_bias, 4)
    nea_b, t_nea = bcast_layout("nea_b", a_log, 4)
    pow2_b, t_pow2 = bcast_layout("pow2_b", pow2_d, 32)
    P.op("act", lambda e: e.activation(out=nea_b, in_=nea_b, func=AF.Exp), [t_nea], [t_nea])
    P.op("dve", lambda e: e.tensor_scalar(out=nea_b, in0=nea_b, scalar1=-1.0, scalar2=None,
                                          op0=ALU.mult), [t_nea], [t_nea])
    gq8, t_gq8 = sbt("gq8", [128, 1])
    gmq8, t_gmq8 = sbt("gmq8", [128, 1])
    for (dst, tdst, src) in ((gq8, t_gq8, g_q), (gmq8, t_gmq8, g_mq)):
        for hh in range(2):
            P.dma("sp", dst[64 * hh:64 * hh + 64, :], src.rearrange("(d o) -> d o", o=1),
                  writes=[tdst], allow_slow_non_contiguous=True)
        P.op("pool", lambda e, dst=dst: e.tensor_scalar(out=dst, in0=dst, scalar1=0.125, scalar2=1.0,
                                                        op0=ALU.mult, op1=ALU.mult), [tdst], [tdst])
    cw, t_cw = sbt("cw", [128, 12, 4])
    for tap in range(4):
        P.dma("sp", cw[:, :, tap], conv_w[tap].rearrange("(j p) -> p j", p=128), writes=[t_cw],
              allow_slow_non_contiguous=True)
    neghalf, t_nh = sbt("neghalf", [128, 8])
    P.op("pool", lambda e: e.memset(neghalf, -0.5), [], [t_nh])

    ps = [nc.alloc_psum_tensor("ps%d" % i, [128, 512], F32).ap() for i in range(8)]
    t_ps = [Tok() for _ in range(8)]
    psb16 = [p_.bitcast(BF16) for p_ in ps]

    stage = [None, None]
    t_stage = [Tok(), Tok()]
    k.wl = 0

    def alloc_stage(n):
        for i in range(2):
            stage[i] = sb("wstage%d" % i, [128, n], F32)

    def load_rows(dst_kc, t_dst, src_rows, n, gain_col, t_gain):
        i = k.wl % 2
        k.wl += 1
        st = stage[i][:, 0:n]
        P.dma("sp", st, src_rows, writes=[t_stage[i]])
        if gain_col is None:
            if k.wl % 2 == 0:
                P.op("act", lambda e: e.activation(out=dst_kc, in_=st, func=AF.Copy),
                     [t_stage[i]], [t_dst])
            else:
                P.op("pool", lambda e: e.tensor_copy(out=dst_kc, in_=st), [t_stage[i]], [t_dst])
        else:
            if k.wl % 2 == 0:
                P.op("act", lambda e: e.activation(out=dst_kc, in_=st, func=AF.Copy, scale=gain_col),
                     [t_stage[i], t_gain], [t_dst])
            else:
                P.op("pool", lambda e: e.tensor_scalar(out=dst_kc, in0=st, scalar1=gain_col, scalar2=1.0,
                                                       op0=ALU.mult, op1=ALU.mult),
                     [t_stage[i], t_gain], [t_dst])

    def load_weight(dst, t_dst, src, c0, c1, gain, t_gain, d0=0):
        n = c1 - c0
        for kc in range(8):
            load_rows(dst[:, kc, d0:d0 + n], t_dst, src[kc * 128:(kc + 1) * 128, c0:c1], n,
                      None if gain is None else gain[:, kc:kc + 1], t_gain)

    xt = [sb("xt%d" % i, [128, D], F32) for i in range(2)]
    t_xt = [Tok(), Tok()]
    hb = [sb("hb%d" % i, [128, D], BF16) for i in range(2)]
    t_hb = [Tok(), Tok()]
    sq_scr, t_sq = sbt("sq_scr", [128, D], BF16)
    stat = [sb("stat%d" % i, [128, 4], F32) for i in range(2)]
    t_stat = [Tok(), Tok()]
    k.nt = 0
    for i in range(2):
        P.op("pool", lambda e, i=i: e.memset(xt[i], 0.0), [], [t_xt[i]])

    def load_x(src_rows, nrows):
        i = k.nt % 2
        k.nt += 1
        P.dma("sp", xt[i][0:nrows, :], src_rows, writes=[t_xt[i]])
        return xt[i], t_xt[i], i

    def norm_T(x, tx, i, hT, t_hT, col, psb):
        s, ts = stat[i], t_stat[i]
        P.op("act", lambda e: e.activation(out=sq_scr, in_=x, func=AF.Square,
                                           accum_out=s[:, 0:1]), [tx], [t_sq, ts])
        P.op("pool", lambda e: e.tensor_scalar(out=s[:, 1:2], in0=s[:, 0:1], scalar1=1.0 / D,
                                               scalar2=EPS, op0=ALU.mult, op1=ALU.add), [ts], [ts])
        P.op("pool", lambda e: e.tensor_tensor(out=s[:, 2:3], in0=s[:, 1:2], in1=neghalf[:, 0:1],
                                               op=ALU.pow), [ts, t_nh], [ts])
        h, th = hb[i], t_hb[i]
        P.op("act", lambda e: e.activation(out=h, in_=x, func=AF.Copy, scale=s[:, 2:3]),
             [tx, ts], [th])
        pb = psb16[psb]
        for kc in range(8):
            P.op("pe", lambda e, kc=kc: e.transpose(out=pb[:, kc * 128:(kc + 1) * 128],
                                                    in_=h[:, kc * 128:(kc + 1) * 128],
                                                    identity=identb),
                 [th, t_identb], [t_ps[psb]])
        P.op("dve", lambda e: e.tensor_copy(out=hT[:, :, col:col + 128],
                                            in_=pb.rearrange("p (k t) -> p k t", k=8)),
             [t_ps[psb]], [t_hT])

    def norm_transpose(src_rows, nrows, hT, t_hT, col, psb):
        x, tx, i = load_x(src_rows, nrows)
        norm_T(x, tx, i, hT, t_hT, col, psb)
        return x, tx

    def rstd_groups(dst, t_dst, ssq, t_ssq, n, width):
        P.op("pool", lambda e: e.tensor_scalar(out=dst, in0=ssq, scalar1=1.0 / width, scalar2=EPS,
                                               op0=ALU.mult, op1=ALU.add), [t_ssq], [t_dst])
        P.op("pool", lambda e: e.tensor_tensor(out=dst, in0=dst, in1=neghalf[:, 0:n], op=ALU.pow),
             [t_dst, t_nh], [t_dst])

    def head_rms(psrc, t_psrc, nh, scr, t_scr, ss, t_ss):
        P.op("act", lambda e: e.activation(out=scr[:, 0:nh * 64], in_=psrc, func=AF.Square),
             [t_psrc], [t_scr])
        P.op("dve", lambda e: e.tensor_reduce(out=ss[:, 0:nh],
                                              in_=scr[:, 0:nh * 64].rearrange("p (h d) -> p h d", h=nh),
                                              axis=AX.X, op=ALU.add), [t_scr], [t_ss])
        rstd_groups(ss[:, nh:2 * nh], t_ss, ss[:, 0:nh], t_ss, nh, 64)

    qscr, t_qscr = sbt("qscr", [128, 512])
    qss, t_qss = sbt("qss", [128, 16])
    PERSIST_TOP = k.top

    def prompt_seq(sq):
        k.top = PERSIST_TOP
        if STOP <= 0:
            return
        row_base = sq * SEQ
        mkT2, t_mkT2 = sbt("mkT2", [128, 2, 256], BF16)
        mv_b, t_mv = sbt("mv_b", [128, 2, 256], BF16)
        o_gdnT, t_ogT = sbt("o_gdnT", [128, 4, SEQ], BF16)
        SEQ_TOP = k.top
        alloc_stage(256)
        wmkv, t_wmkv = sbt("wmkv", [128, 8, 512], BF16)
        load_weight(wmkv, t_wmkv, w_mk, 0, 256, gM, t_gM, 0)
        load_weight(wmkv, t_wmkv, w_mv, 0, 256, gM, t_gM, 256)
        hTm, t_hTm = sbt("hTm", [128, 8, 128], BF16)
        mko = [sbt("mko%d" % i, [128, 512]) for i in range(2)]
        mkb, t_mkb = sbt("mkb", [128, 256], BF16)
        for mt in range(2):
            row0 = sq * MEM + mt * 128
            norm_transpose(memp[row0:row0 + 128, :], 128, hTm, t_hTm, 0, 0)
            for kc in range(8):
                P.op("pe", lambda e, kc=kc: e.matmul(ps[1], lhsT=hTm[:, kc, :], rhs=wmkv[:, kc, :],
                                                     start=(kc == 0), stop=(kc == 7)),
                     [t_hTm, t_wmkv], [t_ps[1]])
            o, to = mko[mt]
            P.op("act", lambda e, o=o: e.activation(out=o[:, 256:512], in_=ps[1][:, 256:512], func=AF.Copy),
                 [t_ps[1]], [to])
            P.op("act", lambda e, mt=mt: e.activation(out=mv_b[:, mt, :], in_=ps[1][:, 256:512], func=AF.Copy),
                 [t_ps[1]], [t_mv])
            head_rms(ps[1][:, 0:256], t_ps[1], 4, qscr, t_qscr, qss, t_qss)
            P.op("dve", lambda e, o=o: e.tensor_tensor(
                out=o[:, 0:256].rearrange("p (h d) -> p h d", h=4),
                in0=ps[1][:, 0:256].rearrange("p (h d) -> p h d", h=4),
                in1=qss[:, 4:8].unsqueeze(2).to_broadcast([128, 4, 64]), op=ALU.mult),
                [t_ps[1], t_qss], [to])
            P.op("pool", lambda e, o=o: e.tensor_tensor(
                out=o[:, 0:256].rearrange("p (h d) -> p h d", h=4),
                in0=o[:, 0:256].rearrange("p (h d) -> p h d", h=4),
                in1=gmk_b.unsqueeze(1).to_broadcast([128, 4, 64]), op=ALU.mult),
                [to, t_gmk], [to])
            P.dma("sp", memk_p[row0:row0 + 128, :], o[:, 0:256], reads=[to])
            P.dma("sp", memv_p[row0:row0 + 128, :], o[:, 256:512], reads=[to])
            P.op("act", lambda e, o=o: e.activation(out=mkb, in_=o[:, 0:256], func=AF.Copy), [to], [t_mkb])
            pb = psb16[2]
            for a in range(2):
                P.op("pe", lambda e, a=a: e.transpose(out=pb[:, a * 128:(a + 1) * 128],
                                                      in_=mkb[:, a * 128:(a + 1) * 128], identity=identb),
                     [t_mkb, t_identb], [t_ps[2]])
            P.op("dve", lambda e, mt=mt: e.tensor_copy(
                out=mkT2[:, :, mt * 128:(mt + 1) * 128],
                in_=pb[:, 0:256].rearrange("p (a t) -> p a t", a=2)), [t_ps[2]], [t_mkT2])
        P.barrier()
        if STOP <= 1:
            return
        k.top = SEQ_TOP
        gdn_phase(sq, o_gdnT, t_ogT)
        P.barrier()
        if STOP <= 2:
            return
        k.top = SEQ_TOP
        o_atT, t_oatT = sbt("o_atT", [128, 4, SEQ], BF16)
        D_TOP = k.top
        dsa_phase(sq, o_atT, t_oatT)
        P.barrier()
        if STOP <= 3:
            return
        k.top = D_TOP
        c1_phase(sq, o_atT, t_oatT, o_gdnT, t_ogT, mkT2, t_mkT2, mv_b, t_mv)
        P.barrier()
        if STOP <= 4:
            return
        k.top = PERSIST_TOP
        c2_phase(sq)
        P.barrier()

    def gdn_phase(sq, o_gdnT, t_ogT):
        row_base = sq * SEQ
        qTg, t_qTg = sbt("qTg", [128, 4, SEQ], BF16)
        kTg, t_kTg = sbt("kTg", [128, 4, SEQ], BF16)
        k_tm, t_ktm = sbt("k_tm", [128, NT, 4, 128], BF16)
        v_tm, t_vtm = sbt("v_tm", [128, NT, 4, 128], BF16)
        sgz, t_sgz = sbt("sgz", [128, NT, 512], BF16)
        gab, t_gab = sbt("gab", [128, NT, 8])
        G_TOP = k.top
        alloc_stage(2056)
        wB, t_wB = sbt("wB", [128, 8, 2056], BF16)
        load_weight(wB, t_wB, w_in, 1092, 3148, gA, t_gA)
        hTg, t_hTg = sbt("hTg", [128, 8, 512], BF16)
        Cq, t_Cq = sbt("Cq", [128, 4, 512])
        cvT, t_cvT = sbt("cvT", [128, 4, 512], BF16)
        Uc = [sbt("Uc%d" % i, [128, 515]) for i in range(2)]
        cacc = [sbt("cacc%d" % i, [128, 512]) for i in range(2)]
        halo, t_halo = sbt("halo", [128, 12, 3])
        sqb, t_sqb = sbt("sqb", [128, 512], BF16)
        lnb, t_lnb = sbt("lnb", [128, 512])
        P.op("pool", lambda e: e.memset(halo, 0.0), [], [t_halo])
        def conv_chunk(j, grp):
            b = 3 + (j % 2)
            for kc in range(8):
                P.op("pe", lambda e, kc=kc: e.matmul(
                    ps[b], lhsT=wB[:, kc, 128 * j:128 * j + 128], rhs=hTg[:, kc, :],
                    start=(kc == 0), stop=(kc == 7)), [t_hTg, t_wB], [t_ps[b]])
            u, tu = Uc[j % 2]
            ca, tca = cacc[j % 2]
            P.op("act", lambda e: e.activation(out=u[:, 3:515], in_=ps[b], func=AF.Copy), [t_ps[b]], [tu])
            P.op("pool", lambda e: e.tensor_copy(out=u[:, 0:3], in_=halo[:, j, :]), [t_halo], [tu])
            P.op("dve", lambda e: e.tensor_scalar(out=ca, in0=u[:, 0:512], scalar1=cw[:, j, 0:1], scalar2=None,
                                                  op0=ALU.mult), [tu, t_cw], [tca])
            for tap in range(1, 4):
                P.op("dve", lambda e, tap=tap: e.scalar_tensor_tensor(
                    out=ca, in0=u[:, tap:tap + 512], scalar=cw[:, j, tap:tap + 1], in1=ca,
                    op0=ALU.mult, op1=ALU.add), [tu, t_cw, tca], [tca])
            P.op("pool", lambda e: e.tensor_copy(out=halo[:, j, :], in_=u[:, 512:515]), [tu], [t_halo])
            if j < 8:
                P.op("act", lambda e: e.activation(out=Cq[:, j % 4, :], in_=ca, func=AF.Silu), [tca], [t_Cq])
            else:
                P.op("act", lambda e: e.activation(out=cvT[:, j - 8, :], in_=ca, func=AF.Silu), [tca], [t_cvT])

        def norm_chunk(j, gc0):
            P.op("act", lambda e: e.activation(out=sqb, in_=Cq[:, j % 4, :], func=AF.Square), [t_Cq], [t_sqb])
            P.op("pe", lambda e: e.matmul(ps[5], lhsT=onesb, rhs=sqb, start=True, stop=True),
                 [t_sqb, t_onesb], [t_ps[5]])
            P.op("act", lambda e: e.activation(out=lnb, in_=ps[5], func=AF.Ln, bias=1e-6, scale=1.0),
                 [t_ps[5]], [t_lnb])
            bias = (-0.5 * float(np.log(128.0))) if j < 4 else 0.0
            P.op("act", lambda e: e.activation(out=lnb, in_=lnb, func=AF.Exp, bias=bias, scale=-0.5),
                 [t_lnb], [t_lnb])
            dstT, tdst = (qTg, t_qTg) if j < 4 else (kTg, t_kTg)
            P.op("dve", lambda e: e.tensor_tensor(out=dstT[:, j % 4, gc0:gc0 + 512], in0=Cq[:, j % 4, :], in1=lnb,
                                                  op=ALU.mult), [t_Cq, t_lnb], [tdst])

        def g_tile(grp, t4):
            ti = grp * 4 + t4
            r0 = row_base + ti * 128
            norm_transpose(xp[r0:r0 + 128, :], 128, hTg, t_hTg, t4 * 128, 0)
            for kc in range(8):
                P.op("pe", lambda e, kc=kc: e.matmul(
                    ps[1], lhsT=hTg[:, kc, t4 * 128:(t4 + 1) * 128], rhs=wB[:, kc, 1536:2048],
                    start=(kc == 0), stop=(kc == 7)), [t_hTg, t_wB], [t_ps[1]])
            for kc in range(8):
                P.op("pe", lambda e, kc=kc: e.matmul(
                    ps[2][:, 0:8], lhsT=hTg[:, kc, t4 * 128:(t4 + 1) * 128], rhs=wB[:, kc, 2048:2056],
                    start=(kc == 0), stop=(kc == 7)), [t_hTg, t_wB], [t_ps[2]])
            P.op("act", lambda e: e.activation(out=sgz[:, ti, :], in_=ps[1], func=AF.Silu), [t_ps[1]], [t_sgz])
            P.op("dve", lambda e: e.tensor_copy(out=gab[:, ti, :], in_=ps[2][:, 0:8]), [t_ps[2]], [t_gab])

        def g_transposes(grp, t4):
            ti = grp * 4 + t4
            gc0 = grp * 512
            for (srcT, tsrc, c0, dst, tdst, b) in ((kTg, t_kTg, gc0 + t4 * 128, k_tm, t_ktm, 6),
                                                   (cvT, t_cvT, t4 * 128, v_tm, t_vtm, 7)):
                pb = psb16[b]
                for h in range(4):
                    P.op("pe", lambda e, h=h, srcT=srcT, c0=c0, pb=pb: e.transpose(
                        out=pb[:, h * 128:(h + 1) * 128], in_=srcT[:, h, c0:c0 + 128], identity=identb),
                        [tsrc, t_identb], [t_ps[b]])
                P.op("act", lambda e, dst=dst, pb=pb: e.activation(
                    out=dst[:, ti, :, :], in_=pb[:, 0:512].rearrange("p (h d) -> p h d", h=4), func=AF.Copy),
                    [t_ps[b]], [tdst])

        for grp in range(4):
            for t4 in range(4):
                g_tile(grp, t4)
            for j in range(0, 4):
                conv_chunk(j, grp)
            for j in range(0, 4):
                norm_chunk(j, grp * 512)
            for j in range(4, 12):
                conv_chunk(j, grp)
            for j in range(4, 8):
                norm_chunk(j, grp * 512)
            for t4 in range(4):
                g_transposes(grp, t4)
        P.barrier()
        if STOP <= 1.5:
            return
        k.top = G_TOP
        gall, t_gall = sbt("gall", [128, NT, 4])
        ball, t_ball = sbt("ball", [128, NT, 4])
        tmpa, t_tmpa = sbt("tmpa", [128, NT, 4])
        tmpb, t_tmpb = sbt("tmpb", [128, NT, 4])
        P.op("dve", lambda e: e.tensor_tensor(out=gall, in0=gab[:, :, 0:4],
                                              in1=dtb_b.unsqueeze(1).to_broadcast([128, NT, 4]), op=ALU.add),
             [t_gab, t_dtb], [t_gall])
        P.op("dve", lambda e: e.tensor_scalar(out=tmpa, in0=gall, scalar1=-1.0, scalar2=None, op0=ALU.mult),
             [t_gall], [t_tmpa])
        P.op("dve", lambda e: e.tensor_tensor(out=tmpa, in0=tmpa, in1=gall, op=ALU.min), [t_tmpa, t_gall], [t_tmpa])
        P.op("act", lambda e: e.activation(out=tmpa, in_=tmpa, func=AF.Exp), [t_tmpa], [t_tmpa])
        P.op("act", lambda e: e.activation(out=tmpa, in_=tmpa, func=AF.Ln, bias=1.0, scale=1.0), [t_tmpa], [t_tmpa])
        P.op("dve", lambda e: e.scalar_tensor_tensor(out=tmpb, in0=gall, scalar=0.0, in1=tmpa,
                                                     op0=ALU.max, op1=ALU.add), [t_gall, t_tmpa], [t_tmpb])
        P.op("dve", lambda e: e.tensor_tensor(out=gall, in0=tmpb,
                                              in1=nea_b.unsqueeze(1).to_broadcast([128, NT, 4]), op=ALU.mult),
             [t_tmpb, t_nea], [t_gall])
        P.op("act", lambda e: e.activation(out=ball, in_=gab[:, :, 4:8], func=AF.Sigmoid), [t_gab], [t_ball])

        S, t_S = sbt("S", [128, 4, 128])
        Sb, t_Sb = sbt("Sb", [128, 4, 128], BF16)
        P.op("pool", lambda e: e.memset(S, 0.0), [], [t_S])
        P.op("pool", lambda e: e.memset(Sb, 0.0), [], [t_Sb])
        NB = 2
        bufs = []
        for i in range(NB):
            bb = {}
            for nm, dt_ in (("Gh", F32), ("E", F32), ("Du", F32), ("Dl", F32), ("L0", BF16), ("L1", BF16),
                            ("M0", BF16), ("M1", BF16), ("P0", BF16), ("P1", BF16), ("kbg", BF16),
                            ("kdec", BF16), ("vb", BF16), ("u", F32), ("wT", BF16), ("qgT", BF16),
                            ("qkT", BF16), ("vnew", BF16), ("on", F32), ("og", BF16)):
                bb[nm] = sbt("%s_%d" % (nm, i), [128, 4, 128], dt_)
            bb["sc"] = sbt("gsc_%d" % i, [128, 40])
            bufs.append(bb)
        k.pbank = 0

        def bank():
            b = k.pbank
            k.pbank = (k.pbank + 1) % 8
            return b

        def gdn_tile(ti):
            B = bufs[ti % NB]
            par = ti % 2
            bstate = [0]

            def bank():
                b_ = par * 4 + bstate[0]
                bstate[0] = (bstate[0] + 1) % 4
                return b_
            c0 = ti * 128
            sc, tsc = B["sc"]
            Gh, tGh = B["Gh"]
            for h in range(4):
                P.op("pool", lambda e, h=h, Gh=Gh, ti=ti: e.tensor_scalar(
                    out=Gh[:, h, :], in0=trile, scalar1=gall[:, ti, h:h + 1], scalar2=1.0,
                    op0=ALU.mult, op1=ALU.mult), [t_trile, t_gall], [tGh])
                yield
            bE, bDu, bDl, bsm = bank(), bank(), bank(), bank()
            for h in range(4):
                P.op("pe", lambda e, h=h, Gh=Gh, bE=bE: e.matmul(ps[bE][:, h * 128:(h + 1) * 128], lhsT=onesf,
                                                                 rhs=Gh[:, h, :], start=True, stop=True),
                     [tGh, t_onesf], [t_ps[bE]])
                yield
                P.op("pe", lambda e, h=h, Gh=Gh, bDu=bDu: e.matmul(ps[bDu][:, h * 128:(h + 1) * 128], lhsT=sgt,
                                                                   rhs=Gh[:, h, :], start=True, stop=True),
                     [tGh, t_sgt], [t_ps[bDu]])
                yield
                P.op("pe", lambda e, h=h, Gh=Gh, bDl=bDl: e.matmul(ps[bDl][:, h * 128:(h + 1) * 128], lhsT=Gh[:, h, :],
                                                                   rhs=sgt, start=True, stop=True),
                     [tGh, t_sgt], [t_ps[bDl]])
                yield
            if GCUT <= 1:
                return
            P.op("pe", lambda e, ti=ti, bsm=bsm: e.matmul(ps[bsm][:, 0:4], lhsT=trile, rhs=gall[:, ti, :],
                                                          start=True, stop=True), [t_trile, t_gall], [t_ps[bsm]])
            yield
            P.op("pe", lambda e, ti=ti, bsm=bsm: e.matmul(ps[bsm][:, 8:12], lhsT=onesf, rhs=gall[:, ti, :],
                                                          start=True, stop=True), [t_onesf, t_gall], [t_ps[bsm]])
            yield
            if GCUT <= 2:
                return
            E, tE = B["E"]
            Du, tDu = B["Du"]
            Dl, tDl = B["Dl"]
            fl = lambda a: a.rearrange("p h c -> p (h c)")
            P.op("act", lambda e, E=E, bE=bE: e.activation(out=fl(E), in_=ps[bE], func=AF.Exp), [t_ps[bE]], [tE])
            yield
            P.op("act", lambda e, Du=Du, bDu=bDu: e.activation(out=fl(Du), in_=ps[bDu], func=AF.Exp), [t_ps[bDu]], [tDu])
            yield
            P.op("act", lambda e, Dl=Dl, bDl=bDl: e.activation(out=fl(Dl), in_=ps[bDl], func=AF.Exp), [t_ps[bDl]], [tDl])
            yield
            P.op("pool", lambda e, Du=Du: e.tensor_tensor(out=Du, in0=Du, in1=trile.unsqueeze(1).to_broadcast([128, 4, 128]),
                                                          op=ALU.mult), [tDu, t_trile], [tDu])
            yield
            P.op("pool", lambda e, Dl=Dl: e.tensor_tensor(out=Dl, in0=Dl, in1=sgt.unsqueeze(1).to_broadcast([128, 4, 128]),
                                                          op=ALU.mult), [tDl, t_sgt], [tDl])
            yield
            if GCUT <= 3:
                return
            P.op("dve", lambda e, sc=sc, bsm=bsm: e.tensor_copy(out=sc[:, 0:4], in_=ps[bsm][:, 0:4]), [t_ps[bsm]], [tsc])
            yield
            P.op("act", lambda e, sc=sc: e.activation(out=sc[:, 4:8], in_=sc[:, 0:4], func=AF.Exp), [tsc], [tsc])
            yield
            P.op("dve", lambda e, sc=sc, bsm=bsm: e.tensor_tensor(out=sc[:, 24:28], in0=ps[bsm][:, 8:12], in1=sc[:, 0:4],
                                                                  op=ALU.subtract), [t_ps[bsm], tsc], [tsc])
            yield
            P.op("act", lambda e, sc=sc: e.activation(out=sc[:, 8:12], in_=sc[:, 24:28], func=AF.Exp), [tsc], [tsc])
            yield
            P.op("act", lambda e, sc=sc, bsm=bsm: e.activation(out=sc[:, 12:16], in_=ps[bsm][:, 8:12], func=AF.Exp),
                 [t_ps[bsm]], [tsc])
            yield
            P.op("dve", lambda e, sc=sc, ti=ti: e.tensor_tensor(out=sc[:, 16:20], in0=sc[:, 4:8], in1=ball[:, ti, :],
                                                                op=ALU.mult), [tsc, t_ball], [tsc])
            yield
            P.op("dve", lambda e, sc=sc, ti=ti: e.tensor_scalar(out=sc[:, 20:24], in0=ball[:, ti, :], scalar1=-1.0,
                                                                scalar2=None, op0=ALU.mult), [t_ball], [tsc])
            yield
            if GCUT <= 4:
                return
            kbg, tkbg = B["kbg"]
            kdec, tkdec = B["kdec"]
            vb, tvb = B["vb"]
            bc = lambda a: a.unsqueeze(2).to_broadcast([128, 4, 128])
            P.op("pool", lambda e, kbg=kbg, sc=sc, ti=ti: e.tensor_tensor(out=kbg, in0=k_tm[:, ti, :, :], in1=bc(sc[:, 16:20]),
                                                                          op=ALU.mult), [t_ktm, tsc], [tkbg])
            yield
            P.op("pool", lambda e, kdec=kdec, sc=sc, ti=ti: e.tensor_tensor(out=kdec, in0=k_tm[:, ti, :, :], in1=bc(sc[:, 8:12]),
                                                                            op=ALU.mult), [t_ktm, tsc], [tkdec])
            yield
            P.op("pool", lambda e, vb=vb, ti=ti: e.tensor_tensor(out=vb, in0=v_tm[:, ti, :, :], in1=bc(ball[:, ti, :]),
                                                                 op=ALU.mult), [t_vtm, t_ball], [tvb])
            yield
            if GCUT <= 5:
                return
            bkk = bank()
            for h in range(4):
                P.op("pe", lambda e, h=h, bkk=bkk: e.matmul(ps[bkk][:, h * 128:(h + 1) * 128], lhsT=kTg[:, h, c0:c0 + 128],
                                                            rhs=kTg[:, h, c0:c0 + 128], start=True, stop=True),
                     [t_kTg], [t_ps[bkk]])
                yield
            Lc, tLc = B["L0"]
            Ln_, tLn = B["L1"]
            Mc, tMc = B["M0"]
            Mn, tMn = B["M1"]
            Pc, tPc = B["P0"]
            Pn, tPn = B["P1"]
            for h in range(4):
                P.op("dve", lambda e, h=h, Lc=Lc, sc=sc, Dl=Dl, bkk=bkk: e.scalar_tensor_tensor(
                    out=Lc[:, h, :], in0=ps[bkk][:, h * 128:(h + 1) * 128], scalar=sc[:, 20 + h:21 + h], in1=Dl[:, h, :],
                    op0=ALU.mult, op1=ALU.mult), [t_ps[bkk], tsc, tDl], [tLc])
                yield
            if GCUT <= 6:
                return
            bM = bank()
            pbm = psb16[bM]
            for h in range(4):
                P.op("pe", lambda e, h=h, Lc=Lc, pbm=pbm: e.transpose(out=pbm[:, h * 128:(h + 1) * 128], in_=Lc[:, h, :],
                                                                      identity=identb), [tLc, t_identb], [t_ps[bM]])
                yield
            pm3 = pbm[:, 0:512].rearrange("p (h c) -> p h c", h=4)
            if GCUT == 61:
                return
            P.op("act", lambda e, Mc=Mc, pm3=pm3: e.activation(out=Mc, in_=pm3, func=AF.Copy), [t_ps[bM]], [tMc])
            yield
            if GCUT == 62:
                return
            P.op("pool", lambda e, Pc=Pc, Mc=Mc: e.tensor_tensor(out=Pc, in0=Mc,
                                                                 in1=identb.unsqueeze(1).to_broadcast([128, 4, 128]),
                                                                 op=ALU.add), [tMc, t_identb], [tPc])
            yield
            if GCUT <= 7 or GCUT in (61, 62):
                return
            for lev in range(6):
                last = (lev == 5)
                bL = bank()
                if not last:
                    bMM = bank()
                    for h in range(4):
                        P.op("pe", lambda e, h=h, Lc=Lc, Mc=Mc, bMM=bMM: e.matmul(
                            ps[bMM][:, h * 128:(h + 1) * 128], lhsT=Lc[:, h, :], rhs=Mc[:, h, :], start=True, stop=True),
                            [tLc, tMc], [t_ps[bMM]])
                        yield
                for h in range(4):
                    P.op("pe", lambda e, h=h, Lc=Lc, Mc=Mc, bL=bL: e.matmul(
                        ps[bL][:, h * 128:(h + 1) * 128], lhsT=Mc[:, h, :], rhs=Lc[:, h, :], start=True, stop=True),
                        [tLc, tMc], [t_ps[bL]])
                    yield
                P.op("dve", lambda e, Ln_=Ln_, bL=bL: e.tensor_copy(out=fl(Ln_), in_=ps[bL]), [t_ps[bL]], [tLn])
                yield
                if not last:
                    P.op("act", lambda e, Mn=Mn, bMM=bMM: e.activation(out=fl(Mn), in_=ps[bMM], func=AF.Copy),
                         [t_ps[bMM]], [tMn])
                    yield
                bP = bank()
                for h in range(4):
                    P.op("pe", lambda e, h=h, Ln_=Ln_, Pc=Pc, bP=bP: e.matmul(
                        ps[bP][:, h * 128:(h + 1) * 128], lhsT=Ln_[:, h, :], rhs=Pc[:, h, :], start=True, stop=True),
                        [tLn, tPc], [t_ps[bP]])
                    yield
                P.op("dve", lambda e, Pn=Pn, Pc=Pc, bP=bP: e.tensor_tensor(out=fl(Pn), in0=ps[bP], in1=fl(Pc), op=ALU.add),
                     [t_ps[bP], tPc], [tPn])
                yield
                Lc, tLc, Ln_, tLn = Ln_, tLn, Lc, tLc
                Mc, tMc, Mn, tMn = Mn, tMn, Mc, tMc
                Pc, tPc, Pn, tPn = Pn, tPn, Pc, tPc
            if GCUT <= 8:
                return
            bu, bw, bq = bank(), bank(), bank()
            for h in range(4):
                P.op("pe", lambda e, h=h, Pc=Pc, vb=vb, bu=bu: e.matmul(ps[bu][:, h * 128:(h + 1) * 128], lhsT=Pc[:, h, :],
                                                                        rhs=vb[:, h, :], start=True, stop=True),
                     [tPc, tvb], [t_ps[bu]])
                yield
                P.op("pe", lambda e, h=h, Pc=Pc, kbg=kbg, bw=bw: e.matmul(ps[bw][:, h * 128:(h + 1) * 128], lhsT=kbg[:, h, :],
                                                                          rhs=Pc[:, h, :], start=True, stop=True),
                     [tPc, tkbg], [t_ps[bw]])
                yield
                P.op("pe", lambda e, h=h, bq=bq: e.matmul(ps[bq][:, h * 128:(h + 1) * 128], lhsT=kTg[:, h, c0:c0 + 128],
                                                          rhs=qTg[:, h, c0:c0 + 128], start=True, stop=True),
                     [t_kTg, t_qTg], [t_ps[bq]])
                yield
            u, tu_ = B["u"]
            wT, twT = B["wT"]
            qgT, tqgT = B["qgT"]
            qkT, tqkT = B["qkT"]
            P.op("act", lambda e, u=u, bu=bu: e.activation(out=fl(u), in_=ps[bu], func=AF.Copy), [t_ps[bu]], [tu_])
            yield
            P.op("act", lambda e, wT=wT, bw=bw: e.activation(out=fl(wT), in_=ps[bw], func=AF.Copy), [t_ps[bw]], [twT])
            yield
            P.op("dve", lambda e, qkT=qkT, Du=Du, bq=bq: e.tensor_tensor(out=fl(qkT), in0=ps[bq], in1=fl(Du), op=ALU.mult),
                 [t_ps[bq], tDu], [tqkT])
            yield
            P.op("pool", lambda e, qgT=qgT, E=E: e.tensor_tensor(out=qgT, in0=qTg[:, :, c0:c0 + 128], in1=E, op=ALU.mult),
                 [t_qTg, tE], [tqgT])
            yield
            if GCUT <= 9:
                return
            yield 'SEQ'
            bws, bo, bs = bank(), bank(), bank()
            for h in range(4):
                P.op("pe", lambda e, h=h, wT=wT, bws=bws: e.matmul(ps[bws][:, h * 128:(h + 1) * 128], lhsT=wT[:, h, :],
                                                                   rhs=Sb[:, h, :], start=True, stop=True),
                     [twT, t_Sb], [t_ps[bws]])
            vnew, tvn = B["vnew"]
            P.op("dve", lambda e, vnew=vnew, u=u, bws=bws: e.tensor_tensor(out=fl(vnew), in0=fl(u), in1=ps[bws],
                                                                           op=ALU.subtract), [tu_, t_ps[bws]], [tvn])
            for h in range(4):
                P.op("pe", lambda e, h=h, qgT=qgT, bo=bo: e.matmul(ps[bo][:, h * 128:(h + 1) * 128], lhsT=qgT[:, h, :],
                                                                   rhs=Sb[:, h, :], start=True, stop=False),
                     [tqgT, t_Sb], [t_ps[bo]])
                P.op("pe", lambda e, h=h, qkT=qkT, vnew=vnew, bo=bo: e.matmul(ps[bo][:, h * 128:(h + 1) * 128], lhsT=qkT[:, h, :],
                                                                              rhs=vnew[:, h, :], start=False, stop=True),
                     [tqkT, tvn], [t_ps[bo]])
            for h in range(4):
                P.op("pe", lambda e, h=h, kdec=kdec, vnew=vnew, bs=bs: e.matmul(ps[bs][:, h * 128:(h + 1) * 128], lhsT=kdec[:, h, :],
                                                                                rhs=vnew[:, h, :], start=True, stop=True),
                     [tkdec, tvn], [t_ps[bs]])
            for h in range(4):
                P.op("dve", lambda e, h=h, sc=sc, bs=bs: e.scalar_tensor_tensor(
                    out=S[:, h, :], in0=S[:, h, :], scalar=sc[:, 12 + h:13 + h], in1=ps[bs][:, h * 128:(h + 1) * 128],
                    op0=ALU.mult, op1=ALU.add), [t_S, tsc, t_ps[bs]], [t_S])
            P.op("act", lambda e: e.activation(out=Sb, in_=S, func=AF.Copy), [t_S], [t_Sb])
            if GCUT <= 10:
                return
            on, ton = B["on"]
            og, tog = B["og"]
            P.op("act", lambda e, on=on, bo=bo: e.activation(out=fl(on), in_=ps[bo], func=AF.Square), [t_ps[bo]], [ton])
            P.op("dve", lambda e, on=on, sc=sc: e.tensor_reduce(out=sc[:, 28:32], in_=on, axis=AX.X, op=ALU.add),
                 [ton], [tsc])
            P.op("pool", lambda e, sc=sc: e.tensor_scalar(out=sc[:, 32:36], in0=sc[:, 28:32], scalar1=1.0 / 128, scalar2=EPS,
                                                          op0=ALU.mult, op1=ALU.add), [tsc], [tsc])
            P.op("pool", lambda e, sc=sc: e.tensor_tensor(out=sc[:, 32:36], in0=sc[:, 32:36], in1=neghalf[:, 0:4], op=ALU.pow),
                 [tsc, t_nh], [tsc])
            P.op("dve", lambda e, on=on, sc=sc, bo=bo: e.tensor_tensor(
                out=on, in0=ps[bo].rearrange("p (h c) -> p h c", h=4), in1=bc(sc[:, 32:36]), op=ALU.mult),
                [t_ps[bo], tsc], [ton])
            P.op("pool", lambda e, on=on: e.tensor_tensor(out=on, in0=on, in1=ggdn_b.unsqueeze(1).to_broadcast([128, 4, 128]),
                                                          op=ALU.mult), [ton, t_ggdn], [ton])
            P.op("pool", lambda e, on=on, og=og, ti=ti: e.tensor_tensor(
                out=og, in0=on, in1=sgz[:, ti, :].rearrange("p (h c) -> p h c", h=4), op=ALU.mult),
                [ton, t_sgz], [tog])
            bt = bank()
            pbt = psb16[bt]
            for h in range(4):
                P.op("pe", lambda e, h=h, og=og, pbt=pbt: e.transpose(out=pbt[:, h * 128:(h + 1) * 128], in_=og[:, h, :],
                                                                      identity=identb), [tog, t_identb], [t_ps[bt]])
            P.op("act", lambda e, pbt=pbt: e.activation(out=o_gdnT[:, :, c0:c0 + 128],
                                                        in_=pbt[:, 0:512].rearrange("p (h c) -> p h c", h=4), func=AF.Copy),
                 [t_ps[bt]], [t_ogT])
        def run_to_seq(gens):
            live = list(gens)
            while live:
                for g_ in list(live):
                    try:
                        if next(g_) == 'SEQ':
                            live.remove(g_)
                    except StopIteration:
                        live.remove(g_)

        def finish_gen(g_):
            for _ in g_:
                pass

        for t2 in range(0, NT if STOP > 1.8 else 2, 2):
            ga_, gb_ = gdn_tile(t2), gdn_tile(t2 + 1)
            run_to_seq([ga_, gb_])
            finish_gen(ga_)
            finish_gen(gb_)
        P.dma("sp", ssm_p[sq * 512:(sq + 1) * 512, :].rearrange("(h d) v -> d h v", h=4), S, reads=[t_S])

    NIT = 18

    def dsa_phase(sq, o_atT, t_oatT):
        row_base = sq * SEQ
        qT2, t_qT2 = sbt("qT2", [128, NT, 512], BF16)
        kT2, t_kT2 = sbt("kT2", [128, SEQ], BF16)
        v_b, t_vb = sbt("v_b", [128, NT, 128], BF16)
        qiT2, t_qiT2 = sbt("qiT2", [128, 2, SEQ], BF16)
        kiT2, t_kiT2 = sbt("kiT2", [128, SEQ], BF16)
        wi_s, t_wi = sbt("wi_s", [128, NT, 4])
        A_TOP = k.top
        alloc_stage(1536)
        wA, t_wA = sbt("wA", [128, 8, 1092], BF16)
        load_weight(wA, t_wA, w_in, 0, 1092, gA, t_gA)
        wConv, t_wConv = sbt("wConv", [128, 8, 1536], BF16)
        load_weight(wConv, t_wConv, w_in, 1092, 2628, gA, t_gA)
        hT1, t_hT1 = sbt("hT1", [128, 8, 128], BF16)
        ko = [sbt("ko%d" % i, [128, 320]) for i in range(2)]
        cvo, t_cvo = sbt("cvo", [128, 1536])
        qnb, t_qnb = sbt("qnb", [128, 512], BF16)
        kb_, t_kb = sbt("kb_", [128, 128], BF16)
        qib, t_qib = sbt("qib", [128, 256], BF16)
        kib, t_kib = sbt("kib", [128, 128], BF16)

        def proj_tile(t):
            r0 = row_base + t * 128
            c0 = t * 128
            norm_transpose(xp[r0:r0 + 128, :], 128, hT1, t_hT1, 0, 0)
            for (b, a0, a1) in ((1, 0, 512), (2, 512, 1024), (3, 1024, 1092)):
                for kc in range(8):
                    P.op("pe", lambda e, kc=kc, b=b, a0=a0, a1=a1: e.matmul(
                        ps[b][:, 0:a1 - a0], lhsT=hT1[:, kc, :], rhs=wA[:, kc, a0:a1],
                        start=(kc == 0), stop=(kc == 7)), [t_hT1, t_wA], [t_ps[b]])
            o, to = ko[t % 2]
            if PCUT <= 1:
                return
            head_rms(ps[1], t_ps[1], 8, qscr, t_qscr, qss, t_qss)
            P.op("dve", lambda e: e.tensor_tensor(
                out=qnb.rearrange("p (r g d) -> p g r d", r=4, g=2),
                in0=ps[1].rearrange("p (g r d) -> p g r d", g=2, r=4),
                in1=qss[:, 8:16].rearrange("p (g r) -> p g r", g=2).unsqueeze(3).to_broadcast([128, 2, 4, 64]),
                op=ALU.mult), [t_ps[1], t_qss], [t_qnb])
            pbq = psb16[4]
            for r in range(4):
                P.op("pe", lambda e, r=r: e.transpose(out=pbq[:, r * 128:(r + 1) * 128], in_=qnb[:, r * 128:(r + 1) * 128],
                                                      identity=identb), [t_qnb, t_identb], [t_ps[4]])
            P.op("act", lambda e: e.activation(out=qT2[:, t, :], in_=pbq[:, 0:512], func=AF.Copy),
                 [t_ps[4]], [t_qT2])
            P.op("pool", lambda e: e.tensor_scalar(out=qT2[:, t, :], in0=qT2[:, t, :], scalar1=gq8[:, 0:1], scalar2=1.0,
                                                   op0=ALU.mult, op1=ALU.mult), [t_qT2, t_gq8], [t_qT2])
            if PCUT <= 2:
                return
            head_rms(ps[2][:, 0:128], t_ps[2], 2, qscr, t_qscr, qss, t_qss)
            P.op("dve", lambda e: e.tensor_tensor(
                out=o[:, 0:128].rearrange("p (h d) -> p h d", h=2),
                in0=ps[2][:, 0:128].rearrange("p (h d) -> p h d", h=2),
                in1=qss[:, 2:4].unsqueeze(2).to_broadcast([128, 2, 64]), op=ALU.mult),
                [t_ps[2], t_qss], [to])
            P.op("pool", lambda e: e.tensor_tensor(
                out=o[:, 0:128].rearrange("p (h d) -> p h d", h=2),
                in0=o[:, 0:128].rearrange("p (h d) -> p h d", h=2),
                in1=gk_b.unsqueeze(1).to_broadcast([128, 2, 64]), op=ALU.mult), [to, t_gk], [to])
            P.op("act", lambda e: e.activation(out=kb_, in_=o[:, 0:128], func=AF.Copy), [to], [t_kb])
            pb5 = psb16[5]
            P.op("pe", lambda e: e.transpose(out=pb5[:, 0:128], in_=kb_, identity=identb), [t_kb, t_identb], [t_ps[5]])
            if PCUT <= 3:
                return
            P.op("act", lambda e: e.activation(out=o[:, 128:256], in_=ps[2][:, 128:256], func=AF.Copy), [t_ps[2]], [to])
            P.op("act", lambda e: e.activation(out=v_b[:, t, :], in_=ps[2][:, 128:256], func=AF.Copy), [t_ps[2]], [t_vb])
            P.op("act", lambda e: e.activation(out=qib, in_=ps[2][:, 256:512], func=AF.Copy, scale=0.125),
                 [t_ps[2]], [t_qib])
            for a in range(2):
                P.op("pe", lambda e, a=a: e.transpose(out=pb5[:, 128 + a * 128:256 + a * 128],
                                                      in_=qib[:, a * 128:(a + 1) * 128], identity=identb),
                     [t_qib, t_identb], [t_ps[5]])
            if PCUT <= 4:
                return
            P.op("act", lambda e: e.activation(out=o[:, 256:320], in_=ps[3][:, 0:64], func=AF.Copy), [t_ps[3]], [to])
            P.op("act", lambda e: e.activation(out=kib[:, 0:64], in_=ps[3][:, 0:64], func=AF.Copy), [t_ps[3]], [t_kib])
            P.op("act", lambda e: e.activation(out=kib[:, 64:128], in_=ps[3][:, 0:64], func=AF.Copy), [t_ps[3]], [t_kib])
            P.op("pe", lambda e: e.transpose(out=pb5[:, 384:512], in_=kib, identity=identb), [t_kib, t_identb], [t_ps[5]])
            P.op("act", lambda e: e.activation(out=wi_s[:, t, :], in_=ps[3][:, 64:68], func=AF.Copy, scale=0.5),
                 [t_ps[3]], [t_wi])
            if PCUT <= 5:
                return
            P.op("act", lambda e: e.activation(out=kT2[:, c0:c0 + 128], in_=pb5[:, 0:128], func=AF.Copy), [t_ps[5]], [t_kT2])
            if PCUT == 51:
                return
            P.op("act", lambda e: e.activation(out=qiT2[:, :, c0:c0 + 128],
                                               in_=pb5[:, 128:384].rearrange("p (a t) -> p a t", a=2), func=AF.Copy),
                 [t_ps[5]], [t_qiT2])
            if PCUT == 52:
                return
            P.op("act", lambda e: e.activation(out=kiT2[:, c0:c0 + 128], in_=pb5[:, 384:512], func=AF.Copy),
                 [t_ps[5]], [t_kiT2])
            if PCUT == 53:
                return
            P.dma("sp", k_p[r0:r0 + 128, :], o[:, 0:128], reads=[to])
            P.dma("sp", v_p[r0:r0 + 128, :], o[:, 128:256], reads=[to])
            P.dma("sp", kidx_p[r0:r0 + 128, :], o[:, 256:320], reads=[to])
            if PCUT <= 6:
                return
            if t == NT - 1:
                for b in range(3):
                    for kc in range(8):
                        P.op("pe", lambda e, kc=kc, b=b: e.matmul(
                            ps[5 + b] if b < 2 else ps[0], lhsT=hT1[:, kc, :], rhs=wConv[:, kc, b * 512:(b + 1) * 512],
                            start=(kc == 0), stop=(kc == 7)), [t_hT1, t_wConv], [t_ps[5 + b] if b < 2 else t_ps[0]])
                for b in range(3):
                    bb = 5 + b if b < 2 else 0
                    P.op("act", lambda e, b=b, bb=bb: e.activation(out=cvo[:, b * 512:(b + 1) * 512], in_=ps[bb],
                                                                   func=AF.Copy), [t_ps[bb]], [t_cvo])
                P.dma("sp", conv_p[sq * 3:(sq + 1) * 3, :], cvo[125:128, :], reads=[t_cvo])

        for t in range(NT):
            proj_tile(t)
        P.barrier()
        if DCUT <= 1:
            return
        k.top = A_TOP
        scb = [sbt("scb%d" % i, [128, SEQ]) for i in range(2)]
        rl = [sbt("rl%d" % i, [128, 512]) for i in range(4)]
        junk2 = [sbt("junk%d" % i, [128, SEQ], BF16) for i in range(2)]
        mask2 = [sbt("mask%d" % i, [128, SEQ], BF16) for i in range(2)]
        maskT2 = [sbt("maskT%d" % i, [128, NT, 128], BF16) for i in range(4)]
        PT = [sbt("PT%d" % i, [128, 4, 128], BF16) for i in range(6)]
        bis2 = [sbt("bis%d" % i, [128, 64]) for i in range(2)]
        rec, t_rec = sbt("rec", [128, 4, 128])

        def q_sb(qt):
            L = (qt + 1) * 128
            q0 = qt * 128
            S_, tS = scb[qt % 2]
            maskT, t_maskT = maskT2[qt % 4]
            bis, t_bis = bis2[qt % 2]
            junk, t_junk = junk2[qt % 2]
            mask, t_mask = mask2[qt % 2]
            nch = (L + 511) // 512
            cnt_ = 0
            for ch in range(nch):
                k0 = ch * 512
                n = min(512, L - k0)
                for ih in range(4):
                    a, b = ih // 2, ih % 2
                    bnk = qt % 2
                    r_, tr = rl[(qt % 2) * 2 + cnt_ % 2]
                    cnt_ += 1
                    P.op("pe", lambda e, a=a, b=b, bnk=bnk, k0=k0, n=n: e.matmul(
                        ps[bnk][:, 0:n], lhsT=qiT2[64 * b:64 * b + 64, a, q0:q0 + 128],
                        rhs=kiT2[64 * b:64 * b + 64, k0:k0 + n], start=True, stop=True),
                        [t_qiT2, t_kiT2], [t_ps[bnk]])
                    yield
                    P.op("act", lambda e, r_=r_, bnk=bnk, n=n: e.activation(out=r_[:, 0:n], in_=ps[bnk][:, 0:n], func=AF.Relu),
                         [t_ps[bnk]], [tr])
                    yield
                    if ih == 0:
                        P.op("dve", lambda e, r_=r_, k0=k0, n=n: e.tensor_scalar(
                            out=S_[:, k0:k0 + n], in0=r_[:, 0:n], scalar1=wi_s[:, qt, 0:1], scalar2=None, op0=ALU.mult),
                            [tr, t_wi], [tS])
                        yield
                    else:
                        P.op("dve", lambda e, r_=r_, k0=k0, n=n, ih=ih: e.scalar_tensor_tensor(
                            out=S_[:, k0:k0 + n], in0=r_[:, 0:n], scalar=wi_s[:, qt, ih:ih + 1], in1=S_[:, k0:k0 + n],
                            op0=ALU.mult, op1=ALU.add), [tr, t_wi, tS], [tS])
                        yield
            if DCUT <= 2:
                return
            dg = S_[:, q0:q0 + 128]
            P.op("dve", lambda e: e.tensor_tensor(out=dg, in0=dg, in1=lowinc, op=ALU.mult), [tS, t_lowinc], [tS])
            yield
            P.op("dve", lambda e: e.tensor_reduce(out=bis[:, 0:1], in_=S_[:, 0:L], axis=AX.X, op=ALU.max), [tS], [t_bis])
            yield
            P.op("dve", lambda e: e.tensor_reduce(out=bis[:, 1:2], in_=S_[:, 0:L], axis=AX.X, op=ALU.min), [tS], [t_bis])
            yield
            P.op("pool", lambda e: e.tensor_tensor(out=dg, in0=dg, in1=tribias, op=ALU.add), [tS, t_tribias], [tS])
            yield
            P.op("dve", lambda e: e.tensor_tensor(out=bis[:, 2:3], in0=bis[:, 0:1], in1=bis[:, 1:2], op=ALU.subtract),
                 [t_bis], [t_bis])
            yield
            P.op("dve", lambda e: e.tensor_scalar(out=bis[:, 3:4], in0=bis[:, 2:3], scalar1=0.5005, scalar2=0.0005,
                                                  op0=ALU.mult, op1=ALU.add), [t_bis], [t_bis])
            yield
            P.op("dve", lambda e: e.tensor_tensor(out=bis[:, 4:5], in0=bis[:, 0:1], in1=bis[:, 3:4], op=ALU.subtract),
                 [t_bis], [t_bis])
            yield
            P.op("dve", lambda e: e.tensor_scalar(out=bis[:, 8:9 + NIT], in0=pow2_b[:, 0:NIT + 1], scalar1=bis[:, 3:4],
                                                  scalar2=None, op0=ALU.mult), [t_bis, t_pow2], [t_bis])
            yield
            for it in range(NIT):
                P.op("dve", lambda e: e.tensor_scalar(out=junk[:, 0:L], in0=S_[:, 0:L], scalar1=bis[:, 4:5], scalar2=None,
                                                      op0=ALU.is_ge, op1=ALU.add, accum_out=bis[:, 5:6]),
                     [tS, t_bis], [t_junk, t_bis])
                yield
                P.op("dve", lambda e: e.tensor_scalar(out=bis[:, 6:7], in0=bis[:, 5:6], scalar1=255.5, scalar2=0.5,
                                                      op0=ALU.is_ge, op1=ALU.subtract), [t_bis], [t_bis])
                yield
                P.op("dve", lambda e, it=it: e.scalar_tensor_tensor(out=bis[:, 4:5], in0=bis[:, 6:7], scalar=bis[:, 8 + it:9 + it],
                                                                    in1=bis[:, 4:5], op0=ALU.mult, op1=ALU.add),
                     [t_bis], [t_bis])
                yield
            P.op("dve", lambda e: e.tensor_tensor(out=bis[:, 7:8], in0=bis[:, 4:5], in1=bis[:, 8 + NIT:9 + NIT], op=ALU.subtract),
                 [t_bis], [t_bis])
            yield
            P.op("dve", lambda e: e.tensor_scalar(out=mask[:, 0:L], in0=S_[:, 0:L], scalar1=bis[:, 7:8], scalar2=None,
                                                  op0=ALU.is_ge), [tS, t_bis], [t_mask])
            yield
            if DCUT <= 3:
                return
            for half in range((qt // 8) + 1):
                nb = min(8, qt + 1 - half * 8)
                for j in range(nb):
                    kb = half * 8 + j
                    P.op("pe", lambda e, kb=kb, j=j, half=half: e.transpose(
                        out=psb16[qt % 2][:, j * 128:(j + 1) * 128], in_=mask[:, kb * 128:(kb + 1) * 128], identity=identb),
                        [t_mask, t_identb], [t_ps[qt % 2]])
                    yield
                P.op("act", lambda e, half=half, nb=nb: e.activation(
                    out=maskT[:, half * 8:half * 8 + nb, :],
                    in_=psb16[qt % 2][:, 0:nb * 128].rearrange("p (j t) -> p j t", j=nb), func=AF.Copy),
                    [t_ps[qt % 2]], [t_maskT])
                yield
        def q_att(qt):
            L = (qt + 1) * 128
            q0 = qt * 128
            maskT, t_maskT = maskT2[qt % 4]
            items = [(kb, g) for kb in range(qt + 1) for g in range(2)]
            LA = 2

            def front(idx):
                kb, g = items[idx]
                bS = 2 + (idx % 4)
                PTt, tPT = PT[idx % len(PT)]
                P.op("pe", lambda e: e.matmul(
                    ps[bS], lhsT=kT2[64 * g:64 * g + 64, kb * 128:(kb + 1) * 128],
                    rhs=qT2[64 * g:64 * g + 64, qt, :], start=True, stop=True),
                    [t_kT2, t_qT2], [t_ps[bS]])
                P.op("act", lambda e: e.activation(out=PTt.rearrange("p r t -> p (r t)"), in_=ps[bS],
                                                   func=AF.Exp), [t_ps[bS]], [tPT])
                P.op("pool", lambda e: e.tensor_tensor(
                    out=PTt, in0=PTt, in1=maskT[:, kb, :].unsqueeze(1).to_broadcast([128, 4, 128]), op=ALU.mult),
                    [tPT, t_maskT], [tPT])

            def back(idx):
                kb, g = items[idx]
                PTt, tPT = PT[idx % len(PT)]
                P.op("pe", lambda e: e.matmul(
                    ps[6][64 * g:64 * g + 64, :], lhsT=v_b[:, kb, 64 * g:64 * g + 64],
                    rhs=PTt.rearrange("p r t -> p (r t)"), start=(kb == 0), stop=(kb == qt)),
                    [tPT, t_vb], [t_ps[6]])
                P.op("pe", lambda e: e.matmul(
                    ps[7][64 * g:64 * g + 64, :], lhsT=onesb[:, 0:64],
                    rhs=PTt.rearrange("p r t -> p (r t)"), start=(kb == 0), stop=(kb == qt)),
                    [tPT, t_onesb], [t_ps[7]])

            n_it = len(items)
            for idx in range(n_it + LA):
                if idx < n_it:
                    front(idx)
                if idx - LA >= 0:
                    back(idx - LA)
            P.op("act", lambda e: e.activation(out=rec.rearrange("p r t -> p (r t)"), in_=ps[7], func=AF.Ln), [t_ps[7]], [t_rec])
            P.op("act", lambda e: e.activation(out=rec.rearrange("p r t -> p (r t)"), in_=rec.rearrange("p r t -> p (r t)"),
                                               func=AF.Exp, scale=-1.0), [t_rec], [t_rec])
            P.op("dve", lambda e: e.tensor_tensor(out=o_atT[:, :, q0:q0 + 128],
                                                  in0=ps[6].rearrange("p (r t) -> p r t", r=4), in1=rec, op=ALU.mult),
                 [t_ps[6], t_rec], [t_oatT])

        def lockstep(gens):
            gens = list(gens)
            while gens:
                for g_ in list(gens):
                    try:
                        next(g_)
                    except StopIteration:
                        gens.remove(g_)

        lockstep([q_sb(0), q_sb(1)])
        for p_ in range(0, NT, 2):
            if p_ + 2 < NT:
                lockstep([q_sb(p_ + 2), q_sb(p_ + 3)])
            q_att(p_)
            q_att(p_ + 1)

    def c1_phase(sq, o_atT, t_oatT, o_gdnT, t_ogT, mkT2, t_mkT2, mv_b, t_mv):
        row_base = sq * SEQ
        alloc_stage(1024)
        Wo_a, t_Woa = sbt("Wo_a", [128, 4, 1024], BF16)
        Wo_g, t_Wog = sbt("Wo_g", [128, 4, 1024], BF16)
        Wmq, t_Wmq = sbt("Wmq", [128, 8, 256], BF16)
        Wmo, t_Wmo = sbt("Wmo", [128, 2, 1024], BF16)
        for r in range(4):
            i = k.wl % 2
            k.wl += 1
            st = stage[i][:, 0:1024]
            for g in range(2):
                P.dma("sp", st[64 * g:64 * g + 64, :], w_out[256 * g + 64 * r:256 * g + 64 * r + 64, :],
                      writes=[t_stage[i]])
            P.op("act", lambda e, st=st, r=r: e.activation(out=Wo_a[:, r, :], in_=st, func=AF.Copy),
                 [t_stage[i]], [t_Woa])
        for h in range(4):
            load_rows(Wo_g[:, h, :], t_Wog, w_out[512 + 128 * h:512 + 128 * h + 128, :], 1024, None, None)
        load_weight(Wmq, t_Wmq, w_mq, 0, 256, gX, t_gX)
        for a in range(2):
            load_rows(Wmo[:, a, :], t_Wmo, w_mo[128 * a:128 * a + 128, :], 1024, None, None)
        x1 = [sbt("x1_%d" % i, [128, D]) for i in range(2)]
        hT2, t_hT2 = sbt("hT2", [128, 8, 128], BF16)
        qmb, t_qmb = sbt("qmb", [128, 256], BF16)
        qmT2, t_qmT2 = sbt("qmT2", [128, 2, 128], BF16)
        PTm, t_PTm = sbt("PTm", [128, 2, 4, 128], BF16)
        omT2, t_omT2 = sbt("omT2", [128, 2, 128], BF16)
        recm, t_recm = sbt("recm", [128, 256])

        def c1_tile(t):
            r0 = row_base + t * 128
            c0 = t * 128
            if CCUT <= 0:
                return
            x, tx, i = load_x(xp[r0:r0 + 128, :], 128)
            xx, txx = x1[t % 2]
            for c in range(2):
                for r in range(4):
                    P.op("pe", lambda e, r=r, c=c: e.matmul(ps[1 + c], lhsT=o_atT[:, r, c0:c0 + 128],
                                                            rhs=Wo_a[:, r, c * 512:(c + 1) * 512], start=(r == 0), stop=False),
                         [t_oatT, t_Woa], [t_ps[1 + c]])
                for h in range(4):
                    P.op("pe", lambda e, h=h, c=c: e.matmul(ps[1 + c], lhsT=o_gdnT[:, h, c0:c0 + 128],
                                                            rhs=Wo_g[:, h, c * 512:(c + 1) * 512], start=False, stop=(h == 3)),
                         [t_ogT, t_Wog], [t_ps[1 + c]])
                P.op("dve", lambda e, c=c: e.tensor_tensor(out=xx[:, c * 512:(c + 1) * 512], in0=ps[1 + c],
                                                           in1=x[:, c * 512:(c + 1) * 512], op=ALU.add),
                     [t_ps[1 + c], tx], [txx])
            if CCUT <= 1:
                return
            norm_T(xx, txx, i, hT2, t_hT2, 0, 0)
            for kc in range(8):
                P.op("pe", lambda e, kc=kc: e.matmul(ps[3][:, 0:256], lhsT=hT2[:, kc, :], rhs=Wmq[:, kc, :],
                                                     start=(kc == 0), stop=(kc == 7)), [t_hT2, t_Wmq], [t_ps[3]])
            if CCUT <= 2:
                return
            head_rms(ps[3][:, 0:256], t_ps[3], 4, qscr, t_qscr, qss, t_qss)
            P.op("dve", lambda e: e.tensor_tensor(
                out=qmb.rearrange("p (h d) -> p h d", h=4), in0=ps[3][:, 0:256].rearrange("p (h d) -> p h d", h=4),
                in1=qss[:, 4:8].unsqueeze(2).to_broadcast([128, 4, 64]), op=ALU.mult), [t_ps[3], t_qss], [t_qmb])
            pb4 = psb16[4]
            for a in range(2):
                P.op("pe", lambda e, a=a: e.transpose(out=pb4[:, a * 128:(a + 1) * 128], in_=qmb[:, a * 128:(a + 1) * 128],
                                                      identity=identb), [t_qmb, t_identb], [t_ps[4]])
            P.op("act", lambda e: e.activation(out=qmT2.rearrange("p a t -> p (a t)"), in_=pb4[:, 0:256], func=AF.Copy),
                 [t_ps[4]], [t_qmT2])
            P.op("pool", lambda e: e.tensor_scalar(out=qmT2.rearrange("p a t -> p (a t)"), in0=qmT2.rearrange("p a t -> p (a t)"),
                                                   scalar1=gmq8[:, 0:1], scalar2=1.0, op0=ALU.mult, op1=ALU.mult),
                 [t_qmT2, t_gmq8], [t_qmT2])
            if CCUT <= 3:
                return
            for b in range(2):
                for mb in range(2):
                    for a in range(2):
                        j = mb * 2 + a
                        P.op("pe", lambda e, mb=mb, a=a, b=b, j=j: e.matmul(
                            ps[5 + b][:, j * 128:(j + 1) * 128], lhsT=mkT2[64 * b:64 * b + 64, a, mb * 128:(mb + 1) * 128],
                            rhs=qmT2[64 * b:64 * b + 64, a, :], start=True, stop=True), [t_mkT2, t_qmT2], [t_ps[5 + b]])
                P.op("act", lambda e, b=b: e.activation(out=PTm[:, b, :, :].rearrange("p j t -> p (j t)"),
                                                        in_=ps[5 + b], func=AF.Exp), [t_ps[5 + b]], [t_PTm])
            if CCUT <= 4:
                return
            for mh in range(4):
                a, b = mh // 2, mh % 2
                for mb in range(2):
                    P.op("pe", lambda e, mb=mb, mh=mh, a=a, b=b: e.matmul(
                        ps[7][64 * b:64 * b + 64, a * 128:(a + 1) * 128], lhsT=mv_b[:, mb, mh * 64:(mh + 1) * 64],
                        rhs=PTm[:, b, mb * 2 + a, :], start=(mb == 0), stop=(mb == 1)), [t_mv, t_PTm], [t_ps[7]])
                for mb in range(2):
                    P.op("pe", lambda e, mb=mb, mh=mh, a=a, b=b: e.matmul(
                        ps[7][64 * b:64 * b + 64, 256 + a * 128:256 + (a + 1) * 128], lhsT=onesb[:, 0:64],
                        rhs=PTm[:, b, mb * 2 + a, :], start=(mb == 0), stop=(mb == 1)), [t_onesb, t_PTm], [t_ps[7]])
            if CCUT <= 5:
                return
            P.op("dve", lambda e: e.reciprocal(out=recm, in_=ps[7][:, 256:512]), [t_ps[7]], [t_recm])
            P.op("dve", lambda e: e.tensor_tensor(out=omT2.rearrange("p a t -> p (a t)"), in0=ps[7][:, 0:256], in1=recm,
                                                  op=ALU.mult), [t_ps[7], t_recm], [t_omT2])
            for c in range(2):
                for a in range(2):
                    P.op("pe", lambda e, a=a, c=c: e.matmul(ps[1 + c], lhsT=omT2[:, a, :],
                                                            rhs=Wmo[:, a, c * 512:(c + 1) * 512], start=(a == 0), stop=(a == 1)),
                         [t_omT2, t_Wmo], [t_ps[1 + c]])
                P.op("dve", lambda e, c=c: e.tensor_tensor(out=xx[:, c * 512:(c + 1) * 512], in0=ps[1 + c],
                                                           in1=xx[:, c * 512:(c + 1) * 512], op=ALU.add),
                     [t_ps[1 + c], txx], [txx])
            P.dma("sp", y_p[r0:r0 + 128, :], xx, reads=[txx])

        for t in range(NT):
            c1_tile(t)

    t_yscr = Tok()

    def c2_phase(sq):
        row_base = sq * SEQ
        alloc_stage(2816)
        Wg, t_Wg = sbt("Wg", [128, 8, 2816], BF16)
        Wu, t_Wu = sbt("Wu", [128, 8, 2816], BF16)
        Wd, t_Wd = sbt("Wd", [128, 22, 1024], BF16)
        load_weight(Wg, t_Wg, w_gate, 0, 2816, gF, t_gF)
        load_weight(Wu, t_Wu, w_up, 0, 2816, gF, t_gF)
        for f in range(22):
            load_rows(Wd[:, f, :], t_Wd, w_down[128 * f:128 * f + 128, :], 1024, None, None)
        hT3, t_hT3 = sbt("hT3", [128, 8, 256], BF16)
        hfT, t_hfT = sbt("hfT", [128, 22, 256], BF16)
        sg = [sbt("sg%d" % i, [128, 256]) for i in range(2)]

        def c2_group(gi):
            xs_ = []
            for t2 in range(2):
                r0 = row_base + (gi * 2 + t2) * 128
                x, tx, i = load_x(y_p[r0:r0 + 128, :], 128)
                norm_T(x, tx, i, hT3, t_hT3, t2 * 128, 0)
                xs_.append((x, tx, r0))
            for f in range(22):
                b = 1 + (f % 2)
                for kc in range(8):
                    P.op("pe", lambda e, kc=kc, f=f, b=b: e.matmul(ps[b][:, 0:256], lhsT=Wg[:, kc, 128 * f:128 * f + 128],
                                                                   rhs=hT3[:, kc, :], start=(kc == 0), stop=(kc == 7)),
                         [t_Wg, t_hT3], [t_ps[b]])
                for kc in range(8):
                    P.op("pe", lambda e, kc=kc, f=f, b=b: e.matmul(ps[b][:, 256:512], lhsT=Wu[:, kc, 128 * f:128 * f + 128],
                                                                   rhs=hT3[:, kc, :], start=(kc == 0), stop=(kc == 7)),
                         [t_Wu, t_hT3], [t_ps[b]])
                s_, ts_ = sg[f % 2]
                P.op("act", lambda e, s_=s_, b=b: e.activation(out=s_, in_=ps[b][:, 0:256], func=AF.Silu), [t_ps[b]], [ts_])
                P.op("dve", lambda e, s_=s_, b=b, f=f: e.tensor_tensor(out=hfT[:, f, :], in0=s_, in1=ps[b][:, 256:512],
                                                                       op=ALU.mult), [ts_, t_ps[b]], [t_hfT])
            for t2 in range(2):
                x, tx, r0 = xs_[t2]
                for c in range(2):
                    for f in range(22):
                        P.op("pe", lambda e, f=f, c=c, t2=t2: e.matmul(ps[3 + c], lhsT=hfT[:, f, t2 * 128:(t2 + 1) * 128],
                                                                       rhs=Wd[:, f, c * 512:(c + 1) * 512],
                                                                       start=(f == 0), stop=(f == 21)),
                             [t_hfT, t_Wd], [t_ps[3 + c]])
                    P.op("dve", lambda e, c=c, x=x: e.tensor_tensor(out=x[:, c * 512:(c + 1) * 512], in0=ps[3 + c],
                                                                    in1=x[:, c * 512:(c + 1) * 512], op=ALU.add),
                         [t_ps[3 + c], tx], [tx])
                P.dma("sp", y_p[r0:r0 + 128, :], x, reads=[tx])

        for gi in range(NT // 2):
            c2_group(gi)

    def sample_group():
        k.top = PERSIST_TOP
        NSB = NS
        proj, t_proj = sbt("s_proj", [128, INW])
        x_keep, t_xk = sbt("s_xkeep", [128, D])
        sso, t_sso = sbt("s_o", [128, 320])
        ss_, t_ss = sbt("s_ss", [128, 64])
        scr, t_scr = sbt("s_scr", [128, 1536])
        S_TOP = k.top
        alloc_stage(INW)
        wAll, t_wAll = sbt("s_wAll", [128, 8, INW], BF16)
        load_weight(wAll, t_wAll, w_in, 0, INW, gA, t_gA)
        hTs, t_hTs = sbt("s_hT", [128, 8, 128], BF16)
        x, tx, xi = load_x(xs[:, :], NSB)
        P.op("pool", lambda e: e.tensor_copy(out=x_keep[0:NSB, :], in_=x[0:NSB, :]), [tx], [t_xk])
        norm_T(x, tx, xi, hTs, t_hTs, 0, 0)
        for c in range(7):
            c0 = c * 512
            n = min(512, INW - c0)
            b = 1 + (c % 2)
            for kc in range(8):
                P.op("pe", lambda e, kc=kc, b=b, c0=c0, n=n: e.matmul(
                    ps[b][0:NSB, 0:n], lhsT=hTs[:, kc, 0:NSB], rhs=wAll[:, kc, c0:c0 + n],
                    start=(kc == 0), stop=(kc == 7)), [t_hTs, t_wAll], [t_ps[b]])
            P.op("act", lambda e, b=b, c0=c0, n=n: e.activation(out=proj[0:NSB, c0:c0 + n], in_=ps[b][0:NSB, 0:n],
                                                                func=AF.Copy), [t_ps[b]], [t_proj])
        pj = proj[0:NSB]
        so = sso[0:NSB]
        P.op("dve", lambda e: e.tensor_tensor(out=scr[0:NSB, 0:128], in0=pj[:, 512:640], in1=pj[:, 512:640], op=ALU.mult),
             [t_proj], [t_scr])
        P.op("dve", lambda e: e.tensor_reduce(out=ss_[0:NSB, 0:2], in_=scr[0:NSB, 0:128].rearrange("p (h d) -> p h d", h=2),
                                              axis=AX.X, op=ALU.add), [t_scr], [t_ss])
        P.op("pool", lambda e: e.tensor_scalar(out=ss_[0:NSB, 2:4], in0=ss_[0:NSB, 0:2], scalar1=1.0 / 64, scalar2=EPS,
                                               op0=ALU.mult, op1=ALU.add), [t_ss], [t_ss])
        P.op("pool", lambda e: e.tensor_tensor(out=ss_[0:NSB, 2:4], in0=ss_[0:NSB, 2:4], in1=neghalf[0:NSB, 0:2], op=ALU.pow),
             [t_ss, t_nh], [t_ss])
        P.op("dve", lambda e: e.tensor_tensor(out=so[:, 0:128].rearrange("p (h d) -> p h d", h=2),
                                              in0=pj[:, 512:640].rearrange("p (h d) -> p h d", h=2),
                                              in1=ss_[0:NSB, 2:4].unsqueeze(2).to_broadcast([NSB, 2, 64]), op=ALU.mult),
             [t_proj, t_ss], [t_sso])
        P.op("pool", lambda e: e.tensor_tensor(out=so[:, 0:128].rearrange("p (h d) -> p h d", h=2),
                                               in0=so[:, 0:128].rearrange("p (h d) -> p h d", h=2),
                                               in1=gk_b[0:NSB].unsqueeze(1).to_broadcast([NSB, 2, 64]), op=ALU.mult),
             [t_sso, t_gk], [t_sso])
        P.op("act", lambda e: e.activation(out=so[:, 128:256], in_=pj[:, 640:768], func=AF.Copy), [t_proj], [t_sso])
        P.op("act", lambda e: e.activation(out=so[:, 256:320], in_=pj[:, 1024:1088], func=AF.Copy), [t_proj], [t_sso])
        P.dma("sp", k_s[:, :], so[:, 0:128], reads=[t_sso])
        P.dma("sp", v_s[:, :], so[:, 128:256], reads=[t_sso])
        P.dma("sp", kidx_s[:, :], so[:, 256:320], reads=[t_sso])
        conv_s3 = conv_s.rearrange("(s r) c -> s r c", r=3)
        st_conv3 = st_conv.rearrange("(s r) c -> s r c", r=3)
        P.dma("sp", conv_s3[:, 2, :], pj[:, 1092:2628], reads=[t_proj])
        P.dma("sp", conv_s3[:, 0:2, :], st_conv3[:, 1:3, :])
        P.barrier()
        k.top = S_TOP
        stc, t_stc = sbt("s_stc", [128, 3, 1536])
        cwb, t_cwb = sbt("s_cwb", [128, 4, 1536])
        P.dma("sp", stc[0:NSB], st_conv3, writes=[t_stc])
        P.dma("sp", cwb[0:NSB], conv_w.rearrange("t c -> (t c)").partition_broadcast(NSB), writes=[t_cwb])
        cc, t_cc = sbt("s_cc", [128, 1536])
        c_ = cc[0:NSB]
        sc_ = scr[0:NSB]
        P.op("dve", lambda e: e.tensor_tensor(out=c_, in0=pj[:, 1092:2628], in1=cwb[0:NSB, 3, :], op=ALU.mult),
             [t_proj, t_cwb], [t_cc])
        for j in range(3):
            P.op("dve", lambda e, j=j: e.tensor_tensor(out=sc_, in0=stc[0:NSB, j, :], in1=cwb[0:NSB, j, :], op=ALU.mult),
                 [t_stc, t_cwb], [t_scr])
            P.op("dve", lambda e: e.tensor_tensor(out=c_, in0=c_, in1=sc_, op=ALU.add), [t_cc, t_scr], [t_cc])
        P.op("act", lambda e: e.activation(out=c_, in_=c_, func=AF.Silu), [t_cc], [t_cc])
        P.op("dve", lambda e: e.tensor_tensor(out=sc_[:, 0:1024], in0=c_[:, 0:1024], in1=c_[:, 0:1024], op=ALU.mult),
             [t_cc], [t_scr])
        P.op("dve", lambda e: e.tensor_reduce(out=ss_[0:NSB, 8:16], in_=sc_[:, 0:1024].rearrange("p (h d) -> p h d", h=8),
                                              axis=AX.X, op=ALU.add), [t_scr], [t_ss])
        P.op("pool", lambda e: e.tensor_scalar(out=ss_[0:NSB, 16:24], in0=ss_[0:NSB, 8:16], scalar1=1.0, scalar2=EPS,
                                               op0=ALU.mult, op1=ALU.add), [t_ss], [t_ss])
        P.op("pool", lambda e: e.tensor_tensor(out=ss_[0:NSB, 16:24], in0=ss_[0:NSB, 16:24], in1=neghalf[0:NSB, 0:8], op=ALU.pow),
             [t_ss, t_nh], [t_ss])
        P.op("pool", lambda e: e.tensor_scalar(out=ss_[0:NSB, 16:20], in0=ss_[0:NSB, 16:20], scalar1=float(128.0 ** -0.5),
                                               scalar2=1.0, op0=ALU.mult, op1=ALU.mult), [t_ss], [t_ss])
        P.op("dve", lambda e: e.tensor_tensor(out=c_[:, 0:1024].rearrange("p (h d) -> p h d", h=8),
                                              in0=c_[:, 0:1024].rearrange("p (h d) -> p h d", h=8),
                                              in1=ss_[0:NSB, 16:24].unsqueeze(2).to_broadcast([NSB, 8, 128]), op=ALU.mult),
             [t_cc, t_ss], [t_cc])
        sv = ss_[0:NSB]
        P.op("dve", lambda e: e.tensor_tensor(out=sv[:, 24:28], in0=pj[:, 3140:3144], in1=dtb_b[0:NSB], op=ALU.add),
             [t_proj, t_dtb], [t_ss])
        P.op("dve", lambda e: e.tensor_scalar(out=sv[:, 28:32], in0=sv[:, 24:28], scalar1=-1.0, scalar2=None, op0=ALU.mult),
             [t_ss], [t_ss])
        P.op("dve", lambda e: e.tensor_tensor(out=sv[:, 28:32], in0=sv[:, 28:32], in1=sv[:, 24:28], op=ALU.min), [t_ss], [t_ss])
        P.op("act", lambda e: e.activation(out=sv[:, 28:32], in_=sv[:, 28:32], func=AF.Exp), [t_ss], [t_ss])
        P.op("act", lambda e: e.activation(out=sv[:, 28:32], in_=sv[:, 28:32], func=AF.Ln, bias=1.0, scale=1.0), [t_ss], [t_ss])
        P.op("dve", lambda e: e.scalar_tensor_tensor(out=sv[:, 32:36], in0=sv[:, 24:28], scalar=0.0, in1=sv[:, 28:32],
                                                     op0=ALU.max, op1=ALU.add), [t_ss], [t_ss])
        P.op("dve", lambda e: e.tensor_tensor(out=sv[:, 32:36], in0=sv[:, 32:36], in1=nea_b[0:NSB], op=ALU.mult),
             [t_ss, t_nea], [t_ss])
        P.op("act", lambda e: e.activation(out=sv[:, 36:40], in_=pj[:, 3144:3148], func=AF.Sigmoid), [t_proj], [t_ss])
        P.op("act", lambda e: e.activation(out=sv[:, 40:44], in_=sv[:, 32:36], func=AF.Exp), [t_ss], [t_ss])
        S0, t_S0 = sbt("s_S0", [128, NSB, 4, 128])
        for i in range(NSB):
            P.dma("sp", S0[:, i, :, :], ssm_in[i * 512:(i + 1) * 512, :].rearrange("(h d) v -> d h v", h=4), writes=[t_S0])
        kqT, t_kqT = sbt("s_kqT", [128, 8, NSB])
        for j in range(8):
            b = 1 + (j % 2)
            P.op("pe", lambda e, j=j, b=b: e.transpose(out=ps[b][:, 0:NSB], in_=c_[:, j * 128:(j + 1) * 128],
                                                       identity=identf[0:NSB, 0:NSB]), [t_cc, t_identf], [t_ps[b]])
            P.op("act", lambda e, j=j, b=b: e.activation(out=kqT[:, j, :], in_=ps[b][:, 0:NSB], func=AF.Copy),
                 [t_ps[b]], [t_kqT])
        eye_b, t_eyeb = sbt("s_eyeb", [128, NSB, NSB])
        P.dma("sp", eye_b, eye16_d.partition_broadcast(128), writes=[t_eyeb])
        kqTm, t_kqTm = sbt("s_kqTm", [128, 8, NSB, NSB])
        P.op("pool", lambda e: e.tensor_tensor(out=kqTm, in0=kqT.unsqueeze(2).to_broadcast([128, 8, NSB, NSB]),
                                               in1=eye_b.unsqueeze(1).to_broadcast([128, 8, NSB, NSB]), op=ALU.mult),
             [t_kqT, t_eyeb], [t_kqTm])
        for h in range(4):
            for i in range(NSB):
                P.op("pe", lambda e, h=h, i=i: e.matmul(ps[3][0:NSB, h * 128:(h + 1) * 128], lhsT=kqTm[:, 4 + h, i, :],
                                                        rhs=S0[:, i, h, :], start=(i == 0), stop=(i == NSB - 1)),
                     [t_kqTm, t_S0], [t_ps[3]])
        dl, t_dl = sbt("s_dl", [128, 4, 128])
        d_ = dl[0:NSB]
        bcs = lambda a: a.unsqueeze(2).to_broadcast([NSB, 4, 128])
        P.op("dve", lambda e: e.tensor_tensor(out=d_, in0=ps[3][0:NSB, :].rearrange("p (h v) -> p h v", h=4),
                                              in1=bcs(sv[:, 40:44]), op=ALU.mult), [t_ps[3], t_ss], [t_dl])
        P.op("dve", lambda e: e.tensor_tensor(out=d_, in0=c_[:, 1024:1536].rearrange("p (h v) -> p h v", h=4), in1=d_,
                                              op=ALU.subtract), [t_cc, t_dl], [t_dl])
        P.op("dve", lambda e: e.tensor_tensor(out=d_, in0=d_, in1=bcs(sv[:, 36:40]), op=ALU.mult), [t_dl, t_ss], [t_dl])
        ckm, t_ckm = sbt("s_ckm", [128, NSB, 512])
        P.op("pool", lambda e: e.tensor_tensor(out=ckm[0:NSB], in0=c_[:, 512:1024].unsqueeze(1).to_broadcast([NSB, NSB, 512]),
                                               in1=identf[0:NSB, 0:NSB].unsqueeze(2).to_broadcast([NSB, NSB, 512]), op=ALU.mult),
             [t_cc, t_identf], [t_ckm])
        adg, t_adg = sbt("s_adg", [128, NSB, 4])
        P.op("pool", lambda e: e.tensor_tensor(out=adg[0:NSB], in0=sv[:, 40:44].unsqueeze(1).to_broadcast([NSB, NSB, 4]),
                                               in1=identf[0:NSB, 0:NSB].unsqueeze(2).to_broadcast([NSB, NSB, 4]), op=ALU.mult),
             [t_ss, t_identf], [t_adg])
        P.op("pe", lambda e: e.matmul(ps[4][:, 0:NSB * 4], lhsT=onesf[0:NSB, :], rhs=adg[0:NSB].rearrange("p i h -> p (i h)"),
                                      start=True, stop=True), [t_adg, t_onesf], [t_ps[4]])
        abc, t_abc = sbt("s_abc", [128, NSB * 4])
        P.op("act", lambda e: e.activation(out=abc, in_=ps[4][:, 0:NSB * 4], func=AF.Copy), [t_ps[4]], [t_abc])
        for i in range(NSB):
            b = 5 + (i % 2)
            for h in range(4):
                P.op("pe", lambda e, h=h, i=i, b=b: e.matmul(ps[b][:, h * 128:(h + 1) * 128], lhsT=ckm[0:NSB, i, h * 128:(h + 1) * 128],
                                                             rhs=d_[:, h, :], start=True, stop=True), [t_ckm, t_dl], [t_ps[b]])
            for h in range(4):
                P.op("dve", lambda e, h=h, i=i, b=b: e.scalar_tensor_tensor(
                    out=S0[:, i, h, :], in0=S0[:, i, h, :], scalar=abc[:, i * 4 + h:i * 4 + h + 1],
                    in1=ps[b][:, h * 128:(h + 1) * 128], op0=ALU.mult, op1=ALU.add), [t_S0, t_abc, t_ps[b]], [t_S0])
            P.dma("sp", ssm_s[i * 512:(i + 1) * 512, :].rearrange("(h d) v -> d h v", h=4), S0[:, i, :, :], reads=[t_S0])
        for h in range(4):
            for i in range(NSB):
                P.op("pe", lambda e, h=h, i=i: e.matmul(ps[7][0:NSB, h * 128:(h + 1) * 128], lhsT=kqTm[:, h, i, :],
                                                        rhs=S0[:, i, h, :], start=(i == 0), stop=(i == NSB - 1)),
                     [t_kqTm, t_S0], [t_ps[7]])
        og, t_og = sbt("s_og", [128, 4, 128])
        o_ = og[0:NSB]
        P.op("act", lambda e: e.activation(out=sc_[:, 0:512], in_=ps[7][0:NSB, :], func=AF.Square), [t_ps[7]], [t_scr])
        P.op("dve", lambda e: e.tensor_reduce(out=sv[:, 44:48], in_=sc_[:, 0:512].rearrange("p (h v) -> p h v", h=4),
                                              axis=AX.X, op=ALU.add), [t_scr], [t_ss])
        P.op("pool", lambda e: e.tensor_scalar(out=sv[:, 48:52], in0=sv[:, 44:48], scalar1=1.0 / 128, scalar2=EPS,
                                               op0=ALU.mult, op1=ALU.add), [t_ss], [t_ss])
        P.op("pool", lambda e: e.tensor_tensor(out=sv[:, 48:52], in0=sv[:, 48:52], in1=neghalf[0:NSB, 0:4], op=ALU.pow),
             [t_ss, t_nh], [t_ss])
        P.op("dve", lambda e: e.tensor_tensor(out=o_, in0=ps[7][0:NSB, :].rearrange("p (h v) -> p h v", h=4),
                                              in1=bcs(sv[:, 48:52]), op=ALU.mult), [t_ps[7], t_ss], [t_og])
        P.op("pool", lambda e: e.tensor_tensor(out=o_, in0=o_, in1=ggdn_b[0:NSB].unsqueeze(1).to_broadcast([NSB, 4, 128]),
                                               op=ALU.mult), [t_og, t_ggdn], [t_og])
        P.op("act", lambda e: e.activation(out=sc_[:, 0:512], in_=pj[:, 2628:3140], func=AF.Silu), [t_proj], [t_scr])
        P.op("dve", lambda e: e.tensor_tensor(out=o_.rearrange("p h v -> p (h v)"), in0=o_.rearrange("p h v -> p (h v)"),
                                              in1=sc_[:, 0:512], op=ALU.mult), [t_og, t_scr], [t_og])
        P.op("pool", lambda e: e.tensor_copy(out=pj[:, 1092:1604], in_=o_.rearrange("p h v -> p (h v)")), [t_og], [t_proj])
        P.barrier()
        k.top = S_TOP
        if STOP == -1:
            return
        sample_dsa(proj, t_proj, sso, t_sso)
        sample_tail(proj, t_proj, x_keep, t_xk)

    def sample_dsa(proj, t_proj, sso, t_sso):
        NSB = NS
        pj = proj[0:NSB]
        U32 = mybir.dt.uint32
        selp, t_selp = sbt("s_selp", [128, 8, 128])
        selo, t_selo = sbt("s_selo", [128, NSB, 128])
        P.dma("sp", selp[0:NSB], selpair_d.rearrange("q i p -> i q p"), writes=[t_selp])
        P.dma("sp", selo[0:NSB], selone_d, writes=[t_selo])
        gq_b, t_gqb = bcast_layout("s_gq_b", g_q, 64)
        iota_b, t_iota = bcast_layout("s_iota", iota64_d, 64)
        pt_i, t_pti = sbt("s_pt_i", [128, 64], I32)
        pt_f, t_ptf = sbt("s_pt_f", [128, 64])
        P.dma("sp", pt_i[0:NSB], ptab, writes=[t_pti])
        P.op("dve", lambda e: e.tensor_copy(out=pt_f[0:NSB], in_=pt_i[0:NSB]), [t_pti], [t_ptf])
        ss2, t_ss2 = sbt("s_ss2", [128, 32])
        scr2, t_scr2 = sbt("s_scr2", [128, 512])
        qn, t_qn = sbt("s_qn", [128, 512])
        qiw, t_qiw = sbt("s_qiw", [128, 260])
        sv = ss2[0:NSB]
        P.op("dve", lambda e: e.tensor_tensor(out=scr2[0:NSB], in0=pj[:, 0:512], in1=pj[:, 0:512], op=ALU.mult), [t_proj], [t_scr2])
        P.op("dve", lambda e: e.tensor_reduce(out=sv[:, 0:8], in_=scr2[0:NSB].rearrange("p (h d) -> p h d", h=8), axis=AX.X,
                                              op=ALU.add), [t_scr2], [t_ss2])
        P.op("pool", lambda e: e.tensor_scalar(out=sv[:, 8:16], in0=sv[:, 0:8], scalar1=1.0 / 64, scalar2=EPS, op0=ALU.mult,
                                               op1=ALU.add), [t_ss2], [t_ss2])
        P.op("pool", lambda e: e.tensor_tensor(out=sv[:, 8:16], in0=sv[:, 8:16], in1=neghalf[0:NSB, 0:8], op=ALU.pow),
             [t_ss2, t_nh], [t_ss2])
        P.op("pool", lambda e: e.tensor_scalar(out=sv[:, 8:16], in0=sv[:, 8:16], scalar1=0.125, scalar2=1.0, op0=ALU.mult,
                                               op1=ALU.mult), [t_ss2], [t_ss2])
        q3 = qn[0:NSB].rearrange("p (h d) -> p h d", h=8)
        P.op("dve", lambda e: e.tensor_tensor(out=q3, in0=pj[:, 0:512].rearrange("p (h d) -> p h d", h=8),
                                              in1=sv[:, 8:16].unsqueeze(2).to_broadcast([NSB, 8, 64]), op=ALU.mult),
             [t_proj, t_ss2], [t_qn])
        P.op("pool", lambda e: e.tensor_tensor(out=q3, in0=q3, in1=gq_b[0:NSB].unsqueeze(1).to_broadcast([NSB, 8, 64]),
                                               op=ALU.mult), [t_qn, t_gqb], [t_qn])
        P.op("act", lambda e: e.activation(out=qiw[0:NSB, 0:256], in_=pj[:, 768:1024], func=AF.Copy, scale=0.125), [t_proj], [t_qiw])
        P.op("act", lambda e: e.activation(out=qiw[0:NSB, 256:260], in_=pj[:, 1088:1092], func=AF.Copy, scale=0.5), [t_proj], [t_qiw])
        scores, t_scores = sbt("s_scores", [128, 8200])
        P.op("dve", lambda e: e.tensor_tensor(out=scr2[0:NSB, 0:256].rearrange("p (h d) -> p h d", h=4),
                                              in0=qiw[0:NSB, 0:256].rearrange("p (h d) -> p h d", h=4),
                                              in1=pj[:, 1024:1088].unsqueeze(1).to_broadcast([NSB, 4, 64]), op=ALU.mult),
             [t_qiw, t_proj], [t_scr2])
        P.op("dve", lambda e: e.tensor_reduce(out=sv[:, 16:20], in_=scr2[0:NSB, 0:256].rearrange("p (h d) -> p h d", h=4),
                                              axis=AX.X, op=ALU.add), [t_scr2], [t_ss2])
        P.op("dve", lambda e: e.tensor_scalar(out=sv[:, 16:20], in0=sv[:, 16:20], scalar1=0.0, scalar2=None, op0=ALU.max),
             [t_ss2], [t_ss2])
        P.op("dve", lambda e: e.tensor_tensor(out=sv[:, 16:20], in0=sv[:, 16:20], in1=qiw[0:NSB, 256:260], op=ALU.mult),
             [t_ss2, t_qiw], [t_ss2])
        P.op("dve", lambda e: e.tensor_reduce(out=scores[0:NSB, 8192:8193], in_=sv[:, 16:20], axis=AX.X, op=ALU.add),
             [t_ss2], [t_scores])
        osT, t_osT = sbt("s_osT", [128, NSB, 8], BF16)
        K_TOP = k.top
        k.K_TOP = K_TOP
        kid = [sbt("s_kid%d" % i, [128, 8192]) for i in range(2)]
        prod, t_prod = sbt("s_prod", [128, 8192])
        ptc = [sbt("s_ptc%d" % i, [128, 1], I32) for i in range(2)]
        qrep, t_qrep = sbt("s_qrep", [128, 260])
        zz, t_zz = sbt("s_zz", [128, 128])
        sc1, t_sc1 = sbt("s_sc1", [128, 128])
        sc2 = [sbt("s_sc2_%d" % i, [128, 128]) for i in range(2)]
        kidx_pages = cache_kidx_d

        def pair(q):
            kd, tkd = kid[q % 2]
            pc, tpc = ptc[q % 2]
            P.dma("sp", pc, ptab[2 * q:2 * q + 2, :].rearrange("s (j o) -> (s j) o", o=1), writes=[tpc])
            P.dma("pool", kd, kidx_pages, reads=[tpc], writes=[tkd],
                  indirect=bass.IndirectOffsetOnAxis(ap=pc, axis=0))
            P.op("pe", lambda e: e.matmul(ps[1][:, 0:260], lhsT=selp[0:NSB, q, :], rhs=qiw[0:NSB, :], start=True, stop=True),
                 [t_selp, t_qiw], [t_ps[1]])
            P.op("act", lambda e: e.activation(out=qrep, in_=ps[1][:, 0:260], func=AF.Copy), [t_ps[1]], [t_qrep])
            so_, tso = sc2[q % 2]
            for h in range(4):
                P.op("pool", lambda e, h=h: e.tensor_tensor(
                    out=prod.rearrange("p (o d) -> p o d", d=64), in0=kd.rearrange("p (o d) -> p o d", d=64),
                    in1=qrep[:, h * 64:(h + 1) * 64].unsqueeze(1).to_broadcast([128, 128, 64]), op=ALU.mult),
                    [tkd, t_qrep], [t_prod])
                P.op("dve", lambda e: e.tensor_reduce(out=zz, in_=prod.rearrange("p (o d) -> p o d", d=64), axis=AX.X,
                                                      op=ALU.add), [t_prod], [t_zz])
                if h == 0:
                    P.op("dve", lambda e: e.tensor_scalar(out=so_, in0=zz, scalar1=0.0, scalar2=qrep[:, 256:257],
                                                          op0=ALU.max, op1=ALU.mult), [t_zz, t_qrep], [tso])
                else:
                    P.op("dve", lambda e, h=h: e.tensor_scalar(out=sc1, in0=zz, scalar1=0.0, scalar2=qrep[:, 256 + h:257 + h],
                                                               op0=ALU.max, op1=ALU.mult), [t_zz, t_qrep], [t_sc1])
                    P.op("dve", lambda e: e.tensor_tensor(out=so_, in0=so_, in1=sc1, op=ALU.add), [tso, t_sc1], [tso])
            for s2 in range(2):
                r = 2 * q + s2
                P.dma("sp", scores[r:r + 1, 0:8192].rearrange("p (j o) -> p j o", o=128), so_[64 * s2:64 * s2 + 64, :],
                      reads=[tso], writes=[t_scores])

        for q in range(NSB // 2):
            pair(q)
        P.barrier()
        k.top = K_TOP
        mx, t_mx = sbt("s_mx", [128, 256])
        ix, t_ix = sbt("s_ix", [128, 256], U32)
        TK_TOP = k.top
        W, t_W = sbt("s_W", [128, 8200])
        Wv = W[0:NSB, 0:8193]
        P.op("pool", lambda e: e.tensor_copy(out=Wv, in_=scores[0:NSB, 0:8193]), [t_scores], [t_W])
        for r in range(32):
            P.op("dve", lambda e, r=r: e.max(out=mx[0:NSB, 8 * r:8 * r + 8], in_=Wv), [t_W], [t_mx])
            P.op("dve", lambda e, r=r: e.max_index(out=ix[0:NSB, 8 * r:8 * r + 8], in_max=mx[0:NSB, 8 * r:8 * r + 8],
                                                   in_values=Wv), [t_W, t_mx], [t_ix])
            P.op("dve", lambda e, r=r: e.match_replace(out=Wv, in_to_replace=mx[0:NSB, 8 * r:8 * r + 8], in_values=Wv,
                                                       imm_value=-1e30), [t_W, t_mx], [t_W])
        P.barrier()
        k.top = TK_TOP
        ixf, t_ixf = sbt("s_ixf", [128, 256])
        pgu, t_pgu = sbt("s_pgu", [128, 256], U32)
        pgf, t_pgf = sbt("s_pgf", [128, 256])
        offf, t_offf = sbt("s_offf", [128, 256])
        eq, t_eq = sbt("s_eq", [128, 256, 64])
        phys, t_phys = sbt("s_phys", [128, 256])
        isf, t_isf = sbt("s_isf", [128, 256])
        n_ = lambda a: a[0:NSB]
        P.op("dve", lambda e: e.tensor_copy(out=n_(ixf), in_=n_(ix)), [t_ix], [t_ixf])
        P.op("dve", lambda e: e.tensor_scalar(out=n_(pgu), in0=n_(ix), scalar1=7, scalar2=None, op0=ALU.logical_shift_right),
             [t_ix], [t_pgu])
        P.op("dve", lambda e: e.tensor_copy(out=n_(pgf), in_=n_(pgu)), [t_pgu], [t_pgf])
        P.op("dve", lambda e: e.scalar_tensor_tensor(out=n_(offf), in0=n_(pgf), scalar=-128.0, in1=n_(ixf), op0=ALU.mult,
                                                     op1=ALU.add), [t_pgf, t_ixf], [t_offf])
        P.op("dve", lambda e: e.tensor_tensor(out=n_(eq), in0=n_(pgf).unsqueeze(2).to_broadcast([NSB, 256, 64]),
                                              in1=n_(iota_b).unsqueeze(1).to_broadcast([NSB, 256, 64]), op=ALU.is_equal),
             [t_pgf, t_iota], [t_eq])
        P.op("dve", lambda e: e.tensor_tensor(out=n_(eq), in0=n_(eq), in1=n_(pt_f).unsqueeze(1).to_broadcast([NSB, 256, 64]),
                                              op=ALU.mult), [t_eq, t_ptf], [t_eq])
        P.op("dve", lambda e: e.tensor_reduce(out=n_(phys), in_=n_(eq), axis=AX.X, op=ALU.add), [t_eq], [t_phys])
        P.op("dve", lambda e: e.scalar_tensor_tensor(out=n_(phys), in0=n_(phys), scalar=128.0, in1=n_(offf), op0=ALU.mult,
                                                     op1=ALU.add), [t_phys, t_offf], [t_phys])
        P.op("dve", lambda e: e.tensor_scalar(out=n_(isf), in0=n_(ixf), scalar1=8191.5, scalar2=None, op0=ALU.is_ge),
             [t_ixf], [t_isf])
        physT, t_physT = sbt("s_physT", [128, 2, NSB], I32)
        isT, t_isT = sbt("s_isT", [128, 2, NSB])
        for b in range(2):
            P.op("pe", lambda e, b=b: e.transpose(out=ps[1][:, b * NSB:(b + 1) * NSB], in_=phys[0:NSB, b * 128:(b + 1) * 128],
                                                  identity=identf[0:NSB, 0:NSB]), [t_phys, t_identf], [t_ps[1]])
            P.op("pe", lambda e, b=b: e.transpose(out=ps[2][:, b * NSB:(b + 1) * NSB], in_=isf[0:NSB, b * 128:(b + 1) * 128],
                                                  identity=identf[0:NSB, 0:NSB]), [t_isf, t_identf], [t_ps[2]])
        P.op("dve", lambda e: e.tensor_copy(out=physT.rearrange("p b i -> p (b i)"), in_=ps[1][:, 0:2 * NSB]), [t_ps[1]], [t_physT])
        P.op("act", lambda e: e.activation(out=isT.rearrange("p b i -> p (b i)"), in_=ps[2][:, 0:2 * NSB], func=AF.Copy),
             [t_ps[2]], [t_isT])
        Kg = [sbt("s_Kg%d" % i, [128, 2, 128]) for i in range(2)]
        Vg = [sbt("s_Vg%d" % i, [128, 2, 128]) for i in range(2)]
        kvrep, t_kvrep = sbt("s_kvrep", [128, 256])
        dif, t_dif = sbt("s_dif", [128, 128])
        prd, t_prd = sbt("s_prd", [128, 512])
        lg, t_lg = sbt("s_lg", [128, 2, 8])
        rcp, t_rcp = sbt("s_rcp", [128, NSB * 8])

        def att(i):
            kg, tkg = Kg[i % 2]
            vg, tvg = Vg[i % 2]
            for b in range(2):
                P.dma("pool", kg[:, b, :], cache_k_d, reads=[t_physT], writes=[tkg],
                      indirect=bass.IndirectOffsetOnAxis(ap=physT[:, b, i:i + 1], axis=0))
                P.dma("pool", vg[:, b, :], cache_v_d, reads=[t_physT], writes=[tvg],
                      indirect=bass.IndirectOffsetOnAxis(ap=physT[:, b, i:i + 1], axis=0))
            P.op("pe", lambda e: e.matmul(ps[3], lhsT=selo[0:NSB, i, :], rhs=qn[0:NSB, :], start=True, stop=True),
                 [t_selo, t_qn], [t_ps[3]])
            P.op("pe", lambda e: e.matmul(ps[4][:, 0:256], lhsT=selo[0:NSB, i, :], rhs=sso[0:NSB, 0:256], start=True, stop=True),
                 [t_selo, t_sso], [t_ps[4]])
            P.op("act", lambda e: e.activation(out=kvrep, in_=ps[4][:, 0:256], func=AF.Copy), [t_ps[4]], [t_kvrep])
            for b in range(2):
                for (t_, tt_, c0) in ((kg, tkg, 0), (vg, tvg, 128)):
                    P.op("dve", lambda e, t_=t_, c0=c0, b=b: e.tensor_tensor(out=dif, in0=kvrep[:, c0:c0 + 128], in1=t_[:, b, :],
                                                                             op=ALU.subtract), [t_kvrep, tt_], [t_dif])
                    P.op("dve", lambda e, t_=t_, b=b: e.scalar_tensor_tensor(out=t_[:, b, :], in0=dif, scalar=isT[:, b, i:i + 1],
                                                                             in1=t_[:, b, :], op0=ALU.mult, op1=ALU.add),
                         [t_dif, t_isT, tt_], [tt_])
                P.op("dve", lambda e, b=b: e.tensor_tensor(
                    out=prd.rearrange("p (g r d) -> p g r d", g=2, r=4),
                    in0=ps[3].rearrange("p (g r d) -> p g r d", g=2, r=4),
                    in1=kg[:, b, :].rearrange("p (g d) -> p g d", g=2).unsqueeze(2).to_broadcast([128, 2, 4, 64]),
                    op=ALU.mult), [t_ps[3], tkg], [t_prd])
                P.op("dve", lambda e, b=b: e.tensor_reduce(out=lg[:, b, :], in_=prd.rearrange("p (h d) -> p h d", h=8),
                                                           axis=AX.X, op=ALU.add), [t_prd], [t_lg])
            P.op("act", lambda e: e.activation(out=lg, in_=lg, func=AF.Exp), [t_lg], [t_lg])
            for g in range(2):
                for b in range(2):
                    P.op("pe", lambda e, g=g, b=b: e.matmul(ps[5][0:64, i * 8 + g * 4:i * 8 + g * 4 + 4],
                                                            lhsT=vg[:, b, g * 64:(g + 1) * 64], rhs=lg[:, b, g * 4:(g + 1) * 4],
                                                            start=(b == 0), stop=(b == 1)), [tvg, t_lg], [t_ps[5]])
                for b in range(2):
                    P.op("pe", lambda e, g=g, b=b: e.matmul(ps[6][0:64, i * 8 + g * 4:i * 8 + g * 4 + 4],
                                                            lhsT=onesf[:, 0:64], rhs=lg[:, b, g * 4:(g + 1) * 4],
                                                            start=(b == 0), stop=(b == 1)), [t_onesf, t_lg], [t_ps[6]])

        for i in range(NSB):
            att(i)
        P.op("dve", lambda e: e.reciprocal(out=rcp[0:64], in_=ps[6][0:64, 0:NSB * 8]), [t_ps[6]], [t_rcp])
        P.op("dve", lambda e: e.tensor_tensor(out=osT[0:64].rearrange("p i h -> p (i h)"), in0=ps[5][0:64, 0:NSB * 8],
                                              in1=rcp[0:64], op=ALU.mult), [t_ps[5], t_rcp], [t_osT])
        k.osT = (osT, t_osT)
        k.selo = (selo, t_selo)
        k.S2_TOP = k.top
        P.barrier()

    def sample_tail(proj, t_proj, x_keep, t_xk):
        NSB = NS
        pj = proj[0:NSB]
        osT, t_osT = k.osT
        selo, t_selo = k.selo
        k.top = k.K_TOP
        alloc_stage(1024)
        Wo_s, t_Wos = sbt("t_Wo_s", [128, 8, 1024], BF16)
        Wo_g, t_Wog = sbt("t_Wo_g", [128, 4, 1024], BF16)
        Wmq, t_Wmq = sbt("t_Wmq", [128, 8, 256], BF16)
        Wmo, t_Wmo = sbt("t_Wmo", [128, 4, 1024], BF16)
        for h in range(8):
            i = k.wl % 2
            k.wl += 1
            st = stage[i][0:64, 0:1024]
            P.dma("sp", st, w_out[64 * h:64 * h + 64, :], writes=[t_stage[i]])
            P.op("act", lambda e, st=st, h=h: e.activation(out=Wo_s[0:64, h, :], in_=st, func=AF.Copy), [t_stage[i]], [t_Wos])
        for h in range(4):
            load_rows(Wo_g[:, h, :], t_Wog, w_out[512 + 128 * h:512 + 128 * h + 128, :], 1024, None, None)
        load_weight(Wmq, t_Wmq, w_mq, 0, 256, gX, t_gX)
        for h in range(4):
            i = k.wl % 2
            k.wl += 1
            st = stage[i][0:64, 0:1024]
            P.dma("sp", st, w_mo[64 * h:64 * h + 64, :], writes=[t_stage[i]])
            P.op("act", lambda e, st=st, h=h: e.activation(out=Wmo[0:64, h, :], in_=st, func=AF.Copy), [t_stage[i]], [t_Wmo])
        gmq_b, t_gmqb = bcast_layout("t_gmq_b", g_mq, 64)
        ogT, t_ogT = sbt("t_ogT", [128, 4, NSB], BF16)
        for h in range(4):
            P.op("pe", lambda e, h=h: e.transpose(out=ps[1][:, h * NSB:(h + 1) * NSB], in_=pj[:, 1092 + h * 128:1092 + (h + 1) * 128],
                                                  identity=identf[0:NSB, 0:NSB]), [t_proj, t_identf], [t_ps[1]])
        P.op("act", lambda e: e.activation(out=ogT.rearrange("p h i -> p (h i)"), in_=ps[1][:, 0:4 * NSB], func=AF.Copy),
             [t_ps[1]], [t_ogT])
        x1, t_x1 = sbt("t_x1", [128, D])
        P.op("pool", lambda e: e.memset(x1, 0.0), [], [t_x1])
        for c in range(2):
            for h in range(8):
                P.op("pe", lambda e, h=h, c=c: e.matmul(ps[2 + c][0:NSB, :], lhsT=osT[0:64, :, h], rhs=Wo_s[0:64, h, c * 512:(c + 1) * 512],
                                                        start=(h == 0), stop=False), [t_osT, t_Wos], [t_ps[2 + c]])
            for h in range(4):
                P.op("pe", lambda e, h=h, c=c: e.matmul(ps[2 + c][0:NSB, :], lhsT=ogT[:, h, :], rhs=Wo_g[:, h, c * 512:(c + 1) * 512],
                                                        start=False, stop=(h == 3)), [t_ogT, t_Wog], [t_ps[2 + c]])
            P.op("dve", lambda e, c=c: e.tensor_tensor(out=x1[0:NSB, c * 512:(c + 1) * 512], in0=ps[2 + c][0:NSB, :],
                                                       in1=x_keep[0:NSB, c * 512:(c + 1) * 512], op=ALU.add),
                 [t_ps[2 + c], t_xk], [t_x1])
        hT2, t_hT2 = sbt("t_hT2", [128, 8, 128], BF16)
        norm_T(x1, t_x1, 0, hT2, t_hT2, 0, 0)
        for kc in range(8):
            P.op("pe", lambda e, kc=kc: e.matmul(ps[4][0:NSB, 0:256], lhsT=hT2[:, kc, 0:NSB], rhs=Wmq[:, kc, :],
                                                 start=(kc == 0), stop=(kc == 7)), [t_hT2, t_Wmq], [t_ps[4]])
        qm, t_qm = sbt("t_qm", [128, 256])
        sq2, t_sq2 = sbt("t_sq2", [128, 256])
        st2, t_st2 = sbt("t_st2", [128, 16])
        P.op("act", lambda e: e.activation(out=sq2[0:NSB], in_=ps[4][0:NSB, 0:256], func=AF.Square), [t_ps[4]], [t_sq2])
        P.op("dve", lambda e: e.tensor_reduce(out=st2[0:NSB, 0:4], in_=sq2[0:NSB].rearrange("p (h d) -> p h d", h=4), axis=AX.X,
                                              op=ALU.add), [t_sq2], [t_st2])
        P.op("pool", lambda e: e.tensor_scalar(out=st2[0:NSB, 4:8], in0=st2[0:NSB, 0:4], scalar1=1.0 / 64, scalar2=EPS,
                                               op0=ALU.mult, op1=ALU.add), [t_st2], [t_st2])
        P.op("pool", lambda e: e.tensor_tensor(out=st2[0:NSB, 4:8], in0=st2[0:NSB, 4:8], in1=neghalf[0:NSB, 0:4], op=ALU.pow),
             [t_st2, t_nh], [t_st2])
        P.op("pool", lambda e: e.tensor_scalar(out=st2[0:NSB, 4:8], in0=st2[0:NSB, 4:8], scalar1=0.125, scalar2=1.0,
                                               op0=ALU.mult, op1=ALU.mult), [t_st2], [t_st2])
        qm3 = qm[0:NSB].rearrange("p (h d) -> p h d", h=4)
        P.op("dve", lambda e: e.tensor_tensor(out=qm3, in0=ps[4][0:NSB, 0:256].rearrange("p (h d) -> p h d", h=4),
                                              in1=st2[0:NSB, 4:8].unsqueeze(2).to_broadcast([NSB, 4, 64]), op=ALU.mult),
             [t_ps[4], t_st2], [t_qm])
        P.op("pool", lambda e: e.tensor_tensor(out=qm3, in0=qm3, in1=gmq_b[0:NSB].unsqueeze(1).to_broadcast([NSB, 4, 64]),
                                               op=ALU.mult), [t_qm, t_gmqb], [t_qm])
        mkt = [sbt("t_mk%d" % i, [128, 2, 256]) for i in range(2)]
        mvt = [sbt("t_mv%d" % i, [128, 2, 256]) for i in range(2)]
        prm, t_prm = sbt("t_prm", [128, 256])
        lgm, t_lgm = sbt("t_lgm", [128, 2, 4])
        omT, t_omT = sbt("t_omT", [128, NSB, 4], BF16)
        rcm, t_rcm = sbt("t_rcm", [128, NSB * 4])

        def xatt(i):
            mk_, tmk = mkt[i % 2]
            mv_, tmv = mvt[i % 2]
            P.dma("sp", mk_, cmk_d[i * 256:(i + 1) * 256, :].rearrange("(t p) c -> p t c", p=128), writes=[tmk])
            P.dma("sp", mv_, cmv_d[i * 256:(i + 1) * 256, :].rearrange("(t p) c -> p t c", p=128), writes=[tmv])
            P.op("pe", lambda e: e.matmul(ps[5][:, 0:256], lhsT=selo[0:NSB, i, :], rhs=qm[0:NSB, :], start=True, stop=True),
                 [t_selo, t_qm], [t_ps[5]])
            for mt in range(2):
                P.op("dve", lambda e, mt=mt: e.tensor_tensor(out=prm, in0=ps[5][:, 0:256], in1=mk_[:, mt, :], op=ALU.mult),
                     [t_ps[5], tmk], [t_prm])
                P.op("dve", lambda e, mt=mt: e.tensor_reduce(out=lgm[:, mt, :], in_=prm.rearrange("p (h d) -> p h d", h=4),
                                                             axis=AX.X, op=ALU.add), [t_prm], [t_lgm])
            P.op("act", lambda e: e.activation(out=lgm, in_=lgm, func=AF.Exp), [t_lgm], [t_lgm])
            for h in range(4):
                for mt in range(2):
                    P.op("pe", lambda e, h=h, mt=mt: e.matmul(ps[6][0:64, i * 4 + h:i * 4 + h + 1], lhsT=mv_[:, mt, h * 64:(h + 1) * 64],
                                                              rhs=lgm[:, mt, h:h + 1], start=(mt == 0), stop=(mt == 1)),
                         [tmv, t_lgm], [t_ps[6]])
                for mt in range(2):
                    P.op("pe", lambda e, h=h, mt=mt: e.matmul(ps[7][0:64, i * 4 + h:i * 4 + h + 1], lhsT=onesf[:, 0:64],
                                                              rhs=lgm[:, mt, h:h + 1], start=(mt == 0), stop=(mt == 1)),
                         [t_onesf, t_lgm], [t_ps[7]])

        for i in range(NSB):
            xatt(i)
        P.op("dve", lambda e: e.reciprocal(out=rcm[0:64], in_=ps[7][0:64, 0:NSB * 4]), [t_ps[7]], [t_rcm])
        P.op("dve", lambda e: e.tensor_tensor(out=omT[0:64].rearrange("p i h -> p (i h)"), in0=ps[6][0:64, 0:NSB * 4],
                                              in1=rcm[0:64], op=ALU.mult), [t_ps[6], t_rcm], [t_omT])
        for c in range(2):
            for h in range(4):
                P.op("pe", lambda e, h=h, c=c: e.matmul(ps[2 + c][0:NSB, :], lhsT=omT[0:64, :, h], rhs=Wmo[0:64, h, c * 512:(c + 1) * 512],
                                                        start=(h == 0), stop=(h == 3)), [t_omT, t_Wmo], [t_ps[2 + c]])
            P.op("dve", lambda e, c=c: e.tensor_tensor(out=x1[0:NSB, c * 512:(c + 1) * 512], in0=ps[2 + c][0:NSB, :],
                                                       in1=x1[0:NSB, c * 512:(c + 1) * 512], op=ALU.add),
                 [t_ps[2 + c], t_x1], [t_x1])
        P.op("pool", lambda e: e.tensor_copy(out=x_keep[0:NSB, :], in_=x1[0:NSB, :]), [t_x1], [t_xk])
        hT3, t_hT3 = sbt("t_hT3", [128, 8, 128], BF16)
        norm_T(x1, t_x1, 1, hT3, t_hT3, 0, 0)
        P.barrier()
        k.top = PERSIST_TOP
        xk2, t_xk2 = sbt("t_xk2", [128, D])
        hT4, t_hT4 = sbt("t_hT4", [128, 8, NSB], BF16)
        P.op("pool", lambda e: e.tensor_copy(out=xk2[0:NSB, :], in_=x_keep[0:NSB, :]), [t_xk], [t_xk2])
        P.op("pool", lambda e: e.tensor_copy(out=hT4, in_=hT3[:, :, 0:NSB]), [t_hT3], [t_hT4])
        P.barrier()
        alloc_stage(2816)
        Wg, t_Wg = sbt("t_Wg", [128, 8, 2816], BF16)
        Wu, t_Wu = sbt("t_Wu", [128, 8, 2816], BF16)
        Wd, t_Wd = sbt("t_Wd", [128, 22, 1024], BF16)
        load_weight(Wg, t_Wg, w_gate, 0, 2816, gF, t_gF)
        load_weight(Wu, t_Wu, w_up, 0, 2816, gF, t_gF)
        for f in range(22):
            load_rows(Wd[:, f, :], t_Wd, w_down[128 * f:128 * f + 128, :], 1024, None, None)
        hf, t_hf = sbt("t_hf", [128, 2816])
        sgs, t_sgs = sbt("t_sgs", [128, 512])
        hfT, t_hfT = sbt("t_hfT", [128, 22, NSB], BF16)
        for c in range(6):
            c0 = c * 512
            n = min(512, 2816 - c0)
            for kc in range(8):
                P.op("pe", lambda e, kc=kc, c0=c0, n=n: e.matmul(ps[1][0:NSB, 0:n], lhsT=hT4[:, kc, :], rhs=Wg[:, kc, c0:c0 + n],
                                                                 start=(kc == 0), stop=(kc == 7)), [t_hT4, t_Wg], [t_ps[1]])
            for kc in range(8):
                P.op("pe", lambda e, kc=kc, c0=c0, n=n: e.matmul(ps[2][0:NSB, 0:n], lhsT=hT4[:, kc, :], rhs=Wu[:, kc, c0:c0 + n],
                                                                 start=(kc == 0), stop=(kc == 7)), [t_hT4, t_Wu], [t_ps[2]])
            P.op("act", lambda e, n=n: e.activation(out=sgs[0:NSB, 0:n], in_=ps[1][0:NSB, 0:n], func=AF.Silu), [t_ps[1]], [t_sgs])
            P.op("dve", lambda e, c0=c0, n=n: e.tensor_tensor(out=hf[0:NSB, c0:c0 + n], in0=sgs[0:NSB, 0:n], in1=ps[2][0:NSB, 0:n],
                                                              op=ALU.mult), [t_sgs, t_ps[2]], [t_hf])
        for f in range(22):
            b = 3 + (f % 2)
            P.op("pe", lambda e, f=f, b=b: e.transpose(out=ps[b][:, 0:NSB], in_=hf[0:NSB, f * 128:(f + 1) * 128],
                                                       identity=identf[0:NSB, 0:NSB]), [t_hf, t_identf], [t_ps[b]])
            P.op("act", lambda e, f=f, b=b: e.activation(out=hfT[:, f, :], in_=ps[b][:, 0:NSB], func=AF.Copy), [t_ps[b]], [t_hfT])
        for c in range(2):
            for f in range(22):
                P.op("pe", lambda e, f=f, c=c: e.matmul(ps[5 + c][0:NSB, :], lhsT=hfT[:, f, :], rhs=Wd[:, f, c * 512:(c + 1) * 512],
                                                        start=(f == 0), stop=(f == 21)), [t_hfT, t_Wd], [t_ps[5 + c]])
            P.op("dve", lambda e, c=c: e.tensor_tensor(out=xk2[0:NSB, c * 512:(c + 1) * 512], in0=ps[5 + c][0:NSB, :],
                                                       in1=xk2[0:NSB, c * 512:(c + 1) * 512], op=ALU.add),
                 [t_ps[5 + c], t_xk2], [t_xk2])
        P.dma("sp", y_s[:, :], xk2[0:NSB, :], reads=[t_xk2])

    if STOP >= 0:
        for sq in range(NSEQ):
            prompt_seq(sq)
    if STOP < 0 or STOP >= 99:
        sample_group()

    P.finish()
    P.emit()
    return nc


_CACHE = {}


def _get_nc(nseq, stop, npool=10240):
    key = (nseq, stop, npool)
    if key not in _CACHE:
        _CACHE[key] = build(nseq, STOP=stop, NPOOL=npool)
    return _CACHE[key]


def kernel(x_prompt, x_sample, mem_prompt, cache_k, cache_v, cache_kidx, page_table,
           state_conv, state_ssm, cache_mem_k, cache_mem_v,
           attn_norm_g, w_in, q_norm_g, k_norm_g, conv_w, a_log, dt_bias, gdn_norm_g, w_out,
           xattn_norm_g, mem_norm_g, w_mq, w_mk, w_mv, mq_norm_g, mk_norm_g, w_mo,
           ffn_norm_g, w_gate, w_up, w_down, _ncores=NCORES, _stop=99):
    B = x_prompt.shape[0]
    nseq = B // _ncores
    NS = x_sample.shape[0] // _ncores
    nc = _get_nc(nseq, _stop, cache_k.shape[1])
    f = lambda a: np.ascontiguousarray(np.asarray(a, dtype=np.float32))
    ii = np.arange(128)
    consts = {
        "ident": np.eye(128, dtype=np.float32),
        "trile": (ii[:, None] <= ii[None, :]).astype(np.float32),
        "sgt": (ii[:, None] > ii[None, :]).astype(np.float32),
        "pow2": (2.0 ** -np.arange(32)).astype(np.float32),
        "eye16": np.eye(16, dtype=np.float32).reshape(256),
        "selpair": np.stack([(np.arange(16)[:, None] == (2 * q_ + np.arange(128)[None, :] // 64)).astype(np.float32)
                             for q_ in range(8)]),
        "selone": np.stack([np.repeat((np.arange(16) == i_)[:, None], 128, axis=1).astype(np.float32)
                            for i_ in range(16)], axis=1),
        "iota64": np.arange(64, dtype=np.float32),
    }
    shared = {
        "attn_norm_g": f(attn_norm_g[0]), "w_in": f(w_in[0]),
        "q_norm_g": f(q_norm_g[0]), "k_norm_g": f(k_norm_g[0]),
        "conv_w": f(conv_w[0]), "a_log": f(a_log[0]), "dt_bias": f(dt_bias[0]),
        "gdn_norm_g": f(gdn_norm_g[0]), "w_out": f(w_out[0]), "xattn_norm_g": f(xattn_norm_g[0]),
        "mem_norm_g": f(mem_norm_g[0]), "w_mq": f(w_mq[0]), "w_mk": f(w_mk[0]), "w_mv": f(w_mv[0]),
        "mq_norm_g": f(mq_norm_g[0]), "mk_norm_g": f(mk_norm_g[0]), "w_mo": f(w_mo[0]),
        "ffn_norm_g": f(ffn_norm_g[0]), "w_gate": f(w_gate[0]), "w_up": f(w_up[0]), "w_down": f(w_down[0]),
    }
    npool = cache_k.shape[1]
    ck_k = f(cache_k[0]).reshape(npool * 128, 128)
    ck_v = f(cache_v[0]).reshape(npool * 128, 128)
    ck_idx = f(cache_kidx[0]).reshape(npool, 8192)
    in_maps = []
    for c in range(_ncores):
        m = {
            "xp": f(x_prompt[c * nseq:(c + 1) * nseq]).reshape(nseq * SEQ, D),
            "memp": f(mem_prompt[c * nseq:(c + 1) * nseq]).reshape(nseq * MEM, D),
            "xs": f(x_sample[c * NS:(c + 1) * NS]).reshape(NS, D),
            "st_conv": f(state_conv[0, c * NS:(c + 1) * NS]).reshape(NS * 3, 1536),
            "ssm_in": f(state_ssm[0, c * NS:(c + 1) * NS]).reshape(NS * 512, 128),
            "ptab": np.ascontiguousarray(np.asarray(page_table[c * NS:(c + 1) * NS], dtype=np.int32)),
            "cmk": f(cache_mem_k[0, c * NS:(c + 1) * NS]).reshape(NS * 256, 256),
            "cmv": f(cache_mem_v[0, c * NS:(c + 1) * NS]).reshape(NS * 256, 256),
            "cache_kidx": ck_idx, "cache_k": ck_k, "cache_v": ck_v,
        }
        m.update(consts)
        m.update(shared)
        in_maps.append(m)
    res = run_bass_kernel_spmd(nc, in_maps, core_ids=list(range(_ncores))).results
    cat = lambda name: np.concatenate([r[name] for r in res], axis=0)
    SB = x_sample.shape[0]
    outs = (
        cat("y_p").reshape(B, SEQ, D),
        cat("y_s").reshape(SB, 1, D),
        cat("k_p").reshape(1, B, SEQ, 2, 64),
        cat("v_p").reshape(1, B, SEQ, 2, 64),
        cat("kidx_p").reshape(1, B, SEQ, 64),
        cat("conv_p").reshape(1, B, 3, 1536),
        cat("ssm_p").reshape(1, B, 4, 128, 128),
        cat("memk_p").reshape(1, B, MEM, 4, 64),
        cat("memv_p").reshape(1, B, MEM, 4, 64),
        cat("k_s").reshape(1, SB, 1, 2, 64),
        cat("v_s").reshape(1, SB, 1, 2, 64),
        cat("kidx_s").reshape(1, SB, 1, 64),
        cat("conv_s").reshape(1, SB, 3, 1536),
        cat("ssm_s").reshape(1, SB, 4, 128, 128),
    )
    return outs
```

```python
import os
import numpy as np
import concourse.bass as bass
import concourse.mybir as mybir
from concourse.bass_utils import run_bass_kernel_spmd

F32 = mybir.dt.float32
BF16 = mybir.dt.bfloat16
I32 = mybir.dt.int32
AF = mybir.ActivationFunctionType
ALU = mybir.AluOpType
AX = mybir.AxisListType

NCORES = 8
D = 1024
SEQ = 2048
NT = SEQ // 128
MEM = 256
INW = 3148
EPS = 1e-6
NDS = 32


class Tok:
    __slots__ = ("w", "r")

    def __init__(self):
        self.w = None
        self.r = {}


class Prog:
    def __init__(self, nc):
        self.nc = nc
        self.names = ["pe", "act", "dve", "pool", "sp"]
        self.sems = []
        self.esem = {}
        for k in self.names:
            self.esem[k] = len(self.sems)
            self.sems.append(nc.alloc_semaphore("es_" + k))
        self.dsem = []
        for i in range(NDS):
            self.dsem.append(len(self.sems))
            self.sems.append(nc.alloc_semaphore("ds_%d" % i))
        self.dval = [0] * NDS
        self.dnext = 0
        self.dnext_sw = 0
        self.cnt = {k: 0 for k in self.names}
        self.seen = {k: {} for k in self.names}
        self.th = {k: [] for k in self.names}

    def _deps(self, e, reads, writes, extra=()):
        d = {}

        def add(s, v):
            if d.get(s, 0) < v:
                d[s] = v

        for t in reads:
            if t.w is not None:
                add(*t.w)
        for t in writes:
            if t.w is not None:
                add(*t.w)
            for s, v in t.r.items():
                add(s, v)
        for s, v in extra:
            add(s, v)
        out = []
        for s, v in d.items():
            if e == "pe" and s == self.esem["pe"]:
                continue
            if self.seen[e].get(s, 0) >= v:
                continue
            self.seen[e][s] = v
            out.append((s, v))
        return out

    def op(self, e, fn, reads=(), writes=()):
        waits = self._deps(e, reads, writes)
        self.cnt[e] += 1
        n = self.cnt[e]
        s = self.esem[e]
        self.th[e].append((waits, fn, s, 1))
        for t in reads:
            if t.r.get(s, 0) < n:
                t.r[s] = n
        for t in writes:
            t.w = (s, n)
            t.r = {}

    def dma(self, q, out, in_, reads=(), writes=(), **kw):
        if q == "pool":
            i = NDS - 8 + self.dnext_sw
            self.dnext_sw = (self.dnext_sw + 1) % 8
        else:
            i = self.dnext
            self.dnext = (self.dnext + 1) % (NDS - 8)
        s = self.dsem[i]
        extra = [(s, self.dval[i])] if self.dval[i] else []
        waits = self._deps(q, reads, writes, extra)
        self.dval[i] += 16
        v = self.dval[i]
        if "indirect" in kw:
            ioff = kw.pop("indirect")
            self.th[q].append((waits, lambda eng: eng.indirect_dma_start(out=out, out_offset=None, in_=in_,
                                                                         in_offset=ioff), s, 16))
        else:
            self.th[q].append((waits, lambda eng: eng.dma_start(out=out, in_=in_, **kw), s, 16))
        for t in reads:
            t.r[s] = v
        for t in writes:
            t.w = (s, v)
            t.r = {}

    def barrier(self):
        tgt = []
        for i in range(NDS):
            if self.dval[i]:
                tgt.append((self.dsem[i], self.dval[i]))
        for k in self.names:
            if self.cnt[k]:
                tgt.append((self.esem[k], self.cnt[k]))
        for e in self.names:
            waits = []
            for s_, v in tgt:
                if s_ == self.esem[e] and e in ("pe", "sp"):
                    continue
                if self.seen[e].get(s_, 0) >= v:
                    continue
                self.seen[e][s_] = v
                waits.append((s_, v))
            self.th[e].append((waits, None, None, 0))

    def finish(self):
        waits = []
        for i in range(NDS):
            if self.dval[i]:
                waits.append((self.dsem[i], self.dval[i]))
        for k in self.names:
            if k != "sp" and self.cnt[k]:
                waits.append((self.esem[k], self.cnt[k]))
        self.th["sp"].append((waits, None, None, 0))

    def emit(self):
        nc = self.nc
        sems = self.sems

        def run(eng, lst):
            for waits, fn, s, inc in lst:
                for ws, wv in waits:
                    eng.wait_ge(sems[ws], wv)
                if fn is not None:
                    fn(eng).then_inc(sems[s], inc)

        with nc.Block() as block:
            @block.tensor
            def _(e):
                run(e, self.th["pe"])

            @block.scalar
            def _(e):
                run(e, self.th["act"])

            @block.vector
            def _(e):
                run(e, self.th["dve"])

            @block.gpsimd
            def _(e):
                run(e, self.th["pool"])

            @block.sync
            def _(e):
                run(e, self.th["sp"])


class K:
    pass


def build(NSEQ, NS=16, STOP=99, NPOOL=10240):
    GCUT = int(os.environ.get('GCUT', '99'))
    DCUT = int(os.environ.get('DCUT', '99'))
    PCUT = int(os.environ.get('PCUT', '99'))
    CCUT = int(os.environ.get('CCUT', '99'))
    nc = bass.Bass("TRN2", target_bir_lowering=False)
    P = Prog(nc)
    k = K()

    def din(name, shape, dt=F32):
        return nc.dram_tensor(name, list(shape), dt, kind="ExternalInput").ap()

    def dout(name, shape, dt=F32):
        return nc.dram_tensor(name, list(shape), dt, kind="ExternalOutput").ap()

    ARENA_W = 53000
    arena = nc.alloc_sbuf_tensor("arena", [128, ARENA_W], F32).ap()
    k.top = 0

    def sb(name, shape, dt=F32):
        n = 1
        for d_ in shape[1:]:
            n *= d_
        words = n if dt in (F32, I32, mybir.dt.uint32) else (n + 1) // 2
        words = (words + 7) // 8 * 8
        off = k.top
        k.top += words
        assert k.top <= ARENA_W, ("SBUF arena overflow", name, k.top)
        a = arena[:, off:off + words]
        if dt != F32:
            a = a.bitcast(dt)
        a = a[:, 0:n]
        if len(shape) > 2:
            names = " ".join("d%d" % i for i in range(len(shape) - 1))
            kw = {"d%d" % i: shape[i + 1] for i in range(len(shape) - 2)}
            a = a.rearrange("p (%s) -> p %s" % (names, names), **kw)
        if shape[0] != 128:
            a = a[0:shape[0]]
        return a

    def sbt(name, shape, dt=F32):
        return sb(name, shape, dt), Tok()

    xp = din("xp", [NSEQ * SEQ, D])
    memp = din("memp", [NSEQ * MEM, D])
    xs = din("xs", [NS, D])
    st_conv = din("st_conv", [NS * 3, 1536])
    ssm_in = din("ssm_in", [NS * 512, 128])
    eye16_d = din("eye16", [256])
    selpair_d = din("selpair", [8, NS, 128])
    selone_d = din("selone", [NS, NS, 128])
    iota64_d = din("iota64", [64])
    ptab = din("ptab", [NS, 64], I32)
    cache_kidx_d = din("cache_kidx", [NPOOL, 8192])
    cache_k_d = din("cache_k", [NPOOL * 128, 128])
    cache_v_d = din("cache_v", [NPOOL * 128, 128])
    cmk_d = din("cmk", [NS * 256, 256])
    cmv_d = din("cmv", [NS * 256, 256])
    ident_d = din("ident", [128, 128])
    trile_d = din("trile", [128, 128])
    sgt_d = din("sgt", [128, 128])
    pow2_d = din("pow2", [32])
    g_attn = din("attn_norm_g", [D])
    w_in = din("w_in", [D, INW])
    g_q = din("q_norm_g", [64])
    g_k = din("k_norm_g", [64])
    conv_w = din("conv_w", [4, 1536])
    a_log = din("a_log", [4])
    dt_bias = din("dt_bias", [4])
    g_gdn = din("gdn_norm_g", [128])
    w_out = din("w_out", [D, D])
    g_x = din("xattn_norm_g", [D])
    g_mem = din("mem_norm_g", [D])
    w_mq = din("w_mq", [D, 256])
    w_mk = din("w_mk", [D, 256])
    w_mv = din("w_mv", [D, 256])
    g_mq = din("mq_norm_g", [64])
    g_mk = din("mk_norm_g", [64])
    w_mo = din("w_mo", [256, D])
    g_f = din("ffn_norm_g", [D])
    w_gate = din("w_gate", [D, 2816])
    w_up = din("w_up", [D, 2816])
    w_down = din("w_down", [2816, D])

    y_p = dout("y_p", [NSEQ * SEQ, D])
    y_s = dout("y_s", [NS, D])
    k_p = dout("k_p", [NSEQ * SEQ, 128])
    v_p = dout("v_p", [NSEQ * SEQ, 128])
    kidx_p = dout("kidx_p", [NSEQ * SEQ, 64])
    conv_p = dout("conv_p", [NSEQ * 3, 1536])
    ssm_p = dout("ssm_p", [NSEQ * 512, 128])
    memk_p = dout("memk_p", [NSEQ * MEM, 256])
    memv_p = dout("memv_p", [NSEQ * MEM, 256])
    k_s = dout("k_s", [NS, 128])
    v_s = dout("v_s", [NS, 128])
    kidx_s = dout("kidx_s", [NS, 64])
    conv_s = dout("conv_s", [NS * 3, 1536])
    ssm_s = dout("ssm_s", [NS * 512, 128])

    identf, t_identf = sbt("identf", [128, 128])
    identb, t_identb = sbt("identb", [128, 128], BF16)
    trile, t_trile = sbt("trile", [128, 128])
    sgt, t_sgt = sbt("sgt", [128, 128])
    onesf, t_onesf = sbt("onesf", [128, 128])
    onesb, t_onesb = sbt("onesb", [128, 128], BF16)
    tribias, t_tribias = sbt("tribias", [128, 128])
    lowinc, t_lowinc = sbt("lowinc", [128, 128])
    P.dma("sp", identf, ident_d, writes=[t_identf])
    P.dma("sp", trile, trile_d, writes=[t_trile])
    P.dma("sp", sgt, sgt_d, writes=[t_sgt])
    P.op("dve", lambda e: e.tensor_copy(out=identb, in_=identf), [t_identf], [t_identb])
    P.op("pool", lambda e: e.memset(onesf, 1.0), [], [t_onesf])
    P.op("pool", lambda e: e.memset(onesb, 1.0), [], [t_onesb])
    P.op("dve", lambda e: e.tensor_tensor(out=lowinc, in0=sgt, in1=identf, op=ALU.add),
         [t_sgt, t_identf], [t_lowinc])
    P.op("dve", lambda e: e.tensor_scalar(out=tribias, in0=lowinc, scalar1=-1.0, scalar2=1e30,
                                          op0=ALU.add, op1=ALU.mult), [t_lowinc], [t_tribias])

    def col_layout(name, src, n):
        t, tok = sbt(name, [128, n])
        P.dma("sp", t, src.rearrange("(c p) -> p c", p=128), writes=[tok],
              allow_slow_non_contiguous=True)
        return t, tok

    def bcast_layout(name, src, n):
        t, tok = sbt(name, [128, n])
        P.dma("sp", t, src.partition_broadcast(128), writes=[tok])
        return t, tok

    gA, t_gA = col_layout("gA", g_attn, 8)
    gM, t_gM = col_layout("gM", g_mem, 8)
    gX, t_gX = col_layout("gX", g_x, 8)
    gF, t_gF = col_layout("gF", g_f, 8)
    gk_b, t_gk = bcast_layout("gk_b", g_k, 64)
    gmk_b, t_gmk = bcast_layout("gmk_b", g_mk, 64)
    ggdn_b, t_ggdn = bcast_layout("ggdn_b", g_gdn, 128)
    dtb_b, t_dtb = bcast_layout("dtb_b", dt_bias, 4)
    nea_b, t_nea = bcast_layout("nea_b", a_log, 4)
    pow2_b, t_pow2 = bcast_layout("pow2_b", pow2_d, 32)
    P.op("act", lambda e: e.activation(out=nea_b, in_=nea_b, func=AF.Exp), [t_nea], [t_nea])
    P.op("dve", lambda e: e.tensor_scalar(out=nea_b, in0=nea_b, scalar1=-1.0, scalar2=None,
                                          op0=ALU.mult), [t_nea], [t_nea])
    gq8, t_gq8 = sbt("gq8", [128, 1])
    gmq8, t_gmq8 = sbt("gmq8", [128, 1])
    for (dst, tdst, src) in ((gq8, t_gq8, g_q), (gmq8, t_gmq8, g_mq)):
        for hh in range(2):
            P.dma("sp", dst[64 * hh:64 * hh + 64, :], src.rearrange("(d o) -> d o", o=1),
                  writes=[tdst], allow_slow_non_contiguous=True)
        P.op("pool", lambda e, dst=dst: e.tensor_scalar(out=dst, in0=dst, scalar1=0.125, scalar2=1.0,
                                                        op0=ALU.mult, op1=ALU.mult), [tdst], [tdst])
    cw, t_cw = sbt("cw", [128, 12, 4])
    for tap in range(4):
        P.dma("sp", cw[:, :, tap], conv_w[tap].rearrange("(j p) -> p j", p=128), writes=[t_cw],
              allow_slow_non_contiguous=True)
    neghalf, t_nh = sbt("neghalf", [128, 8])
    P.op("pool", lambda e: e.memset(neghalf, -0.5), [], [t_nh])

    ps = [nc.alloc_psum_tensor("ps%d" % i, [128, 512], F32).ap() for i in range(8)]
    t_ps = [Tok() for _ in range(8)]
    psb16 = [p_.bitcast(BF16) for p_ in ps]

    stage = [None, None]
    t_stage = [Tok(), Tok()]
    k.wl = 0

    def alloc_stage(n):
        for i in range(2):
            stage[i] = sb("wstage%d" % i, [128, n], F32)

    def load_rows(dst_kc, t_dst, src_rows, n, gain_col, t_gain):
        i = k.wl % 2
        k.wl += 1
        st = stage[i][:, 0:n]
        P.dma("sp", st, src_rows, writes=[t_stage[i]])
        if gain_col is None:
            if k.wl % 2 == 0:
                P.op("act", lambda e: e.activation(out=dst_kc, in_=st, func=AF.Copy),
                     [t_stage[i]], [t_dst])
            else:
                P.op("pool", lambda e: e.tensor_copy(out=dst_kc, in_=st), [t_stage[i]], [t_dst])
        else:
            if k.wl % 2 == 0:
                P.op("act", lambda e: e.activation(out=dst_kc, in_=st, func=AF.Copy, scale=gain_col),
                     [t_stage[i], t_gain], [t_dst])
            else:
                P.op("pool", lambda e: e.tensor_scalar(out=dst_kc, in0=st, scalar1=gain_col, scalar2=1.0,
                                                       op0=ALU.mult, op1=ALU.mult),
                     [t_stage[i], t_gain], [t_dst])

    def load_weight(dst, t_dst, src, c0, c1, gain, t_gain, d0=0):
        n = c1 - c0
        for kc in range(8):
            load_rows(dst[:, kc, d0:d0 + n], t_dst, src[kc * 128:(kc + 1) * 128, c0:c1], n,
                      None if gain is None else gain[:, kc:kc + 1], t_gain)

    xt = [sb("xt%d" % i, [128, D], F32) for i in range(2)]
    t_xt = [Tok(), Tok()]
    hb = [sb("hb%d" % i, [128, D], BF16) for i in range(2)]
    t_hb = [Tok(), Tok()]
    sq_scr, t_sq = sbt("sq_scr", [128, D], BF16)
    stat = [sb("stat%d" % i, [128, 4], F32) for i in range(2)]
    t_stat = [Tok(), Tok()]
    k.nt = 0
    for i in range(2):
        P.op("pool", lambda e, i=i: e.memset(xt[i], 0.0), [], [t_xt[i]])

    k.pref = {}

    def load_x(src_rows, nrows, key=None):
        if key is not None and key in k.pref:
            return k.pref.pop(key)
        i = k.nt % 2
        k.nt += 1
        P.dma("sp", xt[i][0:nrows, :], src_rows, writes=[t_xt[i]])
        return xt[i], t_xt[i], i

    def prefetch_x(src_rows, nrows, key):
        k.pref[key] = load_x(src_rows, nrows)

    def norm_T(x, tx, i, hT, t_hT, col, psb):
        s, ts = stat[i], t_stat[i]
        P.op("act", lambda e: e.activation(out=sq_scr, in_=x, func=AF.Square,
                                           accum_out=s[:, 0:1]), [tx], [t_sq, ts])
        P.op("pool", lambda e: e.tensor_scalar(out=s[:, 1:2], in0=s[:, 0:1], scalar1=1.0 / D,
                                               scalar2=EPS, op0=ALU.mult, op1=ALU.add), [ts], [ts])
        P.op("pool", lambda e: e.tensor_tensor(out=s[:, 2:3], in0=s[:, 1:2], in1=neghalf[:, 0:1],
                                               op=ALU.pow), [ts, t_nh], [ts])
        h, th = hb[i], t_hb[i]
        P.op("act", lambda e: e.activation(out=h, in_=x, func=AF.Copy, scale=s[:, 2:3]),
             [tx, ts], [th])
        pb = psb16[psb]
        for kc in range(8):
            P.op("pe", lambda e, kc=kc: e.transpose(out=pb[:, kc * 128:(kc + 1) * 128],
                                                    in_=h[:, kc * 128:(kc + 1) * 128],
                                                    identity=identb),
                 [th, t_identb], [t_ps[psb]])
        P.op("dve", lambda e: e.tensor_copy(out=hT[:, :, col:col + 128],
                                            in_=pb.rearrange("p (k t) -> p k t", k=8)),
             [t_ps[psb]], [t_hT])

    def norm_transpose(src_rows, nrows, hT, t_hT, col, psb, key=None):
        x, tx, i = load_x(src_rows, nrows, key)
        norm_T(x, tx, i, hT, t_hT, col, psb)
        return x, tx

    def rstd_groups(dst, t_dst, ssq, t_ssq, n, width):
        P.op("pool", lambda e: e.tensor_scalar(out=dst, in0=ssq, scalar1=1.0 / width, scalar2=EPS,
                                               op0=ALU.mult, op1=ALU.add), [t_ssq], [t_dst])
        P.op("pool", lambda e: e.tensor_tensor(out=dst, in0=dst, in1=neghalf[:, 0:n], op=ALU.pow),
             [t_dst, t_nh], [t_dst])

    def head_rms(psrc, t_psrc, nh, scr, t_scr, ss, t_ss):
        P.op("act", lambda e: e.activation(out=scr[:, 0:nh * 64], in_=psrc, func=AF.Square),
             [t_psrc], [t_scr])
        P.op("dve", lambda e: e.tensor_reduce(out=ss[:, 0:nh],
                                              in_=scr[:, 0:nh * 64].rearrange("p (h d) -> p h d", h=nh),
                                              axis=AX.X, op=ALU.add), [t_scr], [t_ss])
        rstd_groups(ss[:, nh:2 * nh], t_ss, ss[:, 0:nh], t_ss, nh, 64)

    qscr, t_qscr = sbt("qscr", [128, 512])
    qss, t_qss = sbt("qss", [128, 16])
    PERSIST_TOP = k.top

    def prompt_seq(sq):
        k.top = PERSIST_TOP
        if STOP <= 0:
            return
        row_base = sq * SEQ
        mkT2, t_mkT2 = sbt("mkT2", [128, 2, 256], BF16)
        mv_b, t_mv = sbt("mv_b", [128, 2, 256], BF16)
        o_gdnT, t_ogT = sbt("o_gdnT", [128, 4, SEQ], BF16)
        SEQ_TOP = k.top
        alloc_stage(256)
        wmkv, t_wmkv = sbt("wmkv", [128, 8, 512], BF16)
        load_weight(wmkv, t_wmkv, w_mk, 0, 256, gM, t_gM, 0)
        load_weight(wmkv, t_wmkv, w_mv, 0, 256, gM, t_gM, 256)
        hTm, t_hTm = sbt("hTm", [128, 8, 128], BF16)
        mko = [sbt("mko%d" % i, [128, 512]) for i in range(2)]
        mkb, t_mkb = sbt("mkb", [128, 256], BF16)
        for mt in range(2):
            row0 = sq * MEM + mt * 128
            norm_transpose(memp[row0:row0 + 128, :], 128, hTm, t_hTm, 0, 0)
            for kc in range(8):
                P.op("pe", lambda e, kc=kc: e.matmul(ps[1], lhsT=hTm[:, kc, :], rhs=wmkv[:, kc, :],
                                                     start=(kc == 0), stop=(kc == 7)),
                     [t_hTm, t_wmkv], [t_ps[1]])
            o, to = mko[mt]
            P.op("act", lambda e, o=o: e.activation(out=o[:, 256:512], in_=ps[1][:, 256:512], func=AF.Copy),
                 [t_ps[1]], [to])
            P.op("act", lambda e, mt=mt: e.activation(out=mv_b[:, mt, :], in_=ps[1][:, 256:512], func=AF.Copy),
                 [t_ps[1]], [t_mv])
            head_rms(ps[1][:, 0:256], t_ps[1], 4, qscr, t_qscr, qss, t_qss)
            P.op("dve", lambda e, o=o: e.tensor_tensor(
                out=o[:, 0:256].rearrange("p (h d) -> p h d", h=4),
                in0=ps[1][:, 0:256].rearrange("p (h d) -> p h d", h=4),
                in1=qss[:, 4:8].unsqueeze(2).to_broadcast([128, 4, 64]), op=ALU.mult),
                [t_ps[1], t_qss], [to])
            P.op("pool", lambda e, o=o: e.tensor_tensor(
                out=o[:, 0:256].rearrange("p (h d) -> p h d", h=4),
                in0=o[:, 0:256].rearrange("p (h d) -> p h d", h=4),
                in1=gmk_b.unsqueeze(1).to_broadcast([128, 4, 64]), op=ALU.mult),
                [to, t_gmk], [to])
            P.dma("sp", memk_p[row0:row0 + 128, :], o[:, 0:256], reads=[to])
            P.dma("sp", memv_p[row0:row0 + 128, :], o[:, 256:512], reads=[to])
            P.op("act", lambda e, o=o: e.activation(out=mkb, in_=o[:, 0:256], func=AF.Copy), [to], [t_mkb])
            pb = psb16[2]
            for a in range(2):
                P.op("pe", lambda e, a=a: e.transpose(out=pb[:, a * 128:(a + 1) * 128],
                                                      in_=mkb[:, a * 128:(a + 1) * 128], identity=identb),
                     [t_mkb, t_identb], [t_ps[2]])
            P.op("dve", lambda e, mt=mt: e.tensor_copy(
                out=mkT2[:, :, mt * 128:(mt + 1) * 128],
                in_=pb[:, 0:256].rearrange("p (a t) -> p a t", a=2)), [t_ps[2]], [t_mkT2])
        P.barrier()
        if STOP <= 1:
            return
        k.top = SEQ_TOP
        gdn_phase(sq, o_gdnT, t_ogT)
        P.barrier()
        if STOP <= 2:
            return
        k.top = SEQ_TOP
        o_atT, t_oatT = sbt("o_atT", [128, 4, SEQ], BF16)
        D_TOP = k.top
        dsa_phase(sq, o_atT, t_oatT)
        P.barrier()
        if STOP <= 3:
            return
        k.top = D_TOP
        c1_phase(sq, o_atT, t_oatT, o_gdnT, t_ogT, mkT2, t_mkT2, mv_b, t_mv)
        P.barrier()
        if STOP <= 4:
            return
        k.top = PERSIST_TOP
        c2_phase(sq)
        P.barrier()

    def gdn_phase(sq, o_gdnT, t_ogT):
        row_base = sq * SEQ
        qTg, t_qTg = sbt("qTg", [128, 4, SEQ], BF16)
        kTg, t_kTg = sbt("kTg", [128, 4, SEQ], BF16)
        k_tm, t_ktm = sbt("k_tm", [128, NT, 4, 128], BF16)
        v_tm, t_vtm = sbt("v_tm", [128, NT, 4, 128], BF16)
        sgz, t_sgz = sbt("sgz", [128, NT, 512], BF16)
        gab, t_gab = sbt("gab", [128, NT, 8])
        G_TOP = k.top
        alloc_stage(2056)
        wB, t_wB = sbt("wB", [128, 8, 2056], BF16)
        load_weight(wB, t_wB, w_in, 1092, 3148, gA, t_gA)
        hTg, t_hTg = sbt("hTg", [128, 8, 512], BF16)
        Cq, t_Cq = sbt("Cq", [128, 4, 512])
        cvT, t_cvT = sbt("cvT", [128, 4, 512], BF16)
        Uc = [sbt("Uc%d" % i, [128, 515]) for i in range(2)]
        cacc = [sbt("cacc%d" % i, [128, 512]) for i in range(2)]
        halo, t_halo = sbt("halo", [128, 12, 3])
        sqb, t_sqb = sbt("sqb", [128, 512], BF16)
        lnb, t_lnb = sbt("lnb", [128, 512])
        P.op("pool", lambda e: e.memset(halo, 0.0), [], [t_halo])
        def conv_chunk(j, grp):
            b = 3 + (j % 2)
            for kc in range(8):
                P.op("pe", lambda e, kc=kc: e.matmul(
                    ps[b], lhsT=wB[:, kc, 128 * j:128 * j + 128], rhs=hTg[:, kc, :],
                    start=(kc == 0), stop=(kc == 7)), [t_hTg, t_wB], [t_ps[b]])
            u, tu = Uc[j % 2]
            ca, tca = cacc[j % 2]
            P.op("act", lambda e: e.activation(out=u[:, 3:515], in_=ps[b], func=AF.Copy), [t_ps[b]], [tu])
            P.op("pool", lambda e: e.tensor_copy(out=u[:, 0:3], in_=halo[:, j, :]), [t_halo], [tu])
            P.op("dve", lambda e: e.tensor_scalar(out=ca, in0=u[:, 0:512], scalar1=cw[:, j, 0:1], scalar2=None,
                                                  op0=ALU.mult), [tu, t_cw], [tca])
            for tap in range(1, 4):
                P.op("dve", lambda e, tap=tap: e.scalar_tensor_tensor(
                    out=ca, in0=u[:, tap:tap + 512], scalar=cw[:, j, tap:tap + 1], in1=ca,
                    op0=ALU.mult, op1=ALU.add), [tu, t_cw, tca], [tca])
            P.op("pool", lambda e: e.tensor_copy(out=halo[:, j, :], in_=u[:, 512:515]), [tu], [t_halo])
            if j < 8:
                P.op("act", lambda e: e.activation(out=Cq[:, j % 4, :], in_=ca, func=AF.Silu), [tca], [t_Cq])
            else:
                P.op("act", lambda e: e.activation(out=cvT[:, j - 8, :], in_=ca, func=AF.Silu), [tca], [t_cvT])

        def norm_chunk(j, gc0):
            P.op("act", lambda e: e.activation(out=sqb, in_=Cq[:, j % 4, :], func=AF.Square), [t_Cq], [t_sqb])
            P.op("pe", lambda e: e.matmul(ps[5], lhsT=onesb, rhs=sqb, start=True, stop=True),
                 [t_sqb, t_onesb], [t_ps[5]])
            P.op("act", lambda e: e.activation(out=lnb, in_=ps[5], func=AF.Ln, bias=1e-6, scale=1.0),
                 [t_ps[5]], [t_lnb])
            bias = (-0.5 * float(np.log(128.0))) if j < 4 else 0.0
            P.op("act", lambda e: e.activation(out=lnb, in_=lnb, func=AF.Exp, bias=bias, scale=-0.5),
                 [t_lnb], [t_lnb])
            dstT, tdst = (qTg, t_qTg) if j < 4 else (kTg, t_kTg)
            P.op("dve", lambda e: e.tensor_tensor(out=dstT[:, j % 4, gc0:gc0 + 512], in0=Cq[:, j % 4, :], in1=lnb,
                                                  op=ALU.mult), [t_Cq, t_lnb], [tdst])

        def g_tile(grp, t4):
            ti = grp * 4 + t4
            r0 = row_base + ti * 128
            norm_transpose(xp[r0:r0 + 128, :], 128, hTg, t_hTg, t4 * 128, 0, key=("g", r0))
            if ti + 1 < NT:
                prefetch_x(xp[r0 + 128:r0 + 256, :], 128, ("g", r0 + 128))
            for kc in range(8):
                P.op("pe", lambda e, kc=kc: e.matmul(
                    ps[1], lhsT=hTg[:, kc, t4 * 128:(t4 + 1) * 128], rhs=wB[:, kc, 1536:2048],
                    start=(kc == 0), stop=(kc == 7)), [t_hTg, t_wB], [t_ps[1]])
            for kc in range(8):
                P.op("pe", lambda e, kc=kc: e.matmul(
                    ps[2][:, 0:8], lhsT=hTg[:, kc, t4 * 128:(t4 + 1) * 128], rhs=wB[:, kc, 2048:2056],
                    start=(kc == 0), stop=(kc == 7)), [t_hTg, t_wB], [t_ps[2]])
            P.op("act", lambda e: e.activation(out=sgz[:, ti, :], in_=ps[1], func=AF.Silu), [t_ps[1]], [t_sgz])
            P.op("dve", lambda e: e.tensor_copy(out=gab[:, ti, :], in_=ps[2][:, 0:8]), [t_ps[2]], [t_gab])

        def g_transposes(grp, t4):
            ti = grp * 4 + t4
            gc0 = grp * 512
            for (srcT, tsrc, c0, dst, tdst, b) in ((kTg, t_kTg, gc0 + t4 * 128, k_tm, t_ktm, 6),
                                                   (cvT, t_cvT, t4 * 128, v_tm, t_vtm, 7)):
                pb = psb16[b]
                for h in range(4):
                    P.op("pe", lambda e, h=h, srcT=srcT, c0=c0, pb=pb: e.transpose(
                        out=pb[:, h * 128:(h + 1) * 128], in_=srcT[:, h, c0:c0 + 128], identity=identb),
                        [tsrc, t_identb], [t_ps[b]])
                P.op("act", lambda e, dst=dst, pb=pb: e.activation(
                    out=dst[:, ti, :, :], in_=pb[:, 0:512].rearrange("p (h d) -> p h d", h=4), func=AF.Copy),
                    [t_ps[b]], [tdst])

        for grp in range(4):
            for t4 in range(4):
                g_tile(grp, t4)
            for j in range(0, 4):
                conv_chunk(j, grp)
            for j in range(0, 4):
                norm_chunk(j, grp * 512)
            for j in range(4, 12):
                conv_chunk(j, grp)
            for j in range(4, 8):
                norm_chunk(j, grp * 512)
            for t4 in range(4):
                g_transposes(grp, t4)
        P.barrier()
        if STOP <= 1.5:
            return
        k.top = G_TOP
        gall, t_gall = sbt("gall", [128, NT, 4])
        ball, t_ball = sbt("ball", [128, NT, 4])
        tmpa, t_tmpa = sbt("tmpa", [128, NT, 4])
        tmpb, t_tmpb = sbt("tmpb", [128, NT, 4])
        P.op("dve", lambda e: e.tensor_tensor(out=gall, in0=gab[:, :, 0:4],
                                              in1=dtb_b.unsqueeze(1).to_broadcast([128, NT, 4]), op=ALU.add),
             [t_gab, t_dtb], [t_gall])
        P.op("dve", lambda e: e.tensor_scalar(out=tmpa, in0=gall, scalar1=-1.0, scalar2=None, op0=ALU.mult),
             [t_gall], [t_tmpa])
        P.op("dve", lambda e: e.tensor_tensor(out=tmpa, in0=tmpa, in1=gall, op=ALU.min), [t_tmpa, t_gall], [t_tmpa])
        P.op("act", lambda e: e.activation(out=tmpa, in_=tmpa, func=AF.Exp), [t_tmpa], [t_tmpa])
        P.op("act", lambda e: e.activation(out=tmpa, in_=tmpa, func=AF.Ln, bias=1.0, scale=1.0), [t_tmpa], [t_tmpa])
        P.op("dve", lambda e: e.scalar_tensor_tensor(out=tmpb, in0=gall, scalar=0.0, in1=tmpa,
                                                     op0=ALU.max, op1=ALU.add), [t_gall, t_tmpa], [t_tmpb])
        P.op("dve", lambda e: e.tensor_tensor(out=gall, in0=tmpb,
                                              in1=nea_b.unsqueeze(1).to_broadcast([128, NT, 4]), op=ALU.mult),
             [t_tmpb, t_nea], [t_gall])
        P.op("act", lambda e: e.activation(out=ball, in_=gab[:, :, 4:8], func=AF.Sigmoid), [t_gab], [t_ball])

        S, t_S = sbt("S", [128, 4, 128])
        Sb, t_Sb = sbt("Sb", [128, 4, 128], BF16)
        P.op("pool", lambda e: e.memset(S, 0.0), [], [t_S])
        P.op("pool", lambda e: e.memset(Sb, 0.0), [], [t_Sb])
        NB = 2
        bufs = []
        for i in range(NB):
            bb = {}
            for nm, dt_ in (("Gh", F32), ("E", F32), ("Du", F32), ("Dl", F32), ("L0", BF16), ("L1", BF16),
                            ("M0", BF16), ("M1", BF16), ("P0", BF16), ("P1", BF16), ("kbg", BF16),
                            ("kdec", BF16), ("vb", BF16), ("u", F32), ("wT", BF16), ("qgT", BF16),
                            ("qkT", BF16), ("vnew", BF16), ("on", F32), ("og", BF16)):
                bb[nm] = sbt("%s_%d" % (nm, i), [128, 4, 128], dt_)
            bb["sc"] = sbt("gsc_%d" % i, [128, 40])
            bufs.append(bb)
        k.pbank = 0

        def bank():
            b = k.pbank
            k.pbank = (k.pbank + 1) % 8
            return b

        def gdn_tile(ti):
            B = bufs[ti % NB]
            par = ti % 2
            bstate = [0]

            def bank():
                b_ = par * 4 + bstate[0]
                bstate[0] = (bstate[0] + 1) % 4
                return b_
            c0 = ti * 128
            sc, tsc = B["sc"]
            Gh, tGh = B["Gh"]
            for h in range(4):
                P.op("pool", lambda e, h=h, Gh=Gh, ti=ti: e.tensor_scalar(
                    out=Gh[:, h, :], in0=trile, scalar1=gall[:, ti, h:h + 1], scalar2=1.0,
                    op0=ALU.mult, op1=ALU.mult), [t_trile, t_gall], [tGh])
                yield
            bE, bDu, bDl, bsm = bank(), bank(), bank(), bank()
            for h in range(4):
                P.op("pe", lambda e, h=h, Gh=Gh, bE=bE: e.matmul(ps[bE][:, h * 128:(h + 1) * 128], lhsT=onesf,
                                                                 rhs=Gh[:, h, :], start=True, stop=True),
                     [tGh, t_onesf], [t_ps[bE]])
                yield
                P.op("pe", lambda e, h=h, Gh=Gh, bDu=bDu: e.matmul(ps[bDu][:, h * 128:(h + 1) * 128], lhsT=sgt,
                                                                   rhs=Gh[:, h, :], start=True, stop=True),
                     [tGh, t_sgt], [t_ps[bDu]])
                yield
                P.op("pe", lambda e, h=h, Gh=Gh, bDl=bDl: e.matmul(ps[bDl][:, h * 128:(h + 1) * 128], lhsT=Gh[:, h, :],
                                                                   rhs=sgt, start=True, stop=True),
                     [tGh, t_sgt], [t_ps[bDl]])
                yield
            if GCUT <= 1:
                return
            P.op("pe", lambda e, ti=ti, bsm=bsm: e.matmul(ps[bsm][:, 0:4], lhsT=trile, rhs=gall[:, ti, :],
                                                          start=True, stop=True), [t_trile, t_gall], [t_ps[bsm]])
            yield
            P.op("pe", lambda e, ti=ti, bsm=bsm: e.matmul(ps[bsm][:, 8:12], lhsT=onesf, rhs=gall[:, ti, :],
                                                          start=True, stop=True), [t_onesf, t_gall], [t_ps[bsm]])
            yield
            if GCUT <= 2:
                return
            E, tE = B["E"]
            Du, tDu = B["Du"]
            Dl, tDl = B["Dl"]
            fl = lambda a: a.rearrange("p h c -> p (h c)")
            P.op("act", lambda e, E=E, bE=bE: e.activation(out=fl(E), in_=ps[bE], func=AF.Exp), [t_ps[bE]], [tE])
            yield
            P.op("act", lambda e, Du=Du, bDu=bDu: e.activation(out=fl(Du), in_=ps[bDu], func=AF.Exp), [t_ps[bDu]], [tDu])
            yield
            P.op("act", lambda e, Dl=Dl, bDl=bDl: e.activation(out=fl(Dl), in_=ps[bDl], func=AF.Exp), [t_ps[bDl]], [tDl])
            yield
            P.op("pool", lambda e, Du=Du: e.tensor_tensor(out=Du, in0=Du, in1=trile.unsqueeze(1).to_broadcast([128, 4, 128]),
                                                          op=ALU.mult), [tDu, t_trile], [tDu])
            yield
            P.op("pool", lambda e, Dl=Dl: e.tensor_tensor(out=Dl, in0=Dl, in1=sgt.unsqueeze(1).to_broadcast([128, 4, 128]),
                                                          op=ALU.mult), [tDl, t_sgt], [tDl])
            yield
            if GCUT <= 3:
                return
            P.op("dve", lambda e, sc=sc, bsm=bsm: e.tensor_copy(out=sc[:, 0:4], in_=ps[bsm][:, 0:4]), [t_ps[bsm]], [tsc])
            yield
            P.op("act", lambda e, sc=sc: e.activation(out=sc[:, 4:8], in_=sc[:, 0:4], func=AF.Exp), [tsc], [tsc])
            yield
            P.op("dve", lambda e, sc=sc, bsm=bsm: e.tensor_tensor(out=sc[:, 24:28], in0=ps[bsm][:, 8:12], in1=sc[:, 0:4],
                                                                  op=ALU.subtract), [t_ps[bsm], tsc], [tsc])
            yield
            P.op("act", lambda e, sc=sc: e.activation(out=sc[:, 8:12], in_=sc[:, 24:28], func=AF.Exp), [tsc], [tsc])
            yield
            P.op("act", lambda e, sc=sc, bsm=bsm: e.activation(out=sc[:, 12:16], in_=ps[bsm][:, 8:12], func=AF.Exp),
                 [t_ps[bsm]], [tsc])
            yield
            P.op("dve", lambda e, sc=sc, ti=ti: e.tensor_tensor(out=sc[:, 16:20], in0=sc[:, 4:8], in1=ball[:, ti, :],
                                                                op=ALU.mult), [tsc, t_ball], [tsc])
            yield
            P.op("dve", lambda e, sc=sc, ti=ti: e.tensor_scalar(out=sc[:, 20:24], in0=ball[:, ti, :], scalar1=-1.0,
                                                                scalar2=None, op0=ALU.mult), [t_ball], [tsc])
            yield
            if GCUT <= 4:
                return
            kbg, tkbg = B["kbg"]
            kdec, tkdec = B["kdec"]
            vb, tvb = B["vb"]
            bc = lambda a: a.unsqueeze(2).to_broadcast([128, 4, 128])
            P.op("pool", lambda e, kbg=kbg, sc=sc, ti=ti: e.tensor_tensor(out=kbg, in0=k_tm[:, ti, :, :], in1=bc(sc[:, 16:20]),
                                                                          op=ALU.mult), [t_ktm, tsc], [tkbg])
            yield
            P.op("pool", lambda e, kdec=kdec, sc=sc, ti=ti: e.tensor_tensor(out=kdec, in0=k_tm[:, ti, :, :], in1=bc(sc[:, 8:12]),
                                                                            op=ALU.mult), [t_ktm, tsc], [tkdec])
            yield
            P.op("pool", lambda e, vb=vb, ti=ti: e.tensor_tensor(out=vb, in0=v_tm[:, ti, :, :], in1=bc(ball[:, ti, :]),
                                                                 op=ALU.mult), [t_vtm, t_ball], [tvb])
            yield
            if GCUT <= 5:
                return
            bkk = bank()
            for h in range(4):
                P.op("pe", lambda e, h=h, bkk=bkk: e.matmul(ps[bkk][:, h * 128:(h + 1) * 128], lhsT=kTg[:, h, c0:c0 + 128],
                                                            rhs=kTg[:, h, c0:c0 + 128], start=True, stop=True),
                     [t_kTg], [t_ps[bkk]])
                yield
            Lc, tLc = B["L0"]
            Ln_, tLn = B["L1"]
            Mc, tMc = B["M0"]
            Mn, tMn = B["M1"]
            Pc, tPc = B["P0"]
            Pn, tPn = B["P1"]
            for h in range(4):
                P.op("dve", lambda e, h=h, Lc=Lc, sc=sc, Dl=Dl, bkk=bkk: e.scalar_tensor_tensor(
                    out=Lc[:, h, :], in0=ps[bkk][:, h * 128:(h + 1) * 128], scalar=sc[:, 20 + h:21 + h], in1=Dl[:, h, :],
                    op0=ALU.mult, op1=ALU.mult), [t_ps[bkk], tsc, tDl], [tLc])
                yield
            if GCUT <= 6:
                return
            bM = bank()
            pbm = psb16[bM]
            for h in range(4):
                P.op("pe", lambda e, h=h, Lc=Lc, pbm=pbm: e.transpose(out=pbm[:, h * 128:(h + 1) * 128], in_=Lc[:, h, :],
                                                                      identity=identb), [tLc, t_identb], [t_ps[bM]])
                yield
            pm3 = pbm[:, 0:512].rearrange("p (h c) -> p h c", h=4)
            if GCUT == 61:
                return
            P.op("act", lambda e, Mc=Mc, pm3=pm3: e.activation(out=Mc, in_=pm3, func=AF.Copy), [t_ps[bM]], [tMc])
            yield
            if GCUT == 62:
                return
            P.op("pool", lambda e, Pc=Pc, Mc=Mc: e.tensor_tensor(out=Pc, in0=Mc,
                                                                 in1=identb.unsqueeze(1).to_broadcast([128, 4, 128]),
                                                                 op=ALU.add), [tMc, t_identb], [tPc])
            yield
            if GCUT <= 7 or GCUT in (61, 62):
                return
            for lev in range(6):
                last = (lev == 5)
                bL = bank()
                if not last:
                    bMM = bank()
                    for h in range(4):
                        P.op("pe", lambda e, h=h, Lc=Lc, Mc=Mc, bMM=bMM: e.matmul(
                            ps[bMM][:, h * 128:(h + 1) * 128], lhsT=Lc[:, h, :], rhs=Mc[:, h, :], start=True, stop=True),
                            [tLc, tMc], [t_ps[bMM]])
                        yield
                for h in range(4):
                    P.op("pe", lambda e, h=h, Lc=Lc, Mc=Mc, bL=bL: e.matmul(
                        ps[bL][:, h * 128:(h + 1) * 128], lhsT=Mc[:, h, :], rhs=Lc[:, h, :], start=True, stop=True),
                        [tLc, tMc], [t_ps[bL]])
                    yield
                P.op("dve", lambda e, Ln_=Ln_, bL=bL: e.tensor_copy(out=fl(Ln_), in_=ps[bL]), [t_ps[bL]], [tLn])
                yield
                if not last:
                    P.op("act", lambda e, Mn=Mn, bMM=bMM: e.activation(out=fl(Mn), in_=ps[bMM], func=AF.Copy),
                         [t_ps[bMM]], [tMn])
                    yield
                bP = bank()
                for h in range(4):
                    P.op("pe", lambda e, h=h, Ln_=Ln_, Pc=Pc, bP=bP: e.matmul(
                        ps[bP][:, h * 128:(h + 1) * 128], lhsT=Ln_[:, h, :], rhs=Pc[:, h, :], start=True, stop=True),
                        [tLn, tPc], [t_ps[bP]])
                    yield
                P.op("dve", lambda e, Pn=Pn, Pc=Pc, bP=bP: e.tensor_tensor(out=fl(Pn), in0=ps[bP], in1=fl(Pc), op=ALU.add),
                     [t_ps[bP], tPc], [tPn])
                yield
                Lc, tLc, Ln_, tLn = Ln_, tLn, Lc, tLc
                Mc, tMc, Mn, tMn = Mn, tMn, Mc, tMc
                Pc, tPc, Pn, tPn = Pn, tPn, Pc, tPc
            if GCUT <= 8:
                return
            bu, bw, bq = bank(), bank(), bank()
            for h in range(4):
                P.op("pe", lambda e, h=h, Pc=Pc, vb=vb, bu=bu: e.matmul(ps[bu][:, h * 128:(h + 1) * 128], lhsT=Pc[:, h, :],
                                                                        rhs=vb[:, h, :], start=True, stop=True),
                     [tPc, tvb], [t_ps[bu]])
                yield
                P.op("pe", lambda e, h=h, Pc=Pc, kbg=kbg, bw=bw: e.matmul(ps[bw][:, h * 128:(h + 1) * 128], lhsT=kbg[:, h, :],
                                                                          rhs=Pc[:, h, :], start=True, stop=True),
                     [tPc, tkbg], [t_ps[bw]])
                yield
                P.op("pe", lambda e, h=h, bq=bq: e.matmul(ps[bq][:, h * 128:(h + 1) * 128], lhsT=kTg[:, h, c0:c0 + 128],
                                                          rhs=qTg[:, h, c0:c0 + 128], start=True, stop=True),
                     [t_kTg, t_qTg], [t_ps[bq]])
                yield
            u, tu_ = B["u"]
            wT, twT = B["wT"]
            qgT, tqgT = B["qgT"]
            qkT, tqkT = B["qkT"]
            P.op("act", lambda e, u=u, bu=bu: e.activation(out=fl(u), in_=ps[bu], func=AF.Copy), [t_ps[bu]], [tu_])
            yield
            P.op("act", lambda e, wT=wT, bw=bw: e.activation(out=fl(wT), in_=ps[bw], func=AF.Copy), [t_ps[bw]], [twT])
            yield
            P.op("dve", lambda e, qkT=qkT, Du=Du, bq=bq: e.tensor_tensor(out=fl(qkT), in0=ps[bq], in1=fl(Du), op=ALU.mult),
                 [t_ps[bq], tDu], [tqkT])
            yield
            P.op("pool", lambda e, qgT=qgT, E=E: e.tensor_tensor(out=qgT, in0=qTg[:, :, c0:c0 + 128], in1=E, op=ALU.mult),
                 [t_qTg, tE], [tqgT])
            yield
            if GCUT <= 9:
                return
            yield 'SEQ'
            bws, bo, bs = bank(), bank(), bank()
            for h in range(4):
                P.op("pe", lambda e, h=h, wT=wT, bws=bws: e.matmul(ps[bws][:, h * 128:(h + 1) * 128], lhsT=wT[:, h, :],
                                                                   rhs=Sb[:, h, :], start=True, stop=True),
                     [twT, t_Sb], [t_ps[bws]])
            vnew, tvn = B["vnew"]
            P.op("dve", lambda e, vnew=vnew, u=u, bws=bws: e.tensor_tensor(out=fl(vnew), in0=fl(u), in1=ps[bws],
                                                                           op=ALU.subtract), [tu_, t_ps[bws]], [tvn])
            for h in range(4):
                P.op("pe", lambda e, h=h, qgT=qgT, bo=bo: e.matmul(ps[bo][:, h * 128:(h + 1) * 128], lhsT=qgT[:, h, :],
                                                                   rhs=Sb[:, h, :], start=True, stop=False),
                     [tqgT, t_Sb], [t_ps[bo]])
                P.op("pe", lambda e, h=h, qkT=qkT, vnew=vnew, bo=bo: e.matmul(ps[bo][:, h * 128:(h + 1) * 128], lhsT=qkT[:, h, :],
                                                                              rhs=vnew[:, h, :], start=False, stop=True),
                     [tqkT, tvn], [t_ps[bo]])
            for h in range(4):
                P.op("pe", lambda e, h=h, kdec=kdec, vnew=vnew, bs=bs: e.matmul(ps[bs][:, h * 128:(h + 1) * 128], lhsT=kdec[:, h, :],
                                                                                rhs=vnew[:, h, :], start=True, stop=True),
                     [tkdec, tvn], [t_ps[bs]])
            for h in range(4):
                P.op("dve", lambda e, h=h, sc=sc, bs=bs: e.scalar_tensor_tensor(
                    out=S[:, h, :], in0=S[:, h, :], scalar=sc[:, 12 + h:13 + h], in1=ps[bs][:, h * 128:(h + 1) * 128],
                    op0=ALU.mult, op1=ALU.add), [t_S, tsc, t_ps[bs]], [t_S])
            P.op("act", lambda e: e.activation(out=Sb, in_=S, func=AF.Copy), [t_S], [t_Sb])
            if GCUT <= 10:
                return
            on, ton = B["on"]
            og, tog = B["og"]
            P.op("act", lambda e, on=on, bo=bo: e.activation(out=fl(on), in_=ps[bo], func=AF.Square), [t_ps[bo]], [ton])
            P.op("dve", lambda e, on=on, sc=sc: e.tensor_reduce(out=sc[:, 28:32], in_=on, axis=AX.X, op=ALU.add),
                 [ton], [tsc])
            P.op("pool", lambda e, sc=sc: e.tensor_scalar(out=sc[:, 32:36], in0=sc[:, 28:32], scalar1=1.0 / 128, scalar2=EPS,
                                                          op0=ALU.mult, op1=ALU.add), [tsc], [tsc])
            P.op("pool", lambda e, sc=sc: e.tensor_tensor(out=sc[:, 32:36], in0=sc[:, 32:36], in1=neghalf[:, 0:4], op=ALU.pow),
                 [tsc, t_nh], [tsc])
            P.op("dve", lambda e, on=on, sc=sc, bo=bo: e.tensor_tensor(
                out=on, in0=ps[bo].rearrange("p (h c) -> p h c", h=4), in1=bc(sc[:, 32:36]), op=ALU.mult),
                [t_ps[bo], tsc], [ton])
            P.op("pool", lambda e, on=on: e.tensor_tensor(out=on, in0=on, in1=ggdn_b.unsqueeze(1).to_broadcast([128, 4, 128]),
                                                          op=ALU.mult), [ton, t_ggdn], [ton])
            P.op("pool", lambda e, on=on, og=og, ti=ti: e.tensor_tensor(
                out=og, in0=on, in1=sgz[:, ti, :].rearrange("p (h c) -> p h c", h=4), op=ALU.mult),
                [ton, t_sgz], [tog])
            bt = bank()
            pbt = psb16[bt]
            for h in range(4):
                P.op("pe", lambda e, h=h, og=og, pbt=pbt: e.transpose(out=pbt[:, h * 128:(h + 1) * 128], in_=og[:, h, :],
                                                                      identity=identb), [tog, t_identb], [t_ps[bt]])
            P.op("act", lambda e, pbt=pbt: e.activation(out=o_gdnT[:, :, c0:c0 + 128],
                                                        in_=pbt[:, 0:512].rearrange("p (h c) -> p h c", h=4), func=AF.Copy),
                 [t_ps[bt]], [t_ogT])
        def run_to_seq(gens):
            live = list(gens)
            while live:
                for g_ in list(live):
                    try:
                        if next(g_) == 'SEQ':
                            live.remove(g_)
                    except StopIteration:
                        live.remove(g_)

        def finish_gen(g_):
            for _ in g_:
                pass

        for t2 in range(0, NT if STOP > 1.8 else 2, 2):
            ga_, gb_ = gdn_tile(t2), gdn_tile(t2 + 1)
            run_to_seq([ga_, gb_])
            finish_gen(ga_)
            finish_gen(gb_)
        P.dma("sp", ssm_p[sq * 512:(sq + 1) * 512, :].rearrange("(h d) v -> d h v", h=4), S, reads=[t_S])

    NIT = 18

    def dsa_phase(sq, o_atT, t_oatT):
        row_base = sq * SEQ
        qT2, t_qT2 = sbt("qT2", [128, NT, 512], BF16)
        kT2, t_kT2 = sbt("kT2", [128, SEQ], BF16)
        v_b, t_vb = sbt("v_b", [128, NT, 128], BF16)
        qiT2, t_qiT2 = sbt("qiT2", [128, 2, SEQ], BF16)
        kiT2, t_kiT2 = sbt("kiT2", [128, SEQ], BF16)
        wi_s, t_wi = sbt("wi_s", [128, NT, 4])
        A_TOP = k.top
        alloc_stage(1536)
        wA, t_wA = sbt("wA", [128, 8, 1092], BF16)
        load_weight(wA, t_wA, w_in, 0, 1092, gA, t_gA)
        wConv, t_wConv = sbt("wConv", [128, 8, 1536], BF16)
        load_weight(wConv, t_wConv, w_in, 1092, 2628, gA, t_gA)
        hT1, t_hT1 = sbt("hT1", [128, 8, 128], BF16)
        ko = [sbt("ko%d" % i, [128, 320]) for i in range(2)]
        cvo, t_cvo = sbt("cvo", [128, 1536])
        qnb, t_qnb = sbt("qnb", [128, 512], BF16)
        kb_, t_kb = sbt("kb_", [128, 128], BF16)
        qib, t_qib = sbt("qib", [128, 256], BF16)
        kib, t_kib = sbt("kib", [128, 128], BF16)

        def proj_tile(t):
            r0 = row_base + t * 128
            c0 = t * 128
            norm_transpose(xp[r0:r0 + 128, :], 128, hT1, t_hT1, 0, 0, key=("d", r0))
            if t + 1 < NT:
                prefetch_x(xp[r0 + 128:r0 + 256, :], 128, ("d", r0 + 128))
            for (b, a0, a1) in ((1, 0, 512), (2, 512, 1024), (3, 1024, 1092)):
                for kc in range(8):
                    P.op("pe", lambda e, kc=kc, b=b, a0=a0, a1=a1: e.matmul(
                        ps[b][:, 0:a1 - a0], lhsT=hT1[:, kc, :], rhs=wA[:, kc, a0:a1],
                        start=(kc == 0), stop=(kc == 7)), [t_hT1, t_wA], [t_ps[b]])
            o, to = ko[t % 2]
            if PCUT <= 1:
                return
            head_rms(ps[1], t_ps[1], 8, qscr, t_qscr, qss, t_qss)
            P.op("dve", lambda e: e.tensor_tensor(
                out=qnb.rearrange("p (r g d) -> p g r d", r=4, g=2),
                in0=ps[1].rearrange("p (g r d) -> p g r d", g=2, r=4),
                in1=qss[:, 8:16].rearrange("p (g r) -> p g r", g=2).unsqueeze(3).to_broadcast([128, 2, 4, 64]),
                op=ALU.mult), [t_ps[1], t_qss], [t_qnb])
            pbq = psb16[4]
            for r in range(4):
                P.op("pe", lambda e, r=r: e.transpose(out=pbq[:, r * 128:(r + 1) * 128], in_=qnb[:, r * 128:(r + 1) * 128],
                                                      identity=identb), [t_qnb, t_identb], [t_ps[4]])
            P.op("act", lambda e: e.activation(out=qT2[:, t, :], in_=pbq[:, 0:512], func=AF.Copy),
                 [t_ps[4]], [t_qT2])
            P.op("pool", lambda e: e.tensor_scalar(out=qT2[:, t, :], in0=qT2[:, t, :], scalar1=gq8[:, 0:1], scalar2=1.0,
                                                   op0=ALU.mult, op1=ALU.mult), [t_qT2, t_gq8], [t_qT2])
            if PCUT <= 2:
                return
            head_rms(ps[2][:, 0:128], t_ps[2], 2, qscr, t_qscr, qss, t_qss)
            P.op("dve", lambda e: e.tensor_tensor(
                out=o[:, 0:128].rearrange("p (h d) -> p h d", h=2),
                in0=ps[2][:, 0:128].rearrange("p (h d) -> p h d", h=2),
                in1=qss[:, 2:4].unsqueeze(2).to_broadcast([128, 2, 64]), op=ALU.mult),
                [t_ps[2], t_qss], [to])
            P.op("pool", lambda e: e.tensor_tensor(
                out=o[:, 0:128].rearrange("p (h d) -> p h d", h=2),
                in0=o[:, 0:128].rearrange("p (h d) -> p h d", h=2),
                in1=gk_b.unsqueeze(1).to_broadcast([128, 2, 64]), op=ALU.mult), [to, t_gk], [to])
            P.op("act", lambda e: e.activation(out=kb_, in_=o[:, 0:128], func=AF.Copy), [to], [t_kb])
            pb5 = psb16[5]
            P.op("pe", lambda e: e.transpose(out=pb5[:, 0:128], in_=kb_, identity=identb), [t_kb, t_identb], [t_ps[5]])
            if PCUT <= 3:
                return
            P.op("act", lambda e: e.activation(out=o[:, 128:256], in_=ps[2][:, 128:256], func=AF.Copy), [t_ps[2]], [to])
            P.op("act", lambda e: e.activation(out=v_b[:, t, :], in_=ps[2][:, 128:256], func=AF.Copy), [t_ps[2]], [t_vb])
            P.op("act", lambda e: e.activation(out=qib, in_=ps[2][:, 256:512], func=AF.Copy, scale=0.125),
                 [t_ps[2]], [t_qib])
            for a in range(2):
                P.op("pe", lambda e, a=a: e.transpose(out=pb5[:, 128 + a * 128:256 + a * 128],
                                                      in_=qib[:, a * 128:(a + 1) * 128], identity=identb),
                     [t_qib, t_identb], [t_ps[5]])
            if PCUT <= 4:
                return
            P.op("act", lambda e: e.activation(out=o[:, 256:320], in_=ps[3][:, 0:64], func=AF.Copy), [t_ps[3]], [to])
            P.op("act", lambda e: e.activation(out=kib[:, 0:64], in_=ps[3][:, 0:64], func=AF.Copy), [t_ps[3]], [t_kib])
            P.op("act", lambda e: e.activation(out=kib[:, 64:128], in_=ps[3][:, 0:64], func=AF.Copy), [t_ps[3]], [t_kib])
            P.op("pe", lambda e: e.transpose(out=pb5[:, 384:512], in_=kib, identity=identb), [t_kib, t_identb], [t_ps[5]])
            P.op("act", lambda e: e.activation(out=wi_s[:, t, :], in_=ps[3][:, 64:68], func=AF.Copy, scale=0.5),
                 [t_ps[3]], [t_wi])
            if PCUT <= 5:
                return
            P.op("act", lambda e: e.activation(out=kT2[:, c0:c0 + 128], in_=pb5[:, 0:128], func=AF.Copy), [t_ps[5]], [t_kT2])
            if PCUT == 51:
                return
            P.op("act", lambda e: e.activation(out=qiT2[:, :, c0:c0 + 128],
                                               in_=pb5[:, 128:384].rearrange("p (a t) -> p a t", a=2), func=AF.Copy),
                 [t_ps[5]], [t_qiT2])
            if PCUT == 52:
                return
            P.op("act", lambda e: e.activation(out=kiT2[:, c0:c0 + 128], in_=pb5[:, 384:512], func=AF.Copy),
                 [t_ps[5]], [t_kiT2])
            if PCUT == 53:
                return
            P.dma("sp", k_p[r0:r0 + 128, :], o[:, 0:128], reads=[to])
            P.dma("sp", v_p[r0:r0 + 128, :], o[:, 128:256], reads=[to])
            P.dma("sp", kidx_p[r0:r0 + 128, :], o[:, 256:320], reads=[to])
            if PCUT <= 6:
                return
            if t == NT - 1:
                for b in range(3):
                    for kc in range(8):
                        P.op("pe", lambda e, kc=kc, b=b: e.matmul(
                            ps[5 + b] if b < 2 else ps[0], lhsT=hT1[:, kc, :], rhs=wConv[:, kc, b * 512:(b + 1) * 512],
                            start=(kc == 0), stop=(kc == 7)), [t_hT1, t_wConv], [t_ps[5 + b] if b < 2 else t_ps[0]])
                for b in range(3):
                    bb = 5 + b if b < 2 else 0
                    P.op("act", lambda e, b=b, bb=bb: e.activation(out=cvo[:, b * 512:(b + 1) * 512], in_=ps[bb],
                                                                   func=AF.Copy), [t_ps[bb]], [t_cvo])
                P.dma("sp", conv_p[sq * 3:(sq + 1) * 3, :], cvo[125:128, :], reads=[t_cvo])

        for t in range(NT):
            proj_tile(t)
        P.barrier()
        if DCUT <= 1:
            return
        k.top = A_TOP
        scb = [sbt("scb%d" % i, [128, SEQ]) for i in range(2)]
        rl = [sbt("rl%d" % i, [128, 512]) for i in range(4)]
        junk2 = [sbt("junk%d" % i, [128, SEQ], BF16) for i in range(2)]
        mask2 = [sbt("mask%d" % i, [128, SEQ], BF16) for i in range(2)]
        maskT2 = [sbt("maskT%d" % i, [128, NT, 128], BF16) for i in range(4)]
        PT = [sbt("PT%d" % i, [128, 4, 128], BF16) for i in range(8)]
        bis2 = [sbt("bis%d" % i, [128, 64]) for i in range(2)]
        rec, t_rec = sbt("rec", [128, 4, 128])

        def q_sb(qt):
            L = (qt + 1) * 128
            q0 = qt * 128
            S_, tS = scb[qt % 2]
            maskT, t_maskT = maskT2[qt % 4]
            bis, t_bis = bis2[qt % 2]
            junk, t_junk = junk2[qt % 2]
            mask, t_mask = mask2[qt % 2]
            nch = (L + 511) // 512
            cnt_ = 0
            for ch in range(nch):
                k0 = ch * 512
                n = min(512, L - k0)
                for ih in range(4):
                    a, b = ih // 2, ih % 2
                    bnk = qt % 2
                    r_, tr = rl[(qt % 2) * 2 + cnt_ % 2]
                    cnt_ += 1
                    P.op("pe", lambda e, a=a, b=b, bnk=bnk, k0=k0, n=n: e.matmul(
                        ps[bnk][:, 0:n], lhsT=qiT2[64 * b:64 * b + 64, a, q0:q0 + 128],
                        rhs=kiT2[64 * b:64 * b + 64, k0:k0 + n], start=True, stop=True),
                        [t_qiT2, t_kiT2], [t_ps[bnk]])
                    yield
                    P.op("act", lambda e, r_=r_, bnk=bnk, n=n: e.activation(out=r_[:, 0:n], in_=ps[bnk][:, 0:n], func=AF.Relu),
                         [t_ps[bnk]], [tr])
                    yield
                    if ih == 0:
                        P.op("dve", lambda e, r_=r_, k0=k0, n=n: e.tensor_scalar(
                            out=S_[:, k0:k0 + n], in0=r_[:, 0:n], scalar1=wi_s[:, qt, 0:1], scalar2=None, op0=ALU.mult),
                            [tr, t_wi], [tS])
                        yield
                    else:
                        P.op("dve", lambda e, r_=r_, k0=k0, n=n, ih=ih: e.scalar_tensor_tensor(
                            out=S_[:, k0:k0 + n], in0=r_[:, 0:n], scalar=wi_s[:, qt, ih:ih + 1], in1=S_[:, k0:k0 + n],
                            op0=ALU.mult, op1=ALU.add), [tr, t_wi, tS], [tS])
                        yield
            if DCUT <= 2:
                return
            dg = S_[:, q0:q0 + 128]
            P.op("dve", lambda e: e.tensor_tensor(out=dg, in0=dg, in1=lowinc, op=ALU.mult), [tS, t_lowinc], [tS])
            yield
            P.op("dve", lambda e: e.tensor_reduce(out=bis[:, 0:1], in_=S_[:, 0:L], axis=AX.X, op=ALU.max), [tS], [t_bis])
            yield
            P.op("dve", lambda e: e.tensor_reduce(out=bis[:, 1:2], in_=S_[:, 0:L], axis=AX.X, op=ALU.min), [tS], [t_bis])
            yield
            P.op("pool", lambda e: e.tensor_tensor(out=dg, in0=dg, in1=tribias, op=ALU.add), [tS, t_tribias], [tS])
            yield
            P.op("dve", lambda e: e.tensor_tensor(out=bis[:, 2:3], in0=bis[:, 0:1], in1=bis[:, 1:2], op=ALU.subtract),
                 [t_bis], [t_bis])
            yield
            P.op("dve", lambda e: e.tensor_scalar(out=bis[:, 3:4], in0=bis[:, 2:3], scalar1=0.5005, scalar2=0.0005,
                                                  op0=ALU.mult, op1=ALU.add), [t_bis], [t_bis])
            yield
            P.op("dve", lambda e: e.tensor_tensor(out=bis[:, 4:5], in0=bis[:, 0:1], in1=bis[:, 3:4], op=ALU.subtract),
                 [t_bis], [t_bis])
            yield
            P.op("dve", lambda e: e.tensor_scalar(out=bis[:, 8:9 + NIT], in0=pow2_b[:, 0:NIT + 1], scalar1=bis[:, 3:4],
                                                  scalar2=None, op0=ALU.mult), [t_bis, t_pow2], [t_bis])
            yield
            for it in range(NIT):
                P.op("dve", lambda e: e.tensor_scalar(out=junk[:, 0:L], in0=S_[:, 0:L], scalar1=bis[:, 4:5], scalar2=None,
                                                      op0=ALU.is_ge, op1=ALU.add, accum_out=bis[:, 5:6]),
                     [tS, t_bis], [t_junk, t_bis])
                yield
                P.op("dve", lambda e: e.tensor_scalar(out=bis[:, 6:7], in0=bis[:, 5:6], scalar1=255.5, scalar2=0.5,
                                                      op0=ALU.is_ge, op1=ALU.subtract), [t_bis], [t_bis])
                yield
                P.op("dve", lambda e, it=it: e.scalar_tensor_tensor(out=bis[:, 4:5], in0=bis[:, 6:7], scalar=bis[:, 8 + it:9 + it],
                                                                    in1=bis[:, 4:5], op0=ALU.mult, op1=ALU.add),
                     [t_bis], [t_bis])
                yield
            P.op("dve", lambda e: e.tensor_tensor(out=bis[:, 7:8], in0=bis[:, 4:5], in1=bis[:, 8 + NIT:9 + NIT], op=ALU.subtract),
                 [t_bis], [t_bis])
            yield
            P.op("dve", lambda e: e.tensor_scalar(out=mask[:, 0:L], in0=S_[:, 0:L], scalar1=bis[:, 7:8], scalar2=None,
                                                  op0=ALU.is_ge), [tS, t_bis], [t_mask])
            yield
            if DCUT <= 3:
                return
            for half in range((qt // 8) + 1):
                nb = min(8, qt + 1 - half * 8)
                for j in range(nb):
                    kb = half * 8 + j
                    P.op("pe", lambda e, kb=kb, j=j, half=half: e.transpose(
                        out=psb16[qt % 2][:, j * 128:(j + 1) * 128], in_=mask[:, kb * 128:(kb + 1) * 128], identity=identb),
                        [t_mask, t_identb], [t_ps[qt % 2]])
                    yield
                P.op("act", lambda e, half=half, nb=nb: e.activation(
                    out=maskT[:, half * 8:half * 8 + nb, :],
                    in_=psb16[qt % 2][:, 0:nb * 128].rearrange("p (j t) -> p j t", j=nb), func=AF.Copy),
                    [t_ps[qt % 2]], [t_maskT])
                yield
        def q_att(qt):
            L = (qt + 1) * 128
            q0 = qt * 128
            maskT, t_maskT = maskT2[qt % 4]
            items = [(kb, g) for kb in range(qt + 1) for g in range(2)]
            LA = 3

            def front(idx):
                kb, g = items[idx]
                bS = 2 + (idx % 4)
                PTt, tPT = PT[idx % len(PT)]
                P.op("pe", lambda e: e.matmul(
                    ps[bS], lhsT=kT2[64 * g:64 * g + 64, kb * 128:(kb + 1) * 128],
                    rhs=qT2[64 * g:64 * g + 64, qt, :], start=True, stop=True),
                    [t_kT2, t_qT2], [t_ps[bS]])
                P.op("act", lambda e: e.activation(out=PTt.rearrange("p r t -> p (r t)"), in_=ps[bS],
                                                   func=AF.Exp), [t_ps[bS]], [tPT])
                P.op("pool", lambda e: e.tensor_tensor(
                    out=PTt, in0=PTt, in1=maskT[:, kb, :].unsqueeze(1).to_broadcast([128, 4, 128]), op=ALU.mult),
                    [tPT, t_maskT], [tPT])

            def back(idx):
                kb, g = items[idx]
                PTt, tPT = PT[idx % len(PT)]
                P.op("pe", lambda e: e.matmul(
                    ps[6][64 * g:64 * g + 64, :], lhsT=v_b[:, kb, 64 * g:64 * g + 64],
                    rhs=PTt.rearrange("p r t -> p (r t)"), start=(kb == 0), stop=(kb == qt)),
                    [tPT, t_vb], [t_ps[6]])
                P.op("pe", lambda e: e.matmul(
                    ps[7][64 * g:64 * g + 64, :], lhsT=onesb[:, 0:64],
                    rhs=PTt.rearrange("p r t -> p (r t)"), start=(kb == 0), stop=(kb == qt)),
                    [tPT, t_onesb], [t_ps[7]])

            n_it = len(items)
            for idx in range(n_it + LA):
                if idx < n_it:
                    front(idx)
                if idx - LA >= 0:
                    back(idx - LA)
            P.op("act", lambda e: e.activation(out=rec.rearrange("p r t -> p (r t)"), in_=ps[7], func=AF.Ln), [t_ps[7]], [t_rec])
            P.op("act", lambda e: e.activation(out=rec.rearrange("p r t -> p (r t)"), in_=rec.rearrange("p r t -> p (r t)"),
                                               func=AF.Exp, scale=-1.0), [t_rec], [t_rec])
            P.op("dve", lambda e: e.tensor_tensor(out=o_atT[:, :, q0:q0 + 128],
                                                  in0=ps[6].rearrange("p (r t) -> p r t", r=4), in1=rec, op=ALU.mult),
                 [t_ps[6], t_rec], [t_oatT])

        def lockstep(gens):
            gens = list(gens)
            while gens:
                for g_ in list(gens):
                    try:
                        next(g_)
                    except StopIteration:
                        gens.remove(g_)

        lockstep([q_sb(0), q_sb(1)])
        for p_ in range(0, NT, 2):
            if p_ + 2 < NT:
                lockstep([q_sb(p_ + 2), q_sb(p_ + 3)])
            q_att(p_)
            q_att(p_ + 1)

    def c1_phase(sq, o_atT, t_oatT, o_gdnT, t_ogT, mkT2, t_mkT2, mv_b, t_mv):
        row_base = sq * SEQ
        alloc_stage(1024)
        Wo_a, t_Woa = sbt("Wo_a", [128, 4, 1024], BF16)
        Wo_g, t_Wog = sbt("Wo_g", [128, 4, 1024], BF16)
        Wmq, t_Wmq = sbt("Wmq", [128, 8, 256], BF16)
        Wmo, t_Wmo = sbt("Wmo", [128, 2, 1024], BF16)
        for r in range(4):
            i = k.wl % 2
            k.wl += 1
            st = stage[i][:, 0:1024]
            for g in range(2):
                P.dma("sp", st[64 * g:64 * g + 64, :], w_out[256 * g + 64 * r:256 * g + 64 * r + 64, :],
                      writes=[t_stage[i]])
            P.op("act", lambda e, st=st, r=r: e.activation(out=Wo_a[:, r, :], in_=st, func=AF.Copy),
                 [t_stage[i]], [t_Woa])
        for h in range(4):
            load_rows(Wo_g[:, h, :], t_Wog, w_out[512 + 128 * h:512 + 128 * h + 128, :], 1024, None, None)
        load_weight(Wmq, t_Wmq, w_mq, 0, 256, gX, t_gX)
        for a in range(2):
            load_rows(Wmo[:, a, :], t_Wmo, w_mo[128 * a:128 * a + 128, :], 1024, None, None)
        x1 = [sbt("x1_%d" % i, [128, D]) for i in range(2)]
        hT2, t_hT2 = sbt("hT2", [128, 8, 128], BF16)
        qmb, t_qmb = sbt("qmb", [128, 256], BF16)
        qmT2, t_qmT2 = sbt("qmT2", [128, 2, 128], BF16)
        PTm, t_PTm = sbt("PTm", [128, 2, 4, 128], BF16)
        omT2, t_omT2 = sbt("omT2", [128, 2, 128], BF16)
        recm, t_recm = sbt("recm", [128, 256])

        def c1_tile(t):
            r0 = row_base + t * 128
            c0 = t * 128
            if CCUT <= 0:
                return
            x, tx, i = load_x(xp[r0:r0 + 128, :], 128, key=("c", r0))
            if t + 1 < NT:
                prefetch_x(xp[r0 + 128:r0 + 256, :], 128, ("c", r0 + 128))
            xx, txx = x1[t % 2]
            for c in range(2):
                for r in range(4):
                    P.op("pe", lambda e, r=r, c=c: e.matmul(ps[1 + c], lhsT=o_atT[:, r, c0:c0 + 128],
                                                            rhs=Wo_a[:, r, c * 512:(c + 1) * 512], start=(r == 0), stop=False),
                         [t_oatT, t_Woa], [t_ps[1 + c]])
                for h in range(4):
                    P.op("pe", lambda e, h=h, c=c: e.matmul(ps[1 + c], lhsT=o_gdnT[:, h, c0:c0 + 128],
                                                            rhs=Wo_g[:, h, c * 512:(c + 1) * 512], start=False, stop=(h == 3)),
                         [t_ogT, t_Wog], [t_ps[1 + c]])
                P.op("dve", lambda e, c=c: e.tensor_tensor(out=xx[:, c * 512:(c + 1) * 512], in0=ps[1 + c],
                                                           in1=x[:, c * 512:(c + 1) * 512], op=ALU.add),
                     [t_ps[1 + c], tx], [txx])
            if CCUT <= 1:
                return
            norm_T(xx, txx, i, hT2, t_hT2, 0, 0)
            for kc in range(8):
                P.op("pe", lambda e, kc=kc: e.matmul(ps[3][:, 0:256], lhsT=hT2[:, kc, :], rhs=Wmq[:, kc, :],
                                                     start=(kc == 0), stop=(kc == 7)), [t_hT2, t_Wmq], [t_ps[3]])
            if CCUT <= 2:
                return
            head_rms(ps[3][:, 0:256], t_ps[3], 4, qscr, t_qscr, qss, t_qss)
            P.op("dve", lambda e: e.tensor_tensor(
                out=qmb.rearrange("p (h d) -> p h d", h=4), in0=ps[3][:, 0:256].rearrange("p (h d) -> p h d", h=4),
                in1=qss[:, 4:8].unsqueeze(2).to_broadcast([128, 4, 64]), op=ALU.mult), [t_ps[3], t_qss], [t_qmb])
            pb4 = psb16[4]
            for a in range(2):
                P.op("pe", lambda e, a=a: e.transpose(out=pb4[:, a * 128:(a + 1) * 128], in_=qmb[:, a * 128:(a + 1) * 128],
                                                      identity=identb), [t_qmb, t_identb], [t_ps[4]])
            P.op("act", lambda e: e.activation(out=qmT2.rearrange("p a t -> p (a t)"), in_=pb4[:, 0:256], func=AF.Copy),
                 [t_ps[4]], [t_qmT2])
            P.op("pool", lambda e: e.tensor_scalar(out=qmT2.rearrange("p a t -> p (a t)"), in0=qmT2.rearrange("p a t -> p (a t)"),
                                                   scalar1=gmq8[:, 0:1], scalar2=1.0, op0=ALU.mult, op1=ALU.mult),
                 [t_qmT2, t_gmq8], [t_qmT2])
            if CCUT <= 3:
                return
            for b in range(2):
                for mb in range(2):
                    for a in range(2):
                        j = mb * 2 + a
                        P.op("pe", lambda e, mb=mb, a=a, b=b, j=j: e.matmul(
                            ps[5 + b][:, j * 128:(j + 1) * 128], lhsT=mkT2[64 * b:64 * b + 64, a, mb * 128:(mb + 1) * 128],
                            rhs=qmT2[64 * b:64 * b + 64, a, :], start=True, stop=True), [t_mkT2, t_qmT2], [t_ps[5 + b]])
                P.op("act", lambda e, b=b: e.activation(out=PTm[:, b, :, :].rearrange("p j t -> p (j t)"),
                                                        in_=ps[5 + b], func=AF.Exp), [t_ps[5 + b]], [t_PTm])
            if CCUT <= 4:
                return
            for mh in range(4):
                a, b = mh // 2, mh % 2
                for mb in range(2):
                    P.op("pe", lambda e, mb=mb, mh=mh, a=a, b=b: e.matmul(
                        ps[7][64 * b:64 * b + 64, a * 128:(a + 1) * 128], lhsT=mv_b[:, mb, mh * 64:(mh + 1) * 64],
                        rhs=PTm[:, b, mb * 2 + a, :], start=(mb == 0), stop=(mb == 1)), [t_mv, t_PTm], [t_ps[7]])
                for mb in range(2):
                    P.op("pe", lambda e, mb=mb, mh=mh, a=a, b=b: e.matmul(
                        ps[7][64 * b:64 * b + 64, 256 + a * 128:256 + (a + 1) * 128], lhsT=onesb[:, 0:64],
                        rhs=PTm[:, b, mb * 2 + a, :], start=(mb == 0), stop=(mb == 1)), [t_onesb, t_PTm], [t_ps[7]])
            if CCUT <= 5:
                return
            P.op("dve", lambda e: e.reciprocal(out=recm, in_=ps[7][:, 256:512]), [t_ps[7]], [t_recm])
            P.op("dve", lambda e: e.tensor_tensor(out=omT2.rearrange("p a t -> p (a t)"), in0=ps[7][:, 0:256], in1=recm,
                                                  op=ALU.mult), [t_ps[7], t_recm], [t_omT2])
            for c in range(2):
                for a in range(2):
                    P.op("pe", lambda e, a=a, c=c: e.matmul(ps[1 + c], lhsT=omT2[:, a, :],
                                                            rhs=Wmo[:, a, c * 512:(c + 1) * 512], start=(a == 0), stop=(a == 1)),
                         [t_omT2, t_Wmo], [t_ps[1 + c]])
                P.op("dve", lambda e, c=c: e.tensor_tensor(out=xx[:, c * 512:(c + 1) * 512], in0=ps[1 + c],
                                                           in1=xx[:, c * 512:(c + 1) * 512], op=ALU.add),
                     [t_ps[1 + c], txx], [txx])
            P.dma("sp", y_p[r0:r0 + 128, :], xx, reads=[txx])

        for t in range(NT):
            c1_tile(t)

    t_yscr = Tok()

    def c2_phase(sq):
        row_base = sq * SEQ
        alloc_stage(2816)
        Wg, t_Wg = sbt("Wg", [128, 8, 2816], BF16)
        Wu, t_Wu = sbt("Wu", [128, 8, 2816], BF16)
        Wd, t_Wd = sbt("Wd", [128, 22, 1024], BF16)
        load_weight(Wg, t_Wg, w_gate, 0, 2816, gF, t_gF)
        load_weight(Wu, t_Wu, w_up, 0, 2816, gF, t_gF)
        for f in range(22):
            load_rows(Wd[:, f, :], t_Wd, w_down[128 * f:128 * f + 128, :], 1024, None, None)
        hT3, t_hT3 = sbt("hT3", [128, 8, 256], BF16)
        hfT, t_hfT = sbt("hfT", [128, 22, 256], BF16)
        sg = [sbt("sg%d" % i, [128, 256]) for i in range(2)]

        def c2_group(gi):
            xs_ = []
            for t2 in range(2):
                r0 = row_base + (gi * 2 + t2) * 128
                x, tx, i = load_x(y_p[r0:r0 + 128, :], 128)
                norm_T(x, tx, i, hT3, t_hT3, t2 * 128, 0)
                xs_.append((x, tx, r0))
            for f in range(22):
                b = 1 + (f % 2)
                for kc in range(8):
                    P.op("pe", lambda e, kc=kc, f=f, b=b: e.matmul(ps[b][:, 0:256], lhsT=Wg[:, kc, 128 * f:128 * f + 128],
                                                                   rhs=hT3[:, kc, :], start=(kc == 0), stop=(kc == 7)),
                         [t_Wg, t_hT3], [t_ps[b]])
                for kc in range(8):
                    P.op("pe", lambda e, kc=kc, f=f, b=b: e.matmul(ps[b][:, 256:512], lhsT=Wu[:, kc, 128 * f:128 * f + 128],
                                                                   rhs=hT3[:, kc, :], start=(kc == 0), stop=(kc == 7)),
                         [t_Wu, t_hT3], [t_ps[b]])
                s_, ts_ = sg[f % 2]
                P.op("act", lambda e, s_=s_, b=b: e.activation(out=s_, in_=ps[b][:, 0:256], func=AF.Silu), [t_ps[b]], [ts_])
                P.op("dve", lambda e, s_=s_, b=b, f=f: e.tensor_tensor(out=hfT[:, f, :], in0=s_, in1=ps[b][:, 256:512],
                                                                       op=ALU.mult), [ts_, t_ps[b]], [t_hfT])
            for t2 in range(2):
                x, tx, r0 = xs_[t2]
                for c in range(2):
                    for f in range(22):
                        P.op("pe", lambda e, f=f, c=c, t2=t2: e.matmul(ps[3 + c], lhsT=hfT[:, f, t2 * 128:(t2 + 1) * 128],
                                                                       rhs=Wd[:, f, c * 512:(c + 1) * 512],
                                                                       start=(f == 0), stop=(f == 21)),
                             [t_hfT, t_Wd], [t_ps[3 + c]])
                    P.op("dve", lambda e, c=c, x=x: e.tensor_tensor(out=x[:, c * 512:(c + 1) * 512], in0=ps[3 + c],
                                                                    in1=x[:, c * 512:(c + 1) * 512], op=ALU.add),
                         [t_ps[3 + c], tx], [tx])
                P.dma("sp", y_p[r0:r0 + 128, :], x, reads=[tx])

        for gi in range(NT // 2):
            c2_group(gi)

    def sample_group():
        k.top = PERSIST_TOP
        NSB = NS
        proj, t_proj = sbt("s_proj", [128, INW])
        x_keep, t_xk = sbt("s_xkeep", [128, D])
        sso, t_sso = sbt("s_o", [128, 320])
        ss_, t_ss = sbt("s_ss", [128, 64])
        scr, t_scr = sbt("s_scr", [128, 1536])
        S_TOP = k.top
        alloc_stage(INW)
        wAll, t_wAll = sbt("s_wAll", [128, 8, INW], BF16)
        load_weight(wAll, t_wAll, w_in, 0, INW, gA, t_gA)
        hTs, t_hTs = sbt("s_hT", [128, 8, 128], BF16)
        x, tx, xi = load_x(xs[:, :], NSB)
        P.op("pool", lambda e: e.tensor_copy(out=x_keep[0:NSB, :], in_=x[0:NSB, :]), [tx], [t_xk])
        norm_T(x, tx, xi, hTs, t_hTs, 0, 0)
        for c in range(7):
            c0 = c * 512
            n = min(512, INW - c0)
            b = 1 + (c % 2)
            for kc in range(8):
                P.op("pe", lambda e, kc=kc, b=b, c0=c0, n=n: e.matmul(
                    ps[b][0:NSB, 0:n], lhsT=hTs[:, kc, 0:NSB], rhs=wAll[:, kc, c0:c0 + n],
                    start=(kc == 0), stop=(kc == 7)), [t_hTs, t_wAll], [t_ps[b]])
            P.op("act", lambda e, b=b, c0=c0, n=n: e.activation(out=proj[0:NSB, c0:c0 + n], in_=ps[b][0:NSB, 0:n],
                                                                func=AF.Copy), [t_ps[b]], [t_proj])
        pj = proj[0:NSB]
        so = sso[0:NSB]
        P.op("dve", lambda e: e.tensor_tensor(out=scr[0:NSB, 0:128], in0=pj[:, 512:640], in1=pj[:, 512:640], op=ALU.mult),
             [t_proj], [t_scr])
        P.op("dve", lambda e: e.tensor_reduce(out=ss_[0:NSB, 0:2], in_=scr[0:NSB, 0:128].rearrange("p (h d) -> p h d", h=2),
                                              axis=AX.X, op=ALU.add), [t_scr], [t_ss])
        P.op("pool", lambda e: e.tensor_scalar(out=ss_[0:NSB, 2:4], in0=ss_[0:NSB, 0:2], scalar1=1.0 / 64, scalar2=EPS,
                                               op0=ALU.mult, op1=ALU.add), [t_ss], [t_ss])
        P.op("pool", lambda e: e.tensor_tensor(out=ss_[0:NSB, 2:4], in0=ss_[0:NSB, 2:4], in1=neghalf[0:NSB, 0:2], op=ALU.pow),
             [t_ss, t_nh], [t_ss])
        P.op("dve", lambda e: e.tensor_tensor(out=so[:, 0:128].rearrange("p (h d) -> p h d", h=2),
                                              in0=pj[:, 512:640].rearrange("p (h d) -> p h d", h=2),
                                              in1=ss_[0:NSB, 2:4].unsqueeze(2).to_broadcast([NSB, 2, 64]), op=ALU.mult),
             [t_proj, t_ss], [t_sso])
        P.op("pool", lambda e: e.tensor_tensor(out=so[:, 0:128].rearrange("p (h d) -> p h d", h=2),
                                               in0=so[:, 0:128].rearrange("p (h d) -> p h d", h=2),
                                               in1=gk_b[0:NSB].unsqueeze(1).to_broadcast([NSB, 2, 64]), op=ALU.mult),
             [t_sso, t_gk], [t_sso])
        P.op("act", lambda e: e.activation(out=so[:, 128:256], in_=pj[:, 640:768], func=AF.Copy), [t_proj], [t_sso])
        P.op("act", lambda e: e.activation(out=so[:, 256:320], in_=pj[:, 1024:1088], func=AF.Copy), [t_proj], [t_sso])
        P.dma("sp", k_s[:, :], so[:, 0:128], reads=[t_sso])
        P.dma("sp", v_s[:, :], so[:, 128:256], reads=[t_sso])
        P.dma("sp", kidx_s[:, :], so[:, 256:320], reads=[t_sso])
        conv_s3 = conv_s.rearrange("(s r) c -> s r c", r=3)
        st_conv3 = st_conv.rearrange("(s r) c -> s r c", r=3)
        P.dma("sp", conv_s3[:, 2, :], pj[:, 1092:2628], reads=[t_proj])
        P.dma("sp", conv_s3[:, 0:2, :], st_conv3[:, 1:3, :])
        P.barrier()
        k.top = S_TOP
        stc, t_stc = sbt("s_stc", [128, 3, 1536])
        cwb, t_cwb = sbt("s_cwb", [128, 4, 1536])
        P.dma("sp", stc[0:NSB], st_conv3, writes=[t_stc])
        P.dma("sp", cwb[0:NSB], conv_w.rearrange("t c -> (t c)").partition_broadcast(NSB), writes=[t_cwb])
        cc, t_cc = sbt("s_cc", [128, 1536])
        c_ = cc[0:NSB]
        sc_ = scr[0:NSB]
        P.op("dve", lambda e: e.tensor_tensor(out=c_, in0=pj[:, 1092:2628], in1=cwb[0:NSB, 3, :], op=ALU.mult),
             [t_proj, t_cwb], [t_cc])
        for j in range(3):
            P.op("dve", lambda e, j=j: e.tensor_tensor(out=sc_, in0=stc[0:NSB, j, :], in1=cwb[0:NSB, j, :], op=ALU.mult),
                 [t_stc, t_cwb], [t_scr])
            P.op("dve", lambda e: e.tensor_tensor(out=c_, in0=c_, in1=sc_, op=ALU.add), [t_cc, t_scr], [t_cc])
        P.op("act", lambda e: e.activation(out=c_, in_=c_, func=AF.Silu), [t_cc], [t_cc])
        P.op("dve", lambda e: e.tensor_tensor(out=sc_[:, 0:1024], in0=c_[:, 0:1024], in1=c_[:, 0:1024], op=ALU.mult),
             [t_cc], [t_scr])
        P.op("dve", lambda e: e.tensor_reduce(out=ss_[0:NSB, 8:16], in_=sc_[:, 0:1024].rearrange("p (h d) -> p h d", h=8),
                                              axis=AX.X, op=ALU.add), [t_scr], [t_ss])
        P.op("pool", lambda e: e.tensor_scalar(out=ss_[0:NSB, 16:24], in0=ss_[0:NSB, 8:16], scalar1=1.0, scalar2=EPS,
                                               op0=ALU.mult, op1=ALU.add), [t_ss], [t_ss])
        P.op("pool", lambda e: e.tensor_tensor(out=ss_[0:NSB, 16:24], in0=ss_[0:NSB, 16:24], in1=neghalf[0:NSB, 0:8], op=ALU.pow),
             [t_ss, t_nh], [t_ss])
        P.op("pool", lambda e: e.tensor_scalar(out=ss_[0:NSB, 16:20], in0=ss_[0:NSB, 16:20], scalar1=float(128.0 ** -0.5),
                                               scalar2=1.0, op0=ALU.mult, op1=ALU.mult), [t_ss], [t_ss])
        P.op("dve", lambda e: e.tensor_tensor(out=c_[:, 0:1024].rearrange("p (h d) -> p h d", h=8),
                                              in0=c_[:, 0:1024].rearrange("p (h d) -> p h d", h=8),
                                              in1=ss_[0:NSB, 16:24].unsqueeze(2).to_broadcast([NSB, 8, 128]), op=ALU.mult),
             [t_cc, t_ss], [t_cc])
        sv = ss_[0:NSB]
        P.op("dve", lambda e: e.tensor_tensor(out=sv[:, 24:28], in0=pj[:, 3140:3144], in1=dtb_b[0:NSB], op=ALU.add),
             [t_proj, t_dtb], [t_ss])
        P.op("dve", lambda e: e.tensor_scalar(out=sv[:, 28:32], in0=sv[:, 24:28], scalar1=-1.0, scalar2=None, op0=ALU.mult),
             [t_ss], [t_ss])
        P.op("dve", lambda e: e.tensor_tensor(out=sv[:, 28:32], in0=sv[:, 28:32], in1=sv[:, 24:28], op=ALU.min), [t_ss], [t_ss])
        P.op("act", lambda e: e.activation(out=sv[:, 28:32], in_=sv[:, 28:32], func=AF.Exp), [t_ss], [t_ss])
        P.op("act", lambda e: e.activation(out=sv[:, 28:32], in_=sv[:, 28:32], func=AF.Ln, bias=1.0, scale=1.0), [t_ss], [t_ss])
        P.op("dve", lambda e: e.scalar_tensor_tensor(out=sv[:, 32:36], in0=sv[:, 24:28], scalar=0.0, in1=sv[:, 28:32],
                                                     op0=ALU.max, op1=ALU.add), [t_ss], [t_ss])
        P.op("dve", lambda e: e.tensor_tensor(out=sv[:, 32:36], in0=sv[:, 32:36], in1=nea_b[0:NSB], op=ALU.mult),
             [t_ss, t_nea], [t_ss])
        P.op("act", lambda e: e.activation(out=sv[:, 36:40], in_=pj[:, 3144:3148], func=AF.Sigmoid), [t_proj], [t_ss])
        P.op("act", lambda e: e.activation(out=sv[:, 40:44], in_=sv[:, 32:36], func=AF.Exp), [t_ss], [t_ss])
        S0, t_S0 = sbt("s_S0", [128, NSB, 4, 128])
        for i in range(NSB):
            P.dma("sp", S0[:, i, :, :], ssm_in[i * 512:(i + 1) * 512, :].rearrange("(h d) v -> d h v", h=4), writes=[t_S0])
        kqT, t_kqT = sbt("s_kqT", [128, 8, NSB])
        for j in range(8):
            b = 1 + (j % 2)
            P.op("pe", lambda e, j=j, b=b: e.transpose(out=ps[b][:, 0:NSB], in_=c_[:, j * 128:(j + 1) * 128],
                                                       identity=identf[0:NSB, 0:NSB]), [t_cc, t_identf], [t_ps[b]])
            P.op("act", lambda e, j=j, b=b: e.activation(out=kqT[:, j, :], in_=ps[b][:, 0:NSB], func=AF.Copy),
                 [t_ps[b]], [t_kqT])
        eye_b, t_eyeb = sbt("s_eyeb", [128, NSB, NSB])
        P.dma("sp", eye_b, eye16_d.partition_broadcast(128), writes=[t_eyeb])
        kqTm, t_kqTm = sbt("s_kqTm", [128, 8, NSB, NSB])
        P.op("pool", lambda e: e.tensor_tensor(out=kqTm, in0=kqT.unsqueeze(2).to_broadcast([128, 8, NSB, NSB]),
                                               in1=eye_b.unsqueeze(1).to_broadcast([128, 8, NSB, NSB]), op=ALU.mult),
             [t_kqT, t_eyeb], [t_kqTm])
        for h in range(4):
            for i in range(NSB):
                P.op("pe", lambda e, h=h, i=i: e.matmul(ps[3][0:NSB, h * 128:(h + 1) * 128], lhsT=kqTm[:, 4 + h, i, :],
                                                        rhs=S0[:, i, h, :], start=(i == 0), stop=(i == NSB - 1)),
                     [t_kqTm, t_S0], [t_ps[3]])
        dl, t_dl = sbt("s_dl", [128, 4, 128])
        d_ = dl[0:NSB]
        bcs = lambda a: a.unsqueeze(2).to_broadcast([NSB, 4, 128])
        P.op("dve", lambda e: e.tensor_tensor(out=d_, in0=ps[3][0:NSB, :].rearrange("p (h v) -> p h v", h=4),
                                              in1=bcs(sv[:, 40:44]), op=ALU.mult), [t_ps[3], t_ss], [t_dl])
        P.op("dve", lambda e: e.tensor_tensor(out=d_, in0=c_[:, 1024:1536].rearrange("p (h v) -> p h v", h=4), in1=d_,
                                              op=ALU.subtract), [t_cc, t_dl], [t_dl])
        P.op("dve", lambda e: e.tensor_tensor(out=d_, in0=d_, in1=bcs(sv[:, 36:40]), op=ALU.mult), [t_dl, t_ss], [t_dl])
        ckm, t_ckm = sbt("s_ckm", [128, NSB, 512])
        P.op("pool", lambda e: e.tensor_tensor(out=ckm[0:NSB], in0=c_[:, 512:1024].unsqueeze(1).to_broadcast([NSB, NSB, 512]),
                                               in1=identf[0:NSB, 0:NSB].unsqueeze(2).to_broadcast([NSB, NSB, 512]), op=ALU.mult),
             [t_cc, t_identf], [t_ckm])
        adg, t_adg = sbt("s_adg", [128, NSB, 4])
        P.op("pool", lambda e: e.tensor_tensor(out=adg[0:NSB], in0=sv[:, 40:44].unsqueeze(1).to_broadcast([NSB, NSB, 4]),
                                               in1=identf[0:NSB, 0:NSB].unsqueeze(2).to_broadcast([NSB, NSB, 4]), op=ALU.mult),
             [t_ss, t_identf], [t_adg])
        P.op("pe", lambda e: e.matmul(ps[4][:, 0:NSB * 4], lhsT=onesf[0:NSB, :], rhs=adg[0:NSB].rearrange("p i h -> p (i h)"),
                                      start=True, stop=True), [t_adg, t_onesf], [t_ps[4]])
        abc, t_abc = sbt("s_abc", [128, NSB * 4])
        P.op("act", lambda e: e.activation(out=abc, in_=ps[4][:, 0:NSB * 4], func=AF.Copy), [t_ps[4]], [t_abc])
        for i in range(NSB):
            b = 5 + (i % 2)
            for h in range(4):
                P.op("pe", lambda e, h=h, i=i, b=b: e.matmul(ps[b][:, h * 128:(h + 1) * 128], lhsT=ckm[0:NSB, i, h * 128:(h + 1) * 128],
                                                             rhs=d_[:, h, :], start=True, stop=True), [t_ckm, t_dl], [t_ps[b]])
            for h in range(4):
                P.op("dve", lambda e, h=h, i=i, b=b: e.scalar_tensor_tensor(
                    out=S0[:, i, h, :], in0=S0[:, i, h, :], scalar=abc[:, i * 4 + h:i * 4 + h + 1],
                    in1=ps[b][:, h * 128:(h + 1) * 128], op0=ALU.mult, op1=ALU.add), [t_S0, t_abc, t_ps[b]], [t_S0])
            P.dma("sp", ssm_s[i * 512:(i + 1) * 512, :].rearrange("(h d) v -> d h v", h=4), S0[:, i, :, :], reads=[t_S0])
        for h in range(4):
            for i in range(NSB):
                P.op("pe", lambda e, h=h, i=i: e.matmul(ps[7][0:NSB, h * 128:(h + 1) * 128], lhsT=kqTm[:, h, i, :],
                                                        rhs=S0[:, i, h, :], start=(i == 0), stop=(i == NSB - 1)),
                     [t_kqTm, t_S0], [t_ps[7]])
        og, t_og = sbt("s_og", [128, 4, 128])
        o_ = og[0:NSB]
        P.op("act", lambda e: e.activation(out=sc_[:, 0:512], in_=ps[7][0:NSB, :], func=AF.Square), [t_ps[7]], [t_scr])
        P.op("dve", lambda e: e.tensor_reduce(out=sv[:, 44:48], in_=sc_[:, 0:512].rearrange("p (h v) -> p h v", h=4),
                                              axis=AX.X, op=ALU.add), [t_scr], [t_ss])
        P.op("pool", lambda e: e.tensor_scalar(out=sv[:, 48:52], in0=sv[:, 44:48], scalar1=1.0 / 128, scalar2=EPS,
                                               op0=ALU.mult, op1=ALU.add), [t_ss], [t_ss])
        P.op("pool", lambda e: e.tensor_tensor(out=sv[:, 48:52], in0=sv[:, 48:52], in1=neghalf[0:NSB, 0:4], op=ALU.pow),
             [t_ss, t_nh], [t_ss])
        P.op("dve", lambda e: e.tensor_tensor(out=o_, in0=ps[7][0:NSB, :].rearrange("p (h v) -> p h v", h=4),
                                              in1=bcs(sv[:, 48:52]), op=ALU.mult), [t_ps[7], t_ss], [t_og])
        P.op("pool", lambda e: e.tensor_tensor(out=o_, in0=o_, in1=ggdn_b[0:NSB].unsqueeze(1).to_broadcast([NSB, 4, 128]),
                                               op=ALU.mult), [t_og, t_ggdn], [t_og])
        P.op("act", lambda e: e.activation(out=sc_[:, 0:512], in_=pj[:, 2628:3140], func=AF.Silu), [t_proj], [t_scr])
        P.op("dve", lambda e: e.tensor_tensor(out=o_.rearrange("p h v -> p (h v)"), in0=o_.rearrange("p h v -> p (h v)"),
                                              in1=sc_[:, 0:512], op=ALU.mult), [t_og, t_scr], [t_og])
        P.op("pool", lambda e: e.tensor_copy(out=pj[:, 1092:1604], in_=o_.rearrange("p h v -> p (h v)")), [t_og], [t_proj])
        P.barrier()
        k.top = S_TOP
        if STOP == -1:
            return
        sample_dsa(proj, t_proj, sso, t_sso)
        sample_tail(proj, t_proj, x_keep, t_xk)

    def sample_dsa(proj, t_proj, sso, t_sso):
        NSB = NS
        pj = proj[0:NSB]
        U32 = mybir.dt.uint32
        selp, t_selp = sbt("s_selp", [128, 8, 128])
        selo, t_selo = sbt("s_selo", [128, NSB, 128])
        P.dma("sp", selp[0:NSB], selpair_d.rearrange("q i p -> i q p"), writes=[t_selp])
        P.dma("sp", selo[0:NSB], selone_d, writes=[t_selo])
        gq_b, t_gqb = bcast_layout("s_gq_b", g_q, 64)
        iota_b, t_iota = bcast_layout("s_iota", iota64_d, 64)
        pt_i, t_pti = sbt("s_pt_i", [128, 64], I32)
        pt_f, t_ptf = sbt("s_pt_f", [128, 64])
        P.dma("sp", pt_i[0:NSB], ptab, writes=[t_pti])
        P.op("dve", lambda e: e.tensor_copy(out=pt_f[0:NSB], in_=pt_i[0:NSB]), [t_pti], [t_ptf])
        ss2, t_ss2 = sbt("s_ss2", [128, 32])
        scr2, t_scr2 = sbt("s_scr2", [128, 512])
        qn, t_qn = sbt("s_qn", [128, 512])
        qiw, t_qiw = sbt("s_qiw", [128, 260])
        sv = ss2[0:NSB]
        P.op("dve", lambda e: e.tensor_tensor(out=scr2[0:NSB], in0=pj[:, 0:512], in1=pj[:, 0:512], op=ALU.mult), [t_proj], [t_scr2])
        P.op("dve", lambda e: e.tensor_reduce(out=sv[:, 0:8], in_=scr2[0:NSB].rearrange("p (h d) -> p h d", h=8), axis=AX.X,
                                              op=ALU.add), [t_scr2], [t_ss2])
        P.op("pool", lambda e: e.tensor_scalar(out=sv[:, 8:16], in0=sv[:, 0:8], scalar1=1.0 / 64, scalar2=EPS, op0=ALU.mult,
                                               op1=ALU.add), [t_ss2], [t_ss2])
        P.op("pool", lambda e: e.tensor_tensor(out=sv[:, 8:16], in0=sv[:, 8:16], in1=neghalf[0:NSB, 0:8], op=ALU.pow),
             [t_ss2, t_nh], [t_ss2])
        P.op("pool", lambda e: e.tensor_scalar(out=sv[:, 8:16], in0=sv[:, 8:16], scalar1=0.125, scalar2=1.0, op0=ALU.mult,
                                               op1=ALU.mult), [t_ss2], [t_ss2])
        q3 = qn[0:NSB].rearrange("p (h d) -> p h d", h=8)
        P.op("dve", lambda e: e.tensor_tensor(out=q3, in0=pj[:, 0:512].rearrange("p (h d) -> p h d", h=8),
                                              in1=sv[:, 8:16].unsqueeze(2).to_broadcast([NSB, 8, 64]), op=ALU.mult),
             [t_proj, t_ss2], [t_qn])
        P.op("pool", lambda e: e.tensor_tensor(out=q3, in0=q3, in1=gq_b[0:NSB].unsqueeze(1).to_broadcast([NSB, 8, 64]),
                                               op=ALU.mult), [t_qn, t_gqb], [t_qn])
        P.op("act", lambda e: e.activation(out=qiw[0:NSB, 0:256], in_=pj[:, 768:1024], func=AF.Copy, scale=0.125), [t_proj], [t_qiw])
        P.op("act", lambda e: e.activation(out=qiw[0:NSB, 256:260], in_=pj[:, 1088:1092], func=AF.Copy, scale=0.5), [t_proj], [t_qiw])
        scores, t_scores = sbt("s_scores", [128, 8200])
        P.op("dve", lambda e: e.tensor_tensor(out=scr2[0:NSB, 0:256].rearrange("p (h d) -> p h d", h=4),
                                              in0=qiw[0:NSB, 0:256].rearrange("p (h d) -> p h d", h=4),
                                              in1=pj[:, 1024:1088].unsqueeze(1).to_broadcast([NSB, 4, 64]), op=ALU.mult),
             [t_qiw, t_proj], [t_scr2])
        P.op("dve", lambda e: e.tensor_reduce(out=sv[:, 16:20], in_=scr2[0:NSB, 0:256].rearrange("p (h d) -> p h d", h=4),
                                              axis=AX.X, op=ALU.add), [t_scr2], [t_ss2])
        P.op("dve", lambda e: e.tensor_scalar(out=sv[:, 16:20], in0=sv[:, 16:20], scalar1=0.0, scalar2=None, op0=ALU.max),
             [t_ss2], [t_ss2])
        P.op("dve", lambda e: e.tensor_tensor(out=sv[:, 16:20], in0=sv[:, 16:20], in1=qiw[0:NSB, 256:260], op=ALU.mult),
             [t_ss2, t_qiw], [t_ss2])
        P.op("dve", lambda e: e.tensor_reduce(out=scores[0:NSB, 8192:8193], in_=sv[:, 16:20], axis=AX.X, op=ALU.add),
             [t_ss2], [t_scores])
        osT, t_osT = sbt("s_osT", [128, NSB, 8], BF16)
        K_TOP = k.top
        k.K_TOP = K_TOP
        kid = [sbt("s_kid%d" % i, [128, 8192]) for i in range(2)]
        prod, t_prod = sbt("s_prod", [128, 8192])
        ptc = [sbt("s_ptc%d" % i, [128, 1], I32) for i in range(2)]
        qrep, t_qrep = sbt("s_qrep", [128, 260])
        zz, t_zz = sbt("s_zz", [128, 128])
        sc1, t_sc1 = sbt("s_sc1", [128, 128])
        sc2 = [sbt("s_sc2_%d" % i, [128, 128]) for i in range(2)]
        kidx_pages = cache_kidx_d

        def pair(q):
            kd, tkd = kid[q % 2]
            pc, tpc = ptc[q % 2]
            P.dma("sp", pc, ptab[2 * q:2 * q + 2, :].rearrange("s (j o) -> (s j) o", o=1), writes=[tpc])
            P.dma("pool", kd, kidx_pages, reads=[tpc], writes=[tkd],
                  indirect=bass.IndirectOffsetOnAxis(ap=pc, axis=0))
            P.op("pe", lambda e: e.matmul(ps[1][:, 0:260], lhsT=selp[0:NSB, q, :], rhs=qiw[0:NSB, :], start=True, stop=True),
                 [t_selp, t_qiw], [t_ps[1]])
            P.op("act", lambda e: e.activation(out=qrep, in_=ps[1][:, 0:260], func=AF.Copy), [t_ps[1]], [t_qrep])
            so_, tso = sc2[q % 2]
            for h in range(4):
                P.op("pool", lambda e, h=h: e.tensor_tensor(
                    out=prod.rearrange("p (o d) -> p o d", d=64), in0=kd.rearrange("p (o d) -> p o d", d=64),
                    in1=qrep[:, h * 64:(h + 1) * 64].unsqueeze(1).to_broadcast([128, 128, 64]), op=ALU.mult),
                    [tkd, t_qrep], [t_prod])
                P.op("dve", lambda e: e.tensor_reduce(out=zz, in_=prod.rearrange("p (o d) -> p o d", d=64), axis=AX.X,
                                                      op=ALU.add), [t_prod], [t_zz])
                if h == 0:
                    P.op("dve", lambda e: e.tensor_scalar(out=so_, in0=zz, scalar1=0.0, scalar2=qrep[:, 256:257],
                                                          op0=ALU.max, op1=ALU.mult), [t_zz, t_qrep], [tso])
                else:
                    P.op("dve", lambda e, h=h: e.tensor_scalar(out=sc1, in0=zz, scalar1=0.0, scalar2=qrep[:, 256 + h:257 + h],
                                                               op0=ALU.max, op1=ALU.mult), [t_zz, t_qrep], [t_sc1])
                    P.op("dve", lambda e: e.tensor_tensor(out=so_, in0=so_, in1=sc1, op=ALU.add), [tso, t_sc1], [tso])
            for s2 in range(2):
                r = 2 * q + s2
                P.dma("sp", scores[r:r + 1, 0:8192].rearrange("p (j o) -> p j o", o=128), so_[64 * s2:64 * s2 + 64, :],
                      reads=[tso], writes=[t_scores])

        for q in range(NSB // 2):
            pair(q)
        P.barrier()
        k.top = K_TOP
        mx, t_mx = sbt("s_mx", [128, 256])
        ix, t_ix = sbt("s_ix", [128, 256], U32)
        TK_TOP = k.top
        W, t_W = sbt("s_W", [128, 8200])
        Wv = W[0:NSB, 0:8193]
        P.op("pool", lambda e: e.tensor_copy(out=Wv, in_=scores[0:NSB, 0:8193]), [t_scores], [t_W])
        for r in range(32):
            P.op("dve", lambda e, r=r: e.max(out=mx[0:NSB, 8 * r:8 * r + 8], in_=Wv), [t_W], [t_mx])
            P.op("dve", lambda e, r=r: e.max_index(out=ix[0:NSB, 8 * r:8 * r + 8], in_max=mx[0:NSB, 8 * r:8 * r + 8],
                                                   in_values=Wv), [t_W, t_mx], [t_ix])
            P.op("dve", lambda e, r=r: e.match_replace(out=Wv, in_to_replace=mx[0:NSB, 8 * r:8 * r + 8], in_values=Wv,
                                                       imm_value=-1e30), [t_W, t_mx], [t_W])
        P.barrier()
        k.top = TK_TOP
        ixf, t_ixf = sbt("s_ixf", [128, 256])
        pgu, t_pgu = sbt("s_pgu", [128, 256], U32)
        pgf, t_pgf = sbt("s_pgf", [128, 256])
        offf, t_offf = sbt("s_offf", [128, 256])
        eq, t_eq = sbt("s_eq", [128, 256, 64])
        phys, t_phys = sbt("s_phys", [128, 256])
        isf, t_isf = sbt("s_isf", [128, 256])
        n_ = lambda a: a[0:NSB]
        P.op("dve", lambda e: e.tensor_copy(out=n_(ixf), in_=n_(ix)), [t_ix], [t_ixf])
        P.op("dve", lambda e: e.tensor_scalar(out=n_(pgu), in0=n_(ix), scalar1=7, scalar2=None, op0=ALU.logical_shift_right),
             [t_ix], [t_pgu])
        P.op("dve", lambda e: e.tensor_copy(out=n_(pgf), in_=n_(pgu)), [t_pgu], [t_pgf])
        P.op("dve", lambda e: e.scalar_tensor_tensor(out=n_(offf), in0=n_(pgf), scalar=-128.0, in1=n_(ixf), op0=ALU.mult,
                                                     op1=ALU.add), [t_pgf, t_ixf], [t_offf])
        P.op("dve", lambda e: e.tensor_tensor(out=n_(eq), in0=n_(pgf).unsqueeze(2).to_broadcast([NSB, 256, 64]),
                                              in1=n_(iota_b).unsqueeze(1).to_broadcast([NSB, 256, 64]), op=ALU.is_equal),
             [t_pgf, t_iota], [t_eq])
        P.op("dve", lambda e: e.tensor_tensor(out=n_(eq), in0=n_(eq), in1=n_(pt_f).unsqueeze(1).to_broadcast([NSB, 256, 64]),
                                              op=ALU.mult), [t_eq, t_ptf], [t_eq])
        P.op("dve", lambda e: e.tensor_reduce(out=n_(phys), in_=n_(eq), axis=AX.X, op=ALU.add), [t_eq], [t_phys])
        P.op("dve", lambda e: e.scalar_tensor_tensor(out=n_(phys), in0=n_(phys), scalar=128.0, in1=n_(offf), op0=ALU.mult,
                                                     op1=ALU.add), [t_phys, t_offf], [t_phys])
        P.op("dve", lambda e: e.tensor_scalar(out=n_(isf), in0=n_(ixf), scalar1=8191.5, scalar2=None, op0=ALU.is_ge),
             [t_ixf], [t_isf])
        physT, t_physT = sbt("s_physT", [128, 2, NSB], I32)
        isT, t_isT = sbt("s_isT", [128, 2, NSB])
        for b in range(2):
            P.op("pe", lambda e, b=b: e.transpose(out=ps[1][:, b * NSB:(b + 1) * NSB], in_=phys[0:NSB, b * 128:(b + 1) * 128],
                                                  identity=identf[0:NSB, 0:NSB]), [t_phys, t_identf], [t_ps[1]])
            P.op("pe", lambda e, b=b: e.transpose(out=ps[2][:, b * NSB:(b + 1) * NSB], in_=isf[0:NSB, b * 128:(b + 1) * 128],
                                                  identity=identf[0:NSB, 0:NSB]), [t_isf, t_identf], [t_ps[2]])
        P.op("dve", lambda e: e.tensor_copy(out=physT.rearrange("p b i -> p (b i)"), in_=ps[1][:, 0:2 * NSB]), [t_ps[1]], [t_physT])
        P.op("act", lambda e: e.activation(out=isT.rearrange("p b i -> p (b i)"), in_=ps[2][:, 0:2 * NSB], func=AF.Copy),
             [t_ps[2]], [t_isT])
        Kg = [sbt("s_Kg%d" % i, [128, 2, 128]) for i in range(2)]
        Vg = [sbt("s_Vg%d" % i, [128, 2, 128]) for i in range(2)]
        kvrep, t_kvrep = sbt("s_kvrep", [128, 256])
        dif, t_dif = sbt("s_dif", [128, 128])
        prd, t_prd = sbt("s_prd", [128, 512])
        lg, t_lg = sbt("s_lg", [128, 2, 8])
        rcp, t_rcp = sbt("s_rcp", [128, NSB * 8])

        def att(i):
            kg, tkg = Kg[i % 2]
            vg, tvg = Vg[i % 2]
            for b in range(2):
                P.dma("pool", kg[:, b, :], cache_k_d, reads=[t_physT], writes=[tkg],
                      indirect=bass.IndirectOffsetOnAxis(ap=physT[:, b, i:i + 1], axis=0))
                P.dma("pool", vg[:, b, :], cache_v_d, reads=[t_physT], writes=[tvg],
                      indirect=bass.IndirectOffsetOnAxis(ap=physT[:, b, i:i + 1], axis=0))
            P.op("pe", lambda e: e.matmul(ps[3], lhsT=selo[0:NSB, i, :], rhs=qn[0:NSB, :], start=True, stop=True),
                 [t_selo, t_qn], [t_ps[3]])
            P.op("pe", lambda e: e.matmul(ps[4][:, 0:256], lhsT=selo[0:NSB, i, :], rhs=sso[0:NSB, 0:256], start=True, stop=True),
                 [t_selo, t_sso], [t_ps[4]])
            P.op("act", lambda e: e.activation(out=kvrep, in_=ps[4][:, 0:256], func=AF.Copy), [t_ps[4]], [t_kvrep])
            for b in range(2):
                for (t_, tt_, c0) in ((kg, tkg, 0), (vg, tvg, 128)):
                    P.op("dve", lambda e, t_=t_, c0=c0, b=b: e.tensor_tensor(out=dif, in0=kvrep[:, c0:c0 + 128], in1=t_[:, b, :],
                                                                             op=ALU.subtract), [t_kvrep, tt_], [t_dif])
                    P.op("dve", lambda e, t_=t_, b=b: e.scalar_tensor_tensor(out=t_[:, b, :], in0=dif, scalar=isT[:, b, i:i + 1],
                                                                             in1=t_[:, b, :], op0=ALU.mult, op1=ALU.add),
                         [t_dif, t_isT, tt_], [tt_])
                P.op("dve", lambda e, b=b: e.tensor_tensor(
                    out=prd.rearrange("p (g r d) -> p g r d", g=2, r=4),
                    in0=ps[3].rearrange("p (g r d) -> p g r d", g=2, r=4),
                    in1=kg[:, b, :].rearrange("p (g d) -> p g d", g=2).unsqueeze(2).to_broadcast([128, 2, 4, 64]),
                    op=ALU.mult), [t_ps[3], tkg], [t_prd])
                P.op("dve", lambda e, b=b: e.tensor_reduce(out=lg[:, b, :], in_=prd.rearrange("p (h d) -> p h d", h=8),
                                                           axis=AX.X, op=ALU.add), [t_prd], [t_lg])
            P.op("act", lambda e: e.activation(out=lg, in_=lg, func=AF.Exp), [t_lg], [t_lg])
            for g in range(2):
                for b in range(2):
                    P.op("pe", lambda e, g=g, b=b: e.matmul(ps[5][0:64, i * 8 + g * 4:i * 8 + g * 4 + 4],
                                                            lhsT=vg[:, b, g * 64:(g + 1) * 64], rhs=lg[:, b, g * 4:(g + 1) * 4],
                                                            start=(b == 0), stop=(b == 1)), [tvg, t_lg], [t_ps[5]])
                for b in range(2):
                    P.op("pe", lambda e, g=g, b=b: e.matmul(ps[6][0:64, i * 8 + g * 4:i * 8 + g * 4 + 4],
                                                            lhsT=onesf[:, 0:64], rhs=lg[:, b, g * 4:(g + 1) * 4],
                                                            start=(b == 0), stop=(b == 1)), [t_onesf, t_lg], [t_ps[6]])

        for i in range(NSB):
            att(i)
        P.op("dve", lambda e: e.reciprocal(out=rcp[0:64], in_=ps[6][0:64, 0:NSB * 8]), [t_ps[6]], [t_rcp])
        P.op("dve", lambda e: e.tensor_tensor(out=osT[0:64].rearrange("p i h -> p (i h)"), in0=ps[5][0:64, 0:NSB * 8],
                                              in1=rcp[0:64], op=ALU.mult), [t_ps[5], t_rcp], [t_osT])
        k.osT = (osT, t_osT)
        k.selo = (selo, t_selo)
        k.S2_TOP = k.top
        P.barrier()

    def sample_tail(proj, t_proj, x_keep, t_xk):
        NSB = NS
        pj = proj[0:NSB]
        osT, t_osT = k.osT
        selo, t_selo = k.selo
        k.top = k.K_TOP
        alloc_stage(1024)
        Wo_s, t_Wos = sbt("t_Wo_s", [128, 8, 1024], BF16)
        Wo_g, t_Wog = sbt("t_Wo_g", [128, 4, 1024], BF16)
        Wmq, t_Wmq = sbt("t_Wmq", [128, 8, 256], BF16)
        Wmo, t_Wmo = sbt("t_Wmo", [128, 4, 1024], BF16)
        for h in range(8):
            i = k.wl % 2
            k.wl += 1
            st = stage[i][0:64, 0:1024]
            P.dma("sp", st, w_out[64 * h:64 * h + 64, :], writes=[t_stage[i]])
            P.op("act", lambda e, st=st, h=h: e.activation(out=Wo_s[0:64, h, :], in_=st, func=AF.Copy), [t_stage[i]], [t_Wos])
        for h in range(4):
            load_rows(Wo_g[:, h, :], t_Wog, w_out[512 + 128 * h:512 + 128 * h + 128, :], 1024, None, None)
        load_weight(Wmq, t_Wmq, w_mq, 0, 256, gX, t_gX)
        for h in range(4):
            i = k.wl % 2
            k.wl += 1
            st = stage[i][0:64, 0:1024]
            P.dma("sp", st, w_mo[64 * h:64 * h + 64, :], writes=[t_stage[i]])
            P.op("act", lambda e, st=st, h=h: e.activation(out=Wmo[0:64, h, :], in_=st, func=AF.Copy), [t_stage[i]], [t_Wmo])
        gmq_b, t_gmqb = bcast_layout("t_gmq_b", g_mq, 64)
        ogT, t_ogT = sbt("t_ogT", [128, 4, NSB], BF16)
        for h in range(4):
            P.op("pe", lambda e, h=h: e.transpose(out=ps[1][:, h * NSB:(h + 1) * NSB], in_=pj[:, 1092 + h * 128:1092 + (h + 1) * 128],
                                                  identity=identf[0:NSB, 0:NSB]), [t_proj, t_identf], [t_ps[1]])
        P.op("act", lambda e: e.activation(out=ogT.rearrange("p h i -> p (h i)"), in_=ps[1][:, 0:4 * NSB], func=AF.Copy),
             [t_ps[1]], [t_ogT])
        x1, t_x1 = sbt("t_x1", [128, D])
        P.op("pool", lambda e: e.memset(x1, 0.0), [], [t_x1])
        for c in range(2):
            for h in range(8):
                P.op("pe", lambda e, h=h, c=c: e.matmul(ps[2 + c][0:NSB, :], lhsT=osT[0:64, :, h], rhs=Wo_s[0:64, h, c * 512:(c + 1) * 512],
                                                        start=(h == 0), stop=False), [t_osT, t_Wos], [t_ps[2 + c]])
            for h in range(4):
                P.op("pe", lambda e, h=h, c=c: e.matmul(ps[2 + c][0:NSB, :], lhsT=ogT[:, h, :], rhs=Wo_g[:, h, c * 512:(c + 1) * 512],
                                                        start=False, stop=(h == 3)), [t_ogT, t_Wog], [t_ps[2 + c]])
            P.op("dve", lambda e, c=c: e.tensor_tensor(out=x1[0:NSB, c * 512:(c + 1) * 512], in0=ps[2 + c][0:NSB, :],
                                                       in1=x_keep[0:NSB, c * 512:(c + 1) * 512], op=ALU.add),
                 [t_ps[2 + c], t_xk], [t_x1])
        hT2, t_hT2 = sbt("t_hT2", [128, 8, 128], BF16)
        norm_T(x1, t_x1, 0, hT2, t_hT2, 0, 0)
        for kc in range(8):
            P.op("pe", lambda e, kc=kc: e.matmul(ps[4][0:NSB, 0:256], lhsT=hT2[:, kc, 0:NSB], rhs=Wmq[:, kc, :],
                                                 start=(kc == 0), stop=(kc == 7)), [t_hT2, t_Wmq], [t_ps[4]])
        qm, t_qm = sbt("t_qm", [128, 256])
        sq2, t_sq2 = sbt("t_sq2", [128, 256])
        st2, t_st2 = sbt("t_st2", [128, 16])
        P.op("act", lambda e: e.activation(out=sq2[0:NSB], in_=ps[4][0:NSB, 0:256], func=AF.Square), [t_ps[4]], [t_sq2])
        P.op("dve", lambda e: e.tensor_reduce(out=st2[0:NSB, 0:4], in_=sq2[0:NSB].rearrange("p (h d) -> p h d", h=4), axis=AX.X,
                                              op=ALU.add), [t_sq2], [t_st2])
        P.op("pool", lambda e: e.tensor_scalar(out=st2[0:NSB, 4:8], in0=st2[0:NSB, 0:4], scalar1=1.0 / 64, scalar2=EPS,
                                               op0=ALU.mult, op1=ALU.add), [t_st2], [t_st2])
        P.op("pool", lambda e: e.tensor_tensor(out=st2[0:NSB, 4:8], in0=st2[0:NSB, 4:8], in1=neghalf[0:NSB, 0:4], op=ALU.pow),
             [t_st2, t_nh], [t_st2])
        P.op("pool", lambda e: e.tensor_scalar(out=st2[0:NSB, 4:8], in0=st2[0:NSB, 4:8], scalar1=0.125, scalar2=1.0,
                                               op0=ALU.mult, op1=ALU.mult), [t_st2], [t_st2])
        qm3 = qm[0:NSB].rearrange("p (h d) -> p h d", h=4)
        P.op("dve", lambda e: e.tensor_tensor(out=qm3, in0=ps[4][0:NSB, 0:256].rearrange("p (h d) -> p h d", h=4),
                                              in1=st2[0:NSB, 4:8].unsqueeze(2).to_broadcast([NSB, 4, 64]), op=ALU.mult),
             [t_ps[4], t_st2], [t_qm])
        P.op("pool", lambda e: e.tensor_tensor(out=qm3, in0=qm3, in1=gmq_b[0:NSB].unsqueeze(1).to_broadcast([NSB, 4, 64]),
                                               op=ALU.mult), [t_qm, t_gmqb], [t_qm])
        mkt = [sbt("t_mk%d" % i, [128, 2, 256]) for i in range(2)]
        mvt = [sbt("t_mv%d" % i, [128, 2, 256]) for i in range(2)]
        prm, t_prm = sbt("t_prm", [128, 256])
        lgm, t_lgm = sbt("t_lgm", [128, 2, 4])
        omT, t_omT = sbt("t_omT", [128, NSB, 4], BF16)
        rcm, t_rcm = sbt("t_rcm", [128, NSB * 4])

        def xatt(i):
            mk_, tmk = mkt[i % 2]
            mv_, tmv = mvt[i % 2]
            P.dma("sp", mk_, cmk_d[i * 256:(i + 1) * 256, :].rearrange("(t p) c -> p t c", p=128), writes=[tmk])
            P.dma("sp", mv_, cmv_d[i * 256:(i + 1) * 256, :].rearrange("(t p) c -> p t c", p=128), writes=[tmv])
            P.op("pe", lambda e: e.matmul(ps[5][:, 0:256], lhsT=selo[0:NSB, i, :], rhs=qm[0:NSB, :], start=True, stop=True),
                 [t_selo, t_qm], [t_ps[5]])
            for mt in range(2):
                P.op("dve", lambda e, mt=mt: e.tensor_tensor(out=prm, in0=ps[5][:, 0:256], in1=mk_[:, mt, :], op=ALU.mult),
                     [t_ps[5], tmk], [t_prm])
                P.op("dve", lambda e, mt=mt: e.tensor_reduce(out=lgm[:, mt, :], in_=prm.rearrange("p (h d) -> p h d", h=4),
                                                             axis=AX.X, op=ALU.add), [t_prm], [t_lgm])
            P.op("act", lambda e: e.activation(out=lgm, in_=lgm, func=AF.Exp), [t_lgm], [t_lgm])
            for h in range(4):
                for mt in range(2):
                    P.op("pe", lambda e, h=h, mt=mt: e.matmul(ps[6][0:64, i * 4 + h:i * 4 + h + 1], lhsT=mv_[:, mt, h * 64:(h + 1) * 64],
                                                              rhs=lgm[:, mt, h:h + 1], start=(mt == 0), stop=(mt == 1)),
                         [tmv, t_lgm], [t_ps[6]])
                for mt in range(2):
                    P.op("pe", lambda e, h=h, mt=mt: e.matmul(ps[7][0:64, i * 4 + h:i * 4 + h + 1], lhsT=onesf[:, 0:64],
                                                              rhs=lgm[:, mt, h:h + 1], start=(mt == 0), stop=(mt == 1)),
                         [t_onesf, t_lgm], [t_ps[7]])

        for i in range(NSB):
            xatt(i)
        P.op("dve", lambda e: e.reciprocal(out=rcm[0:64], in_=ps[7][0:64, 0:NSB * 4]), [t_ps[7]], [t_rcm])
        P.op("dve", lambda e: e.tensor_tensor(out=omT[0:64].rearrange("p i h -> p (i h)"), in0=ps[6][0:64, 0:NSB * 4],
                                              in1=rcm[0:64], op=ALU.mult), [t_ps[6], t_rcm], [t_omT])
        for c in range(2):
            for h in range(4):
                P.op("pe", lambda e, h=h, c=c: e.matmul(ps[2 + c][0:NSB, :], lhsT=omT[0:64, :, h], rhs=Wmo[0:64, h, c * 512:(c + 1) * 512],
                                                        start=(h == 0), stop=(h == 3)), [t_omT, t_Wmo], [t_ps[2 + c]])
            P.op("dve", lambda e, c=c: e.tensor_tensor(out=x1[0:NSB, c * 512:(c + 1) * 512], in0=ps[2 + c][0:NSB, :],
                                                       in1=x1[0:NSB, c * 512:(c + 1) * 512], op=ALU.add),
                 [t_ps[2 + c], t_x1], [t_x1])
        P.op("pool", lambda e: e.tensor_copy(out=x_keep[0:NSB, :], in_=x1[0:NSB, :]), [t_x1], [t_xk])
        hT3, t_hT3 = sbt("t_hT3", [128, 8, 128], BF16)
        norm_T(x1, t_x1, 1, hT3, t_hT3, 0, 0)
        P.barrier()
        k.top = PERSIST_TOP
        xk2, t_xk2 = sbt("t_xk2", [128, D])
        hT4, t_hT4 = sbt("t_hT4", [128, 8, NSB], BF16)
        P.op("pool", lambda e: e.tensor_copy(out=xk2[0:NSB, :], in_=x_keep[0:NSB, :]), [t_xk], [t_xk2])
        P.op("pool", lambda e: e.tensor_copy(out=hT4, in_=hT3[:, :, 0:NSB]), [t_hT3], [t_hT4])
        P.barrier()
        alloc_stage(2816)
        Wg, t_Wg = sbt("t_Wg", [128, 8, 2816], BF16)
        Wu, t_Wu = sbt("t_Wu", [128, 8, 2816], BF16)
        Wd, t_Wd = sbt("t_Wd", [128, 22, 1024], BF16)
        load_weight(Wg, t_Wg, w_gate, 0, 2816, gF, t_gF)
        load_weight(Wu, t_Wu, w_up, 0, 2816, gF, t_gF)
        for f in range(22):
            load_rows(Wd[:, f, :], t_Wd, w_down[128 * f:128 * f + 128, :], 1024, None, None)
        hf, t_hf = sbt("t_hf", [128, 2816])
        sgs, t_sgs = sbt("t_sgs", [128, 512])
        hfT, t_hfT = sbt("t_hfT", [128, 22, NSB], BF16)
        for c in range(6):
            c0 = c * 512
            n = min(512, 2816 - c0)
            for kc in range(8):
                P.op("pe", lambda e, kc=kc, c0=c0, n=n: e.matmul(ps[1][0:NSB, 0:n], lhsT=hT4[:, kc, :], rhs=Wg[:, kc, c0:c0 + n],
                                                                 start=(kc == 0), stop=(kc == 7)), [t_hT4, t_Wg], [t_ps[1]])
            for kc in range(8):
                P.op("pe", lambda e, kc=kc, c0=c0, n=n: e.matmul(ps[2][0:NSB, 0:n], lhsT=hT4[:, kc, :], rhs=Wu[:, kc, c0:c0 + n],
                                                                 start=(kc == 0), stop=(kc == 7)), [t_hT4, t_Wu], [t_ps[2]])
            P.op("act", lambda e, n=n: e.activation(out=sgs[0:NSB, 0:n], in_=ps[1][0:NSB, 0:n], func=AF.Silu), [t_ps[1]], [t_sgs])
            P.op("dve", lambda e, c0=c0, n=n: e.tensor_tensor(out=hf[0:NSB, c0:c0 + n], in0=sgs[0:NSB, 0:n], in1=ps[2][0:NSB, 0:n],
                                                              op=ALU.mult), [t_sgs, t_ps[2]], [t_hf])
        for f in range(22):
            b = 3 + (f % 2)
            P.op("pe", lambda e, f=f, b=b: e.transpose(out=ps[b][:, 0:NSB], in_=hf[0:NSB, f * 128:(f + 1) * 128],
                                                       identity=identf[0:NSB, 0:NSB]), [t_hf, t_identf], [t_ps[b]])
            P.op("act", lambda e, f=f, b=b: e.activation(out=hfT[:, f, :], in_=ps[b][:, 0:NSB], func=AF.Copy), [t_ps[b]], [t_hfT])
        for c in range(2):
            for f in range(22):
                P.op("pe", lambda e, f=f, c=c: e.matmul(ps[5 + c][0:NSB, :], lhsT=hfT[:, f, :], rhs=Wd[:, f, c * 512:(c + 1) * 512],
                                                        start=(f == 0), stop=(f == 21)), [t_hfT, t_Wd], [t_ps[5 + c]])
            P.op("dve", lambda e, c=c: e.tensor_tensor(out=xk2[0:NSB, c * 512:(c + 1) * 512], in0=ps[5 + c][0:NSB, :],
                                                       in1=xk2[0:NSB, c * 512:(c + 1) * 512], op=ALU.add),
                 [t_ps[5 + c], t_xk2], [t_xk2])
        P.dma("sp", y_s[:, :], xk2[0:NSB, :], reads=[t_xk2])

    if STOP >= 0:
        for sq in range(NSEQ):
            prompt_seq(sq)
    if STOP < 0 or STOP >= 99:
        sample_group()

    P.finish()
    P.emit()
    return nc


_CACHE = {}


def _get_nc(nseq, stop, npool=10240):
    key = (nseq, stop, npool)
    if key not in _CACHE:
        _CACHE[key] = build(nseq, STOP=stop, NPOOL=npool)
    return _CACHE[key]


def kernel(x_prompt, x_sample, mem_prompt, cache_k, cache_v, cache_kidx, page_table,
           state_conv, state_ssm, cache_mem_k, cache_mem_v,
           attn_norm_g, w_in, q_norm_g, k_norm_g, conv_w, a_log, dt_bias, gdn_norm_g, w_out,
           xattn_norm_g, mem_norm_g, w_mq, w_mk, w_mv, mq_norm_g, mk_norm_g, w_mo,
           ffn_norm_g, w_gate, w_up, w_down, _ncores=NCORES, _stop=99):
    B = x_prompt.shape[0]
    nseq = B // _ncores
    NS = x_sample.shape[0] // _ncores
    nc = _get_nc(nseq, _stop, cache_k.shape[1])
    f = lambda a: np.ascontiguousarray(np.asarray(a, dtype=np.float32))
    ii = np.arange(128)
    consts = {
        "ident": np.eye(128, dtype=np.float32),
        "trile": (ii[:, None] <= ii[None, :]).astype(np.float32),
        "sgt": (ii[:, None] > ii[None, :]).astype(np.float32),
        "pow2": (2.0 ** -np.arange(32)).astype(np.float32),
        "eye16": np.eye(16, dtype=np.float32).reshape(256),
        "selpair": np.stack([(np.arange(16)[:, None] == (2 * q_ + np.arange(128)[None, :] // 64)).astype(np.float32)
                             for q_ in range(8)]),
        "selone": np.stack([np.repeat((np.arange(16) == i_)[:, None], 128, axis=1).astype(np.float32)
                            for i_ in range(16)], axis=1),
        "iota64": np.arange(64, dtype=np.float32),
    }
    shared = {
        "attn_norm_g": f(attn_norm_g[0]), "w_in": f(w_in[0]),
        "q_norm_g": f(q_norm_g[0]), "k_norm_g": f(k_norm_g[0]),
        "conv_w": f(conv_w[0]), "a_log": f(a_log[0]), "dt_bias": f(dt_bias[0]),
        "gdn_norm_g": f(gdn_norm_g[0]), "w_out": f(w_out[0]), "xattn_norm_g": f(xattn_norm_g[0]),
        "mem_norm_g": f(mem_norm_g[0]), "w_mq": f(w_mq[0]), "w_mk": f(w_mk[0]), "w_mv": f(w_mv[0]),
        "mq_norm_g": f(mq_norm_g[0]), "mk_norm_g": f(mk_norm_g[0]), "w_mo": f(w_mo[0]),
        "ffn_norm_g": f(ffn_norm_g[0]), "w_gate": f(w_gate[0]), "w_up": f(w_up[0]), "w_down": f(w_down[0]),
    }
    npool = cache_k.shape[1]
    ck_k = f(cache_k[0]).reshape(npool * 128, 128)
    ck_v = f(cache_v[0]).reshape(npool * 128, 128)
    ck_idx = f(cache_kidx[0]).reshape(npool, 8192)
    in_maps = []
    for c in range(_ncores):
        m = {
            "xp": f(x_prompt[c * nseq:(c + 1) * nseq]).reshape(nseq * SEQ, D),
            "memp": f(mem_prompt[c * nseq:(c + 1) * nseq]).reshape(nseq * MEM, D),
            "xs": f(x_sample[c * NS:(c + 1) * NS]).reshape(NS, D),
            "st_conv": f(state_conv[0, c * NS:(c + 1) * NS]).reshape(NS * 3, 1536),
            "ssm_in": f(state_ssm[0, c * NS:(c + 1) * NS]).reshape(NS * 512, 128),
            "ptab": np.ascontiguousarray(np.asarray(page_table[c * NS:(c + 1) * NS], dtype=np.int32)),
            "cmk": f(cache_mem_k[0, c * NS:(c + 1) * NS]).reshape(NS * 256, 256),
            "cmv": f(cache_mem_v[0, c * NS:(c + 1) * NS]).reshape(NS * 256, 256),
            "cache_kidx": ck_idx, "cache_k": ck_k, "cache_v": ck_v,
        }
        m.update(consts)
        m.update(shared)
        in_maps.append(m)
    res = run_bass_kernel_spmd(nc, in_maps, core_ids=list(range(_ncores))).results
    cat = lambda name: np.concatenate([r[name] for r in res], axis=0)
    SB = x_sample.shape[0]
    outs = (
        cat("y_p").reshape(B, SEQ, D),
        cat("y_s").reshape(SB, 1, D),
        cat("k_p").reshape(1, B, SEQ, 2, 64),
        cat("v_p").reshape(1, B, SEQ, 2, 64),
        cat("kidx_p").reshape(1, B, SEQ, 64),
        cat("conv_p").reshape(1, B, 3, 1536),
        cat("ssm_p").reshape(1, B, 4, 128, 128),
        cat("memk_p").reshape(1, B, MEM, 4, 64),
        cat("memv_p").reshape(1, B, MEM, 4, 64),
        cat("k_s").reshape(1, SB, 1, 2, 64),
        cat("v_s").reshape(1, SB, 1, 2, 64),
        cat("kidx_s").reshape(1, SB, 1, 64),
        cat("conv_s").reshape(1, SB, 3, 1536),
        cat("ssm_s").reshape(1, SB, 4, 128, 128),
    )
    return outs
```

```python
import os
import numpy as np
import concourse.bass as bass
import concourse.mybir as mybir
from concourse.bass_utils import run_bass_kernel_spmd

F32 = mybir.dt.float32
BF16 = mybir.dt.bfloat16
I32 = mybir.dt.int32
AF = mybir.ActivationFunctionType
ALU = mybir.AluOpType
AX = mybir.AxisListType

NCORES = 8
D = 1024
SEQ = 2048
NT = SEQ // 128
MEM = 256
INW = 3148
EPS = 1e-6
NDS = 32


class Tok:
    __slots__ = ("w", "r")

    def __init__(self):
        self.w = None
        self.r = {}


class Prog:
    def __init__(self, nc):
        self.nc = nc
        self.names = ["pe", "act", "dve", "pool", "sp"]
        self.sems = []
        self.esem = {}
        for k in self.names:
            self.esem[k] = len(self.sems)
            self.sems.append(nc.alloc_semaphore("es_" + k))
        self.dsem = []
        for i in range(NDS):
            self.dsem.append(len(self.sems))
            self.sems.append(nc.alloc_semaphore("ds_%d" % i))
        self.dval = [0] * NDS
        self.dnext = 0
        self.dnext_sw = 0
        self.cnt = {k: 0 for k in self.names}
        self.seen = {k: {} for k in self.names}
        self.th = {k: [] for k in self.names}

    def _deps(self, e, reads, writes, extra=()):
        d = {}

        def add(s, v):
            if d.get(s, 0) < v:
                d[s] = v

        for t in reads:
            if t.w is not None:
                add(*t.w)
        for t in writes:
            if t.w is not None:
                add(*t.w)
            for s, v in t.r.items():
                add(s, v)
        for s, v in extra:
            add(s, v)
        out = []
        for s, v in d.items():
            if e == "pe" and s == self.esem["pe"]:
                continue
            if self.seen[e].get(s, 0) >= v:
                continue
            self.seen[e][s] = v
            out.append((s, v))
        return out

    def op(self, e, fn, reads=(), writes=()):
        waits = self._deps(e, reads, writes)
        self.cnt[e] += 1
        n = self.cnt[e]
        s = self.esem[e]
        self.th[e].append((waits, fn, s, 1))
        for t in reads:
            if t.r.get(s, 0) < n:
                t.r[s] = n
        for t in writes:
            t.w = (s, n)
            t.r = {}

    def dma(self, q, out, in_, reads=(), writes=(), **kw):
        if q == "pool":
            i = NDS - 8 + self.dnext_sw
            self.dnext_sw = (self.dnext_sw + 1) % 8
        else:
            i = self.dnext
            self.dnext = (self.dnext + 1) % (NDS - 8)
        s = self.dsem[i]
        extra = [(s, self.dval[i])] if self.dval[i] else []
        waits = self._deps(q, reads, writes, extra)
        self.dval[i] += 16
        v = self.dval[i]
        if "indirect" in kw:
            ioff = kw.pop("indirect")
            self.th[q].append((waits, lambda eng: eng.indirect_dma_start(out=out, out_offset=None, in_=in_,
                                                                         in_offset=ioff), s, 16))
        else:
            self.th[q].append((waits, lambda eng: eng.dma_start(out=out, in_=in_, **kw), s, 16))
        for t in reads:
            t.r[s] = v
        for t in writes:
            t.w = (s, v)
            t.r = {}

    def barrier(self):
        tgt = []
        for i in range(NDS):
            if self.dval[i]:
                tgt.append((self.dsem[i], self.dval[i]))
        for k in self.names:
            if self.cnt[k]:
                tgt.append((self.esem[k], self.cnt[k]))
        for e in self.names:
            waits = []
            for s_, v in tgt:
                if s_ == self.esem[e] and e in ("pe", "sp"):
                    continue
                if self.seen[e].get(s_, 0) >= v:
                    continue
                self.seen[e][s_] = v
                waits.append((s_, v))
            self.th[e].append((waits, None, None, 0))

    def finish(self):
        waits = []
        for i in range(NDS):
            if self.dval[i]:
                waits.append((self.dsem[i], self.dval[i]))
        for k in self.names:
            if k != "sp" and self.cnt[k]:
                waits.append((self.esem[k], self.cnt[k]))
        self.th["sp"].append((waits, None, None, 0))

    def emit(self):
        nc = self.nc
        sems = self.sems

        def run(eng, lst):
            for waits, fn, s, inc in lst:
                for ws, wv in waits:
                    eng.wait_ge(sems[ws], wv)
                if fn is not None:
                    fn(eng).then_inc(sems[s], inc)

        with nc.Block() as block:
            @block.tensor
            def _(e):
                run(e, self.th["pe"])

            @block.scalar
            def _(e):
                run(e, self.th["act"])

            @block.vector
            def _(e):
                run(e, self.th["dve"])

            @block.gpsimd
            def _(e):
                run(e, self.th["pool"])

            @block.sync
            def _(e):
                run(e, self.th["sp"])


class K:
    pass


def build(NSEQ, NS=16, STOP=99, NPOOL=10240):
    GCUT = int(os.environ.get('GCUT', '99'))
    DCUT = int(os.environ.get('DCUT', '99'))
    PCUT = int(os.environ.get('PCUT', '99'))
    CCUT = int(os.environ.get('CCUT', '99'))
    nc = bass.Bass("TRN2", target_bir_lowering=False)
    P = Prog(nc)
    k = K()

    def din(name, shape, dt=F32):
        return nc.dram_tensor(name, list(shape), dt, kind="ExternalInput").ap()

    def dout(name, shape, dt=F32):
        return nc.dram_tensor(name, list(shape), dt, kind="ExternalOutput").ap()

    ARENA_W = 53000
    arena = nc.alloc_sbuf_tensor("arena", [128, ARENA_W], F32).ap()
    k.top = 0

    def sb(name, shape, dt=F32):
        n = 1
        for d_ in shape[1:]:
            n *= d_
        words = n if dt in (F32, I32, mybir.dt.uint32) else (n + 1) // 2
        words = (words + 7) // 8 * 8
        off = k.top
        k.top += words
        assert k.top <= ARENA_W, ("SBUF arena overflow", name, k.top)
        a = arena[:, off:off + words]
        if dt != F32:
            a = a.bitcast(dt)
        a = a[:, 0:n]
        if len(shape) > 2:
            names = " ".join("d%d" % i for i in range(len(shape) - 1))
            kw = {"d%d" % i: shape[i + 1] for i in range(len(shape) - 2)}
            a = a.rearrange("p (%s) -> p %s" % (names, names), **kw)
        if shape[0] != 128:
            a = a[0:shape[0]]
        return a

    def sbt(name, shape, dt=F32):
        return sb(name, shape, dt), Tok()

    xp = din("xp", [NSEQ * SEQ, D])
    memp = din("memp", [NSEQ * MEM, D])
    xs = din("xs", [NS, D])
    st_conv = din("st_conv", [NS * 3, 1536])
    ssm_in = din("ssm_in", [NS * 512, 128])
    eye16_d = din("eye16", [256])
    selpair_d = din("selpair", [8, NS, 128])
    selone_d = din("selone", [NS, NS, 128])
    iota64_d = din("iota64", [64])
    ptab = din("ptab", [NS, 64], I32)
    cache_kidx_d = din("cache_kidx", [NPOOL, 8192])
    cache_k_d = din("cache_k", [NPOOL * 128, 128])
    cache_v_d = din("cache_v", [NPOOL * 128, 128])
    cmk_d = din("cmk", [NS * 256, 256])
    cmv_d = din("cmv", [NS * 256, 256])
    ident_d = din("ident", [128, 128])
    trile_d = din("trile", [128, 128])
    sgt_d = din("sgt", [128, 128])
    pow2_d = din("pow2", [32])
    g_attn = din("attn_norm_g", [D])
    w_in = din("w_in", [D, INW])
    g_q = din("q_norm_g", [64])
    g_k = din("k_norm_g", [64])
    conv_w = din("conv_w", [4, 1536])
    a_log = din("a_log", [4])
    dt_bias = din("dt_bias", [4])
    g_gdn = din("gdn_norm_g", [128])
    w_out = din("w_out", [D, D])
    g_x = din("xattn_norm_g", [D])
    g_mem = din("mem_norm_g", [D])
    w_mq = din("w_mq", [D, 256])
    w_mk = din("w_mk", [D, 256])
    w_mv = din("w_mv", [D, 256])
    g_mq = din("mq_norm_g", [64])
    g_mk = din("mk_norm_g", [64])
    w_mo = din("w_mo", [256, D])
    g_f = din("ffn_norm_g", [D])
    w_gate = din("w_gate", [D, 2816])
    w_up = din("w_up", [D, 2816])
    w_down = din("w_down", [2816, D])

    y_p = dout("y_p", [NSEQ * SEQ, D])
    y_s = dout("y_s", [NS, D])
    k_p = dout("k_p", [NSEQ * SEQ, 128])
    v_p = dout("v_p", [NSEQ * SEQ, 128])
    kidx_p = dout("kidx_p", [NSEQ * SEQ, 64])
    conv_p = dout("conv_p", [NSEQ * 3, 1536])
    ssm_p = dout("ssm_p", [NSEQ * 512, 128])
    memk_p = dout("memk_p", [NSEQ * MEM, 256])
    memv_p = dout("memv_p", [NSEQ * MEM, 256])
    k_s = dout("k_s", [NS, 128])
    v_s = dout("v_s", [NS, 128])
    kidx_s = dout("kidx_s", [NS, 64])
    conv_s = dout("conv_s", [NS * 3, 1536])
    ssm_s = dout("ssm_s", [NS * 512, 128])

    identf, t_identf = sbt("identf", [128, 128])
    identb, t_identb = sbt("identb", [128, 128], BF16)
    trile, t_trile = sbt("trile", [128, 128])
    sgt, t_sgt = sbt("sgt", [128, 128])
    onesf, t_onesf = sbt("onesf", [128, 128])
    onesb, t_onesb = sbt("onesb", [128, 128], BF16)
    tribias, t_tribias = sbt("tribias", [128, 128])
    lowinc, t_lowinc = sbt("lowinc", [128, 128])
    P.dma("sp", identf, ident_d, writes=[t_identf])
    P.dma("sp", trile, trile_d, writes=[t_trile])
    P.dma("sp", sgt, sgt_d, writes=[t_sgt])
    P.op("dve", lambda e: e.tensor_copy(out=identb, in_=identf), [t_identf], [t_identb])
    P.op("pool", lambda e: e.memset(onesf, 1.0), [], [t_onesf])
    P.op("pool", lambda e: e.memset(onesb, 1.0), [], [t_onesb])
    P.op("dve", lambda e: e.tensor_tensor(out=lowinc, in0=sgt, in1=identf, op=ALU.add),
         [t_sgt, t_identf], [t_lowinc])
    P.op("dve", lambda e: e.tensor_scalar(out=tribias, in0=lowinc, scalar1=-1.0, scalar2=1e30,
                                          op0=ALU.add, op1=ALU.mult), [t_lowinc], [t_tribias])

    def col_layout(name, src, n):
        t, tok = sbt(name, [128, n])
        P.dma("sp", t, src.rearrange("(c p) -> p c", p=128), writes=[tok],
              allow_slow_non_contiguous=True)
        return t, tok

    def bcast_layout(name, src, n):
        t, tok = sbt(name, [128, n])
        P.dma("sp", t, src.partition_broadcast(128), writes=[tok])
        return t, tok

    gA, t_gA = col_layout("gA", g_attn, 8)
    gM, t_gM = col_layout("gM", g_mem, 8)
    gX, t_gX = col_layout("gX", g_x, 8)
    gF, t_gF = col_layout("gF", g_f, 8)
    gk_b, t_gk = bcast_layout("gk_b", g_k, 64)
    gmk_b, t_gmk = bcast_layout("gmk_b", g_mk, 64)
    ggdn_b, t_ggdn = bcast_layout("ggdn_b", g_gdn, 128)
    dtb_b, t_dtb = bcast_layout("dtb_b", dt_bias, 4)
    nea_b, t_nea = bcast_layout("nea_b", a_log, 4)
    pow2_b, t_pow2 = bcast_layout("pow2_b", pow2_d, 32)
    P.op("act", lambda e: e.activation(out=nea_b, in_=nea_b, func=AF.Exp), [t_nea], [t_nea])
    P.op("dve", lambda e: e.tensor_scalar(out=nea_b, in0=nea_b, scalar1=-1.0, scalar2=None,
                                          op0=ALU.mult), [t_nea], [t_nea])
    gq8, t_gq8 = sbt("gq8", [128, 1])
    gmq8, t_gmq8 = sbt("gmq8", [128, 1])
    for (dst, tdst, src) in ((gq8, t_gq8, g_q), (gmq8, t_gmq8, g_mq)):
        for hh in range(2):
            P.dma("sp", dst[64 * hh:64 * hh + 64, :], src.rearrange("(d o) -> d o", o=1),
                  writes=[tdst], allow_slow_non_contiguous=True)
        P.op("pool", lambda e, dst=dst: e.tensor_scalar(out=dst, in0=dst, scalar1=0.125, scalar2=1.0,
                                                        op0=ALU.mult, op1=ALU.mult), [tdst], [tdst])
    cw, t_cw = sbt("cw", [128, 12, 4])
    for tap in range(4):
        P.dma("sp", cw[:, :, tap], conv_w[tap].rearrange("(j p) -> p j", p=128), writes=[t_cw],
              allow_slow_non_contiguous=True)
    neghalf, t_nh = sbt("neghalf", [128, 8])
    P.op("pool", lambda e: e.memset(neghalf, -0.5), [], [t_nh])

    ps = [nc.alloc_psum_tensor("ps%d" % i, [128, 512], F32).ap() for i in range(8)]
    t_ps = [Tok() for _ in range(8)]
    psb16 = [p_.bitcast(BF16) for p_ in ps]

    stage = [None, None]
    t_stage = [Tok(), Tok()]
    k.wl = 0

    def alloc_stage(n):
        for i in range(2):
            stage[i] = sb("wstage%d" % i, [128, n], F32)

    def load_rows(dst_kc, t_dst, src_rows, n, gain_col, t_gain):
        i = k.wl % 2
        k.wl += 1
        st = stage[i][:, 0:n]
        P.dma("sp", st, src_rows, writes=[t_stage[i]])
        if gain_col is None:
            if k.wl % 2 == 0:
                P.op("act", lambda e: e.activation(out=dst_kc, in_=st, func=AF.Copy),
                     [t_stage[i]], [t_dst])
            else:
                P.op("pool", lambda e: e.tensor_copy(out=dst_kc, in_=st), [t_stage[i]], [t_dst])
        else:
            if k.wl % 2 == 0:
                P.op("act", lambda e: e.activation(out=dst_kc, in_=st, func=AF.Copy, scale=gain_col),
                     [t_stage[i], t_gain], [t_dst])
            else:
                P.op("pool", lambda e: e.tensor_scalar(out=dst_kc, in0=st, scalar1=gain_col, scalar2=1.0,
                                                       op0=ALU.mult, op1=ALU.mult),
                     [t_stage[i], t_gain], [t_dst])

    def load_weight(dst, t_dst, src, c0, c1, gain, t_gain, d0=0):
        n = c1 - c0
        for kc in range(8):
            load_rows(dst[:, kc, d0:d0 + n], t_dst, src[kc * 128:(kc + 1) * 128, c0:c1], n,
                      None if gain is None else gain[:, kc:kc + 1], t_gain)

    xt = [sb("xt%d" % i, [128, D], F32) for i in range(2)]
    t_xt = [Tok(), Tok()]
    hb = [sb("hb%d" % i, [128, D], BF16) for i in range(2)]
    t_hb = [Tok(), Tok()]
    sq_scr, t_sq = sbt("sq_scr", [128, D], BF16)
    stat = [sb("stat%d" % i, [128, 4], F32) for i in range(2)]
    t_stat = [Tok(), Tok()]
    k.nt = 0
    for i in range(2):
        P.op("pool", lambda e, i=i: e.memset(xt[i], 0.0), [], [t_xt[i]])

    k.pref = {}

    def load_x(src_rows, nrows, key=None):
        if key is not None and key in k.pref:
            return k.pref.pop(key)
        i = k.nt % 2
        k.nt += 1
        P.dma("sp", xt[i][0:nrows, :], src_rows, writes=[t_xt[i]])
        return xt[i], t_xt[i], i

    def prefetch_x(src_rows, nrows, key):
        k.pref[key] = load_x(src_rows, nrows)

    def norm_T(x, tx, i, hT, t_hT, col, psb):
        s, ts = stat[i], t_stat[i]
        P.op("act", lambda e: e.activation(out=sq_scr, in_=x, func=AF.Square,
                                           accum_out=s[:, 0:1]), [tx], [t_sq, ts])
        P.op("pool", lambda e: e.tensor_scalar(out=s[:, 1:2], in0=s[:, 0:1], scalar1=1.0 / D,
                                               scalar2=EPS, op0=ALU.mult, op1=ALU.add), [ts], [ts])
        P.op("pool", lambda e: e.tensor_tensor(out=s[:, 2:3], in0=s[:, 1:2], in1=neghalf[:, 0:1],
                                               op=ALU.pow), [ts, t_nh], [ts])
        h, th = hb[i], t_hb[i]
        P.op("act", lambda e: e.activation(out=h, in_=x, func=AF.Copy, scale=s[:, 2:3]),
             [tx, ts], [th])
        pb = psb16[psb]
        for kc in range(8):
            P.op("pe", lambda e, kc=kc: e.transpose(out=pb[:, kc * 128:(kc + 1) * 128],
                                                    in_=h[:, kc * 128:(kc + 1) * 128],
                                                    identity=identb),
                 [th, t_identb], [t_ps[psb]])
        P.op("dve", lambda e: e.tensor_copy(out=hT[:, :, col:col + 128],
                                            in_=pb.rearrange("p (k t) -> p k t", k=8)),
             [t_ps[psb]], [t_hT])

    def norm_transpose(src_rows, nrows, hT, t_hT, col, psb, key=None):
        x, tx, i = load_x(src_rows, nrows, key)
        norm_T(x, tx, i, hT, t_hT, col, psb)
        return x, tx

    def rstd_groups(dst, t_dst, ssq, t_ssq, n, width):
        P.op("pool", lambda e: e.tensor_scalar(out=dst, in0=ssq, scalar1=1.0 / width, scalar2=EPS,
                                               op0=ALU.mult, op1=ALU.add), [t_ssq], [t_dst])
        P.op("pool", lambda e: e.tensor_tensor(out=dst, in0=dst, in1=neghalf[:, 0:n], op=ALU.pow),
             [t_dst, t_nh], [t_dst])

    def head_rms(psrc, t_psrc, nh, scr, t_scr, ss, t_ss):
        P.op("act", lambda e: e.activation(out=scr[:, 0:nh * 64], in_=psrc, func=AF.Square),
             [t_psrc], [t_scr])
        P.op("dve", lambda e: e.tensor_reduce(out=ss[:, 0:nh],
                                              in_=scr[:, 0:nh * 64].rearrange("p (h d) -> p h d", h=nh),
                                              axis=AX.X, op=ALU.add), [t_scr], [t_ss])
        rstd_groups(ss[:, nh:2 * nh], t_ss, ss[:, 0:nh], t_ss, nh, 64)

    qscr, t_qscr = sbt("qscr", [128, 512])
    qss, t_qss = sbt("qss", [128, 16])
    PERSIST_TOP = k.top

    def prompt_seq(sq):
        k.top = PERSIST_TOP
        if STOP <= 0:
            return
        row_base = sq * SEQ
        mkT2, t_mkT2 = sbt("mkT2", [128, 2, 256], BF16)
        mv_b, t_mv = sbt("mv_b", [128, 2, 256], BF16)
        o_gdnT, t_ogT = sbt("o_gdnT", [128, 4, SEQ], BF16)
        SEQ_TOP = k.top
        alloc_stage(256)
        wmkv, t_wmkv = sbt("wmkv", [128, 8, 512], BF16)
        load_weight(wmkv, t_wmkv, w_mk, 0, 256, gM, t_gM, 0)
        load_weight(wmkv, t_wmkv, w_mv, 0, 256, gM, t_gM, 256)
        hTm, t_hTm = sbt("hTm", [128, 8, 128], BF16)
        mko = [sbt("mko%d" % i, [128, 512]) for i in range(2)]
        mkb, t_mkb = sbt("mkb", [128, 256], BF16)
        for mt in range(2):
            row0 = sq * MEM + mt * 128
            norm_transpose(memp[row0:row0 + 128, :], 128, hTm, t_hTm, 0, 0)
            for kc in range(8):
                P.op("pe", lambda e, kc=kc: e.matmul(ps[1], lhsT=hTm[:, kc, :], rhs=wmkv[:, kc, :],
                                                     start=(kc == 0), stop=(kc == 7)),
                     [t_hTm, t_wmkv], [t_ps[1]])
            o, to = mko[mt]
            P.op("act", lambda e, o=o: e.activation(out=o[:, 256:512], in_=ps[1][:, 256:512], func=AF.Copy),
                 [t_ps[1]], [to])
            P.op("act", lambda e, mt=mt: e.activation(out=mv_b[:, mt, :], in_=ps[1][:, 256:512], func=AF.Copy),
                 [t_ps[1]], [t_mv])
            head_rms(ps[1][:, 0:256], t_ps[1], 4, qscr, t_qscr, qss, t_qss)
            P.op("dve", lambda e, o=o: e.tensor_tensor(
                out=o[:, 0:256].rearrange("p (h d) -> p h d", h=4),
                in0=ps[1][:, 0:256].rearrange("p (h d) -> p h d", h=4),
                in1=qss[:, 4:8].unsqueeze(2).to_broadcast([128, 4, 64]), op=ALU.mult),
                [t_ps[1], t_qss], [to])
            P.op("pool", lambda e, o=o: e.tensor_tensor(
                out=o[:, 0:256].rearrange("p (h d) -> p h d", h=4),
                in0=o[:, 0:256].rearrange("p (h d) -> p h d", h=4),
                in1=gmk_b.unsqueeze(1).to_broadcast([128, 4, 64]), op=ALU.mult),
                [to, t_gmk], [to])
            P.dma("sp", memk_p[row0:row0 + 128, :], o[:, 0:256], reads=[to])
            P.dma("sp", memv_p[row0:row0 + 128, :], o[:, 256:512], reads=[to])
            P.op("act", lambda e, o=o: e.activation(out=mkb, in_=o[:, 0:256], func=AF.Copy), [to], [t_mkb])
            pb = psb16[2]
            for a in range(2):
                P.op("pe", lambda e, a=a: e.transpose(out=pb[:, a * 128:(a + 1) * 128],
                                                      in_=mkb[:, a * 128:(a + 1) * 128], identity=identb),
                     [t_mkb, t_identb], [t_ps[2]])
            P.op("dve", lambda e, mt=mt: e.tensor_copy(
                out=mkT2[:, :, mt * 128:(mt + 1) * 128],
                in_=pb[:, 0:256].rearrange("p (a t) -> p a t", a=2)), [t_ps[2]], [t_mkT2])
        P.barrier()
        if STOP <= 1:
            return
        k.top = SEQ_TOP
        gdn_phase(sq, o_gdnT, t_ogT)
        P.barrier()
        if STOP <= 2:
            return
        k.top = SEQ_TOP
        o_atT, t_oatT = sbt("o_atT", [128, 4, SEQ], BF16)
        D_TOP = k.top
        dsa_phase(sq, o_atT, t_oatT)
        P.barrier()
        if STOP <= 3:
            return
        k.top = D_TOP
        c1_phase(sq, o_atT, t_oatT, o_gdnT, t_ogT, mkT2, t_mkT2, mv_b, t_mv)
        P.barrier()
        if STOP <= 4:
            return
        k.top = PERSIST_TOP
        c2_phase(sq)
        P.barrier()

    def gdn_phase(sq, o_gdnT, t_ogT):
        row_base = sq * SEQ
        qTg, t_qTg = sbt("qTg", [128, 4, SEQ], BF16)
        kTg, t_kTg = sbt("kTg", [128, 4, SEQ], BF16)
        k_tm, t_ktm = sbt("k_tm", [128, NT, 4, 128], BF16)
        v_tm, t_vtm = sbt("v_tm", [128, NT, 4, 128], BF16)
        sgz, t_sgz = sbt("sgz", [128, NT, 512], BF16)
        gab, t_gab = sbt("gab", [128, NT, 8])
        G_TOP = k.top
        alloc_stage(2056)
        wB, t_wB = sbt("wB", [128, 8, 2056], BF16)
        load_weight(wB, t_wB, w_in, 1092, 3148, gA, t_gA)
        hTg, t_hTg = sbt("hTg", [128, 8, 512], BF16)
        Cq, t_Cq = sbt("Cq", [128, 4, 512])
        cvT, t_cvT = sbt("cvT", [128, 4, 512], BF16)
        Uc = [sbt("Uc%d" % i, [128, 515]) for i in range(2)]
        cacc = [sbt("cacc%d" % i, [128, 512]) for i in range(2)]
        halo, t_halo = sbt("halo", [128, 12, 3])
        sqb, t_sqb = sbt("sqb", [128, 512], BF16)
        lnb, t_lnb = sbt("lnb", [128, 512])
        P.op("pool", lambda e: e.memset(halo, 0.0), [], [t_halo])
        def conv_chunk(j, grp):
            b = 3 + (j % 2)
            for kc in range(8):
                P.op("pe", lambda e, kc=kc: e.matmul(
                    ps[b], lhsT=wB[:, kc, 128 * j:128 * j + 128], rhs=hTg[:, kc, :],
                    start=(kc == 0), stop=(kc == 7)), [t_hTg, t_wB], [t_ps[b]])
            u, tu = Uc[j % 2]
            ca, tca = cacc[j % 2]
            P.op("act", lambda e: e.activation(out=u[:, 3:515], in_=ps[b], func=AF.Copy), [t_ps[b]], [tu])
            P.op("pool", lambda e: e.tensor_copy(out=u[:, 0:3], in_=halo[:, j, :]), [t_halo], [tu])
            P.op("dve", lambda e: e.tensor_scalar(out=ca, in0=u[:, 0:512], scalar1=cw[:, j, 0:1], scalar2=None,
                                                  op0=ALU.mult), [tu, t_cw], [tca])
            for tap in range(1, 4):
                P.op("dve", lambda e, tap=tap: e.scalar_tensor_tensor(
                    out=ca, in0=u[:, tap:tap + 512], scalar=cw[:, j, tap:tap + 1], in1=ca,
                    op0=ALU.mult, op1=ALU.add), [tu, t_cw, tca], [tca])
            P.op("pool", lambda e: e.tensor_copy(out=halo[:, j, :], in_=u[:, 512:515]), [tu], [t_halo])
            if j < 8:
                P.op("act", lambda e: e.activation(out=Cq[:, j % 4, :], in_=ca, func=AF.Silu), [tca], [t_Cq])
            else:
                P.op("act", lambda e: e.activation(out=cvT[:, j - 8, :], in_=ca, func=AF.Silu), [tca], [t_cvT])

        def norm_chunk(j, gc0):
            P.op("act", lambda e: e.activation(out=sqb, in_=Cq[:, j % 4, :], func=AF.Square), [t_Cq], [t_sqb])
            P.op("pe", lambda e: e.matmul(ps[5], lhsT=onesb, rhs=sqb, start=True, stop=True),
                 [t_sqb, t_onesb], [t_ps[5]])
            P.op("act", lambda e: e.activation(out=lnb, in_=ps[5], func=AF.Ln, bias=1e-6, scale=1.0),
                 [t_ps[5]], [t_lnb])
            bias = (-0.5 * float(np.log(128.0))) if j < 4 else 0.0
            P.op("act", lambda e: e.activation(out=lnb, in_=lnb, func=AF.Exp, bias=bias, scale=-0.5),
                 [t_lnb], [t_lnb])
            dstT, tdst = (qTg, t_qTg) if j < 4 else (kTg, t_kTg)
            P.op("dve", lambda e: e.tensor_tensor(out=dstT[:, j % 4, gc0:gc0 + 512], in0=Cq[:, j % 4, :], in1=lnb,
                                                  op=ALU.mult), [t_Cq, t_lnb], [tdst])

        def g_tile(grp, t4):
            ti = grp * 4 + t4
            r0 = row_base + ti * 128
            norm_transpose(xp[r0:r0 + 128, :], 128, hTg, t_hTg, t4 * 128, 0, key=("g", r0))
            if ti + 1 < NT:
                prefetch_x(xp[r0 + 128:r0 + 256, :], 128, ("g", r0 + 128))
            for kc in range(8):
                P.op("pe", lambda e, kc=kc: e.matmul(
                    ps[1], lhsT=hTg[:, kc, t4 * 128:(t4 + 1) * 128], rhs=wB[:, kc, 1536:2048],
                    start=(kc == 0), stop=(kc == 7)), [t_hTg, t_wB], [t_ps[1]])
            for kc in range(8):
                P.op("pe", lambda e, kc=kc: e.matmul(
                    ps[2][:, 0:8], lhsT=hTg[:, kc, t4 * 128:(t4 + 1) * 128], rhs=wB[:, kc, 2048:2056],
                    start=(kc == 0), stop=(kc == 7)), [t_hTg, t_wB], [t_ps[2]])
            P.op("act", lambda e: e.activation(out=sgz[:, ti, :], in_=ps[1], func=AF.Silu), [t_ps[1]], [t_sgz])
            P.op("dve", lambda e: e.tensor_copy(out=gab[:, ti, :], in_=ps[2][:, 0:8]), [t_ps[2]], [t_gab])

        def g_transposes(grp, t4):
            ti = grp * 4 + t4
            gc0 = grp * 512
            for (srcT, tsrc, c0, dst, tdst, b) in ((kTg, t_kTg, gc0 + t4 * 128, k_tm, t_ktm, 6),
                                                   (cvT, t_cvT, t4 * 128, v_tm, t_vtm, 7)):
                pb = psb16[b]
                for h in range(4):
                    P.op("pe", lambda e, h=h, srcT=srcT, c0=c0, pb=pb: e.transpose(
                        out=pb[:, h * 128:(h + 1) * 128], in_=srcT[:, h, c0:c0 + 128], identity=identb),
                        [tsrc, t_identb], [t_ps[b]])
                P.op("act", lambda e, dst=dst, pb=pb: e.activation(
                    out=dst[:, ti, :, :], in_=pb[:, 0:512].rearrange("p (h d) -> p h d", h=4), func=AF.Copy),
                    [t_ps[b]], [tdst])

        for grp in range(4):
            for t4 in range(4):
                g_tile(grp, t4)
            for j in range(0, 4):
                conv_chunk(j, grp)
            for j in range(0, 4):
                norm_chunk(j, grp * 512)
            for j in range(4, 12):
                conv_chunk(j, grp)
            for j in range(4, 8):
                norm_chunk(j, grp * 512)
            for t4 in range(4):
                g_transposes(grp, t4)
        P.barrier()
        if STOP <= 1.5:
            return
        k.top = G_TOP
        gall, t_gall = sbt("gall", [128, NT, 4])
        ball, t_ball = sbt("ball", [128, NT, 4])
        tmpa, t_tmpa = sbt("tmpa", [128, NT, 4])
        tmpb, t_tmpb = sbt("tmpb", [128, NT, 4])
        P.op("dve", lambda e: e.tensor_tensor(out=gall, in0=gab[:, :, 0:4],
                                              in1=dtb_b.unsqueeze(1).to_broadcast([128, NT, 4]), op=ALU.add),
             [t_gab, t_dtb], [t_gall])
        P.op("dve", lambda e: e.tensor_scalar(out=tmpa, in0=gall, scalar1=-1.0, scalar2=None, op0=ALU.mult),
             [t_gall], [t_tmpa])
        P.op("dve", lambda e: e.tensor_tensor(out=tmpa, in0=tmpa, in1=gall, op=ALU.min), [t_tmpa, t_gall], [t_tmpa])
        P.op("act", lambda e: e.activation(out=tmpa, in_=tmpa, func=AF.Exp), [t_tmpa], [t_tmpa])
        P.op("act", lambda e: e.activation(out=tmpa, in_=tmpa, func=AF.Ln, bias=1.0, scale=1.0), [t_tmpa], [t_tmpa])
        P.op("dve", lambda e: e.scalar_tensor_tensor(out=tmpb, in0=gall, scalar=0.0, in1=tmpa,
                                                     op0=ALU.max, op1=ALU.add), [t_gall, t_tmpa], [t_tmpb])
        P.op("dve", lambda e: e.tensor_tensor(out=gall, in0=tmpb,
                                              in1=nea_b.unsqueeze(1).to_broadcast([128, NT, 4]), op=ALU.mult),
             [t_tmpb, t_nea], [t_gall])
        P.op("act", lambda e: e.activation(out=ball, in_=gab[:, :, 4:8], func=AF.Sigmoid), [t_gab], [t_ball])

        S, t_S = sbt("S", [128, 4, 128])
        Sb, t_Sb = sbt("Sb", [128, 4, 128], BF16)
        P.op("pool", lambda e: e.memset(S, 0.0), [], [t_S])
        P.op("pool", lambda e: e.memset(Sb, 0.0), [], [t_Sb])
        NB = 2
        bufs = []
        for i in range(NB):
            bb = {}
            for nm, dt_ in (("Gh", F32), ("E", F32), ("Du", F32), ("Dl", F32), ("L0", BF16), ("L1", BF16),
                            ("M0", BF16), ("M1", BF16), ("P0", BF16), ("P1", BF16), ("kbg", BF16),
                            ("kdec", BF16), ("vb", BF16), ("u", F32), ("wT", BF16), ("qgT", BF16),
                            ("qkT", BF16), ("vnew", BF16), ("on", F32), ("og", BF16)):
                bb[nm] = sbt("%s_%d" % (nm, i), [128, 4, 128], dt_)
            bb["sc"] = sbt("gsc_%d" % i, [128, 40])
            bufs.append(bb)
        k.pbank = 0

        def bank():
            b = k.pbank
            k.pbank = (k.pbank + 1) % 8
            return b

        def gdn_tile(ti):
            B = bufs[ti % NB]
            par = ti % 2
            bstate = [0]

            def bank():
                b_ = par * 4 + bstate[0]
                bstate[0] = (bstate[0] + 1) % 4
                return b_
            c0 = ti * 128
            sc, tsc = B["sc"]
            Gh, tGh = B["Gh"]
            for h in range(4):
                P.op("pool", lambda e, h=h, Gh=Gh, ti=ti: e.tensor_scalar(
                    out=Gh[:, h, :], in0=trile, scalar1=gall[:, ti, h:h + 1], scalar2=1.0,
                    op0=ALU.mult, op1=ALU.mult), [t_trile, t_gall], [tGh])
                yield
            bE, bDu, bDl, bsm = bank(), bank(), bank(), bank()
            for h in range(4):
                P.op("pe", lambda e, h=h, Gh=Gh, bE=bE: e.matmul(ps[bE][:, h * 128:(h + 1) * 128], lhsT=onesf,
                                                                 rhs=Gh[:, h, :], start=True, stop=True),
                     [tGh, t_onesf], [t_ps[bE]])
                yield
                P.op("pe", lambda e, h=h, Gh=Gh, bDu=bDu: e.matmul(ps[bDu][:, h * 128:(h + 1) * 128], lhsT=sgt,
                                                                   rhs=Gh[:, h, :], start=True, stop=True),
                     [tGh, t_sgt], [t_ps[bDu]])
                yield
                P.op("pe", lambda e, h=h, Gh=Gh, bDl=bDl: e.matmul(ps[bDl][:, h * 128:(h + 1) * 128], lhsT=Gh[:, h, :],
                                                                   rhs=sgt, start=True, stop=True),
                     [tGh, t_sgt], [t_ps[bDl]])
                yield
            if GCUT <= 1:
                return
            P.op("pe", lambda e, ti=ti, bsm=bsm: e.matmul(ps[bsm][:, 0:4], lhsT=trile, rhs=gall[:, ti, :],
                                                          start=True, stop=True), [t_trile, t_gall], [t_ps[bsm]])
            yield
            P.op("pe", lambda e, ti=ti, bsm=bsm: e.matmul(ps[bsm][:, 8:12], lhsT=onesf, rhs=gall[:, ti, :],
                                                          start=True, stop=True), [t_onesf, t_gall], [t_ps[bsm]])
            yield
            if GCUT <= 2:
                return
            E, tE = B["E"]
            Du, tDu = B["Du"]
            Dl, tDl = B["Dl"]
            fl = lambda a: a.rearrange("p h c -> p (h c)")
            P.op("act", lambda e, E=E, bE=bE: e.activation(out=fl(E), in_=ps[bE], func=AF.Exp), [t_ps[bE]], [tE])
            yield
            P.op("act", lambda e, Du=Du, bDu=bDu: e.activation(out=fl(Du), in_=ps[bDu], func=AF.Exp), [t_ps[bDu]], [tDu])
            yield
            P.op("act", lambda e, Dl=Dl, bDl=bDl: e.activation(out=fl(Dl), in_=ps[bDl], func=AF.Exp), [t_ps[bDl]], [tDl])
            yield
            P.op("pool", lambda e, Du=Du: e.tensor_tensor(out=Du, in0=Du, in1=trile.unsqueeze(1).to_broadcast([128, 4, 128]),
                                                          op=ALU.mult), [tDu, t_trile], [tDu])
            yield
            P.op("pool", lambda e, Dl=Dl: e.tensor_tensor(out=Dl, in0=Dl, in1=sgt.unsqueeze(1).to_broadcast([128, 4, 128]),
                                                          op=ALU.mult), [tDl, t_sgt], [tDl])
            yield
            if GCUT <= 3:
                return
            P.op("dve", lambda e, sc=sc, bsm=bsm: e.tensor_copy(out=sc[:, 0:4], in_=ps[bsm][:, 0:4]), [t_ps[bsm]], [tsc])
            yield
            P.op("act", lambda e, sc=sc: e.activation(out=sc[:, 4:8], in_=sc[:, 0:4], func=AF.Exp), [tsc], [tsc])
            yield
            P.op("dve", lambda e, sc=sc, bsm=bsm: e.tensor_tensor(out=sc[:, 24:28], in0=ps[bsm][:, 8:12], in1=sc[:, 0:4],
                                                                  op=ALU.subtract), [t_ps[bsm], tsc], [tsc])
            yield
            P.op("act", lambda e, sc=sc: e.activation(out=sc[:, 8:12], in_=sc[:, 24:28], func=AF.Exp), [tsc], [tsc])
            yield
            P.op("act", lambda e, sc=sc, bsm=bsm: e.activation(out=sc[:, 12:16], in_=ps[bsm][:, 8:12], func=AF.Exp),
                 [t_ps[bsm]], [tsc])
            yield
            P.op("dve", lambda e, sc=sc, ti=ti: e.tensor_tensor(out=sc[:, 16:20], in0=sc[:, 4:8], in1=ball[:, ti, :],
                                                                op=ALU.mult), [tsc, t_ball], [tsc])
            yield
            P.op("dve", lambda e, sc=sc, ti=ti: e.tensor_scalar(out=sc[:, 20:24], in0=ball[:, ti, :], scalar1=-1.0,
                                                                scalar2=None, op0=ALU.mult), [t_ball], [tsc])
            yield
            if GCUT <= 4:
                return
            kbg, tkbg = B["kbg"]
            kdec, tkdec = B["kdec"]
            vb, tvb = B["vb"]
            bc = lambda a: a.unsqueeze(2).to_broadcast([128, 4, 128])
            P.op("pool", lambda e, kbg=kbg, sc=sc, ti=ti: e.tensor_tensor(out=kbg, in0=k_tm[:, ti, :, :], in1=bc(sc[:, 16:20]),
                                                                          op=ALU.mult), [t_ktm, tsc], [tkbg])
            yield
            P.op("pool", lambda e, kdec=kdec, sc=sc, ti=ti: e.tensor_tensor(out=kdec, in0=k_tm[:, ti, :, :], in1=bc(sc[:, 8:12]),
                                                                            op=ALU.mult), [t_ktm, tsc], [tkdec])
            yield
            P.op("pool", lambda e, vb=vb, ti=ti: e.tensor_tensor(out=vb, in0=v_tm[:, ti, :, :], in1=bc(ball[:, ti, :]),
                                                                 op=ALU.mult), [t_vtm, t_ball], [tvb])
            yield
            if GCUT <= 5:
                return
            bkk = bank()
            for h in range(4):
                P.op("pe", lambda e, h=h, bkk=bkk: e.matmul(ps[bkk][:, h * 128:(h + 1) * 128], lhsT=kTg[:, h, c0:c0 + 128],
                                                            rhs=kTg[:, h, c0:c0 + 128], start=True, stop=True),
                     [t_kTg], [t_ps[bkk]])
                yield
            Lc, tLc = B["L0"]
            Ln_, tLn = B["L1"]
            Mc, tMc = B["M0"]
            Mn, tMn = B["M1"]
            Pc, tPc = B["P0"]
            Pn, tPn = B["P1"]
            for h in range(4):
                P.op("dve", lambda e, h=h, Lc=Lc, sc=sc, Dl=Dl, bkk=bkk: e.scalar_tensor_tensor(
                    out=Lc[:, h, :], in0=ps[bkk][:, h * 128:(h + 1) * 128], scalar=sc[:, 20 + h:21 + h], in1=Dl[:, h, :],
                    op0=ALU.mult, op1=ALU.mult), [t_ps[bkk], tsc, tDl], [tLc])
                yield
            if GCUT <= 6:
                return
            bM = bank()
            pbm = psb16[bM]
            for h in range(4):
                P.op("pe", lambda e, h=h, Lc=Lc, pbm=pbm: e.transpose(out=pbm[:, h * 128:(h + 1) * 128], in_=Lc[:, h, :],
                                                                      identity=identb), [tLc, t_identb], [t_ps[bM]])
                yield
            pm3 = pbm[:, 0:512].rearrange("p (h c) -> p h c", h=4)
            if GCUT == 61:
                return
            P.op("act", lambda e, Mc=Mc, pm3=pm3: e.activation(out=Mc, in_=pm3, func=AF.Copy), [t_ps[bM]], [tMc])
            yield
            if GCUT == 62:
                return
            P.op("pool", lambda e, Pc=Pc, Mc=Mc: e.tensor_tensor(out=Pc, in0=Mc,
                                                                 in1=identb.unsqueeze(1).to_broadcast([128, 4, 128]),
                                                                 op=ALU.add), [tMc, t_identb], [tPc])
            yield
            if GCUT <= 7 or GCUT in (61, 62):
                return
            for lev in range(6):
                last = (lev == 5)
                bL = bank()
                if not last:
                    bMM = bank()
                    for h in range(4):
                        P.op("pe", lambda e, h=h, Lc=Lc, Mc=Mc, bMM=bMM: e.matmul(
                            ps[bMM][:, h * 128:(h + 1) * 128], lhsT=Lc[:, h, :], rhs=Mc[:, h, :], start=True, stop=True),
                            [tLc, tMc], [t_ps[bMM]])
                        yield
                for h in range(4):
                    P.op("pe", lambda e, h=h, Lc=Lc, Mc=Mc, bL=bL: e.matmul(
                        ps[bL][:, h * 128:(h + 1) * 128], lhsT=Mc[:, h, :], rhs=Lc[:, h, :], start=True, stop=True),
                        [tLc, tMc], [t_ps[bL]])
                    yield
                P.op("dve", lambda e, Ln_=Ln_, bL=bL: e.tensor_copy(out=fl(Ln_), in_=ps[bL]), [t_ps[bL]], [tLn])
                yield
                if not last:
                    P.op("act", lambda e, Mn=Mn, bMM=bMM: e.activation(out=fl(Mn), in_=ps[bMM], func=AF.Copy),
                         [t_ps[bMM]], [tMn])
                    yield
                bP = bank()
                for h in range(4):
                    P.op("pe", lambda e, h=h, Ln_=Ln_, Pc=Pc, bP=bP: e.matmul(
                        ps[bP][:, h * 128:(h + 1) * 128], lhsT=Ln_[:, h, :], rhs=Pc[:, h, :], start=True, stop=True),
                        [tLn, tPc], [t_ps[bP]])
                    yield
                P.op("dve", lambda e, Pn=Pn, Pc=Pc, bP=bP: e.tensor_tensor(out=fl(Pn), in0=ps[bP], in1=fl(Pc), op=ALU.add),
                     [t_ps[bP], tPc], [tPn])
                yield
                Lc, tLc, Ln_, tLn = Ln_, tLn, Lc, tLc
                Mc, tMc, Mn, tMn = Mn, tMn, Mc, tMc
                Pc, tPc, Pn, tPn = Pn, tPn, Pc, tPc
            if GCUT <= 8:
                return
            bu, bw, bq = bank(), bank(), bank()
            for h in range(4):
                P.op("pe", lambda e, h=h, Pc=Pc, vb=vb, bu=bu: e.matmul(ps[bu][:, h * 128:(h + 1) * 128], lhsT=Pc[:, h, :],
                                                                        rhs=vb[:, h, :], start=True, stop=True),
                     [tPc, tvb], [t_ps[bu]])
                yield
                P.op("pe", lambda e, h=h, Pc=Pc, kbg=kbg, bw=bw: e.matmul(ps[bw][:, h * 128:(h + 1) * 128], lhsT=kbg[:, h, :],
                                                                          rhs=Pc[:, h, :], start=True, stop=True),
                     [tPc, tkbg], [t_ps[bw]])
                yield
                P.op("pe", lambda e, h=h, bq=bq: e.matmul(ps[bq][:, h * 128:(h + 1) * 128], lhsT=kTg[:, h, c0:c0 + 128],
                                                          rhs=qTg[:, h, c0:c0 + 128], start=True, stop=True),
                     [t_kTg, t_qTg], [t_ps[bq]])
                yield
            u, tu_ = B["u"]
            wT, twT = B["wT"]
            qgT, tqgT = B["qgT"]
            qkT, tqkT = B["qkT"]
            P.op("act", lambda e, u=u, bu=bu: e.activation(out=fl(u), in_=ps[bu], func=AF.Copy), [t_ps[bu]], [tu_])
            yield
            P.op("act", lambda e, wT=wT, bw=bw: e.activation(out=fl(wT), in_=ps[bw], func=AF.Copy), [t_ps[bw]], [twT])
            yield
            P.op("dve", lambda e, qkT=qkT, Du=Du, bq=bq: e.tensor_tensor(out=fl(qkT), in0=ps[bq], in1=fl(Du), op=ALU.mult),
                 [t_ps[bq], tDu], [tqkT])
            yield
            P.op("pool", lambda e, qgT=qgT, E=E: e.tensor_tensor(out=qgT, in0=qTg[:, :, c0:c0 + 128], in1=E, op=ALU.mult),
                 [t_qTg, tE], [tqgT])
            yield
            if GCUT <= 9:
                return
            yield 'SEQ'
            bws, bo, bs = bank(), bank(), bank()
            for h in range(4):
                P.op("pe", lambda e, h=h, wT=wT, bws=bws: e.matmul(ps[bws][:, h * 128:(h + 1) * 128], lhsT=wT[:, h, :],
                                                                   rhs=Sb[:, h, :], start=True, stop=True),
                     [twT, t_Sb], [t_ps[bws]])
            vnew, tvn = B["vnew"]
            P.op("dve", lambda e, vnew=vnew, u=u, bws=bws: e.tensor_tensor(out=fl(vnew), in0=fl(u), in1=ps[bws],
                                                                           op=ALU.subtract), [tu_, t_ps[bws]], [tvn])
            for h in range(4):
                P.op("pe", lambda e, h=h, qgT=qgT, bo=bo: e.matmul(ps[bo][:, h * 128:(h + 1) * 128], lhsT=qgT[:, h, :],
                                                                   rhs=Sb[:, h, :], start=True, stop=False),
                     [tqgT, t_Sb], [t_ps[bo]])
                P.op("pe", lambda e, h=h, qkT=qkT, vnew=vnew, bo=bo: e.matmul(ps[bo][:, h * 128:(h + 1) * 128], lhsT=qkT[:, h, :],
                                                                              rhs=vnew[:, h, :], start=False, stop=True),
                     [tqkT, tvn], [t_ps[bo]])
            for h in range(4):
                P.op("pe", lambda e, h=h, kdec=kdec, vnew=vnew, bs=bs: e.matmul(ps[bs][:, h * 128:(h + 1) * 128], lhsT=kdec[:, h, :],
                                                                                rhs=vnew[:, h, :], start=True, stop=True),
                     [tkdec, tvn], [t_ps[bs]])
            for h in range(4):
                P.op("dve", lambda e, h=h, sc=sc, bs=bs: e.scalar_tensor_tensor(
                    out=S[:, h, :], in0=S[:, h, :], scalar=sc[:, 12 + h:13 + h], in1=ps[bs][:, h * 128:(h + 1) * 128],
                    op0=ALU.mult, op1=ALU.add), [t_S, tsc, t_ps[bs]], [t_S])
            P.op("act", lambda e: e.activation(out=Sb, in_=S, func=AF.Copy), [t_S], [t_Sb])
            if GCUT <= 10:
                return
            on, ton = B["on"]
            og, tog = B["og"]
            P.op("act", lambda e, on=on, bo=bo: e.activation(out=fl(on), in_=ps[bo], func=AF.Square), [t_ps[bo]], [ton])
            P.op("dve", lambda e, on=on, sc=sc: e.tensor_reduce(out=sc[:, 28:32], in_=on, axis=AX.X, op=ALU.add),
                 [ton], [tsc])
            P.op("pool", lambda e, sc=sc: e.tensor_scalar(out=sc[:, 32:36], in0=sc[:, 28:32], scalar1=1.0 / 128, scalar2=EPS,
                                                          op0=ALU.mult, op1=ALU.add), [tsc], [tsc])
            P.op("pool", lambda e, sc=sc: e.tensor_tensor(out=sc[:, 32:36], in0=sc[:, 32:36], in1=neghalf[:, 0:4], op=ALU.pow),
                 [tsc, t_nh], [tsc])
            P.op("dve", lambda e, on=on, sc=sc, bo=bo: e.tensor_tensor(
                out=on, in0=ps[bo].rearrange("p (h c) -> p h c", h=4), in1=bc(sc[:, 32:36]), op=ALU.mult),
                [t_ps[bo], tsc], [ton])
            P.op("pool", lambda e, on=on: e.tensor_tensor(out=on, in0=on, in1=ggdn_b.unsqueeze(1).to_broadcast([128, 4, 128]),
                                                          op=ALU.mult), [ton, t_ggdn], [ton])
            P.op("pool", lambda e, on=on, og=og, ti=ti: e.tensor_tensor(
                out=og, in0=on, in1=sgz[:, ti, :].rearrange("p (h c) -> p h c", h=4), op=ALU.mult),
                [ton, t_sgz], [tog])
            bt = bank()
            pbt = psb16[bt]
            for h in range(4):
                P.op("pe", lambda e, h=h, og=og, pbt=pbt: e.transpose(out=pbt[:, h * 128:(h + 1) * 128], in_=og[:, h, :],
                                                                      identity=identb), [tog, t_identb], [t_ps[bt]])
            P.op("act", lambda e, pbt=pbt: e.activation(out=o_gdnT[:, :, c0:c0 + 128],
                                                        in_=pbt[:, 0:512].rearrange("p (h c) -> p h c", h=4), func=AF.Copy),
                 [t_ps[bt]], [t_ogT])
        def run_to_seq(gens):
            live = list(gens)
            while live:
                for g_ in list(live):
                    try:
                        if next(g_) == 'SEQ':
                            live.remove(g_)
                    except StopIteration:
                        live.remove(g_)

        def finish_gen(g_):
            for _ in g_:
                pass

        for t2 in range(0, NT if STOP > 1.8 else 2, 2):
            ga_, gb_ = gdn_tile(t2), gdn_tile(t2 + 1)
            run_to_seq([ga_, gb_])
            finish_gen(ga_)
            finish_gen(gb_)
        P.dma("sp", ssm_p[sq * 512:(sq + 1) * 512, :].rearrange("(h d) v -> d h v", h=4), S, reads=[t_S])

    NIT = int(os.environ.get("NIT", "18"))

    def dsa_phase(sq, o_atT, t_oatT):
        row_base = sq * SEQ
        qT2, t_qT2 = sbt("qT2", [128, NT, 512], BF16)
        kT2, t_kT2 = sbt("kT2", [128, SEQ], BF16)
        v_b, t_vb = sbt("v_b", [128, NT, 128], BF16)
        qiT2, t_qiT2 = sbt("qiT2", [128, 2, SEQ], BF16)
        kiT2, t_kiT2 = sbt("kiT2", [128, SEQ], BF16)
        wi_s, t_wi = sbt("wi_s", [128, NT, 4])
        A_TOP = k.top
        alloc_stage(1536)
        wA, t_wA = sbt("wA", [128, 8, 1092], BF16)
        load_weight(wA, t_wA, w_in, 0, 1092, gA, t_gA)
        wConv, t_wConv = sbt("wConv", [128, 8, 1536], BF16)
        load_weight(wConv, t_wConv, w_in, 1092, 2628, gA, t_gA)
        hT1, t_hT1 = sbt("hT1", [128, 8, 128], BF16)
        ko = [sbt("ko%d" % i, [128, 320]) for i in range(2)]
        cvo, t_cvo = sbt("cvo", [128, 1536])
        qnb, t_qnb = sbt("qnb", [128, 512], BF16)
        kb_, t_kb = sbt("kb_", [128, 128], BF16)
        qib, t_qib = sbt("qib", [128, 256], BF16)
        kib, t_kib = sbt("kib", [128, 128], BF16)

        def proj_tile(t):
            r0 = row_base + t * 128
            c0 = t * 128
            norm_transpose(xp[r0:r0 + 128, :], 128, hT1, t_hT1, 0, 0, key=("d", r0))
            if t + 1 < NT:
                prefetch_x(xp[r0 + 128:r0 + 256, :], 128, ("d", r0 + 128))
            for (b, a0, a1) in ((1, 0, 512), (2, 512, 1024), (3, 1024, 1092)):
                for kc in range(8):
                    P.op("pe", lambda e, kc=kc, b=b, a0=a0, a1=a1: e.matmul(
                        ps[b][:, 0:a1 - a0], lhsT=hT1[:, kc, :], rhs=wA[:, kc, a0:a1],
                        start=(kc == 0), stop=(kc == 7)), [t_hT1, t_wA], [t_ps[b]])
            o, to = ko[t % 2]
            if PCUT <= 1:
                return
            head_rms(ps[1], t_ps[1], 8, qscr, t_qscr, qss, t_qss)
            P.op("dve", lambda e: e.tensor_tensor(
                out=qnb.rearrange("p (r g d) -> p g r d", r=4, g=2),
                in0=ps[1].rearrange("p (g r d) -> p g r d", g=2, r=4),
                in1=qss[:, 8:16].rearrange("p (g r) -> p g r", g=2).unsqueeze(3).to_broadcast([128, 2, 4, 64]),
                op=ALU.mult), [t_ps[1], t_qss], [t_qnb])
            pbq = psb16[4]
            for r in range(4):
                P.op("pe", lambda e, r=r: e.transpose(out=pbq[:, r * 128:(r + 1) * 128], in_=qnb[:, r * 128:(r + 1) * 128],
                                                      identity=identb), [t_qnb, t_identb], [t_ps[4]])
            P.op("act", lambda e: e.activation(out=qT2[:, t, :], in_=pbq[:, 0:512], func=AF.Copy),
                 [t_ps[4]], [t_qT2])
            P.op("pool", lambda e: e.tensor_scalar(out=qT2[:, t, :], in0=qT2[:, t, :], scalar1=gq8[:, 0:1], scalar2=1.0,
                                                   op0=ALU.mult, op1=ALU.mult), [t_qT2, t_gq8], [t_qT2])
            if PCUT <= 2:
                return
            head_rms(ps[2][:, 0:128], t_ps[2], 2, qscr, t_qscr, qss, t_qss)
            P.op("dve", lambda e: e.tensor_tensor(
                out=o[:, 0:128].rearrange("p (h d) -> p h d", h=2),
                in0=ps[2][:, 0:128].rearrange("p (h d) -> p h d", h=2),
                in1=qss[:, 2:4].unsqueeze(2).to_broadcast([128, 2, 64]), op=ALU.mult),
                [t_ps[2], t_qss], [to])
            P.op("pool", lambda e: e.tensor_tensor(
                out=o[:, 0:128].rearrange("p (h d) -> p h d", h=2),
                in0=o[:, 0:128].rearrange("p (h d) -> p h d", h=2),
                in1=gk_b.unsqueeze(1).to_broadcast([128, 2, 64]), op=ALU.mult), [to, t_gk], [to])
            P.op("act", lambda e: e.activation(out=kb_, in_=o[:, 0:128], func=AF.Copy), [to], [t_kb])
            pb5 = psb16[5]
            P.op("pe", lambda e: e.transpose(out=pb5[:, 0:128], in_=kb_, identity=identb), [t_kb, t_identb], [t_ps[5]])
            if PCUT <= 3:
                return
            P.op("act", lambda e: e.activation(out=o[:, 128:256], in_=ps[2][:, 128:256], func=AF.Copy), [t_ps[2]], [to])
            P.op("act", lambda e: e.activation(out=v_b[:, t, :], in_=ps[2][:, 128:256], func=AF.Copy), [t_ps[2]], [t_vb])
            P.op("act", lambda e: e.activation(out=qib, in_=ps[2][:, 256:512], func=AF.Copy, scale=0.125),
                 [t_ps[2]], [t_qib])
            for a in range(2):
                P.op("pe", lambda e, a=a: e.transpose(out=pb5[:, 128 + a * 128:256 + a * 128],
                                                      in_=qib[:, a * 128:(a + 1) * 128], identity=identb),
                     [t_qib, t_identb], [t_ps[5]])
            if PCUT <= 4:
                return
            P.op("act", lambda e: e.activation(out=o[:, 256:320], in_=ps[3][:, 0:64], func=AF.Copy), [t_ps[3]], [to])
            P.op("act", lambda e: e.activation(out=kib[:, 0:64], in_=ps[3][:, 0:64], func=AF.Copy), [t_ps[3]], [t_kib])
            P.op("act", lambda e: e.activation(out=kib[:, 64:128], in_=ps[3][:, 0:64], func=AF.Copy), [t_ps[3]], [t_kib])
            P.op("pe", lambda e: e.transpose(out=pb5[:, 384:512], in_=kib, identity=identb), [t_kib, t_identb], [t_ps[5]])
            P.op("act", lambda e: e.activation(out=wi_s[:, t, :], in_=ps[3][:, 64:68], func=AF.Copy, scale=0.5),
                 [t_ps[3]], [t_wi])
            if PCUT <= 5:
                return
            P.op("act", lambda e: e.activation(out=kT2[:, c0:c0 + 128], in_=pb5[:, 0:128], func=AF.Copy), [t_ps[5]], [t_kT2])
            if PCUT == 51:
                return
            P.op("act", lambda e: e.activation(out=qiT2[:, :, c0:c0 + 128],
                                               in_=pb5[:, 128:384].rearrange("p (a t) -> p a t", a=2), func=AF.Copy),
                 [t_ps[5]], [t_qiT2])
            if PCUT == 52:
                return
            P.op("act", lambda e: e.activation(out=kiT2[:, c0:c0 + 128], in_=pb5[:, 384:512], func=AF.Copy),
                 [t_ps[5]], [t_kiT2])
            if PCUT == 53:
                return
            P.dma("sp", k_p[r0:r0 + 128, :], o[:, 0:128], reads=[to])
            P.dma("sp", v_p[r0:r0 + 128, :], o[:, 128:256], reads=[to])
            P.dma("sp", kidx_p[r0:r0 + 128, :], o[:, 256:320], reads=[to])
            if PCUT <= 6:
                return
            if t == NT - 1:
                for b in range(3):
                    for kc in range(8):
                        P.op("pe", lambda e, kc=kc, b=b: e.matmul(
                            ps[5 + b] if b < 2 else ps[0], lhsT=hT1[:, kc, :], rhs=wConv[:, kc, b * 512:(b + 1) * 512],
                            start=(kc == 0), stop=(kc == 7)), [t_hT1, t_wConv], [t_ps[5 + b] if b < 2 else t_ps[0]])
                for b in range(3):
                    bb = 5 + b if b < 2 else 0
                    P.op("act", lambda e, b=b, bb=bb: e.activation(out=cvo[:, b * 512:(b + 1) * 512], in_=ps[bb],
                                                                   func=AF.Copy), [t_ps[bb]], [t_cvo])
                P.dma("sp", conv_p[sq * 3:(sq + 1) * 3, :], cvo[125:128, :], reads=[t_cvo])

        for t in range(NT):
            proj_tile(t)
        P.barrier()
        if DCUT <= 1:
            return
        k.top = A_TOP
        scb = [sbt("scb%d" % i, [128, SEQ]) for i in range(2)]
        rl = [sbt("rl%d" % i, [128, 512]) for i in range(4)]
        junk2 = [sbt("junk%d" % i, [128, SEQ], BF16) for i in range(2)]
        mask2 = [sbt("mask%d" % i, [128, SEQ], BF16) for i in range(2)]
        maskT2 = [sbt("maskT%d" % i, [128, NT, 128], BF16) for i in range(4)]
        PT = [sbt("PT%d" % i, [128, 4, 128], BF16) for i in range(8)]
        bis2 = [sbt("bis%d" % i, [128, 64]) for i in range(2)]
        rec, t_rec = sbt("rec", [128, 4, 128])

        def q_sb(qt):
            L = (qt + 1) * 128
            q0 = qt * 128
            S_, tS = scb[qt % 2]
            maskT, t_maskT = maskT2[qt % 4]
            bis, t_bis = bis2[qt % 2]
            junk, t_junk = junk2[qt % 2]
            mask, t_mask = mask2[qt % 2]
            nch = (L + 511) // 512
            cnt_ = 0
            for ch in range(nch):
                k0 = ch * 512
                n = min(512, L - k0)
                for ih in range(4):
                    a, b = ih // 2, ih % 2
                    bnk = qt % 2
                    r_, tr = rl[(qt % 2) * 2 + cnt_ % 2]
                    cnt_ += 1
                    P.op("pe", lambda e, a=a, b=b, bnk=bnk, k0=k0, n=n: e.matmul(
                        ps[bnk][:, 0:n], lhsT=qiT2[64 * b:64 * b + 64, a, q0:q0 + 128],
                        rhs=kiT2[64 * b:64 * b + 64, k0:k0 + n], start=True, stop=True),
                        [t_qiT2, t_kiT2], [t_ps[bnk]])
                    yield
                    P.op("act", lambda e, r_=r_, bnk=bnk, n=n: e.activation(out=r_[:, 0:n], in_=ps[bnk][:, 0:n], func=AF.Relu),
                         [t_ps[bnk]], [tr])
                    yield
                    if ih == 0:
                        P.op("dve", lambda e, r_=r_, k0=k0, n=n: e.tensor_scalar(
                            out=S_[:, k0:k0 + n], in0=r_[:, 0:n], scalar1=wi_s[:, qt, 0:1], scalar2=None, op0=ALU.mult),
                            [tr, t_wi], [tS])
                        yield
                    else:
                        P.op("dve", lambda e, r_=r_, k0=k0, n=n, ih=ih: e.scalar_tensor_tensor(
                            out=S_[:, k0:k0 + n], in0=r_[:, 0:n], scalar=wi_s[:, qt, ih:ih + 1], in1=S_[:, k0:k0 + n],
                            op0=ALU.mult, op1=ALU.add), [tr, t_wi, tS], [tS])
                        yield
            if DCUT <= 2:
                return
            dg = S_[:, q0:q0 + 128]
            P.op("dve", lambda e: e.tensor_tensor(out=dg, in0=dg, in1=lowinc, op=ALU.mult), [tS, t_lowinc], [tS])
            yield
            P.op("dve", lambda e: e.tensor_reduce(out=bis[:, 0:1], in_=S_[:, 0:L], axis=AX.X, op=ALU.max), [tS], [t_bis])
            yield
            P.op("dve", lambda e: e.tensor_reduce(out=bis[:, 1:2], in_=S_[:, 0:L], axis=AX.X, op=ALU.min), [tS], [t_bis])
            yield
            P.op("pool", lambda e: e.tensor_tensor(out=dg, in0=dg, in1=tribias, op=ALU.add), [tS, t_tribias], [tS])
            yield
            P.op("dve", lambda e: e.tensor_tensor(out=bis[:, 2:3], in0=bis[:, 0:1], in1=bis[:, 1:2], op=ALU.subtract),
                 [t_bis], [t_bis])
            yield
            P.op("dve", lambda e: e.tensor_scalar(out=bis[:, 3:4], in0=bis[:, 2:3], scalar1=0.5005, scalar2=0.0005,
                                                  op0=ALU.mult, op1=ALU.add), [t_bis], [t_bis])
            yield
            P.op("dve", lambda e: e.tensor_tensor(out=bis[:, 4:5], in0=bis[:, 0:1], in1=bis[:, 3:4], op=ALU.subtract),
                 [t_bis], [t_bis])
            yield
            P.op("dve", lambda e: e.tensor_scalar(out=bis[:, 8:9 + NIT], in0=pow2_b[:, 0:NIT + 1], scalar1=bis[:, 3:4],
                                                  scalar2=None, op0=ALU.mult), [t_bis, t_pow2], [t_bis])
            yield
            if (qt % 2 == 1 or os.environ.get("ACTALL") == "1") and os.environ.get("ACTCNT", "1") == "1":
                P.op("dve", lambda e: e.tensor_scalar(out=bis[:, 40:41 + NIT], in0=bis[:, 8:9 + NIT], scalar1=-1.0, scalar2=None,
                                                      op0=ALU.mult), [t_bis], [t_bis])
                yield
                P.op("dve", lambda e: e.tensor_scalar(out=bis[:, 30:31], in0=bis[:, 4:5], scalar1=-1.0, scalar2=None,
                                                      op0=ALU.mult), [t_bis], [t_bis])
                yield
                for it in range(NIT):
                    P.op("act", lambda e: e.activation(out=junk[:, 0:L], in_=S_[:, 0:L], func=AF.Sign, bias=bis[:, 30:31],
                                                       scale=1.0, accum_out=bis[:, 31:32]), [tS, t_bis], [t_junk, t_bis])
                    yield
                    P.op("act", lambda e: e.activation(out=bis[:, 32:33], in_=bis[:, 31:32], func=AF.Sign,
                                                       bias=float(L - 510.5), scale=1.0), [t_bis], [t_bis])
                    yield
                    P.op("act", lambda e, it=it: e.activation(out=bis[:, 30:31], in_=bis[:, 32:33], func=AF.Identity,
                                                              bias=bis[:, 30:31], scale=bis[:, 41 + it:42 + it]),
                         [t_bis], [t_bis])
                    yield
                P.op("dve", lambda e: e.scalar_tensor_tensor(out=bis[:, 7:8], in0=bis[:, 30:31], scalar=-1.0,
                                                             in1=bis[:, 40 + NIT:41 + NIT], op0=ALU.mult, op1=ALU.add),
                     [t_bis], [t_bis])
                yield
            else:
              for _once in (0,):
                for it in range(NIT):
                    P.op("dve", lambda e: e.tensor_scalar(out=junk[:, 0:L], in0=S_[:, 0:L], scalar1=bis[:, 4:5], scalar2=None,
                                                          op0=ALU.is_ge, op1=ALU.add, accum_out=bis[:, 5:6]),
                         [tS, t_bis], [t_junk, t_bis])
                    yield
                    P.op("dve", lambda e: e.tensor_scalar(out=bis[:, 6:7], in0=bis[:, 5:6], scalar1=255.5, scalar2=0.5,
                                                          op0=ALU.is_ge, op1=ALU.subtract), [t_bis], [t_bis])
                    yield
                    P.op("dve", lambda e, it=it: e.scalar_tensor_tensor(out=bis[:, 4:5], in0=bis[:, 6:7], scalar=bis[:, 8 + it:9 + it],
                                                                        in1=bis[:, 4:5], op0=ALU.mult, op1=ALU.add),
                         [t_bis], [t_bis])
                    yield
                P.op("dve", lambda e: e.tensor_tensor(out=bis[:, 7:8], in0=bis[:, 4:5], in1=bis[:, 8 + NIT:9 + NIT], op=ALU.subtract),
                     [t_bis], [t_bis])
                yield
            P.op("dve", lambda e: e.tensor_scalar(out=mask[:, 0:L], in0=S_[:, 0:L], scalar1=bis[:, 7:8], scalar2=None,
                                                  op0=ALU.is_ge), [tS, t_bis], [t_mask])
            yield
            if DCUT <= 3:
                return
            for half in range((qt // 8) + 1):
                nb = min(8, qt + 1 - half * 8)
                for j in range(nb):
                    kb = half * 8 + j
                    P.op("pe", lambda e, kb=kb, j=j, half=half: e.transpose(
                        out=psb16[qt % 2][:, j * 128:(j + 1) * 128], in_=mask[:, kb * 128:(kb + 1) * 128], identity=identb),
                        [t_mask, t_identb], [t_ps[qt % 2]])
                    yield
                P.op("act", lambda e, half=half, nb=nb: e.activation(
                    out=maskT[:, half * 8:half * 8 + nb, :],
                    in_=psb16[qt % 2][:, 0:nb * 128].rearrange("p (j t) -> p j t", j=nb), func=AF.Copy),
                    [t_ps[qt % 2]], [t_maskT])
                yield
        def q_att(qt):
            L = (qt + 1) * 128
            q0 = qt * 128
            maskT, t_maskT = maskT2[qt % 4]
            items = [(kb, g) for kb in range(qt + 1) for g in range(2)]
            LA = 3

            def front(idx):
                kb, g = items[idx]
                bS = 2 + (idx % 4)
                PTt, tPT = PT[idx % len(PT)]
                P.op("pe", lambda e: e.matmul(
                    ps[bS], lhsT=kT2[64 * g:64 * g + 64, kb * 128:(kb + 1) * 128],
                    rhs=qT2[64 * g:64 * g + 64, qt, :], start=True, stop=True),
                    [t_kT2, t_qT2], [t_ps[bS]])
                P.op("act", lambda e: e.activation(out=PTt.rearrange("p r t -> p (r t)"), in_=ps[bS],
                                                   func=AF.Exp), [t_ps[bS]], [tPT])
                P.op("pool", lambda e: e.tensor_tensor(
                    out=PTt, in0=PTt, in1=maskT[:, kb, :].unsqueeze(1).to_broadcast([128, 4, 128]), op=ALU.mult),
                    [tPT, t_maskT], [tPT])

            def back(idx):
                kb, g = items[idx]
                PTt, tPT = PT[idx % len(PT)]
                P.op("pe", lambda e: e.matmul(
                    ps[6][64 * g:64 * g + 64, :], lhsT=v_b[:, kb, 64 * g:64 * g + 64],
                    rhs=PTt.rearrange("p r t -> p (r t)"), start=(kb == 0), stop=(kb == qt)),
                    [tPT, t_vb], [t_ps[6]])
                P.op("pe", lambda e: e.matmul(
                    ps[7][64 * g:64 * g + 64, :], lhsT=onesb[:, 0:64],
                    rhs=PTt.rearrange("p r t -> p (r t)"), start=(kb == 0), stop=(kb == qt)),
                    [tPT, t_onesb], [t_ps[7]])

            n_it = len(items)
            for idx in range(n_it + LA):
                if idx < n_it:
                    front(idx)
                if idx - LA >= 0:
                    back(idx - LA)
            P.op("act", lambda e: e.activation(out=rec.rearrange("p r t -> p (r t)"), in_=ps[7], func=AF.Ln), [t_ps[7]], [t_rec])
            P.op("act", lambda e: e.activation(out=rec.rearrange("p r t -> p (r t)"), in_=rec.rearrange("p r t -> p (r t)"),
                                               func=AF.Exp, scale=-1.0), [t_rec], [t_rec])
            P.op("dve", lambda e: e.tensor_tensor(out=o_atT[:, :, q0:q0 + 128],
                                                  in0=ps[6].rearrange("p (r t) -> p r t", r=4), in1=rec, op=ALU.mult),
                 [t_ps[6], t_rec], [t_oatT])

        def lockstep(gens):
            gens = list(gens)
            while gens:
                for g_ in list(gens):
                    try:
                        next(g_)
                    except StopIteration:
                        gens.remove(g_)

        lockstep([q_sb(0), q_sb(1)])
        for p_ in range(0, NT, 2):
            if p_ + 2 < NT:
                lockstep([q_sb(p_ + 2), q_sb(p_ + 3)])
            q_att(p_)
            q_att(p_ + 1)

    def c1_phase(sq, o_atT, t_oatT, o_gdnT, t_ogT, mkT2, t_mkT2, mv_b, t_mv):
        row_base = sq * SEQ
        alloc_stage(1024)
        Wo_a, t_Woa = sbt("Wo_a", [128, 4, 1024], BF16)
        Wo_g, t_Wog = sbt("Wo_g", [128, 4, 1024], BF16)
        Wmq, t_Wmq = sbt("Wmq", [128, 8, 256], BF16)
        Wmo, t_Wmo = sbt("Wmo", [128, 2, 1024], BF16)
        for r in range(4):
            i = k.wl % 2
            k.wl += 1
            st = stage[i][:, 0:1024]
            for g in range(2):
                P.dma("sp", st[64 * g:64 * g + 64, :], w_out[256 * g + 64 * r:256 * g + 64 * r + 64, :],
                      writes=[t_stage[i]])
            P.op("act", lambda e, st=st, r=r: e.activation(out=Wo_a[:, r, :], in_=st, func=AF.Copy),
                 [t_stage[i]], [t_Woa])
        for h in range(4):
            load_rows(Wo_g[:, h, :], t_Wog, w_out[512 + 128 * h:512 + 128 * h + 128, :], 1024, None, None)
        load_weight(Wmq, t_Wmq, w_mq, 0, 256, gX, t_gX)
        for a in range(2):
            load_rows(Wmo[:, a, :], t_Wmo, w_mo[128 * a:128 * a + 128, :], 1024, None, None)
        x1 = [sbt("x1_%d" % i, [128, D]) for i in range(2)]
        hT2, t_hT2 = sbt("hT2", [128, 8, 128], BF16)
        qmb2 = [sbt("qmb%d" % i, [128, 256], BF16) for i in range(2)]
        qmT2, t_qmT2 = sbt("qmT2", [128, 2, 128], BF16)
        PTm, t_PTm = sbt("PTm", [128, 2, 4, 128], BF16)
        omT2, t_omT2 = sbt("omT2", [128, 2, 128], BF16)
        recm, t_recm = sbt("recm", [128, 256])

        def c1_h1(t):
            r0 = row_base + t * 128
            c0 = t * 128
            if CCUT <= 0:
                return
            x, tx, i = load_x(xp[r0:r0 + 128, :], 128, key=("c", r0))
            if t + 1 < NT:
                prefetch_x(xp[r0 + 128:r0 + 256, :], 128, ("c", r0 + 128))
            xx, txx = x1[t % 2]
            qmb, t_qmb = qmb2[t % 2]
            for c in range(2):
                for r in range(4):
                    P.op("pe", lambda e, r=r, c=c: e.matmul(ps[1 + c], lhsT=o_atT[:, r, c0:c0 + 128],
                                                            rhs=Wo_a[:, r, c * 512:(c + 1) * 512], start=(r == 0), stop=False),
                         [t_oatT, t_Woa], [t_ps[1 + c]])
                for h in range(4):
                    P.op("pe", lambda e, h=h, c=c: e.matmul(ps[1 + c], lhsT=o_gdnT[:, h, c0:c0 + 128],
                                                            rhs=Wo_g[:, h, c * 512:(c + 1) * 512], start=False, stop=(h == 3)),
                         [t_ogT, t_Wog], [t_ps[1 + c]])
                P.op("dve", lambda e, c=c: e.tensor_tensor(out=xx[:, c * 512:(c + 1) * 512], in0=ps[1 + c],
                                                           in1=x[:, c * 512:(c + 1) * 512], op=ALU.add),
                     [t_ps[1 + c], tx], [txx])
            if CCUT <= 1:
                return
            norm_T(xx, txx, i, hT2, t_hT2, 0, 0)
            for kc in range(8):
                P.op("pe", lambda e, kc=kc: e.matmul(ps[3][:, 0:256], lhsT=hT2[:, kc, :], rhs=Wmq[:, kc, :],
                                                     start=(kc == 0), stop=(kc == 7)), [t_hT2, t_Wmq], [t_ps[3]])
            if CCUT <= 2:
                return
            head_rms(ps[3][:, 0:256], t_ps[3], 4, qscr, t_qscr, qss, t_qss)
            P.op("dve", lambda e: e.tensor_tensor(
                out=qmb.rearrange("p (h d) -> p h d", h=4), in0=ps[3][:, 0:256].rearrange("p (h d) -> p h d", h=4),
                in1=qss[:, 4:8].unsqueeze(2).to_broadcast([128, 4, 64]), op=ALU.mult), [t_ps[3], t_qss], [t_qmb])
        def c1_h2(t):
            r0 = row_base + t * 128
            c0 = t * 128
            xx, txx = x1[t % 2]
            qmb, t_qmb = qmb2[t % 2]
            pb4 = psb16[4]
            for a in range(2):
                P.op("pe", lambda e, a=a: e.transpose(out=pb4[:, a * 128:(a + 1) * 128], in_=qmb[:, a * 128:(a + 1) * 128],
                                                      identity=identb), [t_qmb, t_identb], [t_ps[4]])
            P.op("act", lambda e: e.activation(out=qmT2.rearrange("p a t -> p (a t)"), in_=pb4[:, 0:256], func=AF.Copy),
                 [t_ps[4]], [t_qmT2])
            P.op("pool", lambda e: e.tensor_scalar(out=qmT2.rearrange("p a t -> p (a t)"), in0=qmT2.rearrange("p a t -> p (a t)"),
                                                   scalar1=gmq8[:, 0:1], scalar2=1.0, op0=ALU.mult, op1=ALU.mult),
                 [t_qmT2, t_gmq8], [t_qmT2])
            if CCUT <= 3:
                return
            for b in range(2):
                for mb in range(2):
                    for a in range(2):
                        j = mb * 2 + a
                        P.op("pe", lambda e, mb=mb, a=a, b=b, j=j: e.matmul(
                            ps[5 + b][:, j * 128:(j + 1) * 128], lhsT=mkT2[64 * b:64 * b + 64, a, mb * 128:(mb + 1) * 128],
                            rhs=qmT2[64 * b:64 * b + 64, a, :], start=True, stop=True), [t_mkT2, t_qmT2], [t_ps[5 + b]])
                P.op("act", lambda e, b=b: e.activation(out=PTm[:, b, :, :].rearrange("p j t -> p (j t)"),
                                                        in_=ps[5 + b], func=AF.Exp), [t_ps[5 + b]], [t_PTm])
            if CCUT <= 4:
                return
            for mh in range(4):
                a, b = mh // 2, mh % 2
                for mb in range(2):
                    P.op("pe", lambda e, mb=mb, mh=mh, a=a, b=b: e.matmul(
                        ps[7][64 * b:64 * b + 64, a * 128:(a + 1) * 128], lhsT=mv_b[:, mb, mh * 64:(mh + 1) * 64],
                        rhs=PTm[:, b, mb * 2 + a, :], start=(mb == 0), stop=(mb == 1)), [t_mv, t_PTm], [t_ps[7]])
                for mb in range(2):
                    P.op("pe", lambda e, mb=mb, mh=mh, a=a, b=b: e.matmul(
                        ps[7][64 * b:64 * b + 64, 256 + a * 128:256 + (a + 1) * 128], lhsT=onesb[:, 0:64],
                        rhs=PTm[:, b, mb * 2 + a, :], start=(mb == 0), stop=(mb == 1)), [t_onesb, t_PTm], [t_ps[7]])
            if CCUT <= 5:
                return
            P.op("dve", lambda e: e.reciprocal(out=recm, in_=ps[7][:, 256:512]), [t_ps[7]], [t_recm])
            P.op("dve", lambda e: e.tensor_tensor(out=omT2.rearrange("p a t -> p (a t)"), in0=ps[7][:, 0:256], in1=recm,
                                                  op=ALU.mult), [t_ps[7], t_recm], [t_omT2])
            for c in range(2):
                for a in range(2):
                    P.op("pe", lambda e, a=a, c=c: e.matmul(ps[5 + c], lhsT=omT2[:, a, :],
                                                            rhs=Wmo[:, a, c * 512:(c + 1) * 512], start=(a == 0), stop=(a == 1)),
                         [t_omT2, t_Wmo], [t_ps[5 + c]])
                P.op("dve", lambda e, c=c: e.tensor_tensor(out=xx[:, c * 512:(c + 1) * 512], in0=ps[5 + c],
                                                           in1=xx[:, c * 512:(c + 1) * 512], op=ALU.add),
                     [t_ps[5 + c], txx], [txx])
            P.dma("sp", y_p[r0:r0 + 128, :], xx, reads=[txx])

        c1_h1(0)
        for t in range(NT):
            if t + 1 < NT:
                c1_h1(t + 1)
            c1_h2(t)

    t_yscr = Tok()

    def c2_phase(sq):
        row_base = sq * SEQ
        alloc_stage(2816)
        Wg, t_Wg = sbt("Wg", [128, 8, 2816], BF16)
        Wu, t_Wu = sbt("Wu", [128, 8, 2816], BF16)
        Wd, t_Wd = sbt("Wd", [128, 22, 1024], BF16)
        load_weight(Wg, t_Wg, w_gate, 0, 2816, gF, t_gF)
        load_weight(Wu, t_Wu, w_up, 0, 2816, gF, t_gF)
        for f in range(22):
            load_rows(Wd[:, f, :], t_Wd, w_down[128 * f:128 * f + 128, :], 1024, None, None)
        hT3, t_hT3 = sbt("hT3", [128, 8, 256], BF16)
        hfT, t_hfT = sbt("hfT", [128, 22, 256], BF16)
        sg = [sbt("sg%d" % i, [128, 256]) for i in range(2)]

        def c2_group(gi):
            xs_ = []
            for t2 in range(2):
                r0 = row_base + (gi * 2 + t2) * 128
                x, tx, i = load_x(y_p[r0:r0 + 128, :], 128)
                norm_T(x, tx, i, hT3, t_hT3, t2 * 128, 0)
                xs_.append((x, tx, r0))
            for f in range(22):
                b = 1 + (f % 2)
                for kc in range(8):
                    P.op("pe", lambda e, kc=kc, f=f, b=b: e.matmul(ps[b][:, 0:256], lhsT=Wg[:, kc, 128 * f:128 * f + 128],
                                                                   rhs=hT3[:, kc, :], start=(kc == 0), stop=(kc == 7)),
                         [t_Wg, t_hT3], [t_ps[b]])
                for kc in range(8):
                    P.op("pe", lambda e, kc=kc, f=f, b=b: e.matmul(ps[b][:, 256:512], lhsT=Wu[:, kc, 128 * f:128 * f + 128],
                                                                   rhs=hT3[:, kc, :], start=(kc == 0), stop=(kc == 7)),
                         [t_Wu, t_hT3], [t_ps[b]])
                s_, ts_ = sg[f % 2]
                P.op("act", lambda e, s_=s_, b=b: e.activation(out=s_, in_=ps[b][:, 0:256], func=AF.Silu), [t_ps[b]], [ts_])
                P.op("dve", lambda e, s_=s_, b=b, f=f: e.tensor_tensor(out=hfT[:, f, :], in0=s_, in1=ps[b][:, 256:512],
                                                                       op=ALU.mult), [ts_, t_ps[b]], [t_hfT])
            for t2 in range(2):
                x, tx, r0 = xs_[t2]
                for c in range(2):
                    for f in range(22):
                        P.op("pe", lambda e, f=f, c=c, t2=t2: e.matmul(ps[3 + c], lhsT=hfT[:, f, t2 * 128:(t2 + 1) * 128],
                                                                       rhs=Wd[:, f, c * 512:(c + 1) * 512],
                                                                       start=(f == 0), stop=(f == 21)),
                             [t_hfT, t_Wd], [t_ps[3 + c]])
                    P.op("dve", lambda e, c=c, x=x: e.tensor_tensor(out=x[:, c * 512:(c + 1) * 512], in0=ps[3 + c],
                                                                    in1=x[:, c * 512:(c + 1) * 512], op=ALU.add),
                         [t_ps[3 + c], tx], [tx])
                P.dma("sp", y_p[r0:r0 + 128, :], x, reads=[tx])

        for gi in range(NT // 2):
            c2_group(gi)

    def sample_group():
        k.top = PERSIST_TOP
        NSB = NS
        proj, t_proj = sbt("s_proj", [128, INW])
        x_keep, t_xk = sbt("s_xkeep", [128, D])
        sso, t_sso = sbt("s_o", [128, 320])
        ss_, t_ss = sbt("s_ss", [128, 64])
        scr, t_scr = sbt("s_scr", [128, 1536])
        S_TOP = k.top
        alloc_stage(INW)
        wAll, t_wAll = sbt("s_wAll", [128, 8, INW], BF16)
        load_weight(wAll, t_wAll, w_in, 0, INW, gA, t_gA)
        hTs, t_hTs = sbt("s_hT", [128, 8, 128], BF16)
        x, tx, xi = load_x(xs[:, :], NSB)
        P.op("pool", lambda e: e.tensor_copy(out=x_keep[0:NSB, :], in_=x[0:NSB, :]), [tx], [t_xk])
        norm_T(x, tx, xi, hTs, t_hTs, 0, 0)
        for c in range(7):
            c0 = c * 512
            n = min(512, INW - c0)
            b = 1 + (c % 2)
            for kc in range(8):
                P.op("pe", lambda e, kc=kc, b=b, c0=c0, n=n: e.matmul(
                    ps[b][0:NSB, 0:n], lhsT=hTs[:, kc, 0:NSB], rhs=wAll[:, kc, c0:c0 + n],
                    start=(kc == 0), stop=(kc == 7)), [t_hTs, t_wAll], [t_ps[b]])
            P.op("act", lambda e, b=b, c0=c0, n=n: e.activation(out=proj[0:NSB, c0:c0 + n], in_=ps[b][0:NSB, 0:n],
                                                                func=AF.Copy), [t_ps[b]], [t_proj])
        pj = proj[0:NSB]
        so = sso[0:NSB]
        P.op("dve", lambda e: e.tensor_tensor(out=scr[0:NSB, 0:128], in0=pj[:, 512:640], in1=pj[:, 512:640], op=ALU.mult),
             [t_proj], [t_scr])
        P.op("dve", lambda e: e.tensor_reduce(out=ss_[0:NSB, 0:2], in_=scr[0:NSB, 0:128].rearrange("p (h d) -> p h d", h=2),
                                              axis=AX.X, op=ALU.add), [t_scr], [t_ss])
        P.op("pool", lambda e: e.tensor_scalar(out=ss_[0:NSB, 2:4], in0=ss_[0:NSB, 0:2], scalar1=1.0 / 64, scalar2=EPS,
                                               op0=ALU.mult, op1=ALU.add), [t_ss], [t_ss])
        P.op("pool", lambda e: e.tensor_tensor(out=ss_[0:NSB, 2:4], in0=ss_[0:NSB, 2:4], in1=neghalf[0:NSB, 0:2], op=ALU.pow),
             [t_ss, t_nh], [t_ss])
        P.op("dve", lambda e: e.tensor_tensor(out=so[:, 0:128].rearrange("p (h d) -> p h d", h=2),
                                              in0=pj[:, 512:640].rearrange("p (h d) -> p h d", h=2),
                                              in1=ss_[0:NSB, 2:4].unsqueeze(2).to_broadcast([NSB, 2, 64]), op=ALU.mult),
             [t_proj, t_ss], [t_sso])
        P.op("pool", lambda e: e.tensor_tensor(out=so[:, 0:128].rearrange("p (h d) -> p h d", h=2),
                                               in0=so[:, 0:128].rearrange("p (h d) -> p h d", h=2),
                                               in1=gk_b[0:NSB].unsqueeze(1).to_broadcast([NSB, 2, 64]), op=ALU.mult),
             [t_sso, t_gk], [t_sso])
        P.op("act", lambda e: e.activation(out=so[:, 128:256], in_=pj[:, 640:768], func=AF.Copy), [t_proj], [t_sso])
        P.op("act", lambda e: e.activation(out=so[:, 256:320], in_=pj[:, 1024:1088], func=AF.Copy), [t_proj], [t_sso])
        P.dma("sp", k_s[:, :], so[:, 0:128], reads=[t_sso])
        P.dma("sp", v_s[:, :], so[:, 128:256], reads=[t_sso])
        P.dma("sp", kidx_s[:, :], so[:, 256:320], reads=[t_sso])
        conv_s3 = conv_s.rearrange("(s r) c -> s r c", r=3)
        st_conv3 = st_conv.rearrange("(s r) c -> s r c", r=3)
        P.dma("sp", conv_s3[:, 2, :], pj[:, 1092:2628], reads=[t_proj])
        P.dma("sp", conv_s3[:, 0:2, :], st_conv3[:, 1:3, :])
        P.barrier()
        k.top = S_TOP
        stc, t_stc = sbt("s_stc", [128, 3, 1536])
        cwb, t_cwb = sbt("s_cwb", [128, 4, 1536])
        P.dma("sp", stc[0:NSB], st_conv3, writes=[t_stc])
        P.dma("sp", cwb[0:NSB], conv_w.rearrange("t c -> (t c)").partition_broadcast(NSB), writes=[t_cwb])
        cc, t_cc = sbt("s_cc", [128, 1536])
        c_ = cc[0:NSB]
        sc_ = scr[0:NSB]
        P.op("dve", lambda e: e.tensor_tensor(out=c_, in0=pj[:, 1092:2628], in1=cwb[0:NSB, 3, :], op=ALU.mult),
             [t_proj, t_cwb], [t_cc])
        for j in range(3):
            P.op("dve", lambda e, j=j: e.tensor_tensor(out=sc_, in0=stc[0:NSB, j, :], in1=cwb[0:NSB, j, :], op=ALU.mult),
                 [t_stc, t_cwb], [t_scr])
            P.op("dve", lambda e: e.tensor_tensor(out=c_, in0=c_, in1=sc_, op=ALU.add), [t_cc, t_scr], [t_cc])
        P.op("act", lambda e: e.activation(out=c_, in_=c_, func=AF.Silu), [t_cc], [t_cc])
        P.op("dve", lambda e: e.tensor_tensor(out=sc_[:, 0:1024], in0=c_[:, 0:1024], in1=c_[:, 0:1024], op=ALU.mult),
             [t_cc], [t_scr])
        P.op("dve", lambda e: e.tensor_reduce(out=ss_[0:NSB, 8:16], in_=sc_[:, 0:1024].rearrange("p (h d) -> p h d", h=8),
                                              axis=AX.X, op=ALU.add), [t_scr], [t_ss])
        P.op("pool", lambda e: e.tensor_scalar(out=ss_[0:NSB, 16:24], in0=ss_[0:NSB, 8:16], scalar1=1.0, scalar2=EPS,
                                               op0=ALU.mult, op1=ALU.add), [t_ss], [t_ss])
        P.op("pool", lambda e: e.tensor_tensor(out=ss_[0:NSB, 16:24], in0=ss_[0:NSB, 16:24], in1=neghalf[0:NSB, 0:8], op=ALU.pow),
             [t_ss, t_nh], [t_ss])
        P.op("pool", lambda e: e.tensor_scalar(out=ss_[0:NSB, 16:20], in0=ss_[0:NSB, 16:20], scalar1=float(128.0 ** -0.5),
                                               scalar2=1.0, op0=ALU.mult, op1=ALU.mult), [t_ss], [t_ss])
        P.op("dve", lambda e: e.tensor_tensor(out=c_[:, 0:1024].rearrange("p (h d) -> p h d", h=8),
                                              in0=c_[:, 0:1024].rearrange("p (h d) -> p h d", h=8),
                                              in1=ss_[0:NSB, 16:24].unsqueeze(2).to_broadcast([NSB, 8, 128]), op=ALU.mult),
             [t_cc, t_ss], [t_cc])
        sv = ss_[0:NSB]
        P.op("dve", lambda e: e.tensor_tensor(out=sv[:, 24:28], in0=pj[:, 3140:3144], in1=dtb_b[0:NSB], op=ALU.add),
             [t_proj, t_dtb], [t_ss])
        P.op("dve", lambda e: e.tensor_scalar(out=sv[:, 28:32], in0=sv[:, 24:28], scalar1=-1.0, scalar2=None, op0=ALU.mult),
             [t_ss], [t_ss])
        P.op("dve", lambda e: e.tensor_tensor(out=sv[:, 28:32], in0=sv[:, 28:32], in1=sv[:, 24:28], op=ALU.min), [t_ss], [t_ss])
        P.op("act", lambda e: e.activation(out=sv[:, 28:32], in_=sv[:, 28:32], func=AF.Exp), [t_ss], [t_ss])
        P.op("act", lambda e: e.activation(out=sv[:, 28:32], in_=sv[:, 28:32], func=AF.Ln, bias=1.0, scale=1.0), [t_ss], [t_ss])
        P.op("dve", lambda e: e.scalar_tensor_tensor(out=sv[:, 32:36], in0=sv[:, 24:28], scalar=0.0, in1=sv[:, 28:32],
                                                     op0=ALU.max, op1=ALU.add), [t_ss], [t_ss])
        P.op("dve", lambda e: e.tensor_tensor(out=sv[:, 32:36], in0=sv[:, 32:36], in1=nea_b[0:NSB], op=ALU.mult),
             [t_ss, t_nea], [t_ss])
        P.op("act", lambda e: e.activation(out=sv[:, 36:40], in_=pj[:, 3144:3148], func=AF.Sigmoid), [t_proj], [t_ss])
        P.op("act", lambda e: e.activation(out=sv[:, 40:44], in_=sv[:, 32:36], func=AF.Exp), [t_ss], [t_ss])
        S0, t_S0 = sbt("s_S0", [128, NSB, 4, 128])
        for i in range(NSB):
            P.dma("sp", S0[:, i, :, :], ssm_in[i * 512:(i + 1) * 512, :].rearrange("(h d) v -> d h v", h=4), writes=[t_S0])
        kqT, t_kqT = sbt("s_kqT", [128, 8, NSB])
        for j in range(8):
            b = 1 + (j % 2)
            P.op("pe", lambda e, j=j, b=b: e.transpose(out=ps[b][:, 0:NSB], in_=c_[:, j * 128:(j + 1) * 128],
                                                       identity=identf[0:NSB, 0:NSB]), [t_cc, t_identf], [t_ps[b]])
            P.op("act", lambda e, j=j, b=b: e.activation(out=kqT[:, j, :], in_=ps[b][:, 0:NSB], func=AF.Copy),
                 [t_ps[b]], [t_kqT])
        eye_b, t_eyeb = sbt("s_eyeb", [128, NSB, NSB])
        P.dma("sp", eye_b, eye16_d.partition_broadcast(128), writes=[t_eyeb])
        kqTm, t_kqTm = sbt("s_kqTm", [128, 8, NSB, NSB])
        P.op("pool", lambda e: e.tensor_tensor(out=kqTm, in0=kqT.unsqueeze(2).to_broadcast([128, 8, NSB, NSB]),
                                               in1=eye_b.unsqueeze(1).to_broadcast([128, 8, NSB, NSB]), op=ALU.mult),
             [t_kqT, t_eyeb], [t_kqTm])
        for h in range(4):
            for i in range(NSB):
                P.op("pe", lambda e, h=h, i=i: e.matmul(ps[3][0:NSB, h * 128:(h + 1) * 128], lhsT=kqTm[:, 4 + h, i, :],
                                                        rhs=S0[:, i, h, :], start=(i == 0), stop=(i == NSB - 1)),
                     [t_kqTm, t_S0], [t_ps[3]])
        dl, t_dl = sbt("s_dl", [128, 4, 128])
        d_ = dl[0:NSB]
        bcs = lambda a: a.unsqueeze(2).to_broadcast([NSB, 4, 128])
        P.op("dve", lambda e: e.tensor_tensor(out=d_, in0=ps[3][0:NSB, :].rearrange("p (h v) -> p h v", h=4),
                                              in1=bcs(sv[:, 40:44]), op=ALU.mult), [t_ps[3], t_ss], [t_dl])
        P.op("dve", lambda e: e.tensor_tensor(out=d_, in0=c_[:, 1024:1536].rearrange("p (h v) -> p h v", h=4), in1=d_,
                                              op=ALU.subtract), [t_cc, t_dl], [t_dl])
        P.op("dve", lambda e: e.tensor_tensor(out=d_, in0=d_, in1=bcs(sv[:, 36:40]), op=ALU.mult), [t_dl, t_ss], [t_dl])
        ckm, t_ckm = sbt("s_ckm", [128, NSB, 512])
        P.op("pool", lambda e: e.tensor_tensor(out=ckm[0:NSB], in0=c_[:, 512:1024].unsqueeze(1).to_broadcast([NSB, NSB, 512]),
                                               in1=identf[0:NSB, 0:NSB].unsqueeze(2).to_broadcast([NSB, NSB, 512]), op=ALU.mult),
             [t_cc, t_identf], [t_ckm])
        adg, t_adg = sbt("s_adg", [128, NSB, 4])
        P.op("pool", lambda e: e.tensor_tensor(out=adg[0:NSB], in0=sv[:, 40:44].unsqueeze(1).to_broadcast([NSB, NSB, 4]),
                                               in1=identf[0:NSB, 0:NSB].unsqueeze(2).to_broadcast([NSB, NSB, 4]), op=ALU.mult),
             [t_ss, t_identf], [t_adg])
        P.op("pe", lambda e: e.matmul(ps[4][:, 0:NSB * 4], lhsT=onesf[0:NSB, :], rhs=adg[0:NSB].rearrange("p i h -> p (i h)"),
                                      start=True, stop=True), [t_adg, t_onesf], [t_ps[4]])
        abc, t_abc = sbt("s_abc", [128, NSB * 4])
        P.op("act", lambda e: e.activation(out=abc, in_=ps[4][:, 0:NSB * 4], func=AF.Copy), [t_ps[4]], [t_abc])
        for i in range(NSB):
            b = 5 + (i % 2)
            for h in range(4):
                P.op("pe", lambda e, h=h, i=i, b=b: e.matmul(ps[b][:, h * 128:(h + 1) * 128], lhsT=ckm[0:NSB, i, h * 128:(h + 1) * 128],
                                                             rhs=d_[:, h, :], start=True, stop=True), [t_ckm, t_dl], [t_ps[b]])
            for h in range(4):
                P.op("dve", lambda e, h=h, i=i, b=b: e.scalar_tensor_tensor(
                    out=S0[:, i, h, :], in0=S0[:, i, h, :], scalar=abc[:, i * 4 + h:i * 4 + h + 1],
                    in1=ps[b][:, h * 128:(h + 1) * 128], op0=ALU.mult, op1=ALU.add), [t_S0, t_abc, t_ps[b]], [t_S0])
            P.dma("sp", ssm_s[i * 512:(i + 1) * 512, :].rearrange("(h d) v -> d h v", h=4), S0[:, i, :, :], reads=[t_S0])
        for h in range(4):
            for i in range(NSB):
                P.op("pe", lambda e, h=h, i=i: e.matmul(ps[7][0:NSB, h * 128:(h + 1) * 128], lhsT=kqTm[:, h, i, :],
                                                        rhs=S0[:, i, h, :], start=(i == 0), stop=(i == NSB - 1)),
                     [t_kqTm, t_S0], [t_ps[7]])
        og, t_og = sbt("s_og", [128, 4, 128])
        o_ = og[0:NSB]
        P.op("act", lambda e: e.activation(out=sc_[:, 0:512], in_=ps[7][0:NSB, :], func=AF.Square), [t_ps[7]], [t_scr])
        P.op("dve", lambda e: e.tensor_reduce(out=sv[:, 44:48], in_=sc_[:, 0:512].rearrange("p (h v) -> p h v", h=4),
                                              axis=AX.X, op=ALU.add), [t_scr], [t_ss])
        P.op("pool", lambda e: e.tensor_scalar(out=sv[:, 48:52], in0=sv[:, 44:48], scalar1=1.0 / 128, scalar2=EPS,
                                               op0=ALU.mult, op1=ALU.add), [t_ss], [t_ss])
        P.op("pool", lambda e: e.tensor_tensor(out=sv[:, 48:52], in0=sv[:, 48:52], in1=neghalf[0:NSB, 0:4], op=ALU.pow),
             [t_ss, t_nh], [t_ss])
        P.op("dve", lambda e: e.tensor_tensor(out=o_, in0=ps[7][0:NSB, :].rearrange("p (h v) -> p h v", h=4),
                                              in1=bcs(sv[:, 48:52]), op=ALU.mult), [t_ps[7], t_ss], [t_og])
        P.op("pool", lambda e: e.tensor_tensor(out=o_, in0=o_, in1=ggdn_b[0:NSB].unsqueeze(1).to_broadcast([NSB, 4, 128]),
                                               op=ALU.mult), [t_og, t_ggdn], [t_og])
        P.op("act", lambda e: e.activation(out=sc_[:, 0:512], in_=pj[:, 2628:3140], func=AF.Silu), [t_proj], [t_scr])
        P.op("dve", lambda e: e.tensor_tensor(out=o_.rearrange("p h v -> p (h v)"), in0=o_.rearrange("p h v -> p (h v)"),
                                              in1=sc_[:, 0:512], op=ALU.mult), [t_og, t_scr], [t_og])
        P.op("pool", lambda e: e.tensor_copy(out=pj[:, 1092:1604], in_=o_.rearrange("p h v -> p (h v)")), [t_og], [t_proj])
        P.barrier()
        k.top = S_TOP
        if STOP == -1:
            return
        sample_dsa(proj, t_proj, sso, t_sso)
        sample_tail(proj, t_proj, x_keep, t_xk)

    def sample_dsa(proj, t_proj, sso, t_sso):
        NSB = NS
        pj = proj[0:NSB]
        U32 = mybir.dt.uint32
        selp, t_selp = sbt("s_selp", [128, 8, 128])
        selo, t_selo = sbt("s_selo", [128, NSB, 128])
        P.dma("sp", selp[0:NSB], selpair_d.rearrange("q i p -> i q p"), writes=[t_selp])
        P.dma("sp", selo[0:NSB], selone_d, writes=[t_selo])
        gq_b, t_gqb = bcast_layout("s_gq_b", g_q, 64)
        iota_b, t_iota = bcast_layout("s_iota", iota64_d, 64)
        pt_i, t_pti = sbt("s_pt_i", [128, 64], I32)
        pt_f, t_ptf = sbt("s_pt_f", [128, 64])
        P.dma("sp", pt_i[0:NSB], ptab, writes=[t_pti])
        P.op("dve", lambda e: e.tensor_copy(out=pt_f[0:NSB], in_=pt_i[0:NSB]), [t_pti], [t_ptf])
        ss2, t_ss2 = sbt("s_ss2", [128, 32])
        scr2, t_scr2 = sbt("s_scr2", [128, 512])
        qn, t_qn = sbt("s_qn", [128, 512])
        qiw, t_qiw = sbt("s_qiw", [128, 260])
        sv = ss2[0:NSB]
        P.op("dve", lambda e: e.tensor_tensor(out=scr2[0:NSB], in0=pj[:, 0:512], in1=pj[:, 0:512], op=ALU.mult), [t_proj], [t_scr2])
        P.op("dve", lambda e: e.tensor_reduce(out=sv[:, 0:8], in_=scr2[0:NSB].rearrange("p (h d) -> p h d", h=8), axis=AX.X,
                                              op=ALU.add), [t_scr2], [t_ss2])
        P.op("pool", lambda e: e.tensor_scalar(out=sv[:, 8:16], in0=sv[:, 0:8], scalar1=1.0 / 64, scalar2=EPS, op0=ALU.mult,
                                               op1=ALU.add), [t_ss2], [t_ss2])
        P.op("pool", lambda e: e.tensor_tensor(out=sv[:, 8:16], in0=sv[:, 8:16], in1=neghalf[0:NSB, 0:8], op=ALU.pow),
             [t_ss2, t_nh], [t_ss2])
        P.op("pool", lambda e: e.tensor_scalar(out=sv[:, 8:16], in0=sv[:, 8:16], scalar1=0.125, scalar2=1.0, op0=ALU.mult,
                                               op1=ALU.mult), [t_ss2], [t_ss2])
        q3 = qn[0:NSB].rearrange("p (h d) -> p h d", h=8)
        P.op("dve", lambda e: e.tensor_tensor(out=q3, in0=pj[:, 0:512].rearrange("p (h d) -> p h d", h=8),
                                              in1=sv[:, 8:16].unsqueeze(2).to_broadcast([NSB, 8, 64]), op=ALU.mult),
             [t_proj, t_ss2], [t_qn])
        P.op("pool", lambda e: e.tensor_tensor(out=q3, in0=q3, in1=gq_b[0:NSB].unsqueeze(1).to_broadcast([NSB, 8, 64]),
                                               op=ALU.mult), [t_qn, t_gqb], [t_qn])
        P.op("act", lambda e: e.activation(out=qiw[0:NSB, 0:256], in_=pj[:, 768:1024], func=AF.Copy, scale=0.125), [t_proj], [t_qiw])
        P.op("act", lambda e: e.activation(out=qiw[0:NSB, 256:260], in_=pj[:, 1088:1092], func=AF.Copy, scale=0.5), [t_proj], [t_qiw])
        scores, t_scores = sbt("s_scores", [128, 8200])
        P.op("dve", lambda e: e.tensor_tensor(out=scr2[0:NSB, 0:256].rearrange("p (h d) -> p h d", h=4),
                                              in0=qiw[0:NSB, 0:256].rearrange("p (h d) -> p h d", h=4),
                                              in1=pj[:, 1024:1088].unsqueeze(1).to_broadcast([NSB, 4, 64]), op=ALU.mult),
             [t_qiw, t_proj], [t_scr2])
        P.op("dve", lambda e: e.tensor_reduce(out=sv[:, 16:20], in_=scr2[0:NSB, 0:256].rearrange("p (h d) -> p h d", h=4),
                                              axis=AX.X, op=ALU.add), [t_scr2], [t_ss2])
        P.op("dve", lambda e: e.tensor_scalar(out=sv[:, 16:20], in0=sv[:, 16:20], scalar1=0.0, scalar2=None, op0=ALU.max),
             [t_ss2], [t_ss2])
        P.op("dve", lambda e: e.tensor_tensor(out=sv[:, 16:20], in0=sv[:, 16:20], in1=qiw[0:NSB, 256:260], op=ALU.mult),
             [t_ss2, t_qiw], [t_ss2])
        P.op("dve", lambda e: e.tensor_reduce(out=scores[0:NSB, 8192:8193], in_=sv[:, 16:20], axis=AX.X, op=ALU.add),
             [t_ss2], [t_scores])
        osT, t_osT = sbt("s_osT", [128, NSB, 8], BF16)
        K_TOP = k.top
        k.K_TOP = K_TOP
        kid = [sbt("s_kid%d" % i, [128, 8192]) for i in range(2)]
        prod, t_prod = sbt("s_prod", [128, 8192])
        ptc = [sbt("s_ptc%d" % i, [128, 1], I32) for i in range(2)]
        qrep, t_qrep = sbt("s_qrep", [128, 260])
        zz, t_zz = sbt("s_zz", [128, 128])
        sc1, t_sc1 = sbt("s_sc1", [128, 128])
        sc2 = [sbt("s_sc2_%d" % i, [128, 128]) for i in range(2)]
        kidx_pages = cache_kidx_d

        def pair(q):
            kd, tkd = kid[q % 2]
            pc, tpc = ptc[q % 2]
            P.dma("sp", pc, ptab[2 * q:2 * q + 2, :].rearrange("s (j o) -> (s j) o", o=1), writes=[tpc])
            P.dma("pool", kd, kidx_pages, reads=[tpc], writes=[tkd],
                  indirect=bass.IndirectOffsetOnAxis(ap=pc, axis=0))
            P.op("pe", lambda e: e.matmul(ps[1][:, 0:260], lhsT=selp[0:NSB, q, :], rhs=qiw[0:NSB, :], start=True, stop=True),
                 [t_selp, t_qiw], [t_ps[1]])
            P.op("act", lambda e: e.activation(out=qrep, in_=ps[1][:, 0:260], func=AF.Copy), [t_ps[1]], [t_qrep])
            so_, tso = sc2[q % 2]
            for h in range(4):
                P.op("pool", lambda e, h=h: e.tensor_tensor(
                    out=prod.rearrange("p (o d) -> p o d", d=64), in0=kd.rearrange("p (o d) -> p o d", d=64),
                    in1=qrep[:, h * 64:(h + 1) * 64].unsqueeze(1).to_broadcast([128, 128, 64]), op=ALU.mult),
                    [tkd, t_qrep], [t_prod])
                P.op("dve", lambda e: e.tensor_reduce(out=zz, in_=prod.rearrange("p (o d) -> p o d", d=64), axis=AX.X,
                                                      op=ALU.add), [t_prod], [t_zz])
                if h == 0:
                    P.op("dve", lambda e: e.tensor_scalar(out=so_, in0=zz, scalar1=0.0, scalar2=qrep[:, 256:257],
                                                          op0=ALU.max, op1=ALU.mult), [t_zz, t_qrep], [tso])
                else:
                    P.op("dve", lambda e, h=h: e.tensor_scalar(out=sc1, in0=zz, scalar1=0.0, scalar2=qrep[:, 256 + h:257 + h],
                                                               op0=ALU.max, op1=ALU.mult), [t_zz, t_qrep], [t_sc1])
                    P.op("dve", lambda e: e.tensor_tensor(out=so_, in0=so_, in1=sc1, op=ALU.add), [tso, t_sc1], [tso])
            for s2 in range(2):
                r = 2 * q + s2
                P.dma("sp", scores[r:r + 1, 0:8192].rearrange("p (j o) -> p j o", o=128), so_[64 * s2:64 * s2 + 64, :],
                      reads=[tso], writes=[t_scores])

        for q in range(NSB // 2):
            pair(q)
        P.barrier()
        k.top = K_TOP
        mx, t_mx = sbt("s_mx", [128, 256])
        ix, t_ix = sbt("s_ix", [128, 256], U32)
        TK_TOP = k.top
        W, t_W = sbt("s_W", [128, 8200])
        Wv = W[0:NSB, 0:8193]
        P.op("pool", lambda e: e.tensor_copy(out=Wv, in_=scores[0:NSB, 0:8193]), [t_scores], [t_W])
        for r in range(32):
            P.op("dve", lambda e, r=r: e.max(out=mx[0:NSB, 8 * r:8 * r + 8], in_=Wv), [t_W], [t_mx])
            P.op("dve", lambda e, r=r: e.max_index(out=ix[0:NSB, 8 * r:8 * r + 8], in_max=mx[0:NSB, 8 * r:8 * r + 8],
                                                   in_values=Wv), [t_W, t_mx], [t_ix])
            P.op("dve", lambda e, r=r: e.match_replace(out=Wv, in_to_replace=mx[0:NSB, 8 * r:8 * r + 8], in_values=Wv,
                                                       imm_value=-1e30), [t_W, t_mx], [t_W])
        P.barrier()
        k.top = TK_TOP
        ixf, t_ixf = sbt("s_ixf", [128, 256])
        pgu, t_pgu = sbt("s_pgu", [128, 256], U32)
        pgf, t_pgf = sbt("s_pgf", [128, 256])
        offf, t_offf = sbt("s_offf", [128, 256])
        eq, t_eq = sbt("s_eq", [128, 256, 64])
        phys, t_phys = sbt("s_phys", [128, 256])
        isf, t_isf = sbt("s_isf", [128, 256])
        n_ = lambda a: a[0:NSB]
        P.op("dve", lambda e: e.tensor_copy(out=n_(ixf), in_=n_(ix)), [t_ix], [t_ixf])
        P.op("dve", lambda e: e.tensor_scalar(out=n_(pgu), in0=n_(ix), scalar1=7, scalar2=None, op0=ALU.logical_shift_right),
             [t_ix], [t_pgu])
        P.op("dve", lambda e: e.tensor_copy(out=n_(pgf), in_=n_(pgu)), [t_pgu], [t_pgf])
        P.op("dve", lambda e: e.scalar_tensor_tensor(out=n_(offf), in0=n_(pgf), scalar=-128.0, in1=n_(ixf), op0=ALU.mult,
                                                     op1=ALU.add), [t_pgf, t_ixf], [t_offf])
        P.op("dve", lambda e: e.tensor_tensor(out=n_(eq), in0=n_(pgf).unsqueeze(2).to_broadcast([NSB, 256, 64]),
                                              in1=n_(iota_b).unsqueeze(1).to_broadcast([NSB, 256, 64]), op=ALU.is_equal),
             [t_pgf, t_iota], [t_eq])
        P.op("dve", lambda e: e.tensor_tensor(out=n_(eq), in0=n_(eq), in1=n_(pt_f).unsqueeze(1).to_broadcast([NSB, 256, 64]),
                                              op=ALU.mult), [t_eq, t_ptf], [t_eq])
        P.op("dve", lambda e: e.tensor_reduce(out=n_(phys), in_=n_(eq), axis=AX.X, op=ALU.add), [t_eq], [t_phys])
        P.op("dve", lambda e: e.scalar_tensor_tensor(out=n_(phys), in0=n_(phys), scalar=128.0, in1=n_(offf), op0=ALU.mult,
                                                     op1=ALU.add), [t_phys, t_offf], [t_phys])
        P.op("dve", lambda e: e.tensor_scalar(out=n_(isf), in0=n_(ixf), scalar1=8191.5, scalar2=None, op0=ALU.is_ge),
             [t_ixf], [t_isf])
        physT, t_physT = sbt("s_physT", [128, 2, NSB], I32)
        isT, t_isT = sbt("s_isT", [128, 2, NSB])
        for b in range(2):
            P.op("pe", lambda e, b=b: e.transpose(out=ps[1][:, b * NSB:(b + 1) * NSB], in_=phys[0:NSB, b * 128:(b + 1) * 128],
                                                  identity=identf[0:NSB, 0:NSB]), [t_phys, t_identf], [t_ps[1]])
            P.op("pe", lambda e, b=b: e.transpose(out=ps[2][:, b * NSB:(b + 1) * NSB], in_=isf[0:NSB, b * 128:(b + 1) * 128],
                                                  identity=identf[0:NSB, 0:NSB]), [t_isf, t_identf], [t_ps[2]])
        P.op("dve", lambda e: e.tensor_copy(out=physT.rearrange("p b i -> p (b i)"), in_=ps[1][:, 0:2 * NSB]), [t_ps[1]], [t_physT])
        P.op("act", lambda e: e.activation(out=isT.rearrange("p b i -> p (b i)"), in_=ps[2][:, 0:2 * NSB], func=AF.Copy),
             [t_ps[2]], [t_isT])
        Kg = [sbt("s_Kg%d" % i, [128, 2, 128]) for i in range(2)]
        Vg = [sbt("s_Vg%d" % i, [128, 2, 128]) for i in range(2)]
        kvrep, t_kvrep = sbt("s_kvrep", [128, 256])
        dif, t_dif = sbt("s_dif", [128, 128])
        prd, t_prd = sbt("s_prd", [128, 512])
        lg, t_lg = sbt("s_lg", [128, 2, 8])
        rcp, t_rcp = sbt("s_rcp", [128, NSB * 8])

        def att(i):
            kg, tkg = Kg[i % 2]
            vg, tvg = Vg[i % 2]
            for b in range(2):
                P.dma("pool", kg[:, b, :], cache_k_d, reads=[t_physT], writes=[tkg],
                      indirect=bass.IndirectOffsetOnAxis(ap=physT[:, b, i:i + 1], axis=0))
                P.dma("pool", vg[:, b, :], cache_v_d, reads=[t_physT], writes=[tvg],
                      indirect=bass.IndirectOffsetOnAxis(ap=physT[:, b, i:i + 1], axis=0))
            P.op("pe", lambda e: e.matmul(ps[3], lhsT=selo[0:NSB, i, :], rhs=qn[0:NSB, :], start=True, stop=True),
                 [t_selo, t_qn], [t_ps[3]])
            P.op("pe", lambda e: e.matmul(ps[4][:, 0:256], lhsT=selo[0:NSB, i, :], rhs=sso[0:NSB, 0:256], start=True, stop=True),
                 [t_selo, t_sso], [t_ps[4]])
            P.op("act", lambda e: e.activation(out=kvrep, in_=ps[4][:, 0:256], func=AF.Copy), [t_ps[4]], [t_kvrep])
            for b in range(2):
                for (t_, tt_, c0) in ((kg, tkg, 0), (vg, tvg, 128)):
                    P.op("dve", lambda e, t_=t_, c0=c0, b=b: e.tensor_tensor(out=dif, in0=kvrep[:, c0:c0 + 128], in1=t_[:, b, :],
                                                                             op=ALU.subtract), [t_kvrep, tt_], [t_dif])
                    P.op("dve", lambda e, t_=t_, b=b: e.scalar_tensor_tensor(out=t_[:, b, :], in0=dif, scalar=isT[:, b, i:i + 1],
                                                                             in1=t_[:, b, :], op0=ALU.mult, op1=ALU.add),
                         [t_dif, t_isT, tt_], [tt_])
                P.op("dve", lambda e, b=b: e.tensor_tensor(
                    out=prd.rearrange("p (g r d) -> p g r d", g=2, r=4),
                    in0=ps[3].rearrange("p (g r d) -> p g r d", g=2, r=4),
                    in1=kg[:, b, :].rearrange("p (g d) -> p g d", g=2).unsqueeze(2).to_broadcast([128, 2, 4, 64]),
                    op=ALU.mult), [t_ps[3], tkg], [t_prd])
                P.op("dve", lambda e, b=b: e.tensor_reduce(out=lg[:, b, :], in_=prd.rearrange("p (h d) -> p h d", h=8),
                                                           axis=AX.X, op=ALU.add), [t_prd], [t_lg])
            P.op("act", lambda e: e.activation(out=lg, in_=lg, func=AF.Exp), [t_lg], [t_lg])
            for g in range(2):
                for b in range(2):
                    P.op("pe", lambda e, g=g, b=b: e.matmul(ps[5][0:64, i * 8 + g * 4:i * 8 + g * 4 + 4],
                                                            lhsT=vg[:, b, g * 64:(g + 1) * 64], rhs=lg[:, b, g * 4:(g + 1) * 4],
                                                            start=(b == 0), stop=(b == 1)), [tvg, t_lg], [t_ps[5]])
                for b in range(2):
                    P.op("pe", lambda e, g=g, b=b: e.matmul(ps[6][0:64, i * 8 + g * 4:i * 8 + g * 4 + 4],
                                                            lhsT=onesf[:, 0:64], rhs=lg[:, b, g * 4:(g + 1) * 4],
                                                            start=(b == 0), stop=(b == 1)), [t_onesf, t_lg], [t_ps[6]])

        for i in range(NSB):
            att(i)
        P.op("dve", lambda e: e.reciprocal(out=rcp[0:64], in_=ps[6][0:64, 0:NSB * 8]), [t_ps[6]], [t_rcp])
        P.op("dve", lambda e: e.tensor_tensor(out=osT[0:64].rearrange("p i h -> p (i h)"), in0=ps[5][0:64, 0:NSB * 8],
                                              in1=rcp[0:64], op=ALU.mult), [t_ps[5], t_rcp], [t_osT])
        k.osT = (osT, t_osT)
        k.selo = (selo, t_selo)
        k.S2_TOP = k.top
        P.barrier()

    def sample_tail(proj, t_proj, x_keep, t_xk):
        NSB = NS
        pj = proj[0:NSB]
        osT, t_osT = k.osT
        selo, t_selo = k.selo
        k.top = k.K_TOP
        alloc_stage(1024)
        Wo_s, t_Wos = sbt("t_Wo_s", [128, 8, 1024], BF16)
        Wo_g, t_Wog = sbt("t_Wo_g", [128, 4, 1024], BF16)
        Wmq, t_Wmq = sbt("t_Wmq", [128, 8, 256], BF16)
        Wmo, t_Wmo = sbt("t_Wmo", [128, 4, 1024], BF16)
        for h in range(8):
            i = k.wl % 2
            k.wl += 1
            st = stage[i][0:64, 0:1024]
            P.dma("sp", st, w_out[64 * h:64 * h + 64, :], writes=[t_stage[i]])
            P.op("act", lambda e, st=st, h=h: e.activation(out=Wo_s[0:64, h, :], in_=st, func=AF.Copy), [t_stage[i]], [t_Wos])
        for h in range(4):
            load_rows(Wo_g[:, h, :], t_Wog, w_out[512 + 128 * h:512 + 128 * h + 128, :], 1024, None, None)
        load_weight(Wmq, t_Wmq, w_mq, 0, 256, gX, t_gX)
        for h in range(4):
            i = k.wl % 2
            k.wl += 1
            st = stage[i][0:64, 0:1024]
            P.dma("sp", st, w_mo[64 * h:64 * h + 64, :], writes=[t_stage[i]])
            P.op("act", lambda e, st=st, h=h: e.activation(out=Wmo[0:64, h, :], in_=st, func=AF.Copy), [t_stage[i]], [t_Wmo])
        gmq_b, t_gmqb = bcast_layout("t_gmq_b", g_mq, 64)
        ogT, t_ogT = sbt("t_ogT", [128, 4, NSB], BF16)
        for h in range(4):
            P.op("pe", lambda e, h=h: e.transpose(out=ps[1][:, h * NSB:(h + 1) * NSB], in_=pj[:, 1092 + h * 128:1092 + (h + 1) * 128],
                                                  identity=identf[0:NSB, 0:NSB]), [t_proj, t_identf], [t_ps[1]])
        P.op("act", lambda e: e.activation(out=ogT.rearrange("p h i -> p (h i)"), in_=ps[1][:, 0:4 * NSB], func=AF.Copy),
             [t_ps[1]], [t_ogT])
        x1, t_x1 = sbt("t_x1", [128, D])
        P.op("pool", lambda e: e.memset(x1, 0.0), [], [t_x1])
        for c in range(2):
            for h in range(8):
                P.op("pe", lambda e, h=h, c=c: e.matmul(ps[2 + c][0:NSB, :], lhsT=osT[0:64, :, h], rhs=Wo_s[0:64, h, c * 512:(c + 1) * 512],
                                                        start=(h == 0), stop=False), [t_osT, t_Wos], [t_ps[2 + c]])
            for h in range(4):
                P.op("pe", lambda e, h=h, c=c: e.matmul(ps[2 + c][0:NSB, :], lhsT=ogT[:, h, :], rhs=Wo_g[:, h, c * 512:(c + 1) * 512],
                                                        start=False, stop=(h == 3)), [t_ogT, t_Wog], [t_ps[2 + c]])
            P.op("dve", lambda e, c=c: e.tensor_tensor(out=x1[0:NSB, c * 512:(c + 1) * 512], in0=ps[2 + c][0:NSB, :],
                                                       in1=x_keep[0:NSB, c * 512:(c + 1) * 512], op=ALU.add),
                 [t_ps[2 + c], t_xk], [t_x1])
        hT2, t_hT2 = sbt("t_hT2", [128, 8, 128], BF16)
        norm_T(x1, t_x1, 0, hT2, t_hT2, 0, 0)
        for kc in range(8):
            P.op("pe", lambda e, kc=kc: e.matmul(ps[4][0:NSB, 0:256], lhsT=hT2[:, kc, 0:NSB], rhs=Wmq[:, kc, :],
                                                 start=(kc == 0), stop=(kc == 7)), [t_hT2, t_Wmq], [t_ps[4]])
        qm, t_qm = sbt("t_qm", [128, 256])
        sq2, t_sq2 = sbt("t_sq2", [128, 256])
        st2, t_st2 = sbt("t_st2", [128, 16])
        P.op("act", lambda e: e.activation(out=sq2[0:NSB], in_=ps[4][0:NSB, 0:256], func=AF.Square), [t_ps[4]], [t_sq2])
        P.op("dve", lambda e: e.tensor_reduce(out=st2[0:NSB, 0:4], in_=sq2[0:NSB].rearrange("p (h d) -> p h d", h=4), axis=AX.X,
                                              op=ALU.add), [t_sq2], [t_st2])
        P.op("pool", lambda e: e.tensor_scalar(out=st2[0:NSB, 4:8], in0=st2[0:NSB, 0:4], scalar1=1.0 / 64, scalar2=EPS,
                                               op0=ALU.mult, op1=ALU.add), [t_st2], [t_st2])
        P.op("pool", lambda e: e.tensor_tensor(out=st2[0:NSB, 4:8], in0=st2[0:NSB, 4:8], in1=neghalf[0:NSB, 0:4], op=ALU.pow),
             [t_st2, t_nh], [t_st2])
        P.op("pool", lambda e: e.tensor_scalar(out=st2[0:NSB, 4:8], in0=st2[0:NSB, 4:8], scalar1=0.125, scalar2=1.0,
                                               op0=ALU.mult, op1=ALU.mult), [t_st2], [t_st2])
        qm3 = qm[0:NSB].rearrange("p (h d) -> p h d", h=4)
        P.op("dve", lambda e: e.tensor_tensor(out=qm3, in0=ps[4][0:NSB, 0:256].rearrange("p (h d) -> p h d", h=4),
                                              in1=st2[0:NSB, 4:8].unsqueeze(2).to_broadcast([NSB, 4, 64]), op=ALU.mult),
             [t_ps[4], t_st2], [t_qm])
        P.op("pool", lambda e: e.tensor_tensor(out=qm3, in0=qm3, in1=gmq_b[0:NSB].unsqueeze(1).to_broadcast([NSB, 4, 64]),
                                               op=ALU.mult), [t_qm, t_gmqb], [t_qm])
        mkt = [sbt("t_mk%d" % i, [128, 2, 256]) for i in range(2)]
        mvt = [sbt("t_mv%d" % i, [128, 2, 256]) for i in range(2)]
        prm, t_prm = sbt("t_prm", [128, 256])
        lgm, t_lgm = sbt("t_lgm", [128, 2, 4])
        omT, t_omT = sbt("t_omT", [128, NSB, 4], BF16)
        rcm, t_rcm = sbt("t_rcm", [128, NSB * 4])

        def xatt(i):
            mk_, tmk = mkt[i % 2]
            mv_, tmv = mvt[i % 2]
            P.dma("sp", mk_, cmk_d[i * 256:(i + 1) * 256, :].rearrange("(t p) c -> p t c", p=128), writes=[tmk])
            P.dma("sp", mv_, cmv_d[i * 256:(i + 1) * 256, :].rearrange("(t p) c -> p t c", p=128), writes=[tmv])
            P.op("pe", lambda e: e.matmul(ps[5][:, 0:256], lhsT=selo[0:NSB, i, :], rhs=qm[0:NSB, :], start=True, stop=True),
                 [t_selo, t_qm], [t_ps[5]])
            for mt in range(2):
                P.op("dve", lambda e, mt=mt: e.tensor_tensor(out=prm, in0=ps[5][:, 0:256], in1=mk_[:, mt, :], op=ALU.mult),
                     [t_ps[5], tmk], [t_prm])
                P.op("dve", lambda e, mt=mt: e.tensor_reduce(out=lgm[:, mt, :], in_=prm.rearrange("p (h d) -> p h d", h=4),
                                                             axis=AX.X, op=ALU.add), [t_prm], [t_lgm])
            P.op("act", lambda e: e.activation(out=lgm, in_=lgm, func=AF.Exp), [t_lgm], [t_lgm])
            for h in range(4):
                for mt in range(2):
                    P.op("pe", lambda e, h=h, mt=mt: e.matmul(ps[6][0:64, i * 4 + h:i * 4 + h + 1], lhsT=mv_[:, mt, h * 64:(h + 1) * 64],
                                                              rhs=lgm[:, mt, h:h + 1], start=(mt == 0), stop=(mt == 1)),
                         [tmv, t_lgm], [t_ps[6]])
                for mt in range(2):
                    P.op("pe", lambda e, h=h, mt=mt: e.matmul(ps[7][0:64, i * 4 + h:i * 4 + h + 1], lhsT=onesf[:, 0:64],
                                                              rhs=lgm[:, mt, h:h + 1], start=(mt == 0), stop=(mt == 1)),
                         [t_onesf, t_lgm], [t_ps[7]])

        for i in range(NSB):
            xatt(i)
        P.op("dve", lambda e: e.reciprocal(out=rcm[0:64], in_=ps[7][0:64, 0:NSB * 4]), [t_ps[7]], [t_rcm])
        P.op("dve", lambda e: e.tensor_tensor(out=omT[0:64].rearrange("p i h -> p (i h)"), in0=ps[6][0:64, 0:NSB * 4],
                                              in1=rcm[0:64], op=ALU.mult), [t_ps[6], t_rcm], [t_omT])
        for c in range(2):
            for h in range(4):
                P.op("pe", lambda e, h=h, c=c: e.matmul(ps[2 + c][0:NSB, :], lhsT=omT[0:64, :, h], rhs=Wmo[0:64, h, c * 512:(c + 1) * 512],
                                                        start=(h == 0), stop=(h == 3)), [t_omT, t_Wmo], [t_ps[2 + c]])
            P.op("dve", lambda e, c=c: e.tensor_tensor(out=x1[0:NSB, c * 512:(c + 1) * 512], in0=ps[2 + c][0:NSB, :],
                                                       in1=x1[0:NSB, c * 512:(c + 1) * 512], op=ALU.add),
                 [t_ps[2 + c], t_x1], [t_x1])
        P.op("pool", lambda e: e.tensor_copy(out=x_keep[0:NSB, :], in_=x1[0:NSB, :]), [t_x1], [t_xk])
        hT3, t_hT3 = sbt("t_hT3", [128, 8, 128], BF16)
        norm_T(x1, t_x1, 1, hT3, t_hT3, 0, 0)
        P.barrier()
        k.top = PERSIST_TOP
        xk2, t_xk2 = sbt("t_xk2", [128, D])
        hT4, t_hT4 = sbt("t_hT4", [128, 8, NSB], BF16)
        P.op("pool", lambda e: e.tensor_copy(out=xk2[0:NSB, :], in_=x_keep[0:NSB, :]), [t_xk], [t_xk2])
        P.op("pool", lambda e: e.tensor_copy(out=hT4, in_=hT3[:, :, 0:NSB]), [t_hT3], [t_hT4])
        P.barrier()
        alloc_stage(2816)
        Wg, t_Wg = sbt("t_Wg", [128, 8, 2816], BF16)
        Wu, t_Wu = sbt("t_Wu", [128, 8, 2816], BF16)
        Wd, t_Wd = sbt("t_Wd", [128, 22, 1024], BF16)
        load_weight(Wg, t_Wg, w_gate, 0, 2816, gF, t_gF)
        load_weight(Wu, t_Wu, w_up, 0, 2816, gF, t_gF)
        for f in range(22):
            load_rows(Wd[:, f, :], t_Wd, w_down[128 * f:128 * f + 128, :], 1024, None, None)
        hf, t_hf = sbt("t_hf", [128, 2816])
        sgs, t_sgs = sbt("t_sgs", [128, 512])
        hfT, t_hfT = sbt("t_hfT", [128, 22, NSB], BF16)
        for c in range(6):
            c0 = c * 512
            n = min(512, 2816 - c0)
            for kc in range(8):
                P.op("pe", lambda e, kc=kc, c0=c0, n=n: e.matmul(ps[1][0:NSB, 0:n], lhsT=hT4[:, kc, :], rhs=Wg[:, kc, c0:c0 + n],
                                                                 start=(kc == 0), stop=(kc == 7)), [t_hT4, t_Wg], [t_ps[1]])
            for kc in range(8):
                P.op("pe", lambda e, kc=kc, c0=c0, n=n: e.matmul(ps[2][0:NSB, 0:n], lhsT=hT4[:, kc, :], rhs=Wu[:, kc, c0:c0 + n],
                                                                 start=(kc == 0), stop=(kc == 7)), [t_hT4, t_Wu], [t_ps[2]])
            P.op("act", lambda e, n=n: e.activation(out=sgs[0:NSB, 0:n], in_=ps[1][0:NSB, 0:n], func=AF.Silu), [t_ps[1]], [t_sgs])
            P.op("dve", lambda e, c0=c0, n=n: e.tensor_tensor(out=hf[0:NSB, c0:c0 + n], in0=sgs[0:NSB, 0:n], in1=ps[2][0:NSB, 0:n],
                                                              op=ALU.mult), [t_sgs, t_ps[2]], [t_hf])
        for f in range(22):
            b = 3 + (f % 2)
            P.op("pe", lambda e, f=f, b=b: e.transpose(out=ps[b][:, 0:NSB], in_=hf[0:NSB, f * 128:(f + 1) * 128],
                                                       identity=identf[0:NSB, 0:NSB]), [t_hf, t_identf], [t_ps[b]])
            P.op("act", lambda e, f=f, b=b: e.activation(out=hfT[:, f, :], in_=ps[b][:, 0:NSB], func=AF.Copy), [t_ps[b]], [t_hfT])
        for c in range(2):
            for f in range(22):
                P.op("pe", lambda e, f=f, c=c: e.matmul(ps[5 + c][0:NSB, :], lhsT=hfT[:, f, :], rhs=Wd[:, f, c * 512:(c + 1) * 512],
                                                        start=(f == 0), stop=(f == 21)), [t_hfT, t_Wd], [t_ps[5 + c]])
            P.op("dve", lambda e, c=c: e.tensor_tensor(out=xk2[0:NSB, c * 512:(c + 1) * 512], in0=ps[5 + c][0:NSB, :],
                                                       in1=xk2[0:NSB, c * 512:(c + 1) * 512], op=ALU.add),
                 [t_ps[5 + c], t_xk2], [t_xk2])
        P.dma("sp", y_s[:, :], xk2[0:NSB, :], reads=[t_xk2])

    if STOP >= 0:
        for sq in range(NSEQ):
            prompt_seq(sq)
    if STOP < 0 or STOP >= 99:
        sample_group()

    P.finish()
    P.emit()
    return nc


_CACHE = {}


def _get_nc(nseq, stop, npool=10240):
    key = (nseq, stop, npool)
    if key not in _CACHE:
        _CACHE[key] = build(nseq, STOP=stop, NPOOL=npool)
    return _CACHE[key]


def kernel(x_prompt, x_sample, mem_prompt, cache_k, cache_v, cache_kidx, page_table,
           state_conv, state_ssm, cache_mem_k, cache_mem_v,
           attn_norm_g, w_in, q_norm_g, k_norm_g, conv_w, a_log, dt_bias, gdn_norm_g, w_out,
           xattn_norm_g, mem_norm_g, w_mq, w_mk, w_mv, mq_norm_g, mk_norm_g, w_mo,
           ffn_norm_g, w_gate, w_up, w_down, _ncores=NCORES, _stop=99):
    B = x_prompt.shape[0]
    nseq = B // _ncores
    NS = x_sample.shape[0] // _ncores
    nc = _get_nc(nseq, _stop, cache_k.shape[1])
    f = lambda a: np.ascontiguousarray(np.asarray(a, dtype=np.float32))
    ii = np.arange(128)
    consts = {
        "ident": np.eye(128, dtype=np.float32),
        "trile": (ii[:, None] <= ii[None, :]).astype(np.float32),
        "sgt": (ii[:, None] > ii[None, :]).astype(np.float32),
        "pow2": (2.0 ** -np.arange(32)).astype(np.float32),
        "eye16": np.eye(16, dtype=np.float32).reshape(256),
        "selpair": np.stack([(np.arange(16)[:, None] == (2 * q_ + np.arange(128)[None, :] // 64)).astype(np.float32)
                             for q_ in range(8)]),
        "selone": np.stack([np.repeat((np.arange(16) == i_)[:, None], 128, axis=1).astype(np.float32)
                            for i_ in range(16)], axis=1),
        "iota64": np.arange(64, dtype=np.float32),
    }
    shared = {
        "attn_norm_g": f(attn_norm_g[0]), "w_in": f(w_in[0]),
        "q_norm_g": f(q_norm_g[0]), "k_norm_g": f(k_norm_g[0]),
        "conv_w": f(conv_w[0]), "a_log": f(a_log[0]), "dt_bias": f(dt_bias[0]),
        "gdn_norm_g": f(gdn_norm_g[0]), "w_out": f(w_out[0]), "xattn_norm_g": f(xattn_norm_g[0]),
        "mem_norm_g": f(mem_norm_g[0]), "w_mq": f(w_mq[0]), "w_mk": f(w_mk[0]), "w_mv": f(w_mv[0]),
        "mq_norm_g": f(mq_norm_g[0]), "mk_norm_g": f(mk_norm_g[0]), "w_mo": f(w_mo[0]),
        "ffn_norm_g": f(ffn_norm_g[0]), "w_gate": f(w_gate[0]), "w_up": f(w_up[0]), "w_down": f(w_down[0]),
    }
    npool = cache_k.shape[1]
    ck_k = f(cache_k[0]).reshape(npool * 128, 128)
    ck_v = f(cache_v[0]).reshape(npool * 128, 128)
    ck_idx = f(cache_kidx[0]).reshape(npool, 8192)
    in_maps = []
    for c in range(_ncores):
        m = {
            "xp": f(x_prompt[c * nseq:(c + 1) * nseq]).reshape(nseq * SEQ, D),
            "memp": f(mem_prompt[c * nseq:(c + 1) * nseq]).reshape(nseq * MEM, D),
            "xs": f(x_sample[c * NS:(c + 1) * NS]).reshape(NS, D),
            "st_conv": f(state_conv[0, c * NS:(c + 1) * NS]).reshape(NS * 3, 1536),
            "ssm_in": f(state_ssm[0, c * NS:(c + 1) * NS]).reshape(NS * 512, 128),
            "ptab": np.ascontiguousarray(np.asarray(page_table[c * NS:(c + 1) * NS], dtype=np.int32)),
            "cmk": f(cache_mem_k[0, c * NS:(c + 1) * NS]).reshape(NS * 256, 256),
            "cmv": f(cache_mem_v[0, c * NS:(c + 1) * NS]).reshape(NS * 256, 256),
            "cache_kidx": ck_idx, "cache_k": ck_k, "cache_v": ck_v,
        }
        m.update(consts)
        m.update(shared)
        in_maps.append(m)
    res = run_bass_kernel_spmd(nc, in_maps, core_ids=list(range(_ncores))).results
    cat = lambda name: np.concatenate([r[name] for r in res], axis=0)
    SB = x_sample.shape[0]
    outs = (
        cat("y_p").reshape(B, SEQ, D),
        cat("y_s").reshape(SB, 1, D),
        cat("k_p").reshape(1, B, SEQ, 2, 64),
        cat("v_p").reshape(1, B, SEQ, 2, 64),
        cat("kidx_p").reshape(1, B, SEQ, 64),
        cat("conv_p").reshape(1, B, 3, 1536),
        cat("ssm_p").reshape(1, B, 4, 128, 128),
        cat("memk_p").reshape(1, B, MEM, 4, 64),
        cat("memv_p").reshape(1, B, MEM, 4, 64),
        cat("k_s").reshape(1, SB, 1, 2, 64),
        cat("v_s").reshape(1, SB, 1, 2, 64),
        cat("kidx_s").reshape(1, SB, 1, 64),
        cat("conv_s").reshape(1, SB, 3, 1536),
        cat("ssm_s").reshape(1, SB, 4, 128, 128),
    )
    return outs
```

```python
import os
import numpy as np
import concourse.bass as bass
import concourse.mybir as mybir
from concourse.bass_utils import run_bass_kernel_spmd

F32 = mybir.dt.float32
BF16 = mybir.dt.bfloat16
I32 = mybir.dt.int32
AF = mybir.ActivationFunctionType
ALU = mybir.AluOpType
AX = mybir.AxisListType

NCORES = 8
D = 1024
SEQ = 2048
NT = SEQ // 128
MEM = 256
INW = 3148
EPS = 1e-6
NDS = 32


class Tok:
    __slots__ = ("w", "r")

    def __init__(self):
        self.w = None
        self.r = {}


class Prog:
    def __init__(self, nc):
        self.nc = nc
        self.names = ["pe", "act", "dve", "pool", "sp"]
        self.sems = []
        self.esem = {}
        for k in self.names:
            self.esem[k] = len(self.sems)
            self.sems.append(nc.alloc_semaphore("es_" + k))
        self.dsem = []
        for i in range(NDS):
            self.dsem.append(len(self.sems))
            self.sems.append(nc.alloc_semaphore("ds_%d" % i))
        self.dval = [0] * NDS
        self.dnext = 0
        self.dnext_sw = 0
        self.cnt = {k: 0 for k in self.names}
        self.seen = {k: {} for k in self.names}
        self.th = {k: [] for k in self.names}

    def _deps(self, e, reads, writes, extra=()):
        d = {}

        def add(s, v):
            if d.get(s, 0) < v:
                d[s] = v

        for t in reads:
            if t.w is not None:
                add(*t.w)
        for t in writes:
            if t.w is not None:
                add(*t.w)
            for s, v in t.r.items():
                add(s, v)
        for s, v in extra:
            add(s, v)
        out = []
        for s, v in d.items():
            if e == "pe" and s == self.esem["pe"]:
                continue
            if self.seen[e].get(s, 0) >= v:
                continue
            self.seen[e][s] = v
            out.append((s, v))
        return out

    def op(self, e, fn, reads=(), writes=()):
        waits = self._deps(e, reads, writes)
        self.cnt[e] += 1
        n = self.cnt[e]
        s = self.esem[e]
        self.th[e].append((waits, fn, s, 1))
        for t in reads:
            if t.r.get(s, 0) < n:
                t.r[s] = n
        for t in writes:
            t.w = (s, n)
            t.r = {}

    def dma(self, q, out, in_, reads=(), writes=(), **kw):
        if q == "pool":
            i = NDS - 8 + self.dnext_sw
            self.dnext_sw = (self.dnext_sw + 1) % 8
        else:
            i = self.dnext
            self.dnext = (self.dnext + 1) % (NDS - 8)
        s = self.dsem[i]
        extra = [(s, self.dval[i])] if self.dval[i] else []
        waits = self._deps(q, reads, writes, extra)
        self.dval[i] += 16
        v = self.dval[i]
        if "indirect" in kw:
            ioff = kw.pop("indirect")
            self.th[q].append((waits, lambda eng: eng.indirect_dma_start(out=out, out_offset=None, in_=in_,
                                                                         in_offset=ioff), s, 16))
        else:
            self.th[q].append((waits, lambda eng: eng.dma_start(out=out, in_=in_, **kw), s, 16))
        for t in reads:
            t.r[s] = v
        for t in writes:
            t.w = (s, v)
            t.r = {}

    def barrier(self):
        tgt = []
        for i in range(NDS):
            if self.dval[i]:
                tgt.append((self.dsem[i], self.dval[i]))
        for k in self.names:
            if self.cnt[k]:
                tgt.append((self.esem[k], self.cnt[k]))
        for e in self.names:
            waits = []
            for s_, v in tgt:
                if s_ == self.esem[e] and e in ("pe", "sp"):
                    continue
                if self.seen[e].get(s_, 0) >= v:
                    continue
                self.seen[e][s_] = v
                waits.append((s_, v))
            self.th[e].append((waits, None, None, 0))

    def finish(self):
        waits = []
        for i in range(NDS):
            if self.dval[i]:
                waits.append((self.dsem[i], self.dval[i]))
        for k in self.names:
            if k != "sp" and self.cnt[k]:
                waits.append((self.esem[k], self.cnt[k]))
        self.th["sp"].append((waits, None, None, 0))

    def emit(self):
        nc = self.nc
        sems = self.sems

        def run(eng, lst):
            for waits, fn, s, inc in lst:
                for ws, wv in waits:
                    eng.wait_ge(sems[ws], wv)
                if fn is not None:
                    fn(eng).then_inc(sems[s], inc)

        with nc.Block() as block:
            @block.tensor
            def _(e):
                run(e, self.th["pe"])

            @block.scalar
            def _(e):
                run(e, self.th["act"])

            @block.vector
            def _(e):
                run(e, self.th["dve"])

            @block.gpsimd
            def _(e):
                run(e, self.th["pool"])

            @block.sync
            def _(e):
                run(e, self.th["sp"])


class K:
    pass


def build(NSEQ, NS=16, STOP=99, NPOOL=10240):
    GCUT = int(os.environ.get('GCUT', '99'))
    DCUT = int(os.environ.get('DCUT', '99'))
    PCUT = int(os.environ.get('PCUT', '99'))
    CCUT = int(os.environ.get('CCUT', '99'))
    nc = bass.Bass("TRN2", target_bir_lowering=False)
    P = Prog(nc)
    k = K()

    def din(name, shape, dt=F32):
        return nc.dram_tensor(name, list(shape), dt, kind="ExternalInput").ap()

    def dout(name, shape, dt=F32):
        return nc.dram_tensor(name, list(shape), dt, kind="ExternalOutput").ap()

    ARENA_W = 53000
    arena = nc.alloc_sbuf_tensor("arena", [128, ARENA_W], F32).ap()
    k.top = 0

    def sb(name, shape, dt=F32):
        n = 1
        for d_ in shape[1:]:
            n *= d_
        words = n if dt in (F32, I32, mybir.dt.uint32) else (n + 1) // 2
        words = (words + 7) // 8 * 8
        off = k.top
        k.top += words
        assert k.top <= ARENA_W, ("SBUF arena overflow", name, k.top)
        a = arena[:, off:off + words]
        if dt != F32:
            a = a.bitcast(dt)
        a = a[:, 0:n]
        if len(shape) > 2:
            names = " ".join("d%d" % i for i in range(len(shape) - 1))
            kw = {"d%d" % i: shape[i + 1] for i in range(len(shape) - 2)}
            a = a.rearrange("p (%s) -> p %s" % (names, names), **kw)
        if shape[0] != 128:
            a = a[0:shape[0]]
        return a

    def sbt(name, shape, dt=F32):
        return sb(name, shape, dt), Tok()

    xp = din("xp", [NSEQ * SEQ, D])
    memp = din("memp", [NSEQ * MEM, D])
    xs = din("xs", [NS, D])
    st_conv = din("st_conv", [NS * 3, 1536])
    ssm_in = din("ssm_in", [NS * 512, 128])
    eye16_d = din("eye16", [256])
    selpair_d = din("selpair", [8, NS, 128])
    selone_d = din("selone", [NS, NS, 128])
    iota64_d = din("iota64", [64])
    ptab = din("ptab", [NS, 64], I32)
    cache_kidx_d = din("cache_kidx", [NPOOL, 8192])
    cache_k_d = din("cache_k", [NPOOL * 128, 128])
    cache_v_d = din("cache_v", [NPOOL * 128, 128])
    cmk_d = din("cmk", [NS * 256, 256])
    cmv_d = din("cmv", [NS * 256, 256])
    ident_d = din("ident", [128, 128])
    trile_d = din("trile", [128, 128])
    sgt_d = din("sgt", [128, 128])
    pow2_d = din("pow2", [32])
    g_attn = din("attn_norm_g", [D])
    w_in = din("w_in", [D, INW])
    g_q = din("q_norm_g", [64])
    g_k = din("k_norm_g", [64])
    conv_w = din("conv_w", [4, 1536])
    a_log = din("a_log", [4])
    dt_bias = din("dt_bias", [4])
    g_gdn = din("gdn_norm_g", [128])
    w_out = din("w_out", [D, D])
    g_x = din("xattn_norm_g", [D])
    g_mem = din("mem_norm_g", [D])
    w_mq = din("w_mq", [D, 256])
    w_mk = din("w_mk", [D, 256])
    w_mv = din("w_mv", [D, 256])
    g_mq = din("mq_norm_g", [64])
    g_mk = din("mk_norm_g", [64])
    w_mo = din("w_mo", [256, D])
    g_f = din("ffn_norm_g", [D])
    w_gate = din("w_gate", [D, 2816])
    w_up = din("w_up", [D, 2816])
    w_down = din("w_down", [2816, D])

    y_p = dout("y_p", [NSEQ * SEQ, D])
    y_s = dout("y_s", [NS, D])
    k_p = dout("k_p", [NSEQ * SEQ, 128])
    v_p = dout("v_p", [NSEQ * SEQ, 128])
    kidx_p = dout("kidx_p", [NSEQ * SEQ, 64])
    conv_p = dout("conv_p", [NSEQ * 3, 1536])
    ssm_p = dout("ssm_p", [NSEQ * 512, 128])
    memk_p = dout("memk_p", [NSEQ * MEM, 256])
    memv_p = dout("memv_p", [NSEQ * MEM, 256])
    k_s = dout("k_s", [NS, 128])
    v_s = dout("v_s", [NS, 128])
    kidx_s = dout("kidx_s", [NS, 64])
    conv_s = dout("conv_s", [NS * 3, 1536])
    ssm_s = dout("ssm_s", [NS * 512, 128])

    identf, t_identf = sbt("identf", [128, 128])
    identb, t_identb = sbt("identb", [128, 128], BF16)
    trile, t_trile = sbt("trile", [128, 128])
    sgt, t_sgt = sbt("sgt", [128, 128])
    onesf, t_onesf = sbt("onesf", [128, 128])
    onesb, t_onesb = sbt("onesb", [128, 128], BF16)
    tribias, t_tribias = sbt("tribias", [128, 128])
    lowinc, t_lowinc = sbt("lowinc", [128, 128])
    P.dma("sp", identf, ident_d, writes=[t_identf])
    P.dma("sp", trile, trile_d, writes=[t_trile])
    P.dma("sp", sgt, sgt_d, writes=[t_sgt])
    P.op("dve", lambda e: e.tensor_copy(out=identb, in_=identf), [t_identf], [t_identb])
    P.op("pool", lambda e: e.memset(onesf, 1.0), [], [t_onesf])
    P.op("pool", lambda e: e.memset(onesb, 1.0), [], [t_onesb])
    P.op("dve", lambda e: e.tensor_tensor(out=lowinc, in0=sgt, in1=identf, op=ALU.add),
         [t_sgt, t_identf], [t_lowinc])
    P.op("dve", lambda e: e.tensor_scalar(out=tribias, in0=lowinc, scalar1=-1.0, scalar2=1e30,
                                          op0=ALU.add, op1=ALU.mult), [t_lowinc], [t_tribias])

    def col_layout(name, src, n):
        t, tok = sbt(name, [128, n])
        P.dma("sp", t, src.rearrange("(c p) -> p c", p=128), writes=[tok],
              allow_slow_non_contiguous=True)
        return t, tok

    def bcast_layout(name, src, n):
        t, tok = sbt(name, [128, n])
        P.dma("sp", t, src.partition_broadcast(128), writes=[tok])
        return t, tok

    gA, t_gA = col_layout("gA", g_attn, 8)
    gM, t_gM = col_layout("gM", g_mem, 8)
    gX, t_gX = col_layout("gX", g_x, 8)
    gF, t_gF = col_layout("gF", g_f, 8)
    gk_b, t_gk = bcast_layout("gk_b", g_k, 64)
    gmk_b, t_gmk = bcast_layout("gmk_b", g_mk, 64)
    ggdn_b, t_ggdn = bcast_layout("ggdn_b", g_gdn, 128)
    dtb_b, t_dtb = bcast_layout("dtb_b", dt_bias, 4)
    nea_b, t_nea = bcast_layout("nea_b", a_log, 4)
    pow2_b, t_pow2 = bcast_layout("pow2_b", pow2_d, 32)
    P.op("act", lambda e: e.activation(out=nea_b, in_=nea_b, func=AF.Exp), [t_nea], [t_nea])
    P.op("dve", lambda e: e.tensor_scalar(out=nea_b, in0=nea_b, scalar1=-1.0, scalar2=None,
                                          op0=ALU.mult), [t_nea], [t_nea])
    gq8, t_gq8 = sbt("gq8", [128, 1])
    gmq8, t_gmq8 = sbt("gmq8", [128, 1])
    for (dst, tdst, src) in ((gq8, t_gq8, g_q), (gmq8, t_gmq8, g_mq)):
        for hh in range(2):
            P.dma("sp", dst[64 * hh:64 * hh + 64, :], src.rearrange("(d o) -> d o", o=1),
                  writes=[tdst], allow_slow_non_contiguous=True)
        P.op("pool", lambda e, dst=dst: e.tensor_scalar(out=dst, in0=dst, scalar1=0.125, scalar2=1.0,
                                                        op0=ALU.mult, op1=ALU.mult), [tdst], [tdst])
    cw, t_cw = sbt("cw", [128, 12, 4])
    for tap in range(4):
        P.dma("sp", cw[:, :, tap], conv_w[tap].rearrange("(j p) -> p j", p=128), writes=[t_cw],
              allow_slow_non_contiguous=True)
    neghalf, t_nh = sbt("neghalf", [128, 8])
    P.op("pool", lambda e: e.memset(neghalf, -0.5), [], [t_nh])

    ps = [nc.alloc_psum_tensor("ps%d" % i, [128, 512], F32).ap() for i in range(8)]
    t_ps = [Tok() for _ in range(8)]
    psb16 = [p_.bitcast(BF16) for p_ in ps]

    stage = [None, None, None]
    t_stage = [Tok(), Tok(), Tok()]
    k.wl = 0
    k.nst = 2

    def alloc_stage(n, nbuf=3):
        k.nst = nbuf
        for i in range(nbuf):
            stage[i] = sb("wstage%d" % i, [128, n], F32)

    def load_rows(dst_kc, t_dst, src_rows, n, gain_col, t_gain):
        i = k.wl % k.nst
        k.wl += 1
        st = stage[i][:, 0:n]
        P.dma("sp", st, src_rows, writes=[t_stage[i]])
        if gain_col is None:
            if k.wl % 2 == 0:
                P.op("act", lambda e: e.activation(out=dst_kc, in_=st, func=AF.Copy),
                     [t_stage[i]], [t_dst])
            else:
                P.op("pool", lambda e: e.tensor_copy(out=dst_kc, in_=st), [t_stage[i]], [t_dst])
        else:
            if k.wl % 2 == 0:
                P.op("act", lambda e: e.activation(out=dst_kc, in_=st, func=AF.Copy, scale=gain_col),
                     [t_stage[i], t_gain], [t_dst])
            else:
                P.op("pool", lambda e: e.tensor_scalar(out=dst_kc, in0=st, scalar1=gain_col, scalar2=1.0,
                                                       op0=ALU.mult, op1=ALU.mult),
                     [t_stage[i], t_gain], [t_dst])

    def load_weight(dst, t_dst, src, c0, c1, gain, t_gain, d0=0):
        n = c1 - c0
        for kc in range(8):
            load_rows(dst[:, kc, d0:d0 + n], t_dst, src[kc * 128:(kc + 1) * 128, c0:c1], n,
                      None if gain is None else gain[:, kc:kc + 1], t_gain)

    xt = [sb("xt%d" % i, [128, D], F32) for i in range(2)]
    t_xt = [Tok(), Tok()]
    hb = [sb("hb%d" % i, [128, D], BF16) for i in range(2)]
    t_hb = [Tok(), Tok()]
    sq_scr, t_sq = sbt("sq_scr", [128, D], BF16)
    stat = [sb("stat%d" % i, [128, 4], F32) for i in range(2)]
    t_stat = [Tok(), Tok()]
    k.nt = 0
    for i in range(2):
        P.op("pool", lambda e, i=i: e.memset(xt[i], 0.0), [], [t_xt[i]])

    k.pref = {}

    def load_x(src_rows, nrows, key=None):
        if key is not None and key in k.pref:
            return k.pref.pop(key)
        i = k.nt % 2
        k.nt += 1
        P.dma("sp", xt[i][0:nrows, :], src_rows, writes=[t_xt[i]])
        return xt[i], t_xt[i], i

    def prefetch_x(src_rows, nrows, key):
        k.pref[key] = load_x(src_rows, nrows)

    def norm_T(x, tx, i, hT, t_hT, col, psb):
        s, ts = stat[i], t_stat[i]
        P.op("act", lambda e: e.activation(out=sq_scr, in_=x, func=AF.Square,
                                           accum_out=s[:, 0:1]), [tx], [t_sq, ts])
        P.op("pool", lambda e: e.tensor_scalar(out=s[:, 1:2], in0=s[:, 0:1], scalar1=1.0 / D,
                                               scalar2=EPS, op0=ALU.mult, op1=ALU.add), [ts], [ts])
        P.op("pool", lambda e: e.tensor_tensor(out=s[:, 2:3], in0=s[:, 1:2], in1=neghalf[:, 0:1],
                                               op=ALU.pow), [ts, t_nh], [ts])
        h, th = hb[i], t_hb[i]
        P.op("act", lambda e: e.activation(out=h, in_=x, func=AF.Copy, scale=s[:, 2:3]),
             [tx, ts], [th])
        pb = psb16[psb]
        for kc in range(8):
            P.op("pe", lambda e, kc=kc: e.transpose(out=pb[:, kc * 128:(kc + 1) * 128],
                                                    in_=h[:, kc * 128:(kc + 1) * 128],
                                                    identity=identb),
                 [th, t_identb], [t_ps[psb]])
        P.op("dve", lambda e: e.tensor_copy(out=hT[:, :, col:col + 128],
                                            in_=pb.rearrange("p (k t) -> p k t", k=8)),
             [t_ps[psb]], [t_hT])

    def norm_transpose(src_rows, nrows, hT, t_hT, col, psb, key=None):
        x, tx, i = load_x(src_rows, nrows, key)
        norm_T(x, tx, i, hT, t_hT, col, psb)
        return x, tx

    def rstd_groups(dst, t_dst, ssq, t_ssq, n, width):
        P.op("pool", lambda e: e.tensor_scalar(out=dst, in0=ssq, scalar1=1.0 / width, scalar2=EPS,
                                               op0=ALU.mult, op1=ALU.add), [t_ssq], [t_dst])
        P.op("pool", lambda e: e.tensor_tensor(out=dst, in0=dst, in1=neghalf[:, 0:n], op=ALU.pow),
             [t_dst, t_nh], [t_dst])

    def head_rms(psrc, t_psrc, nh, scr, t_scr, ss, t_ss):
        P.op("act", lambda e: e.activation(out=scr[:, 0:nh * 64], in_=psrc, func=AF.Square),
             [t_psrc], [t_scr])
        P.op("dve", lambda e: e.tensor_reduce(out=ss[:, 0:nh],
                                              in_=scr[:, 0:nh * 64].rearrange("p (h d) -> p h d", h=nh),
                                              axis=AX.X, op=ALU.add), [t_scr], [t_ss])
        rstd_groups(ss[:, nh:2 * nh], t_ss, ss[:, 0:nh], t_ss, nh, 64)

    qscr, t_qscr = sbt("qscr", [128, 512])
    qss, t_qss = sbt("qss", [128, 16])
    PERSIST_TOP = k.top

    def prompt_seq(sq):
        k.top = PERSIST_TOP
        if STOP <= 0:
            return
        row_base = sq * SEQ
        mkT2, t_mkT2 = sbt("mkT2", [128, 2, 256], BF16)
        mv_b, t_mv = sbt("mv_b", [128, 2, 256], BF16)
        o_gdnT, t_ogT = sbt("o_gdnT", [128, 4, SEQ], BF16)
        SEQ_TOP = k.top
        alloc_stage(256)
        wmkv, t_wmkv = sbt("wmkv", [128, 8, 512], BF16)
        load_weight(wmkv, t_wmkv, w_mk, 0, 256, gM, t_gM, 0)
        load_weight(wmkv, t_wmkv, w_mv, 0, 256, gM, t_gM, 256)
        hTm, t_hTm = sbt("hTm", [128, 8, 128], BF16)
        mko = [sbt("mko%d" % i, [128, 512]) for i in range(2)]
        mkb, t_mkb = sbt("mkb", [128, 256], BF16)
        for mt in range(2):
            row0 = sq * MEM + mt * 128
            norm_transpose(memp[row0:row0 + 128, :], 128, hTm, t_hTm, 0, 0)
            for kc in range(8):
                P.op("pe", lambda e, kc=kc: e.matmul(ps[1], lhsT=hTm[:, kc, :], rhs=wmkv[:, kc, :],
                                                     start=(kc == 0), stop=(kc == 7)),
                     [t_hTm, t_wmkv], [t_ps[1]])
            o, to = mko[mt]
            P.op("act", lambda e, o=o: e.activation(out=o[:, 256:512], in_=ps[1][:, 256:512], func=AF.Copy),
                 [t_ps[1]], [to])
            P.op("act", lambda e, mt=mt: e.activation(out=mv_b[:, mt, :], in_=ps[1][:, 256:512], func=AF.Copy),
                 [t_ps[1]], [t_mv])
            head_rms(ps[1][:, 0:256], t_ps[1], 4, qscr, t_qscr, qss, t_qss)
            P.op("dve", lambda e, o=o: e.tensor_tensor(
                out=o[:, 0:256].rearrange("p (h d) -> p h d", h=4),
                in0=ps[1][:, 0:256].rearrange("p (h d) -> p h d", h=4),
                in1=qss[:, 4:8].unsqueeze(2).to_broadcast([128, 4, 64]), op=ALU.mult),
                [t_ps[1], t_qss], [to])
            P.op("pool", lambda e, o=o: e.tensor_tensor(
                out=o[:, 0:256].rearrange("p (h d) -> p h d", h=4),
                in0=o[:, 0:256].rearrange("p (h d) -> p h d", h=4),
                in1=gmk_b.unsqueeze(1).to_broadcast([128, 4, 64]), op=ALU.mult),
                [to, t_gmk], [to])
            P.dma("sp", memk_p[row0:row0 + 128, :], o[:, 0:256], reads=[to])
            P.dma("sp", memv_p[row0:row0 + 128, :], o[:, 256:512], reads=[to])
            P.op("act", lambda e, o=o: e.activation(out=mkb, in_=o[:, 0:256], func=AF.Copy), [to], [t_mkb])
            pb = psb16[2]
            for a in range(2):
                P.op("pe", lambda e, a=a: e.transpose(out=pb[:, a * 128:(a + 1) * 128],
                                                      in_=mkb[:, a * 128:(a + 1) * 128], identity=identb),
                     [t_mkb, t_identb], [t_ps[2]])
            P.op("dve", lambda e, mt=mt: e.tensor_copy(
                out=mkT2[:, :, mt * 128:(mt + 1) * 128],
                in_=pb[:, 0:256].rearrange("p (a t) -> p a t", a=2)), [t_ps[2]], [t_mkT2])
        P.barrier()
        if STOP <= 1:
            return
        k.top = SEQ_TOP
        gdn_phase(sq, o_gdnT, t_ogT)
        P.barrier()
        if STOP <= 2:
            return
        k.top = SEQ_TOP
        o_atT, t_oatT = sbt("o_atT", [128, 4, SEQ], BF16)
        D_TOP = k.top
        dsa_phase(sq, o_atT, t_oatT)
        P.barrier()
        if STOP <= 3:
            return
        k.top = D_TOP
        c1_phase(sq, o_atT, t_oatT, o_gdnT, t_ogT, mkT2, t_mkT2, mv_b, t_mv)
        P.barrier()
        if STOP <= 4:
            return
        k.top = PERSIST_TOP
        c2_phase(sq)
        P.barrier()

    def gdn_phase(sq, o_gdnT, t_ogT):
        row_base = sq * SEQ
        qTg, t_qTg = sbt("qTg", [128, 4, SEQ], BF16)
        kTg, t_kTg = sbt("kTg", [128, 4, SEQ], BF16)
        k_tm, t_ktm = sbt("k_tm", [128, NT, 4, 128], BF16)
        v_tm, t_vtm = sbt("v_tm", [128, NT, 4, 128], BF16)
        sgz, t_sgz = sbt("sgz", [128, NT, 512], BF16)
        gab, t_gab = sbt("gab", [128, NT, 8])
        G_TOP = k.top
        alloc_stage(2056, 2)
        wB, t_wB = sbt("wB", [128, 8, 2056], BF16)
        load_weight(wB, t_wB, w_in, 1092, 3148, gA, t_gA)
        hTg, t_hTg = sbt("hTg", [128, 8, 512], BF16)
        Cq, t_Cq = sbt("Cq", [128, 4, 512])
        cvT, t_cvT = sbt("cvT", [128, 4, 512], BF16)
        Uc = [sbt("Uc%d" % i, [128, 515]) for i in range(2)]
        cacc = [sbt("cacc%d" % i, [128, 512]) for i in range(2)]
        halo, t_halo = sbt("halo", [128, 12, 3])
        sqb, t_sqb = sbt("sqb", [128, 512], BF16)
        lnb, t_lnb = sbt("lnb", [128, 512])
        P.op("pool", lambda e: e.memset(halo, 0.0), [], [t_halo])
        def conv_chunk(j, grp):
            b = 3 + (j % 2)
            for kc in range(8):
                P.op("pe", lambda e, kc=kc: e.matmul(
                    ps[b], lhsT=wB[:, kc, 128 * j:128 * j + 128], rhs=hTg[:, kc, :],
                    start=(kc == 0), stop=(kc == 7)), [t_hTg, t_wB], [t_ps[b]])
            u, tu = Uc[j % 2]
            ca, tca = cacc[j % 2]
            P.op("act", lambda e: e.activation(out=u[:, 3:515], in_=ps[b], func=AF.Copy), [t_ps[b]], [tu])
            P.op("pool", lambda e: e.tensor_copy(out=u[:, 0:3], in_=halo[:, j, :]), [t_halo], [tu])
            P.op("dve", lambda e: e.tensor_scalar(out=ca, in0=u[:, 0:512], scalar1=cw[:, j, 0:1], scalar2=None,
                                                  op0=ALU.mult), [tu, t_cw], [tca])
            for tap in range(1, 4):
                P.op("dve", lambda e, tap=tap: e.scalar_tensor_tensor(
                    out=ca, in0=u[:, tap:tap + 512], scalar=cw[:, j, tap:tap + 1], in1=ca,
                    op0=ALU.mult, op1=ALU.add), [tu, t_cw, tca], [tca])
            P.op("pool", lambda e: e.tensor_copy(out=halo[:, j, :], in_=u[:, 512:515]), [tu], [t_halo])
            if j < 8:
                P.op("act", lambda e: e.activation(out=Cq[:, j % 4, :], in_=ca, func=AF.Silu), [tca], [t_Cq])
            else:
                P.op("act", lambda e: e.activation(out=cvT[:, j - 8, :], in_=ca, func=AF.Silu), [tca], [t_cvT])

        def norm_chunk(j, gc0):
            P.op("act", lambda e: e.activation(out=sqb, in_=Cq[:, j % 4, :], func=AF.Square), [t_Cq], [t_sqb])
            P.op("pe", lambda e: e.matmul(ps[5], lhsT=onesb, rhs=sqb, start=True, stop=True),
                 [t_sqb, t_onesb], [t_ps[5]])
            P.op("act", lambda e: e.activation(out=lnb, in_=ps[5], func=AF.Ln, bias=1e-6, scale=1.0),
                 [t_ps[5]], [t_lnb])
            bias = (-0.5 * float(np.log(128.0))) if j < 4 else 0.0
            P.op("act", lambda e: e.activation(out=lnb, in_=lnb, func=AF.Exp, bias=bias, scale=-0.5),
                 [t_lnb], [t_lnb])
            dstT, tdst = (qTg, t_qTg) if j < 4 else (kTg, t_kTg)
            P.op("dve", lambda e: e.tensor_tensor(out=dstT[:, j % 4, gc0:gc0 + 512], in0=Cq[:, j % 4, :], in1=lnb,
                                                  op=ALU.mult), [t_Cq, t_lnb], [tdst])

        def g_tile(grp, t4):
            ti = grp * 4 + t4
            r0 = row_base + ti * 128
            norm_transpose(xp[r0:r0 + 128, :], 128, hTg, t_hTg, t4 * 128, 0, key=("g", r0))
            if ti + 1 < NT:
                prefetch_x(xp[r0 + 128:r0 + 256, :], 128, ("g", r0 + 128))
            for kc in range(8):
                P.op("pe", lambda e, kc=kc: e.matmul(
                    ps[1], lhsT=hTg[:, kc, t4 * 128:(t4 + 1) * 128], rhs=wB[:, kc, 1536:2048],
                    start=(kc == 0), stop=(kc == 7)), [t_hTg, t_wB], [t_ps[1]])
            for kc in range(8):
                P.op("pe", lambda e, kc=kc: e.matmul(
                    ps[2][:, 0:8], lhsT=hTg[:, kc, t4 * 128:(t4 + 1) * 128], rhs=wB[:, kc, 2048:2056],
                    start=(kc == 0), stop=(kc == 7)), [t_hTg, t_wB], [t_ps[2]])
            P.op("act", lambda e: e.activation(out=sgz[:, ti, :], in_=ps[1], func=AF.Silu), [t_ps[1]], [t_sgz])
            P.op("dve", lambda e: e.tensor_copy(out=gab[:, ti, :], in_=ps[2][:, 0:8]), [t_ps[2]], [t_gab])

        def g_transposes(grp, t4):
            ti = grp * 4 + t4
            gc0 = grp * 512
            for (srcT, tsrc, c0, dst, tdst, b) in ((kTg, t_kTg, gc0 + t4 * 128, k_tm, t_ktm, 6),
                                                   (cvT, t_cvT, t4 * 128, v_tm, t_vtm, 7)):
                pb = psb16[b]
                for h in range(4):
                    P.op("pe", lambda e, h=h, srcT=srcT, c0=c0, pb=pb: e.transpose(
                        out=pb[:, h * 128:(h + 1) * 128], in_=srcT[:, h, c0:c0 + 128], identity=identb),
                        [tsrc, t_identb], [t_ps[b]])
                P.op("act", lambda e, dst=dst, pb=pb: e.activation(
                    out=dst[:, ti, :, :], in_=pb[:, 0:512].rearrange("p (h d) -> p h d", h=4), func=AF.Copy),
                    [t_ps[b]], [tdst])

        for grp in range(4):
            for t4 in range(4):
                g_tile(grp, t4)
            for j in range(0, 4):
                conv_chunk(j, grp)
            for j in range(0, 4):
                norm_chunk(j, grp * 512)
            for j in range(4, 12):
                conv_chunk(j, grp)
            for j in range(4, 8):
                norm_chunk(j, grp * 512)
            for t4 in range(4):
                g_transposes(grp, t4)
        P.barrier()
        if STOP <= 1.5:
            return
        k.top = G_TOP
        gall, t_gall = sbt("gall", [128, NT, 4])
        ball, t_ball = sbt("ball", [128, NT, 4])
        tmpa, t_tmpa = sbt("tmpa", [128, NT, 4])
        tmpb, t_tmpb = sbt("tmpb", [128, NT, 4])
        P.op("dve", lambda e: e.tensor_tensor(out=gall, in0=gab[:, :, 0:4],
                                              in1=dtb_b.unsqueeze(1).to_broadcast([128, NT, 4]), op=ALU.add),
             [t_gab, t_dtb], [t_gall])
        P.op("dve", lambda e: e.tensor_scalar(out=tmpa, in0=gall, scalar1=-1.0, scalar2=None, op0=ALU.mult),
             [t_gall], [t_tmpa])
        P.op("dve", lambda e: e.tensor_tensor(out=tmpa, in0=tmpa, in1=gall, op=ALU.min), [t_tmpa, t_gall], [t_tmpa])
        P.op("act", lambda e: e.activation(out=tmpa, in_=tmpa, func=AF.Exp), [t_tmpa], [t_tmpa])
        P.op("act", lambda e: e.activation(out=tmpa, in_=tmpa, func=AF.Ln, bias=1.0, scale=1.0), [t_tmpa], [t_tmpa])
        P.op("dve", lambda e: e.scalar_tensor_tensor(out=tmpb, in0=gall, scalar=0.0, in1=tmpa,
                                                     op0=ALU.max, op1=ALU.add), [t_gall, t_tmpa], [t_tmpb])
        P.op("dve", lambda e: e.tensor_tensor(out=gall, in0=tmpb,
                                              in1=nea_b.unsqueeze(1).to_broadcast([128, NT, 4]), op=ALU.mult),
             [t_tmpb, t_nea], [t_gall])
        P.op("act", lambda e: e.activation(out=ball, in_=gab[:, :, 4:8], func=AF.Sigmoid), [t_gab], [t_ball])

        S, t_S = sbt("S", [128, 4, 128])
        Sb, t_Sb = sbt("Sb", [128, 4, 128], BF16)
        P.op("pool", lambda e: e.memset(S, 0.0), [], [t_S])
        P.op("pool", lambda e: e.memset(Sb, 0.0), [], [t_Sb])
        NB = 2
        bufs = []
        for i in range(NB):
            bb = {}
            for nm, dt_ in (("Gh", F32), ("E", F32), ("Du", F32), ("Dl", F32), ("L0", BF16), ("L1", BF16),
                            ("M0", BF16), ("M1", BF16), ("P0", BF16), ("P1", BF16), ("kbg", BF16),
                            ("kdec", BF16), ("vb", BF16), ("u", F32), ("wT", BF16), ("qgT", BF16),
                            ("qkT", BF16), ("vnew", BF16), ("on", F32), ("og", BF16)):
                bb[nm] = sbt("%s_%d" % (nm, i), [128, 4, 128], dt_)
            bb["sc"] = sbt("gsc_%d" % i, [128, 40])
            bufs.append(bb)
        k.pbank = 0

        def bank():
            b = k.pbank
            k.pbank = (k.pbank + 1) % 8
            return b

        def gdn_tile(ti):
            B = bufs[ti % NB]
            par = ti % 2
            bstate = [0]

            def bank():
                b_ = par * 4 + bstate[0]
                bstate[0] = (bstate[0] + 1) % 4
                return b_
            c0 = ti * 128
            sc, tsc = B["sc"]
            Gh, tGh = B["Gh"]
            for h in range(4):
                P.op("pool", lambda e, h=h, Gh=Gh, ti=ti: e.tensor_scalar(
                    out=Gh[:, h, :], in0=trile, scalar1=gall[:, ti, h:h + 1], scalar2=1.0,
                    op0=ALU.mult, op1=ALU.mult), [t_trile, t_gall], [tGh])
                yield
            bE, bDu, bDl, bsm = bank(), bank(), bank(), bank()
            for h in range(4):
                P.op("pe", lambda e, h=h, Gh=Gh, bE=bE: e.matmul(ps[bE][:, h * 128:(h + 1) * 128], lhsT=onesf,
                                                                 rhs=Gh[:, h, :], start=True, stop=True),
                     [tGh, t_onesf], [t_ps[bE]])
                yield
                P.op("pe", lambda e, h=h, Gh=Gh, bDu=bDu: e.matmul(ps[bDu][:, h * 128:(h + 1) * 128], lhsT=sgt,
                                                                   rhs=Gh[:, h, :], start=True, stop=True),
                     [tGh, t_sgt], [t_ps[bDu]])
                yield
                P.op("pe", lambda e, h=h, Gh=Gh, bDl=bDl: e.matmul(ps[bDl][:, h * 128:(h + 1) * 128], lhsT=Gh[:, h, :],
                                                                   rhs=sgt, start=True, stop=True),
                     [tGh, t_sgt], [t_ps[bDl]])
                yield
            if GCUT <= 1:
                return
            P.op("pe", lambda e, ti=ti, bsm=bsm: e.matmul(ps[bsm][:, 0:4], lhsT=trile, rhs=gall[:, ti, :],
                                                          start=True, stop=True), [t_trile, t_gall], [t_ps[bsm]])
            yield
            P.op("pe", lambda e, ti=ti, bsm=bsm: e.matmul(ps[bsm][:, 8:12], lhsT=onesf, rhs=gall[:, ti, :],
                                                          start=True, stop=True), [t_onesf, t_gall], [t_ps[bsm]])
            yield
            if GCUT <= 2:
                return
            E, tE = B["E"]
            Du, tDu = B["Du"]
            Dl, tDl = B["Dl"]
            fl = lambda a: a.rearrange("p h c -> p (h c)")
            P.op("act", lambda e, E=E, bE=bE: e.activation(out=fl(E), in_=ps[bE], func=AF.Exp), [t_ps[bE]], [tE])
            yield
            P.op("act", lambda e, Du=Du, bDu=bDu: e.activation(out=fl(Du), in_=ps[bDu], func=AF.Exp), [t_ps[bDu]], [tDu])
            yield
            P.op("act", lambda e, Dl=Dl, bDl=bDl: e.activation(out=fl(Dl), in_=ps[bDl], func=AF.Exp), [t_ps[bDl]], [tDl])
            yield
            P.op("pool", lambda e, Du=Du: e.tensor_tensor(out=Du, in0=Du, in1=trile.unsqueeze(1).to_broadcast([128, 4, 128]),
                                                          op=ALU.mult), [tDu, t_trile], [tDu])
            yield
            P.op("pool", lambda e, Dl=Dl: e.tensor_tensor(out=Dl, in0=Dl, in1=sgt.unsqueeze(1).to_broadcast([128, 4, 128]),
                                                          op=ALU.mult), [tDl, t_sgt], [tDl])
            yield
            if GCUT <= 3:
                return
            P.op("dve", lambda e, sc=sc, bsm=bsm: e.tensor_copy(out=sc[:, 0:4], in_=ps[bsm][:, 0:4]), [t_ps[bsm]], [tsc])
            yield
            P.op("act", lambda e, sc=sc: e.activation(out=sc[:, 4:8], in_=sc[:, 0:4], func=AF.Exp), [tsc], [tsc])
            yield
            P.op("dve", lambda e, sc=sc, bsm=bsm: e.tensor_tensor(out=sc[:, 24:28], in0=ps[bsm][:, 8:12], in1=sc[:, 0:4],
                                                                  op=ALU.subtract), [t_ps[bsm], tsc], [tsc])
            yield
            P.op("act", lambda e, sc=sc: e.activation(out=sc[:, 8:12], in_=sc[:, 24:28], func=AF.Exp), [tsc], [tsc])
            yield
            P.op("act", lambda e, sc=sc, bsm=bsm: e.activation(out=sc[:, 12:16], in_=ps[bsm][:, 8:12], func=AF.Exp),
                 [t_ps[bsm]], [tsc])
            yield
            P.op("dve", lambda e, sc=sc, ti=ti: e.tensor_tensor(out=sc[:, 16:20], in0=sc[:, 4:8], in1=ball[:, ti, :],
                                                                op=ALU.mult), [tsc, t_ball], [tsc])
            yield
            P.op("dve", lambda e, sc=sc, ti=ti: e.tensor_scalar(out=sc[:, 20:24], in0=ball[:, ti, :], scalar1=-1.0,
                                                                scalar2=None, op0=ALU.mult), [t_ball], [tsc])
            yield
            if GCUT <= 4:
                return
            kbg, tkbg = B["kbg"]
            kdec, tkdec = B["kdec"]
            vb, tvb = B["vb"]
            bc = lambda a: a.unsqueeze(2).to_broadcast([128, 4, 128])
            P.op("pool", lambda e, kbg=kbg, sc=sc, ti=ti: e.tensor_tensor(out=kbg, in0=k_tm[:, ti, :, :], in1=bc(sc[:, 16:20]),
                                                                          op=ALU.mult), [t_ktm, tsc], [tkbg])
            yield
            P.op("pool", lambda e, kdec=kdec, sc=sc, ti=ti: e.tensor_tensor(out=kdec, in0=k_tm[:, ti, :, :], in1=bc(sc[:, 8:12]),
                                                                            op=ALU.mult), [t_ktm, tsc], [tkdec])
            yield
            P.op("pool", lambda e, vb=vb, ti=ti: e.tensor_tensor(out=vb, in0=v_tm[:, ti, :, :], in1=bc(ball[:, ti, :]),
                                                                 op=ALU.mult), [t_vtm, t_ball], [tvb])
            yield
            if GCUT <= 5:
                return
            bkk = bank()
            for h in range(4):
                P.op("pe", lambda e, h=h, bkk=bkk: e.matmul(ps[bkk][:, h * 128:(h + 1) * 128], lhsT=kTg[:, h, c0:c0 + 128],
                                                            rhs=kTg[:, h, c0:c0 + 128], start=True, stop=True),
                     [t_kTg], [t_ps[bkk]])
                yield
            Lc, tLc = B["L0"]
            Ln_, tLn = B["L1"]
            Mc, tMc = B["M0"]
            Mn, tMn = B["M1"]
            Pc, tPc = B["P0"]
            Pn, tPn = B["P1"]
            for h in range(4):
                P.op("dve", lambda e, h=h, Lc=Lc, sc=sc, Dl=Dl, bkk=bkk: e.scalar_tensor_tensor(
                    out=Lc[:, h, :], in0=ps[bkk][:, h * 128:(h + 1) * 128], scalar=sc[:, 20 + h:21 + h], in1=Dl[:, h, :],
                    op0=ALU.mult, op1=ALU.mult), [t_ps[bkk], tsc, tDl], [tLc])
                yield
            if GCUT <= 6:
                return
            bM = bank()
            pbm = psb16[bM]
            for h in range(4):
                P.op("pe", lambda e, h=h, Lc=Lc, pbm=pbm: e.transpose(out=pbm[:, h * 128:(h + 1) * 128], in_=Lc[:, h, :],
                                                                      identity=identb), [tLc, t_identb], [t_ps[bM]])
                yield
            pm3 = pbm[:, 0:512].rearrange("p (h c) -> p h c", h=4)
            if GCUT == 61:
                return
            P.op("act", lambda e, Mc=Mc, pm3=pm3: e.activation(out=Mc, in_=pm3, func=AF.Copy), [t_ps[bM]], [tMc])
            yield
            if GCUT == 62:
                return
            P.op("pool", lambda e, Pc=Pc, Mc=Mc: e.tensor_tensor(out=Pc, in0=Mc,
                                                                 in1=identb.unsqueeze(1).to_broadcast([128, 4, 128]),
                                                                 op=ALU.add), [tMc, t_identb], [tPc])
            yield
            if GCUT <= 7 or GCUT in (61, 62):
                return
            for lev in range(6):
                last = (lev == 5)
                bL = bank()
                if not last:
                    bMM = bank()
                    for h in range(4):
                        P.op("pe", lambda e, h=h, Lc=Lc, Mc=Mc, bMM=bMM: e.matmul(
                            ps[bMM][:, h * 128:(h + 1) * 128], lhsT=Lc[:, h, :], rhs=Mc[:, h, :], start=True, stop=True),
                            [tLc, tMc], [t_ps[bMM]])
                        yield
                for h in range(4):
                    P.op("pe", lambda e, h=h, Lc=Lc, Mc=Mc, bL=bL: e.matmul(
                        ps[bL][:, h * 128:(h + 1) * 128], lhsT=Mc[:, h, :], rhs=Lc[:, h, :], start=True, stop=True),
                        [tLc, tMc], [t_ps[bL]])
                    yield
                P.op("dve", lambda e, Ln_=Ln_, bL=bL: e.tensor_copy(out=fl(Ln_), in_=ps[bL]), [t_ps[bL]], [tLn])
                yield
                if not last:
                    P.op("act", lambda e, Mn=Mn, bMM=bMM: e.activation(out=fl(Mn), in_=ps[bMM], func=AF.Copy),
                         [t_ps[bMM]], [tMn])
                    yield
                bP = bank()
                for h in range(4):
                    P.op("pe", lambda e, h=h, Ln_=Ln_, Pc=Pc, bP=bP: e.matmul(
                        ps[bP][:, h * 128:(h + 1) * 128], lhsT=Ln_[:, h, :], rhs=Pc[:, h, :], start=True, stop=True),
                        [tLn, tPc], [t_ps[bP]])
                    yield
                P.op("dve", lambda e, Pn=Pn, Pc=Pc, bP=bP: e.tensor_tensor(out=fl(Pn), in0=ps[bP], in1=fl(Pc), op=ALU.add),
                     [t_ps[bP], tPc], [tPn])
                yield
                Lc, tLc, Ln_, tLn = Ln_, tLn, Lc, tLc
                Mc, tMc, Mn, tMn = Mn, tMn, Mc, tMc
                Pc, tPc, Pn, tPn = Pn, tPn, Pc, tPc
            if GCUT <= 8:
                return
            bu, bw, bq = bank(), bank(), bank()
            for h in range(4):
                P.op("pe", lambda e, h=h, Pc=Pc, vb=vb, bu=bu: e.matmul(ps[bu][:, h * 128:(h + 1) * 128], lhsT=Pc[:, h, :],
                                                                        rhs=vb[:, h, :], start=True, stop=True),
                     [tPc, tvb], [t_ps[bu]])
                yield
                P.op("pe", lambda e, h=h, Pc=Pc, kbg=kbg, bw=bw: e.matmul(ps[bw][:, h * 128:(h + 1) * 128], lhsT=kbg[:, h, :],
                                                                          rhs=Pc[:, h, :], start=True, stop=True),
                     [tPc, tkbg], [t_ps[bw]])
                yield
                P.op("pe", lambda e, h=h, bq=bq: e.matmul(ps[bq][:, h * 128:(h + 1) * 128], lhsT=kTg[:, h, c0:c0 + 128],
                                                          rhs=qTg[:, h, c0:c0 + 128], start=True, stop=True),
                     [t_kTg, t_qTg], [t_ps[bq]])
                yield
            u, tu_ = B["u"]
            wT, twT = B["wT"]
            qgT, tqgT = B["qgT"]
            qkT, tqkT = B["qkT"]
            P.op("act", lambda e, u=u, bu=bu: e.activation(out=fl(u), in_=ps[bu], func=AF.Copy), [t_ps[bu]], [tu_])
            yield
            P.op("act", lambda e, wT=wT, bw=bw: e.activation(out=fl(wT), in_=ps[bw], func=AF.Copy), [t_ps[bw]], [twT])
            yield
            P.op("dve", lambda e, qkT=qkT, Du=Du, bq=bq: e.tensor_tensor(out=fl(qkT), in0=ps[bq], in1=fl(Du), op=ALU.mult),
                 [t_ps[bq], tDu], [tqkT])
            yield
            P.op("pool", lambda e, qgT=qgT, E=E: e.tensor_tensor(out=qgT, in0=qTg[:, :, c0:c0 + 128], in1=E, op=ALU.mult),
                 [t_qTg, tE], [tqgT])
            yield
            if GCUT <= 9:
                return
            yield 'SEQ'
            bws, bo, bs = bank(), bank(), bank()
            for h in range(4):
                P.op("pe", lambda e, h=h, wT=wT, bws=bws: e.matmul(ps[bws][:, h * 128:(h + 1) * 128], lhsT=wT[:, h, :],
                                                                   rhs=Sb[:, h, :], start=True, stop=True),
                     [twT, t_Sb], [t_ps[bws]])
            vnew, tvn = B["vnew"]
            P.op("dve", lambda e, vnew=vnew, u=u, bws=bws: e.tensor_tensor(out=fl(vnew), in0=fl(u), in1=ps[bws],
                                                                           op=ALU.subtract), [tu_, t_ps[bws]], [tvn])
            for h in range(4):
                P.op("pe", lambda e, h=h, qgT=qgT, bo=bo: e.matmul(ps[bo][:, h * 128:(h + 1) * 128], lhsT=qgT[:, h, :],
                                                                   rhs=Sb[:, h, :], start=True, stop=False),
                     [tqgT, t_Sb], [t_ps[bo]])
                P.op("pe", lambda e, h=h, qkT=qkT, vnew=vnew, bo=bo: e.matmul(ps[bo][:, h * 128:(h + 1) * 128], lhsT=qkT[:, h, :],
                                                                              rhs=vnew[:, h, :], start=False, stop=True),
                     [tqkT, tvn], [t_ps[bo]])
            for h in range(4):
                P.op("pe", lambda e, h=h, kdec=kdec, vnew=vnew, bs=bs: e.matmul(ps[bs][:, h * 128:(h + 1) * 128], lhsT=kdec[:, h, :],
                                                                                rhs=vnew[:, h, :], start=True, stop=True),
                     [tkdec, tvn], [t_ps[bs]])
            for h in range(4):
                P.op("dve", lambda e, h=h, sc=sc, bs=bs: e.scalar_tensor_tensor(
                    out=S[:, h, :], in0=S[:, h, :], scalar=sc[:, 12 + h:13 + h], in1=ps[bs][:, h * 128:(h + 1) * 128],
                    op0=ALU.mult, op1=ALU.add), [t_S, tsc, t_ps[bs]], [t_S])
            P.op("act", lambda e: e.activation(out=Sb, in_=S, func=AF.Copy), [t_S], [t_Sb])
            if GCUT <= 10:
                return
            on, ton = B["on"]
            og, tog = B["og"]
            P.op("act", lambda e, on=on, bo=bo: e.activation(out=fl(on), in_=ps[bo], func=AF.Square), [t_ps[bo]], [ton])
            P.op("dve", lambda e, on=on, sc=sc: e.tensor_reduce(out=sc[:, 28:32], in_=on, axis=AX.X, op=ALU.add),
                 [ton], [tsc])
            P.op("pool", lambda e, sc=sc: e.tensor_scalar(out=sc[:, 32:36], in0=sc[:, 28:32], scalar1=1.0 / 128, scalar2=EPS,
                                                          op0=ALU.mult, op1=ALU.add), [tsc], [tsc])
            P.op("pool", lambda e, sc=sc: e.tensor_tensor(out=sc[:, 32:36], in0=sc[:, 32:36], in1=neghalf[:, 0:4], op=ALU.pow),
                 [tsc, t_nh], [tsc])
            P.op("dve", lambda e, on=on, sc=sc, bo=bo: e.tensor_tensor(
                out=on, in0=ps[bo].rearrange("p (h c) -> p h c", h=4), in1=bc(sc[:, 32:36]), op=ALU.mult),
                [t_ps[bo], tsc], [ton])
            P.op("pool", lambda e, on=on: e.tensor_tensor(out=on, in0=on, in1=ggdn_b.unsqueeze(1).to_broadcast([128, 4, 128]),
                                                          op=ALU.mult), [ton, t_ggdn], [ton])
            P.op("pool", lambda e, on=on, og=og, ti=ti: e.tensor_tensor(
                out=og, in0=on, in1=sgz[:, ti, :].rearrange("p (h c) -> p h c", h=4), op=ALU.mult),
                [ton, t_sgz], [tog])
            bt = bank()
            pbt = psb16[bt]
            for h in range(4):
                P.op("pe", lambda e, h=h, og=og, pbt=pbt: e.transpose(out=pbt[:, h * 128:(h + 1) * 128], in_=og[:, h, :],
                                                                      identity=identb), [tog, t_identb], [t_ps[bt]])
            P.op("act", lambda e, pbt=pbt: e.activation(out=o_gdnT[:, :, c0:c0 + 128],
                                                        in_=pbt[:, 0:512].rearrange("p (h c) -> p h c", h=4), func=AF.Copy),
                 [t_ps[bt]], [t_ogT])
        def run_to_seq(gens):
            live = list(gens)
            while live:
                for g_ in list(live):
                    try:
                        if next(g_) == 'SEQ':
                            live.remove(g_)
                    except StopIteration:
                        live.remove(g_)

        def finish_gen(g_):
            for _ in g_:
                pass

        for t2 in range(0, NT if STOP > 1.8 else 2, 2):
            ga_, gb_ = gdn_tile(t2), gdn_tile(t2 + 1)
            run_to_seq([ga_, gb_])
            finish_gen(ga_)
            finish_gen(gb_)
        P.dma("sp", ssm_p[sq * 512:(sq + 1) * 512, :].rearrange("(h d) v -> d h v", h=4), S, reads=[t_S])

    NIT = int(os.environ.get("NIT", "18"))

    def dsa_phase(sq, o_atT, t_oatT):
        row_base = sq * SEQ
        qT2, t_qT2 = sbt("qT2", [128, NT, 512], BF16)
        kT2, t_kT2 = sbt("kT2", [128, SEQ], BF16)
        v_b, t_vb = sbt("v_b", [128, NT, 128], BF16)
        qiT2, t_qiT2 = sbt("qiT2", [128, 2, SEQ], BF16)
        kiT2, t_kiT2 = sbt("kiT2", [128, SEQ], BF16)
        wi_s, t_wi = sbt("wi_s", [128, NT, 4])
        A_TOP = k.top
        alloc_stage(1536)
        wA, t_wA = sbt("wA", [128, 8, 1092], BF16)
        load_weight(wA, t_wA, w_in, 0, 1092, gA, t_gA)
        wConv, t_wConv = sbt("wConv", [128, 8, 1536], BF16)
        load_weight(wConv, t_wConv, w_in, 1092, 2628, gA, t_gA)
        hT1, t_hT1 = sbt("hT1", [128, 8, 128], BF16)
        ko = [sbt("ko%d" % i, [128, 320]) for i in range(2)]
        cvo, t_cvo = sbt("cvo", [128, 1536])
        qnb, t_qnb = sbt("qnb", [128, 512], BF16)
        kb_, t_kb = sbt("kb_", [128, 128], BF16)
        qib, t_qib = sbt("qib", [128, 256], BF16)
        kib, t_kib = sbt("kib", [128, 128], BF16)

        def proj_tile(t):
            r0 = row_base + t * 128
            c0 = t * 128
            norm_transpose(xp[r0:r0 + 128, :], 128, hT1, t_hT1, 0, 0, key=("d", r0))
            if t + 1 < NT:
                prefetch_x(xp[r0 + 128:r0 + 256, :], 128, ("d", r0 + 128))
            for (b, a0, a1) in ((1, 0, 512), (2, 512, 1024), (3, 1024, 1092)):
                for kc in range(8):
                    P.op("pe", lambda e, kc=kc, b=b, a0=a0, a1=a1: e.matmul(
                        ps[b][:, 0:a1 - a0], lhsT=hT1[:, kc, :], rhs=wA[:, kc, a0:a1],
                        start=(kc == 0), stop=(kc == 7)), [t_hT1, t_wA], [t_ps[b]])
            o, to = ko[t % 2]
            if PCUT <= 1:
                return
            head_rms(ps[1], t_ps[1], 8, qscr, t_qscr, qss, t_qss)
            P.op("dve", lambda e: e.tensor_tensor(
                out=qnb.rearrange("p (r g d) -> p g r d", r=4, g=2),
                in0=ps[1].rearrange("p (g r d) -> p g r d", g=2, r=4),
                in1=qss[:, 8:16].rearrange("p (g r) -> p g r", g=2).unsqueeze(3).to_broadcast([128, 2, 4, 64]),
                op=ALU.mult), [t_ps[1], t_qss], [t_qnb])
            pbq = psb16[4]
            for r in range(4):
                P.op("pe", lambda e, r=r: e.transpose(out=pbq[:, r * 128:(r + 1) * 128], in_=qnb[:, r * 128:(r + 1) * 128],
                                                      identity=identb), [t_qnb, t_identb], [t_ps[4]])
            P.op("act", lambda e: e.activation(out=qT2[:, t, :], in_=pbq[:, 0:512], func=AF.Copy),
                 [t_ps[4]], [t_qT2])
            P.op("pool", lambda e: e.tensor_scalar(out=qT2[:, t, :], in0=qT2[:, t, :], scalar1=gq8[:, 0:1], scalar2=1.0,
                                                   op0=ALU.mult, op1=ALU.mult), [t_qT2, t_gq8], [t_qT2])
            if PCUT <= 2:
                return
            head_rms(ps[2][:, 0:128], t_ps[2], 2, qscr, t_qscr, qss, t_qss)
            P.op("dve", lambda e: e.tensor_tensor(
                out=o[:, 0:128].rearrange("p (h d) -> p h d", h=2),
                in0=ps[2][:, 0:128].rearrange("p (h d) -> p h d", h=2),
                in1=qss[:, 2:4].unsqueeze(2).to_broadcast([128, 2, 64]), op=ALU.mult),
                [t_ps[2], t_qss], [to])
            P.op("pool", lambda e: e.tensor_tensor(
                out=o[:, 0:128].rearrange("p (h d) -> p h d", h=2),
                in0=o[:, 0:128].rearrange("p (h d) -> p h d", h=2),
                in1=gk_b.unsqueeze(1).to_broadcast([128, 2, 64]), op=ALU.mult), [to, t_gk], [to])
            P.op("act", lambda e: e.activation(out=kb_, in_=o[:, 0:128], func=AF.Copy), [to], [t_kb])
            pb5 = psb16[5]
            P.op("pe", lambda e: e.transpose(out=pb5[:, 0:128], in_=kb_, identity=identb), [t_kb, t_identb], [t_ps[5]])
            if PCUT <= 3:
                return
            P.op("act", lambda e: e.activation(out=o[:, 128:256], in_=ps[2][:, 128:256], func=AF.Copy), [t_ps[2]], [to])
            P.op("act", lambda e: e.activation(out=v_b[:, t, :], in_=ps[2][:, 128:256], func=AF.Copy), [t_ps[2]], [t_vb])
            P.op("act", lambda e: e.activation(out=qib, in_=ps[2][:, 256:512], func=AF.Copy, scale=0.125),
                 [t_ps[2]], [t_qib])
            for a in range(2):
                P.op("pe", lambda e, a=a: e.transpose(out=pb5[:, 128 + a * 128:256 + a * 128],
                                                      in_=qib[:, a * 128:(a + 1) * 128], identity=identb),
                     [t_qib, t_identb], [t_ps[5]])
            if PCUT <= 4:
                return
            P.op("act", lambda e: e.activation(out=o[:, 256:320], in_=ps[3][:, 0:64], func=AF.Copy), [t_ps[3]], [to])
            P.op("act", lambda e: e.activation(out=kib[:, 0:64], in_=ps[3][:, 0:64], func=AF.Copy), [t_ps[3]], [t_kib])
            P.op("act", lambda e: e.activation(out=kib[:, 64:128], in_=ps[3][:, 0:64], func=AF.Copy), [t_ps[3]], [t_kib])
            P.op("pe", lambda e: e.transpose(out=pb5[:, 384:512], in_=kib, identity=identb), [t_kib, t_identb], [t_ps[5]])
            P.op("act", lambda e: e.activation(out=wi_s[:, t, :], in_=ps[3][:, 64:68], func=AF.Copy, scale=0.5),
                 [t_ps[3]], [t_wi])
            if PCUT <= 5:
                return
            P.op("act", lambda e: e.activation(out=kT2[:, c0:c0 + 128], in_=pb5[:, 0:128], func=AF.Copy), [t_ps[5]], [t_kT2])
            if PCUT == 51:
                return
            P.op("act", lambda e: e.activation(out=qiT2[:, :, c0:c0 + 128],
                                               in_=pb5[:, 128:384].rearrange("p (a t) -> p a t", a=2), func=AF.Copy),
                 [t_ps[5]], [t_qiT2])
            if PCUT == 52:
                return
            P.op("act", lambda e: e.activation(out=kiT2[:, c0:c0 + 128], in_=pb5[:, 384:512], func=AF.Copy),
                 [t_ps[5]], [t_kiT2])
            if PCUT == 53:
                return
            P.dma("sp", k_p[r0:r0 + 128, :], o[:, 0:128], reads=[to])
            P.dma("sp", v_p[r0:r0 + 128, :], o[:, 128:256], reads=[to])
            P.dma("sp", kidx_p[r0:r0 + 128, :], o[:, 256:320], reads=[to])
            if PCUT <= 6:
                return
            if t == NT - 1:
                for b in range(3):
                    for kc in range(8):
                        P.op("pe", lambda e, kc=kc, b=b: e.matmul(
                            ps[5 + b] if b < 2 else ps[0], lhsT=hT1[:, kc, :], rhs=wConv[:, kc, b * 512:(b + 1) * 512],
                            start=(kc == 0), stop=(kc == 7)), [t_hT1, t_wConv], [t_ps[5 + b] if b < 2 else t_ps[0]])
                for b in range(3):
                    bb = 5 + b if b < 2 else 0
                    P.op("act", lambda e, b=b, bb=bb: e.activation(out=cvo[:, b * 512:(b + 1) * 512], in_=ps[bb],
                                                                   func=AF.Copy), [t_ps[bb]], [t_cvo])
                P.dma("sp", conv_p[sq * 3:(sq + 1) * 3, :], cvo[125:128, :], reads=[t_cvo])

        for t in range(NT):
            proj_tile(t)
        P.barrier()
        if DCUT <= 1:
            return
        k.top = A_TOP
        scb = [sbt("scb%d" % i, [128, SEQ]) for i in range(2)]
        rl = [sbt("rl%d" % i, [128, 512]) for i in range(4)]
        junk2 = [sbt("junk%d" % i, [128, SEQ], BF16) for i in range(2)]
        mask2 = [sbt("mask%d" % i, [128, SEQ], BF16) for i in range(2)]
        maskT2 = [sbt("maskT%d" % i, [128, NT, 128], BF16) for i in range(4)]
        PT = [sbt("PT%d" % i, [128, 4, 128], BF16) for i in range(8)]
        bis2 = [sbt("bis%d" % i, [128, 64]) for i in range(2)]
        rec, t_rec = sbt("rec", [128, 4, 128])

        def q_sb(qt):
            L = (qt + 1) * 128
            q0 = qt * 128
            S_, tS = scb[qt % 2]
            maskT, t_maskT = maskT2[qt % 4]
            bis, t_bis = bis2[qt % 2]
            junk, t_junk = junk2[qt % 2]
            mask, t_mask = mask2[qt % 2]
            nch = (L + 511) // 512
            cnt_ = 0
            for ch in range(nch):
                k0 = ch * 512
                n = min(512, L - k0)
                for ih in range(4):
                    a, b = ih // 2, ih % 2
                    bnk = qt % 2
                    r_, tr = rl[(qt % 2) * 2 + cnt_ % 2]
                    cnt_ += 1
                    P.op("pe", lambda e, a=a, b=b, bnk=bnk, k0=k0, n=n: e.matmul(
                        ps[bnk][:, 0:n], lhsT=qiT2[64 * b:64 * b + 64, a, q0:q0 + 128],
                        rhs=kiT2[64 * b:64 * b + 64, k0:k0 + n], start=True, stop=True),
                        [t_qiT2, t_kiT2], [t_ps[bnk]])
                    yield
                    P.op("act", lambda e, r_=r_, bnk=bnk, n=n: e.activation(out=r_[:, 0:n], in_=ps[bnk][:, 0:n], func=AF.Relu),
                         [t_ps[bnk]], [tr])
                    yield
                    if ih == 0:
                        P.op("dve", lambda e, r_=r_, k0=k0, n=n: e.tensor_scalar(
                            out=S_[:, k0:k0 + n], in0=r_[:, 0:n], scalar1=wi_s[:, qt, 0:1], scalar2=None, op0=ALU.mult),
                            [tr, t_wi], [tS])
                        yield
                    else:
                        P.op("dve", lambda e, r_=r_, k0=k0, n=n, ih=ih: e.scalar_tensor_tensor(
                            out=S_[:, k0:k0 + n], in0=r_[:, 0:n], scalar=wi_s[:, qt, ih:ih + 1], in1=S_[:, k0:k0 + n],
                            op0=ALU.mult, op1=ALU.add), [tr, t_wi, tS], [tS])
                        yield
            if DCUT <= 2:
                return
            dg = S_[:, q0:q0 + 128]
            P.op("dve", lambda e: e.tensor_tensor(out=dg, in0=dg, in1=lowinc, op=ALU.mult), [tS, t_lowinc], [tS])
            yield
            P.op("dve", lambda e: e.tensor_reduce(out=bis[:, 0:1], in_=S_[:, 0:L], axis=AX.X, op=ALU.max), [tS], [t_bis])
            yield
            P.op("dve", lambda e: e.tensor_reduce(out=bis[:, 1:2], in_=S_[:, 0:L], axis=AX.X, op=ALU.min), [tS], [t_bis])
            yield
            P.op("pool", lambda e: e.tensor_tensor(out=dg, in0=dg, in1=tribias, op=ALU.add), [tS, t_tribias], [tS])
            yield
            P.op("dve", lambda e: e.tensor_tensor(out=bis[:, 2:3], in0=bis[:, 0:1], in1=bis[:, 1:2], op=ALU.subtract),
                 [t_bis], [t_bis])
            yield
            P.op("dve", lambda e: e.tensor_scalar(out=bis[:, 3:4], in0=bis[:, 2:3], scalar1=0.5005, scalar2=0.0005,
                                                  op0=ALU.mult, op1=ALU.add), [t_bis], [t_bis])
            yield
            P.op("dve", lambda e: e.tensor_tensor(out=bis[:, 4:5], in0=bis[:, 0:1], in1=bis[:, 3:4], op=ALU.subtract),
                 [t_bis], [t_bis])
            yield
            P.op("dve", lambda e: e.tensor_scalar(out=bis[:, 8:9 + NIT], in0=pow2_b[:, 0:NIT + 1], scalar1=bis[:, 3:4],
                                                  scalar2=None, op0=ALU.mult), [t_bis, t_pow2], [t_bis])
            yield
            if (qt % 2 == 1 or os.environ.get("ACTALL") == "1") and os.environ.get("ACTCNT", "1") == "1":
                P.op("dve", lambda e: e.tensor_scalar(out=bis[:, 40:41 + NIT], in0=bis[:, 8:9 + NIT], scalar1=-1.0, scalar2=None,
                                                      op0=ALU.mult), [t_bis], [t_bis])
                yield
                P.op("dve", lambda e: e.tensor_scalar(out=bis[:, 30:31], in0=bis[:, 4:5], scalar1=-1.0, scalar2=None,
                                                      op0=ALU.mult), [t_bis], [t_bis])
                yield
                for it in range(NIT):
                    P.op("act", lambda e: e.activation(out=junk[:, 0:L], in_=S_[:, 0:L], func=AF.Sign, bias=bis[:, 30:31],
                                                       scale=1.0, accum_out=bis[:, 31:32]), [tS, t_bis], [t_junk, t_bis])
                    yield
                    P.op("act", lambda e: e.activation(out=bis[:, 32:33], in_=bis[:, 31:32], func=AF.Sign,
                                                       bias=float(L - 510.5), scale=1.0), [t_bis], [t_bis])
                    yield
                    P.op("act", lambda e, it=it: e.activation(out=bis[:, 30:31], in_=bis[:, 32:33], func=AF.Identity,
                                                              bias=bis[:, 30:31], scale=bis[:, 41 + it:42 + it]),
                         [t_bis], [t_bis])
                    yield
                P.op("dve", lambda e: e.scalar_tensor_tensor(out=bis[:, 7:8], in0=bis[:, 30:31], scalar=-1.0,
                                                             in1=bis[:, 40 + NIT:41 + NIT], op0=ALU.mult, op1=ALU.add),
                     [t_bis], [t_bis])
                yield
            else:
              for _once in (0,):
                for it in range(NIT):
                    P.op("dve", lambda e: e.tensor_scalar(out=junk[:, 0:L], in0=S_[:, 0:L], scalar1=bis[:, 4:5], scalar2=None,
                                                          op0=ALU.is_ge, op1=ALU.add, accum_out=bis[:, 5:6]),
                         [tS, t_bis], [t_junk, t_bis])
                    yield
                    P.op("dve", lambda e: e.tensor_scalar(out=bis[:, 6:7], in0=bis[:, 5:6], scalar1=255.5, scalar2=0.5,
                                                          op0=ALU.is_ge, op1=ALU.subtract), [t_bis], [t_bis])
                    yield
                    P.op("dve", lambda e, it=it: e.scalar_tensor_tensor(out=bis[:, 4:5], in0=bis[:, 6:7], scalar=bis[:, 8 + it:9 + it],
                                                                        in1=bis[:, 4:5], op0=ALU.mult, op1=ALU.add),
                         [t_bis], [t_bis])
                    yield
                P.op("dve", lambda e: e.tensor_tensor(out=bis[:, 7:8], in0=bis[:, 4:5], in1=bis[:, 8 + NIT:9 + NIT], op=ALU.subtract),
                     [t_bis], [t_bis])
                yield
            P.op("dve", lambda e: e.tensor_scalar(out=mask[:, 0:L], in0=S_[:, 0:L], scalar1=bis[:, 7:8], scalar2=None,
                                                  op0=ALU.is_ge), [tS, t_bis], [t_mask])
            yield
            if DCUT <= 3:
                return
            for half in range((qt // 8) + 1):
                nb = min(8, qt + 1 - half * 8)
                for j in range(nb):
                    kb = half * 8 + j
                    P.op("pe", lambda e, kb=kb, j=j, half=half: e.transpose(
                        out=psb16[qt % 2][:, j * 128:(j + 1) * 128], in_=mask[:, kb * 128:(kb + 1) * 128], identity=identb),
                        [t_mask, t_identb], [t_ps[qt % 2]])
                    yield
                P.op("act", lambda e, half=half, nb=nb: e.activation(
                    out=maskT[:, half * 8:half * 8 + nb, :],
                    in_=psb16[qt % 2][:, 0:nb * 128].rearrange("p (j t) -> p j t", j=nb), func=AF.Copy),
                    [t_ps[qt % 2]], [t_maskT])
                yield
        def q_att(qt):
            L = (qt + 1) * 128
            q0 = qt * 128
            maskT, t_maskT = maskT2[qt % 4]
            items = [(kb, g) for kb in range(qt + 1) for g in range(2)]
            LA = 3

            def front(idx):
                kb, g = items[idx]
                bS = 2 + (idx % 4)
                PTt, tPT = PT[idx % len(PT)]
                P.op("pe", lambda e: e.matmul(
                    ps[bS], lhsT=kT2[64 * g:64 * g + 64, kb * 128:(kb + 1) * 128],
                    rhs=qT2[64 * g:64 * g + 64, qt, :], start=True, stop=True),
                    [t_kT2, t_qT2], [t_ps[bS]])
                P.op("act", lambda e: e.activation(out=PTt.rearrange("p r t -> p (r t)"), in_=ps[bS],
                                                   func=AF.Exp), [t_ps[bS]], [tPT])
                P.op("pool", lambda e: e.tensor_tensor(
                    out=PTt, in0=PTt, in1=maskT[:, kb, :].unsqueeze(1).to_broadcast([128, 4, 128]), op=ALU.mult),
                    [tPT, t_maskT], [tPT])

            def back(idx):
                kb, g = items[idx]
                PTt, tPT = PT[idx % len(PT)]
                P.op("pe", lambda e: e.matmul(
                    ps[6][64 * g:64 * g + 64, :], lhsT=v_b[:, kb, 64 * g:64 * g + 64],
                    rhs=PTt.rearrange("p r t -> p (r t)"), start=(kb == 0), stop=(kb == qt)),
                    [tPT, t_vb], [t_ps[6]])
                P.op("pe", lambda e: e.matmul(
                    ps[7][64 * g:64 * g + 64, :], lhsT=onesb[:, 0:64],
                    rhs=PTt.rearrange("p r t -> p (r t)"), start=(kb == 0), stop=(kb == qt)),
                    [tPT, t_onesb], [t_ps[7]])

            n_it = len(items)
            for idx in range(n_it + LA):
                if idx < n_it:
                    front(idx)
                if idx - LA >= 0:
                    back(idx - LA)
            P.op("act", lambda e: e.activation(out=rec.rearrange("p r t -> p (r t)"), in_=ps[7], func=AF.Ln), [t_ps[7]], [t_rec])
            P.op("act", lambda e: e.activation(out=rec.rearrange("p r t -> p (r t)"), in_=rec.rearrange("p r t -> p (r t)"),
                                               func=AF.Exp, scale=-1.0), [t_rec], [t_rec])
            P.op("dve", lambda e: e.tensor_tensor(out=o_atT[:, :, q0:q0 + 128],
                                                  in0=ps[6].rearrange("p (r t) -> p r t", r=4), in1=rec, op=ALU.mult),
                 [t_ps[6], t_rec], [t_oatT])

        def lockstep(gens):
            gens = list(gens)
            while gens:
                for g_ in list(gens):
                    try:
                        next(g_)
                    except StopIteration:
                        gens.remove(g_)

        lockstep([q_sb(0), q_sb(1)])
        for p_ in range(0, NT, 2):
            if p_ + 2 < NT:
                lockstep([q_sb(p_ + 2), q_sb(p_ + 3)])
            q_att(p_)
            q_att(p_ + 1)

    def c1_phase(sq, o_atT, t_oatT, o_gdnT, t_ogT, mkT2, t_mkT2, mv_b, t_mv):
        row_base = sq * SEQ
        alloc_stage(1024)
        Wo_a, t_Woa = sbt("Wo_a", [128, 4, 1024], BF16)
        Wo_g, t_Wog = sbt("Wo_g", [128, 4, 1024], BF16)
        Wmq, t_Wmq = sbt("Wmq", [128, 8, 256], BF16)
        Wmo, t_Wmo = sbt("Wmo", [128, 2, 1024], BF16)
        for r in range(4):
            i = k.wl % k.nst
            k.wl += 1
            st = stage[i][:, 0:1024]
            for g in range(2):
                P.dma("sp", st[64 * g:64 * g + 64, :], w_out[256 * g + 64 * r:256 * g + 64 * r + 64, :],
                      writes=[t_stage[i]])
            P.op("act", lambda e, st=st, r=r: e.activation(out=Wo_a[:, r, :], in_=st, func=AF.Copy),
                 [t_stage[i]], [t_Woa])
        for h in range(4):
            load_rows(Wo_g[:, h, :], t_Wog, w_out[512 + 128 * h:512 + 128 * h + 128, :], 1024, None, None)
        load_weight(Wmq, t_Wmq, w_mq, 0, 256, gX, t_gX)
        for a in range(2):
            load_rows(Wmo[:, a, :], t_Wmo, w_mo[128 * a:128 * a + 128, :], 1024, None, None)
        x1 = [sbt("x1_%d" % i, [128, D]) for i in range(2)]
        hT2, t_hT2 = sbt("hT2", [128, 8, 128], BF16)
        qmb2 = [sbt("qmb%d" % i, [128, 256], BF16) for i in range(2)]
        qmT2, t_qmT2 = sbt("qmT2", [128, 2, 128], BF16)
        PTm, t_PTm = sbt("PTm", [128, 2, 4, 128], BF16)
        omT2, t_omT2 = sbt("omT2", [128, 2, 128], BF16)
        recm, t_recm = sbt("recm", [128, 256])

        def c1_h1(t):
            r0 = row_base + t * 128
            c0 = t * 128
            if CCUT <= 0:
                return
            x, tx, i = load_x(xp[r0:r0 + 128, :], 128, key=("c", r0))
            if t + 1 < NT:
                prefetch_x(xp[r0 + 128:r0 + 256, :], 128, ("c", r0 + 128))
            xx, txx = x1[t % 2]
            qmb, t_qmb = qmb2[t % 2]
            for c in range(2):
                for r in range(4):
                    P.op("pe", lambda e, r=r, c=c: e.matmul(ps[1 + c], lhsT=o_atT[:, r, c0:c0 + 128],
                                                            rhs=Wo_a[:, r, c * 512:(c + 1) * 512], start=(r == 0), stop=False),
                         [t_oatT, t_Woa], [t_ps[1 + c]])
                for h in range(4):
                    P.op("pe", lambda e, h=h, c=c: e.matmul(ps[1 + c], lhsT=o_gdnT[:, h, c0:c0 + 128],
                                                            rhs=Wo_g[:, h, c * 512:(c + 1) * 512], start=False, stop=(h == 3)),
                         [t_ogT, t_Wog], [t_ps[1 + c]])
                P.op("dve", lambda e, c=c: e.tensor_tensor(out=xx[:, c * 512:(c + 1) * 512], in0=ps[1 + c],
                                                           in1=x[:, c * 512:(c + 1) * 512], op=ALU.add),
                     [t_ps[1 + c], tx], [txx])
            if CCUT <= 1:
                return
            norm_T(xx, txx, i, hT2, t_hT2, 0, 0)
            for kc in range(8):
                P.op("pe", lambda e, kc=kc: e.matmul(ps[3][:, 0:256], lhsT=hT2[:, kc, :], rhs=Wmq[:, kc, :],
                                                     start=(kc == 0), stop=(kc == 7)), [t_hT2, t_Wmq], [t_ps[3]])
            if CCUT <= 2:
                return
            head_rms(ps[3][:, 0:256], t_ps[3], 4, qscr, t_qscr, qss, t_qss)
            P.op("dve", lambda e: e.tensor_tensor(
                out=qmb.rearrange("p (h d) -> p h d", h=4), in0=ps[3][:, 0:256].rearrange("p (h d) -> p h d", h=4),
                in1=qss[:, 4:8].unsqueeze(2).to_broadcast([128, 4, 64]), op=ALU.mult), [t_ps[3], t_qss], [t_qmb])
        def c1_h2(t):
            r0 = row_base + t * 128
            c0 = t * 128
            xx, txx = x1[t % 2]
            qmb, t_qmb = qmb2[t % 2]
            pb4 = psb16[4]
            for a in range(2):
                P.op("pe", lambda e, a=a: e.transpose(out=pb4[:, a * 128:(a + 1) * 128], in_=qmb[:, a * 128:(a + 1) * 128],
                                                      identity=identb), [t_qmb, t_identb], [t_ps[4]])
            P.op("act", lambda e: e.activation(out=qmT2.rearrange("p a t -> p (a t)"), in_=pb4[:, 0:256], func=AF.Copy),
                 [t_ps[4]], [t_qmT2])
            P.op("pool", lambda e: e.tensor_scalar(out=qmT2.rearrange("p a t -> p (a t)"), in0=qmT2.rearrange("p a t -> p (a t)"),
                                                   scalar1=gmq8[:, 0:1], scalar2=1.0, op0=ALU.mult, op1=ALU.mult),
                 [t_qmT2, t_gmq8], [t_qmT2])
            if CCUT <= 3:
                return
            for b in range(2):
                for mb in range(2):
                    for a in range(2):
                        j = mb * 2 + a
                        P.op("pe", lambda e, mb=mb, a=a, b=b, j=j: e.matmul(
                            ps[5 + b][:, j * 128:(j + 1) * 128], lhsT=mkT2[64 * b:64 * b + 64, a, mb * 128:(mb + 1) * 128],
                            rhs=qmT2[64 * b:64 * b + 64, a, :], start=True, stop=True), [t_mkT2, t_qmT2], [t_ps[5 + b]])
                P.op("act", lambda e, b=b: e.activation(out=PTm[:, b, :, :].rearrange("p j t -> p (j t)"),
                                                        in_=ps[5 + b], func=AF.Exp), [t_ps[5 + b]], [t_PTm])
            if CCUT <= 4:
                return
            for mh in range(4):
                a, b = mh // 2, mh % 2
                for mb in range(2):
                    P.op("pe", lambda e, mb=mb, mh=mh, a=a, b=b: e.matmul(
                        ps[7][64 * b:64 * b + 64, a * 128:(a + 1) * 128], lhsT=mv_b[:, mb, mh * 64:(mh + 1) * 64],
                        rhs=PTm[:, b, mb * 2 + a, :], start=(mb == 0), stop=(mb == 1)), [t_mv, t_PTm], [t_ps[7]])
                for mb in range(2):
                    P.op("pe", lambda e, mb=mb, mh=mh, a=a, b=b: e.matmul(
                        ps[7][64 * b:64 * b + 64, 256 + a * 128:256 + (a + 1) * 128], lhsT=onesb[:, 0:64],
                        rhs=PTm[:, b, mb * 2 + a, :], start=(mb == 0), stop=(mb == 1)), [t_onesb, t_PTm], [t_ps[7]])
            if CCUT <= 5:
                return
            P.op("dve", lambda e: e.reciprocal(out=recm, in_=ps[7][:, 256:512]), [t_ps[7]], [t_recm])
            P.op("dve", lambda e: e.tensor_tensor(out=omT2.rearrange("p a t -> p (a t)"), in0=ps[7][:, 0:256], in1=recm,
                                                  op=ALU.mult), [t_ps[7], t_recm], [t_omT2])
            for c in range(2):
                for a in range(2):
                    P.op("pe", lambda e, a=a, c=c: e.matmul(ps[5 + c], lhsT=omT2[:, a, :],
                                                            rhs=Wmo[:, a, c * 512:(c + 1) * 512], start=(a == 0), stop=(a == 1)),
                         [t_omT2, t_Wmo], [t_ps[5 + c]])
                P.op("dve", lambda e, c=c: e.tensor_tensor(out=xx[:, c * 512:(c + 1) * 512], in0=ps[5 + c],
                                                           in1=xx[:, c * 512:(c + 1) * 512], op=ALU.add),
                     [t_ps[5 + c], txx], [txx])
            P.dma("sp", y_p[r0:r0 + 128, :], xx, reads=[txx])

        c1_h1(0)
        for t in range(NT):
            if t + 1 < NT:
                c1_h1(t + 1)
            c1_h2(t)

    t_yscr = Tok()

    def c2_phase(sq):
        row_base = sq * SEQ
        alloc_stage(2816)
        Wg, t_Wg = sbt("Wg", [128, 8, 2816], BF16)
        Wu, t_Wu = sbt("Wu", [128, 8, 2816], BF16)
        Wd, t_Wd = sbt("Wd", [128, 22, 1024], BF16)
        load_weight(Wg, t_Wg, w_gate, 0, 2816, gF, t_gF)
        load_weight(Wu, t_Wu, w_up, 0, 2816, gF, t_gF)
        for f in range(22):
            load_rows(Wd[:, f, :], t_Wd, w_down[128 * f:128 * f + 128, :], 1024, None, None)
        hT3, t_hT3 = sbt("hT3", [128, 8, 256], BF16)
        hfT, t_hfT = sbt("hfT", [128, 22, 256], BF16)
        sg = [sbt("sg%d" % i, [128, 256]) for i in range(2)]

        def c2_group(gi):
            xs_ = []
            for t2 in range(2):
                r0 = row_base + (gi * 2 + t2) * 128
                x, tx, i = load_x(y_p[r0:r0 + 128, :], 128)
                norm_T(x, tx, i, hT3, t_hT3, t2 * 128, 0)
                xs_.append((x, tx, r0))
            for f in range(22):
                b = 1 + (f % 2)
                for kc in range(8):
                    P.op("pe", lambda e, kc=kc, f=f, b=b: e.matmul(ps[b][:, 0:256], lhsT=Wg[:, kc, 128 * f:128 * f + 128],
                                                                   rhs=hT3[:, kc, :], start=(kc == 0), stop=(kc == 7)),
                         [t_Wg, t_hT3], [t_ps[b]])
                for kc in range(8):
                    P.op("pe", lambda e, kc=kc, f=f, b=b: e.matmul(ps[b][:, 256:512], lhsT=Wu[:, kc, 128 * f:128 * f + 128],
                                                                   rhs=hT3[:, kc, :], start=(kc == 0), stop=(kc == 7)),
                         [t_Wu, t_hT3], [t_ps[b]])
                s_, ts_ = sg[f % 2]
                P.op("act", lambda e, s_=s_, b=b: e.activation(out=s_, in_=ps[b][:, 0:256], func=AF.Silu), [t_ps[b]], [ts_])
                P.op("dve", lambda e, s_=s_, b=b, f=f: e.tensor_tensor(out=hfT[:, f, :], in0=s_, in1=ps[b][:, 256:512],
                                                                       op=ALU.mult), [ts_, t_ps[b]], [t_hfT])
            for t2 in range(2):
                x, tx, r0 = xs_[t2]
                for c in range(2):
                    for f in range(22):
                        P.op("pe", lambda e, f=f, c=c, t2=t2: e.matmul(ps[3 + c], lhsT=hfT[:, f, t2 * 128:(t2 + 1) * 128],
                                                                       rhs=Wd[:, f, c * 512:(c + 1) * 512],
                                                                       start=(f == 0), stop=(f == 21)),
                             [t_hfT, t_Wd], [t_ps[3 + c]])
                    P.op("dve", lambda e, c=c, x=x: e.tensor_tensor(out=x[:, c * 512:(c + 1) * 512], in0=ps[3 + c],
                                                                    in1=x[:, c * 512:(c + 1) * 512], op=ALU.add),
                         [t_ps[3 + c], tx], [tx])
                P.dma("sp", y_p[r0:r0 + 128, :], x, reads=[tx])

        for gi in range(NT // 2):
            c2_group(gi)

    def sample_group():
        k.top = PERSIST_TOP
        NSB = NS
        proj, t_proj = sbt("s_proj", [128, INW])
        x_keep, t_xk = sbt("s_xkeep", [128, D])
        sso, t_sso = sbt("s_o", [128, 320])
        ss_, t_ss = sbt("s_ss", [128, 64])
        scr, t_scr = sbt("s_scr", [128, 1536])
        S_TOP = k.top
        alloc_stage(INW)
        wAll, t_wAll = sbt("s_wAll", [128, 8, INW], BF16)
        load_weight(wAll, t_wAll, w_in, 0, INW, gA, t_gA)
        hTs, t_hTs = sbt("s_hT", [128, 8, 128], BF16)
        x, tx, xi = load_x(xs[:, :], NSB)
        P.op("pool", lambda e: e.tensor_copy(out=x_keep[0:NSB, :], in_=x[0:NSB, :]), [tx], [t_xk])
        norm_T(x, tx, xi, hTs, t_hTs, 0, 0)
        for c in range(7):
            c0 = c * 512
            n = min(512, INW - c0)
            b = 1 + (c % 2)
            for kc in range(8):
                P.op("pe", lambda e, kc=kc, b=b, c0=c0, n=n: e.matmul(
                    ps[b][0:NSB, 0:n], lhsT=hTs[:, kc, 0:NSB], rhs=wAll[:, kc, c0:c0 + n],
                    start=(kc == 0), stop=(kc == 7)), [t_hTs, t_wAll], [t_ps[b]])
            P.op("act", lambda e, b=b, c0=c0, n=n: e.activation(out=proj[0:NSB, c0:c0 + n], in_=ps[b][0:NSB, 0:n],
                                                                func=AF.Copy), [t_ps[b]], [t_proj])
        pj = proj[0:NSB]
        so = sso[0:NSB]
        P.op("dve", lambda e: e.tensor_tensor(out=scr[0:NSB, 0:128], in0=pj[:, 512:640], in1=pj[:, 512:640], op=ALU.mult),
             [t_proj], [t_scr])
        P.op("dve", lambda e: e.tensor_reduce(out=ss_[0:NSB, 0:2], in_=scr[0:NSB, 0:128].rearrange("p (h d) -> p h d", h=2),
                                              axis=AX.X, op=ALU.add), [t_scr], [t_ss])
        P.op("pool", lambda e: e.tensor_scalar(out=ss_[0:NSB, 2:4], in0=ss_[0:NSB, 0:2], scalar1=1.0 / 64, scalar2=EPS,
                                               op0=ALU.mult, op1=ALU.add), [t_ss], [t_ss])
        P.op("pool", lambda e: e.tensor_tensor(out=ss_[0:NSB, 2:4], in0=ss_[0:NSB, 2:4], in1=neghalf[0:NSB, 0:2], op=ALU.pow),
             [t_ss, t_nh], [t_ss])
        P.op("dve", lambda e: e.tensor_tensor(out=so[:, 0:128].rearrange("p (h d) -> p h d", h=2),
                                              in0=pj[:, 512:640].rearrange("p (h d) -> p h d", h=2),
                                              in1=ss_[0:NSB, 2:4].unsqueeze(2).to_broadcast([NSB, 2, 64]), op=ALU.mult),
             [t_proj, t_ss], [t_sso])
        P.op("pool", lambda e: e.tensor_tensor(out=so[:, 0:128].rearrange("p (h d) -> p h d", h=2),
                                               in0=so[:, 0:128].rearrange("p (h d) -> p h d", h=2),
                                               in1=gk_b[0:NSB].unsqueeze(1).to_broadcast([NSB, 2, 64]), op=ALU.mult),
             [t_sso, t_gk], [t_sso])
        P.op("act", lambda e: e.activation(out=so[:, 128:256], in_=pj[:, 640:768], func=AF.Copy), [t_proj], [t_sso])
        P.op("act", lambda e: e.activation(out=so[:, 256:320], in_=pj[:, 1024:1088], func=AF.Copy), [t_proj], [t_sso])
        P.dma("sp", k_s[:, :], so[:, 0:128], reads=[t_sso])
        P.dma("sp", v_s[:, :], so[:, 128:256], reads=[t_sso])
        P.dma("sp", kidx_s[:, :], so[:, 256:320], reads=[t_sso])
        conv_s3 = conv_s.rearrange("(s r) c -> s r c", r=3)
        st_conv3 = st_conv.rearrange("(s r) c -> s r c", r=3)
        P.dma("sp", conv_s3[:, 2, :], pj[:, 1092:2628], reads=[t_proj])
        P.dma("sp", conv_s3[:, 0:2, :], st_conv3[:, 1:3, :])
        P.barrier()
        k.top = S_TOP
        stc, t_stc = sbt("s_stc", [128, 3, 1536])
        cwb, t_cwb = sbt("s_cwb", [128, 4, 1536])
        P.dma("sp", stc[0:NSB], st_conv3, writes=[t_stc])
        P.dma("sp", cwb[0:NSB], conv_w.rearrange("t c -> (t c)").partition_broadcast(NSB), writes=[t_cwb])
        cc, t_cc = sbt("s_cc", [128, 1536])
        c_ = cc[0:NSB]
        sc_ = scr[0:NSB]
        P.op("dve", lambda e: e.tensor_tensor(out=c_, in0=pj[:, 1092:2628], in1=cwb[0:NSB, 3, :], op=ALU.mult),
             [t_proj, t_cwb], [t_cc])
        for j in range(3):
            P.op("dve", lambda e, j=j: e.tensor_tensor(out=sc_, in0=stc[0:NSB, j, :], in1=cwb[0:NSB, j, :], op=ALU.mult),
                 [t_stc, t_cwb], [t_scr])
            P.op("dve", lambda e: e.tensor_tensor(out=c_, in0=c_, in1=sc_, op=ALU.add), [t_cc, t_scr], [t_cc])
        P.op("act", lambda e: e.activation(out=c_, in_=c_, func=AF.Silu), [t_cc], [t_cc])
        P.op("dve", lambda e: e.tensor_tensor(out=sc_[:, 0:1024], in0=c_[:, 0:1024], in1=c_[:, 0:1024], op=ALU.mult),
             [t_cc], [t_scr])
        P.op("dve", lambda e: e.tensor_reduce(out=ss_[0:NSB, 8:16], in_=sc_[:, 0:1024].rearrange("p (h d) -> p h d", h=8),
                                              axis=AX.X, op=ALU.add), [t_scr], [t_ss])
        P.op("pool", lambda e: e.tensor_scalar(out=ss_[0:NSB, 16:24], in0=ss_[0:NSB, 8:16], scalar1=1.0, scalar2=EPS,
                                               op0=ALU.mult, op1=ALU.add), [t_ss], [t_ss])
        P.op("pool", lambda e: e.tensor_tensor(out=ss_[0:NSB, 16:24], in0=ss_[0:NSB, 16:24], in1=neghalf[0:NSB, 0:8], op=ALU.pow),
             [t_ss, t_nh], [t_ss])
        P.op("pool", lambda e: e.tensor_scalar(out=ss_[0:NSB, 16:20], in0=ss_[0:NSB, 16:20], scalar1=float(128.0 ** -0.5),
                                               scalar2=1.0, op0=ALU.mult, op1=ALU.mult), [t_ss], [t_ss])
        P.op("dve", lambda e: e.tensor_tensor(out=c_[:, 0:1024].rearrange("p (h d) -> p h d", h=8),
                                              in0=c_[:, 0:1024].rearrange("p (h d) -> p h d", h=8),
                                              in1=ss_[0:NSB, 16:24].unsqueeze(2).to_broadcast([NSB, 8, 128]), op=ALU.mult),
             [t_cc, t_ss], [t_cc])
        sv = ss_[0:NSB]
        P.op("dve", lambda e: e.tensor_tensor(out=sv[:, 24:28], in0=pj[:, 3140:3144], in1=dtb_b[0:NSB], op=ALU.add),
             [t_proj, t_dtb], [t_ss])
        P.op("dve", lambda e: e.tensor_scalar(out=sv[:, 28:32], in0=sv[:, 24:28], scalar1=-1.0, scalar2=None, op0=ALU.mult),
             [t_ss], [t_ss])
        P.op("dve", lambda e: e.tensor_tensor(out=sv[:, 28:32], in0=sv[:, 28:32], in1=sv[:, 24:28], op=ALU.min), [t_ss], [t_ss])
        P.op("act", lambda e: e.activation(out=sv[:, 28:32], in_=sv[:, 28:32], func=AF.Exp), [t_ss], [t_ss])
        P.op("act", lambda e: e.activation(out=sv[:, 28:32], in_=sv[:, 28:32], func=AF.Ln, bias=1.0, scale=1.0), [t_ss], [t_ss])
        P.op("dve", lambda e: e.scalar_tensor_tensor(out=sv[:, 32:36], in0=sv[:, 24:28], scalar=0.0, in1=sv[:, 28:32],
                                                     op0=ALU.max, op1=ALU.add), [t_ss], [t_ss])
        P.op("dve", lambda e: e.tensor_tensor(out=sv[:, 32:36], in0=sv[:, 32:36], in1=nea_b[0:NSB], op=ALU.mult),
             [t_ss, t_nea], [t_ss])
        P.op("act", lambda e: e.activation(out=sv[:, 36:40], in_=pj[:, 3144:3148], func=AF.Sigmoid), [t_proj], [t_ss])
        P.op("act", lambda e: e.activation(out=sv[:, 40:44], in_=sv[:, 32:36], func=AF.Exp), [t_ss], [t_ss])
        S0, t_S0 = sbt("s_S0", [128, NSB, 4, 128])
        for i in range(NSB):
            P.dma("sp", S0[:, i, :, :], ssm_in[i * 512:(i + 1) * 512, :].rearrange("(h d) v -> d h v", h=4), writes=[t_S0])
        kqT, t_kqT = sbt("s_kqT", [128, 8, NSB])
        for j in range(8):
            b = 1 + (j % 2)
            P.op("pe", lambda e, j=j, b=b: e.transpose(out=ps[b][:, 0:NSB], in_=c_[:, j * 128:(j + 1) * 128],
                                                       identity=identf[0:NSB, 0:NSB]), [t_cc, t_identf], [t_ps[b]])
            P.op("act", lambda e, j=j, b=b: e.activation(out=kqT[:, j, :], in_=ps[b][:, 0:NSB], func=AF.Copy),
                 [t_ps[b]], [t_kqT])
        eye_b, t_eyeb = sbt("s_eyeb", [128, NSB, NSB])
        P.dma("sp", eye_b, eye16_d.partition_broadcast(128), writes=[t_eyeb])
        kqTm, t_kqTm = sbt("s_kqTm", [128, 8, NSB, NSB])
        P.op("pool", lambda e: e.tensor_tensor(out=kqTm, in0=kqT.unsqueeze(2).to_broadcast([128, 8, NSB, NSB]),
                                               in1=eye_b.unsqueeze(1).to_broadcast([128, 8, NSB, NSB]), op=ALU.mult),
             [t_kqT, t_eyeb], [t_kqTm])
        for h in range(4):
            for i in range(NSB):
                P.op("pe", lambda e, h=h, i=i: e.matmul(ps[3][0:NSB, h * 128:(h + 1) * 128], lhsT=kqTm[:, 4 + h, i, :],
                                                        rhs=S0[:, i, h, :], start=(i == 0), stop=(i == NSB - 1)),
                     [t_kqTm, t_S0], [t_ps[3]])
        dl, t_dl = sbt("s_dl", [128, 4, 128])
        d_ = dl[0:NSB]
        bcs = lambda a: a.unsqueeze(2).to_broadcast([NSB, 4, 128])
        P.op("dve", lambda e: e.tensor_tensor(out=d_, in0=ps[3][0:NSB, :].rearrange("p (h v) -> p h v", h=4),
                                              in1=bcs(sv[:, 40:44]), op=ALU.mult), [t_ps[3], t_ss], [t_dl])
        P.op("dve", lambda e: e.tensor_tensor(out=d_, in0=c_[:, 1024:1536].rearrange("p (h v) -> p h v", h=4), in1=d_,
                                              op=ALU.subtract), [t_cc, t_dl], [t_dl])
        P.op("dve", lambda e: e.tensor_tensor(out=d_, in0=d_, in1=bcs(sv[:, 36:40]), op=ALU.mult), [t_dl, t_ss], [t_dl])
        ckm, t_ckm = sbt("s_ckm", [128, NSB, 512])
        P.op("pool", lambda e: e.tensor_tensor(out=ckm[0:NSB], in0=c_[:, 512:1024].unsqueeze(1).to_broadcast([NSB, NSB, 512]),
                                               in1=identf[0:NSB, 0:NSB].unsqueeze(2).to_broadcast([NSB, NSB, 512]), op=ALU.mult),
             [t_cc, t_identf], [t_ckm])
        adg, t_adg = sbt("s_adg", [128, NSB, 4])
        P.op("pool", lambda e: e.tensor_tensor(out=adg[0:NSB], in0=sv[:, 40:44].unsqueeze(1).to_broadcast([NSB, NSB, 4]),
                                               in1=identf[0:NSB, 0:NSB].unsqueeze(2).to_broadcast([NSB, NSB, 4]), op=ALU.mult),
             [t_ss, t_identf], [t_adg])
        P.op("pe", lambda e: e.matmul(ps[4][:, 0:NSB * 4], lhsT=onesf[0:NSB, :], rhs=adg[0:NSB].rearrange("p i h -> p (i h)"),
                                      start=True, stop=True), [t_adg, t_onesf], [t_ps[4]])
        abc, t_abc = sbt("s_abc", [128, NSB * 4])
        P.op("act", lambda e: e.activation(out=abc, in_=ps[4][:, 0:NSB * 4], func=AF.Copy), [t_ps[4]], [t_abc])
        for i in range(NSB):
            b = 5 + (i % 2)
            for h in range(4):
                P.op("pe", lambda e, h=h, i=i, b=b: e.matmul(ps[b][:, h * 128:(h + 1) * 128], lhsT=ckm[0:NSB, i, h * 128:(h + 1) * 128],
                                                             rhs=d_[:, h, :], start=True, stop=True), [t_ckm, t_dl], [t_ps[b]])
            for h in range(4):
                P.op("dve", lambda e, h=h, i=i, b=b: e.scalar_tensor_tensor(
                    out=S0[:, i, h, :], in0=S0[:, i, h, :], scalar=abc[:, i * 4 + h:i * 4 + h + 1],
                    in1=ps[b][:, h * 128:(h + 1) * 128], op0=ALU.mult, op1=ALU.add), [t_S0, t_abc, t_ps[b]], [t_S0])
            P.dma("sp", ssm_s[i * 512:(i + 1) * 512, :].rearrange("(h d) v -> d h v", h=4), S0[:, i, :, :], reads=[t_S0])
        for h in range(4):
            for i in range(NSB):
                P.op("pe", lambda e, h=h, i=i: e.matmul(ps[7][0:NSB, h * 128:(h + 1) * 128], lhsT=kqTm[:, h, i, :],
                                                        rhs=S0[:, i, h, :], start=(i == 0), stop=(i == NSB - 1)),
                     [t_kqTm, t_S0], [t_ps[7]])
        og, t_og = sbt("s_og", [128, 4, 128])
        o_ = og[0:NSB]
        P.op("act", lambda e: e.activation(out=sc_[:, 0:512], in_=ps[7][0:NSB, :], func=AF.Square), [t_ps[7]], [t_scr])
        P.op("dve", lambda e: e.tensor_reduce(out=sv[:, 44:48], in_=sc_[:, 0:512].rearrange("p (h v) -> p h v", h=4),
                                              axis=AX.X, op=ALU.add), [t_scr], [t_ss])
        P.op("pool", lambda e: e.tensor_scalar(out=sv[:, 48:52], in0=sv[:, 44:48], scalar1=1.0 / 128, scalar2=EPS,
                                               op0=ALU.mult, op1=ALU.add), [t_ss], [t_ss])
        P.op("pool", lambda e: e.tensor_tensor(out=sv[:, 48:52], in0=sv[:, 48:52], in1=neghalf[0:NSB, 0:4], op=ALU.pow),
             [t_ss, t_nh], [t_ss])
        P.op("dve", lambda e: e.tensor_tensor(out=o_, in0=ps[7][0:NSB, :].rearrange("p (h v) -> p h v", h=4),
                                              in1=bcs(sv[:, 48:52]), op=ALU.mult), [t_ps[7], t_ss], [t_og])
        P.op("pool", lambda e: e.tensor_tensor(out=o_, in0=o_, in1=ggdn_b[0:NSB].unsqueeze(1).to_broadcast([NSB, 4, 128]),
                                               op=ALU.mult), [t_og, t_ggdn], [t_og])
        P.op("act", lambda e: e.activation(out=sc_[:, 0:512], in_=pj[:, 2628:3140], func=AF.Silu), [t_proj], [t_scr])
        P.op("dve", lambda e: e.tensor_tensor(out=o_.rearrange("p h v -> p (h v)"), in0=o_.rearrange("p h v -> p (h v)"),
                                              in1=sc_[:, 0:512], op=ALU.mult), [t_og, t_scr], [t_og])
        P.op("pool", lambda e: e.tensor_copy(out=pj[:, 1092:1604], in_=o_.rearrange("p h v -> p (h v)")), [t_og], [t_proj])
        P.barrier()
        k.top = S_TOP
        if STOP == -1:
            return
        sample_dsa(proj, t_proj, sso, t_sso)
        sample_tail(proj, t_proj, x_keep, t_xk)

    def sample_dsa(proj, t_proj, sso, t_sso):
        NSB = NS
        pj = proj[0:NSB]
        U32 = mybir.dt.uint32
        selp, t_selp = sbt("s_selp", [128, 8, 128])
        selo, t_selo = sbt("s_selo", [128, NSB, 128])
        P.dma("sp", selp[0:NSB], selpair_d.rearrange("q i p -> i q p"), writes=[t_selp])
        P.dma("sp", selo[0:NSB], selone_d, writes=[t_selo])
        gq_b, t_gqb = bcast_layout("s_gq_b", g_q, 64)
        iota_b, t_iota = bcast_layout("s_iota", iota64_d, 64)
        pt_i, t_pti = sbt("s_pt_i", [128, 64], I32)
        pt_f, t_ptf = sbt("s_pt_f", [128, 64])
        P.dma("sp", pt_i[0:NSB], ptab, writes=[t_pti])
        P.op("dve", lambda e: e.tensor_copy(out=pt_f[0:NSB], in_=pt_i[0:NSB]), [t_pti], [t_ptf])
        ss2, t_ss2 = sbt("s_ss2", [128, 32])
        scr2, t_scr2 = sbt("s_scr2", [128, 512])
        qn, t_qn = sbt("s_qn", [128, 512])
        qiw, t_qiw = sbt("s_qiw", [128, 260])
        sv = ss2[0:NSB]
        P.op("dve", lambda e: e.tensor_tensor(out=scr2[0:NSB], in0=pj[:, 0:512], in1=pj[:, 0:512], op=ALU.mult), [t_proj], [t_scr2])
        P.op("dve", lambda e: e.tensor_reduce(out=sv[:, 0:8], in_=scr2[0:NSB].rearrange("p (h d) -> p h d", h=8), axis=AX.X,
                                              op=ALU.add), [t_scr2], [t_ss2])
        P.op("pool", lambda e: e.tensor_scalar(out=sv[:, 8:16], in0=sv[:, 0:8], scalar1=1.0 / 64, scalar2=EPS, op0=ALU.mult,
                                               op1=ALU.add), [t_ss2], [t_ss2])
        P.op("pool", lambda e: e.tensor_tensor(out=sv[:, 8:16], in0=sv[:, 8:16], in1=neghalf[0:NSB, 0:8], op=ALU.pow),
             [t_ss2, t_nh], [t_ss2])
        P.op("pool", lambda e: e.tensor_scalar(out=sv[:, 8:16], in0=sv[:, 8:16], scalar1=0.125, scalar2=1.0, op0=ALU.mult,
                                               op1=ALU.mult), [t_ss2], [t_ss2])
        q3 = qn[0:NSB].rearrange("p (h d) -> p h d", h=8)
        P.op("dve", lambda e: e.tensor_tensor(out=q3, in0=pj[:, 0:512].rearrange("p (h d) -> p h d", h=8),
                                              in1=sv[:, 8:16].unsqueeze(2).to_broadcast([NSB, 8, 64]), op=ALU.mult),
             [t_proj, t_ss2], [t_qn])
        P.op("pool", lambda e: e.tensor_tensor(out=q3, in0=q3, in1=gq_b[0:NSB].unsqueeze(1).to_broadcast([NSB, 8, 64]),
                                               op=ALU.mult), [t_qn, t_gqb], [t_qn])
        P.op("act", lambda e: e.activation(out=qiw[0:NSB, 0:256], in_=pj[:, 768:1024], func=AF.Copy, scale=0.125), [t_proj], [t_qiw])
        P.op("act", lambda e: e.activation(out=qiw[0:NSB, 256:260], in_=pj[:, 1088:1092], func=AF.Copy, scale=0.5), [t_proj], [t_qiw])
        scores, t_scores = sbt("s_scores", [128, 8200])
        P.op("dve", lambda e: e.tensor_tensor(out=scr2[0:NSB, 0:256].rearrange("p (h d) -> p h d", h=4),
                                              in0=qiw[0:NSB, 0:256].rearrange("p (h d) -> p h d", h=4),
                                              in1=pj[:, 1024:1088].unsqueeze(1).to_broadcast([NSB, 4, 64]), op=ALU.mult),
             [t_qiw, t_proj], [t_scr2])
        P.op("dve", lambda e: e.tensor_reduce(out=sv[:, 16:20], in_=scr2[0:NSB, 0:256].rearrange("p (h d) -> p h d", h=4),
                                              axis=AX.X, op=ALU.add), [t_scr2], [t_ss2])
        P.op("dve", lambda e: e.tensor_scalar(out=sv[:, 16:20], in0=sv[:, 16:20], scalar1=0.0, scalar2=None, op0=ALU.max),
             [t_ss2], [t_ss2])
        P.op("dve", lambda e: e.tensor_tensor(out=sv[:, 16:20], in0=sv[:, 16:20], in1=qiw[0:NSB, 256:260], op=ALU.mult),
             [t_ss2, t_qiw], [t_ss2])
        P.op("dve", lambda e: e.tensor_reduce(out=scores[0:NSB, 8192:8193], in_=sv[:, 16:20], axis=AX.X, op=ALU.add),
             [t_ss2], [t_scores])
        osT, t_osT = sbt("s_osT", [128, NSB, 8], BF16)
        K_TOP = k.top
        k.K_TOP = K_TOP
        kid = [sbt("s_kid%d" % i, [128, 8192]) for i in range(2)]
        prod, t_prod = sbt("s_prod", [128, 8192])
        ptc = [sbt("s_ptc%d" % i, [128, 1], I32) for i in range(2)]
        qrep, t_qrep = sbt("s_qrep", [128, 260])
        zz, t_zz = sbt("s_zz", [128, 128])
        sc1, t_sc1 = sbt("s_sc1", [128, 128])
        sc2 = [sbt("s_sc2_%d" % i, [128, 128]) for i in range(2)]
        kidx_pages = cache_kidx_d

        def pair(q):
            kd, tkd = kid[q % 2]
            pc, tpc = ptc[q % 2]
            P.dma("sp", pc, ptab[2 * q:2 * q + 2, :].rearrange("s (j o) -> (s j) o", o=1), writes=[tpc])
            P.dma("pool", kd, kidx_pages, reads=[tpc], writes=[tkd],
                  indirect=bass.IndirectOffsetOnAxis(ap=pc, axis=0))
            P.op("pe", lambda e: e.matmul(ps[1][:, 0:260], lhsT=selp[0:NSB, q, :], rhs=qiw[0:NSB, :], start=True, stop=True),
                 [t_selp, t_qiw], [t_ps[1]])
            P.op("act", lambda e: e.activation(out=qrep, in_=ps[1][:, 0:260], func=AF.Copy), [t_ps[1]], [t_qrep])
            so_, tso = sc2[q % 2]
            for h in range(4):
                P.op("pool", lambda e, h=h: e.tensor_tensor(
                    out=prod.rearrange("p (o d) -> p o d", d=64), in0=kd.rearrange("p (o d) -> p o d", d=64),
                    in1=qrep[:, h * 64:(h + 1) * 64].unsqueeze(1).to_broadcast([128, 128, 64]), op=ALU.mult),
                    [tkd, t_qrep], [t_prod])
                P.op("dve", lambda e: e.tensor_reduce(out=zz, in_=prod.rearrange("p (o d) -> p o d", d=64), axis=AX.X,
                                                      op=ALU.add), [t_prod], [t_zz])
                if h == 0:
                    P.op("dve", lambda e: e.tensor_scalar(out=so_, in0=zz, scalar1=0.0, scalar2=qrep[:, 256:257],
                                                          op0=ALU.max, op1=ALU.mult), [t_zz, t_qrep], [tso])
                else:
                    P.op("dve", lambda e, h=h: e.tensor_scalar(out=sc1, in0=zz, scalar1=0.0, scalar2=qrep[:, 256 + h:257 + h],
                                                               op0=ALU.max, op1=ALU.mult), [t_zz, t_qrep], [t_sc1])
                    P.op("dve", lambda e: e.tensor_tensor(out=so_, in0=so_, in1=sc1, op=ALU.add), [tso, t_sc1], [tso])
            for s2 in range(2):
                r = 2 * q + s2
                P.dma("sp", scores[r:r + 1, 0:8192].rearrange("p (j o) -> p j o", o=128), so_[64 * s2:64 * s2 + 64, :],
                      reads=[tso], writes=[t_scores])

        for q in range(NSB // 2):
            pair(q)
        P.barrier()
        k.top = K_TOP
        mx, t_mx = sbt("s_mx", [128, 256])
        ix, t_ix = sbt("s_ix", [128, 256], U32)
        TK_TOP = k.top
        W, t_W = sbt("s_W", [128, 8200])
        Wv = W[0:NSB, 0:8193]
        P.op("pool", lambda e: e.tensor_copy(out=Wv, in_=scores[0:NSB, 0:8193]), [t_scores], [t_W])
        for r in range(32):
            P.op("dve", lambda e, r=r: e.max(out=mx[0:NSB, 8 * r:8 * r + 8], in_=Wv), [t_W], [t_mx])
            P.op("dve", lambda e, r=r: e.max_index(out=ix[0:NSB, 8 * r:8 * r + 8], in_max=mx[0:NSB, 8 * r:8 * r + 8],
                                                   in_values=Wv), [t_W, t_mx], [t_ix])
            P.op("dve", lambda e, r=r: e.match_replace(out=Wv, in_to_replace=mx[0:NSB, 8 * r:8 * r + 8], in_values=Wv,
                                                       imm_value=-1e30), [t_W, t_mx], [t_W])
        P.barrier()
        k.top = TK_TOP
        ixf, t_ixf = sbt("s_ixf", [128, 256])
        pgu, t_pgu = sbt("s_pgu", [128, 256], U32)
        pgf, t_pgf = sbt("s_pgf", [128, 256])
        offf, t_offf = sbt("s_offf", [128, 256])
        eq, t_eq = sbt("s_eq", [128, 256, 64])
        phys, t_phys = sbt("s_phys", [128, 256])
        isf, t_isf = sbt("s_isf", [128, 256])
        n_ = lambda a: a[0:NSB]
        P.op("dve", lambda e: e.tensor_copy(out=n_(ixf), in_=n_(ix)), [t_ix], [t_ixf])
        P.op("dve", lambda e: e.tensor_scalar(out=n_(pgu), in0=n_(ix), scalar1=7, scalar2=None, op0=ALU.logical_shift_right),
             [t_ix], [t_pgu])
        P.op("dve", lambda e: e.tensor_copy(out=n_(pgf), in_=n_(pgu)), [t_pgu], [t_pgf])
        P.op("dve", lambda e: e.scalar_tensor_tensor(out=n_(offf), in0=n_(pgf), scalar=-128.0, in1=n_(ixf), op0=ALU.mult,
                                                     op1=ALU.add), [t_pgf, t_ixf], [t_offf])
        P.op("dve", lambda e: e.tensor_tensor(out=n_(eq), in0=n_(pgf).unsqueeze(2).to_broadcast([NSB, 256, 64]),
                                              in1=n_(iota_b).unsqueeze(1).to_broadcast([NSB, 256, 64]), op=ALU.is_equal),
             [t_pgf, t_iota], [t_eq])
        P.op("dve", lambda e: e.tensor_tensor(out=n_(eq), in0=n_(eq), in1=n_(pt_f).unsqueeze(1).to_broadcast([NSB, 256, 64]),
                                              op=ALU.mult), [t_eq, t_ptf], [t_eq])
        P.op("dve", lambda e: e.tensor_reduce(out=n_(phys), in_=n_(eq), axis=AX.X, op=ALU.add), [t_eq], [t_phys])
        P.op("dve", lambda e: e.scalar_tensor_tensor(out=n_(phys), in0=n_(phys), scalar=128.0, in1=n_(offf), op0=ALU.mult,
                                                     op1=ALU.add), [t_phys, t_offf], [t_phys])
        P.op("dve", lambda e: e.tensor_scalar(out=n_(isf), in0=n_(ixf), scalar1=8191.5, scalar2=None, op0=ALU.is_ge),
             [t_ixf], [t_isf])
        physT, t_physT = sbt("s_physT", [128, 2, NSB], I32)
        isT, t_isT = sbt("s_isT", [128, 2, NSB])
        for b in range(2):
            P.op("pe", lambda e, b=b: e.transpose(out=ps[1][:, b * NSB:(b + 1) * NSB], in_=phys[0:NSB, b * 128:(b + 1) * 128],
                                                  identity=identf[0:NSB, 0:NSB]), [t_phys, t_identf], [t_ps[1]])
            P.op("pe", lambda e, b=b: e.transpose(out=ps[2][:, b * NSB:(b + 1) * NSB], in_=isf[0:NSB, b * 128:(b + 1) * 128],
                                                  identity=identf[0:NSB, 0:NSB]), [t_isf, t_identf], [t_ps[2]])
        P.op("dve", lambda e: e.tensor_copy(out=physT.rearrange("p b i -> p (b i)"), in_=ps[1][:, 0:2 * NSB]), [t_ps[1]], [t_physT])
        P.op("act", lambda e: e.activation(out=isT.rearrange("p b i -> p (b i)"), in_=ps[2][:, 0:2 * NSB], func=AF.Copy),
             [t_ps[2]], [t_isT])
        Kg = [sbt("s_Kg%d" % i, [128, 2, 128]) for i in range(2)]
        Vg = [sbt("s_Vg%d" % i, [128, 2, 128]) for i in range(2)]
        kvrep, t_kvrep = sbt("s_kvrep", [128, 256])
        dif, t_dif = sbt("s_dif", [128, 128])
        prd, t_prd = sbt("s_prd", [128, 512])
        lg, t_lg = sbt("s_lg", [128, 2, 8])
        rcp, t_rcp = sbt("s_rcp", [128, NSB * 8])

        def att(i):
            kg, tkg = Kg[i % 2]
            vg, tvg = Vg[i % 2]
            for b in range(2):
                P.dma("pool", kg[:, b, :], cache_k_d, reads=[t_physT], writes=[tkg],
                      indirect=bass.IndirectOffsetOnAxis(ap=physT[:, b, i:i + 1], axis=0))
                P.dma("pool", vg[:, b, :], cache_v_d, reads=[t_physT], writes=[tvg],
                      indirect=bass.IndirectOffsetOnAxis(ap=physT[:, b, i:i + 1], axis=0))
            P.op("pe", lambda e: e.matmul(ps[3], lhsT=selo[0:NSB, i, :], rhs=qn[0:NSB, :], start=True, stop=True),
                 [t_selo, t_qn], [t_ps[3]])
            P.op("pe", lambda e: e.matmul(ps[4][:, 0:256], lhsT=selo[0:NSB, i, :], rhs=sso[0:NSB, 0:256], start=True, stop=True),
                 [t_selo, t_sso], [t_ps[4]])
            P.op("act", lambda e: e.activation(out=kvrep, in_=ps[4][:, 0:256], func=AF.Copy), [t_ps[4]], [t_kvrep])
            for b in range(2):
                for (t_, tt_, c0) in ((kg, tkg, 0), (vg, tvg, 128)):
                    P.op("dve", lambda e, t_=t_, c0=c0, b=b: e.tensor_tensor(out=dif, in0=kvrep[:, c0:c0 + 128], in1=t_[:, b, :],
                                                                             op=ALU.subtract), [t_kvrep, tt_], [t_dif])
                    P.op("dve", lambda e, t_=t_, b=b: e.scalar_tensor_tensor(out=t_[:, b, :], in0=dif, scalar=isT[:, b, i:i + 1],
                                                                             in1=t_[:, b, :], op0=ALU.mult, op1=ALU.add),
                         [t_dif, t_isT, tt_], [tt_])
                P.op("dve", lambda e, b=b: e.tensor_tensor(
                    out=prd.rearrange("p (g r d) -> p g r d", g=2, r=4),
                    in0=ps[3].rearrange("p (g r d) -> p g r d", g=2, r=4),
                    in1=kg[:, b, :].rearrange("p (g d) -> p g d", g=2).unsqueeze(2).to_broadcast([128, 2, 4, 64]),
                    op=ALU.mult), [t_ps[3], tkg], [t_prd])
                P.op("dve", lambda e, b=b: e.tensor_reduce(out=lg[:, b, :], in_=prd.rearrange("p (h d) -> p h d", h=8),
                                                           axis=AX.X, op=ALU.add), [t_prd], [t_lg])
            P.op("act", lambda e: e.activation(out=lg, in_=lg, func=AF.Exp), [t_lg], [t_lg])
            for g in range(2):
                for b in range(2):
                    P.op("pe", lambda e, g=g, b=b: e.matmul(ps[5][0:64, i * 8 + g * 4:i * 8 + g * 4 + 4],
                                                            lhsT=vg[:, b, g * 64:(g + 1) * 64], rhs=lg[:, b, g * 4:(g + 1) * 4],
                                                            start=(b == 0), stop=(b == 1)), [tvg, t_lg], [t_ps[5]])
                for b in range(2):
                    P.op("pe", lambda e, g=g, b=b: e.matmul(ps[6][0:64, i * 8 + g * 4:i * 8 + g * 4 + 4],
                                                            lhsT=onesf[:, 0:64], rhs=lg[:, b, g * 4:(g + 1) * 4],
                                                            start=(b == 0), stop=(b == 1)), [t_onesf, t_lg], [t_ps[6]])

        for i in range(NSB):
            att(i)
        P.op("dve", lambda e: e.reciprocal(out=rcp[0:64], in_=ps[6][0:64, 0:NSB * 8]), [t_ps[6]], [t_rcp])
        P.op("dve", lambda e: e.tensor_tensor(out=osT[0:64].rearrange("p i h -> p (i h)"), in0=ps[5][0:64, 0:NSB * 8],
                                              in1=rcp[0:64], op=ALU.mult), [t_ps[5], t_rcp], [t_osT])
        k.osT = (osT, t_osT)
        k.selo = (selo, t_selo)
        k.S2_TOP = k.top
        P.barrier()

    def sample_tail(proj, t_proj, x_keep, t_xk):
        NSB = NS
        pj = proj[0:NSB]
        osT, t_osT = k.osT
        selo, t_selo = k.selo
        k.top = k.K_TOP
        alloc_stage(1024)
        Wo_s, t_Wos = sbt("t_Wo_s", [128, 8, 1024], BF16)
        Wo_g, t_Wog = sbt("t_Wo_g", [128, 4, 1024], BF16)
        Wmq, t_Wmq = sbt("t_Wmq", [128, 8, 256], BF16)
        Wmo, t_Wmo = sbt("t_Wmo", [128, 4, 1024], BF16)
        for h in range(8):
            i = k.wl % k.nst
            k.wl += 1
            st = stage[i][0:64, 0:1024]
            P.dma("sp", st, w_out[64 * h:64 * h + 64, :], writes=[t_stage[i]])
            P.op("act", lambda e, st=st, h=h: e.activation(out=Wo_s[0:64, h, :], in_=st, func=AF.Copy), [t_stage[i]], [t_Wos])
        for h in range(4):
            load_rows(Wo_g[:, h, :], t_Wog, w_out[512 + 128 * h:512 + 128 * h + 128, :], 1024, None, None)
        load_weight(Wmq, t_Wmq, w_mq, 0, 256, gX, t_gX)
        for h in range(4):
            i = k.wl % k.nst
            k.wl += 1
            st = stage[i][0:64, 0:1024]
            P.dma("sp", st, w_mo[64 * h:64 * h + 64, :], writes=[t_stage[i]])
            P.op("act", lambda e, st=st, h=h: e.activation(out=Wmo[0:64, h, :], in_=st, func=AF.Copy), [t_stage[i]], [t_Wmo])
        gmq_b, t_gmqb = bcast_layout("t_gmq_b", g_mq, 64)
        ogT, t_ogT = sbt("t_ogT", [128, 4, NSB], BF16)
        for h in range(4):
            P.op("pe", lambda e, h=h: e.transpose(out=ps[1][:, h * NSB:(h + 1) * NSB], in_=pj[:, 1092 + h * 128:1092 + (h + 1) * 128],
                                                  identity=identf[0:NSB, 0:NSB]), [t_proj, t_identf], [t_ps[1]])
        P.op("act", lambda e: e.activation(out=ogT.rearrange("p h i -> p (h i)"), in_=ps[1][:, 0:4 * NSB], func=AF.Copy),
             [t_ps[1]], [t_ogT])
        x1, t_x1 = sbt("t_x1", [128, D])
        P.op("pool", lambda e: e.memset(x1, 0.0), [], [t_x1])
        for c in range(2):
            for h in range(8):
                P.op("pe", lambda e, h=h, c=c: e.matmul(ps[2 + c][0:NSB, :], lhsT=osT[0:64, :, h], rhs=Wo_s[0:64, h, c * 512:(c + 1) * 512],
                                                        start=(h == 0), stop=False), [t_osT, t_Wos], [t_ps[2 + c]])
            for h in range(4):
                P.op("pe", lambda e, h=h, c=c: e.matmul(ps[2 + c][0:NSB, :], lhsT=ogT[:, h, :], rhs=Wo_g[:, h, c * 512:(c + 1) * 512],
                                                        start=False, stop=(h == 3)), [t_ogT, t_Wog], [t_ps[2 + c]])
            P.op("dve", lambda e, c=c: e.tensor_tensor(out=x1[0:NSB, c * 512:(c + 1) * 512], in0=ps[2 + c][0:NSB, :],
                                                       in1=x_keep[0:NSB, c * 512:(c + 1) * 512], op=ALU.add),
                 [t_ps[2 + c], t_xk], [t_x1])
        hT2, t_hT2 = sbt("t_hT2", [128, 8, 128], BF16)
        norm_T(x1, t_x1, 0, hT2, t_hT2, 0, 0)
        for kc in range(8):
            P.op("pe", lambda e, kc=kc: e.matmul(ps[4][0:NSB, 0:256], lhsT=hT2[:, kc, 0:NSB], rhs=Wmq[:, kc, :],
                                                 start=(kc == 0), stop=(kc == 7)), [t_hT2, t_Wmq], [t_ps[4]])
        qm, t_qm = sbt("t_qm", [128, 256])
        sq2, t_sq2 = sbt("t_sq2", [128, 256])
        st2, t_st2 = sbt("t_st2", [128, 16])
        P.op("act", lambda e: e.activation(out=sq2[0:NSB], in_=ps[4][0:NSB, 0:256], func=AF.Square), [t_ps[4]], [t_sq2])
        P.op("dve", lambda e: e.tensor_reduce(out=st2[0:NSB, 0:4], in_=sq2[0:NSB].rearrange("p (h d) -> p h d", h=4), axis=AX.X,
                                              op=ALU.add), [t_sq2], [t_st2])
        P.op("pool", lambda e: e.tensor_scalar(out=st2[0:NSB, 4:8], in0=st2[0:NSB, 0:4], scalar1=1.0 / 64, scalar2=EPS,
                                               op0=ALU.mult, op1=ALU.add), [t_st2], [t_st2])
        P.op("pool", lambda e: e.tensor_tensor(out=st2[0:NSB, 4:8], in0=st2[0:NSB, 4:8], in1=neghalf[0:NSB, 0:4], op=ALU.pow),
             [t_st2, t_nh], [t_st2])
        P.op("pool", lambda e: e.tensor_scalar(out=st2[0:NSB, 4:8], in0=st2[0:NSB, 4:8], scalar1=0.125, scalar2=1.0,
                                               op0=ALU.mult, op1=ALU.mult), [t_st2], [t_st2])
        qm3 = qm[0:NSB].rearrange("p (h d) -> p h d", h=4)
        P.op("dve", lambda e: e.tensor_tensor(out=qm3, in0=ps[4][0:NSB, 0:256].rearrange("p (h d) -> p h d", h=4),
                                              in1=st2[0:NSB, 4:8].unsqueeze(2).to_broadcast([NSB, 4, 64]), op=ALU.mult),
             [t_ps[4], t_st2], [t_qm])
        P.op("pool", lambda e: e.tensor_tensor(out=qm3, in0=qm3, in1=gmq_b[0:NSB].unsqueeze(1).to_broadcast([NSB, 4, 64]),
                                               op=ALU.mult), [t_qm, t_gmqb], [t_qm])
        mkt = [sbt("t_mk%d" % i, [128, 2, 256]) for i in range(2)]
        mvt = [sbt("t_mv%d" % i, [128, 2, 256]) for i in range(2)]
        prm, t_prm = sbt("t_prm", [128, 256])
        lgm, t_lgm = sbt("t_lgm", [128, 2, 4])
        omT, t_omT = sbt("t_omT", [128, NSB, 4], BF16)
        rcm, t_rcm = sbt("t_rcm", [128, NSB * 4])

        def xatt(i):
            mk_, tmk = mkt[i % 2]
            mv_, tmv = mvt[i % 2]
            P.dma("sp", mk_, cmk_d[i * 256:(i + 1) * 256, :].rearrange("(t p) c -> p t c", p=128), writes=[tmk])
            P.dma("sp", mv_, cmv_d[i * 256:(i + 1) * 256, :].rearrange("(t p) c -> p t c", p=128), writes=[tmv])
            P.op("pe", lambda e: e.matmul(ps[5][:, 0:256], lhsT=selo[0:NSB, i, :], rhs=qm[0:NSB, :], start=True, stop=True),
                 [t_selo, t_qm], [t_ps[5]])
            for mt in range(2):
                P.op("dve", lambda e, mt=mt: e.tensor_tensor(out=prm, in0=ps[5][:, 0:256], in1=mk_[:, mt, :], op=ALU.mult),
                     [t_ps[5], tmk], [t_prm])
                P.op("dve", lambda e, mt=mt: e.tensor_reduce(out=lgm[:, mt, :], in_=prm.rearrange("p (h d) -> p h d", h=4),
                                                             axis=AX.X, op=ALU.add), [t_prm], [t_lgm])
            P.op("act", lambda e: e.activation(out=lgm, in_=lgm, func=AF.Exp), [t_lgm], [t_lgm])
            for h in range(4):
                for mt in range(2):
                    P.op("pe", lambda e, h=h, mt=mt: e.matmul(ps[6][0:64, i * 4 + h:i * 4 + h + 1], lhsT=mv_[:, mt, h * 64:(h + 1) * 64],
                                                              rhs=lgm[:, mt, h:h + 1], start=(mt == 0), stop=(mt == 1)),
                         [tmv, t_lgm], [t_ps[6]])
                for mt in range(2):
                    P.op("pe", lambda e, h=h, mt=mt: e.matmul(ps[7][0:64, i * 4 + h:i * 4 + h + 1], lhsT=onesf[:, 0:64],
                                                              rhs=lgm[:, mt, h:h + 1], start=(mt == 0), stop=(mt == 1)),
                         [t_onesf, t_lgm], [t_ps[7]])

        for i in range(NSB):
            xatt(i)
        P.op("dve", lambda e: e.reciprocal(out=rcm[0:64], in_=ps[7][0:64, 0:NSB * 4]), [t_ps[7]], [t_rcm])
        P.op("dve", lambda e: e.tensor_tensor(out=omT[0:64].rearrange("p i h -> p (i h)"), in0=ps[6][0:64, 0:NSB * 4],
                                              in1=rcm[0:64], op=ALU.mult), [t_ps[6], t_rcm], [t_omT])
        for c in range(2):
            for h in range(4):
                P.op("pe", lambda e, h=h, c=c: e.matmul(ps[2 + c][0:NSB, :], lhsT=omT[0:64, :, h], rhs=Wmo[0:64, h, c * 512:(c + 1) * 512],
                                                        start=(h == 0), stop=(h == 3)), [t_omT, t_Wmo], [t_ps[2 + c]])
            P.op("dve", lambda e, c=c: e.tensor_tensor(out=x1[0:NSB, c * 512:(c + 1) * 512], in0=ps[2 + c][0:NSB, :],
                                                       in1=x1[0:NSB, c * 512:(c + 1) * 512], op=ALU.add),
                 [t_ps[2 + c], t_x1], [t_x1])
        P.op("pool", lambda e: e.tensor_copy(out=x_keep[0:NSB, :], in_=x1[0:NSB, :]), [t_x1], [t_xk])
        hT3, t_hT3 = sbt("t_hT3", [128, 8, 128], BF16)
        norm_T(x1, t_x1, 1, hT3, t_hT3, 0, 0)
        P.barrier()
        k.top = PERSIST_TOP
        xk2, t_xk2 = sbt("t_xk2", [128, D])
        hT4, t_hT4 = sbt("t_hT4", [128, 8, NSB], BF16)
        P.op("pool", lambda e: e.tensor_copy(out=xk2[0:NSB, :], in_=x_keep[0:NSB, :]), [t_xk], [t_xk2])
        P.op("pool", lambda e: e.tensor_copy(out=hT4, in_=hT3[:, :, 0:NSB]), [t_hT3], [t_hT4])
        P.barrier()
        alloc_stage(2816)
        Wg, t_Wg = sbt("t_Wg", [128, 8, 2816], BF16)
        Wu, t_Wu = sbt("t_Wu", [128, 8, 2816], BF16)
        Wd, t_Wd = sbt("t_Wd", [128, 22, 1024], BF16)
        load_weight(Wg, t_Wg, w_gate, 0, 2816, gF, t_gF)
        load_weight(Wu, t_Wu, w_up, 0, 2816, gF, t_gF)
        for f in range(22):
            load_rows(Wd[:, f, :], t_Wd, w_down[128 * f:128 * f + 128, :], 1024, None, None)
        hf, t_hf = sbt("t_hf", [128, 2816])
        sgs, t_sgs = sbt("t_sgs", [128, 512])
        hfT, t_hfT = sbt("t_hfT", [128, 22, NSB], BF16)
        for c in range(6):
            c0 = c * 512
            n = min(512, 2816 - c0)
            for kc in range(8):
                P.op("pe", lambda e, kc=kc, c0=c0, n=n: e.matmul(ps[1][0:NSB, 0:n], lhsT=hT4[:, kc, :], rhs=Wg[:, kc, c0:c0 + n],
                                                                 start=(kc == 0), stop=(kc == 7)), [t_hT4, t_Wg], [t_ps[1]])
            for kc in range(8):
                P.op("pe", lambda e, kc=kc, c0=c0, n=n: e.matmul(ps[2][0:NSB, 0:n], lhsT=hT4[:, kc, :], rhs=Wu[:, kc, c0:c0 + n],
                                                                 start=(kc == 0), stop=(kc == 7)), [t_hT4, t_Wu], [t_ps[2]])
            P.op("act", lambda e, n=n: e.activation(out=sgs[0:NSB, 0:n], in_=ps[1][0:NSB, 0:n], func=AF.Silu), [t_ps[1]], [t_sgs])
            P.op("dve", lambda e, c0=c0, n=n: e.tensor_tensor(out=hf[0:NSB, c0:c0 + n], in0=sgs[0:NSB, 0:n], in1=ps[2][0:NSB, 0:n],
                                                              op=ALU.mult), [t_sgs, t_ps[2]], [t_hf])
        for f in range(22):
            b = 3 + (f % 2)
            P.op("pe", lambda e, f=f, b=b: e.transpose(out=ps[b][:, 0:NSB], in_=hf[0:NSB, f * 128:(f + 1) * 128],
                                                       identity=identf[0:NSB, 0:NSB]), [t_hf, t_identf], [t_ps[b]])
            P.op("act", lambda e, f=f, b=b: e.activation(out=hfT[:, f, :], in_=ps[b][:, 0:NSB], func=AF.Copy), [t_ps[b]], [t_hfT])
        for c in range(2):
            for f in range(22):
                P.op("pe", lambda e, f=f, c=c: e.matmul(ps[5 + c][0:NSB, :], lhsT=hfT[:, f, :], rhs=Wd[:, f, c * 512:(c + 1) * 512],
                                                        start=(f == 0), stop=(f == 21)), [t_hfT, t_Wd], [t_ps[5 + c]])
            P.op("dve", lambda e, c=c: e.tensor_tensor(out=xk2[0:NSB, c * 512:(c + 1) * 512], in0=ps[5 + c][0:NSB, :],
                                                       in1=xk2[0:NSB, c * 512:(c + 1) * 512], op=ALU.add),
                 [t_ps[5 + c], t_xk2], [t_xk2])
        P.dma("sp", y_s[:, :], xk2[0:NSB, :], reads=[t_xk2])

    if STOP >= 0:
        for sq in range(NSEQ):
            prompt_seq(sq)
    if STOP < 0 or STOP >= 99:
        sample_group()

    P.finish()
    P.emit()
    return nc


_CACHE = {}


def _get_nc(nseq, stop, npool=10240):
    key = (nseq, stop, npool)
    if key not in _CACHE:
        _CACHE[key] = build(nseq, STOP=stop, NPOOL=npool)
    return _CACHE[key]


def kernel(x_prompt, x_sample, mem_prompt, cache_k, cache_v, cache_kidx, page_table,
           state_conv, state_ssm, cache_mem_k, cache_mem_v,
           attn_norm_g, w_in, q_norm_g, k_norm_g, conv_w, a_log, dt_bias, gdn_norm_g, w_out,
           xattn_norm_g, mem_norm_g, w_mq, w_mk, w_mv, mq_norm_g, mk_norm_g, w_mo,
           ffn_norm_g, w_gate, w_up, w_down, _ncores=NCORES, _stop=99):
    B = x_prompt.shape[0]
    nseq = B // _ncores
    NS = x_sample.shape[0] // _ncores
    nc = _get_nc(nseq, _stop, cache_k.shape[1])
    f = lambda a: np.ascontiguousarray(np.asarray(a, dtype=np.float32))
    ii = np.arange(128)
    consts = {
        "ident": np.eye(128, dtype=np.float32),
        "trile": (ii[:, None] <= ii[None, :]).astype(np.float32),
        "sgt": (ii[:, None] > ii[None, :]).astype(np.float32),
        "pow2": (2.0 ** -np.arange(32)).astype(np.float32),
        "eye16": np.eye(16, dtype=np.float32).reshape(256),
        "selpair": np.stack([(np.arange(16)[:, None] == (2 * q_ + np.arange(128)[None, :] // 64)).astype(np.float32)
                             for q_ in range(8)]),
        "selone": np.stack([np.repeat((np.arange(16) == i_)[:, None], 128, axis=1).astype(np.float32)
                            for i_ in range(16)], axis=1),
        "iota64": np.arange(64, dtype=np.float32),
    }
    shared = {
        "attn_norm_g": f(attn_norm_g[0]), "w_in": f(w_in[0]),
        "q_norm_g": f(q_norm_g[0]), "k_norm_g": f(k_norm_g[0]),
        "conv_w": f(conv_w[0]), "a_log": f(a_log[0]), "dt_bias": f(dt_bias[0]),
        "gdn_norm_g": f(gdn_norm_g[0]), "w_out": f(w_out[0]), "xattn_norm_g": f(xattn_norm_g[0]),
        "mem_norm_g": f(mem_norm_g[0]), "w_mq": f(w_mq[0]), "w_mk": f(w_mk[0]), "w_mv": f(w_mv[0]),
        "mq_norm_g": f(mq_norm_g[0]), "mk_norm_g": f(mk_norm_g[0]), "w_mo": f(w_mo[0]),
        "ffn_norm_g": f(ffn_norm_g[0]), "w_gate": f(w_gate[0]), "w_up": f(w_up[0]), "w_down": f(w_down[0]),
    }
    npool = cache_k.shape[1]
    ck_k = f(cache_k[0]).reshape(npool * 128, 128)
    ck_v = f(cache_v[0]).reshape(npool * 128, 128)
    ck_idx = f(cache_kidx[0]).reshape(npool, 8192)
    in_maps = []
    for c in range(_ncores):
        m = {
            "xp": f(x_prompt[c * nseq:(c + 1) * nseq]).reshape(nseq * SEQ, D),
            "memp": f(mem_prompt[c * nseq:(c + 1) * nseq]).reshape(nseq * MEM, D),
            "xs": f(x_sample[c * NS:(c + 1) * NS]).reshape(NS, D),
            "st_conv": f(state_conv[0, c * NS:(c + 1) * NS]).reshape(NS * 3, 1536),
            "ssm_in": f(state_ssm[0, c * NS:(c + 1) * NS]).reshape(NS * 512, 128),
            "ptab": np.ascontiguousarray(np.asarray(page_table[c * NS:(c + 1) * NS], dtype=np.int32)),
            "cmk": f(cache_mem_k[0, c * NS:(c + 1) * NS]).reshape(NS * 256, 256),
            "cmv": f(cache_mem_v[0, c * NS:(c + 1) * NS]).reshape(NS * 256, 256),
            "cache_kidx": ck_idx, "cache_k": ck_k, "cache_v": ck_v,
        }
        m.update(consts)
        m.update(shared)
        in_maps.append(m)
    res = run_bass_kernel_spmd(nc, in_maps, core_ids=list(range(_ncores))).results
    cat = lambda name: np.concatenate([r[name] for r in res], axis=0)
    SB = x_sample.shape[0]
    outs = (
        cat("y_p").reshape(B, SEQ, D),
        cat("y_s").reshape(SB, 1, D),
        cat("k_p").reshape(1, B, SEQ, 2, 64),
        cat("v_p").reshape(1, B, SEQ, 2, 64),
        cat("kidx_p").reshape(1, B, SEQ, 64),
        cat("conv_p").reshape(1, B, 3, 1536),
        cat("ssm_p").reshape(1, B, 4, 128, 128),
        cat("memk_p").reshape(1, B, MEM, 4, 64),
        cat("memv_p").reshape(1, B, MEM, 4, 64),
        cat("k_s").reshape(1, SB, 1, 2, 64),
        cat("v_s").reshape(1, SB, 1, 2, 64),
        cat("kidx_s").reshape(1, SB, 1, 64),
        cat("conv_s").reshape(1, SB, 3, 1536),
        cat("ssm_s").reshape(1, SB, 4, 128, 128),
    )
    return outs
```

```python
import os
import numpy as np
import concourse.bass as bass
import concourse.mybir as mybir
from concourse.bass_utils import run_bass_kernel_spmd

F32 = mybir.dt.float32
BF16 = mybir.dt.bfloat16
I32 = mybir.dt.int32
AF = mybir.ActivationFunctionType
ALU = mybir.AluOpType
AX = mybir.AxisListType

NCORES = 8
D = 1024
SEQ = 2048
NT = SEQ // 128
MEM = 256
INW = 3148
EPS = 1e-6
NDS = 32


class Tok:
    __slots__ = ("w", "r")

    def __init__(self):
        self.w = None
        self.r = {}


class Prog:
    def __init__(self, nc):
        self.nc = nc
        self.names = ["pe", "act", "dve", "pool", "sp"]
        self.sems = []
        self.esem = {}
        for k in self.names:
            self.esem[k] = len(self.sems)
            self.sems.append(nc.alloc_semaphore("es_" + k))
        self.dsem = []
        for i in range(NDS):
            self.dsem.append(len(self.sems))
            self.sems.append(nc.alloc_semaphore("ds_%d" % i))
        self.dval = [0] * NDS
        self.dnext = 0
        self.dnext_sw = 0
        self.cnt = {k: 0 for k in self.names}
        self.seen = {k: {} for k in self.names}
        self.th = {k: [] for k in self.names}

    def _deps(self, e, reads, writes, extra=()):
        d = {}

        def add(s, v):
            if d.get(s, 0) < v:
                d[s] = v

        for t in reads:
            if t.w is not None:
                add(*t.w)
        for t in writes:
            if t.w is not None:
                add(*t.w)
            for s, v in t.r.items():
                add(s, v)
        for s, v in extra:
            add(s, v)
        out = []
        for s, v in d.items():
            if e == "pe" and s == self.esem["pe"]:
                continue
            if self.seen[e].get(s, 0) >= v:
                continue
            self.seen[e][s] = v
            out.append((s, v))
        return out

    def op(self, e, fn, reads=(), writes=()):
        waits = self._deps(e, reads, writes)
        self.cnt[e] += 1
        n = self.cnt[e]
        s = self.esem[e]
        self.th[e].append((waits, fn, s, 1))
        for t in reads:
            if t.r.get(s, 0) < n:
                t.r[s] = n
        for t in writes:
            t.w = (s, n)
            t.r = {}

    def dma(self, q, out, in_, reads=(), writes=(), **kw):
        if q == "pool":
            i = NDS - 8 + self.dnext_sw
            self.dnext_sw = (self.dnext_sw + 1) % 8
        else:
            i = self.dnext
            self.dnext = (self.dnext + 1) % (NDS - 8)
        s = self.dsem[i]
        extra = [(s, self.dval[i])] if self.dval[i] else []
        waits = self._deps(q, reads, writes, extra)
        self.dval[i] += 16
        v = self.dval[i]
        if "indirect" in kw:
            ioff = kw.pop("indirect")
            self.th[q].append((waits, lambda eng: eng.indirect_dma_start(out=out, out_offset=None, in_=in_,
                                                                         in_offset=ioff), s, 16))
        else:
            self.th[q].append((waits, lambda eng: eng.dma_start(out=out, in_=in_, **kw), s, 16))
        for t in reads:
            t.r[s] = v
        for t in writes:
            t.w = (s, v)
            t.r = {}

    def barrier(self):
        tgt = []
        for i in range(NDS):
            if self.dval[i]:
                tgt.append((self.dsem[i], self.dval[i]))
        for k in self.names:
            if self.cnt[k]:
                tgt.append((self.esem[k], self.cnt[k]))
        for e in self.names:
            waits = []
            for s_, v in tgt:
                if s_ == self.esem[e] and e in ("pe", "sp"):
                    continue
                if self.seen[e].get(s_, 0) >= v:
                    continue
                self.seen[e][s_] = v
                waits.append((s_, v))
            self.th[e].append((waits, None, None, 0))

    def finish(self):
        waits = []
        for i in range(NDS):
            if self.dval[i]:
                waits.append((self.dsem[i], self.dval[i]))
        for k in self.names:
            if k != "sp" and self.cnt[k]:
                waits.append((self.esem[k], self.cnt[k]))
        self.th["sp"].append((waits, None, None, 0))

    def emit(self):
        nc = self.nc
        sems = self.sems

        def run(eng, lst):
            for waits, fn, s, inc in lst:
                for ws, wv in waits:
                    eng.wait_ge(sems[ws], wv)
                if fn is not None:
                    fn(eng).then_inc(sems[s], inc)

        with nc.Block() as block:
            @block.tensor
            def _(e):
                run(e, self.th["pe"])

            @block.scalar
            def _(e):
                run(e, self.th["act"])

            @block.vector
            def _(e):
                run(e, self.th["dve"])

            @block.gpsimd
            def _(e):
                run(e, self.th["pool"])

            @block.sync
            def _(e):
                run(e, self.th["sp"])


class K:
    pass


def build(NSEQ, NS=16, STOP=99, NPOOL=10240):
    GCUT = int(os.environ.get('GCUT', '99'))
    DCUT = int(os.environ.get('DCUT', '99'))
    PCUT = int(os.environ.get('PCUT', '99'))
    CCUT = int(os.environ.get('CCUT', '99'))
    nc = bass.Bass("TRN2", target_bir_lowering=False)
    P = Prog(nc)
    k = K()

    def din(name, shape, dt=F32):
        return nc.dram_tensor(name, list(shape), dt, kind="ExternalInput").ap()

    def dout(name, shape, dt=F32):
        return nc.dram_tensor(name, list(shape), dt, kind="ExternalOutput").ap()

    ARENA_W = 53000
    arena = nc.alloc_sbuf_tensor("arena", [128, ARENA_W], F32).ap()
    k.top = 0

    def sb(name, shape, dt=F32):
        n = 1
        for d_ in shape[1:]:
            n *= d_
        words = n if dt in (F32, I32, mybir.dt.uint32) else (n + 1) // 2
        words = (words + 7) // 8 * 8
        off = k.top
        k.top += words
        assert k.top <= ARENA_W, ("SBUF arena overflow", name, k.top)
        a = arena[:, off:off + words]
        if dt != F32:
            a = a.bitcast(dt)
        a = a[:, 0:n]
        if len(shape) > 2:
            names = " ".join("d%d" % i for i in range(len(shape) - 1))
            kw = {"d%d" % i: shape[i + 1] for i in range(len(shape) - 2)}
            a = a.rearrange("p (%s) -> p %s" % (names, names), **kw)
        if shape[0] != 128:
            a = a[0:shape[0]]
        return a

    def sbt(name, shape, dt=F32):
        return sb(name, shape, dt), Tok()

    xp = din("xp", [NSEQ * SEQ, D])
    memp = din("memp", [NSEQ * MEM, D])
    xs = din("xs", [NS, D])
    st_conv = din("st_conv", [NS * 3, 1536])
    ssm_in = din("ssm_in", [NS * 512, 128])
    eye16_d = din("eye16", [256])
    selpair_d = din("selpair", [8, NS, 128])
    selone_d = din("selone", [NS, NS, 128])
    iota64_d = din("iota64", [64])
    ptab = din("ptab", [NS, 64], I32)
    cache_kidx_d = din("cache_kidx", [NPOOL, 8192])
    cache_k_d = din("cache_k", [NPOOL * 128, 128])
    cache_v_d = din("cache_v", [NPOOL * 128, 128])
    cmk_d = din("cmk", [NS * 256, 256])
    cmv_d = din("cmv", [NS * 256, 256])
    ident_d = din("ident", [128, 128])
    trile_d = din("trile", [128, 128])
    sgt_d = din("sgt", [128, 128])
    pow2_d = din("pow2", [32])
    g_attn = din("attn_norm_g", [D])
    w_in = din("w_in", [D, INW])
    g_q = din("q_norm_g", [64])
    g_k = din("k_norm_g", [64])
    conv_w = din("conv_w", [4, 1536])
    a_log = din("a_log", [4])
    dt_bias = din("dt_bias", [4])
    g_gdn = din("gdn_norm_g", [128])
    w_out = din("w_out", [D, D])
    g_x = din("xattn_norm_g", [D])
    g_mem = din("mem_norm_g", [D])
    w_mq = din("w_mq", [D, 256])
    w_mk = din("w_mk", [D, 256])
    w_mv = din("w_mv", [D, 256])
    g_mq = din("mq_norm_g", [64])
    g_mk = din("mk_norm_g", [64])
    w_mo = din("w_mo", [256, D])
    g_f = din("ffn_norm_g", [D])
    w_gate = din("w_gate", [D, 2816])
    w_up = din("w_up", [D, 2816])
    w_down = din("w_down", [2816, D])

    y_p = dout("y_p", [NSEQ * SEQ, D])
    y_s = dout("y_s", [NS, D])
    k_p = dout("k_p", [NSEQ * SEQ, 128])
    v_p = dout("v_p", [NSEQ * SEQ, 128])
    kidx_p = dout("kidx_p", [NSEQ * SEQ, 64])
    conv_p = dout("conv_p", [NSEQ * 3, 1536])
    ssm_p = dout("ssm_p", [NSEQ * 512, 128])
    memk_p = dout("memk_p", [NSEQ * MEM, 256])
    memv_p = dout("memv_p", [NSEQ * MEM, 256])
    k_s = dout("k_s", [NS, 128])
    v_s = dout("v_s", [NS, 128])
    kidx_s = dout("kidx_s", [NS, 64])
    conv_s = dout("conv_s", [NS * 3, 1536])
    ssm_s = dout("ssm_s", [NS * 512, 128])

    identf, t_identf = sbt("identf", [128, 128])
    identb, t_identb = sbt("identb", [128, 128], BF16)
    trile, t_trile = sbt("trile", [128, 128])
    sgt, t_sgt = sbt("sgt", [128, 128])
    onesf, t_onesf = sbt("onesf", [128, 128])
    onesb, t_onesb = sbt("onesb", [128, 128], BF16)
    tribias, t_tribias = sbt("tribias", [128, 128])
    lowinc, t_lowinc = sbt("lowinc", [128, 128])
    P.dma("sp", identf, ident_d, writes=[t_identf])
    P.dma("sp", trile, trile_d, writes=[t_trile])
    P.dma("sp", sgt, sgt_d, writes=[t_sgt])
    P.op("dve", lambda e: e.tensor_copy(out=identb, in_=identf), [t_identf], [t_identb])
    P.op("pool", lambda e: e.memset(onesf, 1.0), [], [t_onesf])
    P.op("pool", lambda e: e.memset(onesb, 1.0), [], [t_onesb])
    P.op("dve", lambda e: e.tensor_tensor(out=lowinc, in0=sgt, in1=identf, op=ALU.add),
         [t_sgt, t_identf], [t_lowinc])
    P.op("dve", lambda e: e.tensor_scalar(out=tribias, in0=lowinc, scalar1=-1.0, scalar2=1e30,
                                          op0=ALU.add, op1=ALU.mult), [t_lowinc], [t_tribias])

    def col_layout(name, src, n):
        t, tok = sbt(name, [128, n])
        P.dma("sp", t, src.rearrange("(c p) -> p c", p=128), writes=[tok],
              allow_slow_non_contiguous=True)
        return t, tok

    def bcast_layout(name, src, n):
        t, tok = sbt(name, [128, n])
        P.dma("sp", t, src.partition_broadcast(128), writes=[tok])
        return t, tok

    gA, t_gA = col_layout("gA", g_attn, 8)
    gM, t_gM = col_layout("gM", g_mem, 8)
    gX, t_gX = col_layout("gX", g_x, 8)
    gF, t_gF = col_layout("gF", g_f, 8)
    gk_b, t_gk = bcast_layout("gk_b", g_k, 64)
    gmk_b, t_gmk = bcast_layout("gmk_b", g_mk, 64)
    ggdn_b, t_ggdn = bcast_layout("ggdn_b", g_gdn, 128)
    dtb_b, t_dtb = bcast_layout("dtb_b", dt_bias, 4)
    nea_b, t_nea = bcast_layout("nea_b", a_log, 4)
    pow2_b, t_pow2 = bcast_layout("pow2_b", pow2_d, 32)
    P.op("act", lambda e: e.activation(out=nea_b, in_=nea_b, func=AF.Exp), [t_nea], [t_nea])
    P.op("dve", lambda e: e.tensor_scalar(out=nea_b, in0=nea_b, scalar1=-1.0, scalar2=None,
                                          op0=ALU.mult), [t_nea], [t_nea])
    gq8, t_gq8 = sbt("gq8", [128, 1])
    gmq8, t_gmq8 = sbt("gmq8", [128, 1])
    for (dst, tdst, src) in ((gq8, t_gq8, g_q), (gmq8, t_gmq8, g_mq)):
        for hh in range(2):
            P.dma("sp", dst[64 * hh:64 * hh + 64, :], src.rearrange("(d o) -> d o", o=1),
                  writes=[tdst], allow_slow_non_contiguous=True)
        P.op("pool", lambda e, dst=dst: e.tensor_scalar(out=dst, in0=dst, scalar1=0.125, scalar2=1.0,
                                                        op0=ALU.mult, op1=ALU.mult), [tdst], [tdst])
    cw, t_cw = sbt("cw", [128, 12, 4])
    for tap in range(4):
        P.dma("sp", cw[:, :, tap], conv_w[tap].rearrange("(j p) -> p j", p=128), writes=[t_cw],
              allow_slow_non_contiguous=True)
    neghalf, t_nh = sbt("neghalf", [128, 8])
    P.op("pool", lambda e: e.memset(neghalf, -0.5), [], [t_nh])

    ps = [nc.alloc_psum_tensor("ps%d" % i, [128, 512], F32).ap() for i in range(8)]
    t_ps = [Tok() for _ in range(8)]
    psb16 = [p_.bitcast(BF16) for p_ in ps]

    stage = [None, None, None, None]
    t_stage = [Tok(), Tok(), Tok(), Tok()]
    k.wl = 0
    k.nst = 2

    def alloc_stage(n, nbuf=4):
        k.nst = nbuf
        for i in range(nbuf):
            stage[i] = sb("wstage%d" % i, [128, n], F32)

    def load_rows(dst_kc, t_dst, src_rows, n, gain_col, t_gain):
        i = k.wl % k.nst
        k.wl += 1
        st = stage[i][:, 0:n]
        P.dma("sp", st, src_rows, writes=[t_stage[i]])
        if gain_col is None:
            if k.wl % 2 == 0:
                P.op("act", lambda e: e.activation(out=dst_kc, in_=st, func=AF.Copy),
                     [t_stage[i]], [t_dst])
            else:
                P.op("pool", lambda e: e.tensor_copy(out=dst_kc, in_=st), [t_stage[i]], [t_dst])
        else:
            if k.wl % 2 == 0:
                P.op("act", lambda e: e.activation(out=dst_kc, in_=st, func=AF.Copy, scale=gain_col),
                     [t_stage[i], t_gain], [t_dst])
            else:
                P.op("pool", lambda e: e.tensor_scalar(out=dst_kc, in0=st, scalar1=gain_col, scalar2=1.0,
                                                       op0=ALU.mult, op1=ALU.mult),
                     [t_stage[i], t_gain], [t_dst])

    def load_weight(dst, t_dst, src, c0, c1, gain, t_gain, d0=0):
        n = c1 - c0
        for kc in range(8):
            load_rows(dst[:, kc, d0:d0 + n], t_dst, src[kc * 128:(kc + 1) * 128, c0:c1], n,
                      None if gain is None else gain[:, kc:kc + 1], t_gain)

    xt = [sb("xt%d" % i, [128, D], F32) for i in range(2)]
    t_xt = [Tok(), Tok()]
    hb = [sb("hb%d" % i, [128, D], BF16) for i in range(2)]
    t_hb = [Tok(), Tok()]
    sq_scr, t_sq = sbt("sq_scr", [128, D], BF16)
    stat = [sb("stat%d" % i, [128, 4], F32) for i in range(2)]
    t_stat = [Tok(), Tok()]
    k.nt = 0
    for i in range(2):
        P.op("pool", lambda e, i=i: e.memset(xt[i], 0.0), [], [t_xt[i]])

    k.pref = {}

    def load_x(src_rows, nrows, key=None):
        if key is not None and key in k.pref:
            return k.pref.pop(key)
        i = k.nt % 2
        k.nt += 1
        P.dma("sp", xt[i][0:nrows, :], src_rows, writes=[t_xt[i]])
        return xt[i], t_xt[i], i

    def prefetch_x(src_rows, nrows, key):
        k.pref[key] = load_x(src_rows, nrows)

    def norm_T(x, tx, i, hT, t_hT, col, psb):
        s, ts = stat[i], t_stat[i]
        P.op("act", lambda e: e.activation(out=sq_scr, in_=x, func=AF.Square,
                                           accum_out=s[:, 0:1]), [tx], [t_sq, ts])
        P.op("pool", lambda e: e.tensor_scalar(out=s[:, 1:2], in0=s[:, 0:1], scalar1=1.0 / D,
                                               scalar2=EPS, op0=ALU.mult, op1=ALU.add), [ts], [ts])
        P.op("pool", lambda e: e.tensor_tensor(out=s[:, 2:3], in0=s[:, 1:2], in1=neghalf[:, 0:1],
                                               op=ALU.pow), [ts, t_nh], [ts])
        h, th = hb[i], t_hb[i]
        P.op("act", lambda e: e.activation(out=h, in_=x, func=AF.Copy, scale=s[:, 2:3]),
             [tx, ts], [th])
        pb = psb16[psb]
        for kc in range(8):
            P.op("pe", lambda e, kc=kc: e.transpose(out=pb[:, kc * 128:(kc + 1) * 128],
                                                    in_=h[:, kc * 128:(kc + 1) * 128],
                                                    identity=identb),
                 [th, t_identb], [t_ps[psb]])
        P.op("dve", lambda e: e.tensor_copy(out=hT[:, :, col:col + 128],
                                            in_=pb.rearrange("p (k t) -> p k t", k=8)),
             [t_ps[psb]], [t_hT])

    def norm_transpose(src_rows, nrows, hT, t_hT, col, psb, key=None):
        x, tx, i = load_x(src_rows, nrows, key)
        norm_T(x, tx, i, hT, t_hT, col, psb)
        return x, tx

    def rstd_groups(dst, t_dst, ssq, t_ssq, n, width):
        P.op("pool", lambda e: e.tensor_scalar(out=dst, in0=ssq, scalar1=1.0 / width, scalar2=EPS,
                                               op0=ALU.mult, op1=ALU.add), [t_ssq], [t_dst])
        P.op("pool", lambda e: e.tensor_tensor(out=dst, in0=dst, in1=neghalf[:, 0:n], op=ALU.pow),
             [t_dst, t_nh], [t_dst])

    def head_rms(psrc, t_psrc, nh, scr, t_scr, ss, t_ss):
        P.op("act", lambda e: e.activation(out=scr[:, 0:nh * 64], in_=psrc, func=AF.Square),
             [t_psrc], [t_scr])
        P.op("dve", lambda e: e.tensor_reduce(out=ss[:, 0:nh],
                                              in_=scr[:, 0:nh * 64].rearrange("p (h d) -> p h d", h=nh),
                                              axis=AX.X, op=ALU.add), [t_scr], [t_ss])
        rstd_groups(ss[:, nh:2 * nh], t_ss, ss[:, 0:nh], t_ss, nh, 64)

    qscr, t_qscr = sbt("qscr", [128, 512])
    qss, t_qss = sbt("qss", [128, 16])
    PERSIST_TOP = k.top

    def prompt_seq(sq):
        k.top = PERSIST_TOP
        if STOP <= 0:
            return
        row_base = sq * SEQ
        mkT2, t_mkT2 = sbt("mkT2", [128, 2, 256], BF16)
        mv_b, t_mv = sbt("mv_b", [128, 2, 256], BF16)
        o_gdnT, t_ogT = sbt("o_gdnT", [128, 4, SEQ], BF16)
        SEQ_TOP = k.top
        alloc_stage(256)
        wmkv, t_wmkv = sbt("wmkv", [128, 8, 512], BF16)
        load_weight(wmkv, t_wmkv, w_mk, 0, 256, gM, t_gM, 0)
        load_weight(wmkv, t_wmkv, w_mv, 0, 256, gM, t_gM, 256)
        hTm, t_hTm = sbt("hTm", [128, 8, 128], BF16)
        mko = [sbt("mko%d" % i, [128, 512]) for i in range(2)]
        mkb, t_mkb = sbt("mkb", [128, 256], BF16)
        for mt in range(2):
            row0 = sq * MEM + mt * 128
            norm_transpose(memp[row0:row0 + 128, :], 128, hTm, t_hTm, 0, 0)
            for kc in range(8):
                P.op("pe", lambda e, kc=kc: e.matmul(ps[1], lhsT=hTm[:, kc, :], rhs=wmkv[:, kc, :],
                                                     start=(kc == 0), stop=(kc == 7)),
                     [t_hTm, t_wmkv], [t_ps[1]])
            o, to = mko[mt]
            P.op("act", lambda e, o=o: e.activation(out=o[:, 256:512], in_=ps[1][:, 256:512], func=AF.Copy),
                 [t_ps[1]], [to])
            P.op("act", lambda e, mt=mt: e.activation(out=mv_b[:, mt, :], in_=ps[1][:, 256:512], func=AF.Copy),
                 [t_ps[1]], [t_mv])
            head_rms(ps[1][:, 0:256], t_ps[1], 4, qscr, t_qscr, qss, t_qss)
            P.op("dve", lambda e, o=o: e.tensor_tensor(
                out=o[:, 0:256].rearrange("p (h d) -> p h d", h=4),
                in0=ps[1][:, 0:256].rearrange("p (h d) -> p h d", h=4),
                in1=qss[:, 4:8].unsqueeze(2).to_broadcast([128, 4, 64]), op=ALU.mult),
                [t_ps[1], t_qss], [to])
            P.op("pool", lambda e, o=o: e.tensor_tensor(
                out=o[:, 0:256].rearrange("p (h d) -> p h d", h=4),
                in0=o[:, 0:256].rearrange("p (h d) -> p h d", h=4),
                in1=gmk_b.unsqueeze(1).to_broadcast([128, 4, 64]), op=ALU.mult),
                [to, t_gmk], [to])
            P.dma("sp", memk_p[row0:row0 + 128, :], o[:, 0:256], reads=[to])
            P.dma("sp", memv_p[row0:row0 + 128, :], o[:, 256:512], reads=[to])
            P.op("act", lambda e, o=o: e.activation(out=mkb, in_=o[:, 0:256], func=AF.Copy), [to], [t_mkb])
            pb = psb16[2]
            for a in range(2):
                P.op("pe", lambda e, a=a: e.transpose(out=pb[:, a * 128:(a + 1) * 128],
                                                      in_=mkb[:, a * 128:(a + 1) * 128], identity=identb),
                     [t_mkb, t_identb], [t_ps[2]])
            P.op("dve", lambda e, mt=mt: e.tensor_copy(
                out=mkT2[:, :, mt * 128:(mt + 1) * 128],
                in_=pb[:, 0:256].rearrange("p (a t) -> p a t", a=2)), [t_ps[2]], [t_mkT2])
        P.barrier()
        if STOP <= 1:
            return
        k.top = SEQ_TOP
        gdn_phase(sq, o_gdnT, t_ogT)
        P.barrier()
        if STOP <= 2:
            return
        k.top = SEQ_TOP
        o_atT, t_oatT = sbt("o_atT", [128, 4, SEQ], BF16)
        D_TOP = k.top
        dsa_phase(sq, o_atT, t_oatT)
        P.barrier()
        if STOP <= 3:
            return
        k.top = D_TOP
        c1_phase(sq, o_atT, t_oatT, o_gdnT, t_ogT, mkT2, t_mkT2, mv_b, t_mv)
        P.barrier()
        if STOP <= 4:
            return
        k.top = PERSIST_TOP
        c2_phase(sq)
        P.barrier()

    def gdn_phase(sq, o_gdnT, t_ogT):
        row_base = sq * SEQ
        qTg, t_qTg = sbt("qTg", [128, 4, SEQ], BF16)
        kTg, t_kTg = sbt("kTg", [128, 4, SEQ], BF16)
        k_tm, t_ktm = sbt("k_tm", [128, NT, 4, 128], BF16)
        v_tm, t_vtm = sbt("v_tm", [128, NT, 4, 128], BF16)
        sgz, t_sgz = sbt("sgz", [128, NT, 512], BF16)
        gab, t_gab = sbt("gab", [128, NT, 8])
        G_TOP = k.top
        alloc_stage(2056, 2)
        wB, t_wB = sbt("wB", [128, 8, 2056], BF16)
        load_weight(wB, t_wB, w_in, 1092, 3148, gA, t_gA)
        hTg, t_hTg = sbt("hTg", [128, 8, 512], BF16)
        Cq, t_Cq = sbt("Cq", [128, 4, 512])
        cvT, t_cvT = sbt("cvT", [128, 4, 512], BF16)
        Uc = [sbt("Uc%d" % i, [128, 515]) for i in range(2)]
        cacc = [sbt("cacc%d" % i, [128, 512]) for i in range(2)]
        halo, t_halo = sbt("halo", [128, 12, 3])
        sqb, t_sqb = sbt("sqb", [128, 512], BF16)
        lnb, t_lnb = sbt("lnb", [128, 512])
        P.op("pool", lambda e: e.memset(halo, 0.0), [], [t_halo])
        def conv_chunk(j, grp):
            b = 3 + (j % 2)
            for kc in range(8):
                P.op("pe", lambda e, kc=kc: e.matmul(
                    ps[b], lhsT=wB[:, kc, 128 * j:128 * j + 128], rhs=hTg[:, kc, :],
                    start=(kc == 0), stop=(kc == 7)), [t_hTg, t_wB], [t_ps[b]])
            u, tu = Uc[j % 2]
            ca, tca = cacc[j % 2]
            P.op("act", lambda e: e.activation(out=u[:, 3:515], in_=ps[b], func=AF.Copy), [t_ps[b]], [tu])
            P.op("pool", lambda e: e.tensor_copy(out=u[:, 0:3], in_=halo[:, j, :]), [t_halo], [tu])
            P.op("dve", lambda e: e.tensor_scalar(out=ca, in0=u[:, 0:512], scalar1=cw[:, j, 0:1], scalar2=None,
                                                  op0=ALU.mult), [tu, t_cw], [tca])
            for tap in range(1, 4):
                P.op("dve", lambda e, tap=tap: e.scalar_tensor_tensor(
                    out=ca, in0=u[:, tap:tap + 512], scalar=cw[:, j, tap:tap + 1], in1=ca,
                    op0=ALU.mult, op1=ALU.add), [tu, t_cw, tca], [tca])
            P.op("pool", lambda e: e.tensor_copy(out=halo[:, j, :], in_=u[:, 512:515]), [tu], [t_halo])
            if j < 8:
                P.op("act", lambda e: e.activation(out=Cq[:, j % 4, :], in_=ca, func=AF.Silu), [tca], [t_Cq])
            else:
                P.op("act", lambda e: e.activation(out=cvT[:, j - 8, :], in_=ca, func=AF.Silu), [tca], [t_cvT])

        def norm_chunk(j, gc0):
            P.op("act", lambda e: e.activation(out=sqb, in_=Cq[:, j % 4, :], func=AF.Square), [t_Cq], [t_sqb])
            P.op("pe", lambda e: e.matmul(ps[5], lhsT=onesb, rhs=sqb, start=True, stop=True),
                 [t_sqb, t_onesb], [t_ps[5]])
            P.op("act", lambda e: e.activation(out=lnb, in_=ps[5], func=AF.Ln, bias=1e-6, scale=1.0),
                 [t_ps[5]], [t_lnb])
            bias = (-0.5 * float(np.log(128.0))) if j < 4 else 0.0
            P.op("act", lambda e: e.activation(out=lnb, in_=lnb, func=AF.Exp, bias=bias, scale=-0.5),
                 [t_lnb], [t_lnb])
            dstT, tdst = (qTg, t_qTg) if j < 4 else (kTg, t_kTg)
            P.op("dve", lambda e: e.tensor_tensor(out=dstT[:, j % 4, gc0:gc0 + 512], in0=Cq[:, j % 4, :], in1=lnb,
                                                  op=ALU.mult), [t_Cq, t_lnb], [tdst])

        def g_tile(grp, t4):
            ti = grp * 4 + t4
            r0 = row_base + ti * 128
            norm_transpose(xp[r0:r0 + 128, :], 128, hTg, t_hTg, t4 * 128, 0, key=("g", r0))
            if ti + 1 < NT:
                prefetch_x(xp[r0 + 128:r0 + 256, :], 128, ("g", r0 + 128))
            for kc in range(8):
                P.op("pe", lambda e, kc=kc: e.matmul(
                    ps[1], lhsT=hTg[:, kc, t4 * 128:(t4 + 1) * 128], rhs=wB[:, kc, 1536:2048],
                    start=(kc == 0), stop=(kc == 7)), [t_hTg, t_wB], [t_ps[1]])
            for kc in range(8):
                P.op("pe", lambda e, kc=kc: e.matmul(
                    ps[2][:, 0:8], lhsT=hTg[:, kc, t4 * 128:(t4 + 1) * 128], rhs=wB[:, kc, 2048:2056],
                    start=(kc == 0), stop=(kc == 7)), [t_hTg, t_wB], [t_ps[2]])
            P.op("act", lambda e: e.activation(out=sgz[:, ti, :], in_=ps[1], func=AF.Silu), [t_ps[1]], [t_sgz])
            P.op("dve", lambda e: e.tensor_copy(out=gab[:, ti, :], in_=ps[2][:, 0:8]), [t_ps[2]], [t_gab])

        def g_transposes(grp, t4):
            ti = grp * 4 + t4
            gc0 = grp * 512
            for (srcT, tsrc, c0, dst, tdst, b) in ((kTg, t_kTg, gc0 + t4 * 128, k_tm, t_ktm, 6),
                                                   (cvT, t_cvT, t4 * 128, v_tm, t_vtm, 7)):
                pb = psb16[b]
                for h in range(4):
                    P.op("pe", lambda e, h=h, srcT=srcT, c0=c0, pb=pb: e.transpose(
                        out=pb[:, h * 128:(h + 1) * 128], in_=srcT[:, h, c0:c0 + 128], identity=identb),
                        [tsrc, t_identb], [t_ps[b]])
                P.op("act", lambda e, dst=dst, pb=pb: e.activation(
                    out=dst[:, ti, :, :], in_=pb[:, 0:512].rearrange("p (h d) -> p h d", h=4), func=AF.Copy),
                    [t_ps[b]], [tdst])

        for grp in range(4):
            for t4 in range(4):
                g_tile(grp, t4)
            for j in range(0, 4):
                conv_chunk(j, grp)
            for j in range(0, 4):
                norm_chunk(j, grp * 512)
            for j in range(4, 12):
                conv_chunk(j, grp)
            for j in range(4, 8):
                norm_chunk(j, grp * 512)
            for t4 in range(4):
                g_transposes(grp, t4)
        P.barrier()
        if STOP <= 1.5:
            return
        k.top = G_TOP
        gall, t_gall = sbt("gall", [128, NT, 4])
        ball, t_ball = sbt("ball", [128, NT, 4])
        tmpa, t_tmpa = sbt("tmpa", [128, NT, 4])
        tmpb, t_tmpb = sbt("tmpb", [128, NT, 4])
        P.op("dve", lambda e: e.tensor_tensor(out=gall, in0=gab[:, :, 0:4],
                                              in1=dtb_b.unsqueeze(1).to_broadcast([128, NT, 4]), op=ALU.add),
             [t_gab, t_dtb], [t_gall])
        P.op("dve", lambda e: e.tensor_scalar(out=tmpa, in0=gall, scalar1=-1.0, scalar2=None, op0=ALU.mult),
             [t_gall], [t_tmpa])
        P.op("dve", lambda e: e.tensor_tensor(out=tmpa, in0=tmpa, in1=gall, op=ALU.min), [t_tmpa, t_gall], [t_tmpa])
        P.op("act", lambda e: e.activation(out=tmpa, in_=tmpa, func=AF.Exp), [t_tmpa], [t_tmpa])
        P.op("act", lambda e: e.activation(out=tmpa, in_=tmpa, func=AF.Ln, bias=1.0, scale=1.0), [t_tmpa], [t_tmpa])
        P.op("dve", lambda e: e.scalar_tensor_tensor(out=tmpb, in0=gall, scalar=0.0, in1=tmpa,
                                                     op0=ALU.max, op1=ALU.add), [t_gall, t_tmpa], [t_tmpb])
        P.op("dve", lambda e: e.tensor_tensor(out=gall, in0=tmpb,
                                              in1=nea_b.unsqueeze(1).to_broadcast([128, NT, 4]), op=ALU.mult),
             [t_tmpb, t_nea], [t_gall])
        P.op("act", lambda e: e.activation(out=ball, in_=gab[:, :, 4:8], func=AF.Sigmoid), [t_gab], [t_ball])

        S, t_S = sbt("S", [128, 4, 128])
        Sb, t_Sb = sbt("Sb", [128, 4, 128], BF16)
        P.op("pool", lambda e: e.memset(S, 0.0), [], [t_S])
        P.op("pool", lambda e: e.memset(Sb, 0.0), [], [t_Sb])
        NB = 2
        bufs = []
        for i in range(NB):
            bb = {}
            for nm, dt_ in (("Gh", F32), ("E", F32), ("Du", F32), ("Dl", F32), ("L0", BF16), ("L1", BF16),
                            ("M0", BF16), ("M1", BF16), ("P0", BF16), ("P1", BF16), ("kbg", BF16),
                            ("kdec", BF16), ("vb", BF16), ("u", F32), ("wT", BF16), ("qgT", BF16),
                            ("qkT", BF16), ("vnew", BF16), ("on", F32), ("og", BF16)):
                bb[nm] = sbt("%s_%d" % (nm, i), [128, 4, 128], dt_)
            bb["sc"] = sbt("gsc_%d" % i, [128, 40])
            bufs.append(bb)
        k.pbank = 0

        def bank():
            b = k.pbank
            k.pbank = (k.pbank + 1) % 8
            return b

        def gdn_tile(ti):
            B = bufs[ti % NB]
            par = ti % 2
            bstate = [0]

            def bank():
                b_ = par * 4 + bstate[0]
                bstate[0] = (bstate[0] + 1) % 4
                return b_
            c0 = ti * 128
            sc, tsc = B["sc"]
            Gh, tGh = B["Gh"]
            for h in range(4):
                P.op("pool", lambda e, h=h, Gh=Gh, ti=ti: e.tensor_scalar(
                    out=Gh[:, h, :], in0=trile, scalar1=gall[:, ti, h:h + 1], scalar2=1.0,
                    op0=ALU.mult, op1=ALU.mult), [t_trile, t_gall], [tGh])
                yield
            bE, bDu, bDl, bsm = bank(), bank(), bank(), bank()
            for h in range(4):
                P.op("pe", lambda e, h=h, Gh=Gh, bE=bE: e.matmul(ps[bE][:, h * 128:(h + 1) * 128], lhsT=onesf,
                                                                 rhs=Gh[:, h, :], start=True, stop=True),
                     [tGh, t_onesf], [t_ps[bE]])
                yield
                P.op("pe", lambda e, h=h, Gh=Gh, bDu=bDu: e.matmul(ps[bDu][:, h * 128:(h + 1) * 128], lhsT=sgt,
                                                                   rhs=Gh[:, h, :], start=True, stop=True),
                     [tGh, t_sgt], [t_ps[bDu]])
                yield
                P.op("pe", lambda e, h=h, Gh=Gh, bDl=bDl: e.matmul(ps[bDl][:, h * 128:(h + 1) * 128], lhsT=Gh[:, h, :],
                                                                   rhs=sgt, start=True, stop=True),
                     [tGh, t_sgt], [t_ps[bDl]])
                yield
            if GCUT <= 1:
                return
            P.op("pe", lambda e, ti=ti, bsm=bsm: e.matmul(ps[bsm][:, 0:4], lhsT=trile, rhs=gall[:, ti, :],
                                                          start=True, stop=True), [t_trile, t_gall], [t_ps[bsm]])
            yield
            P.op("pe", lambda e, ti=ti, bsm=bsm: e.matmul(ps[bsm][:, 8:12], lhsT=onesf, rhs=gall[:, ti, :],
                                                          start=True, stop=True), [t_onesf, t_gall], [t_ps[bsm]])
            yield
            if GCUT <= 2:
                return
            E, tE = B["E"]
            Du, tDu = B["Du"]
            Dl, tDl = B["Dl"]
            fl = lambda a: a.rearrange("p h c -> p (h c)")
            P.op("act", lambda e, E=E, bE=bE: e.activation(out=fl(E), in_=ps[bE], func=AF.Exp), [t_ps[bE]], [tE])
            yield
            P.op("act", lambda e, Du=Du, bDu=bDu: e.activation(out=fl(Du), in_=ps[bDu], func=AF.Exp), [t_ps[bDu]], [tDu])
            yield
            P.op("act", lambda e, Dl=Dl, bDl=bDl: e.activation(out=fl(Dl), in_=ps[bDl], func=AF.Exp), [t_ps[bDl]], [tDl])
            yield
            P.op("pool", lambda e, Du=Du: e.tensor_tensor(out=Du, in0=Du, in1=trile.unsqueeze(1).to_broadcast([128, 4, 128]),
                                                          op=ALU.mult), [tDu, t_trile], [tDu])
            yield
            P.op("pool", lambda e, Dl=Dl: e.tensor_tensor(out=Dl, in0=Dl, in1=sgt.unsqueeze(1).to_broadcast([128, 4, 128]),
                                                          op=ALU.mult), [tDl, t_sgt], [tDl])
            yield
            if GCUT <= 3:
                return
            P.op("dve", lambda e, sc=sc, bsm=bsm: e.tensor_copy(out=sc[:, 0:4], in_=ps[bsm][:, 0:4]), [t_ps[bsm]], [tsc])
            yield
            P.op("act", lambda e, sc=sc: e.activation(out=sc[:, 4:8], in_=sc[:, 0:4], func=AF.Exp), [tsc], [tsc])
            yield
            P.op("dve", lambda e, sc=sc, bsm=bsm: e.tensor_tensor(out=sc[:, 24:28], in0=ps[bsm][:, 8:12], in1=sc[:, 0:4],
                                                                  op=ALU.subtract), [t_ps[bsm], tsc], [tsc])
            yield
            P.op("act", lambda e, sc=sc: e.activation(out=sc[:, 8:12], in_=sc[:, 24:28], func=AF.Exp), [tsc], [tsc])
            yield
            P.op("act", lambda e, sc=sc, bsm=bsm: e.activation(out=sc[:, 12:16], in_=ps[bsm][:, 8:12], func=AF.Exp),
                 [t_ps[bsm]], [tsc])
            yield
            P.op("dve", lambda e, sc=sc, ti=ti: e.tensor_tensor(out=sc[:, 16:20], in0=sc[:, 4:8], in1=ball[:, ti, :],
                                                                op=ALU.mult), [tsc, t_ball], [tsc])
            yield
            P.op("dve", lambda e, sc=sc, ti=ti: e.tensor_scalar(out=sc[:, 20:24], in0=ball[:, ti, :], scalar1=-1.0,
                                                                scalar2=None, op0=ALU.mult), [t_ball], [tsc])
            yield
            if GCUT <= 4:
                return
            kbg, tkbg = B["kbg"]
            kdec, tkdec = B["kdec"]
            vb, tvb = B["vb"]
            bc = lambda a: a.unsqueeze(2).to_broadcast([128, 4, 128])
            P.op("pool", lambda e, kbg=kbg, sc=sc, ti=ti: e.tensor_tensor(out=kbg, in0=k_tm[:, ti, :, :], in1=bc(sc[:, 16:20]),
                                                                          op=ALU.mult), [t_ktm, tsc], [tkbg])
            yield
            P.op("pool", lambda e, kdec=kdec, sc=sc, ti=ti: e.tensor_tensor(out=kdec, in0=k_tm[:, ti, :, :], in1=bc(sc[:, 8:12]),
                                                                            op=ALU.mult), [t_ktm, tsc], [tkdec])
            yield
            P.op("pool", lambda e, vb=vb, ti=ti: e.tensor_tensor(out=vb, in0=v_tm[:, ti, :, :], in1=bc(ball[:, ti, :]),
                                                                 op=ALU.mult), [t_vtm, t_ball], [tvb])
            yield
            if GCUT <= 5:
                return
            bkk = bank()
            for h in range(4):
                P.op("pe", lambda e, h=h, bkk=bkk: e.matmul(ps[bkk][:, h * 128:(h + 1) * 128], lhsT=kTg[:, h, c0:c0 + 128],
                                                            rhs=kTg[:, h, c0:c0 + 128], start=True, stop=True),
                     [t_kTg], [t_ps[bkk]])
                yield
            Lc, tLc = B["L0"]
            Ln_, tLn = B["L1"]
            Mc, tMc = B["M0"]
            Mn, tMn = B["M1"]
            Pc, tPc = B["P0"]
            Pn, tPn = B["P1"]
            for h in range(4):
                P.op("dve", lambda e, h=h, Lc=Lc, sc=sc, Dl=Dl, bkk=bkk: e.scalar_tensor_tensor(
                    out=Lc[:, h, :], in0=ps[bkk][:, h * 128:(h + 1) * 128], scalar=sc[:, 20 + h:21 + h], in1=Dl[:, h, :],
                    op0=ALU.mult, op1=ALU.mult), [t_ps[bkk], tsc, tDl], [tLc])
                yield
            if GCUT <= 6:
                return
            bM = bank()
            pbm = psb16[bM]
            for h in range(4):
                P.op("pe", lambda e, h=h, Lc=Lc, pbm=pbm: e.transpose(out=pbm[:, h * 128:(h + 1) * 128], in_=Lc[:, h, :],
                                                                      identity=identb), [tLc, t_identb], [t_ps[bM]])
                yield
            pm3 = pbm[:, 0:512].rearrange("p (h c) -> p h c", h=4)
            if GCUT == 61:
                return
            P.op("act", lambda e, Mc=Mc, pm3=pm3: e.activation(out=Mc, in_=pm3, func=AF.Copy), [t_ps[bM]], [tMc])
            yield
            if GCUT == 62:
                return
            P.op("pool", lambda e, Pc=Pc, Mc=Mc: e.tensor_tensor(out=Pc, in0=Mc,
                                                                 in1=identb.unsqueeze(1).to_broadcast([128, 4, 128]),
                                                                 op=ALU.add), [tMc, t_identb], [tPc])
            yield
            if GCUT <= 7 or GCUT in (61, 62):
                return
            for lev in range(6):
                last = (lev == 5)
                bL = bank()
                if not last:
                    bMM = bank()
                    for h in range(4):
                        P.op("pe", lambda e, h=h, Lc=Lc, Mc=Mc, bMM=bMM: e.matmul(
                            ps[bMM][:, h * 128:(h + 1) * 128], lhsT=Lc[:, h, :], rhs=Mc[:, h, :], start=True, stop=True),
                            [tLc, tMc], [t_ps[bMM]])
                        yield
                for h in range(4):
                    P.op("pe", lambda e, h=h, Lc=Lc, Mc=Mc, bL=bL: e.matmul(
                        ps[bL][:, h * 128:(h + 1) * 128], lhsT=Mc[:, h, :], rhs=Lc[:, h, :], start=True, stop=True),
                        [tLc, tMc], [t_ps[bL]])
                    yield
                P.op("dve", lambda e, Ln_=Ln_, bL=bL: e.tensor_copy(out=fl(Ln_), in_=ps[bL]), [t_ps[bL]], [tLn])
                yield
                if not last:
                    P.op("act", lambda e, Mn=Mn, bMM=bMM: e.activation(out=fl(Mn), in_=ps[bMM], func=AF.Copy),
                         [t_ps[bMM]], [tMn])
                    yield
                bP = bank()
                for h in range(4):
                    P.op("pe", lambda e, h=h, Ln_=Ln_, Pc=Pc, bP=bP: e.matmul(
                        ps[bP][:, h * 128:(h + 1) * 128], lhsT=Ln_[:, h, :], rhs=Pc[:, h, :], start=True, stop=True),
                        [tLn, tPc], [t_ps[bP]])
                    yield
                P.op("dve", lambda e, Pn=Pn, Pc=Pc, bP=bP: e.tensor_tensor(out=fl(Pn), in0=ps[bP], in1=fl(Pc), op=ALU.add),
                     [t_ps[bP], tPc], [tPn])
                yield
                Lc, tLc, Ln_, tLn = Ln_, tLn, Lc, tLc
                Mc, tMc, Mn, tMn = Mn, tMn, Mc, tMc
                Pc, tPc, Pn, tPn = Pn, tPn, Pc, tPc
            if GCUT <= 8:
                return
            bu, bw, bq = bank(), bank(), bank()
            for h in range(4):
                P.op("pe", lambda e, h=h, Pc=Pc, vb=vb, bu=bu: e.matmul(ps[bu][:, h * 128:(h + 1) * 128], lhsT=Pc[:, h, :],
                                                                        rhs=vb[:, h, :], start=True, stop=True),
                     [tPc, tvb], [t_ps[bu]])
                yield
                P.op("pe", lambda e, h=h, Pc=Pc, kbg=kbg, bw=bw: e.matmul(ps[bw][:, h * 128:(h + 1) * 128], lhsT=kbg[:, h, :],
                                                                          rhs=Pc[:, h, :], start=True, stop=True),
                     [tPc, tkbg], [t_ps[bw]])
                yield
                P.op("pe", lambda e, h=h, bq=bq: e.matmul(ps[bq][:, h * 128:(h + 1) * 128], lhsT=kTg[:, h, c0:c0 + 128],
                                                          rhs=qTg[:, h, c0:c0 + 128], start=True, stop=True),
                     [t_kTg, t_qTg], [t_ps[bq]])
                yield
            u, tu_ = B["u"]
            wT, twT = B["wT"]
            qgT, tqgT = B["qgT"]
            qkT, tqkT = B["qkT"]
            P.op("act", lambda e, u=u, bu=bu: e.activation(out=fl(u), in_=ps[bu], func=AF.Copy), [t_ps[bu]], [tu_])
            yield
            P.op("act", lambda e, wT=wT, bw=bw: e.activation(out=fl(wT), in_=ps[bw], func=AF.Copy), [t_ps[bw]], [twT])
            yield
            P.op("dve", lambda e, qkT=qkT, Du=Du, bq=bq: e.tensor_tensor(out=fl(qkT), in0=ps[bq], in1=fl(Du), op=ALU.mult),
                 [t_ps[bq], tDu], [tqkT])
            yield
            P.op("pool", lambda e, qgT=qgT, E=E: e.tensor_tensor(out=qgT, in0=qTg[:, :, c0:c0 + 128], in1=E, op=ALU.mult),
                 [t_qTg, tE], [tqgT])
            yield
            if GCUT <= 9:
                return
            yield 'SEQ'
            bws, bo, bs = bank(), bank(), bank()
            for h in range(4):
                P.op("pe", lambda e, h=h, wT=wT, bws=bws: e.matmul(ps[bws][:, h * 128:(h + 1) * 128], lhsT=wT[:, h, :],
                                                                   rhs=Sb[:, h, :], start=True, stop=True),
                     [twT, t_Sb], [t_ps[bws]])
            vnew, tvn = B["vnew"]
            P.op("dve", lambda e, vnew=vnew, u=u, bws=bws: e.tensor_tensor(out=fl(vnew), in0=fl(u), in1=ps[bws],
                                                                           op=ALU.subtract), [tu_, t_ps[bws]], [tvn])
            for h in range(4):
                P.op("pe", lambda e, h=h, qgT=qgT, bo=bo: e.matmul(ps[bo][:, h * 128:(h + 1) * 128], lhsT=qgT[:, h, :],
                                                                   rhs=Sb[:, h, :], start=True, stop=False),
                     [tqgT, t_Sb], [t_ps[bo]])
                P.op("pe", lambda e, h=h, qkT=qkT, vnew=vnew, bo=bo: e.matmul(ps[bo][:, h * 128:(h + 1) * 128], lhsT=qkT[:, h, :],
                                                                              rhs=vnew[:, h, :], start=False, stop=True),
                     [tqkT, tvn], [t_ps[bo]])
            for h in range(4):
                P.op("pe", lambda e, h=h, kdec=kdec, vnew=vnew, bs=bs: e.matmul(ps[bs][:, h * 128:(h + 1) * 128], lhsT=kdec[:, h, :],
                                                                                rhs=vnew[:, h, :], start=True, stop=True),
                     [tkdec, tvn], [t_ps[bs]])
            for h in range(4):
                P.op("dve", lambda e, h=h, sc=sc, bs=bs: e.scalar_tensor_tensor(
                    out=S[:, h, :], in0=S[:, h, :], scalar=sc[:, 12 + h:13 + h], in1=ps[bs][:, h * 128:(h + 1) * 128],
                    op0=ALU.mult, op1=ALU.add), [t_S, tsc, t_ps[bs]], [t_S])
            P.op("act", lambda e: e.activation(out=Sb, in_=S, func=AF.Copy), [t_S], [t_Sb])
            if GCUT <= 10:
                return
            on, ton = B["on"]
            og, tog = B["og"]
            P.op("act", lambda e, on=on, bo=bo: e.activation(out=fl(on), in_=ps[bo], func=AF.Square), [t_ps[bo]], [ton])
            P.op("dve", lambda e, on=on, sc=sc: e.tensor_reduce(out=sc[:, 28:32], in_=on, axis=AX.X, op=ALU.add),
                 [ton], [tsc])
            P.op("pool", lambda e, sc=sc: e.tensor_scalar(out=sc[:, 32:36], in0=sc[:, 28:32], scalar1=1.0 / 128, scalar2=EPS,
                                                          op0=ALU.mult, op1=ALU.add), [tsc], [tsc])
            P.op("pool", lambda e, sc=sc: e.tensor_tensor(out=sc[:, 32:36], in0=sc[:, 32:36], in1=neghalf[:, 0:4], op=ALU.pow),
                 [tsc, t_nh], [tsc])
            P.op("dve", lambda e, on=on, sc=sc, bo=bo: e.tensor_tensor(
                out=on, in0=ps[bo].rearrange("p (h c) -> p h c", h=4), in1=bc(sc[:, 32:36]), op=ALU.mult),
                [t_ps[bo], tsc], [ton])
            P.op("pool", lambda e, on=on: e.tensor_tensor(out=on, in0=on, in1=ggdn_b.unsqueeze(1).to_broadcast([128, 4, 128]),
                                                          op=ALU.mult), [ton, t_ggdn], [ton])
            P.op("pool", lambda e, on=on, og=og, ti=ti: e.tensor_tensor(
                out=og, in0=on, in1=sgz[:, ti, :].rearrange("p (h c) -> p h c", h=4), op=ALU.mult),
                [ton, t_sgz], [tog])
            bt = bank()
            pbt = psb16[bt]
            for h in range(4):
                P.op("pe", lambda e, h=h, og=og, pbt=pbt: e.transpose(out=pbt[:, h * 128:(h + 1) * 128], in_=og[:, h, :],
                                                                      identity=identb), [tog, t_identb], [t_ps[bt]])
            P.op("act", lambda e, pbt=pbt: e.activation(out=o_gdnT[:, :, c0:c0 + 128],
                                                        in_=pbt[:, 0:512].rearrange("p (h c) -> p h c", h=4), func=AF.Copy),
                 [t_ps[bt]], [t_ogT])
        def run_to_seq(gens):
            live = list(gens)
            while live:
                for g_ in list(live):
                    try:
                        if next(g_) == 'SEQ':
                            live.remove(g_)
                    except StopIteration:
                        live.remove(g_)

        def finish_gen(g_):
            for _ in g_:
                pass

        for t2 in range(0, NT if STOP > 1.8 else 2, 2):
            ga_, gb_ = gdn_tile(t2), gdn_tile(t2 + 1)
            run_to_seq([ga_, gb_])
            finish_gen(ga_)
            finish_gen(gb_)
        P.dma("sp", ssm_p[sq * 512:(sq + 1) * 512, :].rearrange("(h d) v -> d h v", h=4), S, reads=[t_S])

    NIT = int(os.environ.get("NIT", "18"))

    def dsa_phase(sq, o_atT, t_oatT):
        row_base = sq * SEQ
        qT2, t_qT2 = sbt("qT2", [128, NT, 512], BF16)
        kT2, t_kT2 = sbt("kT2", [128, SEQ], BF16)
        v_b, t_vb = sbt("v_b", [128, NT, 128], BF16)
        qiT2, t_qiT2 = sbt("qiT2", [128, 2, SEQ], BF16)
        kiT2, t_kiT2 = sbt("kiT2", [128, SEQ], BF16)
        wi_s, t_wi = sbt("wi_s", [128, NT, 4])
        A_TOP = k.top
        alloc_stage(1536)
        wA, t_wA = sbt("wA", [128, 8, 1092], BF16)
        load_weight(wA, t_wA, w_in, 0, 1092, gA, t_gA)
        wConv, t_wConv = sbt("wConv", [128, 8, 1536], BF16)
        load_weight(wConv, t_wConv, w_in, 1092, 2628, gA, t_gA)
        hT1, t_hT1 = sbt("hT1", [128, 8, 128], BF16)
        ko = [sbt("ko%d" % i, [128, 320]) for i in range(2)]
        cvo, t_cvo = sbt("cvo", [128, 1536])
        qnb, t_qnb = sbt("qnb", [128, 512], BF16)
        kb_, t_kb = sbt("kb_", [128, 128], BF16)
        qib, t_qib = sbt("qib", [128, 256], BF16)
        kib, t_kib = sbt("kib", [128, 128], BF16)

        def proj_tile(t):
            r0 = row_base + t * 128
            c0 = t * 128
            norm_transpose(xp[r0:r0 + 128, :], 128, hT1, t_hT1, 0, 0, key=("d", r0))
            if t + 1 < NT:
                prefetch_x(xp[r0 + 128:r0 + 256, :], 128, ("d", r0 + 128))
            for (b, a0, a1) in ((1, 0, 512), (2, 512, 1024), (3, 1024, 1092)):
                for kc in range(8):
                    P.op("pe", lambda e, kc=kc, b=b, a0=a0, a1=a1: e.matmul(
                        ps[b][:, 0:a1 - a0], lhsT=hT1[:, kc, :], rhs=wA[:, kc, a0:a1],
                        start=(kc == 0), stop=(kc == 7)), [t_hT1, t_wA], [t_ps[b]])
            o, to = ko[t % 2]
            if PCUT <= 1:
                return
            head_rms(ps[1], t_ps[1], 8, qscr, t_qscr, qss, t_qss)
            P.op("dve", lambda e: e.tensor_tensor(
                out=qnb.rearrange("p (r g d) -> p g r d", r=4, g=2),
                in0=ps[1].rearrange("p (g r d) -> p g r d", g=2, r=4),
                in1=qss[:, 8:16].rearrange("p (g r) -> p g r", g=2).unsqueeze(3).to_broadcast([128, 2, 4, 64]),
                op=ALU.mult), [t_ps[1], t_qss], [t_qnb])
            pbq = psb16[4]
            for r in range(4):
                P.op("pe", lambda e, r=r: e.transpose(out=pbq[:, r * 128:(r + 1) * 128], in_=qnb[:, r * 128:(r + 1) * 128],
                                                      identity=identb), [t_qnb, t_identb], [t_ps[4]])
            P.op("act", lambda e: e.activation(out=qT2[:, t, :], in_=pbq[:, 0:512], func=AF.Copy),
                 [t_ps[4]], [t_qT2])
            P.op("pool", lambda e: e.tensor_scalar(out=qT2[:, t, :], in0=qT2[:, t, :], scalar1=gq8[:, 0:1], scalar2=1.0,
                                                   op0=ALU.mult, op1=ALU.mult), [t_qT2, t_gq8], [t_qT2])
            if PCUT <= 2:
                return
            head_rms(ps[2][:, 0:128], t_ps[2], 2, qscr, t_qscr, qss, t_qss)
            P.op("dve", lambda e: e.tensor_tensor(
                out=o[:, 0:128].rearrange("p (h d) -> p h d", h=2),
                in0=ps[2][:, 0:128].rearrange("p (h d) -> p h d", h=2),
                in1=qss[:, 2:4].unsqueeze(2).to_broadcast([128, 2, 64]), op=ALU.mult),
                [t_ps[2], t_qss], [to])
            P.op("pool", lambda e: e.tensor_tensor(
                out=o[:, 0:128].rearrange("p (h d) -> p h d", h=2),
                in0=o[:, 0:128].rearrange("p (h d) -> p h d", h=2),
                in1=gk_b.unsqueeze(1).to_broadcast([128, 2, 64]), op=ALU.mult), [to, t_gk], [to])
            P.op("act", lambda e: e.activation(out=kb_, in_=o[:, 0:128], func=AF.Copy), [to], [t_kb])
            pb5 = psb16[5]
            P.op("pe", lambda e: e.transpose(out=pb5[:, 0:128], in_=kb_, identity=identb), [t_kb, t_identb], [t_ps[5]])
            if PCUT <= 3:
                return
            P.op("act", lambda e: e.activation(out=o[:, 128:256], in_=ps[2][:, 128:256], func=AF.Copy), [t_ps[2]], [to])
            P.op("act", lambda e: e.activation(out=v_b[:, t, :], in_=ps[2][:, 128:256], func=AF.Copy), [t_ps[2]], [t_vb])
            P.op("act", lambda e: e.activation(out=qib, in_=ps[2][:, 256:512], func=AF.Copy, scale=0.125),
                 [t_ps[2]], [t_qib])
            for a in range(2):
                P.op("pe", lambda e, a=a: e.transpose(out=pb5[:, 128 + a * 128:256 + a * 128],
                                                      in_=qib[:, a * 128:(a + 1) * 128], identity=identb),
                     [t_qib, t_identb], [t_ps[5]])
            if PCUT <= 4:
                return
            P.op("act", lambda e: e.activation(out=o[:, 256:320], in_=ps[3][:, 0:64], func=AF.Copy), [t_ps[3]], [to])
            P.op("act", lambda e: e.activation(out=kib[:, 0:64], in_=ps[3][:, 0:64], func=AF.Copy), [t_ps[3]], [t_kib])
            P.op("act", lambda e: e.activation(out=kib[:, 64:128], in_=ps[3][:, 0:64], func=AF.Copy), [t_ps[3]], [t_kib])
            P.op("pe", lambda e: e.transpose(out=pb5[:, 384:512], in_=kib, identity=identb), [t_kib, t_identb], [t_ps[5]])
            P.op("act", lambda e: e.activation(out=wi_s[:, t, :], in_=ps[3][:, 64:68], func=AF.Copy, scale=0.5),
                 [t_ps[3]], [t_wi])
            if PCUT <= 5:
                return
            P.op("act", lambda e: e.activation(out=kT2[:, c0:c0 + 128], in_=pb5[:, 0:128], func=AF.Copy), [t_ps[5]], [t_kT2])
            if PCUT == 51:
                return
            P.op("act", lambda e: e.activation(out=qiT2[:, :, c0:c0 + 128],
                                               in_=pb5[:, 128:384].rearrange("p (a t) -> p a t", a=2), func=AF.Copy),
                 [t_ps[5]], [t_qiT2])
            if PCUT == 52:
                return
            P.op("act", lambda e: e.activation(out=kiT2[:, c0:c0 + 128], in_=pb5[:, 384:512], func=AF.Copy),
                 [t_ps[5]], [t_kiT2])
            if PCUT == 53:
                return
            P.dma("sp", k_p[r0:r0 + 128, :], o[:, 0:128], reads=[to])
            P.dma("sp", v_p[r0:r0 + 128, :], o[:, 128:256], reads=[to])
            P.dma("sp", kidx_p[r0:r0 + 128, :], o[:, 256:320], reads=[to])
            if PCUT <= 6:
                return
            if t == NT - 1:
                for b in range(3):
                    for kc in range(8):
                        P.op("pe", lambda e, kc=kc, b=b: e.matmul(
                            ps[5 + b] if b < 2 else ps[0], lhsT=hT1[:, kc, :], rhs=wConv[:, kc, b * 512:(b + 1) * 512],
                            start=(kc == 0), stop=(kc == 7)), [t_hT1, t_wConv], [t_ps[5 + b] if b < 2 else t_ps[0]])
                for b in range(3):
                    bb = 5 + b if b < 2 else 0
                    P.op("act", lambda e, b=b, bb=bb: e.activation(out=cvo[:, b * 512:(b + 1) * 512], in_=ps[bb],
                                                                   func=AF.Copy), [t_ps[bb]], [t_cvo])
                P.dma("sp", conv_p[sq * 3:(sq + 1) * 3, :], cvo[125:128, :], reads=[t_cvo])

        for t in range(NT):
            proj_tile(t)
        P.barrier()
        if DCUT <= 1:
            return
        k.top = A_TOP
        scb = [sbt("scb%d" % i, [128, SEQ]) for i in range(2)]
        rl = [sbt("rl%d" % i, [128, 512]) for i in range(4)]
        junk2 = [sbt("junk%d" % i, [128, SEQ], BF16) for i in range(2)]
        mask2 = [sbt("mask%d" % i, [128, SEQ], BF16) for i in range(2)]
        maskT2 = [sbt("maskT%d" % i, [128, NT, 128], BF16) for i in range(4)]
        PT = [sbt("PT%d" % i, [128, 4, 128], BF16) for i in range(8)]
        bis2 = [sbt("bis%d" % i, [128, 64]) for i in range(2)]
        rec, t_rec = sbt("rec", [128, 4, 128])

        def q_sb(qt):
            L = (qt + 1) * 128
            q0 = qt * 128
            S_, tS = scb[qt % 2]
            maskT, t_maskT = maskT2[qt % 4]
            bis, t_bis = bis2[qt % 2]
            junk, t_junk = junk2[qt % 2]
            mask, t_mask = mask2[qt % 2]
            nch = (L + 511) // 512
            cnt_ = 0
            for ch in range(nch):
                k0 = ch * 512
                n = min(512, L - k0)
                for ih in range(4):
                    a, b = ih // 2, ih % 2
                    bnk = qt % 2
                    r_, tr = rl[(qt % 2) * 2 + cnt_ % 2]
                    cnt_ += 1
                    P.op("pe", lambda e, a=a, b=b, bnk=bnk, k0=k0, n=n: e.matmul(
                        ps[bnk][:, 0:n], lhsT=qiT2[64 * b:64 * b + 64, a, q0:q0 + 128],
                        rhs=kiT2[64 * b:64 * b + 64, k0:k0 + n], start=True, stop=True),
                        [t_qiT2, t_kiT2], [t_ps[bnk]])
                    yield
                    P.op("act", lambda e, r_=r_, bnk=bnk, n=n: e.activation(out=r_[:, 0:n], in_=ps[bnk][:, 0:n], func=AF.Relu),
                         [t_ps[bnk]], [tr])
                    yield
                    if ih == 0:
                        P.op("dve", lambda e, r_=r_, k0=k0, n=n: e.tensor_scalar(
                            out=S_[:, k0:k0 + n], in0=r_[:, 0:n], scalar1=wi_s[:, qt, 0:1], scalar2=None, op0=ALU.mult),
                            [tr, t_wi], [tS])
                        yield
                    else:
                        P.op("dve", lambda e, r_=r_, k0=k0, n=n, ih=ih: e.scalar_tensor_tensor(
                            out=S_[:, k0:k0 + n], in0=r_[:, 0:n], scalar=wi_s[:, qt, ih:ih + 1], in1=S_[:, k0:k0 + n],
                            op0=ALU.mult, op1=ALU.add), [tr, t_wi, tS], [tS])
                        yield
            if DCUT <= 2:
                return
            dg = S_[:, q0:q0 + 128]
            P.op("dve", lambda e: e.tensor_tensor(out=dg, in0=dg, in1=lowinc, op=ALU.mult), [tS, t_lowinc], [tS])
            yield
            P.op("dve", lambda e: e.tensor_reduce(out=bis[:, 0:1], in_=S_[:, 0:L], axis=AX.X, op=ALU.max), [tS], [t_bis])
            yield
            P.op("dve", lambda e: e.tensor_reduce(out=bis[:, 1:2], in_=S_[:, 0:L], axis=AX.X, op=ALU.min), [tS], [t_bis])
            yield
            P.op("pool", lambda e: e.tensor_tensor(out=dg, in0=dg, in1=tribias, op=ALU.add), [tS, t_tribias], [tS])
            yield
            P.op("dve", lambda e: e.tensor_tensor(out=bis[:, 2:3], in0=bis[:, 0:1], in1=bis[:, 1:2], op=ALU.subtract),
                 [t_bis], [t_bis])
            yield
            P.op("dve", lambda e: e.tensor_scalar(out=bis[:, 3:4], in0=bis[:, 2:3], scalar1=0.5005, scalar2=0.0005,
                                                  op0=ALU.mult, op1=ALU.add), [t_bis], [t_bis])
            yield
            P.op("dve", lambda e: e.tensor_tensor(out=bis[:, 4:5], in0=bis[:, 0:1], in1=bis[:, 3:4], op=ALU.subtract),
                 [t_bis], [t_bis])
            yield
            P.op("dve", lambda e: e.tensor_scalar(out=bis[:, 8:9 + NIT], in0=pow2_b[:, 0:NIT + 1], scalar1=bis[:, 3:4],
                                                  scalar2=None, op0=ALU.mult), [t_bis, t_pow2], [t_bis])
            yield
            if (qt % 2 == 1 or os.environ.get("ACTALL") == "1") and os.environ.get("ACTCNT", "1") == "1":
                P.op("dve", lambda e: e.tensor_scalar(out=bis[:, 40:41 + NIT], in0=bis[:, 8:9 + NIT], scalar1=-1.0, scalar2=None,
                                                      op0=ALU.mult), [t_bis], [t_bis])
                yield
                P.op("dve", lambda e: e.tensor_scalar(out=bis[:, 30:31], in0=bis[:, 4:5], scalar1=-1.0, scalar2=None,
                                                      op0=ALU.mult), [t_bis], [t_bis])
                yield
                for it in range(NIT):
                    P.op("act", lambda e: e.activation(out=junk[:, 0:L], in_=S_[:, 0:L], func=AF.Sign, bias=bis[:, 30:31],
                                                       scale=1.0, accum_out=bis[:, 31:32]), [tS, t_bis], [t_junk, t_bis])
                    yield
                    P.op("act", lambda e: e.activation(out=bis[:, 32:33], in_=bis[:, 31:32], func=AF.Sign,
                                                       bias=float(L - 510.5), scale=1.0), [t_bis], [t_bis])
                    yield
                    P.op("act", lambda e, it=it: e.activation(out=bis[:, 30:31], in_=bis[:, 32:33], func=AF.Identity,
                                                              bias=bis[:, 30:31], scale=bis[:, 41 + it:42 + it]),
                         [t_bis], [t_bis])
                    yield
                P.op("dve", lambda e: e.scalar_tensor_tensor(out=bis[:, 7:8], in0=bis[:, 30:31], scalar=-1.0,
                                                             in1=bis[:, 40 + NIT:41 + NIT], op0=ALU.mult, op1=ALU.add),
                     [t_bis], [t_bis])
                yield
            else:
              for _once in (0,):
                for it in range(NIT):
                    P.op("dve", lambda e: e.tensor_scalar(out=junk[:, 0:L], in0=S_[:, 0:L], scalar1=bis[:, 4:5], scalar2=None,
                                                          op0=ALU.is_ge, op1=ALU.add, accum_out=bis[:, 5:6]),
                         [tS, t_bis], [t_junk, t_bis])
                    yield
                    P.op("dve", lambda e: e.tensor_scalar(out=bis[:, 6:7], in0=bis[:, 5:6], scalar1=255.5, scalar2=0.5,
                                                          op0=ALU.is_ge, op1=ALU.subtract), [t_bis], [t_bis])
                    yield
                    P.op("dve", lambda e, it=it: e.scalar_tensor_tensor(out=bis[:, 4:5], in0=bis[:, 6:7], scalar=bis[:, 8 + it:9 + it],
                                                                        in1=bis[:, 4:5], op0=ALU.mult, op1=ALU.add),
                         [t_bis], [t_bis])
                    yield
                P.op("dve", lambda e: e.tensor_tensor(out=bis[:, 7:8], in0=bis[:, 4:5], in1=bis[:, 8 + NIT:9 + NIT], op=ALU.subtract),
                     [t_bis], [t_bis])
                yield
            P.op("dve", lambda e: e.tensor_scalar(out=mask[:, 0:L], in0=S_[:, 0:L], scalar1=bis[:, 7:8], scalar2=None,
                                                  op0=ALU.is_ge), [tS, t_bis], [t_mask])
            yield
            if DCUT <= 3:
                return
            for half in range((qt // 8) + 1):
                nb = min(8, qt + 1 - half * 8)
                for j in range(nb):
                    kb = half * 8 + j
                    P.op("pe", lambda e, kb=kb, j=j, half=half: e.transpose(
                        out=psb16[qt % 2][:, j * 128:(j + 1) * 128], in_=mask[:, kb * 128:(kb + 1) * 128], identity=identb),
                        [t_mask, t_identb], [t_ps[qt % 2]])
                    yield
                P.op("act", lambda e, half=half, nb=nb: e.activation(
                    out=maskT[:, half * 8:half * 8 + nb, :],
                    in_=psb16[qt % 2][:, 0:nb * 128].rearrange("p (j t) -> p j t", j=nb), func=AF.Copy),
                    [t_ps[qt % 2]], [t_maskT])
                yield
        def q_att(qt):
            L = (qt + 1) * 128
            q0 = qt * 128
            maskT, t_maskT = maskT2[qt % 4]
            items = [(kb, g) for kb in range(qt + 1) for g in range(2)]
            LA = 3

            def front(idx):
                kb, g = items[idx]
                bS = 2 + (idx % 4)
                PTt, tPT = PT[idx % len(PT)]
                P.op("pe", lambda e: e.matmul(
                    ps[bS], lhsT=kT2[64 * g:64 * g + 64, kb * 128:(kb + 1) * 128],
                    rhs=qT2[64 * g:64 * g + 64, qt, :], start=True, stop=True),
                    [t_kT2, t_qT2], [t_ps[bS]])
                P.op("act", lambda e: e.activation(out=PTt.rearrange("p r t -> p (r t)"), in_=ps[bS],
                                                   func=AF.Exp), [t_ps[bS]], [tPT])
                P.op("pool", lambda e: e.tensor_tensor(
                    out=PTt, in0=PTt, in1=maskT[:, kb, :].unsqueeze(1).to_broadcast([128, 4, 128]), op=ALU.mult),
                    [tPT, t_maskT], [tPT])

            def back(idx):
                kb, g = items[idx]
                PTt, tPT = PT[idx % len(PT)]
                P.op("pe", lambda e: e.matmul(
                    ps[6][64 * g:64 * g + 64, :], lhsT=v_b[:, kb, 64 * g:64 * g + 64],
                    rhs=PTt.rearrange("p r t -> p (r t)"), start=(kb == 0), stop=(kb == qt)),
                    [tPT, t_vb], [t_ps[6]])
                P.op("pe", lambda e: e.matmul(
                    ps[7][64 * g:64 * g + 64, :], lhsT=onesb[:, 0:64],
                    rhs=PTt.rearrange("p r t -> p (r t)"), start=(kb == 0), stop=(kb == qt)),
                    [tPT, t_onesb], [t_ps[7]])

            n_it = len(items)
            for idx in range(n_it + LA):
                if idx < n_it:
                    front(idx)
                if idx - LA >= 0:
                    back(idx - LA)
            P.op("act", lambda e: e.activation(out=rec.rearrange("p r t -> p (r t)"), in_=ps[7], func=AF.Ln), [t_ps[7]], [t_rec])
            P.op("act", lambda e: e.activation(out=rec.rearrange("p r t -> p (r t)"), in_=rec.rearrange("p r t -> p (r t)"),
                                               func=AF.Exp, scale=-1.0), [t_rec], [t_rec])
            P.op("dve", lambda e: e.tensor_tensor(out=o_atT[:, :, q0:q0 + 128],
                                                  in0=ps[6].rearrange("p (r t) -> p r t", r=4), in1=rec, op=ALU.mult),
                 [t_ps[6], t_rec], [t_oatT])

        def lockstep(gens):
            gens = list(gens)
            while gens:
                for g_ in list(gens):
                    try:
                        next(g_)
                    except StopIteration:
                        gens.remove(g_)

        lockstep([q_sb(0), q_sb(1)])
        for p_ in range(0, NT, 2):
            if p_ + 2 < NT:
                lockstep([q_sb(p_ + 2), q_sb(p_ + 3)])
            q_att(p_)
            q_att(p_ + 1)

    def c1_phase(sq, o_atT, t_oatT, o_gdnT, t_ogT, mkT2, t_mkT2, mv_b, t_mv):
        row_base = sq * SEQ
        alloc_stage(1024)
        Wo_a, t_Woa = sbt("Wo_a", [128, 4, 1024], BF16)
        Wo_g, t_Wog = sbt("Wo_g", [128, 4, 1024], BF16)
        Wmq, t_Wmq = sbt("Wmq", [128, 8, 256], BF16)
        Wmo, t_Wmo = sbt("Wmo", [128, 2, 1024], BF16)
        for r in range(4):
            i = k.wl % k.nst
            k.wl += 1
            st = stage[i][:, 0:1024]
            for g in range(2):
                P.dma("sp", st[64 * g:64 * g + 64, :], w_out[256 * g + 64 * r:256 * g + 64 * r + 64, :],
                      writes=[t_stage[i]])
            P.op("act", lambda e, st=st, r=r: e.activation(out=Wo_a[:, r, :], in_=st, func=AF.Copy),
                 [t_stage[i]], [t_Woa])
        for h in range(4):
            load_rows(Wo_g[:, h, :], t_Wog, w_out[512 + 128 * h:512 + 128 * h + 128, :], 1024, None, None)
        load_weight(Wmq, t_Wmq, w_mq, 0, 256, gX, t_gX)
        for a in range(2):
            load_rows(Wmo[:, a, :], t_Wmo, w_mo[128 * a:128 * a + 128, :], 1024, None, None)
        x1 = [sbt("x1_%d" % i, [128, D]) for i in range(2)]
        hT2, t_hT2 = sbt("hT2", [128, 8, 128], BF16)
        qmb2 = [sbt("qmb%d" % i, [128, 256], BF16) for i in range(2)]
        qmT2, t_qmT2 = sbt("qmT2", [128, 2, 128], BF16)
        PTm, t_PTm = sbt("PTm", [128, 2, 4, 128], BF16)
        omT2, t_omT2 = sbt("omT2", [128, 2, 128], BF16)
        recm, t_recm = sbt("recm", [128, 256])

        def c1_h1(t):
            r0 = row_base + t * 128
            c0 = t * 128
            if CCUT <= 0:
                return
            x, tx, i = load_x(xp[r0:r0 + 128, :], 128, key=("c", r0))
            if t + 1 < NT:
                prefetch_x(xp[r0 + 128:r0 + 256, :], 128, ("c", r0 + 128))
            xx, txx = x1[t % 2]
            qmb, t_qmb = qmb2[t % 2]
            for c in range(2):
                for r in range(4):
                    P.op("pe", lambda e, r=r, c=c: e.matmul(ps[1 + c], lhsT=o_atT[:, r, c0:c0 + 128],
                                                            rhs=Wo_a[:, r, c * 512:(c + 1) * 512], start=(r == 0), stop=False),
                         [t_oatT, t_Woa], [t_ps[1 + c]])
                for h in range(4):
                    P.op("pe", lambda e, h=h, c=c: e.matmul(ps[1 + c], lhsT=o_gdnT[:, h, c0:c0 + 128],
                                                            rhs=Wo_g[:, h, c * 512:(c + 1) * 512], start=False, stop=(h == 3)),
                         [t_ogT, t_Wog], [t_ps[1 + c]])
                P.op("dve", lambda e, c=c: e.tensor_tensor(out=xx[:, c * 512:(c + 1) * 512], in0=ps[1 + c],
                                                           in1=x[:, c * 512:(c + 1) * 512], op=ALU.add),
                     [t_ps[1 + c], tx], [txx])
            if CCUT <= 1:
                return
            norm_T(xx, txx, i, hT2, t_hT2, 0, 0)
            for kc in range(8):
                P.op("pe", lambda e, kc=kc: e.matmul(ps[3][:, 0:256], lhsT=hT2[:, kc, :], rhs=Wmq[:, kc, :],
                                                     start=(kc == 0), stop=(kc == 7)), [t_hT2, t_Wmq], [t_ps[3]])
            if CCUT <= 2:
                return
            head_rms(ps[3][:, 0:256], t_ps[3], 4, qscr, t_qscr, qss, t_qss)
            P.op("dve", lambda e: e.tensor_tensor(
                out=qmb.rearrange("p (h d) -> p h d", h=4), in0=ps[3][:, 0:256].rearrange("p (h d) -> p h d", h=4),
                in1=qss[:, 4:8].unsqueeze(2).to_broadcast([128, 4, 64]), op=ALU.mult), [t_ps[3], t_qss], [t_qmb])
        def c1_h2(t):
            r0 = row_base + t * 128
            c0 = t * 128
            xx, txx = x1[t % 2]
            qmb, t_qmb = qmb2[t % 2]
            pb4 = psb16[4]
            for a in range(2):
                P.op("pe", lambda e, a=a: e.transpose(out=pb4[:, a * 128:(a + 1) * 128], in_=qmb[:, a * 128:(a + 1) * 128],
                                                      identity=identb), [t_qmb, t_identb], [t_ps[4]])
            P.op("act", lambda e: e.activation(out=qmT2.rearrange("p a t -> p (a t)"), in_=pb4[:, 0:256], func=AF.Copy),
                 [t_ps[4]], [t_qmT2])
            P.op("pool", lambda e: e.tensor_scalar(out=qmT2.rearrange("p a t -> p (a t)"), in0=qmT2.rearrange("p a t -> p (a t)"),
                                                   scalar1=gmq8[:, 0:1], scalar2=1.0, op0=ALU.mult, op1=ALU.mult),
                 [t_qmT2, t_gmq8], [t_qmT2])
            if CCUT <= 3:
                return
            for b in range(2):
                for mb in range(2):
                    for a in range(2):
                        j = mb * 2 + a
                        P.op("pe", lambda e, mb=mb, a=a, b=b, j=j: e.matmul(
                            ps[5 + b][:, j * 128:(j + 1) * 128], lhsT=mkT2[64 * b:64 * b + 64, a, mb * 128:(mb + 1) * 128],
                            rhs=qmT2[64 * b:64 * b + 64, a, :], start=True, stop=True), [t_mkT2, t_qmT2], [t_ps[5 + b]])
                P.op("act", lambda e, b=b: e.activation(out=PTm[:, b, :, :].rearrange("p j t -> p (j t)"),
                                                        in_=ps[5 + b], func=AF.Exp), [t_ps[5 + b]], [t_PTm])
            if CCUT <= 4:
                return
            for mh in range(4):
                a, b = mh // 2, mh % 2
                for mb in range(2):
                    P.op("pe", lambda e, mb=mb, mh=mh, a=a, b=b: e.matmul(
                        ps[7][64 * b:64 * b + 64, a * 128:(a + 1) * 128], lhsT=mv_b[:, mb, mh * 64:(mh + 1) * 64],
                        rhs=PTm[:, b, mb * 2 + a, :], start=(mb == 0), stop=(mb == 1)), [t_mv, t_PTm], [t_ps[7]])
                for mb in range(2):
                    P.op("pe", lambda e, mb=mb, mh=mh, a=a, b=b: e.matmul(
                        ps[7][64 * b:64 * b + 64, 256 + a * 128:256 + (a + 1) * 128], lhsT=onesb[:, 0:64],
                        rhs=PTm[:, b, mb * 2 + a, :], start=(mb == 0), stop=(mb == 1)), [t_onesb, t_PTm], [t_ps[7]])
            if CCUT <= 5:
                return
            P.op("dve", lambda e: e.reciprocal(out=recm, in_=ps[7][:, 256:512]), [t_ps[7]], [t_recm])
            P.op("dve", lambda e: e.tensor_tensor(out=omT2.rearrange("p a t -> p (a t)"), in0=ps[7][:, 0:256], in1=recm,
                                                  op=ALU.mult), [t_ps[7], t_recm], [t_omT2])
            for c in range(2):
                for a in range(2):
                    P.op("pe", lambda e, a=a, c=c: e.matmul(ps[5 + c], lhsT=omT2[:, a, :],
                                                            rhs=Wmo[:, a, c * 512:(c + 1) * 512], start=(a == 0), stop=(a == 1)),
                         [t_omT2, t_Wmo], [t_ps[5 + c]])
                P.op("dve", lambda e, c=c: e.tensor_tensor(out=xx[:, c * 512:(c + 1) * 512], in0=ps[5 + c],
                                                           in1=xx[:, c * 512:(c + 1) * 512], op=ALU.add),
                     [t_ps[5 + c], txx], [txx])
            P.dma("sp", y_p[r0:r0 + 128, :], xx, reads=[txx])

        c1_h1(0)
        for t in range(NT):
            if t + 1 < NT:
                c1_h1(t + 1)
            c1_h2(t)

    t_yscr = Tok()

    def c2_phase(sq):
        row_base = sq * SEQ
        alloc_stage(2816, 3)
        Wg, t_Wg = sbt("Wg", [128, 8, 2816], BF16)
        Wu, t_Wu = sbt("Wu", [128, 8, 2816], BF16)
        Wd, t_Wd = sbt("Wd", [128, 22, 1024], BF16)
        load_weight(Wg, t_Wg, w_gate, 0, 2816, gF, t_gF)
        load_weight(Wu, t_Wu, w_up, 0, 2816, gF, t_gF)
        for f in range(22):
            load_rows(Wd[:, f, :], t_Wd, w_down[128 * f:128 * f + 128, :], 1024, None, None)
        hT3, t_hT3 = sbt("hT3", [128, 8, 256], BF16)
        hfT, t_hfT = sbt("hfT", [128, 22, 256], BF16)
        sg = [sbt("sg%d" % i, [128, 256]) for i in range(2)]

        def c2_group(gi):
            xs_ = []
            for t2 in range(2):
                r0 = row_base + (gi * 2 + t2) * 128
                x, tx, i = load_x(y_p[r0:r0 + 128, :], 128)
                norm_T(x, tx, i, hT3, t_hT3, t2 * 128, 0)
                xs_.append((x, tx, r0))
            for f in range(22):
                b = 1 + (f % 2)
                for kc in range(8):
                    P.op("pe", lambda e, kc=kc, f=f, b=b: e.matmul(ps[b][:, 0:256], lhsT=Wg[:, kc, 128 * f:128 * f + 128],
                                                                   rhs=hT3[:, kc, :], start=(kc == 0), stop=(kc == 7)),
                         [t_Wg, t_hT3], [t_ps[b]])
                for kc in range(8):
                    P.op("pe", lambda e, kc=kc, f=f, b=b: e.matmul(ps[b][:, 256:512], lhsT=Wu[:, kc, 128 * f:128 * f + 128],
                                                                   rhs=hT3[:, kc, :], start=(kc == 0), stop=(kc == 7)),
                         [t_Wu, t_hT3], [t_ps[b]])
                s_, ts_ = sg[f % 2]
                P.op("act", lambda e, s_=s_, b=b: e.activation(out=s_, in_=ps[b][:, 0:256], func=AF.Silu), [t_ps[b]], [ts_])
                P.op("dve", lambda e, s_=s_, b=b, f=f: e.tensor_tensor(out=hfT[:, f, :], in0=s_, in1=ps[b][:, 256:512],
                                                                       op=ALU.mult), [ts_, t_ps[b]], [t_hfT])
            for t2 in range(2):
                x, tx, r0 = xs_[t2]
                for c in range(2):
                    for f in range(22):
                        P.op("pe", lambda e, f=f, c=c, t2=t2: e.matmul(ps[3 + c], lhsT=hfT[:, f, t2 * 128:(t2 + 1) * 128],
                                                                       rhs=Wd[:, f, c * 512:(c + 1) * 512],
                                                                       start=(f == 0), stop=(f == 21)),
                             [t_hfT, t_Wd], [t_ps[3 + c]])
                    P.op("dve", lambda e, c=c, x=x: e.tensor_tensor(out=x[:, c * 512:(c + 1) * 512], in0=ps[3 + c],
                                                                    in1=x[:, c * 512:(c + 1) * 512], op=ALU.add),
                         [t_ps[3 + c], tx], [tx])
                P.dma("sp", y_p[r0:r0 + 128, :], x, reads=[tx])

        for gi in range(NT // 2):
            c2_group(gi)

    def sample_group():
        k.top = PERSIST_TOP
        NSB = NS
        proj, t_proj = sbt("s_proj", [128, INW])
        x_keep, t_xk = sbt("s_xkeep", [128, D])
        sso, t_sso = sbt("s_o", [128, 320])
        ss_, t_ss = sbt("s_ss", [128, 64])
        scr, t_scr = sbt("s_scr", [128, 1536])
        S_TOP = k.top
        alloc_stage(INW)
        wAll, t_wAll = sbt("s_wAll", [128, 8, INW], BF16)
        load_weight(wAll, t_wAll, w_in, 0, INW, gA, t_gA)
        hTs, t_hTs = sbt("s_hT", [128, 8, 128], BF16)
        x, tx, xi = load_x(xs[:, :], NSB)
        P.op("pool", lambda e: e.tensor_copy(out=x_keep[0:NSB, :], in_=x[0:NSB, :]), [tx], [t_xk])
        norm_T(x, tx, xi, hTs, t_hTs, 0, 0)
        for c in range(7):
            c0 = c * 512
            n = min(512, INW - c0)
            b = 1 + (c % 2)
            for kc in range(8):
                P.op("pe", lambda e, kc=kc, b=b, c0=c0, n=n: e.matmul(
                    ps[b][0:NSB, 0:n], lhsT=hTs[:, kc, 0:NSB], rhs=wAll[:, kc, c0:c0 + n],
                    start=(kc == 0), stop=(kc == 7)), [t_hTs, t_wAll], [t_ps[b]])
            P.op("act", lambda e, b=b, c0=c0, n=n: e.activation(out=proj[0:NSB, c0:c0 + n], in_=ps[b][0:NSB, 0:n],
                                                                func=AF.Copy), [t_ps[b]], [t_proj])
        pj = proj[0:NSB]
        so = sso[0:NSB]
        P.op("dve", lambda e: e.tensor_tensor(out=scr[0:NSB, 0:128], in0=pj[:, 512:640], in1=pj[:, 512:640], op=ALU.mult),
             [t_proj], [t_scr])
        P.op("dve", lambda e: e.tensor_reduce(out=ss_[0:NSB, 0:2], in_=scr[0:NSB, 0:128].rearrange("p (h d) -> p h d", h=2),
                                              axis=AX.X, op=ALU.add), [t_scr], [t_ss])
        P.op("pool", lambda e: e.tensor_scalar(out=ss_[0:NSB, 2:4], in0=ss_[0:NSB, 0:2], scalar1=1.0 / 64, scalar2=EPS,
                                               op0=ALU.mult, op1=ALU.add), [t_ss], [t_ss])
        P.op("pool", lambda e: e.tensor_tensor(out=ss_[0:NSB, 2:4], in0=ss_[0:NSB, 2:4], in1=neghalf[0:NSB, 0:2], op=ALU.pow),
             [t_ss, t_nh], [t_ss])
        P.op("dve", lambda e: e.tensor_tensor(out=so[:, 0:128].rearrange("p (h d) -> p h d", h=2),
                                              in0=pj[:, 512:640].rearrange("p (h d) -> p h d", h=2),
                                              in1=ss_[0:NSB, 2:4].unsqueeze(2).to_broadcast([NSB, 2, 64]), op=ALU.mult),
             [t_proj, t_ss], [t_sso])
        P.op("pool", lambda e: e.tensor_tensor(out=so[:, 0:128].rearrange("p (h d) -> p h d", h=2),
                                               in0=so[:, 0:128].rearrange("p (h d) -> p h d", h=2),
                                               in1=gk_b[0:NSB].unsqueeze(1).to_broadcast([NSB, 2, 64]), op=ALU.mult),
             [t_sso, t_gk], [t_sso])
        P.op("act", lambda e: e.activation(out=so[:, 128:256], in_=pj[:, 640:768], func=AF.Copy), [t_proj], [t_sso])
        P.op("act", lambda e: e.activation(out=so[:, 256:320], in_=pj[:, 1024:1088], func=AF.Copy), [t_proj], [t_sso])
        P.dma("sp", k_s[:, :], so[:, 0:128], reads=[t_sso])
        P.dma("sp", v_s[:, :], so[:, 128:256], reads=[t_sso])
        P.dma("sp", kidx_s[:, :], so[:, 256:320], reads=[t_sso])
        conv_s3 = conv_s.rearrange("(s r) c -> s r c", r=3)
        st_conv3 = st_conv.rearrange("(s r) c -> s r c", r=3)
        P.dma("sp", conv_s3[:, 2, :], pj[:, 1092:2628], reads=[t_proj])
        P.dma("sp", conv_s3[:, 0:2, :], st_conv3[:, 1:3, :])
        P.barrier()
        k.top = S_TOP
        stc, t_stc = sbt("s_stc", [128, 3, 1536])
        cwb, t_cwb = sbt("s_cwb", [128, 4, 1536])
        P.dma("sp", stc[0:NSB], st_conv3, writes=[t_stc])
        P.dma("sp", cwb[0:NSB], conv_w.rearrange("t c -> (t c)").partition_broadcast(NSB), writes=[t_cwb])
        cc, t_cc = sbt("s_cc", [128, 1536])
        c_ = cc[0:NSB]
        sc_ = scr[0:NSB]
        P.op("dve", lambda e: e.tensor_tensor(out=c_, in0=pj[:, 1092:2628], in1=cwb[0:NSB, 3, :], op=ALU.mult),
             [t_proj, t_cwb], [t_cc])
        for j in range(3):
            P.op("dve", lambda e, j=j: e.tensor_tensor(out=sc_, in0=stc[0:NSB, j, :], in1=cwb[0:NSB, j, :], op=ALU.mult),
                 [t_stc, t_cwb], [t_scr])
            P.op("dve", lambda e: e.tensor_tensor(out=c_, in0=c_, in1=sc_, op=ALU.add), [t_cc, t_scr], [t_cc])
        P.op("act", lambda e: e.activation(out=c_, in_=c_, func=AF.Silu), [t_cc], [t_cc])
        P.op("dve", lambda e: e.tensor_tensor(out=sc_[:, 0:1024], in0=c_[:, 0:1024], in1=c_[:, 0:1024], op=ALU.mult),
             [t_cc], [t_scr])
        P.op("dve", lambda e: e.tensor_reduce(out=ss_[0:NSB, 8:16], in_=sc_[:, 0:1024].rearrange("p (h d) -> p h d", h=8),
                                              axis=AX.X, op=ALU.add), [t_scr], [t_ss])
        P.op("pool", lambda e: e.tensor_scalar(out=ss_[0:NSB, 16:24], in0=ss_[0:NSB, 8:16], scalar1=1.0, scalar2=EPS,
                                               op0=ALU.mult, op1=ALU.add), [t_ss], [t_ss])
        P.op("pool", lambda e: e.tensor_tensor(out=ss_[0:NSB, 16:24], in0=ss_[0:NSB, 16:24], in1=neghalf[0:NSB, 0:8], op=ALU.pow),
             [t_ss, t_nh], [t_ss])
        P.op("pool", lambda e: e.tensor_scalar(out=ss_[0:NSB, 16:20], in0=ss_[0:NSB, 16:20], scalar1=float(128.0 ** -0.5),
                                               scalar2=1.0, op0=ALU.mult, op1=ALU.mult), [t_ss], [t_ss])
        P.op("dve", lambda e: e.tensor_tensor(out=c_[:, 0:1024].rearrange("p (h d) -> p h d", h=8),
                                              in0=c_[:, 0:1024].rearrange("p (h d) -> p h d", h=8),
                                              in1=ss_[0:NSB, 16:24].unsqueeze(2).to_broadcast([NSB, 8, 128]), op=ALU.mult),
             [t_cc, t_ss], [t_cc])
        sv = ss_[0:NSB]
        P.op("dve", lambda e: e.tensor_tensor(out=sv[:, 24:28], in0=pj[:, 3140:3144], in1=dtb_b[0:NSB], op=ALU.add),
             [t_proj, t_dtb], [t_ss])
        P.op("dve", lambda e: e.tensor_scalar(out=sv[:, 28:32], in0=sv[:, 24:28], scalar1=-1.0, scalar2=None, op0=ALU.mult),
             [t_ss], [t_ss])
        P.op("dve", lambda e: e.tensor_tensor(out=sv[:, 28:32], in0=sv[:, 28:32], in1=sv[:, 24:28], op=ALU.min), [t_ss], [t_ss])
        P.op("act", lambda e: e.activation(out=sv[:, 28:32], in_=sv[:, 28:32], func=AF.Exp), [t_ss], [t_ss])
        P.op("act", lambda e: e.activation(out=sv[:, 28:32], in_=sv[:, 28:32], func=AF.Ln, bias=1.0, scale=1.0), [t_ss], [t_ss])
        P.op("dve", lambda e: e.scalar_tensor_tensor(out=sv[:, 32:36], in0=sv[:, 24:28], scalar=0.0, in1=sv[:, 28:32],
                                                     op0=ALU.max, op1=ALU.add), [t_ss], [t_ss])
        P.op("dve", lambda e: e.tensor_tensor(out=sv[:, 32:36], in0=sv[:, 32:36], in1=nea_b[0:NSB], op=ALU.mult),
             [t_ss, t_nea], [t_ss])
        P.op("act", lambda e: e.activation(out=sv[:, 36:40], in_=pj[:, 3144:3148], func=AF.Sigmoid), [t_proj], [t_ss])
        P.op("act", lambda e: e.activation(out=sv[:, 40:44], in_=sv[:, 32:36], func=AF.Exp), [t_ss], [t_ss])
        S0, t_S0 = sbt("s_S0", [128, NSB, 4, 128])
        for i in range(NSB):
            P.dma("sp", S0[:, i, :, :], ssm_in[i * 512:(i + 1) * 512, :].rearrange("(h d) v -> d h v", h=4), writes=[t_S0])
        kqT, t_kqT = sbt("s_kqT", [128, 8, NSB])
        for j in range(8):
            b = 1 + (j % 2)
            P.op("pe", lambda e, j=j, b=b: e.transpose(out=ps[b][:, 0:NSB], in_=c_[:, j * 128:(j + 1) * 128],
                                                       identity=identf[0:NSB, 0:NSB]), [t_cc, t_identf], [t_ps[b]])
            P.op("act", lambda e, j=j, b=b: e.activation(out=kqT[:, j, :], in_=ps[b][:, 0:NSB], func=AF.Copy),
                 [t_ps[b]], [t_kqT])
        eye_b, t_eyeb = sbt("s_eyeb", [128, NSB, NSB])
        P.dma("sp", eye_b, eye16_d.partition_broadcast(128), writes=[t_eyeb])
        kqTm, t_kqTm = sbt("s_kqTm", [128, 8, NSB, NSB])
        P.op("pool", lambda e: e.tensor_tensor(out=kqTm, in0=kqT.unsqueeze(2).to_broadcast([128, 8, NSB, NSB]),
                                               in1=eye_b.unsqueeze(1).to_broadcast([128, 8, NSB, NSB]), op=ALU.mult),
             [t_kqT, t_eyeb], [t_kqTm])
        for h in range(4):
            for i in range(NSB):
                P.op("pe", lambda e, h=h, i=i: e.matmul(ps[3][0:NSB, h * 128:(h + 1) * 128], lhsT=kqTm[:, 4 + h, i, :],
                                                        rhs=S0[:, i, h, :], start=(i == 0), stop=(i == NSB - 1)),
                     [t_kqTm, t_S0], [t_ps[3]])
        dl, t_dl = sbt("s_dl", [128, 4, 128])
        d_ = dl[0:NSB]
        bcs = lambda a: a.unsqueeze(2).to_broadcast([NSB, 4, 128])
        P.op("dve", lambda e: e.tensor_tensor(out=d_, in0=ps[3][0:NSB, :].rearrange("p (h v) -> p h v", h=4),
                                              in1=bcs(sv[:, 40:44]), op=ALU.mult), [t_ps[3], t_ss], [t_dl])
        P.op("dve", lambda e: e.tensor_tensor(out=d_, in0=c_[:, 1024:1536].rearrange("p (h v) -> p h v", h=4), in1=d_,
                                              op=ALU.subtract), [t_cc, t_dl], [t_dl])
        P.op("dve", lambda e: e.tensor_tensor(out=d_, in0=d_, in1=bcs(sv[:, 36:40]), op=ALU.mult), [t_dl, t_ss], [t_dl])
        ckm, t_ckm = sbt("s_ckm", [128, NSB, 512])
        P.op("pool", lambda e: e.tensor_tensor(out=ckm[0:NSB], in0=c_[:, 512:1024].unsqueeze(1).to_broadcast([NSB, NSB, 512]),
                                               in1=identf[0:NSB, 0:NSB].unsqueeze(2).to_broadcast([NSB, NSB, 512]), op=ALU.mult),
             [t_cc, t_identf], [t_ckm])
        adg, t_adg = sbt("s_adg", [128, NSB, 4])
        P.op("pool", lambda e: e.tensor_tensor(out=adg[0:NSB], in0=sv[:, 40:44].unsqueeze(1).to_broadcast([NSB, NSB, 4]),
                                               in1=identf[0:NSB, 0:NSB].unsqueeze(2).to_broadcast([NSB, NSB, 4]), op=ALU.mult),
             [t_ss, t_identf], [t_adg])
        P.op("pe", lambda e: e.matmul(ps[4][:, 0:NSB * 4], lhsT=onesf[0:NSB, :], rhs=adg[0:NSB].rearrange("p i h -> p (i h)"),
                                      start=True, stop=True), [t_adg, t_onesf], [t_ps[4]])
        abc, t_abc = sbt("s_abc", [128, NSB * 4])
        P.op("act", lambda e: e.activation(out=abc, in_=ps[4][:, 0:NSB * 4], func=AF.Copy), [t_ps[4]], [t_abc])
        for i in range(NSB):
            b = 5 + (i % 2)
            for h in range(4):
                P.op("pe", lambda e, h=h, i=i, b=b: e.matmul(ps[b][:, h * 128:(h + 1) * 128], lhsT=ckm[0:NSB, i, h * 128:(h + 1) * 128],
                                                             rhs=d_[:, h, :], start=True, stop=True), [t_ckm, t_dl], [t_ps[b]])
            for h in range(4):
                P.op("dve", lambda e, h=h, i=i, b=b: e.scalar_tensor_tensor(
                    out=S0[:, i, h, :], in0=S0[:, i, h, :], scalar=abc[:, i * 4 + h:i * 4 + h + 1],
                    in1=ps[b][:, h * 128:(h + 1) * 128], op0=ALU.mult, op1=ALU.add), [t_S0, t_abc, t_ps[b]], [t_S0])
            P.dma("sp", ssm_s[i * 512:(i + 1) * 512, :].rearrange("(h d) v -> d h v", h=4), S0[:, i, :, :], reads=[t_S0])
        for h in range(4):
            for i in range(NSB):
                P.op("pe", lambda e, h=h, i=i: e.matmul(ps[7][0:NSB, h * 128:(h + 1) * 128], lhsT=kqTm[:, h, i, :],
                                                        rhs=S0[:, i, h, :], start=(i == 0), stop=(i == NSB - 1)),
                     [t_kqTm, t_S0], [t_ps[7]])
        og, t_og = sbt("s_og", [128, 4, 128])
        o_ = og[0:NSB]
        P.op("act", lambda e: e.activation(out=sc_[:, 0:512], in_=ps[7][0:NSB, :], func=AF.Square), [t_ps[7]], [t_scr])
        P.op("dve", lambda e: e.tensor_reduce(out=sv[:, 44:48], in_=sc_[:, 0:512].rearrange("p (h v) -> p h v", h=4),
                                              axis=AX.X, op=ALU.add), [t_scr], [t_ss])
        P.op("pool", lambda e: e.tensor_scalar(out=sv[:, 48:52], in0=sv[:, 44:48], scalar1=1.0 / 128, scalar2=EPS,
                                               op0=ALU.mult, op1=ALU.add), [t_ss], [t_ss])
        P.op("pool", lambda e: e.tensor_tensor(out=sv[:, 48:52], in0=sv[:, 48:52], in1=neghalf[0:NSB, 0:4], op=ALU.pow),
             [t_ss, t_nh], [t_ss])
        P.op("dve", lambda e: e.tensor_tensor(out=o_, in0=ps[7][0:NSB, :].rearrange("p (h v) -> p h v", h=4),
                                              in1=bcs(sv[:, 48:52]), op=ALU.mult), [t_ps[7], t_ss], [t_og])
        P.op("pool", lambda e: e.tensor_tensor(out=o_, in0=o_, in1=ggdn_b[0:NSB].unsqueeze(1).to_broadcast([NSB, 4, 128]),
                                               op=ALU.mult), [t_og, t_ggdn], [t_og])
        P.op("act", lambda e: e.activation(out=sc_[:, 0:512], in_=pj[:, 2628:3140], func=AF.Silu), [t_proj], [t_scr])
        P.op("dve", lambda e: e.tensor_tensor(out=o_.rearrange("p h v -> p (h v)"), in0=o_.rearrange("p h v -> p (h v)"),
                                              in1=sc_[:, 0:512], op=ALU.mult), [t_og, t_scr], [t_og])
        P.op("pool", lambda e: e.tensor_copy(out=pj[:, 1092:1604], in_=o_.rearrange("p h v -> p (h v)")), [t_og], [t_proj])
        P.barrier()
        k.top = S_TOP
        if STOP == -1:
            return
        sample_dsa(proj, t_proj, sso, t_sso)
        sample_tail(proj, t_proj, x_keep, t_xk)

    def sample_dsa(proj, t_proj, sso, t_sso):
        NSB = NS
        pj = proj[0:NSB]
        U32 = mybir.dt.uint32
        selp, t_selp = sbt("s_selp", [128, 8, 128])
        selo, t_selo = sbt("s_selo", [128, NSB, 128])
        P.dma("sp", selp[0:NSB], selpair_d.rearrange("q i p -> i q p"), writes=[t_selp])
        P.dma("sp", selo[0:NSB], selone_d, writes=[t_selo])
        gq_b, t_gqb = bcast_layout("s_gq_b", g_q, 64)
        iota_b, t_iota = bcast_layout("s_iota", iota64_d, 64)
        pt_i, t_pti = sbt("s_pt_i", [128, 64], I32)
        pt_f, t_ptf = sbt("s_pt_f", [128, 64])
        P.dma("sp", pt_i[0:NSB], ptab, writes=[t_pti])
        P.op("dve", lambda e: e.tensor_copy(out=pt_f[0:NSB], in_=pt_i[0:NSB]), [t_pti], [t_ptf])
        ss2, t_ss2 = sbt("s_ss2", [128, 32])
        scr2, t_scr2 = sbt("s_scr2", [128, 512])
        qn, t_qn = sbt("s_qn", [128, 512])
        qiw, t_qiw = sbt("s_qiw", [128, 260])
        sv = ss2[0:NSB]
        P.op("dve", lambda e: e.tensor_tensor(out=scr2[0:NSB], in0=pj[:, 0:512], in1=pj[:, 0:512], op=ALU.mult), [t_proj], [t_scr2])
        P.op("dve", lambda e: e.tensor_reduce(out=sv[:, 0:8], in_=scr2[0:NSB].rearrange("p (h d) -> p h d", h=8), axis=AX.X,
                                              op=ALU.add), [t_scr2], [t_ss2])
        P.op("pool", lambda e: e.tensor_scalar(out=sv[:, 8:16], in0=sv[:, 0:8], scalar1=1.0 / 64, scalar2=EPS, op0=ALU.mult,
                                               op1=ALU.add), [t_ss2], [t_ss2])
        P.op("pool", lambda e: e.tensor_tensor(out=sv[:, 8:16], in0=sv[:, 8:16], in1=neghalf[0:NSB, 0:8], op=ALU.pow),
             [t_ss2, t_nh], [t_ss2])
        P.op("pool", lambda e: e.tensor_scalar(out=sv[:, 8:16], in0=sv[:, 8:16], scalar1=0.125, scalar2=1.0, op0=ALU.mult,
                                               op1=ALU.mult), [t_ss2], [t_ss2])
        q3 = qn[0:NSB].rearrange("p (h d) -> p h d", h=8)
        P.op("dve", lambda e: e.tensor_tensor(out=q3, in0=pj[:, 0:512].rearrange("p (h d) -> p h d", h=8),
                                              in1=sv[:, 8:16].unsqueeze(2).to_broadcast([NSB, 8, 64]), op=ALU.mult),
             [t_proj, t_ss2], [t_qn])
        P.op("pool", lambda e: e.tensor_tensor(out=q3, in0=q3, in1=gq_b[0:NSB].unsqueeze(1).to_broadcast([NSB, 8, 64]),
                                               op=ALU.mult), [t_qn, t_gqb], [t_qn])
        P.op("act", lambda e: e.activation(out=qiw[0:NSB, 0:256], in_=pj[:, 768:1024], func=AF.Copy, scale=0.125), [t_proj], [t_qiw])
        P.op("act", lambda e: e.activation(out=qiw[0:NSB, 256:260], in_=pj[:, 1088:1092], func=AF.Copy, scale=0.5), [t_proj], [t_qiw])
        scores, t_scores = sbt("s_scores", [128, 8200])
        P.op("dve", lambda e: e.tensor_tensor(out=scr2[0:NSB, 0:256].rearrange("p (h d) -> p h d", h=4),
                                              in0=qiw[0:NSB, 0:256].rearrange("p (h d) -> p h d", h=4),
                                              in1=pj[:, 1024:1088].unsqueeze(1).to_broadcast([NSB, 4, 64]), op=ALU.mult),
             [t_qiw, t_proj], [t_scr2])
        P.op("dve", lambda e: e.tensor_reduce(out=sv[:, 16:20], in_=scr2[0:NSB, 0:256].rearrange("p (h d) -> p h d", h=4),
                                              axis=AX.X, op=ALU.add), [t_scr2], [t_ss2])
        P.op("dve", lambda e: e.tensor_scalar(out=sv[:, 16:20], in0=sv[:, 16:20], scalar1=0.0, scalar2=None, op0=ALU.max),
             [t_ss2], [t_ss2])
        P.op("dve", lambda e: e.tensor_tensor(out=sv[:, 16:20], in0=sv[:, 16:20], in1=qiw[0:NSB, 256:260], op=ALU.mult),
             [t_ss2, t_qiw], [t_ss2])
        P.op("dve", lambda e: e.tensor_reduce(out=scores[0:NSB, 8192:8193], in_=sv[:, 16:20], axis=AX.X, op=ALU.add),
             [t_ss2], [t_scores])
        osT, t_osT = sbt("s_osT", [128, NSB, 8], BF16)
        K_TOP = k.top
        k.K_TOP = K_TOP
        kid = [sbt("s_kid%d" % i, [128, 8192]) for i in range(2)]
        prod, t_prod = sbt("s_prod", [128, 8192])
        ptc = [sbt("s_ptc%d" % i, [128, 1], I32) for i in range(2)]
        qrep, t_qrep = sbt("s_qrep", [128, 260])
        zz, t_zz = sbt("s_zz", [128, 128])
        sc1, t_sc1 = sbt("s_sc1", [128, 128])
        sc2 = [sbt("s_sc2_%d" % i, [128, 128]) for i in range(2)]
        kidx_pages = cache_kidx_d

        def pair(q):
            kd, tkd = kid[q % 2]
            pc, tpc = ptc[q % 2]
            P.dma("sp", pc, ptab[2 * q:2 * q + 2, :].rearrange("s (j o) -> (s j) o", o=1), writes=[tpc])
            P.dma("pool", kd, kidx_pages, reads=[tpc], writes=[tkd],
                  indirect=bass.IndirectOffsetOnAxis(ap=pc, axis=0))
            P.op("pe", lambda e: e.matmul(ps[1][:, 0:260], lhsT=selp[0:NSB, q, :], rhs=qiw[0:NSB, :], start=True, stop=True),
                 [t_selp, t_qiw], [t_ps[1]])
            P.op("act", lambda e: e.activation(out=qrep, in_=ps[1][:, 0:260], func=AF.Copy), [t_ps[1]], [t_qrep])
            so_, tso = sc2[q % 2]
            for h in range(4):
                P.op("pool", lambda e, h=h: e.tensor_tensor(
                    out=prod.rearrange("p (o d) -> p o d", d=64), in0=kd.rearrange("p (o d) -> p o d", d=64),
                    in1=qrep[:, h * 64:(h + 1) * 64].unsqueeze(1).to_broadcast([128, 128, 64]), op=ALU.mult),
                    [tkd, t_qrep], [t_prod])
                P.op("dve", lambda e: e.tensor_reduce(out=zz, in_=prod.rearrange("p (o d) -> p o d", d=64), axis=AX.X,
                                                      op=ALU.add), [t_prod], [t_zz])
                if h == 0:
                    P.op("dve", lambda e: e.tensor_scalar(out=so_, in0=zz, scalar1=0.0, scalar2=qrep[:, 256:257],
                                                          op0=ALU.max, op1=ALU.mult), [t_zz, t_qrep], [tso])
                else:
                    P.op("dve", lambda e, h=h: e.tensor_scalar(out=sc1, in0=zz, scalar1=0.0, scalar2=qrep[:, 256 + h:257 + h],
                                                               op0=ALU.max, op1=ALU.mult), [t_zz, t_qrep], [t_sc1])
                    P.op("dve", lambda e: e.tensor_tensor(out=so_, in0=so_, in1=sc1, op=ALU.add), [tso, t_sc1], [tso])
            for s2 in range(2):
                r = 2 * q + s2
                P.dma("sp", scores[r:r + 1, 0:8192].rearrange("p (j o) -> p j o", o=128), so_[64 * s2:64 * s2 + 64, :],
                      reads=[tso], writes=[t_scores])

        for q in range(NSB // 2):
            pair(q)
        P.barrier()
        k.top = K_TOP
        mx, t_mx = sbt("s_mx", [128, 256])
        ix, t_ix = sbt("s_ix", [128, 256], U32)
        TK_TOP = k.top
        W, t_W = sbt("s_W", [128, 8200])
        Wv = W[0:NSB, 0:8193]
        P.op("pool", lambda e: e.tensor_copy(out=Wv, in_=scores[0:NSB, 0:8193]), [t_scores], [t_W])
        for r in range(32):
            P.op("dve", lambda e, r=r: e.max(out=mx[0:NSB, 8 * r:8 * r + 8], in_=Wv), [t_W], [t_mx])
            P.op("dve", lambda e, r=r: e.max_index(out=ix[0:NSB, 8 * r:8 * r + 8], in_max=mx[0:NSB, 8 * r:8 * r + 8],
                                                   in_values=Wv), [t_W, t_mx], [t_ix])
            P.op("dve", lambda e, r=r: e.match_replace(out=Wv, in_to_replace=mx[0:NSB, 8 * r:8 * r + 8], in_values=Wv,
                                                       imm_value=-1e30), [t_W, t_mx], [t_W])
        P.barrier()
        k.top = TK_TOP
        ixf, t_ixf = sbt("s_ixf", [128, 256])
        pgu, t_pgu = sbt("s_pgu", [128, 256], U32)
        pgf, t_pgf = sbt("s_pgf", [128, 256])
        offf, t_offf = sbt("s_offf", [128, 256])
        eq, t_eq = sbt("s_eq", [128, 256, 64])
        phys, t_phys = sbt("s_phys", [128, 256])
        isf, t_isf = sbt("s_isf", [128, 256])
        n_ = lambda a: a[0:NSB]
        P.op("dve", lambda e: e.tensor_copy(out=n_(ixf), in_=n_(ix)), [t_ix], [t_ixf])
        P.op("dve", lambda e: e.tensor_scalar(out=n_(pgu), in0=n_(ix), scalar1=7, scalar2=None, op0=ALU.logical_shift_right),
             [t_ix], [t_pgu])
        P.op("dve", lambda e: e.tensor_copy(out=n_(pgf), in_=n_(pgu)), [t_pgu], [t_pgf])
        P.op("dve", lambda e: e.scalar_tensor_tensor(out=n_(offf), in0=n_(pgf), scalar=-128.0, in1=n_(ixf), op0=ALU.mult,
                                                     op1=ALU.add), [t_pgf, t_ixf], [t_offf])
        P.op("dve", lambda e: e.tensor_tensor(out=n_(eq), in0=n_(pgf).unsqueeze(2).to_broadcast([NSB, 256, 64]),
                                              in1=n_(iota_b).unsqueeze(1).to_broadcast([NSB, 256, 64]), op=ALU.is_equal),
             [t_pgf, t_iota], [t_eq])
        P.op("dve", lambda e: e.tensor_tensor(out=n_(eq), in0=n_(eq), in1=n_(pt_f).unsqueeze(1).to_broadcast([NSB, 256, 64]),
                                              op=ALU.mult), [t_eq, t_ptf], [t_eq])
        P.op("dve", lambda e: e.tensor_reduce(out=n_(phys), in_=n_(eq), axis=AX.X, op=ALU.add), [t_eq], [t_phys])
        P.op("dve", lambda e: e.scalar_tensor_tensor(out=n_(phys), in0=n_(phys), scalar=128.0, in1=n_(offf), op0=ALU.mult,
                                                     op1=ALU.add), [t_phys, t_offf], [t_phys])
        P.op("dve", lambda e: e.tensor_scalar(out=n_(isf), in0=n_(ixf), scalar1=8191.5, scalar2=None, op0=ALU.is_ge),
             [t_ixf], [t_isf])
        physT, t_physT = sbt("s_physT", [128, 2, NSB], I32)
        isT, t_isT = sbt("s_isT", [128, 2, NSB])
        for b in range(2):
            P.op("pe", lambda e, b=b: e.transpose(out=ps[1][:, b * NSB:(b + 1) * NSB], in_=phys[0:NSB, b * 128:(b + 1) * 128],
                                                  identity=identf[0:NSB, 0:NSB]), [t_phys, t_identf], [t_ps[1]])
            P.op("pe", lambda e, b=b: e.transpose(out=ps[2][:, b * NSB:(b + 1) * NSB], in_=isf[0:NSB, b * 128:(b + 1) * 128],
                                                  identity=identf[0:NSB, 0:NSB]), [t_isf, t_identf], [t_ps[2]])
        P.op("dve", lambda e: e.tensor_copy(out=physT.rearrange("p b i -> p (b i)"), in_=ps[1][:, 0:2 * NSB]), [t_ps[1]], [t_physT])
        P.op("act", lambda e: e.activation(out=isT.rearrange("p b i -> p (b i)"), in_=ps[2][:, 0:2 * NSB], func=AF.Copy),
             [t_ps[2]], [t_isT])
        Kg = [sbt("s_Kg%d" % i, [128, 2, 128]) for i in range(2)]
        Vg = [sbt("s_Vg%d" % i, [128, 2, 128]) for i in range(2)]
        kvrep, t_kvrep = sbt("s_kvrep", [128, 256])
        dif, t_dif = sbt("s_dif", [128, 128])
        prd, t_prd = sbt("s_prd", [128, 512])
        lg, t_lg = sbt("s_lg", [128, 2, 8])
        rcp, t_rcp = sbt("s_rcp", [128, NSB * 8])

        def att(i):
            kg, tkg = Kg[i % 2]
            vg, tvg = Vg[i % 2]
            for b in range(2):
                P.dma("pool", kg[:, b, :], cache_k_d, reads=[t_physT], writes=[tkg],
                      indirect=bass.IndirectOffsetOnAxis(ap=physT[:, b, i:i + 1], axis=0))
                P.dma("pool", vg[:, b, :], cache_v_d, reads=[t_physT], writes=[tvg],
                      indirect=bass.IndirectOffsetOnAxis(ap=physT[:, b, i:i + 1], axis=0))
            P.op("pe", lambda e: e.matmul(ps[3], lhsT=selo[0:NSB, i, :], rhs=qn[0:NSB, :], start=True, stop=True),
                 [t_selo, t_qn], [t_ps[3]])
            P.op("pe", lambda e: e.matmul(ps[4][:, 0:256], lhsT=selo[0:NSB, i, :], rhs=sso[0:NSB, 0:256], start=True, stop=True),
                 [t_selo, t_sso], [t_ps[4]])
            P.op("act", lambda e: e.activation(out=kvrep, in_=ps[4][:, 0:256], func=AF.Copy), [t_ps[4]], [t_kvrep])
            for b in range(2):
                for (t_, tt_, c0) in ((kg, tkg, 0), (vg, tvg, 128)):
                    P.op("dve", lambda e, t_=t_, c0=c0, b=b: e.tensor_tensor(out=dif, in0=kvrep[:, c0:c0 + 128], in1=t_[:, b, :],
                                                                             op=ALU.subtract), [t_kvrep, tt_], [t_dif])
                    P.op("dve", lambda e, t_=t_, b=b: e.scalar_tensor_tensor(out=t_[:, b, :], in0=dif, scalar=isT[:, b, i:i + 1],
                                                                             in1=t_[:, b, :], op0=ALU.mult, op1=ALU.add),
                         [t_dif, t_isT, tt_], [tt_])
                P.op("dve", lambda e, b=b: e.tensor_tensor(
                    out=prd.rearrange("p (g r d) -> p g r d", g=2, r=4),
                    in0=ps[3].rearrange("p (g r d) -> p g r d", g=2, r=4),
                    in1=kg[:, b, :].rearrange("p (g d) -> p g d", g=2).unsqueeze(2).to_broadcast([128, 2, 4, 64]),
                    op=ALU.mult), [t_ps[3], tkg], [t_prd])
                P.op("dve", lambda e, b=b: e.tensor_reduce(out=lg[:, b, :], in_=prd.rearrange("p (h d) -> p h d", h=8),
                                                           axis=AX.X, op=ALU.add), [t_prd], [t_lg])
            P.op("act", lambda e: e.activation(out=lg, in_=lg, func=AF.Exp), [t_lg], [t_lg])
            for g in range(2):
                for b in range(2):
                    P.op("pe", lambda e, g=g, b=b: e.matmul(ps[5][0:64, i * 8 + g * 4:i * 8 + g * 4 + 4],
                                                            lhsT=vg[:, b, g * 64:(g + 1) * 64], rhs=lg[:, b, g * 4:(g + 1) * 4],
                                                            start=(b == 0), stop=(b == 1)), [tvg, t_lg], [t_ps[5]])
                for b in range(2):
                    P.op("pe", lambda e, g=g, b=b: e.matmul(ps[6][0:64, i * 8 + g * 4:i * 8 + g * 4 + 4],
                                                            lhsT=onesf[:, 0:64], rhs=lg[:, b, g * 4:(g + 1) * 4],
                                                            start=(b == 0), stop=(b == 1)), [t_onesf, t_lg], [t_ps[6]])

        for i in range(NSB):
            att(i)
        P.op("dve", lambda e: e.reciprocal(out=rcp[0:64], in_=ps[6][0:64, 0:NSB * 8]), [t_ps[6]], [t_rcp])
        P.op("dve", lambda e: e.tensor_tensor(out=osT[0:64].rearrange("p i h -> p (i h)"), in0=ps[5][0:64, 0:NSB * 8],
                                              in1=rcp[0:64], op=ALU.mult), [t_ps[5], t_rcp], [t_osT])
        k.osT = (osT, t_osT)
        k.selo = (selo, t_selo)
        k.S2_TOP = k.top
        P.barrier()

    def sample_tail(proj, t_proj, x_keep, t_xk):
        NSB = NS
        pj = proj[0:NSB]
        osT, t_osT = k.osT
        selo, t_selo = k.selo
        k.top = k.K_TOP
        alloc_stage(1024)
        Wo_s, t_Wos = sbt("t_Wo_s", [128, 8, 1024], BF16)
        Wo_g, t_Wog = sbt("t_Wo_g", [128, 4, 1024], BF16)
        Wmq, t_Wmq = sbt("t_Wmq", [128, 8, 256], BF16)
        Wmo, t_Wmo = sbt("t_Wmo", [128, 4, 1024], BF16)
        for h in range(8):
            i = k.wl % k.nst
            k.wl += 1
            st = stage[i][0:64, 0:1024]
            P.dma("sp", st, w_out[64 * h:64 * h + 64, :], writes=[t_stage[i]])
            P.op("act", lambda e, st=st, h=h: e.activation(out=Wo_s[0:64, h, :], in_=st, func=AF.Copy), [t_stage[i]], [t_Wos])
        for h in range(4):
            load_rows(Wo_g[:, h, :], t_Wog, w_out[512 + 128 * h:512 + 128 * h + 128, :], 1024, None, None)
        load_weight(Wmq, t_Wmq, w_mq, 0, 256, gX, t_gX)
        for h in range(4):
            i = k.wl % k.nst
            k.wl += 1
            st = stage[i][0:64, 0:1024]
            P.dma("sp", st, w_mo[64 * h:64 * h + 64, :], writes=[t_stage[i]])
            P.op("act", lambda e, st=st, h=h: e.activation(out=Wmo[0:64, h, :], in_=st, func=AF.Copy), [t_stage[i]], [t_Wmo])
        gmq_b, t_gmqb = bcast_layout("t_gmq_b", g_mq, 64)
        ogT, t_ogT = sbt("t_ogT", [128, 4, NSB], BF16)
        for h in range(4):
            P.op("pe", lambda e, h=h: e.transpose(out=ps[1][:, h * NSB:(h + 1) * NSB], in_=pj[:, 1092 + h * 128:1092 + (h + 1) * 128],
                                                  identity=identf[0:NSB, 0:NSB]), [t_proj, t_identf], [t_ps[1]])
        P.op("act", lambda e: e.activation(out=ogT.rearrange("p h i -> p (h i)"), in_=ps[1][:, 0:4 * NSB], func=AF.Copy),
             [t_ps[1]], [t_ogT])
        x1, t_x1 = sbt("t_x1", [128, D])
        P.op("pool", lambda e: e.memset(x1, 0.0), [], [t_x1])
        for c in range(2):
            for h in range(8):
                P.op("pe", lambda e, h=h, c=c: e.matmul(ps[2 + c][0:NSB, :], lhsT=osT[0:64, :, h], rhs=Wo_s[0:64, h, c * 512:(c + 1) * 512],
                                                        start=(h == 0), stop=False), [t_osT, t_Wos], [t_ps[2 + c]])
            for h in range(4):
                P.op("pe", lambda e, h=h, c=c: e.matmul(ps[2 + c][0:NSB, :], lhsT=ogT[:, h, :], rhs=Wo_g[:, h, c * 512:(c + 1) * 512],
                                                        start=False, stop=(h == 3)), [t_ogT, t_Wog], [t_ps[2 + c]])
            P.op("dve", lambda e, c=c: e.tensor_tensor(out=x1[0:NSB, c * 512:(c + 1) * 512], in0=ps[2 + c][0:NSB, :],
                                                       in1=x_keep[0:NSB, c * 512:(c + 1) * 512], op=ALU.add),
                 [t_ps[2 + c], t_xk], [t_x1])
        hT2, t_hT2 = sbt("t_hT2", [128, 8, 128], BF16)
        norm_T(x1, t_x1, 0, hT2, t_hT2, 0, 0)
        for kc in range(8):
            P.op("pe", lambda e, kc=kc: e.matmul(ps[4][0:NSB, 0:256], lhsT=hT2[:, kc, 0:NSB], rhs=Wmq[:, kc, :],
                                                 start=(kc == 0), stop=(kc == 7)), [t_hT2, t_Wmq], [t_ps[4]])
        qm, t_qm = sbt("t_qm", [128, 256])
        sq2, t_sq2 = sbt("t_sq2", [128, 256])
        st2, t_st2 = sbt("t_st2", [128, 16])
        P.op("act", lambda e: e.activation(out=sq2[0:NSB], in_=ps[4][0:NSB, 0:256], func=AF.Square), [t_ps[4]], [t_sq2])
        P.op("dve", lambda e: e.tensor_reduce(out=st2[0:NSB, 0:4], in_=sq2[0:NSB].rearrange("p (h d) -> p h d", h=4), axis=AX.X,
                                              op=ALU.add), [t_sq2], [t_st2])
        P.op("pool", lambda e: e.tensor_scalar(out=st2[0:NSB, 4:8], in0=st2[0:NSB, 0:4], scalar1=1.0 / 64, scalar2=EPS,
                                               op0=ALU.mult, op1=ALU.add), [t_st2], [t_st2])
        P.op("pool", lambda e: e.tensor_tensor(out=st2[0:NSB, 4:8], in0=st2[0:NSB, 4:8], in1=neghalf[0:NSB, 0:4], op=ALU.pow),
             [t_st2, t_nh], [t_st2])
        P.op("pool", lambda e: e.tensor_scalar(out=st2[0:NSB, 4:8], in0=st2[0:NSB, 4:8], scalar1=0.125, scalar2=1.0,
                                               op0=ALU.mult, op1=ALU.mult), [t_st2], [t_st2])
        qm3 = qm[0:NSB].rearrange("p (h d) -> p h d", h=4)
        P.op("dve", lambda e: e.tensor_tensor(out=qm3, in0=ps[4][0:NSB, 0:256].rearrange("p (h d) -> p h d", h=4),
                                              in1=st2[0:NSB, 4:8].unsqueeze(2).to_broadcast([NSB, 4, 64]), op=ALU.mult),
             [t_ps[4], t_st2], [t_qm])
        P.op("pool", lambda e: e.tensor_tensor(out=qm3, in0=qm3, in1=gmq_b[0:NSB].unsqueeze(1).to_broadcast([NSB, 4, 64]),
                                               op=ALU.mult), [t_qm, t_gmqb], [t_qm])
        mkt = [sbt("t_mk%d" % i, [128, 2, 256]) for i in range(2)]
        mvt = [sbt("t_mv%d" % i, [128, 2, 256]) for i in range(2)]
        prm, t_prm = sbt("t_prm", [128, 256])
        lgm, t_lgm = sbt("t_lgm", [128, 2, 4])
        omT, t_omT = sbt("t_omT", [128, NSB, 4], BF16)
        rcm, t_rcm = sbt("t_rcm", [128, NSB * 4])

        def xatt(i):
            mk_, tmk = mkt[i % 2]
            mv_, tmv = mvt[i % 2]
            P.dma("sp", mk_, cmk_d[i * 256:(i + 1) * 256, :].rearrange("(t p) c -> p t c", p=128), writes=[tmk])
            P.dma("sp", mv_, cmv_d[i * 256:(i + 1) * 256, :].rearrange("(t p) c -> p t c", p=128), writes=[tmv])
            P.op("pe", lambda e: e.matmul(ps[5][:, 0:256], lhsT=selo[0:NSB, i, :], rhs=qm[0:NSB, :], start=True, stop=True),
                 [t_selo, t_qm], [t_ps[5]])
            for mt in range(2):
                P.op("dve", lambda e, mt=mt: e.tensor_tensor(out=prm, in0=ps[5][:, 0:256], in1=mk_[:, mt, :], op=ALU.mult),
                     [t_ps[5], tmk], [t_prm])
                P.op("dve", lambda e, mt=mt: e.tensor_reduce(out=lgm[:, mt, :], in_=prm.rearrange("p (h d) -> p h d", h=4),
                                                             axis=AX.X, op=ALU.add), [t_prm], [t_lgm])
            P.op("act", lambda e: e.activation(out=lgm, in_=lgm, func=AF.Exp), [t_lgm], [t_lgm])
            for h in range(4):
                for mt in range(2):
                    P.op("pe", lambda e, h=h, mt=mt: e.matmul(ps[6][0:64, i * 4 + h:i * 4 + h + 1], lhsT=mv_[:, mt, h * 64:(h + 1) * 64],
                                                              rhs=lgm[:, mt, h:h + 1], start=(mt == 0), stop=(mt == 1)),
                         [tmv, t_lgm], [t_ps[6]])
                for mt in range(2):
                    P.op("pe", lambda e, h=h, mt=mt: e.matmul(ps[7][0:64, i * 4 + h:i * 4 + h + 1], lhsT=onesf[:, 0:64],
                                                              rhs=lgm[:, mt, h:h + 1], start=(mt == 0), stop=(mt == 1)),
                         [t_onesf, t_lgm], [t_ps[7]])

        for i in range(NSB):
            xatt(i)
        P.op("dve", lambda e: e.reciprocal(out=rcm[0:64], in_=ps[7][0:64, 0:NSB * 4]), [t_ps[7]], [t_rcm])
        P.op("dve", lambda e: e.tensor_tensor(out=omT[0:64].rearrange("p i h -> p (i h)"), in0=ps[6][0:64, 0:NSB * 4],
                                              in1=rcm[0:64], op=ALU.mult), [t_ps[6], t_rcm], [t_omT])
        for c in range(2):
            for h in range(4):
                P.op("pe", lambda e, h=h, c=c: e.matmul(ps[2 + c][0:NSB, :], lhsT=omT[0:64, :, h], rhs=Wmo[0:64, h, c * 512:(c + 1) * 512],
                                                        start=(h == 0), stop=(h == 3)), [t_omT, t_Wmo], [t_ps[2 + c]])
            P.op("dve", lambda e, c=c: e.tensor_tensor(out=x1[0:NSB, c * 512:(c + 1) * 512], in0=ps[2 + c][0:NSB, :],
                                                       in1=x1[0:NSB, c * 512:(c + 1) * 512], op=ALU.add),
                 [t_ps[2 + c], t_x1], [t_x1])
        P.op("pool", lambda e: e.tensor_copy(out=x_keep[0:NSB, :], in_=x1[0:NSB, :]), [t_x1], [t_xk])
        hT3, t_hT3 = sbt("t_hT3", [128, 8, 128], BF16)
        norm_T(x1, t_x1, 1, hT3, t_hT3, 0, 0)
        P.barrier()
        k.top = PERSIST_TOP
        xk2, t_xk2 = sbt("t_xk2", [128, D])
        hT4, t_hT4 = sbt("t_hT4", [128, 8, NSB], BF16)
        P.op("pool", lambda e: e.tensor_copy(out=xk2[0:NSB, :], in_=x_keep[0:NSB, :]), [t_xk], [t_xk2])
        P.op("pool", lambda e: e.tensor_copy(out=hT4, in_=hT3[:, :, 0:NSB]), [t_hT3], [t_hT4])
        P.barrier()
        alloc_stage(2816, 3)
        Wg, t_Wg = sbt("t_Wg", [128, 8, 2816], BF16)
        Wu, t_Wu = sbt("t_Wu", [128, 8, 2816], BF16)
        Wd, t_Wd = sbt("t_Wd", [128, 22, 1024], BF16)
        load_weight(Wg, t_Wg, w_gate, 0, 2816, gF, t_gF)
        load_weight(Wu, t_Wu, w_up, 0, 2816, gF, t_gF)
        for f in range(22):
            load_rows(Wd[:, f, :], t_Wd, w_down[128 * f:128 * f + 128, :], 1024, None, None)
        hf, t_hf = sbt("t_hf", [128, 2816])
        sgs, t_sgs = sbt("t_sgs", [128, 512])
        hfT, t_hfT = sbt("t_hfT", [128, 22, NSB], BF16)
        for c in range(6):
            c0 = c * 512
            n = min(512, 2816 - c0)
            for kc in range(8):
                P.op("pe", lambda e, kc=kc, c0=c0, n=n: e.matmul(ps[1][0:NSB, 0:n], lhsT=hT4[:, kc, :], rhs=Wg[:, kc, c0:c0 + n],
                                                                 start=(kc == 0), stop=(kc == 7)), [t_hT4, t_Wg], [t_ps[1]])
            for kc in range(8):
                P.op("pe", lambda e, kc=kc, c0=c0, n=n: e.matmul(ps[2][0:NSB, 0:n], lhsT=hT4[:, kc, :], rhs=Wu[:, kc, c0:c0 + n],
                                                                 start=(kc == 0), stop=(kc == 7)), [t_hT4, t_Wu], [t_ps[2]])
            P.op("act", lambda e, n=n: e.activation(out=sgs[0:NSB, 0:n], in_=ps[1][0:NSB, 0:n], func=AF.Silu), [t_ps[1]], [t_sgs])
            P.op("dve", lambda e, c0=c0, n=n: e.tensor_tensor(out=hf[0:NSB, c0:c0 + n], in0=sgs[0:NSB, 0:n], in1=ps[2][0:NSB, 0:n],
                                                              op=ALU.mult), [t_sgs, t_ps[2]], [t_hf])
        for f in range(22):
            b = 3 + (f % 2)
            P.op("pe", lambda e, f=f, b=b: e.transpose(out=ps[b][:, 0:NSB], in_=hf[0:NSB, f * 128:(f + 1) * 128],
                                                       identity=identf[0:NSB, 0:NSB]), [t_hf, t_identf], [t_ps[b]])
            P.op("act", lambda e, f=f, b=b: e.activation(out=hfT[:, f, :], in_=ps[b][:, 0:NSB], func=AF.Copy), [t_ps[b]], [t_hfT])
        for c in range(2):
            for f in range(22):
                P.op("pe", lambda e, f=f, c=c: e.matmul(ps[5 + c][0:NSB, :], lhsT=hfT[:, f, :], rhs=Wd[:, f, c * 512:(c + 1) * 512],
                                                        start=(f == 0), stop=(f == 21)), [t_hfT, t_Wd], [t_ps[5 + c]])
            P.op("dve", lambda e, c=c: e.tensor_tensor(out=xk2[0:NSB, c * 512:(c + 1) * 512], in0=ps[5 + c][0:NSB, :],
                                                       in1=xk2[0:NSB, c * 512:(c + 1) * 512], op=ALU.add),
                 [t_ps[5 + c], t_xk2], [t_xk2])
        P.dma("sp", y_s[:, :], xk2[0:NSB, :], reads=[t_xk2])

    if STOP >= 0:
        for sq in range(NSEQ):
            prompt_seq(sq)
    if STOP < 0 or STOP >= 99:
        sample_group()

    P.finish()
    P.emit()
    return nc


_CACHE = {}


def _get_nc(nseq, stop, npool=10240):
    key = (nseq, stop, npool)
    if key not in _CACHE:
        _CACHE[key] = build(nseq, STOP=stop, NPOOL=npool)
    return _CACHE[key]


def kernel(x_prompt, x_sample, mem_prompt, cache_k, cache_v, cache_kidx, page_table,
           state_conv, state_ssm, cache_mem_k, cache_mem_v,
           attn_norm_g, w_in, q_norm_g, k_norm_g, conv_w, a_log, dt_bias, gdn_norm_g, w_out,
           xattn_norm_g, mem_norm_g, w_mq, w_mk, w_mv, mq_norm_g, mk_norm_g, w_mo,
           ffn_norm_g, w_gate, w_up, w_down, _ncores=NCORES, _stop=99):
    B = x_prompt.shape[0]
    nseq = B // _ncores
    NS = x_sample.shape[0] // _ncores
    nc = _get_nc(nseq, _stop, cache_k.shape[1])
    f = lambda a: np.ascontiguousarray(np.asarray(a, dtype=np.float32))
    ii = np.arange(128)
    consts = {
        "ident": np.eye(128, dtype=np.float32),
        "trile": (ii[:, None] <= ii[None, :]).astype(np.float32),
        "sgt": (ii[:, None] > ii[None, :]).astype(np.float32),
        "pow2": (2.0 ** -np.arange(32)).astype(np.float32),
        "eye16": np.eye(16, dtype=np.float32).reshape(256),
        "selpair": np.stack([(np.arange(16)[:, None] == (2 * q_ + np.arange(128)[None, :] // 64)).astype(np.float32)
                             for q_ in range(8)]),
        "selone": np.stack([np.repeat((np.arange(16) == i_)[:, None], 128, axis=1).astype(np.float32)
                            for i_ in range(16)], axis=1),
        "iota64": np.arange(64, dtype=np.float32),
    }
    shared = {
        "attn_norm_g": f(attn_norm_g[0]), "w_in": f(w_in[0]),
        "q_norm_g": f(q_norm_g[0]), "k_norm_g": f(k_norm_g[0]),
        "conv_w": f(conv_w[0]), "a_log": f(a_log[0]), "dt_bias": f(dt_bias[0]),
        "gdn_norm_g": f(gdn_norm_g[0]), "w_out": f(w_out[0]), "xattn_norm_g": f(xattn_norm_g[0]),
        "mem_norm_g": f(mem_norm_g[0]), "w_mq": f(w_mq[0]), "w_mk": f(w_mk[0]), "w_mv": f(w_mv[0]),
        "mq_norm_g": f(mq_norm_g[0]), "mk_norm_g": f(mk_norm_g[0]), "w_mo": f(w_mo[0]),
        "ffn_norm_g": f(ffn_norm_g[0]), "w_gate": f(w_gate[0]), "w_up": f(w_up[0]), "w_down": f(w_down[0]),
    }
    npool = cache_k.shape[1]
    ck_k = f(cache_k[0]).reshape(npool * 128, 128)
    ck_v = f(cache_v[0]).reshape(npool * 128, 128)
    ck_idx = f(cache_kidx[0]).reshape(npool, 8192)
    in_maps = []
    for c in range(_ncores):
        m = {
            "xp": f(x_prompt[c * nseq:(c + 1) * nseq]).reshape(nseq * SEQ, D),
            "memp": f(mem_prompt[c * nseq:(c + 1) * nseq]).reshape(nseq * MEM, D),
            "xs": f(x_sample[c * NS:(c + 1) * NS]).reshape(NS, D),
            "st_conv": f(state_conv[0, c * NS:(c + 1) * NS]).reshape(NS * 3, 1536),
            "ssm_in": f(state_ssm[0, c * NS:(c + 1) * NS]).reshape(NS * 512, 128),
            "ptab": np.ascontiguousarray(np.asarray(page_table[c * NS:(c + 1) * NS], dtype=np.int32)),
            "cmk": f(cache_mem_k[0, c * NS:(c + 1) * NS]).reshape(NS * 256, 256),
            "cmv": f(cache_mem_v[0, c * NS:(c + 1) * NS]).reshape(NS * 256, 256),
            "cache_kidx": ck_idx, "cache_k": ck_k, "cache_v": ck_v,
        }
        m.update(consts)
        m.update(shared)
        in_maps.append(m)
    res = run_bass_kernel_spmd(nc, in_maps, core_ids=list(range(_ncores))).results
    cat = lambda name: np.concatenate([r[name] for r in res], axis=0)
    SB = x_sample.shape[0]
    outs = (
        cat("y_p").reshape(B, SEQ, D),
        cat("y_s").reshape(SB, 1, D),
        cat("k_p").reshape(1, B, SEQ, 2, 64),
        cat("v_p").reshape(1, B, SEQ, 2, 64),
        cat("kidx_p").reshape(1, B, SEQ, 64),
        cat("conv_p").reshape(1, B, 3, 1536),
        cat("ssm_p").reshape(1, B, 4, 128, 128),
        cat("memk_p").reshape(1, B, MEM, 4, 64),
        cat("memv_p").reshape(1, B, MEM, 4, 64),
        cat("k_s").reshape(1, SB, 1, 2, 64),
        cat("v_s").reshape(1, SB, 1, 2, 64),
        cat("kidx_s").reshape(1, SB, 1, 64),
        cat("conv_s").reshape(1, SB, 3, 1536),
        cat("ssm_s").reshape(1, SB, 4, 128, 128),
    )
    return outs
```
